# Optimizing a Trainium2 kernel written in Bass

```python
import math
import jax, jax.numpy as jnp
from jax import lax
import numpy as np

D_MODEL = 1024
BATCH = 8
SEQ = 4096
DEPTH = 4

GRID_W = 64
CTX_LEN = 256

N_MIXERS = 3
N_ATTN_LAYERS = (DEPTH + 2) // 3
N_CONV_LAYERS = (DEPTH + 1) // 3
N_HYENA_LAYERS = DEPTH // 3

HEAD_DIM = 128
N_HEADS = D_MODEL // HEAD_DIM
N_KV_HEADS = N_HEADS // 4
GQA_GROUP = N_HEADS // N_KV_HEADS
Q_BLOCK = 128
ROPE_THETA = 10000.0
ROPE_PAIRS_PER_AXIS = HEAD_DIM // 4

CONV_WIDTH = 31

HYENA_ORDER = 2
SHORT_CONV_WIDTH = 3
FILTER_BANDS = 8
FILTER_EMB = 1 + 2 * FILTER_BANDS
FILTER_HIDDEN = 64
DECAY_TARGET = 1e-2
FAST_DECAY_PCT = 0.3
SLOW_DECAY_PCT = 1.5

N_GROUPS = 4
EXPERTS_PER_GROUP = 8
N_EXPERTS = N_GROUPS * EXPERTS_PER_GROUP
TOP_K = 2
D_EXPERT = D_MODEL // 4

EPS = 1e-6

kernel_name = "hybrid_attn_conformer_hyena_hmoe_prefix_dit"


def rmsnorm(x, g):
    xf = x.astype(jnp.float32)
    y = xf * lax.rsqrt(jnp.mean(xf * xf, axis=-1, keepdims=True) + EPS)
    return (y * g.astype(jnp.float32)).astype(x.dtype)


def layernorm(x, g, b):
    xf = x.astype(jnp.float32)
    mu = jnp.mean(xf, axis=-1, keepdims=True)
    xc = xf - mu
    y = xc * lax.rsqrt(jnp.mean(xc * xc, axis=-1, keepdims=True) + EPS)
    return (y * g.astype(jnp.float32) + b.astype(jnp.float32)).astype(x.dtype)


def modulated_norm(x, g, shift, scale):
    return rmsnorm(x, g) * (1.0 + scale) + shift


def depthwise_conv(x, w, b):
    k = w.shape[0]
    y = lax.conv_general_dilated(
        x, w[:, None, :], window_strides=(1,), padding=[((k - 1) // 2, k // 2)],
        dimension_numbers=("NWC", "WIO", "NWC"), feature_group_count=x.shape[-1])
    return y + b


def axial_angles(rows):
    row = jnp.repeat(jnp.arange(rows), GRID_W).astype(jnp.float32)
    col = jnp.tile(jnp.arange(GRID_W), rows).astype(jnp.float32)
    inv_freq = ROPE_THETA ** (-jnp.arange(ROPE_PAIRS_PER_AXIS, dtype=jnp.float32) / ROPE_PAIRS_PER_AXIS)
    return row[:, None] * inv_freq, col[:, None] * inv_freq


def rope_rotate(x, ang):
    cos = jnp.cos(ang)[:, None, :].astype(x.dtype)
    sin = jnp.sin(ang)[:, None, :].astype(x.dtype)
    x1, x2 = jnp.split(x, 2, axis=-1)
    return jnp.concatenate([x1 * cos - x2 * sin, x1 * sin + x2 * cos], axis=-1)


def axial_rope(x, ang_row, ang_col):
    xr, xc = jnp.split(x, 2, axis=-1)
    return jnp.concatenate([rope_rotate(xr, ang_row), rope_rotate(xc, ang_col)], axis=-1)


def attend(q, k, v):
    s = jnp.einsum("bkgqd,bksd->bkgqs", q, k, preferred_element_type=jnp.float32) * (HEAD_DIM ** -0.5)
    p = jax.nn.softmax(s, axis=-1).astype(v.dtype)
    return jnp.einsum("bkgqs,bksd->bkgqd", p, v)


def attention_mixer(h_ctx, h_lat, ang_row, ang_col, w_q, w_kv, q_gain, k_gain, w_o, with_ctx):
    B, S, D = h_lat.shape

    def q_proj(h):
        q = rmsnorm((h @ w_q).reshape(B, h.shape[1], N_HEADS, HEAD_DIM), q_gain)
        return q

    def kv_proj(h):
        kv = (h @ w_kv).reshape(B, h.shape[1], 2, N_KV_HEADS, HEAD_DIM)
        return rmsnorm(kv[:, :, 0], k_gain), kv[:, :, 1]

    def q_heads(q):
        return q.reshape(B, q.shape[1], N_KV_HEADS, GQA_GROUP, HEAD_DIM).transpose(0, 2, 3, 1, 4)

    def kv_heads(t):
        return t.transpose(0, 2, 1, 3)

    k_c, v_c = kv_proj(h_ctx)
    k_l, v_l = kv_proj(h_lat)
    k_l = axial_rope(k_l, ang_row, ang_col)
    q_l = axial_rope(q_proj(h_lat), ang_row, ang_col)

    k_c, v_c = kv_heads(k_c), kv_heads(v_c)
    k_all = jnp.concatenate([k_c, kv_heads(k_l)], axis=2)
    v_all = jnp.concatenate([v_c, kv_heads(v_l)], axis=2)

    n_blocks = S // Q_BLOCK
    qb = q_heads(q_l).reshape(B, N_KV_HEADS, GQA_GROUP, n_blocks, Q_BLOCK, HEAD_DIM)
    qb = qb.transpose(3, 0, 1, 2, 4, 5)
    ob = lax.map(lambda qblk: attend(qblk, k_all, v_all), qb)
    o_l = ob.transpose(1, 0, 4, 2, 3, 5).reshape(B, S, D) @ w_o

    o_c = None
    if with_ctx:
        oc = attend(q_heads(q_proj(h_ctx)), k_c, v_c)
        o_c = oc.transpose(0, 3, 1, 2, 4).reshape(B, h_ctx.shape[1], D) @ w_o
    return o_c, o_l


def conformer_conv_mixer(h_ctx, h_lat, w_pw1, b_pw1, w_dw, b_dw, ln_g, ln_b, w_pw2, b_pw2, with_ctx):
    def f(h):
        a, g = jnp.split(h @ w_pw1 + b_pw1, 2, axis=-1)
        u = a * jax.nn.sigmoid(g)
        u = depthwise_conv(u, w_dw, b_dw)
        u = jax.nn.silu(layernorm(u, ln_g, ln_b))
        return u @ w_pw2 + b_pw2
    return (f(h_ctx) if with_ctx else None), f(h_lat)


def hyena_filter_spectrum(L, f_w1, f_b1, f_freq1, f_w2, f_b2, f_freq2, f_w3):
    f32 = jnp.float32
    d = f_w3.shape[-1] // (2 * HYENA_ORDER)
    pos = jnp.arange(L, dtype=f32)[:, None]
    t = pos / (L - 1)
    w = 2.0 * math.pi * pos / L
    bands = jnp.linspace(1e-4, FILTER_BANDS - 1, FILTER_BANDS, dtype=f32)
    feats = jnp.concatenate([t, jnp.cos(bands * w), -jnp.sin(bands * w)], axis=-1)
    z = jnp.sin(f_freq1.astype(f32) * (feats @ f_w1.astype(f32) + f_b1.astype(f32)))
    z = jnp.sin(f_freq2.astype(f32) * (z @ f_w2.astype(f32) + f_b2.astype(f32)))
    h = (z @ f_w3.astype(f32)).reshape(L, 2, HYENA_ORDER, d)
    deltas = jnp.linspace(math.log(DECAY_TARGET) / SLOW_DECAY_PCT,
                          math.log(DECAY_TARGET) / FAST_DECAY_PCT, d, dtype=f32)
    decay = jnp.exp(-t * jnp.abs(deltas))
    h = h * decay[:, None, None, :]
    two_sided = jnp.concatenate(
        [h[:, 0], jnp.zeros((1, HYENA_ORDER, d), f32), h[:0:-1, 1]], axis=0)
    two_sided = two_sided / jnp.sum(jnp.abs(two_sided), axis=0, keepdims=True)
    return jnp.fft.rfft(two_sided, axis=0)


def long_conv(z, h_spec, bias):
    L = z.shape[1]
    zf32 = z.astype(jnp.float32)
    zf = jnp.fft.rfft(zf32, n=2 * L, axis=1)
    y = jnp.fft.irfft(zf * h_spec[None], n=2 * L, axis=1)[:, :L]
    return (y + zf32 * bias.astype(jnp.float32)).astype(z.dtype)


def hyena_mixer(h_ctx, h_lat, w_in, b_in, w_short, b_short, f_w1, f_b1, f_freq1, f_w2, f_b2,
                f_freq2, f_w3, long_bias, w_out, b_out, with_ctx):
    def f(h):
        L = h.shape[1]
        u = depthwise_conv(h @ w_in + b_in, w_short, b_short)
        v, x1, x2 = jnp.split(u, 3, axis=-1)
        spec = hyena_filter_spectrum(L, f_w1, f_b1, f_freq1, f_w2, f_b2, f_freq2, f_w3)
        z = v
        for n, gate in enumerate((x1, x2)):
            z = gate * long_conv(z, spec[:, n], long_bias[n])
        return z @ w_out + b_out
    return (f(h_ctx) if with_ctx else None), f(h_lat)


def hierarchical_moe(x, w_group, b_group, w_router, b_router, w_gate, w_up, w_down):
    f32 = jnp.float32
    xf = x.astype(f32)
    g_prob = jax.nn.softmax(xf @ w_group.astype(f32) + b_group.astype(f32), axis=-1)
    g_p, g_idx = lax.top_k(g_prob, 1)
    e_logits = (xf @ w_router.astype(f32) + b_router.astype(f32)).reshape(-1, N_GROUPS, EXPERTS_PER_GROUP)
    e_logits = jnp.einsum("ng,nge->ne", jax.nn.one_hot(g_idx[:, 0], N_GROUPS, dtype=f32), e_logits)
    e_p, e_idx = lax.top_k(jax.nn.softmax(e_logits, axis=-1), TOP_K)
    w_tok = g_p * (e_p / jnp.sum(e_p, axis=-1, keepdims=True))
    flat_idx = g_idx * EXPERTS_PER_GROUP + e_idx
    combine = jnp.einsum("nk,nke->ne", w_tok, jax.nn.one_hot(flat_idx, N_EXPERTS, dtype=f32))
    combine = combine.reshape(-1, N_GROUPS, EXPERTS_PER_GROUP).astype(x.dtype)
    out = jnp.zeros_like(x)
    for g in range(N_GROUPS):
        a = jnp.einsum("nd,edf->nef", x, w_gate[g])
        b = jnp.einsum("nd,edf->nef", x, w_up[g])
        hid = jax.nn.silu(a) * b * combine[:, g, :, None]
        out = out + jnp.einsum("nef,efd->nd", hid, w_down[g])
    return out


def setup_inputs(seed: int = 0) -> dict:
    key = jax.random.key(seed)
    ks = iter(jax.random.split(key, 64))
    D = D_MODEL

    def nrm(shape, scale):
        return scale * jax.random.normal(next(ks), shape, jnp.float32)

    def gain(shape):
        return 1.0 + nrm(shape, 0.05)

    nA, nC, nH = N_ATTN_LAYERS, N_CONV_LAYERS, N_HYENA_LAYERS
    return {
        "x": nrm((BATCH, SEQ, D), 1.0),
        "c": nrm((BATCH, D), 1.0),
        "ctx": nrm((BATCH, CTX_LEN, D), 1.0),
        "c_ctx": nrm((D,), 1.0),
        "w_mod": nrm((DEPTH, D, 6 * D), 0.3 * D ** -0.5),
        "b_mod": nrm((DEPTH, 6 * D), 0.01),
        "norm_mix_g": gain((DEPTH, D)),
        "norm_ffn_g": gain((DEPTH, D)),
        "attn_w_q": nrm((nA, D, N_HEADS * HEAD_DIM), D ** -0.5),
        "attn_w_kv": nrm((nA, D, 2 * N_KV_HEADS * HEAD_DIM), D ** -0.5),
        "attn_q_gain": gain((nA, HEAD_DIM)),
        "attn_k_gain": gain((nA, HEAD_DIM)),
        "attn_w_o": nrm((nA, N_HEADS * HEAD_DIM, D), D ** -0.5),
        "conv_w_pw1": nrm((nC, D, 2 * D), D ** -0.5),
        "conv_b_pw1": nrm((nC, 2 * D), 0.01),
        "conv_w_dw": nrm((nC, CONV_WIDTH, D), CONV_WIDTH ** -0.5),
        "conv_b_dw": nrm((nC, D), 0.01),
        "conv_ln_g": gain((nC, D)),
        "conv_ln_b": nrm((nC, D), 0.01),
        "conv_w_pw2": nrm((nC, D, D), D ** -0.5),
        "conv_b_pw2": nrm((nC, D), 0.01),
        "hy_w_in": nrm((nH, D, 3 * D), D ** -0.5),
        "hy_b_in": nrm((nH, 3 * D), 0.01),
        "hy_w_short": nrm((nH, SHORT_CONV_WIDTH, 3 * D), SHORT_CONV_WIDTH ** -0.5),
        "hy_b_short": nrm((nH, 3 * D), 0.01),
        "hy_f_w1": nrm((nH, FILTER_EMB, FILTER_HIDDEN), FILTER_EMB ** -0.5),
        "hy_f_b1": nrm((nH, FILTER_HIDDEN), 0.01),
        "hy_f_freq1": gain((nH, FILTER_HIDDEN)),
        "hy_f_w2": nrm((nH, FILTER_HIDDEN, FILTER_HIDDEN), FILTER_HIDDEN ** -0.5),
        "hy_f_b2": nrm((nH, FILTER_HIDDEN), 0.01),
        "hy_f_freq2": gain((nH, FILTER_HIDDEN)),
        "hy_f_w3": nrm((nH, FILTER_HIDDEN, 2 * HYENA_ORDER * D), FILTER_HIDDEN ** -0.5),
        "hy_long_bias": nrm((nH, HYENA_ORDER, D), 1.0),
        "hy_w_out": nrm((nH, D, D), D ** -0.5),
        "hy_b_out": nrm((nH, D), 0.01),
        "moe_w_group": nrm((DEPTH, D, N_GROUPS), D ** -0.5),
        "moe_b_group": nrm((DEPTH, N_GROUPS), 0.01),
        "moe_w_router": nrm((DEPTH, D, N_EXPERTS), D ** -0.5),
        "moe_b_router": nrm((DEPTH, N_EXPERTS), 0.01),
        "moe_w_gate": nrm((DEPTH, N_GROUPS, EXPERTS_PER_GROUP, D, D_EXPERT), D ** -0.5),
        "moe_w_up": nrm((DEPTH, N_GROUPS, EXPERTS_PER_GROUP, D, D_EXPERT), D ** -0.5),
        "moe_w_down": nrm((DEPTH, N_GROUPS, EXPERTS_PER_GROUP, D_EXPERT, D), D_EXPERT ** -0.5),
        "final_norm_g": gain((D,)),
    }


def reference(x, c, ctx, c_ctx, w_mod, b_mod, norm_mix_g, norm_ffn_g,
              attn_w_q, attn_w_kv, attn_q_gain, attn_k_gain, attn_w_o,
              conv_w_pw1, conv_b_pw1, conv_w_dw, conv_b_dw, conv_ln_g, conv_ln_b, conv_w_pw2, conv_b_pw2,
              hy_w_in, hy_b_in, hy_w_short, hy_b_short, hy_f_w1, hy_f_b1, hy_f_freq1, hy_f_w2, hy_f_b2,
              hy_f_freq2, hy_f_w3, hy_long_bias, hy_w_out, hy_b_out,
              moe_w_group, moe_b_group, moe_w_router, moe_b_router, moe_w_gate, moe_w_up, moe_w_down,
              final_norm_g):
    B, S, D = x.shape
    n_ctx = ctx.shape[1]
    rows = S // GRID_W
    ang_row, ang_col = axial_angles(rows)
    silu_c = jax.nn.silu(c)
    silu_cc = jax.nn.silu(c_ctx)
    x_lat, x_ctx = x, ctx

    for i in range(DEPTH):
        kind, slot = i % N_MIXERS, i // N_MIXERS
        with_ctx = i < DEPTH - 1
        needs_ctx_input = with_ctx or kind == 0

        sh1, sc1, g1, sh2, sc2, g2 = jnp.split((silu_c @ w_mod[i] + b_mod[i])[:, None, :], 6, axis=-1)
        csh1, csc1, cg1, csh2, csc2, cg2 = jnp.split(silu_cc @ w_mod[i] + b_mod[i], 6, axis=-1)

        h_l = modulated_norm(x_lat, norm_mix_g[i], sh1, sc1)
        h_c = modulated_norm(x_ctx, norm_mix_g[i], csh1, csc1) if needs_ctx_input else None
        if kind == 0:
            y_c, y_l = attention_mixer(h_c, h_l, ang_row, ang_col, attn_w_q[slot], attn_w_kv[slot],
                                       attn_q_gain[slot], attn_k_gain[slot], attn_w_o[slot], with_ctx)
        elif kind == 1:
            y_c, y_l = conformer_conv_mixer(h_c, h_l, conv_w_pw1[slot], conv_b_pw1[slot], conv_w_dw[slot],
                                            conv_b_dw[slot], conv_ln_g[slot], conv_ln_b[slot],
                                            conv_w_pw2[slot], conv_b_pw2[slot], with_ctx)
        else:
            y_c, y_l = hyena_mixer(h_c, h_l, hy_w_in[slot], hy_b_in[slot], hy_w_short[slot], hy_b_short[slot],
                                   hy_f_w1[slot], hy_f_b1[slot], hy_f_freq1[slot], hy_f_w2[slot],
                                   hy_f_b2[slot], hy_f_freq2[slot], hy_f_w3[slot], hy_long_bias[slot],
                                   hy_w_out[slot], hy_b_out[slot], with_ctx)
        x_lat = x_lat + g1 * y_l
        if with_ctx:
            x_ctx = x_ctx + cg1 * y_c

        h_l = modulated_norm(x_lat, norm_ffn_g[i], sh2, sc2)
        moe_args = (moe_w_group[i], moe_b_group[i], moe_w_router[i], moe_b_router[i],
                    moe_w_gate[i], moe_w_up[i], moe_w_down[i])
        if with_ctx:
            h_c = modulated_norm(x_ctx, norm_ffn_g[i], csh2, csc2)
            tokens = jnp.concatenate([h_c, h_l], axis=1)
            y = hierarchical_moe(tokens.reshape(-1, D), *moe_args).reshape(B, n_ctx + S, D)
            x_ctx = x_ctx + cg2 * y[:, :n_ctx]
            x_lat = x_lat + g2 * y[:, n_ctx:]
        else:
            y_l = hierarchical_moe(h_l.reshape(-1, D), *moe_args).reshape(B, S, D)
            x_lat = x_lat + g2 * y_l

    return rmsnorm(x_lat, final_norm_g)
```

```python
import math
import numpy as np
import ml_dtypes
import concourse.bass as bass
import concourse.mybir as mybir
from concourse.bass_utils import run_bass_kernel_spmd

F32 = mybir.dt.float32
BF16 = mybir.dt.bfloat16
I32 = mybir.dt.int32
ALU = mybir.AluOpType
AF = mybir.ActivationFunctionType
AX = mybir.AxisListType

D = 1024
DC = 8
CTX = 256
SEQ = 4096
NT = CTX + SEQ
DEPTH = 4
EPS = 1e-6
NE = 32
DEXP = 256
BLOCKS = [(0, CTX)] + [(CTX + 512 * i, 512) for i in range(SEQ // 512)]


class T:
    __slots__ = ("t", "lw", "rd", "name")

    def __init__(self, t, name=""):
        self.t = t
        self.lw = None
        self.rd = {}
        self.name = name

    def __getitem__(self, k):
        return self.t[k]


class Lane:
    def __init__(self, name, eng, sem, step):
        self.name, self.eng, self.sem, self.step = name, eng, sem, step
        self.count = 0
        self.seen = {}


class MK:
    def __init__(self, nc, n_dma_sems=40):
        self.nc = nc
        self._stack = []
        self.lanes = {}
        for name, eng in (("pe", nc.tensor), ("act", nc.scalar), ("dve", nc.vector),
                          ("pool", nc.gpsimd), ("sp", nc.sync)):
            sem = self._enter(nc.semaphore("s_" + name))
            self.lanes[name] = Lane(name, eng, sem, 1)
        self.dma_lanes = []
        for i in range(n_dma_sems):
            sem = self._enter(nc.semaphore("d_%d" % i))
            ln = Lane("dma%d" % i, None, sem, 16)
            self.lanes[ln.name] = ln
            self.dma_lanes.append(ln)
        self.dma_rr = 0
        self.n_inst = 0
        self.uid = 0

    def _enter(self, cm):
        v = cm.__enter__()
        self._stack.append(cm)
        return v

    def mark(self):
        return len(self._stack)

    def release(self, mark):
        self.barrier()
        while len(self._stack) > mark:
            self._stack.pop().__exit__(None, None, None)

    def close(self):
        while self._stack:
            self._stack.pop().__exit__(None, None, None)

    def barrier(self):
        for a in ("pe", "act", "dve", "pool", "sp"):
            la = self.lanes[a]
            for b, lb in self.lanes.items():
                if b == a or lb.count == 0:
                    continue
                if la.seen.get(b, 0) >= lb.count:
                    continue
                la.seen[b] = lb.count
                la.eng.wait_ge(lb.sem, lb.count)

    def sbuf(self, name, shape, dt):
        self.uid += 1
        return T(self._enter(self.nc.sbuf_tensor("%s_%d" % (name, self.uid), list(shape), dt)), name)

    def psum(self, name, shape, dt=F32):
        self.uid += 1
        return T(self._enter(self.nc.psum_tensor("%s_%d" % (name, self.uid), list(shape), dt)), name)

    def _deps(self, lane, reads, writes):
        need = {}
        raw_same = 0
        for t in reads:
            if t.lw is not None:
                ln, idx = t.lw
                if need.get(ln, 0) < idx:
                    need[ln] = idx
                if ln == lane.name:
                    raw_same = max(raw_same, idx)
        for t in writes:
            if t.lw is not None:
                ln, idx = t.lw
                if ln != lane.name and need.get(ln, 0) < idx:
                    need[ln] = idx
            for ln, idx in t.rd.items():
                if ln != lane.name and need.get(ln, 0) < idx:
                    need[ln] = idx
        return need, raw_same

    def _record(self, lane, reads, writes):
        idx = lane.count
        for t in reads:
            if t.rd.get(lane.name, 0) < idx:
                t.rd[lane.name] = idx
        for t in writes:
            t.lw = (lane.name, idx)
            t.rd = {}

    def op(self, lane_name, fn, reads=(), writes=()):
        lane = self.lanes[lane_name]
        need, raw_same = self._deps(lane, reads, writes)
        for ln, idx in need.items():
            if ln == lane.name:
                if lane.name == "pe" or raw_same <= lane.count - 3:
                    continue
                idx = raw_same
            if lane.seen.get(ln, 0) >= idx:
                continue
            lane.seen[ln] = idx
            lane.eng.wait_ge(self.lanes[ln].sem, idx)
        inst = fn(lane.eng)
        lane.count += 1
        inst.then_inc(lane.sem, 1)
        self._record(lane, reads, writes)
        self.n_inst += 1
        return inst

    def dma(self, q, out, in_, reads=(), writes=(), **kw):
        qlane = self.lanes[q]
        dl = self.dma_lanes[self.dma_rr]
        self.dma_rr = (self.dma_rr + 1) % len(self.dma_lanes)
        need, _ = self._deps(dl, reads, writes)
        if dl.count > 0:
            need[dl.name] = max(need.get(dl.name, 0), dl.count)
        for ln, idx in need.items():
            if qlane.seen.get(ln, 0) >= idx:
                continue
            qlane.seen[ln] = idx
            qlane.eng.wait_ge(self.lanes[ln].sem, idx)
        inst = qlane.eng.dma_start(out=out, in_=in_, **kw)
        dl.count += 16
        inst.then_inc(dl.sem, 16)
        self._record(dl, reads, writes)
        self.n_inst += 1
        return inst

    def finish(self):
        sp = self.lanes["sp"]
        for dl in self.dma_lanes:
            if dl.count and sp.seen.get(dl.name, 0) < dl.count:
                sp.seen[dl.name] = dl.count
                sp.eng.wait_ge(dl.sem, dl.count)
        self.barrier()


def _vec_pc(v):
    v = np.asarray(v, np.float32)
    return np.ascontiguousarray(v.reshape(-1, 128).T)


class Prog:
    def __init__(self, cfg=None):
        self.cfg = cfg or {}
        self.nc = bass.Bass("TRN2", target_bir_lowering=False)
        self.mk = MK(self.nc)
        self.ins = {}
        self.dram = {}

    def inp(self, name, shape, dt=F32):
        t = self.nc.dram_tensor(name, list(shape), dt, kind="ExternalInput")
        self.ins[name] = t
        return T(t, name)

    def scratch(self, name, shape, dt, out=False):
        kind = "ExternalOutput" if (out or name in self.cfg.get("dump", ())) else "Internal"
        t = self.nc.dram_tensor(name, list(shape), dt, kind=kind)
        self.dram[name] = T(t, name)
        return self.dram[name]

    def setup_consts(self):
        mk = self.mk
        self.ps = [mk.psum("ps%d" % i, [128, 512], F32) for i in range(8)]
        self.ident_f = mk.sbuf("ident_f", [128, 128], F32)
        self.ident_b = mk.sbuf("ident_b", [128, 128], BF16)
        self.ones_b = mk.sbuf("ones_b", [128, 128], BF16)
        self.avg_b = mk.sbuf("avg_b", [128, 128], BF16)
        mk.op("pool", lambda e: e.memset(self.ident_f[:], 1.0), writes=[self.ident_f])
        mk.op("pool", lambda e: e.affine_select(out=self.ident_f[:], in_=self.ident_f[:],
              pattern=[[-1, 128]], compare_op=ALU.is_equal, fill=0.0, base=0,
              channel_multiplier=1), reads=[self.ident_f], writes=[self.ident_f])
        mk.op("dve", lambda e: e.tensor_copy(self.ident_b[:], self.ident_f[:]),
              reads=[self.ident_f], writes=[self.ident_b])
        mk.op("pool", lambda e: e.memset(self.ones_b[:], 1.0), writes=[self.ones_b])
        mk.op("pool", lambda e: e.memset(self.avg_b[:], 1.0 / D), writes=[self.avg_b])
        self.eps_t = mk.sbuf("eps_t", [128, 1], F32)
        mk.op("pool", lambda e: e.memset(self.eps_t[:], EPS), writes=[self.eps_t])

    def stage_input(self, x_in, ctx_in, XT):
        mk = self.mk
        m0 = mk.mark()
        xin = [mk.sbuf("xin%d" % i, [128, 4, D], F32) for i in range(2)]
        xo = [mk.sbuf("xo%d" % i, [128, DC, 512], F32) for i in range(2)]
        for bi, (t0, tn) in enumerate(BLOCKS):
            nt = tn // 128
            buf = xin[bi % 2]
            src = ctx_in if bi == 0 else x_in
            r0 = 0 if bi == 0 else t0 - CTX
            mk.dma("sp", buf[:, 0:nt, :], src[r0:r0 + tn, :].rearrange("(a p) d -> p a d", p=128),
                   reads=[src], writes=[buf])
            ob = xo[bi % 2]
            for j in range(DC):
                ps = self.ps[j % 8]
                for a in range(nt):
                    mk.op("pe", lambda e, ps=ps, a=a, j=j, buf=buf: e.transpose(
                        ps[:, a * 128:(a + 1) * 128], buf[:, a, j * 128:(j + 1) * 128], self.ident_f[:]),
                        reads=[buf, self.ident_f], writes=[ps])
                eng = "act" if j % 2 else "dve"
                if eng == "act":
                    mk.op("act", lambda e, ps=ps, j=j, ob=ob: e.copy(ob[:, j, 0:tn], ps[:, 0:tn]),
                          reads=[ps], writes=[ob])
                else:
                    mk.op("dve", lambda e, ps=ps, j=j, ob=ob: e.tensor_copy(ob[:, j, 0:tn], ps[:, 0:tn]),
                          reads=[ps], writes=[ob])
            mk.dma("pool", XT[:, t0:t0 + tn].rearrange("(j p) t -> p j t", p=128), ob[:, :, 0:tn],
                   reads=[ob], writes=self.XTr)
        mk.release(m0)

    def stage_output(self, XT, y_out, fg):
        mk = self.mk
        m0 = mk.mark()
        xb = [mk.sbuf("fx%d" % i, [128, DC, 512], F32) for i in range(2)]
        sq = mk.sbuf("fsq", [128, DC, 512], BF16)
        rstd = mk.sbuf("frstd", [128, 512], F32)
        yo = [mk.sbuf("fy%d" % i, [128, 4, D], F32) for i in range(2)]
        for bi, (t0, tn) in enumerate(BLOCKS):
            if bi == 0:
                continue
            buf = xb[bi % 2]
            mk.dma("sp", buf[:, :, 0:tn], XT[:, t0:t0 + tn].rearrange("(j p) t -> p j t", p=128),
                   reads=self.XTr, writes=[buf])
            self._rstd(buf, sq, rstd, tn, self.ps[0])
            for j in range(DC):
                mk.op("dve", lambda e, j=j, buf=buf: e.scalar_tensor_tensor(
                    out=buf[:, j, 0:tn], in0=buf[:, j, 0:tn], scalar=fg[:, j:j + 1], in1=rstd[:, 0:tn],
                    op0=ALU.mult, op1=ALU.mult), reads=[buf, rstd, fg], writes=[buf])
            ob = yo[bi % 2]
            nt = tn // 128
            for a in range(nt):
                for h in range(2):
                    ps = self.ps[1 + (a * 2 + h) % 7]
                    for jj in range(4):
                        j = h * 4 + jj
                        mk.op("pe", lambda e, ps=ps, a=a, j=j, jj=jj, buf=buf: e.transpose(
                            ps[:, jj * 128:(jj + 1) * 128], buf[:, j, a * 128:(a + 1) * 128], self.ident_f[:]),
                            reads=[buf, self.ident_f], writes=[ps])
                    if h:
                        mk.op("act", lambda e, ps=ps, a=a, h=h, ob=ob: e.copy(
                            ob[:, a, h * 512:(h + 1) * 512], ps[:, :]), reads=[ps], writes=[ob])
                    else:
                        mk.op("dve", lambda e, ps=ps, a=a, h=h, ob=ob: e.tensor_copy(
                            ob[:, a, h * 512:(h + 1) * 512], ps[:, :]), reads=[ps], writes=[ob])
            r0 = t0 - CTX
            mk.dma("pool", y_out[r0:r0 + tn, :].rearrange("(a p) d -> p a d", p=128), ob[:, 0:nt, :],
                   reads=[ob], writes=[y_out])
        mk.release(m0)

    def _rstd(self, buf, sq, rstd, tn, ps, eps=EPS):
        mk = self.mk
        for j in range(DC):
            mk.op("act", lambda e, j=j: e.activation(out=sq[:, j, 0:tn], in_=buf[:, j, 0:tn], func=AF.Square),
                  reads=[buf], writes=[sq])
        for j in range(DC):
            mk.op("pe", lambda e, j=j: e.matmul(ps[:, 0:tn], self.avg_b[:], sq[:, j, 0:tn],
                                                 start=(j == 0), stop=(j == DC - 1)),
                  reads=[self.avg_b, sq], writes=[ps])
        mk.op("act", lambda e: e.activation(out=rstd[:, 0:tn], in_=ps[:, 0:tn], func=AF.Sqrt, bias=self.eps_t[:, 0:1]),
              reads=[ps, self.eps_t], writes=[rstd])
        mk.op("dve", lambda e: e.reciprocal(rstd[:, 0:tn], rstd[:, 0:tn]), reads=[rstd], writes=[rstd])

    def ap3(self, t2d, lo, n, inner):
        return t2d[:, lo:lo + n * inner].rearrange("p (a b) -> p a b", b=inner)

    def stage_mod(self, i, sT, w_mod, bmod, gmix, gffn):
        mk = self.mk
        m0 = mk.mark()
        wb = [mk.sbuf("wmod%d" % k, [128, DC, 512], F32) for k in range(2)]
        bm = mk.sbuf("bm", [128, 48], F32)
        gm = mk.sbuf("gm", [128, 2, DC], F32)
        mod = mk.sbuf("mod", [128, 48, 2], F32)
        mk.dma("sp", bm[:], bmod[i, :, :], reads=[bmod], writes=[bm])
        mk.dma("sp", gm[:, 0, :], gmix[i, :, :], reads=[gmix], writes=[gm])
        mk.dma("sp", gm[:, 1, :], gffn[i, :, :], reads=[gffn], writes=[gm])
        ps = self.ps[0]
        for s in range(12):
            w = wb[s % 2]
            mk.dma("sp", w[:], w_mod[i, :, s * 512:(s + 1) * 512].rearrange("(k p) m -> p k m", p=128),
                   reads=[w_mod], writes=[w])
            for mm in range(4):
                m = s * 4 + mm
                for k in range(DC):
                    mk.op("pe", lambda e, w=w, mm=mm, m=m, k=k: e.matmul(
                        ps[:, 2 * m:2 * m + 2], w[:, k, mm * 128:(mm + 1) * 128], sT[:, k, :],
                        start=(k == 0), stop=(k == DC - 1)), reads=[w, sT], writes=[ps])
        psv = ps[:, 0:96].rearrange("p (m r) -> p m r", r=2)
        for r in range(2):
            mk.op("dve", lambda e, r=r: e.tensor_tensor(out=mod[:, :, r], in0=psv[:, :, r], in1=bm[:, :], op=ALU.add),
                  reads=[ps, bm], writes=[mod])
        for r in range(2):
            for nm, sc_c, sh_c, g_c, gi in (("1", 8, 0, 16, 0), ("2", 32, 24, 40, 1)):
                gs = self.mv[("gs" + nm, r)]
                mk.op("dve", lambda e, gs=gs, sc_c=sc_c, gi=gi, r=r: e.scalar_tensor_tensor(
                    out=gs[:], in0=mod[:, sc_c:sc_c + 8, r], scalar=1.0, in1=gm[:, gi, :],
                    op0=ALU.add, op1=ALU.mult), reads=[mod, gm], writes=[gs])
                sh = self.mv[("sh" + nm, r)]
                mk.op("dve", lambda e, sh=sh, sh_c=sh_c, r=r: e.tensor_copy(sh[:], mod[:, sh_c:sh_c + 8, r]),
                      reads=[mod], writes=[sh])
                g = self.mv[("g" + nm, r)]
                mk.op("dve", lambda e, g=g, g_c=g_c, r=r: e.tensor_copy(g[:], mod[:, g_c:g_c + 8, r]),
                      reads=[mod], writes=[g])
        mk.release(m0)

    def alloc_mv(self):
        self.mv = {}
        for r in range(2):
            for nm in ("gs1", "sh1", "g1", "gs2", "sh2", "g2"):
                self.mv[(nm, r)] = self.mk.sbuf("mv_%s_%d" % (nm, r), [128, DC], F32)

    def stage_norm(self, XT, HT, which, router=None):
        mk = self.mk
        m0 = mk.mark()
        xb = [mk.sbuf("nx%d" % k, [128, DC, 512], F32) for k in range(2)]
        sq = mk.sbuf("nsq", [128, DC, 512], BF16)
        rstd = mk.sbuf("nrstd", [128, 512], F32)
        hb = [mk.sbuf("nhb%d" % k, [128, DC, 512], BF16) for k in range(2)]
        if router is not None:
            htk = [mk.sbuf("nhtk%d" % k, [128, D], BF16) for k in range(2)]
            rt = {k: mk.sbuf("rt_" + k, [128, n], F32) for k, n in
                  (("lg", 36), ("gmax", 1), ("ngmax", 1), ("ge", 4), ("gsum", 1), ("gp", 1), ("mg", 4), ("es", 8),
                   ("t8", 8), ("dd", 1), ("w2", 1), ("a1", 1), ("a2", 1), ("m1", 8), ("m2", 8), ("cw", 8),
                   ("comb", 32))}
        for bi, (t0, tn) in enumerate(BLOCKS):
            r = 1 if bi == 0 else 0
            gs, sh = self.mv[("gs" + which, r)], self.mv[("sh" + which, r)]
            buf = xb[bi % 2]
            mk.dma("sp", buf[:, :, 0:tn], XT[:, t0:t0 + tn].rearrange("(j p) t -> p j t", p=128),
                   reads=self.XTr, writes=[buf])
            self._rstd(buf, sq, rstd, tn, self.ps[0])
            h = hb[bi % 2]
            for j in range(DC):
                mk.op("dve", lambda e, j=j, buf=buf, gs=gs: e.scalar_tensor_tensor(
                    out=buf[:, j, 0:tn], in0=buf[:, j, 0:tn], scalar=gs[:, j:j + 1], in1=rstd[:, 0:tn],
                    op0=ALU.mult, op1=ALU.mult), reads=[buf, rstd, gs], writes=[buf])
                if router is not None:
                    mk.op("dve", lambda e, j=j, buf=buf, sh=sh: e.tensor_scalar(
                        out=buf[:, j, 0:tn], in0=buf[:, j, 0:tn], scalar1=sh[:, j:j + 1], scalar2=None,
                        op0=ALU.add), reads=[buf, sh], writes=[buf])
                    mk.op("act", lambda e, j=j, buf=buf, h=h: e.copy(h[:, j, 0:tn], buf[:, j, 0:tn]),
                          reads=[buf], writes=[h])
                else:
                    mk.op("act", lambda e, j=j, buf=buf, h=h, sh=sh: e.activation(
                        out=h[:, j, 0:tn], in_=buf[:, j, 0:tn], func=AF.Identity, bias=sh[:, j:j + 1]),
                        reads=[buf, sh], writes=[h])
            mk.dma("pool", HT[:, t0:t0 + tn].rearrange("(j p) t -> p j t", p=128), h[:, :, 0:tn],
                   reads=[h], writes=[HT])
            if router is not None:
                for a in range(tn // 128):
                    if "M1a" in router:
                        self._route_tile_sparse(buf, a, t0 + a * 128, router, rt)
                        pb = self.ps[5 + (a % 2)]
                        pbb = pb[:, 0:512].bitcast(BF16)
                        for j in range(DC):
                            mk.op("pe", lambda e, pbb=pbb, j=j, a=a, h=h: e.transpose(
                                pbb[:, j * 128:(j + 1) * 128], h[:, j, a * 128:(a + 1) * 128], self.ident_b[:]),
                                reads=[h, self.ident_b], writes=[pb])
                        ht_ = htk[a % 2]
                        mk.op("act", lambda e, pbb=pbb, ht_=ht_: e.copy(ht_[:], pbb[:, :]), reads=[pb], writes=[ht_])
                        mk.dma("act", self.dram["HTOK"][t0 + a * 128:t0 + (a + 1) * 128, :], ht_[:], reads=[ht_],
                               writes=[self.dram["HTOK"]])
                    else:
                        self._route_tile(buf, a, t0 + a * 128, router, rt)
        mk.release(m0)

    def _route_tile(self, hf, a, tok0, R, rt):
        mk = self.mk
        ps = self.ps[1 + (a % 2)]
        Wr, br, combT = R["Wr"], R["br"], R["combT"]
        for k in range(DC):
            mk.op("pe", lambda e, k=k: e.matmul(ps[:, 0:36], hf[:, k, a * 128:(a + 1) * 128], Wr[:, k, :],
                                                 start=(k == 0), stop=(k == DC - 1)),
                  reads=[hf, Wr], writes=[ps])
        lg, gmax, ngmax, ge, gsum, gp, mg, es = (rt[k] for k in ("lg", "gmax", "ngmax", "ge", "gsum", "gp", "mg", "es"))
        t8, dd, w2, a1, a2, m1, m2, cw, comb = (rt[k] for k in ("t8", "dd", "w2", "a1", "a2", "m1", "m2", "cw", "comb"))
        V = lambda fn, reads, writes: mk.op("dve", fn, reads=reads, writes=writes)
        V(lambda e: e.tensor_tensor(out=lg[:], in0=ps[:, 0:36], in1=br[:], op=ALU.add), [ps, br], [lg])
        V(lambda e: e.reduce_max(out=gmax[:], in_=lg[:, 0:4], axis=AX.X), [lg], [gmax])
        V(lambda e: e.tensor_scalar(out=ngmax[:], in0=gmax[:], scalar1=-1.0, scalar2=None, op0=ALU.mult), [gmax], [ngmax])
        mk.op("act", lambda e: e.activation(out=ge[:], in_=lg[:, 0:4], func=AF.Exp, bias=ngmax[:, 0:1],
                                            accum_out=gsum[:]), reads=[lg, ngmax], writes=[ge, gsum])
        V(lambda e: e.reciprocal(gp[:], gsum[:]), [gsum], [gp])
        V(lambda e: e.tensor_scalar(out=mg[:], in0=lg[:, 0:4], scalar1=gmax[:, 0:1], scalar2=None, op0=ALU.is_equal),
          [lg, gmax], [mg])
        V(lambda e: e.tensor_scalar(out=es[:], in0=lg[:, 4:12], scalar1=mg[:, 0:1], scalar2=None, op0=ALU.mult),
          [lg, mg], [es])
        for g in range(1, 4):
            V(lambda e, g=g: e.scalar_tensor_tensor(out=es[:], in0=lg[:, 4 + 8 * g:12 + 8 * g], scalar=mg[:, g:g + 1],
                                                    in1=es[:], op0=ALU.mult, op1=ALU.add), [lg, mg, es], [es])
        V(lambda e: e.max(out=t8[:], in_=es[:]), [es], [t8])
        V(lambda e: e.tensor_tensor(out=dd[:], in0=t8[:, 1:2], in1=t8[:, 0:1], op=ALU.subtract), [t8], [dd])
        mk.op("act", lambda e: e.activation(out=w2[:], in_=dd[:], func=AF.Sigmoid), reads=[dd], writes=[w2])
        V(lambda e: e.tensor_tensor(out=a2[:], in0=w2[:], in1=gp[:], op=ALU.mult), [w2, gp], [a2])
        V(lambda e: e.tensor_tensor(out=a1[:], in0=gp[:], in1=a2[:], op=ALU.subtract), [gp, a2], [a1])
        V(lambda e: e.tensor_scalar(out=m1[:], in0=es[:], scalar1=t8[:, 0:1], scalar2=a1[:, 0:1], op0=ALU.is_equal,
                                    op1=ALU.mult), [es, t8, a1], [m1])
        V(lambda e: e.tensor_scalar(out=m2[:], in0=es[:], scalar1=t8[:, 1:2], scalar2=a2[:, 0:1], op0=ALU.is_equal,
                                    op1=ALU.mult), [es, t8, a2], [m2])
        V(lambda e: e.tensor_tensor(out=cw[:], in0=m1[:], in1=m2[:], op=ALU.add), [m1, m2], [cw])
        for g in range(4):
            V(lambda e, g=g: e.tensor_scalar(out=comb[:, 8 * g:8 * g + 8], in0=cw[:], scalar1=mg[:, g:g + 1],
                                             scalar2=None, op0=ALU.mult), [cw, mg], [comb])
        ps2 = self.ps[3 + (a % 2)]
        mk.op("pe", lambda e: e.transpose(ps2[0:32, 0:128], comb[:, :], self.ident_f[:]),
              reads=[comb, self.ident_f], writes=[ps2])
        mk.op("act", lambda e: e.copy(combT[:, tok0:tok0 + 128], ps2[0:32, 0:128]), reads=[ps2], writes=[combT])

    def stage_moe(self, i, XT, HT, HID, combT, sel, w_gate, w_up, w_down):
        mk = self.mk
        m0 = mk.mark()
        hT = mk.sbuf("moe_hT", [128, DC, NT], BF16)
        for j in range(DC):
            mk.dma("sp", hT[:, j, :], HT[j * 128:(j + 1) * 128, :], reads=[HT], writes=[hT])
        wgu = [mk.sbuf("wgu%d" % k, [128, DC, 512], BF16) for k in range(2)]
        cbs = [mk.sbuf("cbs%d" % k, [128, 512], F32) for k in range(2)]
        sl = [mk.sbuf("sl%d" % k, [128, 512], F32) for k in range(2)]
        tt = [mk.sbuf("tt%d" % k, [128, 512], F32) for k in range(2)]
        hid = [mk.sbuf("hid%d" % k, [128, 2, 512], BF16) for k in range(2)]
        n = 0
        for ex in range(NE):
            g, el = ex // 8, ex % 8
            w = wgu[ex % 2]
            mk.dma("pool", w[:, :, 0:256], w_gate[i, g, el, :, :].rearrange("(k p) f -> p k f", p=128),
                   reads=[w_gate], writes=[w])
            mk.dma("pool", w[:, :, 256:512], w_up[i, g, el, :, :].rearrange("(k p) f -> p k f", p=128),
                   reads=[w_up], writes=[w])
            for bi, (t0, tn) in enumerate(BLOCKS):
                pc = self.ps[n % 2]
                mk.op("pe", lambda e, pc=pc, ex=ex: e.matmul(pc[:, 0:tn], sel[:, ex, :], combT[:, t0:t0 + tn],
                                                            start=True, stop=True),
                      reads=[sel, combT], writes=[pc])
                cb = cbs[n % 2]
                mk.op("act", lambda e, pc=pc, cb=cb: e.copy(cb[:, 0:tn], pc[:, 0:tn]), reads=[pc], writes=[cb])
                hd = hid[n % 2]
                for fc in range(2):
                    q = (2 * n + fc) % 3
                    pg, pu = self.ps[2 + 2 * q], self.ps[3 + 2 * q]
                    for k in range(DC):
                        mk.op("pe", lambda e, pg=pg, k=k, fc=fc, w=w: e.matmul(
                            pg[:, 0:tn], w[:, k, fc * 128:(fc + 1) * 128], hT[:, k, t0:t0 + tn],
                            start=(k == 0), stop=(k == DC - 1)), reads=[w, hT], writes=[pg])
                    for k in range(DC):
                        mk.op("pe", lambda e, pu=pu, k=k, fc=fc, w=w: e.matmul(
                            pu[:, 0:tn], w[:, k, 256 + fc * 128:256 + (fc + 1) * 128], hT[:, k, t0:t0 + tn],
                            start=(k == 0), stop=(k == DC - 1)), reads=[w, hT], writes=[pu])
                    s_, t_ = sl[fc], tt[fc]
                    mk.op("act", lambda e, pg=pg, s_=s_: e.activation(out=s_[:, 0:tn], in_=pg[:, 0:tn], func=AF.Silu),
                          reads=[pg], writes=[s_])
                    mk.op("dve", lambda e, pu=pu, s_=s_, t_=t_: e.tensor_tensor(
                        out=t_[:, 0:tn], in0=pu[:, 0:tn], in1=s_[:, 0:tn], op=ALU.mult), reads=[pu, s_], writes=[t_])
                    mk.op("pool", lambda e, t_=t_, cb=cb, hd=hd, fc=fc: e.tensor_tensor(
                        out=hd[:, fc, 0:tn], in0=t_[:, 0:tn], in1=cb[:, 0:tn], op=ALU.mult), reads=[t_, cb], writes=[hd])
                mk.dma("act", HID[ex * 256:(ex + 1) * 256, t0:t0 + tn].rearrange("(c p) t -> p c t", p=128),
                       hd[:, :, 0:tn], reads=[hd], writes=[HID])
                n += 1
        mk.release(m0)
        m0 = mk.mark()
        hb = [mk.sbuf("p2h%d" % k, [128, 64, 512], BF16) for k in range(1)]
        wd = [mk.sbuf("p2w%d" % k, [128, 8, D], BF16) for k in range(2)]
        xr = [mk.sbuf("p2x%d" % k, [128, 512], F32) for k in range(3)]
        wdv = w_down[i].rearrange("g e f d -> (g e f) d")
        n = 0
        for bi, (t0, tn) in enumerate(BLOCKS):
            r = 1 if bi == 0 else 0
            g2 = self.mv[("g2", r)]
            h = hb[0]
            for c8 in range(8):
                mk.dma("sp", h[:, c8 * 8:(c8 + 1) * 8, 0:tn],
                       HID[c8 * 1024:(c8 + 1) * 1024, t0:t0 + tn].rearrange("(c p) t -> p c t", p=128),
                       reads=[HID], writes=[h])
            for kg in range(8):
                w = wd[n % 2]
                n += 1
                mk.dma("pool", w[:], wdv[kg * 1024:(kg + 1) * 1024, :].rearrange("(c p) d -> p c d", p=128),
                       reads=[w_down], writes=[w])
                for kk in range(8):
                    kc = kg * 8 + kk
                    for dc in range(DC):
                        mk.op("pe", lambda e, dc=dc, kk=kk, kc=kc, w=w: e.matmul(
                            self.ps[dc][:, 0:tn], w[:, kk, dc * 128:(dc + 1) * 128], h[:, kc, 0:tn],
                            start=(kc == 0), stop=(kc == 63)), reads=[w, h], writes=[self.ps[dc]])
            for dc in range(DC):
                x = xr[dc % 3]
                mk.dma("sp", x[:, 0:tn], XT[dc * 128:(dc + 1) * 128, t0:t0 + tn], reads=[self.XTr[dc]], writes=[x])
                mk.op("dve", lambda e, dc=dc, x=x, g2=g2: e.scalar_tensor_tensor(
                    out=x[:, 0:tn], in0=self.ps[dc][:, 0:tn], scalar=g2[:, dc:dc + 1], in1=x[:, 0:tn],
                    op0=ALU.mult, op1=ALU.add), reads=[self.ps[dc], g2, x], writes=[x])
                mk.dma("act", XT[dc * 128:(dc + 1) * 128, t0:t0 + tn], x[:, 0:tn], reads=[x], writes=[self.XTr[dc]])
        mk.release(m0)


def build_program(cfg=None):
    cfg = cfg or {}
    p = Prog(cfg)
    mk = p.mk
    layers = cfg.get("layers", list(range(DEPTH)))
    I = {}
    I["x"] = p.inp("x", [SEQ, D])
    I["ctx"] = p.inp("ctx", [CTX, D])
    I["c_pc"] = p.inp("c_pc", [128, DC, 2])
    I["w_mod"] = p.inp("w_mod", [DEPTH, D, 6 * D])
    I["bmod_pc"] = p.inp("bmod_pc", [DEPTH, 128, 48])
    I["gmix_pc"] = p.inp("gmix_pc", [DEPTH, 128, DC])
    I["gffn_pc"] = p.inp("gffn_pc", [DEPTH, 128, DC])
    I["fg_pc"] = p.inp("fg_pc", [128, DC])
    I["wr_pc"] = p.inp("wr_pc", [DEPTH, 128, DC, 36])
    I["br_bc"] = p.inp("br_bc", [DEPTH, 128, 36])
    I["sel"] = p.inp("sel", [32, NE, 128], BF16)
    sparse = cfg.get("sparse", True)
    if sparse:
        for li in range(DEPTH):
            I["moe_wgu%d" % li] = p.inp("moe_wgu%d" % li, [NE * 128, DC * 512])
            I["moe_wdn%d" % li] = p.inp("moe_wdn%d" % li, [NE * 128, 2 * D])
        I["jgrid"] = p.inp("jgrid", [128, NBLK])
        I["pidx"] = p.inp("pidx", [128, 1])
    else:
        I["moe_w_gate"] = p.inp("moe_w_gate", [DEPTH, 4, 8, D, DEXP])
        I["moe_w_up"] = p.inp("moe_w_up", [DEPTH, 4, 8, D, DEXP])
        I["moe_w_down"] = p.inp("moe_w_down", [DEPTH, 4, 8, DEXP, D])
    I["attn_w_q"] = p.inp("attn_w_q", [2, D, D])
    I["attn_w_kv"] = p.inp("attn_w_kv", [2, D, 512])
    I["attn_w_o"] = p.inp("attn_w_o", [2, D, D])
    I["qkgain_pc"] = p.inp("qkgain_pc", [2, 128, 2])
    I["conv_w_pw1"] = p.inp("conv_w_pw1", [1, D, 2 * D])
    I["conv_w_pw2"] = p.inp("conv_w_pw2", [1, D, D])
    I["conv_b_pw1_pc"] = p.inp("conv_b_pw1_pc", [1, 128, 16])
    I["conv_w_dw_pc"] = p.inp("conv_w_dw_pc", [1, 128, DC, 31])
    I["conv_b_dw_pc"] = p.inp("conv_b_dw_pc", [1, 128, DC])
    I["conv_ln_g_pc"] = p.inp("conv_ln_g_pc", [1, 128, DC])
    I["conv_ln_b_pc"] = p.inp("conv_ln_b_pc", [1, 128, DC])
    I["conv_b_pw2_pc"] = p.inp("conv_b_pw2_pc", [1, 128, DC])
    I["hy_w_in"] = p.inp("hy_w_in", [1, D, 3 * D])
    I["hy_w_out"] = p.inp("hy_w_out", [1, D, D])
    I["hy_b_in_pc"] = p.inp("hy_b_in_pc", [128, 24])
    I["hy_w_short_pc"] = p.inp("hy_w_short_pc", [128, 24, 3])
    I["hy_b_short_pc"] = p.inp("hy_b_short_pc", [128, 24])
    I["hy_b_out_pc"] = p.inp("hy_b_out_pc", [128, DC])
    I["hy_lbias_pc"] = p.inp("hy_lbias_pc", [2, 128, DC])
    I["hy_f_w1"] = p.inp("hy_f_w1", [1, 17, 64])
    I["hy_f_w2"] = p.inp("hy_f_w2", [1, 64, 64])
    I["hy_f_w3"] = p.inp("hy_f_w3", [1, 64, 4 * D])
    I["hy_fvec"] = p.inp("hy_fvec", [64, 4])
    I["hy_dabs"] = p.inp("hy_dabs", [64, D])
    for tag, L_ in (("lat", SEQ), ("ctx", CTX)):
        N1_ = 2 * L_ // 64
        I["hy_F1_" + tag] = p.inp("hy_F1_" + tag, [N1_ // 2, 6, N1_], BF16)
        I["hy_G_" + tag] = p.inp("hy_G_" + tag, [128, N1_, 5, 128], BF16)
        I["hy_Fi_" + tag] = p.inp("hy_Fi_" + tag, [N1_, 2, N1_ // 2], BF16)
        I["hy_lneg_" + tag] = p.inp("hy_lneg_" + tag, [N1_ // 2, 64])
        I["hy_featsT_" + tag] = p.inp("hy_featsT_" + tag, [17, L_])
    I["ropeR"] = p.inp("ropeR", [128, 128], BF16)
    I["ropeC"] = p.inp("ropeC", [128, SEQ])
    I["ropeS"] = p.inp("ropeS", [128, SEQ])
    y = T(p.nc.dram_tensor("y", [SEQ, D], F32, kind="ExternalOutput"), "y")
    QT = p.scratch("QT", [D, NT], BF16)
    OT = p.scratch("OT", [D, NT], BF16)
    XT = p.scratch("XT", [D, NT], F32)
    p.XTr = [T(XT.t, "XTr%d" % k) for k in range(DC)]
    VT = p.scratch("VT", [D, NT], F32)
    if 2 in layers and cfg.get("mixers", True):
        p.scratch("hyU0", [3 * D, NT], F32)
        p.scratch("hyZ", [3 * D, NT], F32)
        p.scratch("hyZ1", [D, NT], F32)
        p.scratch("hyA", [2, 128, 64, D], BF16)
        p.scratch("hyC", [2, 64, 128, D], BF16)
        for o in range(2):
            p.scratch("hyHH_lat%d" % o, [2, 128, 128, D], BF16)
            p.scratch("hyHH_ctx%d" % o, [2, 8, 128, D], BF16)
    HT = p.scratch("HT", [D, NT], BF16)
    if sparse:
        p.scratch("HTOK", [NT, D], BF16)
        p.scratch("HS", [NSLOT, D], BF16)
        p.scratch("YS", [NSLOT, D], F32)
    else:
        HID = p.scratch("HID", [NE * DEXP, NT], BF16)
    p.setup_consts()
    p.alloc_mv()
    fg = mk.sbuf("fg", [128, DC], F32)
    sT = mk.sbuf("sT", [128, DC, 2], F32)
    sel = mk.sbuf("sel", [32, NE, 128], BF16)
    if sparse:
        SR = p.alloc_sparse()
        mk.dma("sp", SR["jg"][:], I["jgrid"][:, :], writes=[SR["jg"]])
        mk.dma("sp", SR["pidx"][:], I["pidx"][:, :], writes=[SR["pidx"]])
    else:
        combT = mk.sbuf("combT", [32, NT], BF16)
    Wr = mk.sbuf("Wr", [128, DC, 36], F32)
    br = mk.sbuf("br", [128, 36], F32)
    mk.dma("sp", fg[:], I["fg_pc"][:, :], reads=[I["fg_pc"]], writes=[fg])
    mk.dma("sp", sT[:], I["c_pc"][:, :, :], reads=[I["c_pc"]], writes=[sT])
    mk.dma("sp", sel[:], I["sel"][:, :, :], reads=[I["sel"]], writes=[sel])
    mk.op("act", lambda e: e.activation(out=sT[:], in_=sT[:], func=AF.Silu), reads=[sT], writes=[sT])
    p.stage_input(I["x"], I["ctx"], XT)
    for i in layers:
        p.stage_mod(i, sT, I["w_mod"], I["bmod_pc"], I["gmix_pc"], I["gffn_pc"])
        if cfg.get("mixers", True):
            kind, slot = i % 3, i // 3
            with_ctx = i < DEPTH - 1
            p.stage_norm(XT, HT, "1")
            if kind == 0:
                p.stage_attn(slot, XT, HT, QT, OT, I, with_ctx)
            elif kind == 1:
                p.stage_conf(slot, XT, HT, QT, VT, I)
            else:
                p.stage_hyena(slot, XT, HT, I)
        if cfg.get("moe", True) is False:
            continue
        mk.dma("sp", Wr[:], I["wr_pc"][i, :, :, :], reads=[I["wr_pc"]], writes=[Wr])
        mk.dma("sp", br[:], I["br_bc"][i, :, :], reads=[I["br_bc"]], writes=[br])
        if sparse:
            SR["Wr"], SR["br"] = Wr, br
            p.stage_norm(XT, HT, "2", router=SR)
            p.stage_moe_sparse(i, XT, SR, I)
        else:
            p.stage_norm(XT, HT, "2", router=dict(Wr=Wr, br=br, combT=combT))
            p.stage_moe(i, XT, HT, HID, combT, sel, I["moe_w_gate"], I["moe_w_up"], I["moe_w_down"])
    p.stage_output(XT, y, fg)
    mk.finish()
    mk.close()
    return p


_HYC = {}


def host_inputs(inp, b, sparse=True):
    f = lambda a: np.ascontiguousarray(np.asarray(a, np.float32))
    m = {}
    m["x"] = f(inp["x"][b])
    m["ctx"] = f(inp["ctx"][b])
    m["c_pc"] = np.ascontiguousarray(np.stack([_vec_pc(inp["c"][b]), _vec_pc(inp["c_ctx"])], axis=-1))
    m["w_mod"] = f(inp["w_mod"])
    m["bmod_pc"] = np.stack([_vec_pc(inp["b_mod"][i]) for i in range(DEPTH)])
    m["gmix_pc"] = np.stack([_vec_pc(inp["norm_mix_g"][i]) for i in range(DEPTH)])
    m["gffn_pc"] = np.stack([_vec_pc(inp["norm_ffn_g"][i]) for i in range(DEPTH)])
    m["fg_pc"] = _vec_pc(inp["final_norm_g"])
    wr = np.concatenate([np.asarray(inp["moe_w_group"]), np.asarray(inp["moe_w_router"])], axis=-1)
    m["wr_pc"] = np.ascontiguousarray(wr.reshape(DEPTH, DC, 128, 36).transpose(0, 2, 1, 3)).astype(np.float32)
    brr = np.concatenate([np.asarray(inp["moe_b_group"]), np.asarray(inp["moe_b_router"])], axis=-1)
    m["br_bc"] = np.ascontiguousarray(np.broadcast_to(brr[:, None, :], (DEPTH, 128, 36))).astype(np.float32)
    sel = np.zeros((32, NE, 128), np.float32)
    for e in range(NE):
        sel[e, e, :] = 1.0
    m["sel"] = sel.astype(ml_dtypes.bfloat16)
    m["attn_w_q"] = f(inp["attn_w_q"]); m["attn_w_kv"] = f(inp["attn_w_kv"]); m["attn_w_o"] = f(inp["attn_w_o"])
    m["qkgain_pc"] = np.ascontiguousarray(np.stack([np.asarray(inp["attn_q_gain"], np.float32),
                                                    np.asarray(inp["attn_k_gain"], np.float32)], axis=-1))
    m["conv_w_pw1"] = f(inp["conv_w_pw1"]); m["conv_w_pw2"] = f(inp["conv_w_pw2"])
    m["conv_b_pw1_pc"] = _vec_pc(inp["conv_b_pw1"][0])[None]
    m["conv_w_dw_pc"] = np.ascontiguousarray(np.asarray(inp["conv_w_dw"][0], np.float32).reshape(31, DC, 128).transpose(2, 1, 0))[None]
    for nm in ("conv_b_dw", "conv_ln_g", "conv_ln_b", "conv_b_pw2"):
        m[nm + "_pc"] = _vec_pc(inp[nm][0])[None]
    m["hy_w_in"] = f(inp["hy_w_in"]); m["hy_w_out"] = f(inp["hy_w_out"])
    m["hy_b_in_pc"] = _vec_pc(inp["hy_b_in"][0]); m["hy_b_short_pc"] = _vec_pc(inp["hy_b_short"][0])
    m["hy_w_short_pc"] = np.ascontiguousarray(np.stack([_vec_pc(inp["hy_w_short"][0, k]) for k in range(3)], axis=-1))
    m["hy_b_out_pc"] = _vec_pc(inp["hy_b_out"][0])
    m["hy_lbias_pc"] = np.stack([_vec_pc(inp["hy_long_bias"][0, o]) for o in range(2)])
    m["hy_f_w1"] = f(inp["hy_f_w1"]); m["hy_f_w2"] = f(inp["hy_f_w2"]); m["hy_f_w3"] = f(inp["hy_f_w3"])
    m["hy_fvec"] = np.ascontiguousarray(np.stack([np.asarray(inp[k][0], np.float32) for k in
                                                  ("hy_f_b1", "hy_f_freq1", "hy_f_b2", "hy_f_freq2")], axis=-1))
    dl = np.abs(np.linspace(math.log(1e-2) / 1.5, math.log(1e-2) / 0.3, D, dtype=np.float32))
    m["hy_dabs"] = np.ascontiguousarray(np.broadcast_to(dl[None, :], (64, D))).astype(np.float32)
    for tag, L_ in (("lat", SEQ), ("ctx", CTX)):
        hc = _HYC[tag] if tag in _HYC else _HYC.setdefault(tag, hy_consts(L_))
        m["hy_F1_" + tag] = hc["F1"]; m["hy_G_" + tag] = hc["G"]; m["hy_Fi_" + tag] = hc["Fi"]
        m["hy_lneg_" + tag] = hc["lneg"]; m["hy_featsT_" + tag] = hc["featsT"]
    RT = np.zeros((128, 128), np.float32)
    for base in (0, 64):
        for dd in range(32):
            RT[base + dd + 32, base + dd] = -1.0
            RT[base + dd, base + dd + 32] = 1.0
    m["ropeR"] = RT.astype(ml_dtypes.bfloat16)
    inv = (np.float32(10000.0) ** (-np.arange(32, dtype=np.float32) / np.float32(32))).astype(np.float32)
    tt = np.arange(SEQ)
    ang = np.zeros((128, SEQ), np.float32)
    for dd in range(128):
        pos = (tt // 64) if dd < 64 else (tt % 64)
        ang[dd] = pos.astype(np.float32) * inv[dd % 32]
    m["ropeC"] = np.cos(ang).astype(np.float32)
    m["ropeS"] = np.sin(ang).astype(np.float32)
    if sparse:
        wg = np.asarray(inp["moe_w_gate"], np.float32).reshape(DEPTH, NE, DC, 128, DEXP)
        wu = np.asarray(inp["moe_w_up"], np.float32).reshape(DEPTH, NE, DC, 128, DEXP)
        wd = np.asarray(inp["moe_w_down"], np.float32).reshape(DEPTH, NE, 2, 128, D)
        for li in range(DEPTH):
            m["moe_wgu%d" % li] = np.ascontiguousarray(np.concatenate([wg[li], wu[li]], axis=-1).transpose(0, 2, 1, 3)).reshape(NE * 128, DC * 512)
            m["moe_wdn%d" % li] = np.ascontiguousarray(wd[li].transpose(0, 2, 1, 3)).reshape(NE * 128, 2 * D)
        m["jgrid"] = np.ascontiguousarray(np.broadcast_to((np.arange(NBLK, dtype=np.float32) * BSL)[None, :], (128, NBLK)))
        m["pidx"] = np.arange(128, dtype=np.float32)[:, None].copy()
    else:
        m["moe_w_gate"] = f(inp["moe_w_gate"])
        m["moe_w_up"] = f(inp["moe_w_up"])
        m["moe_w_down"] = f(inp["moe_w_down"])
    return m


def _attn_methods():
    def load_w(self, name, src_ap, kc, m, q="pool"):
        mk = self.mk
        w = mk.sbuf(name, [128, kc, m], BF16)
        step = max(1, 4096 // m)
        for k0 in range(0, kc, step):
            k1 = min(kc, k0 + step)
            mk.dma(q, w[:, k0:k1, :], src_ap[k0 * 128:k1 * 128, :].rearrange("(k p) m -> p k m", p=128), writes=[w])
        return w

    def linear_resid(self, XT, srcT, W, KC, gname, bias=None, skip_ctx=False):
        mk = self.mk
        m0 = mk.mark()
        sb = [mk.sbuf("lr_s%d" % k, [128, KC, 512], BF16) for k in range(2)]
        xr = [mk.sbuf("lr_x%d" % k, [128, 512], F32) for k in range(3)]
        gb = None
        if bias is not None:
            gb = [mk.sbuf("lr_gb%d" % r, [128, DC], F32) for r in range(2)]
            for r in range(2):
                mk.op("dve", lambda e, r=r: e.tensor_tensor(out=gb[r][:], in0=bias[:], in1=self.mv[(gname, r)][:],
                                                            op=ALU.mult), reads=[bias, self.mv[(gname, r)]], writes=[gb[r]])
        n = 0
        for bi, (t0, tn) in enumerate(BLOCKS):
            if skip_ctx and bi == 0:
                continue
            r = 1 if bi == 0 else 0
            g = self.mv[(gname, r)]
            s = sb[bi % 2]
            mk.dma("sp", s[:, :, 0:tn], srcT[:, t0:t0 + tn].rearrange("(k p) t -> p k t", p=128),
                   reads=[srcT], writes=[s])
            for dc in range(DC):
                ps = self.ps[n % 4]
                x = xr[n % 3]
                n += 1
                mk.dma("sp", x[:, 0:tn], XT[dc * 128:(dc + 1) * 128, t0:t0 + tn], reads=[self.XTr[dc]], writes=[x])
                for k in range(KC):
                    mk.op("pe", lambda e, ps=ps, k=k, dc=dc, s=s: e.matmul(
                        ps[:, 0:tn], W[:, k, dc * 128:(dc + 1) * 128], s[:, k, 0:tn],
                        start=(k == 0), stop=(k == KC - 1)), reads=[W, s], writes=[ps])
                mk.op("dve", lambda e, ps=ps, dc=dc, x=x, g=g: e.scalar_tensor_tensor(
                    out=x[:, 0:tn], in0=ps[:, 0:tn], scalar=g[:, dc:dc + 1], in1=x[:, 0:tn],
                    op0=ALU.mult, op1=ALU.add), reads=[ps, g, x], writes=[x])
                if gb is not None:
                    mk.op("dve", lambda e, dc=dc, x=x, r=r: e.tensor_scalar(
                        out=x[:, 0:tn], in0=x[:, 0:tn], scalar1=gb[r][:, dc:dc + 1], scalar2=None, op0=ALU.add),
                        reads=[x, gb[r]], writes=[x])
                mk.dma("act", XT[dc * 128:(dc + 1) * 128, t0:t0 + tn], x[:, 0:tn], reads=[x], writes=[self.XTr[dc]])
        mk.release(m0)

    def stage_attn(self, slot, XT, HT, QT, OT, I, with_ctx):
        mk = self.mk
        mA = mk.mark()
        KT = mk.sbuf("KT", [128, 2, NT], BF16)
        Vs = mk.sbuf("Vs", [128, NT // 128, 256], BF16)
        gains = mk.sbuf("qkgain", [128, 2], F32)
        RT = mk.sbuf("RT", [128, 128], BF16)
        avgh = mk.sbuf("avgh", [128, 128], BF16)
        mk.dma("sp", gains[:], I["qkgain_pc"][slot, :, :], writes=[gains])
        mk.dma("sp", RT[:], I["ropeR"][:, :], writes=[RT])
        mk.op("pool", lambda e: e.memset(avgh[:], 1.0 / 128), writes=[avgh])
        m0 = mk.mark()
        wq = self.load_w("wq", I["attn_w_q"][slot], DC, D)
        wkv = self.load_w("wkv", I["attn_w_kv"][slot], DC, 512)
        hb = [mk.sbuf("ah%d" % k, [128, DC, 512], BF16) for k in range(2)]
        cs = [mk.sbuf("acs%d" % k, [128, 2, 512], F32) for k in range(2)]
        sqq = [mk.sbuf("asq%d" % k, [128, 512], BF16) for k in range(2)]
        rs = [mk.sbuf("ars%d" % k, [128, 512], F32) for k in range(2)]
        qn = [mk.sbuf("aqn%d" % k, [128, 512], F32) for k in range(2)]
        qb = [mk.sbuf("aqb%d" % k, [128, 512], BF16) for k in range(2)]
        t1 = [mk.sbuf("at1%d" % k, [128, 512], F32) for k in range(2)]
        t2 = [mk.sbuf("at2%d" % k, [128, 512], F32) for k in range(2)]
        qo = [mk.sbuf("aqo%d" % k, [128, 512], BF16) for k in range(3)]
        n = 0
        for bi, (t0, tn) in enumerate(BLOCKS):
            h = hb[bi % 2]
            mk.dma("sp", h[:, :, 0:tn], HT[:, t0:t0 + tn].rearrange("(k p) t -> p k t", p=128), reads=[HT], writes=[h])
            c = cs[bi % 2]
            if bi > 0:
                mk.dma("sp", c[:, 0, 0:tn], I["ropeC"][:, t0 - CTX:t0 - CTX + tn], writes=[c])
                mk.dma("sp", c[:, 1, 0:tn], I["ropeS"][:, t0 - CTX:t0 - CTX + tn], writes=[c])
            for hh in range(10):
                isq = hh < 8
                if isq and bi == 0 and not with_ctx:
                    continue
                W = wq if isq else wkv
                c0 = hh * 128 if isq else (hh - 8) * 128
                gcol = 0 if isq else 1
                ps = self.ps[n % 2]
                pz = self.ps[2 + n % 2]
                pr = self.ps[4 + n % 2]
                i2 = n % 2
                n += 1
                for k in range(DC):
                    mk.op("pe", lambda e, ps=ps, k=k, W=W, c0=c0, h=h: e.matmul(
                        ps[:, 0:tn], W[:, k, c0:c0 + 128], h[:, k, 0:tn], start=(k == 0), stop=(k == DC - 1)),
                        reads=[W, h], writes=[ps])
                mk.op("act", lambda e, ps=ps, i2=i2: e.activation(out=sqq[i2][:, 0:tn], in_=ps[:, 0:tn], func=AF.Square),
                      reads=[ps], writes=[sqq[i2]])
                mk.op("pe", lambda e, pz=pz, i2=i2: e.matmul(pz[:, 0:tn], avgh[:], sqq[i2][:, 0:tn], start=True, stop=True),
                      reads=[avgh, sqq[i2]], writes=[pz])
                mk.op("act", lambda e, pz=pz, i2=i2: e.activation(out=rs[i2][:, 0:tn], in_=pz[:, 0:tn], func=AF.Sqrt,
                                                                 bias=self.eps_t[:, 0:1]), reads=[pz, self.eps_t], writes=[rs[i2]])
                mk.op("dve", lambda e, i2=i2: e.reciprocal(rs[i2][:, 0:tn], rs[i2][:, 0:tn]), reads=[rs[i2]], writes=[rs[i2]])
                mk.op("dve", lambda e, ps=ps, i2=i2, gcol=gcol: e.scalar_tensor_tensor(
                    out=qn[i2][:, 0:tn], in0=ps[:, 0:tn], scalar=gains[:, gcol:gcol + 1], in1=rs[i2][:, 0:tn],
                    op0=ALU.mult, op1=ALU.mult), reads=[ps, gains, rs[i2]], writes=[qn[i2]])
                if isq:
                    dst_t = qo[n % 3]
                    dst = dst_t[:, 0:tn]
                else:
                    dst_t = KT
                    dst = KT[:, hh - 8, t0:t0 + tn]
                if bi == 0:
                    mk.op("act", lambda e, i2=i2, dst=dst: e.copy(dst, qn[i2][:, 0:tn]), reads=[qn[i2]], writes=[dst_t])
                else:
                    mk.op("act", lambda e, i2=i2: e.copy(qb[i2][:, 0:tn], qn[i2][:, 0:tn]), reads=[qn[i2]], writes=[qb[i2]])
                    mk.op("pe", lambda e, pr=pr, i2=i2: e.matmul(pr[:, 0:tn], RT[:], qb[i2][:, 0:tn], start=True, stop=True),
                          reads=[RT, qb[i2]], writes=[pr])
                    mk.op("pool", lambda e, i2=i2, c=c: e.tensor_tensor(out=t1[i2][:, 0:tn], in0=qn[i2][:, 0:tn],
                                                                       in1=c[:, 0, 0:tn], op=ALU.mult),
                          reads=[qn[i2], c], writes=[t1[i2]])
                    mk.op("dve", lambda e, pr=pr, i2=i2, c=c: e.tensor_tensor(out=t2[i2][:, 0:tn], in0=pr[:, 0:tn],
                                                                             in1=c[:, 1, 0:tn], op=ALU.mult),
                          reads=[pr, c], writes=[t2[i2]])
                    mk.op("pool", lambda e, i2=i2, dst=dst: e.tensor_tensor(out=dst, in0=t1[i2][:, 0:tn],
                                                                           in1=t2[i2][:, 0:tn], op=ALU.add),
                          reads=[t1[i2], t2[i2]], writes=[dst_t])
                if isq:
                    mk.dma("act", QT[hh * 128:(hh + 1) * 128, t0:t0 + tn], dst, reads=[dst_t], writes=[QT])
            for a in range(tn // 128):
                pv = self.ps[6 + a % 2]
                for k in range(DC):
                    mk.op("pe", lambda e, pv=pv, k=k, a=a, h=h: e.matmul(
                        pv[:, 0:256], h[:, k, a * 128:(a + 1) * 128], wkv[:, k, 256:512],
                        start=(k == 0), stop=(k == DC - 1)), reads=[h, wkv], writes=[pv])
                ti = t0 // 128 + a
                mk.op("act", lambda e, pv=pv, ti=ti: e.copy(Vs[:, ti, :], pv[:, 0:256]), reads=[pv], writes=[Vs])
        mk.release(m0)
        m0 = mk.mark()
        qblk = [mk.sbuf("bq%d" % k, [128, 512], BF16) for k in range(2)]
        pT = [mk.sbuf("bp%d" % k, [128, 512], BF16) for k in range(3)]
        rz = [mk.sbuf("brz%d" % k, [128, 512], F32) for k in range(2)]
        zacc = [mk.sbuf("bza%d" % k, [128, 512], F32) for k in range(2)]
        ones_f = mk.sbuf("bones_f", [128, 128], F32)
        mk.op("pool", lambda e: e.memset(ones_f[:], 1.0), writes=[ones_f])
        ob = [mk.sbuf("bo%d" % k, [128, 512], BF16) for k in range(2)]
        scale = 128 ** -0.5
        nb = 0
        nk = 0
        for head in range(8):
            kvh = head // 4
            for bi, (t0, tn) in enumerate(BLOCKS):
                if bi == 0 and not with_ctx:
                    continue
                nkc = 2 if bi == 0 else NT // 128
                q = qblk[nb % 2]
                mk.dma("sp", q[:, 0:tn], QT[head * 128:(head + 1) * 128, t0:t0 + tn], reads=[QT], writes=[q])
                pO = self.ps[3 + nb % 2]
                pZ = self.ps[5 + nb % 2]
                def issue_S(kc_, idx):
                    pS_ = self.ps[idx % 3]
                    mk.op("pe", lambda e, pS_=pS_, kc_=kc_, q=q: e.matmul(
                        pS_[:, 0:tn], KT[:, kvh, kc_ * 128:(kc_ + 1) * 128], q[:, 0:tn], start=True, stop=True),
                        reads=[KT, q], writes=[pS_])
                issue_S(0, nk)
                for kc in range(nkc):
                    pS = self.ps[nk % 3]
                    p_ = pT[nk % 3]
                    if kc + 1 < nkc:
                        issue_S(kc + 1, nk + 1)
                    nk += 1
                    mk.op("act", lambda e, pS=pS, p_=p_: e.activation(out=p_[:, 0:tn], in_=pS[:, 0:tn], func=AF.Exp,
                                                                     scale=scale), reads=[pS], writes=[p_])
                    mk.op("pe", lambda e, pO=pO, kc=kc, p_=p_: e.matmul(
                        pO[:, 0:tn], Vs[:, kc, kvh * 128:(kvh + 1) * 128], p_[:, 0:tn],
                        start=(kc == 0), stop=(kc == nkc - 1)), reads=[Vs, p_], writes=[pO])
                    mk.op("pe", lambda e, pZ=pZ, kc=kc, p_=p_: e.matmul(
                        pZ[:, 0:tn], self.ones_b[:], p_[:, 0:tn], start=(kc == 0), stop=(kc == nkc - 1)),
                        reads=[self.ones_b, p_], writes=[pZ])
                r_ = rz[nb % 2]
                o_ = ob[nb % 2]
                mk.op("dve", lambda e, pZ=pZ, r_=r_: e.reciprocal(r_[:, 0:tn], pZ[:, 0:tn]), reads=[pZ], writes=[r_])
                mk.op("dve", lambda e, pO=pO, r_=r_, o_=o_: e.tensor_tensor(out=o_[:, 0:tn], in0=pO[:, 0:tn],
                                                                           in1=r_[:, 0:tn], op=ALU.mult),
                      reads=[pO, r_], writes=[o_])
                mk.dma("act", OT[head * 128:(head + 1) * 128, t0:t0 + tn], o_[:, 0:tn], reads=[o_], writes=[OT])
                nb += 1
        mk.release(m0)
        mk.release(mA)
        m0 = mk.mark()
        wo = self.load_w("wo", I["attn_w_o"][slot], DC, D)
        self.linear_resid(XT, OT, wo, DC, "g1", skip_ctx=not with_ctx)
        mk.release(m0)

    Prog.load_w = load_w
    Prog.linear_resid = linear_resid
    Prog.stage_attn = stage_attn


_attn_methods()


def _conf_methods():
    def stage_conf(self, slot, XT, HT, UT, VT, I):
        mk = self.mk
        m0 = mk.mark()
        W = self.load_w("wpw1", I["conv_w_pw1"][slot], DC, 2 * D)
        b1 = mk.sbuf("cb1", [128, 16], F32)
        mk.dma("sp", b1[:], I["conv_b_pw1_pc"][slot, :, :], writes=[b1])
        hb = [mk.sbuf("ch%d" % k, [128, DC, 512], BF16) for k in range(2)]
        sg = [mk.sbuf("csg%d" % k, [128, 512], F32) for k in range(2)]
        ub = [mk.sbuf("cu%d" % k, [128, DC, 512], BF16) for k in range(2)]
        n = 0
        for bi, (t0, tn) in enumerate(BLOCKS):
            h = hb[bi % 2]
            u = ub[bi % 2]
            mk.dma("sp", h[:, :, 0:tn], HT[:, t0:t0 + tn].rearrange("(k p) t -> p k t", p=128), reads=[HT], writes=[h])
            for j in range(DC):
                pa, pg = self.ps[(2 * n) % 8], self.ps[(2 * n + 1) % 8]
                s_ = sg[n % 2]
                n += 1
                for k in range(DC):
                    mk.op("pe", lambda e, pa=pa, k=k, j=j, h=h: e.matmul(
                        pa[:, 0:tn], W[:, k, j * 128:(j + 1) * 128], h[:, k, 0:tn], start=(k == 0), stop=(k == DC - 1)),
                        reads=[W, h], writes=[pa])
                for k in range(DC):
                    mk.op("pe", lambda e, pg=pg, k=k, j=j, h=h: e.matmul(
                        pg[:, 0:tn], W[:, k, D + j * 128:D + (j + 1) * 128], h[:, k, 0:tn], start=(k == 0), stop=(k == DC - 1)),
                        reads=[W, h], writes=[pg])
                mk.op("act", lambda e, pg=pg, s_=s_, j=j: e.activation(out=s_[:, 0:tn], in_=pg[:, 0:tn], func=AF.Sigmoid,
                                                                      bias=b1[:, 8 + j:9 + j]), reads=[pg, b1], writes=[s_])
                mk.op("dve", lambda e, pa=pa, s_=s_, j=j, u=u: e.scalar_tensor_tensor(
                    out=u[:, j, 0:tn], in0=pa[:, 0:tn], scalar=b1[:, j:j + 1], in1=s_[:, 0:tn], op0=ALU.add, op1=ALU.mult),
                    reads=[pa, b1, s_], writes=[u])
            mk.dma("act", UT[:, t0:t0 + tn].rearrange("(k p) t -> p k t", p=128), u[:, :, 0:tn], reads=[u], writes=[UT])
        mk.release(m0)
        m0 = mk.mark()
        wdw = mk.sbuf("cwdw", [128, DC, 31], F32)
        bdw = mk.sbuf("cbdw", [128, DC], F32)
        mk.dma("sp", wdw[:], I["conv_w_dw_pc"][slot, :, :, :], writes=[wdw])
        mk.dma("sp", bdw[:], I["conv_b_dw_pc"][slot, :, :], writes=[bdw])
        up = [mk.sbuf("cup%d" % k, [128, NT + 60], BF16) for k in range(2)]
        dg = [mk.sbuf("cdg%d" % k, [128, 31, 128], BF16) for k in range(2)]
        vo = [mk.sbuf("cvo%d" % k, [128, 512], F32) for k in range(3)]
        for k in range(2):
            mk.op("pool", lambda e, k=k: e.memset(up[k][:], 0.0), writes=[up[k]])
        n = 0
        for j in range(DC):
            u = up[j % 2]
            d_ = dg[j % 2]
            mk.dma("sp", u[:, 15:15 + CTX], UT[j * 128:(j + 1) * 128, 0:CTX], reads=[UT], writes=[u])
            mk.dma("sp", u[:, 45 + CTX:45 + CTX + SEQ], UT[j * 128:(j + 1) * 128, CTX:NT], reads=[UT], writes=[u])
            for k in range(31):
                mk.op("dve", lambda e, k=k, j=j, d_=d_: e.tensor_scalar(
                    out=d_[:, k, :], in0=self.ident_b[:], scalar1=wdw[:, j, k:k + 1], scalar2=None, op0=ALU.mult),
                    reads=[self.ident_b, wdw], writes=[d_])
            for bi, (t0, tn) in enumerate(BLOCKS):
                base = 0 if bi == 0 else 30
                ps = self.ps[n % 4]
                v = vo[n % 3]
                n += 1
                for k in range(31):
                    mk.op("pe", lambda e, ps=ps, k=k, u=u, d_=d_, s0=base + t0 + k: e.matmul(
                        ps[:, 0:tn], d_[:, k, :], u[:, s0:s0 + tn], start=(k == 0), stop=(k == 30)),
                        reads=[d_, u], writes=[ps])
                mk.op("act", lambda e, ps=ps, v=v, j=j: e.activation(out=v[:, 0:tn], in_=ps[:, 0:tn], func=AF.Identity,
                                                                    bias=bdw[:, j:j + 1]), reads=[ps, bdw], writes=[v])
                mk.dma("act", VT[j * 128:(j + 1) * 128, t0:t0 + tn], v[:, 0:tn], reads=[v], writes=[VT])
        mk.release(m0)
        m0 = mk.mark()
        lng = mk.sbuf("clng", [128, DC], F32)
        lnb = mk.sbuf("clnb", [128, DC], F32)
        mk.dma("sp", lng[:], I["conv_ln_g_pc"][slot, :, :], writes=[lng])
        mk.dma("sp", lnb[:], I["conv_ln_b_pc"][slot, :, :], writes=[lnb])
        avgf = mk.sbuf("cavgf", [128, 128], F32)
        mk.op("pool", lambda e: e.memset(avgf[:], 1.0 / D), writes=[avgf])
        vb = [mk.sbuf("cv%d" % k, [128, DC, 512], F32) for k in range(2)]
        sq = mk.sbuf("csq", [128, DC, 512], F32)
        mu = mk.sbuf("cmu", [128, 512], F32)
        var = mk.sbuf("cvar", [128, 512], F32)
        ob = [mk.sbuf("co%d" % k, [128, DC, 512], BF16) for k in range(2)]
        for bi, (t0, tn) in enumerate(BLOCKS):
            v = vb[bi % 2]
            o = ob[bi % 2]
            mk.dma("sp", v[:, :, 0:tn], VT[:, t0:t0 + tn].rearrange("(k p) t -> p k t", p=128), reads=[VT], writes=[v])
            pm, pq = self.ps[0], self.ps[1]
            for j in range(DC):
                mk.op("act", lambda e, j=j, v=v: e.activation(out=sq[:, j, 0:tn], in_=v[:, j, 0:tn], func=AF.Square),
                      reads=[v], writes=[sq])
            for j in range(DC):
                mk.op("pe", lambda e, j=j, v=v: e.matmul(pm[:, 0:tn], avgf[:], v[:, j, 0:tn], start=(j == 0), stop=(j == DC - 1)),
                      reads=[avgf, v], writes=[pm])
            for j in range(DC):
                mk.op("pe", lambda e, j=j: e.matmul(pq[:, 0:tn], avgf[:], sq[:, j, 0:tn], start=(j == 0), stop=(j == DC - 1)),
                      reads=[avgf, sq], writes=[pq])
            mk.op("act", lambda e: e.copy(mu[:, 0:tn], pm[:, 0:tn]), reads=[pm], writes=[mu])
            mk.op("dve", lambda e: e.tensor_tensor(out=var[:, 0:tn], in0=mu[:, 0:tn], in1=mu[:, 0:tn], op=ALU.mult),
                  reads=[mu], writes=[var])
            mk.op("dve", lambda e: e.tensor_tensor(out=var[:, 0:tn], in0=pq[:, 0:tn], in1=var[:, 0:tn], op=ALU.subtract),
                  reads=[pq, var], writes=[var])
            mk.op("act", lambda e: e.activation(out=var[:, 0:tn], in_=var[:, 0:tn], func=AF.Sqrt, bias=self.eps_t[:, 0:1]),
                  reads=[var, self.eps_t], writes=[var])
            mk.op("dve", lambda e: e.reciprocal(var[:, 0:tn], var[:, 0:tn]), reads=[var], writes=[var])
            for j in range(DC):
                mk.op("pool", lambda e, j=j, v=v: e.tensor_tensor(out=v[:, j, 0:tn], in0=v[:, j, 0:tn], in1=mu[:, 0:tn],
                                                                 op=ALU.subtract), reads=[v, mu], writes=[v])
                mk.op("dve", lambda e, j=j, v=v: e.scalar_tensor_tensor(
                    out=v[:, j, 0:tn], in0=v[:, j, 0:tn], scalar=lng[:, j:j + 1], in1=var[:, 0:tn], op0=ALU.mult, op1=ALU.mult),
                    reads=[v, lng, var], writes=[v])
                mk.op("act", lambda e, j=j, v=v, o=o: e.activation(out=o[:, j, 0:tn], in_=v[:, j, 0:tn], func=AF.Silu,
                                                                  bias=lnb[:, j:j + 1]), reads=[v, lnb], writes=[o])
            mk.dma("act", HT[:, t0:t0 + tn].rearrange("(k p) t -> p k t", p=128), o[:, :, 0:tn], reads=[o], writes=[HT])
        mk.release(m0)
        m0 = mk.mark()
        w2 = self.load_w("wpw2", I["conv_w_pw2"][slot], DC, D)
        b2 = mk.sbuf("cb2", [128, DC], F32)
        mk.dma("sp", b2[:], I["conv_b_pw2_pc"][slot, :, :], writes=[b2])
        self.linear_resid(XT, HT, w2, DC, "g1", bias=b2)
        mk.release(m0)

    Prog.stage_conf = stage_conf


_conf_methods()


def hy_consts(L):
    N = 2 * L
    N1 = N // 64
    H = N1 // 2
    bf = ml_dtypes.bfloat16
    c = {}
    n1 = np.arange(H)[:, None].astype(np.float64)
    k1 = np.arange(N1)[None, :].astype(np.float64)
    def cs(ang):
        return np.cos(ang), -np.sin(ang)
    fc, fs = cs(2 * np.pi * n1 * k1 / N1)
    bc, bs = cs(2 * np.pi * (N1 - 1 - n1) * k1 / N1)
    b0c, b0s = cs(2 * np.pi * (N1 - n1) * k1 / N1)
    c["F1"] = np.stack([fc, fs, bc, bs, b0c, b0s], 1).astype(bf)
    n2 = np.arange(64)[:, None].astype(np.float64)
    k2 = np.arange(64)[None, :].astype(np.float64)
    G = np.zeros((N1, 128, 5, 128), np.float64)
    for kk in range(N1):
        ang = 2 * np.pi * (n2 * kk / N + n2 * k2 / 64)
        Gr, Gi = np.cos(ang), -np.sin(ang)
        Mr, Mi = Gr.T, -Gi.T
        blocks = [((Gr, Gi), (-Gi, Gr)), ((-Gi, Gr), (-Gr, -Gi)), ((Gr, Gr), (-Gi, -Gi)),
                  ((Gi, Gi), (Gr, Gr)), ((Mr, Mi), (-Mi, Mr))]
        for f, ((a, b), (cc, d)) in enumerate(blocks):
            G[kk, 0:64, f, 0:64] = a
            G[kk, 0:64, f, 64:128] = b
            G[kk, 64:128, f, 0:64] = cc
            G[kk, 64:128, f, 64:128] = d
    c["G"] = np.ascontiguousarray(G.transpose(1, 0, 2, 3)).astype(bf)
    t1 = np.arange(H)[None, :].astype(np.float64)
    kk1 = np.arange(N1)[:, None].astype(np.float64)
    th = 2 * np.pi * t1 * kk1 / N1
    c["Fi"] = np.stack([np.cos(th) / N, -np.sin(th) / N], 1).astype(bf)
    pos = (64 * np.arange(H)[:, None] + np.arange(64)[None, :]).astype(np.float32)
    c["lneg"] = (-(pos / np.float32(L - 1))).astype(np.float32)
    p = np.arange(L, dtype=np.float32)[:, None]
    t = p / np.float32(L - 1)
    w = np.float32(2.0 * math.pi) * p / np.float32(L)
    bands = np.linspace(1e-4, 7, 8, dtype=np.float32)
    feats = np.concatenate([t, np.cos(bands * w), -np.sin(bands * w)], axis=-1).astype(np.float32)
    c["featsT"] = np.ascontiguousarray(feats.T)
    return c


def _hy_methods():
    def hy_filter(self, S, I, HH):
        mk = self.mk
        L, N1, H, tag = S["L"], S["N1"], S["H"], S["tag"]
        m0 = mk.mark()
        ft = mk.sbuf("hf_ft", [17, L], F32)
        W1 = mk.sbuf("hf_w1", [17, 64], F32)
        W2 = mk.sbuf("hf_w2", [64, 64], F32)
        W3 = mk.sbuf("hf_w3", [64, 4096], BF16)
        fv = mk.sbuf("hf_fv", [64, 4], F32)
        fb = mk.sbuf("hf_fb", [64, 2], F32)
        z1 = mk.sbuf("hf_z1", [64, L], F32)
        z2 = mk.sbuf("hf_z2", [64, L], F32)
        z2b = mk.sbuf("hf_z2b", [64, L], BF16)
        tmp = mk.sbuf("hf_tmp", [64, 512], F32)
        F1 = mk.sbuf("hf_F1", [max(H, 1), 6, N1], BF16)
        lneg = mk.sbuf("hf_lneg", [H, 64], F32)
        dab = mk.sbuf("hf_dab", [H, D], F32)
        mk.dma("sp", ft[:], I["hy_featsT_" + tag][:, :], writes=[ft])
        mk.dma("sp", W1[:], I["hy_f_w1"][0, :, :], writes=[W1])
        mk.dma("sp", W2[:], I["hy_f_w2"][0, :, :], writes=[W2])
        mk.dma("pool", W3[:], I["hy_f_w3"][0, :, :], writes=[W3])
        mk.dma("sp", fv[:], I["hy_fvec"][:, :], writes=[fv])
        mk.dma("sp", F1[:], I["hy_F1_" + tag][:, :, :], writes=[F1])
        mk.dma("sp", lneg[:], I["hy_lneg_" + tag][:, :], writes=[lneg])
        mk.dma("sp", dab[:], I["hy_dabs"][0:H, :], writes=[dab])
        mk.op("dve", lambda e: e.tensor_tensor(out=fb[:, 0:1], in0=fv[:, 0:1], in1=fv[:, 1:2], op=ALU.mult), reads=[fv], writes=[fb])
        mk.op("dve", lambda e: e.tensor_tensor(out=fb[:, 1:2], in0=fv[:, 2:3], in1=fv[:, 3:4], op=ALU.mult), reads=[fv], writes=[fb])
        for layer, (Wm, src, dst, kin) in enumerate(((W1, ft, z1, 17), (W2, z1, z2, 64))):
            for c0 in range(0, L, 512):
                cn = min(512, L - c0)
                ps = self.ps[(c0 // 512) % 2]
                mk.op("pe", lambda e, ps=ps, Wm=Wm, src=src, c0=c0, cn=cn, kin=kin: e.matmul(
                    ps[0:64, 0:cn], Wm[0:kin, :], src[0:kin, c0:c0 + cn], start=True, stop=True), reads=[Wm, src], writes=[ps])
                d_ = dst[:, c0:c0 + cn]
                mk.op("dve", lambda e, ps=ps, d_=d_, cn=cn, layer=layer: e.tensor_scalar(
                    out=d_, in0=ps[0:64, 0:cn], scalar1=fv[:, 2 * layer + 1:2 * layer + 2], scalar2=fb[:, layer:layer + 1],
                    op0=ALU.mult, op1=ALU.add), reads=[ps, fv, fb], writes=[dst])
                for _ in range(2):
                    mk.op("dve", lambda e, d_=d_, cn=cn: e.tensor_scalar(out=tmp[:, 0:cn], in0=d_, scalar1=math.pi,
                          scalar2=2 * math.pi, op0=ALU.is_gt, op1=ALU.mult), reads=[dst], writes=[tmp])
                    mk.op("dve", lambda e, d_=d_, cn=cn: e.tensor_tensor(out=d_, in0=d_, in1=tmp[:, 0:cn], op=ALU.subtract),
                          reads=[dst, tmp], writes=[dst])
                    mk.op("dve", lambda e, d_=d_, cn=cn: e.tensor_scalar(out=tmp[:, 0:cn], in0=d_, scalar1=-math.pi,
                          scalar2=2 * math.pi, op0=ALU.is_lt, op1=ALU.mult), reads=[dst], writes=[tmp])
                    mk.op("dve", lambda e, d_=d_, cn=cn: e.tensor_tensor(out=d_, in0=d_, in1=tmp[:, 0:cn], op=ALU.add),
                          reads=[dst, tmp], writes=[dst])
                mk.op("act", lambda e, d_=d_: e.activation(out=d_, in_=d_, func=AF.Sin), reads=[dst], writes=[dst])
        mk.op("act", lambda e: e.copy(z2b[:], z2[:]), reads=[z2], writes=[z2b])
        dec = [mk.sbuf("hf_dec%d" % k, [H, D], F32) for k in range(4)]
        hd = [mk.sbuf("hf_hd%d" % k, [H, D], F32) for k in range(4)]
        hdb = [mk.sbuf("hf_hdb%d" % k, [H, D], BF16) for k in range(4)]
        ha = [mk.sbuf("hf_ha%d" % k, [H, D], BF16) for k in range(4)]
        Asb = [mk.sbuf("hf_A%d" % k, [N1, 2, D], BF16) for k in range(2)]
        rn = mk.sbuf("hf_rn", [128, D], F32)
        A = self.dram["hyA"]
        Bt = [mk.sbuf("hf_B%d" % k, [128, D], BF16) for k in range(2)]
        Gt = [mk.sbuf("hf_Gt%d" % k, [128, 2, 128], BF16) for k in range(2)]
        Ho = [mk.sbuf("hf_Ho%d" % k, [128, D], BF16) for k in range(4)]
        for o in range(2):
            pn = (self.ps[6], self.ps[7])
            for n2 in range(64):
                n2b = (64 - n2) % 64
                hb2 = []
                for dr in range(2):
                    col = n2 if dr == 0 else n2b
                    i2 = dr + 2 * (n2 % 2)
                    for hf in range(2):
                        ph = self.ps[dr * 2 + hf]
                        w0 = (dr * 2 + o) * D + hf * 512
                        mk.op("pe", lambda e, ph=ph, col=col, w0=w0: e.matmul(
                            ph[0:H, :], z2b[:, col:L:64], W3[:, w0:w0 + 512], start=True, stop=True),
                            reads=[z2b, W3], writes=[ph])
                    mk.op("act", lambda e, i2=i2, col=col: e.activation(out=dec[i2][:], in_=dab[:], func=AF.Exp,
                                                                       scale=lneg[:, col:col + 1]), reads=[dab, lneg], writes=[dec[i2]])
                    for hf in range(2):
                        ph = self.ps[dr * 2 + hf]
                        mk.op("dve", lambda e, ph=ph, i2=i2, hf=hf: e.tensor_tensor(
                            out=hd[i2][:, hf * 512:(hf + 1) * 512], in0=ph[0:H, :], in1=dec[i2][:, hf * 512:(hf + 1) * 512],
                            op=ALU.mult), reads=[ph, dec[i2]], writes=[hd[i2]])
                    if dr == 1 and n2 == 0:
                        mk.op("dve", lambda e, i2=i2: e.memset(hd[i2][0:1, :], 0.0), reads=[hd[i2]], writes=[hd[i2]])
                    hb_ = hdb[(n2 % 2) * 2 + dr]
                    hb2.append(hb_)
                    mk.op("act", lambda e, i2=i2, hb_=hb_: e.copy(hb_[:], hd[i2][:]), reads=[hd[i2]], writes=[hb_])
                    mk.op("dve", lambda e, i2=i2: e.scalar_tensor_tensor(out=ha[i2][:], in0=hd[i2][:], scalar=-1.0, in1=hd[i2][:],
                                                                        op0=ALU.mult, op1=ALU.max), reads=[hd[i2]], writes=[ha[i2]])
                    for hf in range(2):
                        first = (n2 == 0 and dr == 0)
                        last = (n2 == 63 and dr == 1)
                        mk.op("pe", lambda e, i2=i2, hf=hf, first=first, last=last: e.matmul(
                            pn[hf][:, :], self.ones_b[0:H, :], ha[i2][:, hf * 512:(hf + 1) * 512], start=first, stop=last),
                            reads=[self.ones_b, ha[i2]], writes=[pn[hf]])
                As = Asb[n2 % 2]
                fbi = 4 if n2 == 0 else 2
                for ri in range(2):
                    for hf in range(2):
                        pA = self.ps[4 + hf]
                        mk.op("pe", lambda e, pA=pA, ri=ri, hf=hf, hb2=hb2: e.matmul(
                            pA[0:N1, :], F1[0:H, ri, :], hb2[0][:, hf * 512:(hf + 1) * 512], start=True, stop=False),
                            reads=[F1, hb2[0]], writes=[pA])
                        mk.op("pe", lambda e, pA=pA, ri=ri, hf=hf, hb2=hb2, fbi=fbi: e.matmul(
                            pA[0:N1, :], F1[0:H, fbi + ri, :], hb2[1][:, hf * 512:(hf + 1) * 512], start=False, stop=True),
                            reads=[F1, hb2[1]], writes=[pA])
                        eng = "act" if hf else "dve"
                        if eng == "act":
                            mk.op("act", lambda e, pA=pA, ri=ri, hf=hf, As=As: e.copy(As[:, ri, hf * 512:(hf + 1) * 512], pA[0:N1, :]),
                                  reads=[pA], writes=[As])
                        else:
                            mk.op("dve", lambda e, pA=pA, ri=ri, hf=hf, As=As: e.tensor_copy(As[:, ri, hf * 512:(hf + 1) * 512], pA[0:N1, :]),
                                  reads=[pA], writes=[As])
                for ri in range(2):
                    mk.dma("act", A[ri, 0:N1, n2, :], As[:, ri, :], reads=[As], writes=[A])
            for hf in range(2):
                mk.op("dve", lambda e, hf=hf: e.reciprocal(rn[:, hf * 512:(hf + 1) * 512], pn[hf][:, :]), reads=[pn[hf]], writes=[rn])
            for kk in range(N1):
                B = Bt[kk % 2]
                for ri in range(2):
                    mk.dma("sp", B[ri * 64:(ri + 1) * 64, :], A[ri, kk, :, :], reads=[A], writes=[B])
                g_ = Gt[kk % 2]
                mk.dma("sp", g_[:], I["hy_G_" + tag][:, kk, 2:4, :], writes=[g_])
                for ri in range(2):
                    ho = Ho[(2 * kk + ri) % 4]
                    for hf in range(2):
                        pX = self.ps[ri * 2 + hf]
                        mk.op("pe", lambda e, pX=pX, ri=ri, hf=hf, B=B, g_=g_: e.matmul(
                            pX[:, :], g_[:, ri, :], B[:, hf * 512:(hf + 1) * 512], start=True, stop=True), reads=[g_, B], writes=[pX])
                        mk.op("dve", lambda e, pX=pX, hf=hf, ho=ho: e.tensor_tensor(
                            out=ho[:, hf * 512:(hf + 1) * 512], in0=pX[:, :], in1=rn[:, hf * 512:(hf + 1) * 512], op=ALU.mult),
                            reads=[pX, rn], writes=[ho])
                    mk.dma("act", HH[o][ri, kk, :, :], ho[:], reads=[ho], writes=[HH[o]])
        mk.release(m0)

    Prog.hy_filter = hy_filter


_hy_methods()


def _hy_methods2():
    def hy_conv(self, S, o, I, zsrc, zrow0, gate, grow0, lbias, HH, dst, dst_bf16):
        mk = self.mk
        L, N1, H, tag, col0 = S["L"], S["N1"], S["H"], S["tag"], S["col0"]
        A, Cs = self.dram["hyA"], self.dram["hyC"]
        m0 = mk.mark()
        zb = mk.sbuf("hc_zb", [128, DC, L], BF16)
        F1 = mk.sbuf("hc_F1", [max(H, 1), 6, N1], BF16)
        mk.dma("sp", F1[:], I["hy_F1_" + tag][:, :, :], writes=[F1])
        for j in range(DC):
            mk.dma("pool", zb[:, j, :], zsrc[zrow0 + j * 128:zrow0 + (j + 1) * 128, col0:col0 + L], reads=[zsrc], writes=[zb])
        zT = [mk.sbuf("hc_zT%d" % k, [H, D], BF16) for k in range(2)]
        Asb = [mk.sbuf("hc_A%d" % k, [N1, 2, D], BF16) for k in range(2)]
        for n2 in range(64):
            pt = self.ps[n2 % 2]
            ptb = pt[:, 0:512].bitcast(BF16)
            for j in range(DC):
                mk.op("pe", lambda e, ptb=ptb, j=j, n2=n2: e.transpose(ptb[0:H, j * 128:(j + 1) * 128], zb[:, j, n2:L:64],
                                                                      self.ident_b[:]), reads=[zb, self.ident_b], writes=[pt])
            z_ = zT[n2 % 2]
            mk.op("act", lambda e, ptb=ptb, z_=z_: e.copy(z_[:], ptb[0:H, :]), reads=[pt], writes=[z_])
            As = Asb[n2 % 2]
            for ri in range(2):
                for hf in range(2):
                    pA = self.ps[2 + ri * 2 + hf]
                    mk.op("pe", lambda e, pA=pA, ri=ri, hf=hf, z_=z_: e.matmul(
                        pA[0:N1, :], F1[0:H, ri, :], z_[:, hf * 512:(hf + 1) * 512], start=True, stop=True),
                        reads=[F1, z_], writes=[pA])
                    if hf:
                        mk.op("act", lambda e, pA=pA, ri=ri, hf=hf, As=As: e.copy(As[:, ri, hf * 512:(hf + 1) * 512], pA[0:N1, :]),
                              reads=[pA], writes=[As])
                    else:
                        mk.op("dve", lambda e, pA=pA, ri=ri, hf=hf, As=As: e.tensor_copy(As[:, ri, hf * 512:(hf + 1) * 512], pA[0:N1, :]),
                              reads=[pA], writes=[As])
            for ri in range(2):
                mk.dma("act", A[ri, 0:N1, n2, :], As[:, ri, :], reads=[As], writes=[A])
        mk.release(m0)
        m0 = mk.mark()
        Bt = [mk.sbuf("hc_B%d" % k, [128, D], BF16) for k in range(2)]
        Gt = [mk.sbuf("hc_G%d" % k, [128, 5, 128], BF16) for k in range(2)]
        Hr = [mk.sbuf("hc_Hr%d" % k, [128, D], BF16) for k in range(2)]
        Hi = [mk.sbuf("hc_Hi%d" % k, [128, D], BF16) for k in range(2)]
        ta = [mk.sbuf("hc_ta%d" % k, [128, D], F32) for k in range(2)]
        tb = [mk.sbuf("hc_tb%d" % k, [128, D], F32) for k in range(2)]
        Y = [mk.sbuf("hc_Y%d" % k, [128, D], BF16) for k in range(2)]
        Cb = [mk.sbuf("hc_C%d" % k, [128, D], BF16) for k in range(2)]
        for kk in range(N1):
            i2 = kk % 2
            B, g_, hr, hi = Bt[i2], Gt[i2], Hr[i2], Hi[i2]
            for ri in range(2):
                mk.dma("sp", B[ri * 64:(ri + 1) * 64, :], A[ri, kk, :, :], reads=[A], writes=[B])
            mk.dma("sp", g_[:], I["hy_G_" + tag][:, kk, :, :], writes=[g_])
            mk.dma("sp", hr[:], HH[o][0, kk, :, :], reads=[HH[o]], writes=[hr])
            mk.dma("sp", hi[:], HH[o][1, kk, :, :], reads=[HH[o]], writes=[hi])
            for hf in range(2):
                sl = slice(hf * 512, (hf + 1) * 512)
                pa, pb = self.ps[hf * 2], self.ps[hf * 2 + 1]
                mk.op("pe", lambda e, pa=pa, sl=sl, B=B, g_=g_: e.matmul(pa[:, :], g_[:, 0, :], B[:, sl], start=True, stop=True),
                      reads=[g_, B], writes=[pa])
                mk.op("pe", lambda e, pb=pb, sl=sl, B=B, g_=g_: e.matmul(pb[:, :], g_[:, 1, :], B[:, sl], start=True, stop=True),
                      reads=[g_, B], writes=[pb])
                mk.op("dve", lambda e, pa=pa, sl=sl, hr=hr, i2=i2: e.tensor_tensor(out=ta[i2][:, sl], in0=pa[:, :], in1=hr[:, sl],
                                                                                  op=ALU.mult), reads=[pa, hr], writes=[ta[i2]])
                mk.op("dve", lambda e, pb=pb, sl=sl, hi=hi, i2=i2: e.tensor_tensor(out=tb[i2][:, sl], in0=pb[:, :], in1=hi[:, sl],
                                                                                  op=ALU.mult), reads=[pb, hi], writes=[tb[i2]])
                mk.op("pool", lambda e, sl=sl, i2=i2: e.tensor_tensor(out=Y[i2][:, sl], in0=ta[i2][:, sl], in1=tb[i2][:, sl],
                                                                     op=ALU.add), reads=[ta[i2], tb[i2]], writes=[Y[i2]])
            for hf in range(2):
                sl = slice(hf * 512, (hf + 1) * 512)
                pc = self.ps[4 + (2 * kk + hf) % 4]
                mk.op("pe", lambda e, pc=pc, sl=sl, i2=i2, g_=g_: e.matmul(pc[:, :], g_[:, 4, :], Y[i2][:, sl], start=True, stop=True),
                      reads=[g_, Y[i2]], writes=[pc])
                mk.op("act", lambda e, pc=pc, sl=sl, i2=i2: e.copy(Cb[i2][:, sl], pc[:, :]), reads=[pc], writes=[Cb[i2]])
            for ri in range(2):
                mk.dma("act", Cs[ri, :, kk, :], Cb[i2][ri * 64:(ri + 1) * 64, :], reads=[Cb[i2]], writes=[Cs])
        mk.release(m0)
        m0 = mk.mark()
        Fi = mk.sbuf("hc_Fi", [N1, 2, max(H, 1)], BF16)
        mk.dma("sp", Fi[:], I["hy_Fi_" + tag][:, :, :], writes=[Fi])
        lb = mk.sbuf("hc_lb", [128, DC], F32)
        mk.dma("sp", lb[:], lbias, writes=[lb])
        yh = mk.sbuf("hc_yh", [128, 4, L], F32)
        Ct = [mk.sbuf("hc_Ct%d" % k, [N1, 2, 512], BF16) for k in range(3)]
        zr = [mk.sbuf("hc_zr%d" % k, [128, L], F32) for k in range(2)]
        gr = [mk.sbuf("hc_gr%d" % k, [128, L], F32) for k in range(2)]
        ob = [mk.sbuf("hc_ob%d" % k, [128, L], BF16 if dst_bf16 else F32) for k in range(2)]
        per_bank = max(1, 512 // (4 * H))
        for half in range(2):
            for t2 in range(64):
                c_ = Ct[t2 % 3]
                for ri in range(2):
                    mk.dma("sp", c_[:, ri, :], Cs[ri, t2, 0:N1, half * 512:(half + 1) * 512], reads=[Cs], writes=[c_])
                slot = t2 % per_bank
                py = self.ps[(t2 // per_bank) % 8]
                for c4 in range(4):
                    o0 = slot * 4 * H + c4 * H
                    mk.op("pe", lambda e, py=py, o0=o0, c4=c4, c_=c_: e.matmul(
                        py[:, o0:o0 + H], c_[:, 0, c4 * 128:(c4 + 1) * 128], Fi[:, 0, :], start=True, stop=False),
                        reads=[c_, Fi], writes=[py])
                    mk.op("pe", lambda e, py=py, o0=o0, c4=c4, c_=c_: e.matmul(
                        py[:, o0:o0 + H], c_[:, 1, c4 * 128:(c4 + 1) * 128], Fi[:, 1, :], start=False, stop=True),
                        reads=[c_, Fi], writes=[py])
                src = py[:, slot * 4 * H:(slot + 1) * 4 * H].rearrange("p (a b) -> p a b", b=H)
                if t2 % 2:
                    mk.op("act", lambda e, src=src, t2=t2: e.copy(yh[:, :, t2:L:64], src), reads=[py], writes=[yh])
                else:
                    mk.op("dve", lambda e, src=src, t2=t2: e.tensor_copy(yh[:, :, t2:L:64], src), reads=[py], writes=[yh])
            for c4 in range(4):
                cj = half * 4 + c4
                z_, g_, o_ = zr[c4 % 2], gr[c4 % 2], ob[c4 % 2]
                mk.dma("sp", z_[:], zsrc[zrow0 + cj * 128:zrow0 + (cj + 1) * 128, col0:col0 + L], reads=[zsrc], writes=[z_])
                mk.dma("sp", g_[:], gate[grow0 + cj * 128:grow0 + (cj + 1) * 128, col0:col0 + L], reads=[gate], writes=[g_])
                mk.op("dve", lambda e, z_=z_, cj=cj, c4=c4: e.scalar_tensor_tensor(
                    out=z_[:], in0=z_[:], scalar=lb[:, cj:cj + 1], in1=yh[:, c4, :], op0=ALU.mult, op1=ALU.add),
                    reads=[z_, lb, yh], writes=[z_])
                mk.op("pool", lambda e, z_=z_, g_=g_, o_=o_: e.tensor_tensor(out=o_[:], in0=z_[:], in1=g_[:], op=ALU.mult),
                      reads=[z_, g_], writes=[o_])
                mk.dma("act", dst[cj * 128:(cj + 1) * 128, col0:col0 + L], o_[:], reads=[o_], writes=[dst])
        mk.release(m0)

    def stage_hyena(self, slot, XT, HT, I):
        mk = self.mk
        U0, ZT, Z1 = self.dram["hyU0"], self.dram["hyZ"], self.dram["hyZ1"]
        m0 = mk.mark()
        W = self.load_w("hy_win", I["hy_w_in"][slot], DC, 3 * D)
        bi_ = mk.sbuf("hy_bin", [128, 24], F32)
        mk.dma("sp", bi_[:], I["hy_b_in_pc"][:, :], writes=[bi_])
        hb = [mk.sbuf("hy_h%d" % k, [128, DC, 512], BF16) for k in range(2)]
        uo = [mk.sbuf("hy_uo%d" % k, [128, 512], F32) for k in range(3)]
        n = 0
        for bi, (t0, tn) in enumerate(BLOCKS):
            h = hb[bi % 2]
            mk.dma("sp", h[:, :, 0:tn], HT[:, t0:t0 + tn].rearrange("(k p) t -> p k t", p=128), reads=[HT], writes=[h])
            for m in range(24):
                ps = self.ps[n % 4]
                u = uo[n % 3]
                n += 1
                for k in range(DC):
                    mk.op("pe", lambda e, ps=ps, k=k, m=m, h=h: e.matmul(
                        ps[:, 0:tn], W[:, k, m * 128:(m + 1) * 128], h[:, k, 0:tn], start=(k == 0), stop=(k == DC - 1)),
                        reads=[W, h], writes=[ps])
                mk.op("act", lambda e, ps=ps, u=u, m=m: e.activation(out=u[:, 0:tn], in_=ps[:, 0:tn], func=AF.Identity,
                                                                    bias=bi_[:, m:m + 1]), reads=[ps, bi_], writes=[u])
                mk.dma("act", U0[m * 128:(m + 1) * 128, t0:t0 + tn], u[:, 0:tn], reads=[u], writes=[U0])
        mk.release(m0)
        m0 = mk.mark()
        ws = mk.sbuf("hy_ws", [128, 24, 3], F32)
        bs = mk.sbuf("hy_bs", [128, 24], F32)
        mk.dma("sp", ws[:], I["hy_w_short_pc"][:, :, :], writes=[ws])
        mk.dma("sp", bs[:], I["hy_b_short_pc"][:, :], writes=[bs])
        W_ = NT + 4
        ub = [mk.sbuf("hy_ub%d" % k, [128, W_], F32) for k in range(2)]
        vb = [mk.sbuf("hy_vb%d" % k, [128, W_], F32) for k in range(2)]
        for k in range(2):
            mk.op("pool", lambda e, k=k: e.memset(ub[k][:], 0.0), writes=[ub[k]])
        for m in range(24):
            u, v = ub[m % 2], vb[m % 2]
            mk.dma("sp", u[:, 1:1 + CTX], U0[m * 128:(m + 1) * 128, 0:CTX], reads=[U0], writes=[u])
            mk.dma("sp", u[:, 3 + CTX:3 + NT], U0[m * 128:(m + 1) * 128, CTX:NT], reads=[U0], writes=[u])
            n_ = W_ - 2
            mk.op("dve", lambda e, u=u, v=v, m=m: e.tensor_scalar(out=v[:, 1:1 + n_], in0=u[:, 0:n_], scalar1=ws[:, m, 0:1],
                                                                 scalar2=bs[:, m:m + 1], op0=ALU.mult, op1=ALU.add),
                  reads=[u, ws, bs], writes=[v])
            mk.op("dve", lambda e, u=u, v=v, m=m: e.scalar_tensor_tensor(out=v[:, 1:1 + n_], in0=u[:, 1:1 + n_], scalar=ws[:, m, 1:2],
                                                                        in1=v[:, 1:1 + n_], op0=ALU.mult, op1=ALU.add),
                  reads=[u, ws, v], writes=[v])
            mk.op("dve", lambda e, u=u, v=v, m=m: e.scalar_tensor_tensor(out=v[:, 1:1 + n_], in0=u[:, 2:2 + n_], scalar=ws[:, m, 2:3],
                                                                        in1=v[:, 1:1 + n_], op0=ALU.mult, op1=ALU.add),
                  reads=[u, ws, v], writes=[v])
            mk.dma("act", ZT[m * 128:(m + 1) * 128, 0:CTX], v[:, 1:1 + CTX], reads=[v], writes=[ZT])
            mk.dma("act", ZT[m * 128:(m + 1) * 128, CTX:NT], v[:, 3 + CTX:3 + NT], reads=[v], writes=[ZT])
        mk.release(m0)
        for S in (dict(L=SEQ, N1=128, H=64, tag="lat", col0=CTX), dict(L=CTX, N1=8, H=4, tag="ctx", col0=0)):
            HH = [self.dram["hyHH_%s%d" % (S["tag"], o)] for o in range(2)]
            self.hy_filter(S, I, HH)
            self.hy_conv(S, 0, I, ZT, 0, ZT, D, I["hy_lbias_pc"][0, :, :], HH, Z1, False)
            self.hy_conv(S, 1, I, Z1, 0, ZT, 2 * D, I["hy_lbias_pc"][1, :, :], HH, HT, True)
        m0 = mk.mark()
        wo = self.load_w("hy_wout", I["hy_w_out"][slot], DC, D)
        bo = mk.sbuf("hy_bo", [128, DC], F32)
        mk.dma("sp", bo[:], I["hy_b_out_pc"][:, :], writes=[bo])
        self.linear_resid(XT, HT, wo, DC, "g1", bias=bo)
        mk.release(m0)

    Prog.hy_conv = hy_conv
    Prog.stage_hyena = stage_hyena


_hy_methods2()


_PROG = {}


def kernel(**inputs):
    inp = {k: np.asarray(v) for k, v in inputs.items()}
    if "p" not in _PROG:
        _PROG["p"] = build_program()
    p = _PROG["p"]
    shared = None
    in_maps = []
    for b in range(8):
        m = host_inputs(inp, b)
        if shared is None:
            shared = m
        else:
            for k in m:
                if k not in ("x", "ctx", "c_pc"):
                    m[k] = shared[k]
        in_maps.append(m)
    res = run_bass_kernel_spmd(p.nc, in_maps, core_ids=list(range(8)))
    out = np.stack([np.asarray(r["y"], dtype=np.float32) for r in res.results], axis=0)
    return out


BSL = 512
BSH = 9
NBLK = 49
NSLOT = NBLK * BSL
NTILE = NT // 128
IOA = bass.IndirectOffsetOnAxis


def _sparse_methods():
    def indirect(mk, out, out_off, in_, in_off, reads, writes):
        qlane = mk.lanes["pool"]
        dl = mk.dma_lanes[mk.dma_rr]
        mk.dma_rr = (mk.dma_rr + 1) % len(mk.dma_lanes)
        need, _ = mk._deps(dl, reads, writes)
        if dl.count > 0:
            need[dl.name] = max(need.get(dl.name, 0), dl.count)
        for ln, ix in need.items():
            if qlane.seen.get(ln, 0) >= ix:
                continue
            qlane.seen[ln] = ix
            qlane.eng.wait_ge(mk.lanes[ln].sem, ix)
        inst = qlane.eng.indirect_dma_start(out=out, out_offset=out_off, in_=in_, in_offset=in_off)
        dl.count += 16
        inst.then_inc(dl.sem, 16)
        mk._record(dl, reads, writes)
        mk.n_inst += 1

    MK.indirect = indirect

    def alloc_sparse(self):
        mk = self.mk
        R = {}
        R["M1a"] = mk.sbuf("sp_M1a", [128, NTILE, 32], F32)
        R["M2a"] = mk.sbuf("sp_M2a", [128, NTILE, 32], F32)
        R["A12"] = mk.sbuf("sp_A12", [128, NTILE, 2], F32)
        R["POSi"] = mk.sbuf("sp_POSi", [128, NTILE, 2], I32)
        R["IDXW"] = mk.sbuf("sp_IDXW", [128, NBLK], I32)
        R["U"] = mk.sbuf("sp_U", [128, 128], BF16)
        R["jg"] = mk.sbuf("sp_jg", [128, NBLK], F32)
        R["pidx"] = mk.sbuf("sp_pidx", [128, 1], F32)
        R["ones32"] = mk.sbuf("sp_ones32", [128, 32], F32)
        mk.op("pool", lambda e: e.memset(R["U"][:], 1.0), writes=[R["U"]])
        mk.op("pool", lambda e: e.affine_select(out=R["U"][:], in_=R["U"][:], pattern=[[1, 128]], compare_op=ALU.is_gt,
                                                fill=0.0, base=0, channel_multiplier=-1), reads=[R["U"]], writes=[R["U"]])
        mk.op("pool", lambda e: e.memset(R["ones32"][:], 1.0), writes=[R["ones32"]])
        return R

    def route_tile_sparse(self, hf, a, tok0, R, rt):
        mk = self.mk
        ps = self.ps[1 + (a % 2)]
        Wr, br = R["Wr"], R["br"]
        ti = tok0 // 128
        for k in range(DC):
            mk.op("pe", lambda e, k=k: e.matmul(ps[:, 0:36], hf[:, k, a * 128:(a + 1) * 128], Wr[:, k, :],
                                                 start=(k == 0), stop=(k == DC - 1)), reads=[hf, Wr], writes=[ps])
        lg, gmax, ngmax, ge, gsum, gp, mg, es = (rt[k] for k in ("lg", "gmax", "ngmax", "ge", "gsum", "gp", "mg", "es"))
        t8, dd, w2, m1, m2 = (rt[k] for k in ("t8", "dd", "w2", "m1", "m2"))
        M1a, M2a, A12 = R["M1a"], R["M2a"], R["A12"]
        V = lambda fn, reads, writes: mk.op("dve", fn, reads=reads, writes=writes)
        V(lambda e: e.tensor_tensor(out=lg[:], in0=ps[:, 0:36], in1=br[:], op=ALU.add), [ps, br], [lg])
        V(lambda e: e.reduce_max(out=gmax[:], in_=lg[:, 0:4], axis=AX.X), [lg], [gmax])
        V(lambda e: e.tensor_scalar(out=ngmax[:], in0=gmax[:], scalar1=-1.0, scalar2=None, op0=ALU.mult), [gmax], [ngmax])
        mk.op("act", lambda e: e.activation(out=ge[:], in_=lg[:, 0:4], func=AF.Exp, bias=ngmax[:, 0:1],
                                            accum_out=gsum[:]), reads=[lg, ngmax], writes=[ge, gsum])
        V(lambda e: e.reciprocal(gp[:], gsum[:]), [gsum], [gp])
        V(lambda e: e.tensor_scalar(out=mg[:], in0=lg[:, 0:4], scalar1=gmax[:, 0:1], scalar2=None, op0=ALU.is_equal),
          [lg, gmax], [mg])
        V(lambda e: e.tensor_scalar(out=es[:], in0=lg[:, 4:12], scalar1=mg[:, 0:1], scalar2=None, op0=ALU.mult),
          [lg, mg], [es])
        for g in range(1, 4):
            V(lambda e, g=g: e.scalar_tensor_tensor(out=es[:], in0=lg[:, 4 + 8 * g:12 + 8 * g], scalar=mg[:, g:g + 1],
                                                    in1=es[:], op0=ALU.mult, op1=ALU.add), [lg, mg, es], [es])
        V(lambda e: e.max(out=t8[:], in_=es[:]), [es], [t8])
        V(lambda e: e.tensor_tensor(out=dd[:], in0=t8[:, 1:2], in1=t8[:, 0:1], op=ALU.subtract), [t8], [dd])
        mk.op("act", lambda e: e.activation(out=w2[:], in_=dd[:], func=AF.Sigmoid), reads=[dd], writes=[w2])
        V(lambda e: e.tensor_tensor(out=A12[:, ti, 1:2], in0=w2[:], in1=gp[:], op=ALU.mult), [w2, gp], [A12])
        V(lambda e: e.tensor_tensor(out=A12[:, ti, 0:1], in0=gp[:], in1=A12[:, ti, 1:2], op=ALU.subtract), [gp, A12], [A12])
        V(lambda e: e.tensor_scalar(out=m1[:], in0=es[:], scalar1=t8[:, 0:1], scalar2=None, op0=ALU.is_equal), [es, t8], [m1])
        V(lambda e: e.tensor_scalar(out=m2[:], in0=es[:], scalar1=t8[:, 1:2], scalar2=None, op0=ALU.is_equal), [es, t8], [m2])
        for g in range(4):
            V(lambda e, g=g: e.tensor_scalar(out=M1a[:, ti, 8 * g:8 * g + 8], in0=m1[:], scalar1=mg[:, g:g + 1],
                                             scalar2=None, op0=ALU.mult), [m1, mg], [M1a])
            V(lambda e, g=g: e.tensor_scalar(out=M2a[:, ti, 8 * g:8 * g + 8], in0=m2[:], scalar1=mg[:, g:g + 1],
                                             scalar2=None, op0=ALU.mult), [m2, mg], [M2a])

    def stage_moe_sparse(self, i, XT, R, I):
        mk = self.mk
        HTOK, HS, YS = self.dram["HTOK"], self.dram["HS"], self.dram["YS"]
        M1a, M2a, A12, POSi, IDXW = R["M1a"], R["M2a"], R["A12"], R["POSi"], R["IDXW"]
        m0 = mk.mark()
        Mb = mk.sbuf("sq_Mb", [128, NTILE, 32], BF16)
        P = mk.sbuf("sq_P", [128, NTILE, 32], F32)
        prod = mk.sbuf("sq_prod", [128, NTILE, 32], F32)
        posf = mk.sbuf("sq_posf", [128, NTILE, 2], F32)
        nf = mk.sbuf("sq_nf", [128, 32], F32)
        ni = mk.sbuf("sq_ni", [128, 32], I32)
        pn = mk.sbuf("sq_pn", [128, 32], F32)
        incl = mk.sbuf("sq_incl", [128, 32], F32)
        excl = mk.sbuf("sq_excl", [128, 32], F32)
        EB = mk.sbuf("sq_EB", [128, NBLK], F32)
        mk.op("dve", lambda e: e.tensor_tensor(out=Mb[:], in0=M1a[:], in1=M2a[:], op=ALU.add), reads=[M1a, M2a], writes=[Mb])
        for ti in range(NTILE):
            pb = self.ps[ti // 12]
            c0 = (ti % 12) * 32
            for tj in range(ti):
                mk.op("pe", lambda e, pb=pb, c0=c0, tj=tj: e.matmul(pb[:, c0:c0 + 32], self.ones_b[:], Mb[:, tj, :],
                                                                    start=(tj == 0), stop=False), reads=[self.ones_b, Mb], writes=[pb])
            mk.op("pe", lambda e, pb=pb, c0=c0, ti=ti: e.matmul(pb[:, c0:c0 + 32], R["U"][:], Mb[:, ti, :],
                                                                start=(ti == 0), stop=True), reads=[R["U"], Mb], writes=[pb])
        pt = self.ps[3]
        for tj in range(NTILE):
            mk.op("pe", lambda e, tj=tj: e.matmul(pt[:, 0:32], self.ones_b[:], Mb[:, tj, :], start=(tj == 0),
                                                  stop=(tj == NTILE - 1)), reads=[self.ones_b, Mb], writes=[pt])
        V = lambda fn, reads, writes: mk.op("dve", fn, reads=reads, writes=writes)
        V(lambda e: e.tensor_scalar(out=nf[:], in0=pt[:, 0:32], scalar1=float(BSL - 1), scalar2=None, op0=ALU.add), [pt], [nf])
        V(lambda e: e.tensor_copy(ni[:], nf[:]), [nf], [ni])
        V(lambda e: e.tensor_single_scalar(out=ni[:], in_=ni[:], scalar=BSH, op=ALU.arith_shift_right), [ni], [ni])
        V(lambda e: e.tensor_single_scalar(out=ni[:], in_=ni[:], scalar=BSH, op=ALU.logical_shift_left), [ni], [ni])
        V(lambda e: e.tensor_copy(pn[:], ni[:]), [ni], [pn])
        V(lambda e: e.tensor_tensor_scan(out=incl[:], data0=R["ones32"][:], data1=pn[:], initial=0.0, op0=ALU.mult, op1=ALU.add),
          [R["ones32"], pn], [incl])
        V(lambda e: e.tensor_tensor(out=excl[:], in0=incl[:], in1=pn[:], op=ALU.subtract), [incl, pn], [excl])
        for ti in range(NTILE):
            pb = self.ps[ti // 12]
            c0 = (ti % 12) * 32
            V(lambda e, pb=pb, c0=c0, ti=ti: e.tensor_tensor(out=P[:, ti, :], in0=pb[:, c0:c0 + 32], in1=excl[:], op=ALU.add),
              [pb, excl], [P])
        for k, Mk in enumerate((M1a, M2a)):
            V(lambda e, Mk=Mk: e.tensor_tensor(out=prod[:], in0=Mk[:], in1=P[:], op=ALU.mult), [Mk, P], [prod])
            V(lambda e, k=k: e.reduce_sum(out=posf[:, :, k], in_=prod[:], axis=AX.X), [prod], [posf])
        V(lambda e: e.tensor_copy(POSi[:], posf[:]), [posf], [POSi])
        V(lambda e: e.memset(EB[:], 0.0), [], [EB])
        for ex in range(NE):
            V(lambda e, ex=ex: e.scalar_tensor_tensor(out=EB[:], in0=R["jg"][:], scalar=incl[:, ex:ex + 1], in1=EB[:],
                                                      op0=ALU.is_ge, op1=ALU.add), [R["jg"], incl, EB], [EB])
        V(lambda e: e.tensor_scalar(out=EB[:], in0=EB[:], scalar1=float(NE - 1), scalar2=128.0, op0=ALU.min, op1=ALU.mult), [EB], [EB])
        V(lambda e: e.tensor_scalar(out=EB[:], in0=EB[:], scalar1=R["pidx"][:, 0:1], scalar2=None, op0=ALU.add), [EB, R["pidx"]], [EB])
        V(lambda e: e.tensor_copy(IDXW[:], EB[:]), [EB], [IDXW])
        if self.cfg.get("dbg_pos"):
            d1 = T(self.nc.dram_tensor("dbg_pos", [128, NTILE, 2], I32, kind="ExternalOutput"), "dbg_pos")
            d2 = T(self.nc.dram_tensor("dbg_idx", [128, NBLK], I32, kind="ExternalOutput"), "dbg_idx")
            d3 = T(self.nc.dram_tensor("dbg_incl", [128, 32], F32, kind="ExternalOutput"), "dbg_incl")
            d4 = T(self.nc.dram_tensor("dbg_P", [128, NTILE, 32], F32, kind="ExternalOutput"), "dbg_P")
            d5 = T(self.nc.dram_tensor("dbg_M1", [128, NTILE, 32], F32, kind="ExternalOutput"), "dbg_M1")
            d6 = T(self.nc.dram_tensor("dbg_A12", [128, NTILE, 2], F32, kind="ExternalOutput"), "dbg_A12")
            mk.dma("sp", d1[:, :, :], POSi[:], reads=[POSi])
            mk.dma("sp", d2[:, :], IDXW[:], reads=[IDXW])
            mk.dma("sp", d3[:, :], incl[:], reads=[incl])
            mk.dma("sp", d4[:, :, :], P[:], reads=[P])
            mk.dma("sp", d5[:, :, :], M1a[:], reads=[M1a])
            mk.dma("sp", d6[:, :, :], A12[:], reads=[A12])
            mk.release(m0)
            return
        mk.release(m0)
        m0 = mk.mark()
        ht = [mk.sbuf("sq_ht%d" % k, [128, D], BF16) for k in range(3)]
        for ti in range(NTILE):
            h = ht[ti % 3]
            mk.dma("sp", h[:], HTOK[ti * 128:(ti + 1) * 128, :], reads=[HTOK], writes=[h])
            for k in range(2):
                mk.indirect(HS[:, :], IOA(ap=POSi[:, ti, k:k + 1], axis=0), h[:], None, [h, POSi], [HS])
        mk.release(m0)
        m0 = mk.mark()
        wgu = [mk.sbuf("sq_wgu%d" % k, [128, DC, 512], BF16) for k in range(2)]
        wdn = [mk.sbuf("sq_wdn%d" % k, [128, 2, D], BF16) for k in range(2)]
        NA = BSL // 128
        hs = [mk.sbuf("sq_hs%d" % k, [128, NA, D], BF16) for k in range(2)]
        hsT = [mk.sbuf("sq_hsT%d" % k, [128, DC, BSL], BF16) for k in range(2)]
        sl = [mk.sbuf("sq_sl%d" % k, [128, BSL], F32) for k in range(2)]
        hid = [mk.sbuf("sq_hid%d" % k, [128, 2, BSL], BF16) for k in range(2)]
        yo = [mk.sbuf("sq_yo%d" % k, [128, D], F32) for k in range(3)]
        WGU, WDN = I["moe_wgu%d" % i], I["moe_wdn%d" % i]
        ny = 0
        for j in range(NBLK):
            w, wd_, h, hT_, hd = wgu[j % 2], wdn[j % 2], hs[j % 2], hsT[j % 2], hid[j % 2]
            mk.indirect(w[:].rearrange("p a b -> p (a b)"), None, WGU[:, :], IOA(ap=IDXW[:, j:j + 1], axis=0), [IDXW], [w])
            mk.indirect(wd_[:].rearrange("p a b -> p (a b)"), None, WDN[:, :], IOA(ap=IDXW[:, j:j + 1], axis=0), [IDXW], [wd_])
            mk.dma("sp", h[:], HS[j * BSL:(j + 1) * BSL, :].rearrange("(a p) d -> p a d", p=128), reads=[HS], writes=[h])
            for hh in range(4):
                pb = self.ps[hh % 2]
                pbb = pb[:, 0:512].bitcast(BF16)
                for kk in range(2):
                    k = hh * 2 + kk
                    for a in range(NA):
                        mk.op("pe", lambda e, pbb=pbb, kk=kk, k=k, a=a, h=h: e.transpose(
                            pbb[:, kk * BSL + a * 128:kk * BSL + (a + 1) * 128], h[:, a, k * 128:(k + 1) * 128], self.ident_b[:]),
                            reads=[h, self.ident_b], writes=[pb])
                dstv = hT_[:, hh * 2:(hh + 1) * 2, :].rearrange("p a b -> p (a b)")
                if hh % 2:
                    mk.op("act", lambda e, pbb=pbb, dstv=dstv: e.copy(dstv, pbb[:, :]), reads=[pb], writes=[hT_])
                else:
                    mk.op("dve", lambda e, pbb=pbb, dstv=dstv: e.tensor_copy(dstv, pbb[:, :]), reads=[pb], writes=[hT_])
            for fc in range(2):
                pg, pu = self.ps[2 + 2 * fc], self.ps[3 + 2 * fc]
                for k in range(DC):
                    mk.op("pe", lambda e, pg=pg, k=k, fc=fc, w=w, hT_=hT_: e.matmul(
                        pg[:, :], w[:, k, fc * 128:(fc + 1) * 128], hT_[:, k, :], start=(k == 0), stop=(k == DC - 1)),
                        reads=[w, hT_], writes=[pg])
                for k in range(DC):
                    mk.op("pe", lambda e, pu=pu, k=k, fc=fc, w=w, hT_=hT_: e.matmul(
                        pu[:, :], w[:, k, 256 + fc * 128:256 + (fc + 1) * 128], hT_[:, k, :], start=(k == 0), stop=(k == DC - 1)),
                        reads=[w, hT_], writes=[pu])
                s_ = sl[fc]
                mk.op("act", lambda e, pg=pg, s_=s_: e.activation(out=s_[:], in_=pg[:, :], func=AF.Silu), reads=[pg], writes=[s_])
                mk.op("dve", lambda e, pu=pu, s_=s_, hd=hd, fc=fc: e.tensor_tensor(out=hd[:, fc, :], in0=pu[:, :], in1=s_[:],
                                                                                  op=ALU.mult), reads=[pu, s_], writes=[hd])
            for a in range(NA):
                y_ = yo[ny % 3]
                ny += 1
                for dh in range(2):
                    pd = self.ps[6 + dh]
                    for fc in range(2):
                        mk.op("pe", lambda e, pd=pd, fc=fc, a=a, dh=dh, hd=hd, wd_=wd_: e.matmul(
                            pd[:, :], hd[:, fc, a * 128:(a + 1) * 128], wd_[:, fc, dh * 512:(dh + 1) * 512], start=(fc == 0), stop=(fc == 1)),
                            reads=[hd, wd_], writes=[pd])
                    if dh:
                        mk.op("act", lambda e, pd=pd, y_=y_, dh=dh: e.copy(y_[:, dh * 512:(dh + 1) * 512], pd[:, :]), reads=[pd], writes=[y_])
                    else:
                        mk.op("dve", lambda e, pd=pd, y_=y_, dh=dh: e.tensor_copy(y_[:, dh * 512:(dh + 1) * 512], pd[:, :]), reads=[pd], writes=[y_])
                mk.dma("act", YS[j * BSL + a * 128:j * BSL + (a + 1) * 128, :], y_[:], reads=[y_], writes=[YS])
        mk.release(m0)
        m0 = mk.mark()
        r1 = [mk.sbuf("sq_r1%d" % k, [128, D], F32) for k in range(2)]
        r2 = [mk.sbuf("sq_r2%d" % k, [128, D], F32) for k in range(2)]
        xr = [mk.sbuf("sq_x%d" % k, [128, 512], F32) for k in range(3)]
        n = 0
        for bi, (t0, tn) in enumerate(BLOCKS):
            r = 1 if bi == 0 else 0
            g2 = self.mv[("g2", r)]
            for a in range(tn // 128):
                ti = t0 // 128 + a
                a_, b_ = r1[ti % 2], r2[ti % 2]
                mk.indirect(a_[:], None, YS[:, :], IOA(ap=POSi[:, ti, 0:1], axis=0), [YS, POSi], [a_])
                mk.indirect(b_[:], None, YS[:, :], IOA(ap=POSi[:, ti, 1:2], axis=0), [YS, POSi], [b_])
                mk.op("dve", lambda e, a_=a_, ti=ti: e.tensor_scalar(out=a_[:], in0=a_[:], scalar1=A12[:, ti, 0:1], scalar2=None,
                                                                    op0=ALU.mult), reads=[a_, A12], writes=[a_])
                mk.op("dve", lambda e, a_=a_, b_=b_, ti=ti: e.scalar_tensor_tensor(out=a_[:], in0=b_[:], scalar=A12[:, ti, 1:2], in1=a_[:],
                                                                                  op0=ALU.mult, op1=ALU.add), reads=[a_, b_, A12], writes=[a_])
                for dc in range(DC):
                    mk.op("pe", lambda e, dc=dc, a=a, a_=a_: e.transpose(self.ps[dc][:, a * 128:(a + 1) * 128],
                                                                         a_[:, dc * 128:(dc + 1) * 128], self.ident_f[:]),
                          reads=[a_, self.ident_f], writes=[self.ps[dc]])
            for dc in range(DC):
                x = xr[n % 3]
                n += 1
                mk.dma("sp", x[:, 0:tn], XT[dc * 128:(dc + 1) * 128, t0:t0 + tn], reads=[self.XTr[dc]], writes=[x])
                mk.op("dve", lambda e, dc=dc, x=x, g2=g2: e.scalar_tensor_tensor(
                    out=x[:, 0:tn], in0=self.ps[dc][:, 0:tn], scalar=g2[:, dc:dc + 1], in1=x[:, 0:tn],
                    op0=ALU.mult, op1=ALU.add), reads=[self.ps[dc], g2, x], writes=[x])
                mk.dma("act", XT[dc * 128:(dc + 1) * 128, t0:t0 + tn], x[:, 0:tn], reads=[x], writes=[self.XTr[dc]])
        mk.release(m0)

    Prog.alloc_sparse = alloc_sparse
    Prog._route_tile_sparse = route_tile_sparse
    Prog.stage_moe_sparse = stage_moe_sparse


_sparse_methods()
```

```python
import math
import numpy as np
import ml_dtypes
import concourse.bass as bass
import concourse.mybir as mybir
from concourse.bass_utils import run_bass_kernel_spmd

F32 = mybir.dt.float32
BF16 = mybir.dt.bfloat16
I32 = mybir.dt.int32
ALU = mybir.AluOpType
AF = mybir.ActivationFunctionType
AX = mybir.AxisListType

D = 1024
DC = 8
CTX = 256
SEQ = 4096
NT = CTX + SEQ
DEPTH = 4
EPS = 1e-6
NE = 32
DEXP = 256
BLOCKS = [(0, CTX)] + [(CTX + 512 * i, 512) for i in range(SEQ // 512)]


class T:
    __slots__ = ("t", "lw", "rd", "name")

    def __init__(self, t, name=""):
        self.t = t
        self.lw = None
        self.rd = {}
        self.name = name

    def __getitem__(self, k):
        return self.t[k]


class Lane:
    def __init__(self, name, eng, sem, step):
        self.name, self.eng, self.sem, self.step = name, eng, sem, step
        self.count = 0
        self.seen = {}


class MK:
    def __init__(self, nc, n_dma_sems=40):
        self.nc = nc
        self._stack = []
        self.lanes = {}
        for name, eng in (("pe", nc.tensor), ("act", nc.scalar), ("dve", nc.vector),
                          ("pool", nc.gpsimd), ("sp", nc.sync)):
            sem = self._enter(nc.semaphore("s_" + name))
            self.lanes[name] = Lane(name, eng, sem, 1)
        self.dma_lanes = []
        for i in range(n_dma_sems):
            sem = self._enter(nc.semaphore("d_%d" % i))
            ln = Lane("dma%d" % i, None, sem, 16)
            self.lanes[ln.name] = ln
            self.dma_lanes.append(ln)
        self.dma_rr = 0
        self.n_inst = 0
        self.uid = 0

    def _enter(self, cm):
        v = cm.__enter__()
        self._stack.append(cm)
        return v

    def mark(self):
        return len(self._stack)

    def release(self, mark):
        self.barrier()
        while len(self._stack) > mark:
            self._stack.pop().__exit__(None, None, None)

    def close(self):
        while self._stack:
            self._stack.pop().__exit__(None, None, None)

    def barrier(self):
        for a in ("pe", "act", "dve", "pool", "sp"):
            la = self.lanes[a]
            for b, lb in self.lanes.items():
                if b == a or lb.count == 0:
                    continue
                if la.seen.get(b, 0) >= lb.count:
                    continue
                la.seen[b] = lb.count
                la.eng.wait_ge(lb.sem, lb.count)

    def sbuf(self, name, shape, dt):
        self.uid += 1
        return T(self._enter(self.nc.sbuf_tensor("%s_%d" % (name, self.uid), list(shape), dt)), name)

    def psum(self, name, shape, dt=F32):
        self.uid += 1
        return T(self._enter(self.nc.psum_tensor("%s_%d" % (name, self.uid), list(shape), dt)), name)

    def _deps(self, lane, reads, writes):
        need = {}
        raw_same = 0
        for t in reads:
            if t.lw is not None:
                ln, idx = t.lw
                if need.get(ln, 0) < idx:
                    need[ln] = idx
                if ln == lane.name:
                    raw_same = max(raw_same, idx)
        for t in writes:
            if t.lw is not None:
                ln, idx = t.lw
                if ln != lane.name and need.get(ln, 0) < idx:
                    need[ln] = idx
            for ln, idx in t.rd.items():
                if ln != lane.name and need.get(ln, 0) < idx:
                    need[ln] = idx
        return need, raw_same

    def _record(self, lane, reads, writes):
        idx = lane.count
        for t in reads:
            if t.rd.get(lane.name, 0) < idx:
                t.rd[lane.name] = idx
        for t in writes:
            t.lw = (lane.name, idx)
            t.rd = {}

    def op(self, lane_name, fn, reads=(), writes=()):
        lane = self.lanes[lane_name]
        need, raw_same = self._deps(lane, reads, writes)
        for ln, idx in need.items():
            if ln == lane.name:
                if lane.name == "pe" or raw_same <= lane.count - 3:
                    continue
                idx = raw_same
            if lane.seen.get(ln, 0) >= idx:
                continue
            lane.seen[ln] = idx
            lane.eng.wait_ge(self.lanes[ln].sem, idx)
        inst = fn(lane.eng)
        lane.count += 1
        inst.then_inc(lane.sem, 1)
        self._record(lane, reads, writes)
        self.n_inst += 1
        return inst

    def dma(self, q, out, in_, reads=(), writes=(), **kw):
        qlane = self.lanes[q]
        dl = self.dma_lanes[self.dma_rr]
        self.dma_rr = (self.dma_rr + 1) % len(self.dma_lanes)
        need, _ = self._deps(dl, reads, writes)
        if dl.count > 0:
            need[dl.name] = max(need.get(dl.name, 0), dl.count)
        for ln, idx in need.items():
            if qlane.seen.get(ln, 0) >= idx:
                continue
            qlane.seen[ln] = idx
            qlane.eng.wait_ge(self.lanes[ln].sem, idx)
        inst = qlane.eng.dma_start(out=out, in_=in_, **kw)
        dl.count += 16
        inst.then_inc(dl.sem, 16)
        self._record(dl, reads, writes)
        self.n_inst += 1
        return inst

    def finish(self):
        sp = self.lanes["sp"]
        for dl in self.dma_lanes:
            if dl.count and sp.seen.get(dl.name, 0) < dl.count:
                sp.seen[dl.name] = dl.count
                sp.eng.wait_ge(dl.sem, dl.count)
        self.barrier()


def _vec_pc(v):
    v = np.asarray(v, np.float32)
    return np.ascontiguousarray(v.reshape(-1, 128).T)


class Prog:
    def __init__(self, cfg=None):
        self.cfg = cfg or {}
        self.nc = bass.Bass("TRN2", target_bir_lowering=False)
        self.mk = MK(self.nc)
        self.ins = {}
        self.dram = {}

    def inp(self, name, shape, dt=F32):
        t = self.nc.dram_tensor(name, list(shape), dt, kind="ExternalInput")
        self.ins[name] = t
        return T(t, name)

    def scratch(self, name, shape, dt, out=False):
        kind = "ExternalOutput" if (out or name in self.cfg.get("dump", ())) else "Internal"
        t = self.nc.dram_tensor(name, list(shape), dt, kind=kind)
        self.dram[name] = T(t, name)
        return self.dram[name]

    def setup_consts(self):
        mk = self.mk
        self.ps = [mk.psum("ps%d" % i, [128, 512], F32) for i in range(8)]
        self.ident_f = mk.sbuf("ident_f", [128, 128], F32)
        self.ident_b = mk.sbuf("ident_b", [128, 128], BF16)
        self.ones_b = mk.sbuf("ones_b", [128, 128], BF16)
        self.avg_b = mk.sbuf("avg_b", [128, 128], BF16)
        mk.op("pool", lambda e: e.memset(self.ident_f[:], 1.0), writes=[self.ident_f])
        mk.op("pool", lambda e: e.affine_select(out=self.ident_f[:], in_=self.ident_f[:],
              pattern=[[-1, 128]], compare_op=ALU.is_equal, fill=0.0, base=0,
              channel_multiplier=1), reads=[self.ident_f], writes=[self.ident_f])
        mk.op("dve", lambda e: e.tensor_copy(self.ident_b[:], self.ident_f[:]),
              reads=[self.ident_f], writes=[self.ident_b])
        mk.op("pool", lambda e: e.memset(self.ones_b[:], 1.0), writes=[self.ones_b])
        mk.op("pool", lambda e: e.memset(self.avg_b[:], 1.0 / D), writes=[self.avg_b])
        self.eps_t = mk.sbuf("eps_t", [128, 1], F32)
        mk.op("pool", lambda e: e.memset(self.eps_t[:], EPS), writes=[self.eps_t])

    def stage_input(self, x_in, ctx_in, XT):
        mk = self.mk
        m0 = mk.mark()
        xin = [mk.sbuf("xin%d" % i, [128, 4, D], F32) for i in range(2)]
        xo = [mk.sbuf("xo%d" % i, [128, DC, 512], F32) for i in range(2)]
        for bi, (t0, tn) in enumerate(BLOCKS):
            nt = tn // 128
            buf = xin[bi % 2]
            src = ctx_in if bi == 0 else x_in
            r0 = 0 if bi == 0 else t0 - CTX
            mk.dma("sp", buf[:, 0:nt, :], src[r0:r0 + tn, :].rearrange("(a p) d -> p a d", p=128),
                   reads=[src], writes=[buf])
            ob = xo[bi % 2]
            for j in range(DC):
                ps = self.ps[j % 8]
                for a in range(nt):
                    mk.op("pe", lambda e, ps=ps, a=a, j=j, buf=buf: e.transpose(
                        ps[:, a * 128:(a + 1) * 128], buf[:, a, j * 128:(j + 1) * 128], self.ident_f[:]),
                        reads=[buf, self.ident_f], writes=[ps])
                eng = "act" if j % 2 else "dve"
                if eng == "act":
                    mk.op("act", lambda e, ps=ps, j=j, ob=ob: e.copy(ob[:, j, 0:tn], ps[:, 0:tn]),
                          reads=[ps], writes=[ob])
                else:
                    mk.op("dve", lambda e, ps=ps, j=j, ob=ob: e.tensor_copy(ob[:, j, 0:tn], ps[:, 0:tn]),
                          reads=[ps], writes=[ob])
            mk.dma("pool", XT[:, t0:t0 + tn].rearrange("(j p) t -> p j t", p=128), ob[:, :, 0:tn],
                   reads=[ob], writes=self.XTr)
        mk.release(m0)

    def stage_output(self, XT, y_out, fg):
        mk = self.mk
        m0 = mk.mark()
        xb = [mk.sbuf("fx%d" % i, [128, DC, 512], F32) for i in range(2)]
        sq = mk.sbuf("fsq", [128, DC, 512], BF16)
        rstd = mk.sbuf("frstd", [128, 512], F32)
        yo = [mk.sbuf("fy%d" % i, [128, 4, D], F32) for i in range(2)]
        for bi, (t0, tn) in enumerate(BLOCKS):
            if bi == 0:
                continue
            buf = xb[bi % 2]
            mk.dma("sp", buf[:, :, 0:tn], XT[:, t0:t0 + tn].rearrange("(j p) t -> p j t", p=128),
                   reads=self.XTr, writes=[buf])
            self._rstd(buf, sq, rstd, tn, self.ps[0])
            for j in range(DC):
                mk.op("dve", lambda e, j=j, buf=buf: e.scalar_tensor_tensor(
                    out=buf[:, j, 0:tn], in0=buf[:, j, 0:tn], scalar=fg[:, j:j + 1], in1=rstd[:, 0:tn],
                    op0=ALU.mult, op1=ALU.mult), reads=[buf, rstd, fg], writes=[buf])
            ob = yo[bi % 2]
            nt = tn // 128
            for a in range(nt):
                for h in range(2):
                    ps = self.ps[1 + (a * 2 + h) % 7]
                    for jj in range(4):
                        j = h * 4 + jj
                        mk.op("pe", lambda e, ps=ps, a=a, j=j, jj=jj, buf=buf: e.transpose(
                            ps[:, jj * 128:(jj + 1) * 128], buf[:, j, a * 128:(a + 1) * 128], self.ident_f[:]),
                            reads=[buf, self.ident_f], writes=[ps])
                    if h:
                        mk.op("act", lambda e, ps=ps, a=a, h=h, ob=ob: e.copy(
                            ob[:, a, h * 512:(h + 1) * 512], ps[:, :]), reads=[ps], writes=[ob])
                    else:
                        mk.op("dve", lambda e, ps=ps, a=a, h=h, ob=ob: e.tensor_copy(
                            ob[:, a, h * 512:(h + 1) * 512], ps[:, :]), reads=[ps], writes=[ob])
            r0 = t0 - CTX
            mk.dma("pool", y_out[r0:r0 + tn, :].rearrange("(a p) d -> p a d", p=128), ob[:, 0:nt, :],
                   reads=[ob], writes=[y_out])
        mk.release(m0)

    def _rstd(self, buf, sq, rstd, tn, ps, eps=EPS):
        mk = self.mk
        for j in range(DC):
            mk.op("act", lambda e, j=j: e.activation(out=sq[:, j, 0:tn], in_=buf[:, j, 0:tn], func=AF.Square),
                  reads=[buf], writes=[sq])
        for j in range(DC):
            mk.op("pe", lambda e, j=j: e.matmul(ps[:, 0:tn], self.avg_b[:], sq[:, j, 0:tn],
                                                 start=(j == 0), stop=(j == DC - 1)),
                  reads=[self.avg_b, sq], writes=[ps])
        mk.op("act", lambda e: e.activation(out=rstd[:, 0:tn], in_=ps[:, 0:tn], func=AF.Sqrt, bias=self.eps_t[:, 0:1]),
              reads=[ps, self.eps_t], writes=[rstd])
        mk.op("dve", lambda e: e.reciprocal(rstd[:, 0:tn], rstd[:, 0:tn]), reads=[rstd], writes=[rstd])

    def ap3(self, t2d, lo, n, inner):
        return t2d[:, lo:lo + n * inner].rearrange("p (a b) -> p a b", b=inner)

    def stage_mod(self, i, sT, w_mod, bmod, gmix, gffn):
        mk = self.mk
        m0 = mk.mark()
        wb = [mk.sbuf("wmod%d" % k, [128, DC, 512], F32) for k in range(2)]
        bm = mk.sbuf("bm", [128, 48], F32)
        gm = mk.sbuf("gm", [128, 2, DC], F32)
        mod = mk.sbuf("mod", [128, 48, 2], F32)
        mk.dma("sp", bm[:], bmod[i, :, :], reads=[bmod], writes=[bm])
        mk.dma("sp", gm[:, 0, :], gmix[i, :, :], reads=[gmix], writes=[gm])
        mk.dma("sp", gm[:, 1, :], gffn[i, :, :], reads=[gffn], writes=[gm])
        ps = self.ps[0]
        for s in range(12):
            w = wb[s % 2]
            mk.dma("sp", w[:], w_mod[i, :, s * 512:(s + 1) * 512].rearrange("(k p) m -> p k m", p=128),
                   reads=[w_mod], writes=[w])
            for mm in range(4):
                m = s * 4 + mm
                for k in range(DC):
                    mk.op("pe", lambda e, w=w, mm=mm, m=m, k=k: e.matmul(
                        ps[:, 2 * m:2 * m + 2], w[:, k, mm * 128:(mm + 1) * 128], sT[:, k, :],
                        start=(k == 0), stop=(k == DC - 1)), reads=[w, sT], writes=[ps])
        psv = ps[:, 0:96].rearrange("p (m r) -> p m r", r=2)
        for r in range(2):
            mk.op("dve", lambda e, r=r: e.tensor_tensor(out=mod[:, :, r], in0=psv[:, :, r], in1=bm[:, :], op=ALU.add),
                  reads=[ps, bm], writes=[mod])
        for r in range(2):
            for nm, sc_c, sh_c, g_c, gi in (("1", 8, 0, 16, 0), ("2", 32, 24, 40, 1)):
                gs = self.mv[("gs" + nm, r)]
                mk.op("dve", lambda e, gs=gs, sc_c=sc_c, gi=gi, r=r: e.scalar_tensor_tensor(
                    out=gs[:], in0=mod[:, sc_c:sc_c + 8, r], scalar=1.0, in1=gm[:, gi, :],
                    op0=ALU.add, op1=ALU.mult), reads=[mod, gm], writes=[gs])
                sh = self.mv[("sh" + nm, r)]
                mk.op("dve", lambda e, sh=sh, sh_c=sh_c, r=r: e.tensor_copy(sh[:], mod[:, sh_c:sh_c + 8, r]),
                      reads=[mod], writes=[sh])
                g = self.mv[("g" + nm, r)]
                mk.op("dve", lambda e, g=g, g_c=g_c, r=r: e.tensor_copy(g[:], mod[:, g_c:g_c + 8, r]),
                      reads=[mod], writes=[g])
        mk.release(m0)

    def alloc_mv(self):
        self.mv = {}
        for r in range(2):
            for nm in ("gs1", "sh1", "g1", "gs2", "sh2", "g2"):
                self.mv[(nm, r)] = self.mk.sbuf("mv_%s_%d" % (nm, r), [128, DC], F32)

    def stage_norm(self, XT, HT, which, router=None):
        mk = self.mk
        m0 = mk.mark()
        xb = [mk.sbuf("nx%d" % k, [128, DC, 512], F32) for k in range(2)]
        sq = mk.sbuf("nsq", [128, DC, 512], BF16)
        rstd = mk.sbuf("nrstd", [128, 512], F32)
        hb = [mk.sbuf("nhb%d" % k, [128, DC, 512], BF16) for k in range(2)]
        if router is not None:
            htk = [mk.sbuf("nhtk%d" % k, [128, D], BF16) for k in range(2)]
            rt = {k: mk.sbuf("rt_" + k, [128, n], F32) for k, n in
                  (("lg", 36), ("gmax", 1), ("ngmax", 1), ("ge", 4), ("gsum", 1), ("gp", 1), ("mg", 4), ("es", 8),
                   ("t8", 8), ("dd", 1), ("w2", 1), ("a1", 1), ("a2", 1), ("m1", 8), ("m2", 8), ("cw", 8),
                   ("comb", 32))}
        for bi, (t0, tn) in enumerate(BLOCKS):
            r = 1 if bi == 0 else 0
            gs, sh = self.mv[("gs" + which, r)], self.mv[("sh" + which, r)]
            buf = xb[bi % 2]
            mk.dma("sp", buf[:, :, 0:tn], XT[:, t0:t0 + tn].rearrange("(j p) t -> p j t", p=128),
                   reads=self.XTr, writes=[buf])
            self._rstd(buf, sq, rstd, tn, self.ps[0])
            h = hb[bi % 2]
            for j in range(DC):
                mk.op("dve", lambda e, j=j, buf=buf, gs=gs: e.scalar_tensor_tensor(
                    out=buf[:, j, 0:tn], in0=buf[:, j, 0:tn], scalar=gs[:, j:j + 1], in1=rstd[:, 0:tn],
                    op0=ALU.mult, op1=ALU.mult), reads=[buf, rstd, gs], writes=[buf])
                if router is not None:
                    mk.op("dve", lambda e, j=j, buf=buf, sh=sh: e.tensor_scalar(
                        out=buf[:, j, 0:tn], in0=buf[:, j, 0:tn], scalar1=sh[:, j:j + 1], scalar2=None,
                        op0=ALU.add), reads=[buf, sh], writes=[buf])
                    mk.op("act", lambda e, j=j, buf=buf, h=h: e.copy(h[:, j, 0:tn], buf[:, j, 0:tn]),
                          reads=[buf], writes=[h])
                else:
                    mk.op("act", lambda e, j=j, buf=buf, h=h, sh=sh: e.activation(
                        out=h[:, j, 0:tn], in_=buf[:, j, 0:tn], func=AF.Identity, bias=sh[:, j:j + 1]),
                        reads=[buf, sh], writes=[h])
            mk.dma("pool", HT[:, t0:t0 + tn].rearrange("(j p) t -> p j t", p=128), h[:, :, 0:tn],
                   reads=[h], writes=[HT])
            if router is not None:
                for a in range(tn // 128):
                    if "M1a" in router:
                        self._route_tile_sparse(buf, a, t0 + a * 128, router, rt)
                        pb = self.ps[5 + (a % 2)]
                        pbb = pb[:, 0:512].bitcast(BF16)
                        for j in range(DC):
                            mk.op("pe", lambda e, pbb=pbb, j=j, a=a, h=h: e.transpose(
                                pbb[:, j * 128:(j + 1) * 128], h[:, j, a * 128:(a + 1) * 128], self.ident_b[:]),
                                reads=[h, self.ident_b], writes=[pb])
                        ht_ = htk[a % 2]
                        mk.op("act", lambda e, pbb=pbb, ht_=ht_: e.copy(ht_[:], pbb[:, :]), reads=[pb], writes=[ht_])
                        mk.dma("act", self.dram["HTOK"][t0 + a * 128:t0 + (a + 1) * 128, :], ht_[:], reads=[ht_],
                               writes=[self.dram["HTOK"]])
                    else:
                        self._route_tile(buf, a, t0 + a * 128, router, rt)
        mk.release(m0)

    def _route_tile(self, hf, a, tok0, R, rt):
        mk = self.mk
        ps = self.ps[1 + (a % 2)]
        Wr, br, combT = R["Wr"], R["br"], R["combT"]
        for k in range(DC):
            mk.op("pe", lambda e, k=k: e.matmul(ps[:, 0:36], hf[:, k, a * 128:(a + 1) * 128], Wr[:, k, :],
                                                 start=(k == 0), stop=(k == DC - 1)),
                  reads=[hf, Wr], writes=[ps])
        lg, gmax, ngmax, ge, gsum, gp, mg, es = (rt[k] for k in ("lg", "gmax", "ngmax", "ge", "gsum", "gp", "mg", "es"))
        t8, dd, w2, a1, a2, m1, m2, cw, comb = (rt[k] for k in ("t8", "dd", "w2", "a1", "a2", "m1", "m2", "cw", "comb"))
        V = lambda fn, reads, writes: mk.op("dve", fn, reads=reads, writes=writes)
        V(lambda e: e.tensor_tensor(out=lg[:], in0=ps[:, 0:36], in1=br[:], op=ALU.add), [ps, br], [lg])
        V(lambda e: e.reduce_max(out=gmax[:], in_=lg[:, 0:4], axis=AX.X), [lg], [gmax])
        V(lambda e: e.tensor_scalar(out=ngmax[:], in0=gmax[:], scalar1=-1.0, scalar2=None, op0=ALU.mult), [gmax], [ngmax])
        mk.op("act", lambda e: e.activation(out=ge[:], in_=lg[:, 0:4], func=AF.Exp, bias=ngmax[:, 0:1],
                                            accum_out=gsum[:]), reads=[lg, ngmax], writes=[ge, gsum])
        V(lambda e: e.reciprocal(gp[:], gsum[:]), [gsum], [gp])
        V(lambda e: e.tensor_scalar(out=mg[:], in0=lg[:, 0:4], scalar1=gmax[:, 0:1], scalar2=None, op0=ALU.is_equal),
          [lg, gmax], [mg])
        V(lambda e: e.tensor_scalar(out=es[:], in0=lg[:, 4:12], scalar1=mg[:, 0:1], scalar2=None, op0=ALU.mult),
          [lg, mg], [es])
        for g in range(1, 4):
            V(lambda e, g=g: e.scalar_tensor_tensor(out=es[:], in0=lg[:, 4 + 8 * g:12 + 8 * g], scalar=mg[:, g:g + 1],
                                                    in1=es[:], op0=ALU.mult, op1=ALU.add), [lg, mg, es], [es])
        V(lambda e: e.max(out=t8[:], in_=es[:]), [es], [t8])
        V(lambda e: e.tensor_tensor(out=dd[:], in0=t8[:, 1:2], in1=t8[:, 0:1], op=ALU.subtract), [t8], [dd])
        mk.op("act", lambda e: e.activation(out=w2[:], in_=dd[:], func=AF.Sigmoid), reads=[dd], writes=[w2])
        V(lambda e: e.tensor_tensor(out=a2[:], in0=w2[:], in1=gp[:], op=ALU.mult), [w2, gp], [a2])
        V(lambda e: e.tensor_tensor(out=a1[:], in0=gp[:], in1=a2[:], op=ALU.subtract), [gp, a2], [a1])
        V(lambda e: e.tensor_scalar(out=m1[:], in0=es[:], scalar1=t8[:, 0:1], scalar2=a1[:, 0:1], op0=ALU.is_equal,
                                    op1=ALU.mult), [es, t8, a1], [m1])
        V(lambda e: e.tensor_scalar(out=m2[:], in0=es[:], scalar1=t8[:, 1:2], scalar2=a2[:, 0:1], op0=ALU.is_equal,
                                    op1=ALU.mult), [es, t8, a2], [m2])
        V(lambda e: e.tensor_tensor(out=cw[:], in0=m1[:], in1=m2[:], op=ALU.add), [m1, m2], [cw])
        for g in range(4):
            V(lambda e, g=g: e.tensor_scalar(out=comb[:, 8 * g:8 * g + 8], in0=cw[:], scalar1=mg[:, g:g + 1],
                                             scalar2=None, op0=ALU.mult), [cw, mg], [comb])
        ps2 = self.ps[3 + (a % 2)]
        mk.op("pe", lambda e: e.transpose(ps2[0:32, 0:128], comb[:, :], self.ident_f[:]),
              reads=[comb, self.ident_f], writes=[ps2])
        mk.op("act", lambda e: e.copy(combT[:, tok0:tok0 + 128], ps2[0:32, 0:128]), reads=[ps2], writes=[combT])

    def stage_moe(self, i, XT, HT, HID, combT, sel, w_gate, w_up, w_down):
        mk = self.mk
        m0 = mk.mark()
        hT = mk.sbuf("moe_hT", [128, DC, NT], BF16)
        for j in range(DC):
            mk.dma("sp", hT[:, j, :], HT[j * 128:(j + 1) * 128, :], reads=[HT], writes=[hT])
        wgu = [mk.sbuf("wgu%d" % k, [128, DC, 512], BF16) for k in range(2)]
        cbs = [mk.sbuf("cbs%d" % k, [128, 512], F32) for k in range(2)]
        sl = [mk.sbuf("sl%d" % k, [128, 512], F32) for k in range(2)]
        tt = [mk.sbuf("tt%d" % k, [128, 512], F32) for k in range(2)]
        hid = [mk.sbuf("hid%d" % k, [128, 2, 512], BF16) for k in range(2)]
        n = 0
        for ex in range(NE):
            g, el = ex // 8, ex % 8
            w = wgu[ex % 2]
            mk.dma("pool", w[:, :, 0:256], w_gate[i, g, el, :, :].rearrange("(k p) f -> p k f", p=128),
                   reads=[w_gate], writes=[w])
            mk.dma("pool", w[:, :, 256:512], w_up[i, g, el, :, :].rearrange("(k p) f -> p k f", p=128),
                   reads=[w_up], writes=[w])
            for bi, (t0, tn) in enumerate(BLOCKS):
                pc = self.ps[n % 2]
                mk.op("pe", lambda e, pc=pc, ex=ex: e.matmul(pc[:, 0:tn], sel[:, ex, :], combT[:, t0:t0 + tn],
                                                            start=True, stop=True),
                      reads=[sel, combT], writes=[pc])
                cb = cbs[n % 2]
                mk.op("act", lambda e, pc=pc, cb=cb: e.copy(cb[:, 0:tn], pc[:, 0:tn]), reads=[pc], writes=[cb])
                hd = hid[n % 2]
                for fc in range(2):
                    q = (2 * n + fc) % 3
                    pg, pu = self.ps[2 + 2 * q], self.ps[3 + 2 * q]
                    for k in range(DC):
                        mk.op("pe", lambda e, pg=pg, k=k, fc=fc, w=w: e.matmul(
                            pg[:, 0:tn], w[:, k, fc * 128:(fc + 1) * 128], hT[:, k, t0:t0 + tn],
                            start=(k == 0), stop=(k == DC - 1)), reads=[w, hT], writes=[pg])
                    for k in range(DC):
                        mk.op("pe", lambda e, pu=pu, k=k, fc=fc, w=w: e.matmul(
                            pu[:, 0:tn], w[:, k, 256 + fc * 128:256 + (fc + 1) * 128], hT[:, k, t0:t0 + tn],
                            start=(k == 0), stop=(k == DC - 1)), reads=[w, hT], writes=[pu])
                    s_, t_ = sl[fc], tt[fc]
                    mk.op("act", lambda e, pg=pg, s_=s_: e.activation(out=s_[:, 0:tn], in_=pg[:, 0:tn], func=AF.Silu),
                          reads=[pg], writes=[s_])
                    mk.op("dve", lambda e, pu=pu, s_=s_, t_=t_: e.tensor_tensor(
                        out=t_[:, 0:tn], in0=pu[:, 0:tn], in1=s_[:, 0:tn], op=ALU.mult), reads=[pu, s_], writes=[t_])
                    mk.op("pool", lambda e, t_=t_, cb=cb, hd=hd, fc=fc: e.tensor_tensor(
                        out=hd[:, fc, 0:tn], in0=t_[:, 0:tn], in1=cb[:, 0:tn], op=ALU.mult), reads=[t_, cb], writes=[hd])
                mk.dma("act", HID[ex * 256:(ex + 1) * 256, t0:t0 + tn].rearrange("(c p) t -> p c t", p=128),
                       hd[:, :, 0:tn], reads=[hd], writes=[HID])
                n += 1
        mk.release(m0)
        m0 = mk.mark()
        hb = [mk.sbuf("p2h%d" % k, [128, 64, 512], BF16) for k in range(1)]
        wd = [mk.sbuf("p2w%d" % k, [128, 8, D], BF16) for k in range(2)]
        xr = [mk.sbuf("p2x%d" % k, [128, 512], F32) for k in range(3)]
        wdv = w_down[i].rearrange("g e f d -> (g e f) d")
        n = 0
        for bi, (t0, tn) in enumerate(BLOCKS):
            r = 1 if bi == 0 else 0
            g2 = self.mv[("g2", r)]
            h = hb[0]
            for c8 in range(8):
                mk.dma("sp", h[:, c8 * 8:(c8 + 1) * 8, 0:tn],
                       HID[c8 * 1024:(c8 + 1) * 1024, t0:t0 + tn].rearrange("(c p) t -> p c t", p=128),
                       reads=[HID], writes=[h])
            for kg in range(8):
                w = wd[n % 2]
                n += 1
                mk.dma("pool", w[:], wdv[kg * 1024:(kg + 1) * 1024, :].rearrange("(c p) d -> p c d", p=128),
                       reads=[w_down], writes=[w])
                for kk in range(8):
                    kc = kg * 8 + kk
                    for dc in range(DC):
                        mk.op("pe", lambda e, dc=dc, kk=kk, kc=kc, w=w: e.matmul(
                            self.ps[dc][:, 0:tn], w[:, kk, dc * 128:(dc + 1) * 128], h[:, kc, 0:tn],
                            start=(kc == 0), stop=(kc == 63)), reads=[w, h], writes=[self.ps[dc]])
            for dc in range(DC):
                x = xr[dc % 3]
                mk.dma("sp", x[:, 0:tn], XT[dc * 128:(dc + 1) * 128, t0:t0 + tn], reads=[self.XTr[dc]], writes=[x])
                mk.op("dve", lambda e, dc=dc, x=x, g2=g2: e.scalar_tensor_tensor(
                    out=x[:, 0:tn], in0=self.ps[dc][:, 0:tn], scalar=g2[:, dc:dc + 1], in1=x[:, 0:tn],
                    op0=ALU.mult, op1=ALU.add), reads=[self.ps[dc], g2, x], writes=[x])
                mk.dma("act", XT[dc * 128:(dc + 1) * 128, t0:t0 + tn], x[:, 0:tn], reads=[x], writes=[self.XTr[dc]])
        mk.release(m0)


def build_program(cfg=None):
    cfg = cfg or {}
    p = Prog(cfg)
    mk = p.mk
    layers = cfg.get("layers", list(range(DEPTH)))
    I = {}
    I["x"] = p.inp("x", [SEQ, D])
    I["ctx"] = p.inp("ctx", [CTX, D])
    I["c_pc"] = p.inp("c_pc", [128, DC, 2])
    I["w_mod"] = p.inp("w_mod", [DEPTH, D, 6 * D])
    I["bmod_pc"] = p.inp("bmod_pc", [DEPTH, 128, 48])
    I["gmix_pc"] = p.inp("gmix_pc", [DEPTH, 128, DC])
    I["gffn_pc"] = p.inp("gffn_pc", [DEPTH, 128, DC])
    I["fg_pc"] = p.inp("fg_pc", [128, DC])
    I["wr_pc"] = p.inp("wr_pc", [DEPTH, 128, DC, 36])
    I["br_bc"] = p.inp("br_bc", [DEPTH, 128, 36])
    I["sel"] = p.inp("sel", [32, NE, 128], BF16)
    sparse = cfg.get("sparse", True)
    if sparse:
        for li in range(DEPTH):
            I["moe_wgu%d" % li] = p.inp("moe_wgu%d" % li, [NE * 128, DC * 512])
            I["moe_wdn%d" % li] = p.inp("moe_wdn%d" % li, [NE * 128, 2 * D])
        I["jgrid"] = p.inp("jgrid", [128, NBLK])
        I["pidx"] = p.inp("pidx", [128, 1])
    else:
        I["moe_w_gate"] = p.inp("moe_w_gate", [DEPTH, 4, 8, D, DEXP])
        I["moe_w_up"] = p.inp("moe_w_up", [DEPTH, 4, 8, D, DEXP])
        I["moe_w_down"] = p.inp("moe_w_down", [DEPTH, 4, 8, DEXP, D])
    I["attn_w_q"] = p.inp("attn_w_q", [2, D, D])
    I["attn_w_kv"] = p.inp("attn_w_kv", [2, D, 512])
    I["attn_w_o"] = p.inp("attn_w_o", [2, D, D])
    I["qkgain_pc"] = p.inp("qkgain_pc", [2, 128, 2])
    I["conv_w_pw1"] = p.inp("conv_w_pw1", [1, D, 2 * D])
    I["conv_w_pw2"] = p.inp("conv_w_pw2", [1, D, D])
    I["conv_b_pw1_pc"] = p.inp("conv_b_pw1_pc", [1, 128, 16])
    I["conv_w_dw_pc"] = p.inp("conv_w_dw_pc", [1, 128, DC, 31])
    I["conv_b_dw_pc"] = p.inp("conv_b_dw_pc", [1, 128, DC])
    I["conv_ln_g_pc"] = p.inp("conv_ln_g_pc", [1, 128, DC])
    I["conv_ln_b_pc"] = p.inp("conv_ln_b_pc", [1, 128, DC])
    I["conv_b_pw2_pc"] = p.inp("conv_b_pw2_pc", [1, 128, DC])
    I["hy_w_in"] = p.inp("hy_w_in", [1, D, 3 * D])
    I["hy_w_out"] = p.inp("hy_w_out", [1, D, D])
    I["hy_b_in_pc"] = p.inp("hy_b_in_pc", [128, 24])
    I["hy_w_short_pc"] = p.inp("hy_w_short_pc", [128, 24, 3])
    I["hy_b_short_pc"] = p.inp("hy_b_short_pc", [128, 24])
    I["hy_b_out_pc"] = p.inp("hy_b_out_pc", [128, DC])
    I["hy_lbias_pc"] = p.inp("hy_lbias_pc", [2, 128, DC])
    I["hy_f_w1"] = p.inp("hy_f_w1", [1, 17, 64])
    I["hy_f_w2"] = p.inp("hy_f_w2", [1, 64, 64])
    I["hy_f_w3"] = p.inp("hy_f_w3", [1, 64, 4 * D])
    I["hy_fvec"] = p.inp("hy_fvec", [64, 4])
    I["hy_dabs"] = p.inp("hy_dabs", [64, D])
    for tag, L_ in (("lat", SEQ), ("ctx", CTX)):
        N1_ = 2 * L_ // 64
        I["hy_F1_" + tag] = p.inp("hy_F1_" + tag, [N1_ // 2, 6, N1_], BF16)
        I["hy_G_" + tag] = p.inp("hy_G_" + tag, [128, N1_, 5, 128], BF16)
        I["hy_Fi_" + tag] = p.inp("hy_Fi_" + tag, [N1_, 2, N1_ // 2], BF16)
        I["hy_lneg_" + tag] = p.inp("hy_lneg_" + tag, [N1_ // 2, 64])
        I["hy_featsT_" + tag] = p.inp("hy_featsT_" + tag, [17, L_])
    I["hyd_F"] = p.inp("hyd_F", [128, 2, 3, 512], BF16)
    I["hyd_Fi"] = p.inp("hyd_Fi", [128, 4, 2, CTX], BF16)
    I["hyd_lneg"] = p.inp("hyd_lneg", [128, 2])
    I["ropeR"] = p.inp("ropeR", [128, 128], BF16)
    I["ropeC"] = p.inp("ropeC", [128, SEQ])
    I["ropeS"] = p.inp("ropeS", [128, SEQ])
    y = T(p.nc.dram_tensor("y", [SEQ, D], F32, kind="ExternalOutput"), "y")
    QT = p.scratch("QT", [D, NT], BF16)
    OT = p.scratch("OT", [D, NT], BF16)
    XT = p.scratch("XT", [D, NT], F32)
    p.XTr = [T(XT.t, "XTr%d" % k) for k in range(DC)]
    VT = p.scratch("VT", [D, NT], F32)
    if 2 in layers and cfg.get("mixers", True):
        p.scratch("hyU0", [3 * D, NT], F32)
        p.scratch("hyZ", [3 * D, NT], F32)
        p.scratch("hyZ1", [D, NT], F32)
        p.scratch("hyA", [2, 128, 64, D], BF16)
        p.scratch("hyC", [2, 64, 128, D], BF16)
        for o in range(2):
            p.scratch("hyHH_lat%d" % o, [2, 128, 128, D], BF16)
            p.scratch("hyHH_ctx%d" % o, [2, 8, 128, D], BF16)
    HT = p.scratch("HT", [D, NT], BF16)
    p.dram["HT"] = HT
    if sparse:
        p.scratch("HTOK", [NT, D], BF16)
        p.scratch("HS", [NSLOT, D], BF16)
        p.scratch("YS", [NSLOT, D], F32)
    else:
        HID = p.scratch("HID", [NE * DEXP, NT], BF16)
    p.setup_consts()
    p.alloc_mv()
    fg = mk.sbuf("fg", [128, DC], F32)
    sT = mk.sbuf("sT", [128, DC, 2], F32)
    sel = mk.sbuf("sel", [32, NE, 128], BF16)
    if sparse:
        SR = p.alloc_sparse()
        mk.dma("sp", SR["jg"][:], I["jgrid"][:, :], writes=[SR["jg"]])
        mk.dma("sp", SR["pidx"][:], I["pidx"][:, :], writes=[SR["pidx"]])
    else:
        combT = mk.sbuf("combT", [32, NT], BF16)
    Wr = mk.sbuf("Wr", [128, DC, 36], F32)
    br = mk.sbuf("br", [128, 36], F32)
    mk.dma("sp", fg[:], I["fg_pc"][:, :], reads=[I["fg_pc"]], writes=[fg])
    mk.dma("sp", sT[:], I["c_pc"][:, :, :], reads=[I["c_pc"]], writes=[sT])
    mk.dma("sp", sel[:], I["sel"][:, :, :], reads=[I["sel"]], writes=[sel])
    mk.op("act", lambda e: e.activation(out=sT[:], in_=sT[:], func=AF.Silu), reads=[sT], writes=[sT])
    p.stage_input(I["x"], I["ctx"], XT)
    for i in layers:
        p.stage_mod(i, sT, I["w_mod"], I["bmod_pc"], I["gmix_pc"], I["gffn_pc"])
        if cfg.get("mixers", True):
            kind, slot = i % 3, i // 3
            with_ctx = i < DEPTH - 1
            p.stage_norm(XT, HT, "1")
            if kind == 0:
                p.stage_attn(slot, XT, HT, QT, OT, I, with_ctx)
            elif kind == 1:
                p.stage_conf(slot, XT, HT, QT, VT, I)
            else:
                p.stage_hyena(slot, XT, HT, I)
        if cfg.get("moe", True) is False:
            continue
        mk.dma("sp", Wr[:], I["wr_pc"][i, :, :, :], reads=[I["wr_pc"]], writes=[Wr])
        mk.dma("sp", br[:], I["br_bc"][i, :, :], reads=[I["br_bc"]], writes=[br])
        if sparse:
            SR["Wr"], SR["br"] = Wr, br
            p.stage_norm(XT, HT, "2", router=SR)
            p.stage_moe_sparse(i, XT, SR, I)
        else:
            p.stage_norm(XT, HT, "2", router=dict(Wr=Wr, br=br, combT=combT))
            p.stage_moe(i, XT, HT, HID, combT, sel, I["moe_w_gate"], I["moe_w_up"], I["moe_w_down"])
    p.stage_output(XT, y, fg)
    mk.finish()
    mk.close()
    return p


_HYC = {}


def host_inputs(inp, b, sparse=True):
    f = lambda a: np.ascontiguousarray(np.asarray(a, np.float32))
    m = {}
    m["x"] = f(inp["x"][b])
    m["ctx"] = f(inp["ctx"][b])
    m["c_pc"] = np.ascontiguousarray(np.stack([_vec_pc(inp["c"][b]), _vec_pc(inp["c_ctx"])], axis=-1))
    m["w_mod"] = f(inp["w_mod"])
    m["bmod_pc"] = np.stack([_vec_pc(inp["b_mod"][i]) for i in range(DEPTH)])
    m["gmix_pc"] = np.stack([_vec_pc(inp["norm_mix_g"][i]) for i in range(DEPTH)])
    m["gffn_pc"] = np.stack([_vec_pc(inp["norm_ffn_g"][i]) for i in range(DEPTH)])
    m["fg_pc"] = _vec_pc(inp["final_norm_g"])
    wr = np.concatenate([np.asarray(inp["moe_w_group"]), np.asarray(inp["moe_w_router"])], axis=-1)
    m["wr_pc"] = np.ascontiguousarray(wr.reshape(DEPTH, DC, 128, 36).transpose(0, 2, 1, 3)).astype(np.float32)
    brr = np.concatenate([np.asarray(inp["moe_b_group"]), np.asarray(inp["moe_b_router"])], axis=-1)
    m["br_bc"] = np.ascontiguousarray(np.broadcast_to(brr[:, None, :], (DEPTH, 128, 36))).astype(np.float32)
    sel = np.zeros((32, NE, 128), np.float32)
    for e in range(NE):
        sel[e, e, :] = 1.0
    m["sel"] = sel.astype(ml_dtypes.bfloat16)
    m["attn_w_q"] = f(inp["attn_w_q"]); m["attn_w_kv"] = f(inp["attn_w_kv"]); m["attn_w_o"] = f(inp["attn_w_o"])
    m["qkgain_pc"] = np.ascontiguousarray(np.stack([np.asarray(inp["attn_q_gain"], np.float32),
                                                    np.asarray(inp["attn_k_gain"], np.float32)], axis=-1))
    m["conv_w_pw1"] = f(inp["conv_w_pw1"]); m["conv_w_pw2"] = f(inp["conv_w_pw2"])
    m["conv_b_pw1_pc"] = _vec_pc(inp["conv_b_pw1"][0])[None]
    m["conv_w_dw_pc"] = np.ascontiguousarray(np.asarray(inp["conv_w_dw"][0], np.float32).reshape(31, DC, 128).transpose(2, 1, 0))[None]
    for nm in ("conv_b_dw", "conv_ln_g", "conv_ln_b", "conv_b_pw2"):
        m[nm + "_pc"] = _vec_pc(inp[nm][0])[None]
    m["hy_w_in"] = f(inp["hy_w_in"]); m["hy_w_out"] = f(inp["hy_w_out"])
    m["hy_b_in_pc"] = _vec_pc(inp["hy_b_in"][0]); m["hy_b_short_pc"] = _vec_pc(inp["hy_b_short"][0])
    m["hy_w_short_pc"] = np.ascontiguousarray(np.stack([_vec_pc(inp["hy_w_short"][0, k]) for k in range(3)], axis=-1))
    m["hy_b_out_pc"] = _vec_pc(inp["hy_b_out"][0])
    m["hy_lbias_pc"] = np.stack([_vec_pc(inp["hy_long_bias"][0, o]) for o in range(2)])
    m["hy_f_w1"] = f(inp["hy_f_w1"]); m["hy_f_w2"] = f(inp["hy_f_w2"]); m["hy_f_w3"] = f(inp["hy_f_w3"])
    m["hy_fvec"] = np.ascontiguousarray(np.stack([np.asarray(inp[k][0], np.float32) for k in
                                                  ("hy_f_b1", "hy_f_freq1", "hy_f_b2", "hy_f_freq2")], axis=-1))
    dl = np.abs(np.linspace(math.log(1e-2) / 1.5, math.log(1e-2) / 0.3, D, dtype=np.float32))
    m["hy_dabs"] = np.ascontiguousarray(np.broadcast_to(dl[None, :], (64, D))).astype(np.float32)
    for tag, L_ in (("lat", SEQ), ("ctx", CTX)):
        hc = _HYC[tag] if tag in _HYC else _HYC.setdefault(tag, hy_consts(L_))
        m["hy_F1_" + tag] = hc["F1"]; m["hy_G_" + tag] = hc["G"]; m["hy_Fi_" + tag] = hc["Fi"]
        m["hy_lneg_" + tag] = hc["lneg"]; m["hy_featsT_" + tag] = hc["featsT"]
    hdc = _HYC["hyd"] if "hyd" in _HYC else _HYC.setdefault("hyd", hyd_consts())
    m["hyd_F"] = hdc["F"]; m["hyd_Fi"] = hdc["Fi"]; m["hyd_lneg"] = hdc["lneg"]
    RT = np.zeros((128, 128), np.float32)
    for base in (0, 64):
        for dd in range(32):
            RT[base + dd + 32, base + dd] = -1.0
            RT[base + dd, base + dd + 32] = 1.0
    m["ropeR"] = RT.astype(ml_dtypes.bfloat16)
    inv = (np.float32(10000.0) ** (-np.arange(32, dtype=np.float32) / np.float32(32))).astype(np.float32)
    tt = np.arange(SEQ)
    ang = np.zeros((128, SEQ), np.float32)
    for dd in range(128):
        pos = (tt // 64) if dd < 64 else (tt % 64)
        ang[dd] = pos.astype(np.float32) * inv[dd % 32]
    m["ropeC"] = np.cos(ang).astype(np.float32)
    m["ropeS"] = np.sin(ang).astype(np.float32)
    if sparse:
        wg = np.asarray(inp["moe_w_gate"], np.float32).reshape(DEPTH, NE, DC, 128, DEXP)
        wu = np.asarray(inp["moe_w_up"], np.float32).reshape(DEPTH, NE, DC, 128, DEXP)
        wd = np.asarray(inp["moe_w_down"], np.float32).reshape(DEPTH, NE, 2, 128, D)
        for li in range(DEPTH):
            m["moe_wgu%d" % li] = np.ascontiguousarray(np.concatenate([wg[li], wu[li]], axis=-1).transpose(0, 2, 1, 3)).reshape(NE * 128, DC * 512)
            m["moe_wdn%d" % li] = np.ascontiguousarray(wd[li].transpose(0, 2, 1, 3)).reshape(NE * 128, 2 * D)
        m["jgrid"] = np.ascontiguousarray(np.broadcast_to((np.arange(NBLK, dtype=np.float32) * BSL)[None, :], (128, NBLK)))
        m["pidx"] = np.arange(128, dtype=np.float32)[:, None].copy()
    else:
        m["moe_w_gate"] = f(inp["moe_w_gate"])
        m["moe_w_up"] = f(inp["moe_w_up"])
        m["moe_w_down"] = f(inp["moe_w_down"])
    return m


def _attn_methods():
    def load_w(self, name, src_ap, kc, m, q="pool"):
        mk = self.mk
        w = mk.sbuf(name, [128, kc, m], BF16)
        step = max(1, 4096 // m)
        for k0 in range(0, kc, step):
            k1 = min(kc, k0 + step)
            mk.dma(q, w[:, k0:k1, :], src_ap[k0 * 128:k1 * 128, :].rearrange("(k p) m -> p k m", p=128), writes=[w])
        return w

    def linear_resid(self, XT, srcT, W, KC, gname, bias=None, skip_ctx=False):
        mk = self.mk
        m0 = mk.mark()
        sb = [mk.sbuf("lr_s%d" % k, [128, KC, 512], BF16) for k in range(2)]
        xr = [mk.sbuf("lr_x%d" % k, [128, 512], F32) for k in range(3)]
        gb = None
        if bias is not None:
            gb = [mk.sbuf("lr_gb%d" % r, [128, DC], F32) for r in range(2)]
            for r in range(2):
                mk.op("dve", lambda e, r=r: e.tensor_tensor(out=gb[r][:], in0=bias[:], in1=self.mv[(gname, r)][:],
                                                            op=ALU.mult), reads=[bias, self.mv[(gname, r)]], writes=[gb[r]])
        n = 0
        for bi, (t0, tn) in enumerate(BLOCKS):
            if skip_ctx and bi == 0:
                continue
            r = 1 if bi == 0 else 0
            g = self.mv[(gname, r)]
            s = sb[bi % 2]
            mk.dma("sp", s[:, :, 0:tn], srcT[:, t0:t0 + tn].rearrange("(k p) t -> p k t", p=128),
                   reads=[srcT], writes=[s])
            for dc in range(DC):
                ps = self.ps[n % 4]
                x = xr[n % 3]
                n += 1
                mk.dma("sp", x[:, 0:tn], XT[dc * 128:(dc + 1) * 128, t0:t0 + tn], reads=[self.XTr[dc]], writes=[x])
                for k in range(KC):
                    mk.op("pe", lambda e, ps=ps, k=k, dc=dc, s=s: e.matmul(
                        ps[:, 0:tn], W[:, k, dc * 128:(dc + 1) * 128], s[:, k, 0:tn],
                        start=(k == 0), stop=(k == KC - 1)), reads=[W, s], writes=[ps])
                mk.op("dve", lambda e, ps=ps, dc=dc, x=x, g=g: e.scalar_tensor_tensor(
                    out=x[:, 0:tn], in0=ps[:, 0:tn], scalar=g[:, dc:dc + 1], in1=x[:, 0:tn],
                    op0=ALU.mult, op1=ALU.add), reads=[ps, g, x], writes=[x])
                if gb is not None:
                    mk.op("dve", lambda e, dc=dc, x=x, r=r: e.tensor_scalar(
                        out=x[:, 0:tn], in0=x[:, 0:tn], scalar1=gb[r][:, dc:dc + 1], scalar2=None, op0=ALU.add),
                        reads=[x, gb[r]], writes=[x])
                mk.dma("act", XT[dc * 128:(dc + 1) * 128, t0:t0 + tn], x[:, 0:tn], reads=[x], writes=[self.XTr[dc]])
        mk.release(m0)

    def stage_attn(self, slot, XT, HT, QT, OT, I, with_ctx):
        mk = self.mk
        mA = mk.mark()
        KT = mk.sbuf("KT", [128, 2, NT], BF16)
        Vs = mk.sbuf("Vs", [128, NT // 128, 256], BF16)
        gains = mk.sbuf("qkgain", [128, 2], F32)
        RT = mk.sbuf("RT", [128, 128], BF16)
        avgh = mk.sbuf("avgh", [128, 128], BF16)
        mk.dma("sp", gains[:], I["qkgain_pc"][slot, :, :], writes=[gains])
        mk.dma("sp", RT[:], I["ropeR"][:, :], writes=[RT])
        mk.op("pool", lambda e: e.memset(avgh[:], 1.0 / 128), writes=[avgh])
        m0 = mk.mark()
        wq = self.load_w("wq", I["attn_w_q"][slot], DC, D)
        wkv = self.load_w("wkv", I["attn_w_kv"][slot], DC, 512)
        hb = [mk.sbuf("ah%d" % k, [128, DC, 512], BF16) for k in range(2)]
        cs = [mk.sbuf("acs%d" % k, [128, 2, 512], F32) for k in range(2)]
        sqq = [mk.sbuf("asq%d" % k, [128, 512], BF16) for k in range(2)]
        rs = [mk.sbuf("ars%d" % k, [128, 512], F32) for k in range(2)]
        qn = [mk.sbuf("aqn%d" % k, [128, 512], F32) for k in range(2)]
        qb = [mk.sbuf("aqb%d" % k, [128, 512], BF16) for k in range(2)]
        t1 = [mk.sbuf("at1%d" % k, [128, 512], F32) for k in range(2)]
        t2 = [mk.sbuf("at2%d" % k, [128, 512], F32) for k in range(2)]
        qo = [mk.sbuf("aqo%d" % k, [128, 512], BF16) for k in range(3)]
        n = 0
        for bi, (t0, tn) in enumerate(BLOCKS):
            h = hb[bi % 2]
            mk.dma("sp", h[:, :, 0:tn], HT[:, t0:t0 + tn].rearrange("(k p) t -> p k t", p=128), reads=[HT], writes=[h])
            c = cs[bi % 2]
            if bi > 0:
                mk.dma("sp", c[:, 0, 0:tn], I["ropeC"][:, t0 - CTX:t0 - CTX + tn], writes=[c])
                mk.dma("sp", c[:, 1, 0:tn], I["ropeS"][:, t0 - CTX:t0 - CTX + tn], writes=[c])
            for hh in range(10):
                isq = hh < 8
                if isq and bi == 0 and not with_ctx:
                    continue
                W = wq if isq else wkv
                c0 = hh * 128 if isq else (hh - 8) * 128
                gcol = 0 if isq else 1
                ps = self.ps[n % 2]
                pz = self.ps[2 + n % 2]
                pr = self.ps[4 + n % 2]
                i2 = n % 2
                n += 1
                for k in range(DC):
                    mk.op("pe", lambda e, ps=ps, k=k, W=W, c0=c0, h=h: e.matmul(
                        ps[:, 0:tn], W[:, k, c0:c0 + 128], h[:, k, 0:tn], start=(k == 0), stop=(k == DC - 1)),
                        reads=[W, h], writes=[ps])
                mk.op("act", lambda e, ps=ps, i2=i2: e.activation(out=sqq[i2][:, 0:tn], in_=ps[:, 0:tn], func=AF.Square),
                      reads=[ps], writes=[sqq[i2]])
                mk.op("pe", lambda e, pz=pz, i2=i2: e.matmul(pz[:, 0:tn], avgh[:], sqq[i2][:, 0:tn], start=True, stop=True),
                      reads=[avgh, sqq[i2]], writes=[pz])
                mk.op("act", lambda e, pz=pz, i2=i2: e.activation(out=rs[i2][:, 0:tn], in_=pz[:, 0:tn], func=AF.Sqrt,
                                                                 bias=self.eps_t[:, 0:1]), reads=[pz, self.eps_t], writes=[rs[i2]])
                mk.op("dve", lambda e, i2=i2: e.reciprocal(rs[i2][:, 0:tn], rs[i2][:, 0:tn]), reads=[rs[i2]], writes=[rs[i2]])
                mk.op("dve", lambda e, ps=ps, i2=i2, gcol=gcol: e.scalar_tensor_tensor(
                    out=qn[i2][:, 0:tn], in0=ps[:, 0:tn], scalar=gains[:, gcol:gcol + 1], in1=rs[i2][:, 0:tn],
                    op0=ALU.mult, op1=ALU.mult), reads=[ps, gains, rs[i2]], writes=[qn[i2]])
                if isq:
                    dst_t = qo[n % 3]
                    dst = dst_t[:, 0:tn]
                else:
                    dst_t = KT
                    dst = KT[:, hh - 8, t0:t0 + tn]
                if bi == 0:
                    mk.op("act", lambda e, i2=i2, dst=dst: e.copy(dst, qn[i2][:, 0:tn]), reads=[qn[i2]], writes=[dst_t])
                else:
                    mk.op("act", lambda e, i2=i2: e.copy(qb[i2][:, 0:tn], qn[i2][:, 0:tn]), reads=[qn[i2]], writes=[qb[i2]])
                    mk.op("pe", lambda e, pr=pr, i2=i2: e.matmul(pr[:, 0:tn], RT[:], qb[i2][:, 0:tn], start=True, stop=True),
                          reads=[RT, qb[i2]], writes=[pr])
                    mk.op("pool", lambda e, i2=i2, c=c: e.tensor_tensor(out=t1[i2][:, 0:tn], in0=qn[i2][:, 0:tn],
                                                                       in1=c[:, 0, 0:tn], op=ALU.mult),
                          reads=[qn[i2], c], writes=[t1[i2]])
                    mk.op("dve", lambda e, pr=pr, i2=i2, c=c: e.tensor_tensor(out=t2[i2][:, 0:tn], in0=pr[:, 0:tn],
                                                                             in1=c[:, 1, 0:tn], op=ALU.mult),
                          reads=[pr, c], writes=[t2[i2]])
                    mk.op("pool", lambda e, i2=i2, dst=dst: e.tensor_tensor(out=dst, in0=t1[i2][:, 0:tn],
                                                                           in1=t2[i2][:, 0:tn], op=ALU.add),
                          reads=[t1[i2], t2[i2]], writes=[dst_t])
                if isq:
                    mk.dma("act", QT[hh * 128:(hh + 1) * 128, t0:t0 + tn], dst, reads=[dst_t], writes=[QT])
            for a in range(tn // 128):
                pv = self.ps[6 + a % 2]
                for k in range(DC):
                    mk.op("pe", lambda e, pv=pv, k=k, a=a, h=h: e.matmul(
                        pv[:, 0:256], h[:, k, a * 128:(a + 1) * 128], wkv[:, k, 256:512],
                        start=(k == 0), stop=(k == DC - 1)), reads=[h, wkv], writes=[pv])
                ti = t0 // 128 + a
                mk.op("act", lambda e, pv=pv, ti=ti: e.copy(Vs[:, ti, :], pv[:, 0:256]), reads=[pv], writes=[Vs])
        mk.release(m0)
        m0 = mk.mark()
        qblk = [mk.sbuf("bq%d" % k, [128, 512], BF16) for k in range(2)]
        pT = [mk.sbuf("bp%d" % k, [128, 512], BF16) for k in range(3)]
        rz = [mk.sbuf("brz%d" % k, [128, 512], F32) for k in range(2)]
        zacc = [mk.sbuf("bza%d" % k, [128, 512], F32) for k in range(2)]
        ones_f = mk.sbuf("bones_f", [128, 128], F32)
        mk.op("pool", lambda e: e.memset(ones_f[:], 1.0), writes=[ones_f])
        ob = [mk.sbuf("bo%d" % k, [128, 512], BF16) for k in range(2)]
        scale = 128 ** -0.5
        nb = 0
        nk = 0
        for head in range(8):
            kvh = head // 4
            for bi, (t0, tn) in enumerate(BLOCKS):
                if bi == 0 and not with_ctx:
                    continue
                nkc = 2 if bi == 0 else NT // 128
                q = qblk[nb % 2]
                mk.dma("sp", q[:, 0:tn], QT[head * 128:(head + 1) * 128, t0:t0 + tn], reads=[QT], writes=[q])
                pO = self.ps[3 + nb % 2]
                pZ = self.ps[5 + nb % 2]
                def issue_S(kc_, idx):
                    pS_ = self.ps[idx % 3]
                    mk.op("pe", lambda e, pS_=pS_, kc_=kc_, q=q: e.matmul(
                        pS_[:, 0:tn], KT[:, kvh, kc_ * 128:(kc_ + 1) * 128], q[:, 0:tn], start=True, stop=True),
                        reads=[KT, q], writes=[pS_])
                issue_S(0, nk)
                for kc in range(nkc):
                    pS = self.ps[nk % 3]
                    p_ = pT[nk % 3]
                    if kc + 1 < nkc:
                        issue_S(kc + 1, nk + 1)
                    nk += 1
                    mk.op("act", lambda e, pS=pS, p_=p_: e.activation(out=p_[:, 0:tn], in_=pS[:, 0:tn], func=AF.Exp,
                                                                     scale=scale), reads=[pS], writes=[p_])
                    mk.op("pe", lambda e, pO=pO, kc=kc, p_=p_: e.matmul(
                        pO[:, 0:tn], Vs[:, kc, kvh * 128:(kvh + 1) * 128], p_[:, 0:tn],
                        start=(kc == 0), stop=(kc == nkc - 1)), reads=[Vs, p_], writes=[pO])
                    mk.op("pe", lambda e, pZ=pZ, kc=kc, p_=p_: e.matmul(
                        pZ[:, 0:tn], self.ones_b[:], p_[:, 0:tn], start=(kc == 0), stop=(kc == nkc - 1)),
                        reads=[self.ones_b, p_], writes=[pZ])
                r_ = rz[nb % 2]
                o_ = ob[nb % 2]
                mk.op("dve", lambda e, pZ=pZ, r_=r_: e.reciprocal(r_[:, 0:tn], pZ[:, 0:tn]), reads=[pZ], writes=[r_])
                mk.op("dve", lambda e, pO=pO, r_=r_, o_=o_: e.tensor_tensor(out=o_[:, 0:tn], in0=pO[:, 0:tn],
                                                                           in1=r_[:, 0:tn], op=ALU.mult),
                      reads=[pO, r_], writes=[o_])
                mk.dma("act", OT[head * 128:(head + 1) * 128, t0:t0 + tn], o_[:, 0:tn], reads=[o_], writes=[OT])
                nb += 1
        mk.release(m0)
        mk.release(mA)
        m0 = mk.mark()
        wo = self.load_w("wo", I["attn_w_o"][slot], DC, D)
        self.linear_resid(XT, OT, wo, DC, "g1", skip_ctx=not with_ctx)
        mk.release(m0)

    Prog.load_w = load_w
    Prog.linear_resid = linear_resid
    Prog.stage_attn = stage_attn


_attn_methods()


def _conf_methods():
    def stage_conf(self, slot, XT, HT, UT, VT, I):
        mk = self.mk
        m0 = mk.mark()
        W = self.load_w("wpw1", I["conv_w_pw1"][slot], DC, 2 * D)
        b1 = mk.sbuf("cb1", [128, 16], F32)
        mk.dma("sp", b1[:], I["conv_b_pw1_pc"][slot, :, :], writes=[b1])
        hb = [mk.sbuf("ch%d" % k, [128, DC, 512], BF16) for k in range(2)]
        sg = [mk.sbuf("csg%d" % k, [128, 512], F32) for k in range(2)]
        ub = [mk.sbuf("cu%d" % k, [128, DC, 512], BF16) for k in range(2)]
        n = 0
        for bi, (t0, tn) in enumerate(BLOCKS):
            h = hb[bi % 2]
            u = ub[bi % 2]
            mk.dma("sp", h[:, :, 0:tn], HT[:, t0:t0 + tn].rearrange("(k p) t -> p k t", p=128), reads=[HT], writes=[h])
            for j in range(DC):
                pa, pg = self.ps[(2 * n) % 8], self.ps[(2 * n + 1) % 8]
                s_ = sg[n % 2]
                n += 1
                for k in range(DC):
                    mk.op("pe", lambda e, pa=pa, k=k, j=j, h=h: e.matmul(
                        pa[:, 0:tn], W[:, k, j * 128:(j + 1) * 128], h[:, k, 0:tn], start=(k == 0), stop=(k == DC - 1)),
                        reads=[W, h], writes=[pa])
                for k in range(DC):
                    mk.op("pe", lambda e, pg=pg, k=k, j=j, h=h: e.matmul(
                        pg[:, 0:tn], W[:, k, D + j * 128:D + (j + 1) * 128], h[:, k, 0:tn], start=(k == 0), stop=(k == DC - 1)),
                        reads=[W, h], writes=[pg])
                mk.op("act", lambda e, pg=pg, s_=s_, j=j: e.activation(out=s_[:, 0:tn], in_=pg[:, 0:tn], func=AF.Sigmoid,
                                                                      bias=b1[:, 8 + j:9 + j]), reads=[pg, b1], writes=[s_])
                mk.op("dve", lambda e, pa=pa, s_=s_, j=j, u=u: e.scalar_tensor_tensor(
                    out=u[:, j, 0:tn], in0=pa[:, 0:tn], scalar=b1[:, j:j + 1], in1=s_[:, 0:tn], op0=ALU.add, op1=ALU.mult),
                    reads=[pa, b1, s_], writes=[u])
            mk.dma("act", UT[:, t0:t0 + tn].rearrange("(k p) t -> p k t", p=128), u[:, :, 0:tn], reads=[u], writes=[UT])
        mk.release(m0)
        m0 = mk.mark()
        wdw = mk.sbuf("cwdw", [128, DC, 31], F32)
        bdw = mk.sbuf("cbdw", [128, DC], F32)
        mk.dma("sp", wdw[:], I["conv_w_dw_pc"][slot, :, :, :], writes=[wdw])
        mk.dma("sp", bdw[:], I["conv_b_dw_pc"][slot, :, :], writes=[bdw])
        up = [mk.sbuf("cup%d" % k, [128, NT + 60], BF16) for k in range(2)]
        dg = [mk.sbuf("cdg%d" % k, [128, 31, 128], BF16) for k in range(2)]
        vo = [mk.sbuf("cvo%d" % k, [128, 512], F32) for k in range(3)]
        for k in range(2):
            mk.op("pool", lambda e, k=k: e.memset(up[k][:], 0.0), writes=[up[k]])
        n = 0
        for j in range(DC):
            u = up[j % 2]
            d_ = dg[j % 2]
            mk.dma("sp", u[:, 15:15 + CTX], UT[j * 128:(j + 1) * 128, 0:CTX], reads=[UT], writes=[u])
            mk.dma("sp", u[:, 45 + CTX:45 + CTX + SEQ], UT[j * 128:(j + 1) * 128, CTX:NT], reads=[UT], writes=[u])
            for k in range(31):
                mk.op("dve", lambda e, k=k, j=j, d_=d_: e.tensor_scalar(
                    out=d_[:, k, :], in0=self.ident_b[:], scalar1=wdw[:, j, k:k + 1], scalar2=None, op0=ALU.mult),
                    reads=[self.ident_b, wdw], writes=[d_])
            for bi, (t0, tn) in enumerate(BLOCKS):
                base = 0 if bi == 0 else 30
                ps = self.ps[n % 4]
                v = vo[n % 3]
                n += 1
                for k in range(31):
                    mk.op("pe", lambda e, ps=ps, k=k, u=u, d_=d_, s0=base + t0 + k: e.matmul(
                        ps[:, 0:tn], d_[:, k, :], u[:, s0:s0 + tn], start=(k == 0), stop=(k == 30)),
                        reads=[d_, u], writes=[ps])
                mk.op("act", lambda e, ps=ps, v=v, j=j: e.activation(out=v[:, 0:tn], in_=ps[:, 0:tn], func=AF.Identity,
                                                                    bias=bdw[:, j:j + 1]), reads=[ps, bdw], writes=[v])
                mk.dma("act", VT[j * 128:(j + 1) * 128, t0:t0 + tn], v[:, 0:tn], reads=[v], writes=[VT])
        mk.release(m0)
        m0 = mk.mark()
        lng = mk.sbuf("clng", [128, DC], F32)
        lnb = mk.sbuf("clnb", [128, DC], F32)
        mk.dma("sp", lng[:], I["conv_ln_g_pc"][slot, :, :], writes=[lng])
        mk.dma("sp", lnb[:], I["conv_ln_b_pc"][slot, :, :], writes=[lnb])
        avgf = mk.sbuf("cavgf", [128, 128], F32)
        mk.op("pool", lambda e: e.memset(avgf[:], 1.0 / D), writes=[avgf])
        vb = [mk.sbuf("cv%d" % k, [128, DC, 512], F32) for k in range(2)]
        sq = mk.sbuf("csq", [128, DC, 512], F32)
        mu = mk.sbuf("cmu", [128, 512], F32)
        var = mk.sbuf("cvar", [128, 512], F32)
        ob = [mk.sbuf("co%d" % k, [128, DC, 512], BF16) for k in range(2)]
        for bi, (t0, tn) in enumerate(BLOCKS):
            v = vb[bi % 2]
            o = ob[bi % 2]
            mk.dma("sp", v[:, :, 0:tn], VT[:, t0:t0 + tn].rearrange("(k p) t -> p k t", p=128), reads=[VT], writes=[v])
            pm, pq = self.ps[0], self.ps[1]
            for j in range(DC):
                mk.op("act", lambda e, j=j, v=v: e.activation(out=sq[:, j, 0:tn], in_=v[:, j, 0:tn], func=AF.Square),
                      reads=[v], writes=[sq])
            for j in range(DC):
                mk.op("pe", lambda e, j=j, v=v: e.matmul(pm[:, 0:tn], avgf[:], v[:, j, 0:tn], start=(j == 0), stop=(j == DC - 1)),
                      reads=[avgf, v], writes=[pm])
            for j in range(DC):
                mk.op("pe", lambda e, j=j: e.matmul(pq[:, 0:tn], avgf[:], sq[:, j, 0:tn], start=(j == 0), stop=(j == DC - 1)),
                      reads=[avgf, sq], writes=[pq])
            mk.op("act", lambda e: e.copy(mu[:, 0:tn], pm[:, 0:tn]), reads=[pm], writes=[mu])
            mk.op("dve", lambda e: e.tensor_tensor(out=var[:, 0:tn], in0=mu[:, 0:tn], in1=mu[:, 0:tn], op=ALU.mult),
                  reads=[mu], writes=[var])
            mk.op("dve", lambda e: e.tensor_tensor(out=var[:, 0:tn], in0=pq[:, 0:tn], in1=var[:, 0:tn], op=ALU.subtract),
                  reads=[pq, var], writes=[var])
            mk.op("act", lambda e: e.activation(out=var[:, 0:tn], in_=var[:, 0:tn], func=AF.Sqrt, bias=self.eps_t[:, 0:1]),
                  reads=[var, self.eps_t], writes=[var])
            mk.op("dve", lambda e: e.reciprocal(var[:, 0:tn], var[:, 0:tn]), reads=[var], writes=[var])
            for j in range(DC):
                mk.op("pool", lambda e, j=j, v=v: e.tensor_tensor(out=v[:, j, 0:tn], in0=v[:, j, 0:tn], in1=mu[:, 0:tn],
                                                                 op=ALU.subtract), reads=[v, mu], writes=[v])
                mk.op("dve", lambda e, j=j, v=v: e.scalar_tensor_tensor(
                    out=v[:, j, 0:tn], in0=v[:, j, 0:tn], scalar=lng[:, j:j + 1], in1=var[:, 0:tn], op0=ALU.mult, op1=ALU.mult),
                    reads=[v, lng, var], writes=[v])
                mk.op("act", lambda e, j=j, v=v, o=o: e.activation(out=o[:, j, 0:tn], in_=v[:, j, 0:tn], func=AF.Silu,
                                                                  bias=lnb[:, j:j + 1]), reads=[v, lnb], writes=[o])
            mk.dma("act", HT[:, t0:t0 + tn].rearrange("(k p) t -> p k t", p=128), o[:, :, 0:tn], reads=[o], writes=[HT])
        mk.release(m0)
        m0 = mk.mark()
        w2 = self.load_w("wpw2", I["conv_w_pw2"][slot], DC, D)
        b2 = mk.sbuf("cb2", [128, DC], F32)
        mk.dma("sp", b2[:], I["conv_b_pw2_pc"][slot, :, :], writes=[b2])
        self.linear_resid(XT, HT, w2, DC, "g1", bias=b2)
        mk.release(m0)

    Prog.stage_conf = stage_conf


_conf_methods()


def hy_consts(L):
    N = 2 * L
    N1 = N // 64
    H = N1 // 2
    bf = ml_dtypes.bfloat16
    c = {}
    n1 = np.arange(H)[:, None].astype(np.float64)
    k1 = np.arange(N1)[None, :].astype(np.float64)
    def cs(ang):
        return np.cos(ang), -np.sin(ang)
    fc, fs = cs(2 * np.pi * n1 * k1 / N1)
    bc, bs = cs(2 * np.pi * (N1 - 1 - n1) * k1 / N1)
    b0c, b0s = cs(2 * np.pi * (N1 - n1) * k1 / N1)
    c["F1"] = np.stack([fc, fs, bc, bs, b0c, b0s], 1).astype(bf)
    n2 = np.arange(64)[:, None].astype(np.float64)
    k2 = np.arange(64)[None, :].astype(np.float64)
    G = np.zeros((N1, 128, 5, 128), np.float64)
    for kk in range(N1):
        ang = 2 * np.pi * (n2 * kk / N + n2 * k2 / 64)
        Gr, Gi = np.cos(ang), -np.sin(ang)
        Mr, Mi = Gr.T, -Gi.T
        blocks = [((Gr, Gi), (-Gi, Gr)), ((-Gi, Gr), (-Gr, -Gi)), ((Gr, Gr), (-Gi, -Gi)),
                  ((Gi, Gi), (Gr, Gr)), ((Mr, Mi), (-Mi, Mr))]
        for f, ((a, b), (cc, d)) in enumerate(blocks):
            G[kk, 0:64, f, 0:64] = a
            G[kk, 0:64, f, 64:128] = b
            G[kk, 64:128, f, 0:64] = cc
            G[kk, 64:128, f, 64:128] = d
    c["G"] = np.ascontiguousarray(G.transpose(1, 0, 2, 3)).astype(bf)
    t1 = np.arange(H)[None, :].astype(np.float64)
    kk1 = np.arange(N1)[:, None].astype(np.float64)
    th = 2 * np.pi * t1 * kk1 / N1
    c["Fi"] = np.stack([np.cos(th) / N, -np.sin(th) / N], 1).astype(bf)
    pos = (64 * np.arange(H)[:, None] + np.arange(64)[None, :]).astype(np.float32)
    c["lneg"] = (-(pos / np.float32(L - 1))).astype(np.float32)
    p = np.arange(L, dtype=np.float32)[:, None]
    t = p / np.float32(L - 1)
    w = np.float32(2.0 * math.pi) * p / np.float32(L)
    bands = np.linspace(1e-4, 7, 8, dtype=np.float32)
    feats = np.concatenate([t, np.cos(bands * w), -np.sin(bands * w)], axis=-1).astype(np.float32)
    c["featsT"] = np.ascontiguousarray(feats.T)
    return c


def _hy_methods():
    def hy_filter(self, S, I, HH):
        mk = self.mk
        L, N1, H, tag = S["L"], S["N1"], S["H"], S["tag"]
        m0 = mk.mark()
        ft = mk.sbuf("hf_ft", [17, L], F32)
        W1 = mk.sbuf("hf_w1", [17, 64], F32)
        W2 = mk.sbuf("hf_w2", [64, 64], F32)
        W3 = mk.sbuf("hf_w3", [64, 4096], BF16)
        fv = mk.sbuf("hf_fv", [64, 4], F32)
        fb = mk.sbuf("hf_fb", [64, 2], F32)
        z1 = mk.sbuf("hf_z1", [64, L], F32)
        z2 = mk.sbuf("hf_z2", [64, L], F32)
        z2b = mk.sbuf("hf_z2b", [64, L], BF16)
        tmp = mk.sbuf("hf_tmp", [64, 512], F32)
        F1 = mk.sbuf("hf_F1", [max(H, 1), 6, N1], BF16)
        lneg = mk.sbuf("hf_lneg", [H, 64], F32)
        dab = mk.sbuf("hf_dab", [H, D], F32)
        mk.dma("sp", ft[:], I["hy_featsT_" + tag][:, :], writes=[ft])
        mk.dma("sp", W1[:], I["hy_f_w1"][0, :, :], writes=[W1])
        mk.dma("sp", W2[:], I["hy_f_w2"][0, :, :], writes=[W2])
        mk.dma("pool", W3[:], I["hy_f_w3"][0, :, :], writes=[W3])
        mk.dma("sp", fv[:], I["hy_fvec"][:, :], writes=[fv])
        mk.dma("sp", F1[:], I["hy_F1_" + tag][:, :, :], writes=[F1])
        mk.dma("sp", lneg[:], I["hy_lneg_" + tag][:, :], writes=[lneg])
        mk.dma("sp", dab[:], I["hy_dabs"][0:H, :], writes=[dab])
        mk.op("dve", lambda e: e.tensor_tensor(out=fb[:, 0:1], in0=fv[:, 0:1], in1=fv[:, 1:2], op=ALU.mult), reads=[fv], writes=[fb])
        mk.op("dve", lambda e: e.tensor_tensor(out=fb[:, 1:2], in0=fv[:, 2:3], in1=fv[:, 3:4], op=ALU.mult), reads=[fv], writes=[fb])
        for layer, (Wm, src, dst, kin) in enumerate(((W1, ft, z1, 17), (W2, z1, z2, 64))):
            for c0 in range(0, L, 512):
                cn = min(512, L - c0)
                ps = self.ps[(c0 // 512) % 2]
                mk.op("pe", lambda e, ps=ps, Wm=Wm, src=src, c0=c0, cn=cn, kin=kin: e.matmul(
                    ps[0:64, 0:cn], Wm[0:kin, :], src[0:kin, c0:c0 + cn], start=True, stop=True), reads=[Wm, src], writes=[ps])
                d_ = dst[:, c0:c0 + cn]
                mk.op("dve", lambda e, ps=ps, d_=d_, cn=cn, layer=layer: e.tensor_scalar(
                    out=d_, in0=ps[0:64, 0:cn], scalar1=fv[:, 2 * layer + 1:2 * layer + 2], scalar2=fb[:, layer:layer + 1],
                    op0=ALU.mult, op1=ALU.add), reads=[ps, fv, fb], writes=[dst])
                for _ in range(2):
                    mk.op("dve", lambda e, d_=d_, cn=cn: e.tensor_scalar(out=tmp[:, 0:cn], in0=d_, scalar1=math.pi,
                          scalar2=2 * math.pi, op0=ALU.is_gt, op1=ALU.mult), reads=[dst], writes=[tmp])
                    mk.op("dve", lambda e, d_=d_, cn=cn: e.tensor_tensor(out=d_, in0=d_, in1=tmp[:, 0:cn], op=ALU.subtract),
                          reads=[dst, tmp], writes=[dst])
                    mk.op("dve", lambda e, d_=d_, cn=cn: e.tensor_scalar(out=tmp[:, 0:cn], in0=d_, scalar1=-math.pi,
                          scalar2=2 * math.pi, op0=ALU.is_lt, op1=ALU.mult), reads=[dst], writes=[tmp])
                    mk.op("dve", lambda e, d_=d_, cn=cn: e.tensor_tensor(out=d_, in0=d_, in1=tmp[:, 0:cn], op=ALU.add),
                          reads=[dst, tmp], writes=[dst])
                mk.op("act", lambda e, d_=d_: e.activation(out=d_, in_=d_, func=AF.Sin), reads=[dst], writes=[dst])
        mk.op("act", lambda e: e.copy(z2b[:], z2[:]), reads=[z2], writes=[z2b])
        dec = [mk.sbuf("hf_dec%d" % k, [H, D], F32) for k in range(4)]
        hd = [mk.sbuf("hf_hd%d" % k, [H, D], F32) for k in range(4)]
        hdb = [mk.sbuf("hf_hdb%d" % k, [H, D], BF16) for k in range(4)]
        ha = [mk.sbuf("hf_ha%d" % k, [H, D], BF16) for k in range(4)]
        Asb = [mk.sbuf("hf_A%d" % k, [N1, 2, D], BF16) for k in range(2)]
        rn = mk.sbuf("hf_rn", [128, D], F32)
        A = self.dram["hyA"]
        Bt = [mk.sbuf("hf_B%d" % k, [128, D], BF16) for k in range(2)]
        Gt = [mk.sbuf("hf_Gt%d" % k, [128, 2, 128], BF16) for k in range(2)]
        Ho = [mk.sbuf("hf_Ho%d" % k, [128, D], BF16) for k in range(4)]
        for o in range(2):
            pn = (self.ps[6], self.ps[7])
            for n2 in range(64):
                n2b = (64 - n2) % 64
                hb2 = []
                for dr in range(2):
                    col = n2 if dr == 0 else n2b
                    i2 = dr + 2 * (n2 % 2)
                    for hf in range(2):
                        ph = self.ps[dr * 2 + hf]
                        w0 = (dr * 2 + o) * D + hf * 512
                        mk.op("pe", lambda e, ph=ph, col=col, w0=w0: e.matmul(
                            ph[0:H, :], z2b[:, col:L:64], W3[:, w0:w0 + 512], start=True, stop=True),
                            reads=[z2b, W3], writes=[ph])
                    mk.op("act", lambda e, i2=i2, col=col: e.activation(out=dec[i2][:], in_=dab[:], func=AF.Exp,
                                                                       scale=lneg[:, col:col + 1]), reads=[dab, lneg], writes=[dec[i2]])
                    for hf in range(2):
                        ph = self.ps[dr * 2 + hf]
                        mk.op("dve", lambda e, ph=ph, i2=i2, hf=hf: e.tensor_tensor(
                            out=hd[i2][:, hf * 512:(hf + 1) * 512], in0=ph[0:H, :], in1=dec[i2][:, hf * 512:(hf + 1) * 512],
                            op=ALU.mult), reads=[ph, dec[i2]], writes=[hd[i2]])
                    if dr == 1 and n2 == 0:
                        mk.op("dve", lambda e, i2=i2: e.memset(hd[i2][0:1, :], 0.0), reads=[hd[i2]], writes=[hd[i2]])
                    hb_ = hdb[(n2 % 2) * 2 + dr]
                    hb2.append(hb_)
                    mk.op("act", lambda e, i2=i2, hb_=hb_: e.copy(hb_[:], hd[i2][:]), reads=[hd[i2]], writes=[hb_])
                    mk.op("dve", lambda e, i2=i2: e.scalar_tensor_tensor(out=ha[i2][:], in0=hd[i2][:], scalar=-1.0, in1=hd[i2][:],
                                                                        op0=ALU.mult, op1=ALU.max), reads=[hd[i2]], writes=[ha[i2]])
                    for hf in range(2):
                        first = (n2 == 0 and dr == 0)
                        last = (n2 == 63 and dr == 1)
                        mk.op("pe", lambda e, i2=i2, hf=hf, first=first, last=last: e.matmul(
                            pn[hf][:, :], self.ones_b[0:H, :], ha[i2][:, hf * 512:(hf + 1) * 512], start=first, stop=last),
                            reads=[self.ones_b, ha[i2]], writes=[pn[hf]])
                As = Asb[n2 % 2]
                fbi = 4 if n2 == 0 else 2
                for ri in range(2):
                    for hf in range(2):
                        pA = self.ps[4 + hf]
                        mk.op("pe", lambda e, pA=pA, ri=ri, hf=hf, hb2=hb2: e.matmul(
                            pA[0:N1, :], F1[0:H, ri, :], hb2[0][:, hf * 512:(hf + 1) * 512], start=True, stop=False),
                            reads=[F1, hb2[0]], writes=[pA])
                        mk.op("pe", lambda e, pA=pA, ri=ri, hf=hf, hb2=hb2, fbi=fbi: e.matmul(
                            pA[0:N1, :], F1[0:H, fbi + ri, :], hb2[1][:, hf * 512:(hf + 1) * 512], start=False, stop=True),
                            reads=[F1, hb2[1]], writes=[pA])
                        eng = "act" if hf else "dve"
                        if eng == "act":
                            mk.op("act", lambda e, pA=pA, ri=ri, hf=hf, As=As: e.copy(As[:, ri, hf * 512:(hf + 1) * 512], pA[0:N1, :]),
                                  reads=[pA], writes=[As])
                        else:
                            mk.op("dve", lambda e, pA=pA, ri=ri, hf=hf, As=As: e.tensor_copy(As[:, ri, hf * 512:(hf + 1) * 512], pA[0:N1, :]),
                                  reads=[pA], writes=[As])
                for ri in range(2):
                    mk.dma("act", A[ri, 0:N1, n2, :], As[:, ri, :], reads=[As], writes=[A])
            for hf in range(2):
                mk.op("dve", lambda e, hf=hf: e.reciprocal(rn[:, hf * 512:(hf + 1) * 512], pn[hf][:, :]), reads=[pn[hf]], writes=[rn])
            for kk in range(N1):
                B = Bt[kk % 2]
                for ri in range(2):
                    mk.dma("sp", B[ri * 64:(ri + 1) * 64, :], A[ri, kk, :, :], reads=[A], writes=[B])
                g_ = Gt[kk % 2]
                mk.dma("sp", g_[:], I["hy_G_" + tag][:, kk, 2:4, :], writes=[g_])
                for ri in range(2):
                    ho = Ho[(2 * kk + ri) % 4]
                    for hf in range(2):
                        pX = self.ps[ri * 2 + hf]
                        mk.op("pe", lambda e, pX=pX, ri=ri, hf=hf, B=B, g_=g_: e.matmul(
                            pX[:, :], g_[:, ri, :], B[:, hf * 512:(hf + 1) * 512], start=True, stop=True), reads=[g_, B], writes=[pX])
                        mk.op("dve", lambda e, pX=pX, hf=hf, ho=ho: e.tensor_tensor(
                            out=ho[:, hf * 512:(hf + 1) * 512], in0=pX[:, :], in1=rn[:, hf * 512:(hf + 1) * 512], op=ALU.mult),
                            reads=[pX, rn], writes=[ho])
                    mk.dma("act", HH[o][ri, kk, :, :], ho[:], reads=[ho], writes=[HH[o]])
        mk.release(m0)

    Prog.hy_filter = hy_filter


_hy_methods()


def _hy_methods2():
    def hy_conv(self, S, o, I, zsrc, zrow0, gate, grow0, lbias, HH, dst, dst_bf16):
        mk = self.mk
        L, N1, H, tag, col0 = S["L"], S["N1"], S["H"], S["tag"], S["col0"]
        A, Cs = self.dram["hyA"], self.dram["hyC"]
        m0 = mk.mark()
        zb = mk.sbuf("hc_zb", [128, DC, L], BF16)
        F1 = mk.sbuf("hc_F1", [max(H, 1), 6, N1], BF16)
        mk.dma("sp", F1[:], I["hy_F1_" + tag][:, :, :], writes=[F1])
        for j in range(DC):
            mk.dma("pool", zb[:, j, :], zsrc[zrow0 + j * 128:zrow0 + (j + 1) * 128, col0:col0 + L], reads=[zsrc], writes=[zb])
        zT = [mk.sbuf("hc_zT%d" % k, [H, D], BF16) for k in range(2)]
        Asb = [mk.sbuf("hc_A%d" % k, [N1, 2, D], BF16) for k in range(2)]
        for n2 in range(64):
            pt = self.ps[n2 % 2]
            ptb = pt[:, 0:512].bitcast(BF16)
            for j in range(DC):
                mk.op("pe", lambda e, ptb=ptb, j=j, n2=n2: e.transpose(ptb[0:H, j * 128:(j + 1) * 128], zb[:, j, n2:L:64],
                                                                      self.ident_b[:]), reads=[zb, self.ident_b], writes=[pt])
            z_ = zT[n2 % 2]
            mk.op("act", lambda e, ptb=ptb, z_=z_: e.copy(z_[:], ptb[0:H, :]), reads=[pt], writes=[z_])
            As = Asb[n2 % 2]
            for ri in range(2):
                for hf in range(2):
                    pA = self.ps[2 + ri * 2 + hf]
                    mk.op("pe", lambda e, pA=pA, ri=ri, hf=hf, z_=z_: e.matmul(
                        pA[0:N1, :], F1[0:H, ri, :], z_[:, hf * 512:(hf + 1) * 512], start=True, stop=True),
                        reads=[F1, z_], writes=[pA])
                    if hf:
                        mk.op("act", lambda e, pA=pA, ri=ri, hf=hf, As=As: e.copy(As[:, ri, hf * 512:(hf + 1) * 512], pA[0:N1, :]),
                              reads=[pA], writes=[As])
                    else:
                        mk.op("dve", lambda e, pA=pA, ri=ri, hf=hf, As=As: e.tensor_copy(As[:, ri, hf * 512:(hf + 1) * 512], pA[0:N1, :]),
                              reads=[pA], writes=[As])
            for ri in range(2):
                mk.dma("act", A[ri, 0:N1, n2, :], As[:, ri, :], reads=[As], writes=[A])
        mk.release(m0)
        m0 = mk.mark()
        Bt = [mk.sbuf("hc_B%d" % k, [128, D], BF16) for k in range(2)]
        Gt = [mk.sbuf("hc_G%d" % k, [128, 5, 128], BF16) for k in range(2)]
        Hr = [mk.sbuf("hc_Hr%d" % k, [128, D], BF16) for k in range(2)]
        Hi = [mk.sbuf("hc_Hi%d" % k, [128, D], BF16) for k in range(2)]
        ta = [mk.sbuf("hc_ta%d" % k, [128, D], F32) for k in range(2)]
        tb = [mk.sbuf("hc_tb%d" % k, [128, D], F32) for k in range(2)]
        Y = [mk.sbuf("hc_Y%d" % k, [128, D], BF16) for k in range(2)]
        Cb = [mk.sbuf("hc_C%d" % k, [128, D], BF16) for k in range(2)]
        for kk in range(N1):
            i2 = kk % 2
            B, g_, hr, hi = Bt[i2], Gt[i2], Hr[i2], Hi[i2]
            for ri in range(2):
                mk.dma("sp", B[ri * 64:(ri + 1) * 64, :], A[ri, kk, :, :], reads=[A], writes=[B])
            mk.dma("sp", g_[:], I["hy_G_" + tag][:, kk, :, :], writes=[g_])
            mk.dma("sp", hr[:], HH[o][0, kk, :, :], reads=[HH[o]], writes=[hr])
            mk.dma("sp", hi[:], HH[o][1, kk, :, :], reads=[HH[o]], writes=[hi])
            for hf in range(2):
                sl = slice(hf * 512, (hf + 1) * 512)
                pa, pb = self.ps[hf * 2], self.ps[hf * 2 + 1]
                mk.op("pe", lambda e, pa=pa, sl=sl, B=B, g_=g_: e.matmul(pa[:, :], g_[:, 0, :], B[:, sl], start=True, stop=True),
                      reads=[g_, B], writes=[pa])
                mk.op("pe", lambda e, pb=pb, sl=sl, B=B, g_=g_: e.matmul(pb[:, :], g_[:, 1, :], B[:, sl], start=True, stop=True),
                      reads=[g_, B], writes=[pb])
                mk.op("dve", lambda e, pa=pa, sl=sl, hr=hr, i2=i2: e.tensor_tensor(out=ta[i2][:, sl], in0=pa[:, :], in1=hr[:, sl],
                                                                                  op=ALU.mult), reads=[pa, hr], writes=[ta[i2]])
                mk.op("dve", lambda e, pb=pb, sl=sl, hi=hi, i2=i2: e.tensor_tensor(out=tb[i2][:, sl], in0=pb[:, :], in1=hi[:, sl],
                                                                                  op=ALU.mult), reads=[pb, hi], writes=[tb[i2]])
                mk.op("pool", lambda e, sl=sl, i2=i2: e.tensor_tensor(out=Y[i2][:, sl], in0=ta[i2][:, sl], in1=tb[i2][:, sl],
                                                                     op=ALU.add), reads=[ta[i2], tb[i2]], writes=[Y[i2]])
            for hf in range(2):
                sl = slice(hf * 512, (hf + 1) * 512)
                pc = self.ps[4 + (2 * kk + hf) % 4]
                mk.op("pe", lambda e, pc=pc, sl=sl, i2=i2, g_=g_: e.matmul(pc[:, :], g_[:, 4, :], Y[i2][:, sl], start=True, stop=True),
                      reads=[g_, Y[i2]], writes=[pc])
                mk.op("act", lambda e, pc=pc, sl=sl, i2=i2: e.copy(Cb[i2][:, sl], pc[:, :]), reads=[pc], writes=[Cb[i2]])
            for ri in range(2):
                mk.dma("act", Cs[ri, :, kk, :], Cb[i2][ri * 64:(ri + 1) * 64, :], reads=[Cb[i2]], writes=[Cs])
        mk.release(m0)
        m0 = mk.mark()
        Fi = mk.sbuf("hc_Fi", [N1, 2, max(H, 1)], BF16)
        mk.dma("sp", Fi[:], I["hy_Fi_" + tag][:, :, :], writes=[Fi])
        lb = mk.sbuf("hc_lb", [128, DC], F32)
        mk.dma("sp", lb[:], lbias, writes=[lb])
        yh = mk.sbuf("hc_yh", [128, 4, L], F32)
        Ct = [mk.sbuf("hc_Ct%d" % k, [N1, 2, 512], BF16) for k in range(3)]
        zr = [mk.sbuf("hc_zr%d" % k, [128, L], F32) for k in range(2)]
        gr = [mk.sbuf("hc_gr%d" % k, [128, L], F32) for k in range(2)]
        ob = [mk.sbuf("hc_ob%d" % k, [128, L], BF16 if dst_bf16 else F32) for k in range(2)]
        per_bank = max(1, 512 // (4 * H))
        for half in range(2):
            for t2 in range(64):
                c_ = Ct[t2 % 3]
                for ri in range(2):
                    mk.dma("sp", c_[:, ri, :], Cs[ri, t2, 0:N1, half * 512:(half + 1) * 512], reads=[Cs], writes=[c_])
                slot = t2 % per_bank
                py = self.ps[(t2 // per_bank) % 8]
                for c4 in range(4):
                    o0 = slot * 4 * H + c4 * H
                    mk.op("pe", lambda e, py=py, o0=o0, c4=c4, c_=c_: e.matmul(
                        py[:, o0:o0 + H], c_[:, 0, c4 * 128:(c4 + 1) * 128], Fi[:, 0, :], start=True, stop=False),
                        reads=[c_, Fi], writes=[py])
                    mk.op("pe", lambda e, py=py, o0=o0, c4=c4, c_=c_: e.matmul(
                        py[:, o0:o0 + H], c_[:, 1, c4 * 128:(c4 + 1) * 128], Fi[:, 1, :], start=False, stop=True),
                        reads=[c_, Fi], writes=[py])
                src = py[:, slot * 4 * H:(slot + 1) * 4 * H].rearrange("p (a b) -> p a b", b=H)
                if t2 % 2:
                    mk.op("act", lambda e, src=src, t2=t2: e.copy(yh[:, :, t2:L:64], src), reads=[py], writes=[yh])
                else:
                    mk.op("dve", lambda e, src=src, t2=t2: e.tensor_copy(yh[:, :, t2:L:64], src), reads=[py], writes=[yh])
            for c4 in range(4):
                cj = half * 4 + c4
                z_, g_, o_ = zr[c4 % 2], gr[c4 % 2], ob[c4 % 2]
                mk.dma("sp", z_[:], zsrc[zrow0 + cj * 128:zrow0 + (cj + 1) * 128, col0:col0 + L], reads=[zsrc], writes=[z_])
                mk.dma("sp", g_[:], gate[grow0 + cj * 128:grow0 + (cj + 1) * 128, col0:col0 + L], reads=[gate], writes=[g_])
                mk.op("dve", lambda e, z_=z_, cj=cj, c4=c4: e.scalar_tensor_tensor(
                    out=z_[:], in0=z_[:], scalar=lb[:, cj:cj + 1], in1=yh[:, c4, :], op0=ALU.mult, op1=ALU.add),
                    reads=[z_, lb, yh], writes=[z_])
                mk.op("pool", lambda e, z_=z_, g_=g_, o_=o_: e.tensor_tensor(out=o_[:], in0=z_[:], in1=g_[:], op=ALU.mult),
                      reads=[z_, g_], writes=[o_])
                mk.dma("act", dst[cj * 128:(cj + 1) * 128, col0:col0 + L], o_[:], reads=[o_], writes=[dst])
        mk.release(m0)

    def stage_hyena(self, slot, XT, HT, I):
        mk = self.mk
        U0, ZT, Z1 = self.dram["hyU0"], self.dram["hyZ"], self.dram["hyZ1"]
        m0 = mk.mark()
        W = self.load_w("hy_win", I["hy_w_in"][slot], DC, 3 * D)
        bi_ = mk.sbuf("hy_bin", [128, 24], F32)
        mk.dma("sp", bi_[:], I["hy_b_in_pc"][:, :], writes=[bi_])
        hb = [mk.sbuf("hy_h%d" % k, [128, DC, 512], BF16) for k in range(2)]
        uo = [mk.sbuf("hy_uo%d" % k, [128, 512], F32) for k in range(3)]
        n = 0
        for bi, (t0, tn) in enumerate(BLOCKS):
            h = hb[bi % 2]
            mk.dma("sp", h[:, :, 0:tn], HT[:, t0:t0 + tn].rearrange("(k p) t -> p k t", p=128), reads=[HT], writes=[h])
            for m in range(24):
                ps = self.ps[n % 4]
                u = uo[n % 3]
                n += 1
                for k in range(DC):
                    mk.op("pe", lambda e, ps=ps, k=k, m=m, h=h: e.matmul(
                        ps[:, 0:tn], W[:, k, m * 128:(m + 1) * 128], h[:, k, 0:tn], start=(k == 0), stop=(k == DC - 1)),
                        reads=[W, h], writes=[ps])
                mk.op("act", lambda e, ps=ps, u=u, m=m: e.activation(out=u[:, 0:tn], in_=ps[:, 0:tn], func=AF.Identity,
                                                                    bias=bi_[:, m:m + 1]), reads=[ps, bi_], writes=[u])
                mk.dma("act", U0[m * 128:(m + 1) * 128, t0:t0 + tn], u[:, 0:tn], reads=[u], writes=[U0])
        mk.release(m0)
        m0 = mk.mark()
        ws = mk.sbuf("hy_ws", [128, 24, 3], F32)
        bs = mk.sbuf("hy_bs", [128, 24], F32)
        mk.dma("sp", ws[:], I["hy_w_short_pc"][:, :, :], writes=[ws])
        mk.dma("sp", bs[:], I["hy_b_short_pc"][:, :], writes=[bs])
        W_ = NT + 4
        ub = [mk.sbuf("hy_ub%d" % k, [128, W_], F32) for k in range(2)]
        vb = [mk.sbuf("hy_vb%d" % k, [128, W_], F32) for k in range(2)]
        for k in range(2):
            mk.op("pool", lambda e, k=k: e.memset(ub[k][:], 0.0), writes=[ub[k]])
        for m in range(24):
            u, v = ub[m % 2], vb[m % 2]
            mk.dma("sp", u[:, 1:1 + CTX], U0[m * 128:(m + 1) * 128, 0:CTX], reads=[U0], writes=[u])
            mk.dma("sp", u[:, 3 + CTX:3 + NT], U0[m * 128:(m + 1) * 128, CTX:NT], reads=[U0], writes=[u])
            n_ = W_ - 2
            mk.op("dve", lambda e, u=u, v=v, m=m: e.tensor_scalar(out=v[:, 1:1 + n_], in0=u[:, 0:n_], scalar1=ws[:, m, 0:1],
                                                                 scalar2=bs[:, m:m + 1], op0=ALU.mult, op1=ALU.add),
                  reads=[u, ws, bs], writes=[v])
            mk.op("dve", lambda e, u=u, v=v, m=m: e.scalar_tensor_tensor(out=v[:, 1:1 + n_], in0=u[:, 1:1 + n_], scalar=ws[:, m, 1:2],
                                                                        in1=v[:, 1:1 + n_], op0=ALU.mult, op1=ALU.add),
                  reads=[u, ws, v], writes=[v])
            mk.op("dve", lambda e, u=u, v=v, m=m: e.scalar_tensor_tensor(out=v[:, 1:1 + n_], in0=u[:, 2:2 + n_], scalar=ws[:, m, 2:3],
                                                                        in1=v[:, 1:1 + n_], op0=ALU.mult, op1=ALU.add),
                  reads=[u, ws, v], writes=[v])
            mk.dma("act", ZT[m * 128:(m + 1) * 128, 0:CTX], v[:, 1:1 + CTX], reads=[v], writes=[ZT])
            mk.dma("act", ZT[m * 128:(m + 1) * 128, CTX:NT], v[:, 3 + CTX:3 + NT], reads=[v], writes=[ZT])
        mk.release(m0)
        seqs = [dict(L=SEQ, N1=128, H=64, tag="lat", col0=CTX)]
        if not self.cfg.get("ctx_direct", True):
            seqs.append(dict(L=CTX, N1=8, H=4, tag="ctx", col0=0))
        for S in seqs:
            HH = [self.dram["hyHH_%s%d" % (S["tag"], o)] for o in range(2)]
            self.hy_filter(S, I, HH)
            self.hy_conv(S, 0, I, ZT, 0, ZT, D, I["hy_lbias_pc"][0, :, :], HH, Z1, False)
            self.hy_conv(S, 1, I, Z1, 0, ZT, 2 * D, I["hy_lbias_pc"][1, :, :], HH, HT, True)
        if self.cfg.get("ctx_direct", True):
            self.hy_ctx_direct(I)
        m0 = mk.mark()
        wo = self.load_w("hy_wout", I["hy_w_out"][slot], DC, D)
        bo = mk.sbuf("hy_bo", [128, DC], F32)
        mk.dma("sp", bo[:], I["hy_b_out_pc"][:, :], writes=[bo])
        self.linear_resid(XT, HT, wo, DC, "g1", bias=bo)
        mk.release(m0)

    Prog.hy_conv = hy_conv
    Prog.stage_hyena = stage_hyena


_hy_methods2()


_PROG = {}


def kernel(**inputs):
    inp = {k: np.asarray(v) for k, v in inputs.items()}
    if "p" not in _PROG:
        _PROG["p"] = build_program()
    p = _PROG["p"]
    shared = None
    in_maps = []
    for b in range(8):
        m = host_inputs(inp, b)
        if shared is None:
            shared = m
        else:
            for k in m:
                if k not in ("x", "ctx", "c_pc"):
                    m[k] = shared[k]
        in_maps.append(m)
    res = run_bass_kernel_spmd(p.nc, in_maps, core_ids=list(range(8)))
    out = np.stack([np.asarray(r["y"], dtype=np.float32) for r in res.results], axis=0)
    return out


BSL = 256
BSH = 8
NBLK = 66
NSLOT = NBLK * BSL
NTILE = NT // 128
IOA = bass.IndirectOffsetOnAxis


def _sparse_methods():
    def indirect(mk, out, out_off, in_, in_off, reads, writes):
        qlane = mk.lanes["pool"]
        dl = mk.dma_lanes[mk.dma_rr]
        mk.dma_rr = (mk.dma_rr + 1) % len(mk.dma_lanes)
        need, _ = mk._deps(dl, reads, writes)
        if dl.count > 0:
            need[dl.name] = max(need.get(dl.name, 0), dl.count)
        for ln, ix in need.items():
            if qlane.seen.get(ln, 0) >= ix:
                continue
            qlane.seen[ln] = ix
            qlane.eng.wait_ge(mk.lanes[ln].sem, ix)
        inst = qlane.eng.indirect_dma_start(out=out, out_offset=out_off, in_=in_, in_offset=in_off)
        dl.count += 16
        inst.then_inc(dl.sem, 16)
        mk._record(dl, reads, writes)
        mk.n_inst += 1

    MK.indirect = indirect

    def alloc_sparse(self):
        mk = self.mk
        R = {}
        R["M1a"] = mk.sbuf("sp_M1a", [128, NTILE, 32], F32)
        R["M2a"] = mk.sbuf("sp_M2a", [128, NTILE, 32], F32)
        R["A12"] = mk.sbuf("sp_A12", [128, NTILE, 2], F32)
        R["POSi"] = mk.sbuf("sp_POSi", [128, NTILE, 2], I32)
        R["IDXW"] = mk.sbuf("sp_IDXW", [128, NBLK], I32)
        R["U"] = mk.sbuf("sp_U", [128, 128], BF16)
        R["jg"] = mk.sbuf("sp_jg", [128, NBLK], F32)
        R["pidx"] = mk.sbuf("sp_pidx", [128, 1], F32)
        R["ones32"] = mk.sbuf("sp_ones32", [128, 32], F32)
        mk.op("pool", lambda e: e.memset(R["U"][:], 1.0), writes=[R["U"]])
        mk.op("pool", lambda e: e.affine_select(out=R["U"][:], in_=R["U"][:], pattern=[[1, 128]], compare_op=ALU.is_gt,
                                                fill=0.0, base=0, channel_multiplier=-1), reads=[R["U"]], writes=[R["U"]])
        mk.op("pool", lambda e: e.memset(R["ones32"][:], 1.0), writes=[R["ones32"]])
        return R

    def route_tile_sparse(self, hf, a, tok0, R, rt):
        mk = self.mk
        ps = self.ps[1 + (a % 2)]
        Wr, br = R["Wr"], R["br"]
        ti = tok0 // 128
        for k in range(DC):
            mk.op("pe", lambda e, k=k: e.matmul(ps[:, 0:36], hf[:, k, a * 128:(a + 1) * 128], Wr[:, k, :],
                                                 start=(k == 0), stop=(k == DC - 1)), reads=[hf, Wr], writes=[ps])
        lg, gmax, ngmax, ge, gsum, gp, mg, es = (rt[k] for k in ("lg", "gmax", "ngmax", "ge", "gsum", "gp", "mg", "es"))
        t8, dd, w2, m1, m2 = (rt[k] for k in ("t8", "dd", "w2", "m1", "m2"))
        M1a, M2a, A12 = R["M1a"], R["M2a"], R["A12"]
        V = lambda fn, reads, writes: mk.op("dve", fn, reads=reads, writes=writes)
        V(lambda e: e.tensor_tensor(out=lg[:], in0=ps[:, 0:36], in1=br[:], op=ALU.add), [ps, br], [lg])
        V(lambda e: e.reduce_max(out=gmax[:], in_=lg[:, 0:4], axis=AX.X), [lg], [gmax])
        V(lambda e: e.tensor_scalar(out=ngmax[:], in0=gmax[:], scalar1=-1.0, scalar2=None, op0=ALU.mult), [gmax], [ngmax])
        mk.op("act", lambda e: e.activation(out=ge[:], in_=lg[:, 0:4], func=AF.Exp, bias=ngmax[:, 0:1],
                                            accum_out=gsum[:]), reads=[lg, ngmax], writes=[ge, gsum])
        V(lambda e: e.reciprocal(gp[:], gsum[:]), [gsum], [gp])
        V(lambda e: e.tensor_scalar(out=mg[:], in0=lg[:, 0:4], scalar1=gmax[:, 0:1], scalar2=None, op0=ALU.is_equal),
          [lg, gmax], [mg])
        V(lambda e: e.tensor_scalar(out=es[:], in0=lg[:, 4:12], scalar1=mg[:, 0:1], scalar2=None, op0=ALU.mult),
          [lg, mg], [es])
        for g in range(1, 4):
            V(lambda e, g=g: e.scalar_tensor_tensor(out=es[:], in0=lg[:, 4 + 8 * g:12 + 8 * g], scalar=mg[:, g:g + 1],
                                                    in1=es[:], op0=ALU.mult, op1=ALU.add), [lg, mg, es], [es])
        V(lambda e: e.max(out=t8[:], in_=es[:]), [es], [t8])
        V(lambda e: e.tensor_tensor(out=dd[:], in0=t8[:, 1:2], in1=t8[:, 0:1], op=ALU.subtract), [t8], [dd])
        mk.op("act", lambda e: e.activation(out=w2[:], in_=dd[:], func=AF.Sigmoid), reads=[dd], writes=[w2])
        V(lambda e: e.tensor_tensor(out=A12[:, ti, 1:2], in0=w2[:], in1=gp[:], op=ALU.mult), [w2, gp], [A12])
        V(lambda e: e.tensor_tensor(out=A12[:, ti, 0:1], in0=gp[:], in1=A12[:, ti, 1:2], op=ALU.subtract), [gp, A12], [A12])
        V(lambda e: e.tensor_scalar(out=m1[:], in0=es[:], scalar1=t8[:, 0:1], scalar2=None, op0=ALU.is_equal), [es, t8], [m1])
        V(lambda e: e.tensor_scalar(out=m2[:], in0=es[:], scalar1=t8[:, 1:2], scalar2=None, op0=ALU.is_equal), [es, t8], [m2])
        for g in range(4):
            V(lambda e, g=g: e.tensor_scalar(out=M1a[:, ti, 8 * g:8 * g + 8], in0=m1[:], scalar1=mg[:, g:g + 1],
                                             scalar2=None, op0=ALU.mult), [m1, mg], [M1a])
            V(lambda e, g=g: e.tensor_scalar(out=M2a[:, ti, 8 * g:8 * g + 8], in0=m2[:], scalar1=mg[:, g:g + 1],
                                             scalar2=None, op0=ALU.mult), [m2, mg], [M2a])

    def stage_moe_sparse(self, i, XT, R, I):
        mk = self.mk
        HTOK, HS, YS = self.dram["HTOK"], self.dram["HS"], self.dram["YS"]
        M1a, M2a, A12, POSi, IDXW = R["M1a"], R["M2a"], R["A12"], R["POSi"], R["IDXW"]
        m0 = mk.mark()
        Mb = mk.sbuf("sq_Mb", [128, NTILE, 32], BF16)
        P = mk.sbuf("sq_P", [128, NTILE, 32], F32)
        prod = mk.sbuf("sq_prod", [128, NTILE, 32], F32)
        posf = mk.sbuf("sq_posf", [128, NTILE, 2], F32)
        nf = mk.sbuf("sq_nf", [128, 32], F32)
        ni = mk.sbuf("sq_ni", [128, 32], I32)
        pn = mk.sbuf("sq_pn", [128, 32], F32)
        incl = mk.sbuf("sq_incl", [128, 32], F32)
        excl = mk.sbuf("sq_excl", [128, 32], F32)
        EB = mk.sbuf("sq_EB", [128, NBLK], F32)
        mk.op("dve", lambda e: e.tensor_tensor(out=Mb[:], in0=M1a[:], in1=M2a[:], op=ALU.add), reads=[M1a, M2a], writes=[Mb])
        for ti in range(NTILE):
            pb = self.ps[ti // 12]
            c0 = (ti % 12) * 32
            for tj in range(ti):
                mk.op("pe", lambda e, pb=pb, c0=c0, tj=tj: e.matmul(pb[:, c0:c0 + 32], self.ones_b[:], Mb[:, tj, :],
                                                                    start=(tj == 0), stop=False), reads=[self.ones_b, Mb], writes=[pb])
            mk.op("pe", lambda e, pb=pb, c0=c0, ti=ti: e.matmul(pb[:, c0:c0 + 32], R["U"][:], Mb[:, ti, :],
                                                                start=(ti == 0), stop=True), reads=[R["U"], Mb], writes=[pb])
        pt = self.ps[3]
        for tj in range(NTILE):
            mk.op("pe", lambda e, tj=tj: e.matmul(pt[:, 0:32], self.ones_b[:], Mb[:, tj, :], start=(tj == 0),
                                                  stop=(tj == NTILE - 1)), reads=[self.ones_b, Mb], writes=[pt])
        V = lambda fn, reads, writes: mk.op("dve", fn, reads=reads, writes=writes)
        V(lambda e: e.tensor_scalar(out=nf[:], in0=pt[:, 0:32], scalar1=float(BSL - 1), scalar2=None, op0=ALU.add), [pt], [nf])
        V(lambda e: e.tensor_copy(ni[:], nf[:]), [nf], [ni])
        V(lambda e: e.tensor_single_scalar(out=ni[:], in_=ni[:], scalar=BSH, op=ALU.arith_shift_right), [ni], [ni])
        V(lambda e: e.tensor_single_scalar(out=ni[:], in_=ni[:], scalar=BSH, op=ALU.logical_shift_left), [ni], [ni])
        V(lambda e: e.tensor_copy(pn[:], ni[:]), [ni], [pn])
        V(lambda e: e.tensor_tensor_scan(out=incl[:], data0=R["ones32"][:], data1=pn[:], initial=0.0, op0=ALU.mult, op1=ALU.add),
          [R["ones32"], pn], [incl])
        V(lambda e: e.tensor_tensor(out=excl[:], in0=incl[:], in1=pn[:], op=ALU.subtract), [incl, pn], [excl])
        for ti in range(NTILE):
            pb = self.ps[ti // 12]
            c0 = (ti % 12) * 32
            V(lambda e, pb=pb, c0=c0, ti=ti: e.tensor_tensor(out=P[:, ti, :], in0=pb[:, c0:c0 + 32], in1=excl[:], op=ALU.add),
              [pb, excl], [P])
        for k, Mk in enumerate((M1a, M2a)):
            V(lambda e, Mk=Mk: e.tensor_tensor(out=prod[:], in0=Mk[:], in1=P[:], op=ALU.mult), [Mk, P], [prod])
            V(lambda e, k=k: e.reduce_sum(out=posf[:, :, k], in_=prod[:], axis=AX.X), [prod], [posf])
        V(lambda e: e.tensor_copy(POSi[:], posf[:]), [posf], [POSi])
        V(lambda e: e.memset(EB[:], 0.0), [], [EB])
        for ex in range(NE):
            V(lambda e, ex=ex: e.scalar_tensor_tensor(out=EB[:], in0=R["jg"][:], scalar=incl[:, ex:ex + 1], in1=EB[:],
                                                      op0=ALU.is_ge, op1=ALU.add), [R["jg"], incl, EB], [EB])
        V(lambda e: e.tensor_scalar(out=EB[:], in0=EB[:], scalar1=float(NE - 1), scalar2=128.0, op0=ALU.min, op1=ALU.mult), [EB], [EB])
        V(lambda e: e.tensor_scalar(out=EB[:], in0=EB[:], scalar1=R["pidx"][:, 0:1], scalar2=None, op0=ALU.add), [EB, R["pidx"]], [EB])
        V(lambda e: e.tensor_copy(IDXW[:], EB[:]), [EB], [IDXW])
        if self.cfg.get("dbg_pos"):
            d1 = T(self.nc.dram_tensor("dbg_pos", [128, NTILE, 2], I32, kind="ExternalOutput"), "dbg_pos")
            d2 = T(self.nc.dram_tensor("dbg_idx", [128, NBLK], I32, kind="ExternalOutput"), "dbg_idx")
            d3 = T(self.nc.dram_tensor("dbg_incl", [128, 32], F32, kind="ExternalOutput"), "dbg_incl")
            d4 = T(self.nc.dram_tensor("dbg_P", [128, NTILE, 32], F32, kind="ExternalOutput"), "dbg_P")
            d5 = T(self.nc.dram_tensor("dbg_M1", [128, NTILE, 32], F32, kind="ExternalOutput"), "dbg_M1")
            d6 = T(self.nc.dram_tensor("dbg_A12", [128, NTILE, 2], F32, kind="ExternalOutput"), "dbg_A12")
            mk.dma("sp", d1[:, :, :], POSi[:], reads=[POSi])
            mk.dma("sp", d2[:, :], IDXW[:], reads=[IDXW])
            mk.dma("sp", d3[:, :], incl[:], reads=[incl])
            mk.dma("sp", d4[:, :, :], P[:], reads=[P])
            mk.dma("sp", d5[:, :, :], M1a[:], reads=[M1a])
            mk.dma("sp", d6[:, :, :], A12[:], reads=[A12])
            mk.release(m0)
            return
        mk.release(m0)
        m0 = mk.mark()
        ht = [mk.sbuf("sq_ht%d" % k, [128, D], BF16) for k in range(3)]
        for ti in range(NTILE):
            h = ht[ti % 3]
            mk.dma("sp", h[:], HTOK[ti * 128:(ti + 1) * 128, :], reads=[HTOK], writes=[h])
            for k in range(2):
                mk.indirect(HS[:, :], IOA(ap=POSi[:, ti, k:k + 1], axis=0), h[:], None, [h, POSi], [HS])
        mk.release(m0)
        m0 = mk.mark()
        wgu = [mk.sbuf("sq_wgu%d" % k, [128, DC, 512], BF16) for k in range(2)]
        wdn = [mk.sbuf("sq_wdn%d" % k, [128, 2, D], BF16) for k in range(2)]
        NA = BSL // 128
        hs = [mk.sbuf("sq_hs%d" % k, [128, NA, D], BF16) for k in range(2)]
        hsT = [mk.sbuf("sq_hsT%d" % k, [128, DC, BSL], BF16) for k in range(2)]
        sl = [mk.sbuf("sq_sl%d" % k, [128, BSL], F32) for k in range(2)]
        hid = [mk.sbuf("sq_hid%d" % k, [128, 2, BSL], BF16) for k in range(2)]
        yo = [mk.sbuf("sq_yo%d" % k, [128, D], F32) for k in range(3)]
        WGU, WDN = I["moe_wgu%d" % i], I["moe_wdn%d" % i]
        ny = 0
        for j in range(NBLK):
            w, wd_, h, hT_, hd = wgu[j % 2], wdn[j % 2], hs[j % 2], hsT[j % 2], hid[j % 2]
            mk.indirect(w[:].rearrange("p a b -> p (a b)"), None, WGU[:, :], IOA(ap=IDXW[:, j:j + 1], axis=0), [IDXW], [w])
            mk.indirect(wd_[:].rearrange("p a b -> p (a b)"), None, WDN[:, :], IOA(ap=IDXW[:, j:j + 1], axis=0), [IDXW], [wd_])
            mk.dma("sp", h[:], HS[j * BSL:(j + 1) * BSL, :].rearrange("(a p) d -> p a d", p=128), reads=[HS], writes=[h])
            KPB = 1024 // BSL
            for hh in range(DC // KPB):
                pb = self.ps[hh % 2]
                pbb = pb[:, 0:512].bitcast(BF16)
                for kk in range(KPB):
                    k = hh * KPB + kk
                    for a in range(NA):
                        mk.op("pe", lambda e, pbb=pbb, kk=kk, k=k, a=a, h=h: e.transpose(
                            pbb[:, kk * BSL + a * 128:kk * BSL + (a + 1) * 128], h[:, a, k * 128:(k + 1) * 128], self.ident_b[:]),
                            reads=[h, self.ident_b], writes=[pb])
                dstv = hT_[:, hh * KPB:(hh + 1) * KPB, :].rearrange("p a b -> p (a b)")
                if hh % 2:
                    mk.op("act", lambda e, pbb=pbb, dstv=dstv: e.copy(dstv, pbb[:, :]), reads=[pb], writes=[hT_])
                else:
                    mk.op("dve", lambda e, pbb=pbb, dstv=dstv: e.tensor_copy(dstv, pbb[:, :]), reads=[pb], writes=[hT_])
            for fc in range(2):
                pg, pu = self.ps[2 + 2 * fc], self.ps[3 + 2 * fc]
                for k in range(DC):
                    mk.op("pe", lambda e, pg=pg, k=k, fc=fc, w=w, hT_=hT_: e.matmul(
                        pg[:, 0:BSL], w[:, k, fc * 128:(fc + 1) * 128], hT_[:, k, :], start=(k == 0), stop=(k == DC - 1)),
                        reads=[w, hT_], writes=[pg])
                for k in range(DC):
                    mk.op("pe", lambda e, pu=pu, k=k, fc=fc, w=w, hT_=hT_: e.matmul(
                        pu[:, 0:BSL], w[:, k, 256 + fc * 128:256 + (fc + 1) * 128], hT_[:, k, :], start=(k == 0), stop=(k == DC - 1)),
                        reads=[w, hT_], writes=[pu])
                s_ = sl[fc]
                mk.op("act", lambda e, pg=pg, s_=s_: e.activation(out=s_[:], in_=pg[:, 0:BSL], func=AF.Silu), reads=[pg], writes=[s_])
                mk.op("dve", lambda e, pu=pu, s_=s_, hd=hd, fc=fc: e.tensor_tensor(out=hd[:, fc, :], in0=pu[:, 0:BSL], in1=s_[:],
                                                                                  op=ALU.mult), reads=[pu, s_], writes=[hd])
            for a in range(NA):
                y_ = yo[ny % 3]
                ny += 1
                for dh in range(2):
                    pd = self.ps[6 + dh]
                    for fc in range(2):
                        mk.op("pe", lambda e, pd=pd, fc=fc, a=a, dh=dh, hd=hd, wd_=wd_: e.matmul(
                            pd[:, :], hd[:, fc, a * 128:(a + 1) * 128], wd_[:, fc, dh * 512:(dh + 1) * 512], start=(fc == 0), stop=(fc == 1)),
                            reads=[hd, wd_], writes=[pd])
                    if dh:
                        mk.op("act", lambda e, pd=pd, y_=y_, dh=dh: e.copy(y_[:, dh * 512:(dh + 1) * 512], pd[:, :]), reads=[pd], writes=[y_])
                    else:
                        mk.op("dve", lambda e, pd=pd, y_=y_, dh=dh: e.tensor_copy(y_[:, dh * 512:(dh + 1) * 512], pd[:, :]), reads=[pd], writes=[y_])
                mk.dma("act", YS[j * BSL + a * 128:j * BSL + (a + 1) * 128, :], y_[:], reads=[y_], writes=[YS])
        mk.release(m0)
        m0 = mk.mark()
        r1 = [mk.sbuf("sq_r1%d" % k, [128, D], F32) for k in range(2)]
        r2 = [mk.sbuf("sq_r2%d" % k, [128, D], F32) for k in range(2)]
        xr = [mk.sbuf("sq_x%d" % k, [128, 512], F32) for k in range(3)]
        n = 0
        for bi, (t0, tn) in enumerate(BLOCKS):
            r = 1 if bi == 0 else 0
            g2 = self.mv[("g2", r)]
            for a in range(tn // 128):
                ti = t0 // 128 + a
                a_, b_ = r1[ti % 2], r2[ti % 2]
                mk.indirect(a_[:], None, YS[:, :], IOA(ap=POSi[:, ti, 0:1], axis=0), [YS, POSi], [a_])
                mk.indirect(b_[:], None, YS[:, :], IOA(ap=POSi[:, ti, 1:2], axis=0), [YS, POSi], [b_])
                mk.op("dve", lambda e, a_=a_, ti=ti: e.tensor_scalar(out=a_[:], in0=a_[:], scalar1=A12[:, ti, 0:1], scalar2=None,
                                                                    op0=ALU.mult), reads=[a_, A12], writes=[a_])
                mk.op("dve", lambda e, a_=a_, b_=b_, ti=ti: e.scalar_tensor_tensor(out=a_[:], in0=b_[:], scalar=A12[:, ti, 1:2], in1=a_[:],
                                                                                  op0=ALU.mult, op1=ALU.add), reads=[a_, b_, A12], writes=[a_])
                for dc in range(DC):
                    mk.op("pe", lambda e, dc=dc, a=a, a_=a_: e.transpose(self.ps[dc][:, a * 128:(a + 1) * 128],
                                                                         a_[:, dc * 128:(dc + 1) * 128], self.ident_f[:]),
                          reads=[a_, self.ident_f], writes=[self.ps[dc]])
            for dc in range(DC):
                x = xr[n % 3]
                n += 1
                mk.dma("sp", x[:, 0:tn], XT[dc * 128:(dc + 1) * 128, t0:t0 + tn], reads=[self.XTr[dc]], writes=[x])
                mk.op("dve", lambda e, dc=dc, x=x, g2=g2: e.scalar_tensor_tensor(
                    out=x[:, 0:tn], in0=self.ps[dc][:, 0:tn], scalar=g2[:, dc:dc + 1], in1=x[:, 0:tn],
                    op0=ALU.mult, op1=ALU.add), reads=[self.ps[dc], g2, x], writes=[x])
                mk.dma("act", XT[dc * 128:(dc + 1) * 128, t0:t0 + tn], x[:, 0:tn], reads=[x], writes=[self.XTr[dc]])
        mk.release(m0)

    Prog.alloc_sparse = alloc_sparse
    Prog._route_tile_sparse = route_tile_sparse
    Prog.stage_moe_sparse = stage_moe_sparse


_sparse_methods()


def hyd_consts():
    bf = ml_dtypes.bfloat16
    L, N = CTX, 2 * CTX
    c = {}
    t = (np.arange(2)[None, :, None] * 128 + np.arange(128)[:, None, None]).astype(np.float64)
    k = np.arange(N)[None, None, :].astype(np.float64)
    ang = 2 * np.pi * t * k / N
    c["F"] = np.stack([np.cos(ang), -np.sin(ang), np.sin(ang)], axis=2).astype(bf)
    kk = (np.arange(4)[None, :, None] * 128 + np.arange(128)[:, None, None]).astype(np.float64)
    tt = np.arange(L)[None, None, :].astype(np.float64)
    a2 = 2 * np.pi * kk * tt / N
    c["Fi"] = np.stack([np.cos(a2) / N, -np.sin(a2) / N], axis=2).astype(bf)
    pos = (np.arange(2)[None, :] * 128 + np.arange(128)[:, None]).astype(np.float32)
    c["lneg"] = (-(pos / np.float32(L - 1))).astype(np.float32)
    return c


def _hyd_methods():
    def hy_ctx_direct(self, I):
        mk = self.mk
        L = CTX
        ZT, Z1, HT = self.dram["hyZ"], self.dram["hyZ1"], self.dram["HT"]
        m0 = mk.mark()
        Fd = mk.sbuf("hd_F", [128, 2, 3, 512], BF16)
        Fi = mk.sbuf("hd_Fi", [128, 4, 2, L], BF16)
        lneg = mk.sbuf("hd_lneg", [128, 2], F32)
        dab = mk.sbuf("hd_dab", [128, D], F32)
        ft = mk.sbuf("hd_ft", [17, L], F32)
        W1 = mk.sbuf("hd_w1", [17, 64], F32)
        W2 = mk.sbuf("hd_w2", [64, 64], F32)
        W3 = mk.sbuf("hd_w3", [64, 4096], BF16)
        fv = mk.sbuf("hd_fv", [64, 4], F32)
        fb = mk.sbuf("hd_fb", [64, 2], F32)
        z1 = mk.sbuf("hd_z1", [64, L], F32)
        z2 = mk.sbuf("hd_z2", [64, L], F32)
        z2b = mk.sbuf("hd_z2b", [64, L], BF16)
        tmp = mk.sbuf("hd_tmp", [64, L], F32)
        mk.dma("sp", Fd[:], I["hyd_F"][:, :, :, :], writes=[Fd])
        mk.dma("sp", Fi[:], I["hyd_Fi"][:, :, :, :], writes=[Fi])
        mk.dma("sp", lneg[:], I["hyd_lneg"][:, :], writes=[lneg])
        mk.dma("sp", dab[0:64, :], I["hy_dabs"][:, :], writes=[dab])
        mk.dma("sp", dab[64:128, :], I["hy_dabs"][:, :], writes=[dab])
        mk.dma("sp", ft[:], I["hy_featsT_ctx"][:, :], writes=[ft])
        mk.dma("sp", W1[:], I["hy_f_w1"][0, :, :], writes=[W1])
        mk.dma("sp", W2[:], I["hy_f_w2"][0, :, :], writes=[W2])
        mk.dma("pool", W3[:], I["hy_f_w3"][0, :, :], writes=[W3])
        mk.dma("sp", fv[:], I["hy_fvec"][:, :], writes=[fv])
        mk.op("dve", lambda e: e.tensor_tensor(out=fb[:, 0:1], in0=fv[:, 0:1], in1=fv[:, 1:2], op=ALU.mult), reads=[fv], writes=[fb])
        mk.op("dve", lambda e: e.tensor_tensor(out=fb[:, 1:2], in0=fv[:, 2:3], in1=fv[:, 3:4], op=ALU.mult), reads=[fv], writes=[fb])
        for layer, (Wm, src, dst, kin) in enumerate(((W1, ft, z1, 17), (W2, z1, z2, 64))):
            ps = self.ps[layer]
            mk.op("pe", lambda e, ps=ps, Wm=Wm, src=src, kin=kin: e.matmul(ps[0:64, 0:L], Wm[0:kin, :], src[0:kin, :],
                                                                          start=True, stop=True), reads=[Wm, src], writes=[ps])
            mk.op("dve", lambda e, ps=ps, dst=dst, layer=layer: e.tensor_scalar(
                out=dst[:], in0=ps[0:64, 0:L], scalar1=fv[:, 2 * layer + 1:2 * layer + 2], scalar2=fb[:, layer:layer + 1],
                op0=ALU.mult, op1=ALU.add), reads=[ps, fv, fb], writes=[dst])
            for _ in range(2):
                mk.op("dve", lambda e, dst=dst: e.tensor_scalar(out=tmp[:], in0=dst[:], scalar1=math.pi, scalar2=2 * math.pi,
                                                                op0=ALU.is_gt, op1=ALU.mult), reads=[dst], writes=[tmp])
                mk.op("dve", lambda e, dst=dst: e.tensor_tensor(out=dst[:], in0=dst[:], in1=tmp[:], op=ALU.subtract),
                      reads=[dst, tmp], writes=[dst])
                mk.op("dve", lambda e, dst=dst: e.tensor_scalar(out=tmp[:], in0=dst[:], scalar1=-math.pi, scalar2=2 * math.pi,
                                                                op0=ALU.is_lt, op1=ALU.mult), reads=[dst], writes=[tmp])
                mk.op("dve", lambda e, dst=dst: e.tensor_tensor(out=dst[:], in0=dst[:], in1=tmp[:], op=ALU.add),
                      reads=[dst, tmp], writes=[dst])
            mk.op("act", lambda e, dst=dst: e.activation(out=dst[:], in_=dst[:], func=AF.Sin), reads=[dst], writes=[dst])
        mk.op("act", lambda e: e.copy(z2b[:], z2[:]), reads=[z2], writes=[z2b])
        Hf = mk.sbuf("hd_Hf", [128, 2, 2, 4, D], BF16)
        dec = [mk.sbuf("hd_dec%d" % k, [128, D], F32) for k in range(2)]
        hd = [mk.sbuf("hd_hd%d" % k, [128, D], F32) for k in range(2)]
        hdb = [mk.sbuf("hd_hdb%d" % k, [128, D], BF16) for k in range(4)]
        ha = [mk.sbuf("hd_ha%d" % k, [128, D], BF16) for k in range(2)]
        rn = mk.sbuf("hd_rn", [128, D], F32)
        for o in range(2):
            pn = (self.ps[6], self.ps[7])
            n = 0
            for pt in range(2):
                for dr in range(2):
                    i2 = n % 2
                    for hf in range(2):
                        ph = self.ps[i2 * 2 + hf]
                        w0 = (dr * 2 + o) * D + hf * 512
                        mk.op("pe", lambda e, ph=ph, pt=pt, w0=w0: e.matmul(ph[:, :], z2b[:, pt * 128:(pt + 1) * 128], W3[:, w0:w0 + 512],
                                                                            start=True, stop=True), reads=[z2b, W3], writes=[ph])
                    mk.op("act", lambda e, i2=i2, pt=pt: e.activation(out=dec[i2][:], in_=dab[:], func=AF.Exp, scale=lneg[:, pt:pt + 1]),
                          reads=[dab, lneg], writes=[dec[i2]])
                    for hf in range(2):
                        ph = self.ps[i2 * 2 + hf]
                        sl_ = slice(hf * 512, (hf + 1) * 512)
                        mk.op("dve", lambda e, ph=ph, i2=i2, sl_=sl_: e.tensor_tensor(out=hd[i2][:, sl_], in0=ph[:, :], in1=dec[i2][:, sl_],
                                                                                     op=ALU.mult), reads=[ph, dec[i2]], writes=[hd[i2]])
                    if dr == 1 and pt == 0:
                        mk.op("dve", lambda e, i2=i2: e.memset(hd[i2][0:1, :], 0.0), reads=[hd[i2]], writes=[hd[i2]])
                    hb_ = hdb[pt * 2 + dr]
                    mk.op("act", lambda e, i2=i2, hb_=hb_: e.copy(hb_[:], hd[i2][:]), reads=[hd[i2]], writes=[hb_])
                    mk.op("dve", lambda e, i2=i2: e.scalar_tensor_tensor(out=ha[i2][:], in0=hd[i2][:], scalar=-1.0, in1=hd[i2][:],
                                                                        op0=ALU.mult, op1=ALU.max), reads=[hd[i2]], writes=[ha[i2]])
                    for hf in range(2):
                        mk.op("pe", lambda e, i2=i2, hf=hf, n=n: e.matmul(pn[hf][:, :], self.ones_b[:], ha[i2][:, hf * 512:(hf + 1) * 512],
                                                                          start=(n == 0), stop=(n == 3)), reads=[self.ones_b, ha[i2]], writes=[pn[hf]])
                    n += 1
            for hf in range(2):
                mk.op("dve", lambda e, hf=hf: e.reciprocal(rn[:, hf * 512:(hf + 1) * 512], pn[hf][:, :]), reads=[pn[hf]], writes=[rn])
            q = 0
            for kc in range(4):
                for ri in range(2):
                    for hf in range(2):
                        pa = self.ps[4 + q % 2]
                        q += 1
                        sl_ = slice(hf * 512, (hf + 1) * 512)
                        steps = [(pt, dr) for pt in range(2) for dr in range(2)]
                        for si, (pt, dr) in enumerate(steps):
                            kind = ri if dr == 0 else (0 if ri == 0 else 2)
                            mk.op("pe", lambda e, pa=pa, pt=pt, dr=dr, kind=kind, kc=kc, sl_=sl_, si=si: e.matmul(
                                pa[:, :], Fd[:, pt, kind, kc * 128:(kc + 1) * 128], hdb[pt * 2 + dr][:, sl_],
                                start=(si == 0), stop=(si == 3)), reads=[Fd, hdb[pt * 2 + dr]], writes=[pa])
                        mk.op("dve", lambda e, pa=pa, o=o, ri=ri, kc=kc, sl_=sl_: e.tensor_tensor(
                            out=Hf[:, o, ri, kc, sl_], in0=pa[:, :], in1=rn[:, sl_], op=ALU.mult), reads=[pa, rn], writes=[Hf])
        zf = mk.sbuf("hd_zf", [128, DC, L], F32)
        gt = mk.sbuf("hd_gt", [128, DC, L], F32)
        zb = mk.sbuf("hd_zb", [128, DC, L], BF16)
        zT = mk.sbuf("hd_zT", [128, 2, D], BF16)
        Y = mk.sbuf("hd_Y", [128, 2, 4, D], BF16)
        ta = [mk.sbuf("hd_ta%d" % k, [128, 512], F32) for k in range(2)]
        tb = [mk.sbuf("hd_tb%d" % k, [128, 512], F32) for k in range(2)]
        lb = mk.sbuf("hd_lb", [128, 2, DC], F32)
        rr = [mk.sbuf("hd_rr%d" % k, [128, L], F32) for k in range(2)]
        obf = mk.sbuf("hd_obf", [128, DC, L], F32)
        obb = mk.sbuf("hd_obb", [128, DC, L], BF16)
        for o in range(2):
            mk.dma("sp", lb[:, o, :], I["hy_lbias_pc"][o, :, :], writes=[lb])
        for o in range(2):
            zsrc = ZT if o == 0 else Z1
            mk.dma("sp", zf[:], zsrc[0:D, 0:L].rearrange("(j p) t -> p j t", p=128), reads=[zsrc], writes=[zf])
            g0 = (1 + o) * D
            mk.dma("sp", gt[:], ZT[g0:g0 + D, 0:L].rearrange("(j p) t -> p j t", p=128), reads=[ZT], writes=[gt])
            mk.op("act", lambda e: e.copy(zb[:], zf[:]), reads=[zf], writes=[zb])
            for pt in range(2):
                pb = self.ps[pt]
                pbb = pb[:, 0:512].bitcast(BF16)
                for j in range(DC):
                    mk.op("pe", lambda e, pbb=pbb, j=j, pt=pt: e.transpose(pbb[:, j * 128:(j + 1) * 128], zb[:, j, pt * 128:(pt + 1) * 128],
                                                                          self.ident_b[:]), reads=[zb, self.ident_b], writes=[pb])
                mk.op("act", lambda e, pbb=pbb, pt=pt: e.copy(zT[:, pt, :], pbb[:, :]), reads=[pb], writes=[zT])
            for kc in range(4):
                for ri in range(2):
                    for hf in range(2):
                        px = self.ps[2 + ri * 2 + hf]
                        for pt in range(2):
                            mk.op("pe", lambda e, px=px, pt=pt, ri=ri, kc=kc, hf=hf: e.matmul(
                                px[:, :], Fd[:, pt, ri, kc * 128:(kc + 1) * 128], zT[:, pt, hf * 512:(hf + 1) * 512],
                                start=(pt == 0), stop=(pt == 1)), reads=[Fd, zT], writes=[px])
                for hf in range(2):
                    sl_ = slice(hf * 512, (hf + 1) * 512)
                    pxr, pxi = self.ps[2 + hf], self.ps[4 + hf]
                    a_, b_ = ta[hf], tb[hf]
                    mk.op("dve", lambda e, pxr=pxr, a_=a_, kc=kc, sl_=sl_, o=o: e.tensor_tensor(out=a_[:], in0=pxr[:, :], in1=Hf[:, o, 0, kc, sl_],
                                                                                             op=ALU.mult), reads=[pxr, Hf], writes=[a_])
                    mk.op("dve", lambda e, pxi=pxi, b_=b_, kc=kc, sl_=sl_, o=o: e.tensor_tensor(out=b_[:], in0=pxi[:, :], in1=Hf[:, o, 1, kc, sl_],
                                                                                             op=ALU.mult), reads=[pxi, Hf], writes=[b_])
                    mk.op("pool", lambda e, a_=a_, b_=b_, kc=kc, sl_=sl_: e.tensor_tensor(out=Y[:, 0, kc, sl_], in0=a_[:], in1=b_[:],
                                                                                         op=ALU.subtract), reads=[a_, b_], writes=[Y])
                    mk.op("dve", lambda e, pxr=pxr, a_=a_, kc=kc, sl_=sl_, o=o: e.tensor_tensor(out=a_[:], in0=pxr[:, :], in1=Hf[:, o, 1, kc, sl_],
                                                                                             op=ALU.mult), reads=[pxr, Hf], writes=[a_])
                    mk.op("dve", lambda e, pxi=pxi, b_=b_, kc=kc, sl_=sl_, o=o: e.tensor_tensor(out=b_[:], in0=pxi[:, :], in1=Hf[:, o, 0, kc, sl_],
                                                                                             op=ALU.mult), reads=[pxi, Hf], writes=[b_])
                    mk.op("pool", lambda e, a_=a_, b_=b_, kc=kc, sl_=sl_: e.tensor_tensor(out=Y[:, 1, kc, sl_], in0=a_[:], in1=b_[:],
                                                                                         op=ALU.add), reads=[a_, b_], writes=[Y])
            ob = obf if o == 0 else obb
            for cj in range(DC):
                py = self.ps[6 + cj % 2]
                for kc in range(4):
                    for ri in range(2):
                        mk.op("pe", lambda e, py=py, kc=kc, ri=ri, cj=cj: e.matmul(
                            py[:, 0:L], Y[:, ri, kc, cj * 128:(cj + 1) * 128], Fi[:, kc, ri, :],
                            start=(kc == 0 and ri == 0), stop=(kc == 3 and ri == 1)), reads=[Y, Fi], writes=[py])
                r_ = rr[cj % 2]
                mk.op("dve", lambda e, py=py, r_=r_, cj=cj, o=o: e.scalar_tensor_tensor(out=r_[:], in0=zf[:, cj, :], scalar=lb[:, o, cj:cj + 1],
                                                                                     in1=py[:, 0:L], op0=ALU.mult, op1=ALU.add),
                      reads=[zf, lb, py], writes=[r_])
                mk.op("pool", lambda e, r_=r_, cj=cj, ob=ob: e.tensor_tensor(out=ob[:, cj, :], in0=r_[:], in1=gt[:, cj, :], op=ALU.mult),
                      reads=[r_, gt], writes=[ob])
            dst = Z1 if o == 0 else HT
            mk.dma("act", dst[0:D, 0:L].rearrange("(j p) t -> p j t", p=128), ob[:], reads=[ob], writes=[dst])
        mk.release(m0)

    Prog.hy_ctx_direct = hy_ctx_direct


_hyd_methods()
```

```python
import math
import numpy as np
import ml_dtypes
import concourse.bass as bass
import concourse.mybir as mybir
from concourse.bass_utils import run_bass_kernel_spmd

F32 = mybir.dt.float32
BF16 = mybir.dt.bfloat16
I32 = mybir.dt.int32
ALU = mybir.AluOpType
AF = mybir.ActivationFunctionType
AX = mybir.AxisListType

D = 1024
DC = 8
CTX = 256
SEQ = 4096
NT = CTX + SEQ
DEPTH = 4
EPS = 1e-6
NE = 32
DEXP = 256
BLOCKS = [(0, CTX)] + [(CTX + 512 * i, 512) for i in range(SEQ // 512)]


class T:
    __slots__ = ("t", "lw", "rd", "name")

    def __init__(self, t, name=""):
        self.t = t
        self.lw = None
        self.rd = {}
        self.name = name

    def __getitem__(self, k):
        return self.t[k]


class Lane:
    def __init__(self, name, eng, sem, step):
        self.name, self.eng, self.sem, self.step = name, eng, sem, step
        self.count = 0
        self.seen = {}


class MK:
    def __init__(self, nc, n_dma_sems=40):
        self.nc = nc
        self._stack = []
        self.lanes = {}
        for name, eng in (("pe", nc.tensor), ("act", nc.scalar), ("dve", nc.vector),
                          ("pool", nc.gpsimd), ("sp", nc.sync)):
            sem = self._enter(nc.semaphore("s_" + name))
            self.lanes[name] = Lane(name, eng, sem, 1)
        self.dma_lanes = []
        for i in range(n_dma_sems):
            sem = self._enter(nc.semaphore("d_%d" % i))
            ln = Lane("dma%d" % i, None, sem, 16)
            self.lanes[ln.name] = ln
            self.dma_lanes.append(ln)
        self.dma_rr = 0
        self.n_inst = 0
        self.uid = 0

    def _enter(self, cm):
        v = cm.__enter__()
        self._stack.append(cm)
        return v

    def mark(self):
        return len(self._stack)

    def release(self, mark):
        self.barrier()
        while len(self._stack) > mark:
            self._stack.pop().__exit__(None, None, None)

    def close(self):
        while self._stack:
            self._stack.pop().__exit__(None, None, None)

    def barrier(self):
        for a in ("pe", "act", "dve", "pool", "sp"):
            la = self.lanes[a]
            for b, lb in self.lanes.items():
                if b == a or lb.count == 0:
                    continue
                if la.seen.get(b, 0) >= lb.count:
                    continue
                la.seen[b] = lb.count
                la.eng.wait_ge(lb.sem, lb.count)

    def sbuf(self, name, shape, dt):
        self.uid += 1
        return T(self._enter(self.nc.sbuf_tensor("%s_%d" % (name, self.uid), list(shape), dt)), name)

    def psum(self, name, shape, dt=F32):
        self.uid += 1
        return T(self._enter(self.nc.psum_tensor("%s_%d" % (name, self.uid), list(shape), dt)), name)

    def _deps(self, lane, reads, writes):
        need = {}
        raw_same = 0
        for t in reads:
            if t.lw is not None:
                ln, idx = t.lw
                if need.get(ln, 0) < idx:
                    need[ln] = idx
                if ln == lane.name:
                    raw_same = max(raw_same, idx)
        for t in writes:
            if t.lw is not None:
                ln, idx = t.lw
                if ln != lane.name and need.get(ln, 0) < idx:
                    need[ln] = idx
            for ln, idx in t.rd.items():
                if ln != lane.name and need.get(ln, 0) < idx:
                    need[ln] = idx
        return need, raw_same

    def _record(self, lane, reads, writes):
        idx = lane.count
        for t in reads:
            if t.rd.get(lane.name, 0) < idx:
                t.rd[lane.name] = idx
        for t in writes:
            t.lw = (lane.name, idx)
            t.rd = {}

    def op(self, lane_name, fn, reads=(), writes=()):
        lane = self.lanes[lane_name]
        need, raw_same = self._deps(lane, reads, writes)
        for ln, idx in need.items():
            if ln == lane.name:
                if lane.name == "pe" or raw_same <= lane.count - 3:
                    continue
                idx = raw_same
            if lane.seen.get(ln, 0) >= idx:
                continue
            lane.seen[ln] = idx
            lane.eng.wait_ge(self.lanes[ln].sem, idx)
        inst = fn(lane.eng)
        lane.count += 1
        inst.then_inc(lane.sem, 1)
        self._record(lane, reads, writes)
        self.n_inst += 1
        return inst

    def dma(self, q, out, in_, reads=(), writes=(), **kw):
        qlane = self.lanes[q]
        dl = self.dma_lanes[self.dma_rr]
        self.dma_rr = (self.dma_rr + 1) % len(self.dma_lanes)
        need, _ = self._deps(dl, reads, writes)
        if dl.count > 0:
            need[dl.name] = max(need.get(dl.name, 0), dl.count)
        for ln, idx in need.items():
            if qlane.seen.get(ln, 0) >= idx:
                continue
            qlane.seen[ln] = idx
            qlane.eng.wait_ge(self.lanes[ln].sem, idx)
        inst = qlane.eng.dma_start(out=out, in_=in_, **kw)
        dl.count += 16
        inst.then_inc(dl.sem, 16)
        self._record(dl, reads, writes)
        self.n_inst += 1
        return inst

    def finish(self):
        sp = self.lanes["sp"]
        for dl in self.dma_lanes:
            if dl.count and sp.seen.get(dl.name, 0) < dl.count:
                sp.seen[dl.name] = dl.count
                sp.eng.wait_ge(dl.sem, dl.count)
        self.barrier()


def _vec_pc(v):
    v = np.asarray(v, np.float32)
    return np.ascontiguousarray(v.reshape(-1, 128).T)


class Prog:
    def __init__(self, cfg=None):
        self.cfg = cfg or {}
        self.nc = bass.Bass("TRN2", target_bir_lowering=False)
        self.mk = MK(self.nc)
        self.ins = {}
        self.dram = {}

    def inp(self, name, shape, dt=F32):
        t = self.nc.dram_tensor(name, list(shape), dt, kind="ExternalInput")
        self.ins[name] = t
        return T(t, name)

    def scratch(self, name, shape, dt, out=False):
        kind = "ExternalOutput" if (out or name in self.cfg.get("dump", ())) else "Internal"
        t = self.nc.dram_tensor(name, list(shape), dt, kind=kind)
        self.dram[name] = T(t, name)
        return self.dram[name]

    def setup_consts(self):
        mk = self.mk
        self.ps = [mk.psum("ps%d" % i, [128, 512], F32) for i in range(8)]
        self.ident_f = mk.sbuf("ident_f", [128, 128], F32)
        self.ident_b = mk.sbuf("ident_b", [128, 128], BF16)
        self.ones_b = mk.sbuf("ones_b", [128, 128], BF16)
        self.avg_b = mk.sbuf("avg_b", [128, 128], BF16)
        mk.op("pool", lambda e: e.memset(self.ident_f[:], 1.0), writes=[self.ident_f])
        mk.op("pool", lambda e: e.affine_select(out=self.ident_f[:], in_=self.ident_f[:],
              pattern=[[-1, 128]], compare_op=ALU.is_equal, fill=0.0, base=0,
              channel_multiplier=1), reads=[self.ident_f], writes=[self.ident_f])
        mk.op("dve", lambda e: e.tensor_copy(self.ident_b[:], self.ident_f[:]),
              reads=[self.ident_f], writes=[self.ident_b])
        mk.op("pool", lambda e: e.memset(self.ones_b[:], 1.0), writes=[self.ones_b])
        mk.op("pool", lambda e: e.memset(self.avg_b[:], 1.0 / D), writes=[self.avg_b])
        self.eps_t = mk.sbuf("eps_t", [128, 1], F32)
        mk.op("pool", lambda e: e.memset(self.eps_t[:], EPS), writes=[self.eps_t])

    def stage_input(self, x_in, ctx_in, XT):
        mk = self.mk
        m0 = mk.mark()
        xin = [mk.sbuf("xin%d" % i, [128, 4, D], F32) for i in range(2)]
        xo = [mk.sbuf("xo%d" % i, [128, DC, 512], F32) for i in range(2)]
        for bi, (t0, tn) in enumerate(BLOCKS):
            nt = tn // 128
            buf = xin[bi % 2]
            src = ctx_in if bi == 0 else x_in
            r0 = 0 if bi == 0 else t0 - CTX
            mk.dma("sp", buf[:, 0:nt, :], src[r0:r0 + tn, :].rearrange("(a p) d -> p a d", p=128),
                   reads=[src], writes=[buf])
            ob = xo[bi % 2]
            for j in range(DC):
                ps = self.ps[j % 8]
                for a in range(nt):
                    mk.op("pe", lambda e, ps=ps, a=a, j=j, buf=buf: e.transpose(
                        ps[:, a * 128:(a + 1) * 128], buf[:, a, j * 128:(j + 1) * 128], self.ident_f[:]),
                        reads=[buf, self.ident_f], writes=[ps])
                eng = "act" if j % 2 else "dve"
                if eng == "act":
                    mk.op("act", lambda e, ps=ps, j=j, ob=ob: e.copy(ob[:, j, 0:tn], ps[:, 0:tn]),
                          reads=[ps], writes=[ob])
                else:
                    mk.op("dve", lambda e, ps=ps, j=j, ob=ob: e.tensor_copy(ob[:, j, 0:tn], ps[:, 0:tn]),
                          reads=[ps], writes=[ob])
            mk.dma("pool", XT[:, t0:t0 + tn].rearrange("(j p) t -> p j t", p=128), ob[:, :, 0:tn],
                   reads=[ob], writes=self.XTr)
        mk.release(m0)

    def stage_output(self, XT, y_out, fg):
        mk = self.mk
        m0 = mk.mark()
        xb = [mk.sbuf("fx%d" % i, [128, DC, 512], F32) for i in range(2)]
        sq = mk.sbuf("fsq", [128, DC, 512], BF16)
        rstd = mk.sbuf("frstd", [128, 512], F32)
        yo = [mk.sbuf("fy%d" % i, [128, 4, D], F32) for i in range(2)]
        for bi, (t0, tn) in enumerate(BLOCKS):
            if bi == 0:
                continue
            buf = xb[bi % 2]
            mk.dma("sp", buf[:, :, 0:tn], XT[:, t0:t0 + tn].rearrange("(j p) t -> p j t", p=128),
                   reads=self.XTr, writes=[buf])
            self._rstd(buf, sq, rstd, tn, self.ps[0])
            for j in range(DC):
                mk.op("dve", lambda e, j=j, buf=buf: e.scalar_tensor_tensor(
                    out=buf[:, j, 0:tn], in0=buf[:, j, 0:tn], scalar=fg[:, j:j + 1], in1=rstd[:, 0:tn],
                    op0=ALU.mult, op1=ALU.mult), reads=[buf, rstd, fg], writes=[buf])
            ob = yo[bi % 2]
            nt = tn // 128
            for a in range(nt):
                for h in range(2):
                    ps = self.ps[1 + (a * 2 + h) % 7]
                    for jj in range(4):
                        j = h * 4 + jj
                        mk.op("pe", lambda e, ps=ps, a=a, j=j, jj=jj, buf=buf: e.transpose(
                            ps[:, jj * 128:(jj + 1) * 128], buf[:, j, a * 128:(a + 1) * 128], self.ident_f[:]),
                            reads=[buf, self.ident_f], writes=[ps])
                    if h:
                        mk.op("act", lambda e, ps=ps, a=a, h=h, ob=ob: e.copy(
                            ob[:, a, h * 512:(h + 1) * 512], ps[:, :]), reads=[ps], writes=[ob])
                    else:
                        mk.op("dve", lambda e, ps=ps, a=a, h=h, ob=ob: e.tensor_copy(
                            ob[:, a, h * 512:(h + 1) * 512], ps[:, :]), reads=[ps], writes=[ob])
            r0 = t0 - CTX
            mk.dma("pool", y_out[r0:r0 + tn, :].rearrange("(a p) d -> p a d", p=128), ob[:, 0:nt, :],
                   reads=[ob], writes=[y_out])
        mk.release(m0)

    def _rstd(self, buf, sq, rstd, tn, ps, eps=EPS):
        mk = self.mk
        for j in range(DC):
            mk.op("act", lambda e, j=j: e.activation(out=sq[:, j, 0:tn], in_=buf[:, j, 0:tn], func=AF.Square),
                  reads=[buf], writes=[sq])
        for j in range(DC):
            mk.op("pe", lambda e, j=j: e.matmul(ps[:, 0:tn], self.avg_b[:], sq[:, j, 0:tn],
                                                 start=(j == 0), stop=(j == DC - 1)),
                  reads=[self.avg_b, sq], writes=[ps])
        mk.op("act", lambda e: e.activation(out=rstd[:, 0:tn], in_=ps[:, 0:tn], func=AF.Sqrt, bias=self.eps_t[:, 0:1]),
              reads=[ps, self.eps_t], writes=[rstd])
        mk.op("dve", lambda e: e.reciprocal(rstd[:, 0:tn], rstd[:, 0:tn]), reads=[rstd], writes=[rstd])

    def ap3(self, t2d, lo, n, inner):
        return t2d[:, lo:lo + n * inner].rearrange("p (a b) -> p a b", b=inner)

    def stage_mod(self, i, sT, w_mod, bmod, gmix, gffn):
        mk = self.mk
        m0 = mk.mark()
        wb = [mk.sbuf("wmod%d" % k, [128, DC, 512], F32) for k in range(2)]
        bm = mk.sbuf("bm", [128, 48], F32)
        gm = mk.sbuf("gm", [128, 2, DC], F32)
        mod = mk.sbuf("mod", [128, 48, 2], F32)
        mk.dma("sp", bm[:], bmod[i, :, :], reads=[bmod], writes=[bm])
        mk.dma("sp", gm[:, 0, :], gmix[i, :, :], reads=[gmix], writes=[gm])
        mk.dma("sp", gm[:, 1, :], gffn[i, :, :], reads=[gffn], writes=[gm])
        ps = self.ps[0]
        for s in range(12):
            w = wb[s % 2]
            mk.dma("sp", w[:], w_mod[i, :, s * 512:(s + 1) * 512].rearrange("(k p) m -> p k m", p=128),
                   reads=[w_mod], writes=[w])
            for mm in range(4):
                m = s * 4 + mm
                for k in range(DC):
                    mk.op("pe", lambda e, w=w, mm=mm, m=m, k=k: e.matmul(
                        ps[:, 2 * m:2 * m + 2], w[:, k, mm * 128:(mm + 1) * 128], sT[:, k, :],
                        start=(k == 0), stop=(k == DC - 1)), reads=[w, sT], writes=[ps])
        psv = ps[:, 0:96].rearrange("p (m r) -> p m r", r=2)
        for r in range(2):
            mk.op("dve", lambda e, r=r: e.tensor_tensor(out=mod[:, :, r], in0=psv[:, :, r], in1=bm[:, :], op=ALU.add),
                  reads=[ps, bm], writes=[mod])
        for r in range(2):
            for nm, sc_c, sh_c, g_c, gi in (("1", 8, 0, 16, 0), ("2", 32, 24, 40, 1)):
                gs = self.mv[("gs" + nm, r)]
                mk.op("dve", lambda e, gs=gs, sc_c=sc_c, gi=gi, r=r: e.scalar_tensor_tensor(
                    out=gs[:], in0=mod[:, sc_c:sc_c + 8, r], scalar=1.0, in1=gm[:, gi, :],
                    op0=ALU.add, op1=ALU.mult), reads=[mod, gm], writes=[gs])
                sh = self.mv[("sh" + nm, r)]
                mk.op("dve", lambda e, sh=sh, sh_c=sh_c, r=r: e.tensor_copy(sh[:], mod[:, sh_c:sh_c + 8, r]),
                      reads=[mod], writes=[sh])
                g = self.mv[("g" + nm, r)]
                mk.op("dve", lambda e, g=g, g_c=g_c, r=r: e.tensor_copy(g[:], mod[:, g_c:g_c + 8, r]),
                      reads=[mod], writes=[g])
        mk.release(m0)

    def alloc_mv(self):
        self.mv = {}
        for r in range(2):
            for nm in ("gs1", "sh1", "g1", "gs2", "sh2", "g2"):
                self.mv[(nm, r)] = self.mk.sbuf("mv_%s_%d" % (nm, r), [128, DC], F32)

    def stage_norm(self, XT, HT, which, router=None):
        mk = self.mk
        m0 = mk.mark()
        xb = [mk.sbuf("nx%d" % k, [128, DC, 512], F32) for k in range(2)]
        sq = mk.sbuf("nsq", [128, DC, 512], BF16)
        rstd = mk.sbuf("nrstd", [128, 512], F32)
        hb = [mk.sbuf("nhb%d" % k, [128, DC, 512], BF16) for k in range(2)]
        if router is not None:
            htk = [mk.sbuf("nhtk%d" % k, [128, D], BF16) for k in range(2)]
            rt = {k: mk.sbuf("rt_" + k, [128, n], F32) for k, n in
                  (("lg", 36), ("gmax", 1), ("ngmax", 1), ("ge", 4), ("gsum", 1), ("gp", 1), ("mg", 4), ("es", 8),
                   ("t8", 8), ("dd", 1), ("w2", 1), ("a1", 1), ("a2", 1), ("m1", 8), ("m2", 8), ("cw", 8),
                   ("comb", 32))}
        for bi, (t0, tn) in enumerate(BLOCKS):
            r = 1 if bi == 0 else 0
            gs, sh = self.mv[("gs" + which, r)], self.mv[("sh" + which, r)]
            buf = xb[bi % 2]
            mk.dma("sp", buf[:, :, 0:tn], XT[:, t0:t0 + tn].rearrange("(j p) t -> p j t", p=128),
                   reads=self.XTr, writes=[buf])
            self._rstd(buf, sq, rstd, tn, self.ps[0])
            h = hb[bi % 2]
            for j in range(DC):
                mk.op("dve", lambda e, j=j, buf=buf, gs=gs: e.scalar_tensor_tensor(
                    out=buf[:, j, 0:tn], in0=buf[:, j, 0:tn], scalar=gs[:, j:j + 1], in1=rstd[:, 0:tn],
                    op0=ALU.mult, op1=ALU.mult), reads=[buf, rstd, gs], writes=[buf])
                if router is not None:
                    mk.op("dve", lambda e, j=j, buf=buf, sh=sh: e.tensor_scalar(
                        out=buf[:, j, 0:tn], in0=buf[:, j, 0:tn], scalar1=sh[:, j:j + 1], scalar2=None,
                        op0=ALU.add), reads=[buf, sh], writes=[buf])
                    mk.op("act", lambda e, j=j, buf=buf, h=h: e.copy(h[:, j, 0:tn], buf[:, j, 0:tn]),
                          reads=[buf], writes=[h])
                else:
                    mk.op("act", lambda e, j=j, buf=buf, h=h, sh=sh: e.activation(
                        out=h[:, j, 0:tn], in_=buf[:, j, 0:tn], func=AF.Identity, bias=sh[:, j:j + 1]),
                        reads=[buf, sh], writes=[h])
            mk.dma("pool", HT[:, t0:t0 + tn].rearrange("(j p) t -> p j t", p=128), h[:, :, 0:tn],
                   reads=[h], writes=[HT])
            if router is not None:
                for a in range(tn // 128):
                    if "M1a" in router:
                        self._route_tile_sparse(buf, a, t0 + a * 128, router, rt)
                        pb = self.ps[5 + (a % 2)]
                        pbb = pb[:, 0:512].bitcast(BF16)
                        for j in range(DC):
                            mk.op("pe", lambda e, pbb=pbb, j=j, a=a, h=h: e.transpose(
                                pbb[:, j * 128:(j + 1) * 128], h[:, j, a * 128:(a + 1) * 128], self.ident_b[:]),
                                reads=[h, self.ident_b], writes=[pb])
                        ht_ = htk[a % 2]
                        mk.op("act", lambda e, pbb=pbb, ht_=ht_: e.copy(ht_[:], pbb[:, :]), reads=[pb], writes=[ht_])
                        mk.dma("act", self.dram["HTOK"][t0 + a * 128:t0 + (a + 1) * 128, :], ht_[:], reads=[ht_],
                               writes=[self.dram["HTOK"]])
                    else:
                        self._route_tile(buf, a, t0 + a * 128, router, rt)
        mk.release(m0)

    def _route_tile(self, hf, a, tok0, R, rt):
        mk = self.mk
        ps = self.ps[1 + (a % 2)]
        Wr, br, combT = R["Wr"], R["br"], R["combT"]
        for k in range(DC):
            mk.op("pe", lambda e, k=k: e.matmul(ps[:, 0:36], hf[:, k, a * 128:(a + 1) * 128], Wr[:, k, :],
                                                 start=(k == 0), stop=(k == DC - 1)),
                  reads=[hf, Wr], writes=[ps])
        lg, gmax, ngmax, ge, gsum, gp, mg, es = (rt[k] for k in ("lg", "gmax", "ngmax", "ge", "gsum", "gp", "mg", "es"))
        t8, dd, w2, a1, a2, m1, m2, cw, comb = (rt[k] for k in ("t8", "dd", "w2", "a1", "a2", "m1", "m2", "cw", "comb"))
        V = lambda fn, reads, writes: mk.op("dve", fn, reads=reads, writes=writes)
        V(lambda e: e.tensor_tensor(out=lg[:], in0=ps[:, 0:36], in1=br[:], op=ALU.add), [ps, br], [lg])
        V(lambda e: e.reduce_max(out=gmax[:], in_=lg[:, 0:4], axis=AX.X), [lg], [gmax])
        V(lambda e: e.tensor_scalar(out=ngmax[:], in0=gmax[:], scalar1=-1.0, scalar2=None, op0=ALU.mult), [gmax], [ngmax])
        mk.op("act", lambda e: e.activation(out=ge[:], in_=lg[:, 0:4], func=AF.Exp, bias=ngmax[:, 0:1],
                                            accum_out=gsum[:]), reads=[lg, ngmax], writes=[ge, gsum])
        V(lambda e: e.reciprocal(gp[:], gsum[:]), [gsum], [gp])
        V(lambda e: e.tensor_scalar(out=mg[:], in0=lg[:, 0:4], scalar1=gmax[:, 0:1], scalar2=None, op0=ALU.is_equal),
          [lg, gmax], [mg])
        V(lambda e: e.tensor_scalar(out=es[:], in0=lg[:, 4:12], scalar1=mg[:, 0:1], scalar2=None, op0=ALU.mult),
          [lg, mg], [es])
        for g in range(1, 4):
            V(lambda e, g=g: e.scalar_tensor_tensor(out=es[:], in0=lg[:, 4 + 8 * g:12 + 8 * g], scalar=mg[:, g:g + 1],
                                                    in1=es[:], op0=ALU.mult, op1=ALU.add), [lg, mg, es], [es])
        V(lambda e: e.max(out=t8[:], in_=es[:]), [es], [t8])
        V(lambda e: e.tensor_tensor(out=dd[:], in0=t8[:, 1:2], in1=t8[:, 0:1], op=ALU.subtract), [t8], [dd])
        mk.op("act", lambda e: e.activation(out=w2[:], in_=dd[:], func=AF.Sigmoid), reads=[dd], writes=[w2])
        V(lambda e: e.tensor_tensor(out=a2[:], in0=w2[:], in1=gp[:], op=ALU.mult), [w2, gp], [a2])
        V(lambda e: e.tensor_tensor(out=a1[:], in0=gp[:], in1=a2[:], op=ALU.subtract), [gp, a2], [a1])
        V(lambda e: e.tensor_scalar(out=m1[:], in0=es[:], scalar1=t8[:, 0:1], scalar2=a1[:, 0:1], op0=ALU.is_equal,
                                    op1=ALU.mult), [es, t8, a1], [m1])
        V(lambda e: e.tensor_scalar(out=m2[:], in0=es[:], scalar1=t8[:, 1:2], scalar2=a2[:, 0:1], op0=ALU.is_equal,
                                    op1=ALU.mult), [es, t8, a2], [m2])
        V(lambda e: e.tensor_tensor(out=cw[:], in0=m1[:], in1=m2[:], op=ALU.add), [m1, m2], [cw])
        for g in range(4):
            V(lambda e, g=g: e.tensor_scalar(out=comb[:, 8 * g:8 * g + 8], in0=cw[:], scalar1=mg[:, g:g + 1],
                                             scalar2=None, op0=ALU.mult), [cw, mg], [comb])
        ps2 = self.ps[3 + (a % 2)]
        mk.op("pe", lambda e: e.transpose(ps2[0:32, 0:128], comb[:, :], self.ident_f[:]),
              reads=[comb, self.ident_f], writes=[ps2])
        mk.op("act", lambda e: e.copy(combT[:, tok0:tok0 + 128], ps2[0:32, 0:128]), reads=[ps2], writes=[combT])

    def stage_moe(self, i, XT, HT, HID, combT, sel, w_gate, w_up, w_down):
        mk = self.mk
        m0 = mk.mark()
        hT = mk.sbuf("moe_hT", [128, DC, NT], BF16)
        for j in range(DC):
            mk.dma("sp", hT[:, j, :], HT[j * 128:(j + 1) * 128, :], reads=[HT], writes=[hT])
        wgu = [mk.sbuf("wgu%d" % k, [128, DC, 512], BF16) for k in range(2)]
        cbs = [mk.sbuf("cbs%d" % k, [128, 512], F32) for k in range(2)]
        sl = [mk.sbuf("sl%d" % k, [128, 512], F32) for k in range(2)]
        tt = [mk.sbuf("tt%d" % k, [128, 512], F32) for k in range(2)]
        hid = [mk.sbuf("hid%d" % k, [128, 2, 512], BF16) for k in range(2)]
        n = 0
        for ex in range(NE):
            g, el = ex // 8, ex % 8
            w = wgu[ex % 2]
            mk.dma("pool", w[:, :, 0:256], w_gate[i, g, el, :, :].rearrange("(k p) f -> p k f", p=128),
                   reads=[w_gate], writes=[w])
            mk.dma("pool", w[:, :, 256:512], w_up[i, g, el, :, :].rearrange("(k p) f -> p k f", p=128),
                   reads=[w_up], writes=[w])
            for bi, (t0, tn) in enumerate(BLOCKS):
                pc = self.ps[n % 2]
                mk.op("pe", lambda e, pc=pc, ex=ex: e.matmul(pc[:, 0:tn], sel[:, ex, :], combT[:, t0:t0 + tn],
                                                            start=True, stop=True),
                      reads=[sel, combT], writes=[pc])
                cb = cbs[n % 2]
                mk.op("act", lambda e, pc=pc, cb=cb: e.copy(cb[:, 0:tn], pc[:, 0:tn]), reads=[pc], writes=[cb])
                hd = hid[n % 2]
                for fc in range(2):
                    q = (2 * n + fc) % 3
                    pg, pu = self.ps[2 + 2 * q], self.ps[3 + 2 * q]
                    for k in range(DC):
                        mk.op("pe", lambda e, pg=pg, k=k, fc=fc, w=w: e.matmul(
                            pg[:, 0:tn], w[:, k, fc * 128:(fc + 1) * 128], hT[:, k, t0:t0 + tn],
                            start=(k == 0), stop=(k == DC - 1)), reads=[w, hT], writes=[pg])
                    for k in range(DC):
                        mk.op("pe", lambda e, pu=pu, k=k, fc=fc, w=w: e.matmul(
                            pu[:, 0:tn], w[:, k, 256 + fc * 128:256 + (fc + 1) * 128], hT[:, k, t0:t0 + tn],
                            start=(k == 0), stop=(k == DC - 1)), reads=[w, hT], writes=[pu])
                    s_, t_ = sl[fc], tt[fc]
                    mk.op("act", lambda e, pg=pg, s_=s_: e.activation(out=s_[:, 0:tn], in_=pg[:, 0:tn], func=AF.Silu),
                          reads=[pg], writes=[s_])
                    mk.op("dve", lambda e, pu=pu, s_=s_, t_=t_: e.tensor_tensor(
                        out=t_[:, 0:tn], in0=pu[:, 0:tn], in1=s_[:, 0:tn], op=ALU.mult), reads=[pu, s_], writes=[t_])
                    mk.op("pool", lambda e, t_=t_, cb=cb, hd=hd, fc=fc: e.tensor_tensor(
                        out=hd[:, fc, 0:tn], in0=t_[:, 0:tn], in1=cb[:, 0:tn], op=ALU.mult), reads=[t_, cb], writes=[hd])
                mk.dma("act", HID[ex * 256:(ex + 1) * 256, t0:t0 + tn].rearrange("(c p) t -> p c t", p=128),
                       hd[:, :, 0:tn], reads=[hd], writes=[HID])
                n += 1
        mk.release(m0)
        m0 = mk.mark()
        hb = [mk.sbuf("p2h%d" % k, [128, 64, 512], BF16) for k in range(1)]
        wd = [mk.sbuf("p2w%d" % k, [128, 8, D], BF16) for k in range(2)]
        xr = [mk.sbuf("p2x%d" % k, [128, 512], F32) for k in range(3)]
        wdv = w_down[i].rearrange("g e f d -> (g e f) d")
        n = 0
        for bi, (t0, tn) in enumerate(BLOCKS):
            r = 1 if bi == 0 else 0
            g2 = self.mv[("g2", r)]
            h = hb[0]
            for c8 in range(8):
                mk.dma("sp", h[:, c8 * 8:(c8 + 1) * 8, 0:tn],
                       HID[c8 * 1024:(c8 + 1) * 1024, t0:t0 + tn].rearrange("(c p) t -> p c t", p=128),
                       reads=[HID], writes=[h])
            for kg in range(8):
                w = wd[n % 2]
                n += 1
                mk.dma("pool", w[:], wdv[kg * 1024:(kg + 1) * 1024, :].rearrange("(c p) d -> p c d", p=128),
                       reads=[w_down], writes=[w])
                for kk in range(8):
                    kc = kg * 8 + kk
                    for dc in range(DC):
                        mk.op("pe", lambda e, dc=dc, kk=kk, kc=kc, w=w: e.matmul(
                            self.ps[dc][:, 0:tn], w[:, kk, dc * 128:(dc + 1) * 128], h[:, kc, 0:tn],
                            start=(kc == 0), stop=(kc == 63)), reads=[w, h], writes=[self.ps[dc]])
            for dc in range(DC):
                x = xr[dc % 3]
                mk.dma("sp", x[:, 0:tn], XT[dc * 128:(dc + 1) * 128, t0:t0 + tn], reads=[self.XTr[dc]], writes=[x])
                mk.op("dve", lambda e, dc=dc, x=x, g2=g2: e.scalar_tensor_tensor(
                    out=x[:, 0:tn], in0=self.ps[dc][:, 0:tn], scalar=g2[:, dc:dc + 1], in1=x[:, 0:tn],
                    op0=ALU.mult, op1=ALU.add), reads=[self.ps[dc], g2, x], writes=[x])
                mk.dma("act", XT[dc * 128:(dc + 1) * 128, t0:t0 + tn], x[:, 0:tn], reads=[x], writes=[self.XTr[dc]])
        mk.release(m0)


def build_program(cfg=None):
    cfg = cfg or {}
    p = Prog(cfg)
    mk = p.mk
    layers = cfg.get("layers", list(range(DEPTH)))
    I = {}
    I["x"] = p.inp("x", [SEQ, D])
    I["ctx"] = p.inp("ctx", [CTX, D])
    I["c_pc"] = p.inp("c_pc", [128, DC, 2])
    I["w_mod"] = p.inp("w_mod", [DEPTH, D, 6 * D])
    I["bmod_pc"] = p.inp("bmod_pc", [DEPTH, 128, 48])
    I["gmix_pc"] = p.inp("gmix_pc", [DEPTH, 128, DC])
    I["gffn_pc"] = p.inp("gffn_pc", [DEPTH, 128, DC])
    I["fg_pc"] = p.inp("fg_pc", [128, DC])
    I["wr_pc"] = p.inp("wr_pc", [DEPTH, 128, DC, 36])
    I["br_bc"] = p.inp("br_bc", [DEPTH, 128, 36])
    I["sel"] = p.inp("sel", [32, NE, 128], BF16)
    sparse = cfg.get("sparse", True)
    if sparse:
        for li in range(DEPTH):
            I["moe_wgu%d" % li] = p.inp("moe_wgu%d" % li, [NE * 128, DC * 512])
            I["moe_wdn%d" % li] = p.inp("moe_wdn%d" % li, [NE * 128, 2 * D])
        I["jgrid"] = p.inp("jgrid", [128, NBLK])
        I["pidx"] = p.inp("pidx", [128, 1])
    else:
        I["moe_w_gate"] = p.inp("moe_w_gate", [DEPTH, 4, 8, D, DEXP])
        I["moe_w_up"] = p.inp("moe_w_up", [DEPTH, 4, 8, D, DEXP])
        I["moe_w_down"] = p.inp("moe_w_down", [DEPTH, 4, 8, DEXP, D])
    I["attn_w_q"] = p.inp("attn_w_q", [2, D, D])
    I["attn_w_kv"] = p.inp("attn_w_kv", [2, D, 512])
    I["attn_w_o"] = p.inp("attn_w_o", [2, D, D])
    I["qkgain_pc"] = p.inp("qkgain_pc", [2, 128, 2])
    I["conv_w_pw1"] = p.inp("conv_w_pw1", [1, D, 2 * D])
    I["conv_w_pw2"] = p.inp("conv_w_pw2", [1, D, D])
    I["conv_b_pw1_pc"] = p.inp("conv_b_pw1_pc", [1, 128, 16])
    I["conv_w_dw_pc"] = p.inp("conv_w_dw_pc", [1, 128, DC, 31])
    I["conv_b_dw_pc"] = p.inp("conv_b_dw_pc", [1, 128, DC])
    I["conv_ln_g_pc"] = p.inp("conv_ln_g_pc", [1, 128, DC])
    I["conv_ln_b_pc"] = p.inp("conv_ln_b_pc", [1, 128, DC])
    I["conv_b_pw2_pc"] = p.inp("conv_b_pw2_pc", [1, 128, DC])
    I["hy_w_in"] = p.inp("hy_w_in", [1, D, 3 * D])
    I["hy_w_out"] = p.inp("hy_w_out", [1, D, D])
    I["hy_b_in_pc"] = p.inp("hy_b_in_pc", [128, 24])
    I["hy_w_short_pc"] = p.inp("hy_w_short_pc", [128, 24, 3])
    I["hy_b_short_pc"] = p.inp("hy_b_short_pc", [128, 24])
    I["hy_b_out_pc"] = p.inp("hy_b_out_pc", [128, DC])
    I["hy_lbias_pc"] = p.inp("hy_lbias_pc", [2, 128, DC])
    I["hy_f_w1"] = p.inp("hy_f_w1", [1, 17, 64])
    I["hy_f_w2"] = p.inp("hy_f_w2", [1, 64, 64])
    I["hy_f_w3"] = p.inp("hy_f_w3", [1, 64, 4 * D])
    I["hy_fvec"] = p.inp("hy_fvec", [64, 4])
    I["hy_dabs"] = p.inp("hy_dabs", [64, D])
    for tag, L_ in (("lat", SEQ), ("ctx", CTX)):
        N1_ = 2 * L_ // 64
        I["hy_F1_" + tag] = p.inp("hy_F1_" + tag, [N1_ // 2, 6, N1_], BF16)
        I["hy_G_" + tag] = p.inp("hy_G_" + tag, [128, N1_, 5, 128], BF16)
        I["hy_Fi_" + tag] = p.inp("hy_Fi_" + tag, [N1_, 2, N1_ // 2], BF16)
        I["hy_lneg_" + tag] = p.inp("hy_lneg_" + tag, [N1_ // 2, 64])
        I["hy_featsT_" + tag] = p.inp("hy_featsT_" + tag, [17, L_])
    I["hyd_F"] = p.inp("hyd_F", [128, 2, 3, 512], BF16)
    I["hyd_Fi"] = p.inp("hyd_Fi", [128, 4, 2, CTX], BF16)
    I["hyd_lneg"] = p.inp("hyd_lneg", [128, 2])
    I["ropeR"] = p.inp("ropeR", [128, 128], BF16)
    I["ropeC"] = p.inp("ropeC", [128, SEQ])
    I["ropeS"] = p.inp("ropeS", [128, SEQ])
    y = T(p.nc.dram_tensor("y", [SEQ, D], F32, kind="ExternalOutput"), "y")
    QT = p.scratch("QT", [D, NT], BF16)
    OT = p.scratch("OT", [D, NT], BF16)
    XT = p.scratch("XT", [D, NT], F32)
    p.XTr = [T(XT.t, "XTr%d" % k) for k in range(DC)]
    VT = p.scratch("VT", [D, NT], F32)
    if 2 in layers and cfg.get("mixers", True):
        p.scratch("hyU0", [3 * D, NT], F32)
        p.scratch("hyZ", [3 * D, NT], F32)
        p.scratch("hyZ1", [D, NT], F32)
        p.scratch("hyA", [2, 128, 64, D], BF16)
        p.scratch("hyC", [2, 64, 128, D], BF16)
        for o in range(2):
            p.scratch("hyHH_lat%d" % o, [2, 128, 128, D], BF16)
            p.scratch("hyHH_ctx%d" % o, [2, 8, 128, D], BF16)
    HT = p.scratch("HT", [D, NT], BF16)
    p.dram["HT"] = HT
    if sparse:
        p.scratch("HTOK", [NT, D], BF16)
        p.scratch("HS", [NSLOT, D], BF16)
        p.scratch("YS", [NSLOT, D], F32)
    else:
        HID = p.scratch("HID", [NE * DEXP, NT], BF16)
    p.setup_consts()
    p.alloc_mv()
    fg = mk.sbuf("fg", [128, DC], F32)
    sT = mk.sbuf("sT", [128, DC, 2], F32)
    sel = mk.sbuf("sel", [32, NE, 128], BF16)
    if sparse:
        SR = p.alloc_sparse()
        mk.dma("sp", SR["jg"][:], I["jgrid"][:, :], writes=[SR["jg"]])
        mk.dma("sp", SR["pidx"][:], I["pidx"][:, :], writes=[SR["pidx"]])
    else:
        combT = mk.sbuf("combT", [32, NT], BF16)
    Wr = mk.sbuf("Wr", [128, DC, 36], F32)
    br = mk.sbuf("br", [128, 36], F32)
    mk.dma("sp", fg[:], I["fg_pc"][:, :], reads=[I["fg_pc"]], writes=[fg])
    mk.dma("sp", sT[:], I["c_pc"][:, :, :], reads=[I["c_pc"]], writes=[sT])
    mk.dma("sp", sel[:], I["sel"][:, :, :], reads=[I["sel"]], writes=[sel])
    mk.op("act", lambda e: e.activation(out=sT[:], in_=sT[:], func=AF.Silu), reads=[sT], writes=[sT])
    p.stage_input(I["x"], I["ctx"], XT)
    for i in layers:
        p.stage_mod(i, sT, I["w_mod"], I["bmod_pc"], I["gmix_pc"], I["gffn_pc"])
        if cfg.get("mixers", True):
            kind, slot = i % 3, i // 3
            with_ctx = i < DEPTH - 1
            p.stage_norm(XT, HT, "1")
            if kind == 0:
                p.stage_attn(slot, XT, HT, QT, OT, I, with_ctx)
            elif kind == 1:
                p.stage_conf(slot, XT, HT, QT, VT, I)
            else:
                p.stage_hyena(slot, XT, HT, I)
        if cfg.get("moe", True) is False:
            continue
        mk.dma("sp", Wr[:], I["wr_pc"][i, :, :, :], reads=[I["wr_pc"]], writes=[Wr])
        mk.dma("sp", br[:], I["br_bc"][i, :, :], reads=[I["br_bc"]], writes=[br])
        if sparse:
            SR["Wr"], SR["br"] = Wr, br
            p.stage_norm(XT, HT, "2", router=SR)
            p.stage_moe_sparse(i, XT, SR, I)
        else:
            p.stage_norm(XT, HT, "2", router=dict(Wr=Wr, br=br, combT=combT))
            p.stage_moe(i, XT, HT, HID, combT, sel, I["moe_w_gate"], I["moe_w_up"], I["moe_w_down"])
    p.stage_output(XT, y, fg)
    mk.finish()
    mk.close()
    return p


_HYC = {}


def host_inputs(inp, b, sparse=True):
    f = lambda a: np.ascontiguousarray(np.asarray(a, np.float32))
    m = {}
    m["x"] = f(inp["x"][b])
    m["ctx"] = f(inp["ctx"][b])
    m["c_pc"] = np.ascontiguousarray(np.stack([_vec_pc(inp["c"][b]), _vec_pc(inp["c_ctx"])], axis=-1))
    m["w_mod"] = f(inp["w_mod"])
    m["bmod_pc"] = np.stack([_vec_pc(inp["b_mod"][i]) for i in range(DEPTH)])
    m["gmix_pc"] = np.stack([_vec_pc(inp["norm_mix_g"][i]) for i in range(DEPTH)])
    m["gffn_pc"] = np.stack([_vec_pc(inp["norm_ffn_g"][i]) for i in range(DEPTH)])
    m["fg_pc"] = _vec_pc(inp["final_norm_g"])
    wr = np.concatenate([np.asarray(inp["moe_w_group"]), np.asarray(inp["moe_w_router"])], axis=-1)
    m["wr_pc"] = np.ascontiguousarray(wr.reshape(DEPTH, DC, 128, 36).transpose(0, 2, 1, 3)).astype(np.float32)
    brr = np.concatenate([np.asarray(inp["moe_b_group"]), np.asarray(inp["moe_b_router"])], axis=-1)
    m["br_bc"] = np.ascontiguousarray(np.broadcast_to(brr[:, None, :], (DEPTH, 128, 36))).astype(np.float32)
    sel = np.zeros((32, NE, 128), np.float32)
    for e in range(NE):
        sel[e, e, :] = 1.0
    m["sel"] = sel.astype(ml_dtypes.bfloat16)
    m["attn_w_q"] = f(inp["attn_w_q"]); m["attn_w_kv"] = f(inp["attn_w_kv"]); m["attn_w_o"] = f(inp["attn_w_o"])
    m["qkgain_pc"] = np.ascontiguousarray(np.stack([np.asarray(inp["attn_q_gain"], np.float32),
                                                    np.asarray(inp["attn_k_gain"], np.float32)], axis=-1))
    m["conv_w_pw1"] = f(inp["conv_w_pw1"]); m["conv_w_pw2"] = f(inp["conv_w_pw2"])
    m["conv_b_pw1_pc"] = _vec_pc(inp["conv_b_pw1"][0])[None]
    m["conv_w_dw_pc"] = np.ascontiguousarray(np.asarray(inp["conv_w_dw"][0], np.float32).reshape(31, DC, 128).transpose(2, 1, 0))[None]
    for nm in ("conv_b_dw", "conv_ln_g", "conv_ln_b", "conv_b_pw2"):
        m[nm + "_pc"] = _vec_pc(inp[nm][0])[None]
    m["hy_w_in"] = f(inp["hy_w_in"]); m["hy_w_out"] = f(inp["hy_w_out"])
    m["hy_b_in_pc"] = _vec_pc(inp["hy_b_in"][0]); m["hy_b_short_pc"] = _vec_pc(inp["hy_b_short"][0])
    m["hy_w_short_pc"] = np.ascontiguousarray(np.stack([_vec_pc(inp["hy_w_short"][0, k]) for k in range(3)], axis=-1))
    m["hy_b_out_pc"] = _vec_pc(inp["hy_b_out"][0])
    m["hy_lbias_pc"] = np.stack([_vec_pc(inp["hy_long_bias"][0, o]) for o in range(2)])
    m["hy_f_w1"] = f(inp["hy_f_w1"]); m["hy_f_w2"] = f(inp["hy_f_w2"]); m["hy_f_w3"] = f(inp["hy_f_w3"])
    m["hy_fvec"] = np.ascontiguousarray(np.stack([np.asarray(inp[k][0], np.float32) for k in
                                                  ("hy_f_b1", "hy_f_freq1", "hy_f_b2", "hy_f_freq2")], axis=-1))
    dl = np.abs(np.linspace(math.log(1e-2) / 1.5, math.log(1e-2) / 0.3, D, dtype=np.float32))
    m["hy_dabs"] = np.ascontiguousarray(np.broadcast_to(dl[None, :], (64, D))).astype(np.float32)
    for tag, L_ in (("lat", SEQ), ("ctx", CTX)):
        hc = _HYC[tag] if tag in _HYC else _HYC.setdefault(tag, hy_consts(L_))
        m["hy_F1_" + tag] = hc["F1"]; m["hy_G_" + tag] = hc["G"]; m["hy_Fi_" + tag] = hc["Fi"]
        m["hy_lneg_" + tag] = hc["lneg"]; m["hy_featsT_" + tag] = hc["featsT"]
    hdc = _HYC["hyd"] if "hyd" in _HYC else _HYC.setdefault("hyd", hyd_consts())
    m["hyd_F"] = hdc["F"]; m["hyd_Fi"] = hdc["Fi"]; m["hyd_lneg"] = hdc["lneg"]
    RT = np.zeros((128, 128), np.float32)
    for base in (0, 64):
        for dd in range(32):
            RT[base + dd + 32, base + dd] = -1.0
            RT[base + dd, base + dd + 32] = 1.0
    m["ropeR"] = RT.astype(ml_dtypes.bfloat16)
    inv = (np.float32(10000.0) ** (-np.arange(32, dtype=np.float32) / np.float32(32))).astype(np.float32)
    tt = np.arange(SEQ)
    ang = np.zeros((128, SEQ), np.float32)
    for dd in range(128):
        pos = (tt // 64) if dd < 64 else (tt % 64)
        ang[dd] = pos.astype(np.float32) * inv[dd % 32]
    m["ropeC"] = np.cos(ang).astype(np.float32)
    m["ropeS"] = np.sin(ang).astype(np.float32)
    if sparse:
        wg = np.asarray(inp["moe_w_gate"], np.float32).reshape(DEPTH, NE, DC, 128, DEXP)
        wu = np.asarray(inp["moe_w_up"], np.float32).reshape(DEPTH, NE, DC, 128, DEXP)
        wd = np.asarray(inp["moe_w_down"], np.float32).reshape(DEPTH, NE, 2, 128, D)
        for li in range(DEPTH):
            m["moe_wgu%d" % li] = np.ascontiguousarray(np.concatenate([wg[li], wu[li]], axis=-1).transpose(0, 2, 1, 3)).reshape(NE * 128, DC * 512)
            m["moe_wdn%d" % li] = np.ascontiguousarray(wd[li].transpose(0, 2, 1, 3)).reshape(NE * 128, 2 * D)
        m["jgrid"] = np.ascontiguousarray(np.broadcast_to((np.arange(NBLK, dtype=np.float32) * BSL)[None, :], (128, NBLK)))
        m["pidx"] = np.arange(128, dtype=np.float32)[:, None].copy()
    else:
        m["moe_w_gate"] = f(inp["moe_w_gate"])
        m["moe_w_up"] = f(inp["moe_w_up"])
        m["moe_w_down"] = f(inp["moe_w_down"])
    return m


def _attn_methods():
    def load_w(self, name, src_ap, kc, m, q="pool"):
        mk = self.mk
        w = mk.sbuf(name, [128, kc, m], BF16)
        step = max(1, 4096 // m)
        for k0 in range(0, kc, step):
            k1 = min(kc, k0 + step)
            mk.dma(q, w[:, k0:k1, :], src_ap[k0 * 128:k1 * 128, :].rearrange("(k p) m -> p k m", p=128), writes=[w])
        return w

    def linear_resid(self, XT, srcT, W, KC, gname, bias=None, skip_ctx=False):
        mk = self.mk
        m0 = mk.mark()
        sb = [mk.sbuf("lr_s%d" % k, [128, KC, 512], BF16) for k in range(2)]
        xr = [mk.sbuf("lr_x%d" % k, [128, 512], F32) for k in range(3)]
        gb = None
        if bias is not None:
            gb = [mk.sbuf("lr_gb%d" % r, [128, DC], F32) for r in range(2)]
            for r in range(2):
                mk.op("dve", lambda e, r=r: e.tensor_tensor(out=gb[r][:], in0=bias[:], in1=self.mv[(gname, r)][:],
                                                            op=ALU.mult), reads=[bias, self.mv[(gname, r)]], writes=[gb[r]])
        n = 0
        for bi, (t0, tn) in enumerate(BLOCKS):
            if skip_ctx and bi == 0:
                continue
            r = 1 if bi == 0 else 0
            g = self.mv[(gname, r)]
            s = sb[bi % 2]
            mk.dma("sp", s[:, :, 0:tn], srcT[:, t0:t0 + tn].rearrange("(k p) t -> p k t", p=128),
                   reads=[srcT], writes=[s])
            for dc in range(DC):
                ps = self.ps[n % 4]
                x = xr[n % 3]
                n += 1
                mk.dma("sp", x[:, 0:tn], XT[dc * 128:(dc + 1) * 128, t0:t0 + tn], reads=[self.XTr[dc]], writes=[x])
                for k in range(KC):
                    mk.op("pe", lambda e, ps=ps, k=k, dc=dc, s=s: e.matmul(
                        ps[:, 0:tn], W[:, k, dc * 128:(dc + 1) * 128], s[:, k, 0:tn],
                        start=(k == 0), stop=(k == KC - 1)), reads=[W, s], writes=[ps])
                mk.op("dve", lambda e, ps=ps, dc=dc, x=x, g=g: e.scalar_tensor_tensor(
                    out=x[:, 0:tn], in0=ps[:, 0:tn], scalar=g[:, dc:dc + 1], in1=x[:, 0:tn],
                    op0=ALU.mult, op1=ALU.add), reads=[ps, g, x], writes=[x])
                if gb is not None:
                    mk.op("dve", lambda e, dc=dc, x=x, r=r: e.tensor_scalar(
                        out=x[:, 0:tn], in0=x[:, 0:tn], scalar1=gb[r][:, dc:dc + 1], scalar2=None, op0=ALU.add),
                        reads=[x, gb[r]], writes=[x])
                mk.dma("act", XT[dc * 128:(dc + 1) * 128, t0:t0 + tn], x[:, 0:tn], reads=[x], writes=[self.XTr[dc]])
        mk.release(m0)

    def stage_attn(self, slot, XT, HT, QT, OT, I, with_ctx):
        mk = self.mk
        mA = mk.mark()
        KT = mk.sbuf("KT", [128, 2, NT], BF16)
        Vs = mk.sbuf("Vs", [128, NT // 128, 256], BF16)
        gains = mk.sbuf("qkgain", [128, 2], F32)
        RT = mk.sbuf("RT", [128, 128], BF16)
        avgh = mk.sbuf("avgh", [128, 128], BF16)
        mk.dma("sp", gains[:], I["qkgain_pc"][slot, :, :], writes=[gains])
        mk.dma("sp", RT[:], I["ropeR"][:, :], writes=[RT])
        mk.op("pool", lambda e: e.memset(avgh[:], 1.0 / 128), writes=[avgh])
        m0 = mk.mark()
        wq = self.load_w("wq", I["attn_w_q"][slot], DC, D)
        wkv = self.load_w("wkv", I["attn_w_kv"][slot], DC, 512)
        hb = [mk.sbuf("ah%d" % k, [128, DC, 512], BF16) for k in range(2)]
        cs = [mk.sbuf("acs%d" % k, [128, 2, 512], F32) for k in range(2)]
        sqq = [mk.sbuf("asq%d" % k, [128, 512], BF16) for k in range(2)]
        rs = [mk.sbuf("ars%d" % k, [128, 512], F32) for k in range(2)]
        qn = [mk.sbuf("aqn%d" % k, [128, 512], F32) for k in range(2)]
        qb = [mk.sbuf("aqb%d" % k, [128, 512], BF16) for k in range(2)]
        t1 = [mk.sbuf("at1%d" % k, [128, 512], F32) for k in range(2)]
        t2 = [mk.sbuf("at2%d" % k, [128, 512], F32) for k in range(2)]
        qo = [mk.sbuf("aqo%d" % k, [128, 512], BF16) for k in range(3)]
        n = 0
        for bi, (t0, tn) in enumerate(BLOCKS):
            h = hb[bi % 2]
            mk.dma("sp", h[:, :, 0:tn], HT[:, t0:t0 + tn].rearrange("(k p) t -> p k t", p=128), reads=[HT], writes=[h])
            c = cs[bi % 2]
            if bi > 0:
                mk.dma("sp", c[:, 0, 0:tn], I["ropeC"][:, t0 - CTX:t0 - CTX + tn], writes=[c])
                mk.dma("sp", c[:, 1, 0:tn], I["ropeS"][:, t0 - CTX:t0 - CTX + tn], writes=[c])
            for hh in range(10):
                isq = hh < 8
                if isq and bi == 0 and not with_ctx:
                    continue
                W = wq if isq else wkv
                c0 = hh * 128 if isq else (hh - 8) * 128
                gcol = 0 if isq else 1
                ps = self.ps[n % 2]
                pz = self.ps[2 + n % 2]
                pr = self.ps[4 + n % 2]
                i2 = n % 2
                n += 1
                for k in range(DC):
                    mk.op("pe", lambda e, ps=ps, k=k, W=W, c0=c0, h=h: e.matmul(
                        ps[:, 0:tn], W[:, k, c0:c0 + 128], h[:, k, 0:tn], start=(k == 0), stop=(k == DC - 1)),
                        reads=[W, h], writes=[ps])
                mk.op("act", lambda e, ps=ps, i2=i2: e.activation(out=sqq[i2][:, 0:tn], in_=ps[:, 0:tn], func=AF.Square),
                      reads=[ps], writes=[sqq[i2]])
                mk.op("pe", lambda e, pz=pz, i2=i2: e.matmul(pz[:, 0:tn], avgh[:], sqq[i2][:, 0:tn], start=True, stop=True),
                      reads=[avgh, sqq[i2]], writes=[pz])
                mk.op("act", lambda e, pz=pz, i2=i2: e.activation(out=rs[i2][:, 0:tn], in_=pz[:, 0:tn], func=AF.Sqrt,
                                                                 bias=self.eps_t[:, 0:1]), reads=[pz, self.eps_t], writes=[rs[i2]])
                mk.op("dve", lambda e, i2=i2: e.reciprocal(rs[i2][:, 0:tn], rs[i2][:, 0:tn]), reads=[rs[i2]], writes=[rs[i2]])
                mk.op("dve", lambda e, ps=ps, i2=i2, gcol=gcol: e.scalar_tensor_tensor(
                    out=qn[i2][:, 0:tn], in0=ps[:, 0:tn], scalar=gains[:, gcol:gcol + 1], in1=rs[i2][:, 0:tn],
                    op0=ALU.mult, op1=ALU.mult), reads=[ps, gains, rs[i2]], writes=[qn[i2]])
                if isq:
                    dst_t = qo[n % 3]
                    dst = dst_t[:, 0:tn]
                else:
                    dst_t = KT
                    dst = KT[:, hh - 8, t0:t0 + tn]
                if bi == 0:
                    mk.op("act", lambda e, i2=i2, dst=dst: e.copy(dst, qn[i2][:, 0:tn]), reads=[qn[i2]], writes=[dst_t])
                else:
                    mk.op("act", lambda e, i2=i2: e.copy(qb[i2][:, 0:tn], qn[i2][:, 0:tn]), reads=[qn[i2]], writes=[qb[i2]])
                    mk.op("pe", lambda e, pr=pr, i2=i2: e.matmul(pr[:, 0:tn], RT[:], qb[i2][:, 0:tn], start=True, stop=True),
                          reads=[RT, qb[i2]], writes=[pr])
                    mk.op("pool", lambda e, i2=i2, c=c: e.tensor_tensor(out=t1[i2][:, 0:tn], in0=qn[i2][:, 0:tn],
                                                                       in1=c[:, 0, 0:tn], op=ALU.mult),
                          reads=[qn[i2], c], writes=[t1[i2]])
                    mk.op("dve", lambda e, pr=pr, i2=i2, c=c: e.tensor_tensor(out=t2[i2][:, 0:tn], in0=pr[:, 0:tn],
                                                                             in1=c[:, 1, 0:tn], op=ALU.mult),
                          reads=[pr, c], writes=[t2[i2]])
                    mk.op("pool", lambda e, i2=i2, dst=dst: e.tensor_tensor(out=dst, in0=t1[i2][:, 0:tn],
                                                                           in1=t2[i2][:, 0:tn], op=ALU.add),
                          reads=[t1[i2], t2[i2]], writes=[dst_t])
                if isq:
                    mk.dma("act", QT[hh * 128:(hh + 1) * 128, t0:t0 + tn], dst, reads=[dst_t], writes=[QT])
            for a in range(tn // 128):
                pv = self.ps[6 + a % 2]
                for k in range(DC):
                    mk.op("pe", lambda e, pv=pv, k=k, a=a, h=h: e.matmul(
                        pv[:, 0:256], h[:, k, a * 128:(a + 1) * 128], wkv[:, k, 256:512],
                        start=(k == 0), stop=(k == DC - 1)), reads=[h, wkv], writes=[pv])
                ti = t0 // 128 + a
                mk.op("act", lambda e, pv=pv, ti=ti: e.copy(Vs[:, ti, :], pv[:, 0:256]), reads=[pv], writes=[Vs])
        mk.release(m0)
        m0 = mk.mark()
        qblk = [mk.sbuf("bq%d" % k, [128, 512], BF16) for k in range(2)]
        pT = [mk.sbuf("bp%d" % k, [128, 512], BF16) for k in range(3)]
        rz = [mk.sbuf("brz%d" % k, [128, 512], F32) for k in range(2)]
        zacc = [mk.sbuf("bza%d" % k, [128, 512], F32) for k in range(2)]
        ones_f = mk.sbuf("bones_f", [128, 128], F32)
        mk.op("pool", lambda e: e.memset(ones_f[:], 1.0), writes=[ones_f])
        ob = [mk.sbuf("bo%d" % k, [128, 512], BF16) for k in range(2)]
        scale = 128 ** -0.5
        nb = 0
        nk = 0
        for head in range(8):
            kvh = head // 4
            for bi, (t0, tn) in enumerate(BLOCKS):
                if bi == 0 and not with_ctx:
                    continue
                nkc = 2 if bi == 0 else NT // 128
                q = qblk[nb % 2]
                mk.dma("sp", q[:, 0:tn], QT[head * 128:(head + 1) * 128, t0:t0 + tn], reads=[QT], writes=[q])
                pO = self.ps[3 + nb % 2]
                pZ = self.ps[5 + nb % 2]
                def issue_S(kc_, idx):
                    pS_ = self.ps[idx % 3]
                    mk.op("pe", lambda e, pS_=pS_, kc_=kc_, q=q: e.matmul(
                        pS_[:, 0:tn], KT[:, kvh, kc_ * 128:(kc_ + 1) * 128], q[:, 0:tn], start=True, stop=True),
                        reads=[KT, q], writes=[pS_])
                issue_S(0, nk)
                for kc in range(nkc):
                    pS = self.ps[nk % 3]
                    p_ = pT[nk % 3]
                    if kc + 1 < nkc:
                        issue_S(kc + 1, nk + 1)
                    nk += 1
                    mk.op("act", lambda e, pS=pS, p_=p_: e.activation(out=p_[:, 0:tn], in_=pS[:, 0:tn], func=AF.Exp,
                                                                     scale=scale), reads=[pS], writes=[p_])
                    mk.op("pe", lambda e, pO=pO, kc=kc, p_=p_: e.matmul(
                        pO[:, 0:tn], Vs[:, kc, kvh * 128:(kvh + 1) * 128], p_[:, 0:tn],
                        start=(kc == 0), stop=(kc == nkc - 1)), reads=[Vs, p_], writes=[pO])
                    mk.op("pe", lambda e, pZ=pZ, kc=kc, p_=p_: e.matmul(
                        pZ[:, 0:tn], self.ones_b[:], p_[:, 0:tn], start=(kc == 0), stop=(kc == nkc - 1)),
                        reads=[self.ones_b, p_], writes=[pZ])
                r_ = rz[nb % 2]
                o_ = ob[nb % 2]
                mk.op("dve", lambda e, pZ=pZ, r_=r_: e.reciprocal(r_[:, 0:tn], pZ[:, 0:tn]), reads=[pZ], writes=[r_])
                mk.op("dve", lambda e, pO=pO, r_=r_, o_=o_: e.tensor_tensor(out=o_[:, 0:tn], in0=pO[:, 0:tn],
                                                                           in1=r_[:, 0:tn], op=ALU.mult),
                      reads=[pO, r_], writes=[o_])
                mk.dma("act", OT[head * 128:(head + 1) * 128, t0:t0 + tn], o_[:, 0:tn], reads=[o_], writes=[OT])
                nb += 1
        mk.release(m0)
        mk.release(mA)
        m0 = mk.mark()
        wo = self.load_w("wo", I["attn_w_o"][slot], DC, D)
        self.linear_resid(XT, OT, wo, DC, "g1", skip_ctx=not with_ctx)
        mk.release(m0)

    Prog.load_w = load_w
    Prog.linear_resid = linear_resid
    Prog.stage_attn = stage_attn


_attn_methods()


def _conf_methods():
    def stage_conf(self, slot, XT, HT, UT, VT, I):
        mk = self.mk
        m0 = mk.mark()
        W = self.load_w("wpw1", I["conv_w_pw1"][slot], DC, 2 * D)
        b1 = mk.sbuf("cb1", [128, 16], F32)
        mk.dma("sp", b1[:], I["conv_b_pw1_pc"][slot, :, :], writes=[b1])
        hb = [mk.sbuf("ch%d" % k, [128, DC, 512], BF16) for k in range(2)]
        sg = [mk.sbuf("csg%d" % k, [128, 512], F32) for k in range(2)]
        ub = [mk.sbuf("cu%d" % k, [128, DC, 512], BF16) for k in range(2)]
        n = 0
        for bi, (t0, tn) in enumerate(BLOCKS):
            h = hb[bi % 2]
            u = ub[bi % 2]
            mk.dma("sp", h[:, :, 0:tn], HT[:, t0:t0 + tn].rearrange("(k p) t -> p k t", p=128), reads=[HT], writes=[h])
            for j in range(DC):
                pa, pg = self.ps[(2 * n) % 8], self.ps[(2 * n + 1) % 8]
                s_ = sg[n % 2]
                n += 1
                for k in range(DC):
                    mk.op("pe", lambda e, pa=pa, k=k, j=j, h=h: e.matmul(
                        pa[:, 0:tn], W[:, k, j * 128:(j + 1) * 128], h[:, k, 0:tn], start=(k == 0), stop=(k == DC - 1)),
                        reads=[W, h], writes=[pa])
                for k in range(DC):
                    mk.op("pe", lambda e, pg=pg, k=k, j=j, h=h: e.matmul(
                        pg[:, 0:tn], W[:, k, D + j * 128:D + (j + 1) * 128], h[:, k, 0:tn], start=(k == 0), stop=(k == DC - 1)),
                        reads=[W, h], writes=[pg])
                mk.op("act", lambda e, pg=pg, s_=s_, j=j: e.activation(out=s_[:, 0:tn], in_=pg[:, 0:tn], func=AF.Sigmoid,
                                                                      bias=b1[:, 8 + j:9 + j]), reads=[pg, b1], writes=[s_])
                mk.op("dve", lambda e, pa=pa, s_=s_, j=j, u=u: e.scalar_tensor_tensor(
                    out=u[:, j, 0:tn], in0=pa[:, 0:tn], scalar=b1[:, j:j + 1], in1=s_[:, 0:tn], op0=ALU.add, op1=ALU.mult),
                    reads=[pa, b1, s_], writes=[u])
            mk.dma("act", UT[:, t0:t0 + tn].rearrange("(k p) t -> p k t", p=128), u[:, :, 0:tn], reads=[u], writes=[UT])
        mk.release(m0)
        m0 = mk.mark()
        wdw = mk.sbuf("cwdw", [128, DC, 31], F32)
        bdw = mk.sbuf("cbdw", [128, DC], F32)
        mk.dma("sp", wdw[:], I["conv_w_dw_pc"][slot, :, :, :], writes=[wdw])
        mk.dma("sp", bdw[:], I["conv_b_dw_pc"][slot, :, :], writes=[bdw])
        up = [mk.sbuf("cup%d" % k, [128, NT + 60], BF16) for k in range(2)]
        dg = [mk.sbuf("cdg%d" % k, [128, 31, 128], BF16) for k in range(2)]
        vo = [mk.sbuf("cvo%d" % k, [128, 512], F32) for k in range(3)]
        for k in range(2):
            mk.op("pool", lambda e, k=k: e.memset(up[k][:], 0.0), writes=[up[k]])
        n = 0
        for j in range(DC):
            u = up[j % 2]
            d_ = dg[j % 2]
            mk.dma("sp", u[:, 15:15 + CTX], UT[j * 128:(j + 1) * 128, 0:CTX], reads=[UT], writes=[u])
            mk.dma("sp", u[:, 45 + CTX:45 + CTX + SEQ], UT[j * 128:(j + 1) * 128, CTX:NT], reads=[UT], writes=[u])
            for k in range(31):
                mk.op("dve", lambda e, k=k, j=j, d_=d_: e.tensor_scalar(
                    out=d_[:, k, :], in0=self.ident_b[:], scalar1=wdw[:, j, k:k + 1], scalar2=None, op0=ALU.mult),
                    reads=[self.ident_b, wdw], writes=[d_])
            for bi, (t0, tn) in enumerate(BLOCKS):
                base = 0 if bi == 0 else 30
                ps = self.ps[n % 4]
                v = vo[n % 3]
                n += 1
                for k in range(31):
                    mk.op("pe", lambda e, ps=ps, k=k, u=u, d_=d_, s0=base + t0 + k: e.matmul(
                        ps[:, 0:tn], d_[:, k, :], u[:, s0:s0 + tn], start=(k == 0), stop=(k == 30)),
                        reads=[d_, u], writes=[ps])
                mk.op("act", lambda e, ps=ps, v=v, j=j: e.activation(out=v[:, 0:tn], in_=ps[:, 0:tn], func=AF.Identity,
                                                                    bias=bdw[:, j:j + 1]), reads=[ps, bdw], writes=[v])
                mk.dma("act", VT[j * 128:(j + 1) * 128, t0:t0 + tn], v[:, 0:tn], reads=[v], writes=[VT])
        mk.release(m0)
        m0 = mk.mark()
        lng = mk.sbuf("clng", [128, DC], F32)
        lnb = mk.sbuf("clnb", [128, DC], F32)
        mk.dma("sp", lng[:], I["conv_ln_g_pc"][slot, :, :], writes=[lng])
        mk.dma("sp", lnb[:], I["conv_ln_b_pc"][slot, :, :], writes=[lnb])
        avgf = mk.sbuf("cavgf", [128, 128], F32)
        mk.op("pool", lambda e: e.memset(avgf[:], 1.0 / D), writes=[avgf])
        vb = [mk.sbuf("cv%d" % k, [128, DC, 512], F32) for k in range(2)]
        sq = mk.sbuf("csq", [128, DC, 512], F32)
        mu = mk.sbuf("cmu", [128, 512], F32)
        var = mk.sbuf("cvar", [128, 512], F32)
        ob = [mk.sbuf("co%d" % k, [128, DC, 512], BF16) for k in range(2)]
        for bi, (t0, tn) in enumerate(BLOCKS):
            v = vb[bi % 2]
            o = ob[bi % 2]
            mk.dma("sp", v[:, :, 0:tn], VT[:, t0:t0 + tn].rearrange("(k p) t -> p k t", p=128), reads=[VT], writes=[v])
            pm, pq = self.ps[0], self.ps[1]
            for j in range(DC):
                mk.op("act", lambda e, j=j, v=v: e.activation(out=sq[:, j, 0:tn], in_=v[:, j, 0:tn], func=AF.Square),
                      reads=[v], writes=[sq])
            for j in range(DC):
                mk.op("pe", lambda e, j=j, v=v: e.matmul(pm[:, 0:tn], avgf[:], v[:, j, 0:tn], start=(j == 0), stop=(j == DC - 1)),
                      reads=[avgf, v], writes=[pm])
            for j in range(DC):
                mk.op("pe", lambda e, j=j: e.matmul(pq[:, 0:tn], avgf[:], sq[:, j, 0:tn], start=(j == 0), stop=(j == DC - 1)),
                      reads=[avgf, sq], writes=[pq])
            mk.op("act", lambda e: e.copy(mu[:, 0:tn], pm[:, 0:tn]), reads=[pm], writes=[mu])
            mk.op("dve", lambda e: e.tensor_tensor(out=var[:, 0:tn], in0=mu[:, 0:tn], in1=mu[:, 0:tn], op=ALU.mult),
                  reads=[mu], writes=[var])
            mk.op("dve", lambda e: e.tensor_tensor(out=var[:, 0:tn], in0=pq[:, 0:tn], in1=var[:, 0:tn], op=ALU.subtract),
                  reads=[pq, var], writes=[var])
            mk.op("act", lambda e: e.activation(out=var[:, 0:tn], in_=var[:, 0:tn], func=AF.Sqrt, bias=self.eps_t[:, 0:1]),
                  reads=[var, self.eps_t], writes=[var])
            mk.op("dve", lambda e: e.reciprocal(var[:, 0:tn], var[:, 0:tn]), reads=[var], writes=[var])
            for j in range(DC):
                mk.op("pool", lambda e, j=j, v=v: e.tensor_tensor(out=v[:, j, 0:tn], in0=v[:, j, 0:tn], in1=mu[:, 0:tn],
                                                                 op=ALU.subtract), reads=[v, mu], writes=[v])
                mk.op("dve", lambda e, j=j, v=v: e.scalar_tensor_tensor(
                    out=v[:, j, 0:tn], in0=v[:, j, 0:tn], scalar=lng[:, j:j + 1], in1=var[:, 0:tn], op0=ALU.mult, op1=ALU.mult),
                    reads=[v, lng, var], writes=[v])
                mk.op("act", lambda e, j=j, v=v, o=o: e.activation(out=o[:, j, 0:tn], in_=v[:, j, 0:tn], func=AF.Silu,
                                                                  bias=lnb[:, j:j + 1]), reads=[v, lnb], writes=[o])
            mk.dma("act", HT[:, t0:t0 + tn].rearrange("(k p) t -> p k t", p=128), o[:, :, 0:tn], reads=[o], writes=[HT])
        mk.release(m0)
        m0 = mk.mark()
        w2 = self.load_w("wpw2", I["conv_w_pw2"][slot], DC, D)
        b2 = mk.sbuf("cb2", [128, DC], F32)
        mk.dma("sp", b2[:], I["conv_b_pw2_pc"][slot, :, :], writes=[b2])
        self.linear_resid(XT, HT, w2, DC, "g1", bias=b2)
        mk.release(m0)

    Prog.stage_conf = stage_conf


_conf_methods()


def hy_consts(L):
    N = 2 * L
    N1 = N // 64
    H = N1 // 2
    bf = ml_dtypes.bfloat16
    c = {}
    n1 = np.arange(H)[:, None].astype(np.float64)
    k1 = np.arange(N1)[None, :].astype(np.float64)
    def cs(ang):
        return np.cos(ang), -np.sin(ang)
    fc, fs = cs(2 * np.pi * n1 * k1 / N1)
    bc, bs = cs(2 * np.pi * (N1 - 1 - n1) * k1 / N1)
    b0c, b0s = cs(2 * np.pi * (N1 - n1) * k1 / N1)
    c["F1"] = np.stack([fc, fs, bc, bs, b0c, b0s], 1).astype(bf)
    n2 = np.arange(64)[:, None].astype(np.float64)
    k2 = np.arange(64)[None, :].astype(np.float64)
    G = np.zeros((N1, 128, 5, 128), np.float64)
    for kk in range(N1):
        ang = 2 * np.pi * (n2 * kk / N + n2 * k2 / 64)
        Gr, Gi = np.cos(ang), -np.sin(ang)
        Mr, Mi = Gr.T, -Gi.T
        blocks = [((Gr, Gi), (-Gi, Gr)), ((-Gi, Gr), (-Gr, -Gi)), ((Gr, Gr), (-Gi, -Gi)),
                  ((Gi, Gi), (Gr, Gr)), ((Mr, Mi), (-Mi, Mr))]
        for f, ((a, b), (cc, d)) in enumerate(blocks):
            G[kk, 0:64, f, 0:64] = a
            G[kk, 0:64, f, 64:128] = b
            G[kk, 64:128, f, 0:64] = cc
            G[kk, 64:128, f, 64:128] = d
    c["G"] = np.ascontiguousarray(G.transpose(1, 0, 2, 3)).astype(bf)
    t1 = np.arange(H)[None, :].astype(np.float64)
    kk1 = np.arange(N1)[:, None].astype(np.float64)
    th = 2 * np.pi * t1 * kk1 / N1
    c["Fi"] = np.stack([np.cos(th) / N, -np.sin(th) / N], 1).astype(bf)
    pos = (64 * np.arange(H)[:, None] + np.arange(64)[None, :]).astype(np.float32)
    c["lneg"] = (-(pos / np.float32(L - 1))).astype(np.float32)
    p = np.arange(L, dtype=np.float32)[:, None]
    t = p / np.float32(L - 1)
    w = np.float32(2.0 * math.pi) * p / np.float32(L)
    bands = np.linspace(1e-4, 7, 8, dtype=np.float32)
    feats = np.concatenate([t, np.cos(bands * w), -np.sin(bands * w)], axis=-1).astype(np.float32)
    c["featsT"] = np.ascontiguousarray(feats.T)
    return c


def _hy_methods():
    def hy_filter(self, S, I, HH):
        mk = self.mk
        L, N1, H, tag = S["L"], S["N1"], S["H"], S["tag"]
        m0 = mk.mark()
        ft = mk.sbuf("hf_ft", [17, L], F32)
        W1 = mk.sbuf("hf_w1", [17, 64], F32)
        W2 = mk.sbuf("hf_w2", [64, 64], F32)
        W3 = mk.sbuf("hf_w3", [64, 4096], BF16)
        fv = mk.sbuf("hf_fv", [64, 4], F32)
        fb = mk.sbuf("hf_fb", [64, 2], F32)
        z1 = mk.sbuf("hf_z1", [64, L], F32)
        z2 = mk.sbuf("hf_z2", [64, L], F32)
        z2b = mk.sbuf("hf_z2b", [64, L], BF16)
        tmp = mk.sbuf("hf_tmp", [64, 512], F32)
        F1 = mk.sbuf("hf_F1", [max(H, 1), 6, N1], BF16)
        lneg = mk.sbuf("hf_lneg", [H, 64], F32)
        dab = mk.sbuf("hf_dab", [H, D], F32)
        mk.dma("sp", ft[:], I["hy_featsT_" + tag][:, :], writes=[ft])
        mk.dma("sp", W1[:], I["hy_f_w1"][0, :, :], writes=[W1])
        mk.dma("sp", W2[:], I["hy_f_w2"][0, :, :], writes=[W2])
        mk.dma("pool", W3[:], I["hy_f_w3"][0, :, :], writes=[W3])
        mk.dma("sp", fv[:], I["hy_fvec"][:, :], writes=[fv])
        mk.dma("sp", F1[:], I["hy_F1_" + tag][:, :, :], writes=[F1])
        mk.dma("sp", lneg[:], I["hy_lneg_" + tag][:, :], writes=[lneg])
        mk.dma("sp", dab[:], I["hy_dabs"][0:H, :], writes=[dab])
        mk.op("dve", lambda e: e.tensor_tensor(out=fb[:, 0:1], in0=fv[:, 0:1], in1=fv[:, 1:2], op=ALU.mult), reads=[fv], writes=[fb])
        mk.op("dve", lambda e: e.tensor_tensor(out=fb[:, 1:2], in0=fv[:, 2:3], in1=fv[:, 3:4], op=ALU.mult), reads=[fv], writes=[fb])
        for layer, (Wm, src, dst, kin) in enumerate(((W1, ft, z1, 17), (W2, z1, z2, 64))):
            for c0 in range(0, L, 512):
                cn = min(512, L - c0)
                ps = self.ps[(c0 // 512) % 2]
                mk.op("pe", lambda e, ps=ps, Wm=Wm, src=src, c0=c0, cn=cn, kin=kin: e.matmul(
                    ps[0:64, 0:cn], Wm[0:kin, :], src[0:kin, c0:c0 + cn], start=True, stop=True), reads=[Wm, src], writes=[ps])
                d_ = dst[:, c0:c0 + cn]
                mk.op("dve", lambda e, ps=ps, d_=d_, cn=cn, layer=layer: e.tensor_scalar(
                    out=d_, in0=ps[0:64, 0:cn], scalar1=fv[:, 2 * layer + 1:2 * layer + 2], scalar2=fb[:, layer:layer + 1],
                    op0=ALU.mult, op1=ALU.add), reads=[ps, fv, fb], writes=[dst])
                for _ in range(2):
                    mk.op("dve", lambda e, d_=d_, cn=cn: e.tensor_scalar(out=tmp[:, 0:cn], in0=d_, scalar1=math.pi,
                          scalar2=2 * math.pi, op0=ALU.is_gt, op1=ALU.mult), reads=[dst], writes=[tmp])
                    mk.op("dve", lambda e, d_=d_, cn=cn: e.tensor_tensor(out=d_, in0=d_, in1=tmp[:, 0:cn], op=ALU.subtract),
                          reads=[dst, tmp], writes=[dst])
                    mk.op("dve", lambda e, d_=d_, cn=cn: e.tensor_scalar(out=tmp[:, 0:cn], in0=d_, scalar1=-math.pi,
                          scalar2=2 * math.pi, op0=ALU.is_lt, op1=ALU.mult), reads=[dst], writes=[tmp])
                    mk.op("dve", lambda e, d_=d_, cn=cn: e.tensor_tensor(out=d_, in0=d_, in1=tmp[:, 0:cn], op=ALU.add),
                          reads=[dst, tmp], writes=[dst])
                mk.op("act", lambda e, d_=d_: e.activation(out=d_, in_=d_, func=AF.Sin), reads=[dst], writes=[dst])
        mk.op("act", lambda e: e.copy(z2b[:], z2[:]), reads=[z2], writes=[z2b])
        dec = [mk.sbuf("hf_dec%d" % k, [H, D], F32) for k in range(4)]
        hd = [mk.sbuf("hf_hd%d" % k, [H, D], F32) for k in range(4)]
        hdb = [mk.sbuf("hf_hdb%d" % k, [H, D], BF16) for k in range(4)]
        ha = [mk.sbuf("hf_ha%d" % k, [H, D], BF16) for k in range(4)]
        Asb = [mk.sbuf("hf_A%d" % k, [N1, 2, D], BF16) for k in range(2)]
        rn = mk.sbuf("hf_rn", [128, D], F32)
        A = self.dram["hyA"]
        Bt = [mk.sbuf("hf_B%d" % k, [128, D], BF16) for k in range(2)]
        Gt = [mk.sbuf("hf_Gt%d" % k, [128, 2, 128], BF16) for k in range(2)]
        Ho = [mk.sbuf("hf_Ho%d" % k, [128, D], BF16) for k in range(4)]
        for o in range(2):
            pn = (self.ps[6], self.ps[7])
            units = [(n2_, dr_) for n2_ in range(64) for dr_ in range(2)]

            def issue_h(ui):
                n2_, dr_ = units[ui]
                col_ = n2_ if dr_ == 0 else (64 - n2_) % 64
                for hf in range(2):
                    ph = self.ps[(ui % 2) * 2 + hf]
                    w0 = (dr_ * 2 + o) * D + hf * 512
                    mk.op("pe", lambda e, ph=ph, col_=col_, w0=w0: e.matmul(
                        ph[0:H, :], z2b[:, col_:L:64], W3[:, w0:w0 + 512], start=True, stop=True),
                        reads=[z2b, W3], writes=[ph])
                i2_ = dr_ + 2 * (n2_ % 2)
                mk.op("act", lambda e, i2_=i2_, col_=col_: e.activation(out=dec[i2_][:], in_=dab[:], func=AF.Exp,
                                                                       scale=lneg[:, col_:col_ + 1]), reads=[dab, lneg], writes=[dec[i2_]])
            issue_h(0)
            for n2 in range(64):
                n2b = (64 - n2) % 64
                hb2 = []
                for dr in range(2):
                    ui = n2 * 2 + dr
                    col = n2 if dr == 0 else n2b
                    i2 = dr + 2 * (n2 % 2)
                    if ui + 1 < len(units):
                        issue_h(ui + 1)
                    for hf in range(2):
                        ph = self.ps[(ui % 2) * 2 + hf]
                        mk.op("dve", lambda e, ph=ph, i2=i2, hf=hf: e.tensor_tensor(
                            out=hd[i2][:, hf * 512:(hf + 1) * 512], in0=ph[0:H, :], in1=dec[i2][:, hf * 512:(hf + 1) * 512],
                            op=ALU.mult), reads=[ph, dec[i2]], writes=[hd[i2]])
                    if dr == 1 and n2 == 0:
                        mk.op("dve", lambda e, i2=i2: e.memset(hd[i2][0:1, :], 0.0), reads=[hd[i2]], writes=[hd[i2]])
                    hb_ = hdb[(n2 % 2) * 2 + dr]
                    hb2.append(hb_)
                    mk.op("act", lambda e, i2=i2, hb_=hb_: e.copy(hb_[:], hd[i2][:]), reads=[hd[i2]], writes=[hb_])
                    mk.op("dve", lambda e, i2=i2: e.scalar_tensor_tensor(out=ha[i2][:], in0=hd[i2][:], scalar=-1.0, in1=hd[i2][:],
                                                                        op0=ALU.mult, op1=ALU.max), reads=[hd[i2]], writes=[ha[i2]])
                    for hf in range(2):
                        first = (n2 == 0 and dr == 0)
                        last = (n2 == 63 and dr == 1)
                        mk.op("pe", lambda e, i2=i2, hf=hf, first=first, last=last: e.matmul(
                            pn[hf][:, :], self.ones_b[0:H, :], ha[i2][:, hf * 512:(hf + 1) * 512], start=first, stop=last),
                            reads=[self.ones_b, ha[i2]], writes=[pn[hf]])
                As = Asb[n2 % 2]
                fbi = 4 if n2 == 0 else 2
                for ri in range(2):
                    for hf in range(2):
                        pA = self.ps[4 + hf]
                        mk.op("pe", lambda e, pA=pA, ri=ri, hf=hf, hb2=hb2: e.matmul(
                            pA[0:N1, :], F1[0:H, ri, :], hb2[0][:, hf * 512:(hf + 1) * 512], start=True, stop=False),
                            reads=[F1, hb2[0]], writes=[pA])
                        mk.op("pe", lambda e, pA=pA, ri=ri, hf=hf, hb2=hb2, fbi=fbi: e.matmul(
                            pA[0:N1, :], F1[0:H, fbi + ri, :], hb2[1][:, hf * 512:(hf + 1) * 512], start=False, stop=True),
                            reads=[F1, hb2[1]], writes=[pA])
                        eng = "act" if hf else "dve"
                        if eng == "act":
                            mk.op("act", lambda e, pA=pA, ri=ri, hf=hf, As=As: e.copy(As[:, ri, hf * 512:(hf + 1) * 512], pA[0:N1, :]),
                                  reads=[pA], writes=[As])
                        else:
                            mk.op("dve", lambda e, pA=pA, ri=ri, hf=hf, As=As: e.tensor_copy(As[:, ri, hf * 512:(hf + 1) * 512], pA[0:N1, :]),
                                  reads=[pA], writes=[As])
                for ri in range(2):
                    mk.dma("act", A[ri, 0:N1, n2, :], As[:, ri, :], reads=[As], writes=[A])
            for hf in range(2):
                mk.op("dve", lambda e, hf=hf: e.reciprocal(rn[:, hf * 512:(hf + 1) * 512], pn[hf][:, :]), reads=[pn[hf]], writes=[rn])
            for kk in range(N1):
                B = Bt[kk % 2]
                for ri in range(2):
                    mk.dma("sp", B[ri * 64:(ri + 1) * 64, :], A[ri, kk, :, :], reads=[A], writes=[B])
                g_ = Gt[kk % 2]
                mk.dma("sp", g_[:], I["hy_G_" + tag][:, kk, 2:4, :], writes=[g_])
                for ri in range(2):
                    ho = Ho[(2 * kk + ri) % 4]
                    for hf in range(2):
                        pX = self.ps[(kk % 2) * 4 + ri * 2 + hf]
                        mk.op("pe", lambda e, pX=pX, ri=ri, hf=hf, B=B, g_=g_: e.matmul(
                            pX[:, :], g_[:, ri, :], B[:, hf * 512:(hf + 1) * 512], start=True, stop=True), reads=[g_, B], writes=[pX])
                        mk.op("dve", lambda e, pX=pX, hf=hf, ho=ho: e.tensor_tensor(
                            out=ho[:, hf * 512:(hf + 1) * 512], in0=pX[:, :], in1=rn[:, hf * 512:(hf + 1) * 512], op=ALU.mult),
                            reads=[pX, rn], writes=[ho])
                    mk.dma("act", HH[o][ri, kk, :, :], ho[:], reads=[ho], writes=[HH[o]])
        mk.release(m0)

    Prog.hy_filter = hy_filter


_hy_methods()


def _hy_methods2():
    def hy_conv(self, S, o, I, zsrc, zrow0, gate, grow0, lbias, HH, dst, dst_bf16):
        mk = self.mk
        L, N1, H, tag, col0 = S["L"], S["N1"], S["H"], S["tag"], S["col0"]
        A, Cs = self.dram["hyA"], self.dram["hyC"]
        m0 = mk.mark()
        zb = mk.sbuf("hc_zb", [128, DC, L], BF16)
        F1 = mk.sbuf("hc_F1", [max(H, 1), 6, N1], BF16)
        mk.dma("sp", F1[:], I["hy_F1_" + tag][:, :, :], writes=[F1])
        for j in range(DC):
            mk.dma("pool", zb[:, j, :], zsrc[zrow0 + j * 128:zrow0 + (j + 1) * 128, col0:col0 + L], reads=[zsrc], writes=[zb])
        zT = [mk.sbuf("hc_zT%d" % k, [H, D], BF16) for k in range(2)]
        Asb = [mk.sbuf("hc_A%d" % k, [N1, 2, D], BF16) for k in range(2)]
        for n2 in range(64):
            pt = self.ps[n2 % 2]
            ptb = pt[:, 0:512].bitcast(BF16)
            for j in range(DC):
                mk.op("pe", lambda e, ptb=ptb, j=j, n2=n2: e.transpose(ptb[0:H, j * 128:(j + 1) * 128], zb[:, j, n2:L:64],
                                                                      self.ident_b[:]), reads=[zb, self.ident_b], writes=[pt])
            z_ = zT[n2 % 2]
            mk.op("act", lambda e, ptb=ptb, z_=z_: e.copy(z_[:], ptb[0:H, :]), reads=[pt], writes=[z_])
            As = Asb[n2 % 2]
            for ri in range(2):
                for hf in range(2):
                    pA = self.ps[2 + ri * 2 + hf]
                    mk.op("pe", lambda e, pA=pA, ri=ri, hf=hf, z_=z_: e.matmul(
                        pA[0:N1, :], F1[0:H, ri, :], z_[:, hf * 512:(hf + 1) * 512], start=True, stop=True),
                        reads=[F1, z_], writes=[pA])
                    if hf:
                        mk.op("act", lambda e, pA=pA, ri=ri, hf=hf, As=As: e.copy(As[:, ri, hf * 512:(hf + 1) * 512], pA[0:N1, :]),
                              reads=[pA], writes=[As])
                    else:
                        mk.op("dve", lambda e, pA=pA, ri=ri, hf=hf, As=As: e.tensor_copy(As[:, ri, hf * 512:(hf + 1) * 512], pA[0:N1, :]),
                              reads=[pA], writes=[As])
            for ri in range(2):
                mk.dma("act", A[ri, 0:N1, n2, :], As[:, ri, :], reads=[As], writes=[A])
        mk.release(m0)
        m0 = mk.mark()
        Bt = [mk.sbuf("hc_B%d" % k, [128, D], BF16) for k in range(2)]
        Gt = [mk.sbuf("hc_G%d" % k, [128, 5, 128], BF16) for k in range(2)]
        Hr = [mk.sbuf("hc_Hr%d" % k, [128, D], BF16) for k in range(2)]
        Hi = [mk.sbuf("hc_Hi%d" % k, [128, D], BF16) for k in range(2)]
        ta = [mk.sbuf("hc_ta%d" % k, [128, D], F32) for k in range(2)]
        tb = [mk.sbuf("hc_tb%d" % k, [128, D], F32) for k in range(2)]
        Y = [mk.sbuf("hc_Y%d" % k, [128, D], BF16) for k in range(2)]
        Cb = [mk.sbuf("hc_C%d" % k, [128, D], BF16) for k in range(2)]
        def f2_load(kk):
            i2 = kk % 2
            B, g_, hr, hi = Bt[i2], Gt[i2], Hr[i2], Hi[i2]
            for ri in range(2):
                mk.dma("sp", B[ri * 64:(ri + 1) * 64, :], A[ri, kk, :, :], reads=[A], writes=[B])
            mk.dma("sp", g_[:], I["hy_G_" + tag][:, kk, :, :], writes=[g_])
            mk.dma("sp", hr[:], HH[o][0, kk, :, :], reads=[HH[o]], writes=[hr])
            mk.dma("sp", hi[:], HH[o][1, kk, :, :], reads=[HH[o]], writes=[hi])

        def f2_X(u):
            kk, hf = divmod(u, 2)
            i2 = kk % 2
            sl = slice(hf * 512, (hf + 1) * 512)
            pa, pb = self.ps[(u % 2) * 2], self.ps[(u % 2) * 2 + 1]
            mk.op("pe", lambda e: e.matmul(pa[:, :], Gt[i2][:, 0, :], Bt[i2][:, sl], start=True, stop=True),
                  reads=[Gt[i2], Bt[i2]], writes=[pa])
            mk.op("pe", lambda e: e.matmul(pb[:, :], Gt[i2][:, 1, :], Bt[i2][:, sl], start=True, stop=True),
                  reads=[Gt[i2], Bt[i2]], writes=[pb])
        f2_load(0)
        if N1 > 1:
            f2_load(1)
        f2_X(0)
        for u in range(2 * N1):
            kk, hf = divmod(u, 2)
            i2 = kk % 2
            hr, hi = Hr[i2], Hi[i2]
            sl = slice(hf * 512, (hf + 1) * 512)
            pa, pb = self.ps[(u % 2) * 2], self.ps[(u % 2) * 2 + 1]
            if u + 1 < 2 * N1:
                f2_X(u + 1)
            mk.op("dve", lambda e, pa=pa, sl=sl, hr=hr, i2=i2: e.tensor_tensor(out=ta[i2][:, sl], in0=pa[:, :], in1=hr[:, sl],
                                                                              op=ALU.mult), reads=[pa, hr], writes=[ta[i2]])
            mk.op("dve", lambda e, pb=pb, sl=sl, hi=hi, i2=i2: e.tensor_tensor(out=tb[i2][:, sl], in0=pb[:, :], in1=hi[:, sl],
                                                                              op=ALU.mult), reads=[pb, hi], writes=[tb[i2]])
            mk.op("pool", lambda e, sl=sl, i2=i2: e.tensor_tensor(out=Y[i2][:, sl], in0=ta[i2][:, sl], in1=tb[i2][:, sl],
                                                                 op=ALU.add), reads=[ta[i2], tb[i2]], writes=[Y[i2]])
            pc = self.ps[4 + u % 4]
            mk.op("pe", lambda e, pc=pc, sl=sl, i2=i2: e.matmul(pc[:, :], Gt[i2][:, 4, :], Y[i2][:, sl], start=True, stop=True),
                  reads=[Gt[i2], Y[i2]], writes=[pc])
            mk.op("act", lambda e, pc=pc, sl=sl, i2=i2: e.copy(Cb[i2][:, sl], pc[:, :]), reads=[pc], writes=[Cb[i2]])
            if hf == 1:
                for ri in range(2):
                    mk.dma("act", Cs[ri, :, kk, :], Cb[i2][ri * 64:(ri + 1) * 64, :], reads=[Cb[i2]], writes=[Cs])
                if kk + 2 < N1:
                    f2_load(kk + 2)
        mk.release(m0)
        m0 = mk.mark()
        Fi = mk.sbuf("hc_Fi", [N1, 2, max(H, 1)], BF16)
        mk.dma("sp", Fi[:], I["hy_Fi_" + tag][:, :, :], writes=[Fi])
        lb = mk.sbuf("hc_lb", [128, DC], F32)
        mk.dma("sp", lb[:], lbias, writes=[lb])
        yh = mk.sbuf("hc_yh", [128, 4, L], F32)
        Ct = [mk.sbuf("hc_Ct%d" % k, [N1, 2, 512], BF16) for k in range(3)]
        zr = [mk.sbuf("hc_zr%d" % k, [128, L], F32) for k in range(2)]
        gr = [mk.sbuf("hc_gr%d" % k, [128, L], F32) for k in range(2)]
        ob = [mk.sbuf("hc_ob%d" % k, [128, L], BF16 if dst_bf16 else F32) for k in range(2)]
        per_bank = max(1, 512 // (4 * H))
        for half in range(2):
            for t2 in range(64):
                c_ = Ct[t2 % 3]
                mk.dma("sp", c_[:], Cs[:, t2, 0:N1, half * 512:(half + 1) * 512].rearrange("r k c -> k r c"), reads=[Cs], writes=[c_])
                slot = t2 % per_bank
                py = self.ps[(t2 // per_bank) % 8]
                for c4 in range(4):
                    o0 = slot * 4 * H + c4 * H
                    mk.op("pe", lambda e, py=py, o0=o0, c4=c4, c_=c_: e.matmul(
                        py[:, o0:o0 + H], c_[:, 0, c4 * 128:(c4 + 1) * 128], Fi[:, 0, :], start=True, stop=False),
                        reads=[c_, Fi], writes=[py])
                    mk.op("pe", lambda e, py=py, o0=o0, c4=c4, c_=c_: e.matmul(
                        py[:, o0:o0 + H], c_[:, 1, c4 * 128:(c4 + 1) * 128], Fi[:, 1, :], start=False, stop=True),
                        reads=[c_, Fi], writes=[py])
                src = py[:, slot * 4 * H:(slot + 1) * 4 * H].rearrange("p (a b) -> p a b", b=H)
                if t2 % 2:
                    mk.op("act", lambda e, src=src, t2=t2: e.copy(yh[:, :, t2:L:64], src), reads=[py], writes=[yh])
                else:
                    mk.op("dve", lambda e, src=src, t2=t2: e.tensor_copy(yh[:, :, t2:L:64], src), reads=[py], writes=[yh])
            for c4 in range(4):
                cj = half * 4 + c4
                z_, g_, o_ = zr[c4 % 2], gr[c4 % 2], ob[c4 % 2]
                mk.dma("sp", z_[:], zsrc[zrow0 + cj * 128:zrow0 + (cj + 1) * 128, col0:col0 + L], reads=[zsrc], writes=[z_])
                mk.dma("sp", g_[:], gate[grow0 + cj * 128:grow0 + (cj + 1) * 128, col0:col0 + L], reads=[gate], writes=[g_])
                mk.op("dve", lambda e, z_=z_, cj=cj, c4=c4: e.scalar_tensor_tensor(
                    out=z_[:], in0=z_[:], scalar=lb[:, cj:cj + 1], in1=yh[:, c4, :], op0=ALU.mult, op1=ALU.add),
                    reads=[z_, lb, yh], writes=[z_])
                mk.op("pool", lambda e, z_=z_, g_=g_, o_=o_: e.tensor_tensor(out=o_[:], in0=z_[:], in1=g_[:], op=ALU.mult),
                      reads=[z_, g_], writes=[o_])
                mk.dma("act", dst[cj * 128:(cj + 1) * 128, col0:col0 + L], o_[:], reads=[o_], writes=[dst])
        mk.release(m0)

    def stage_hyena(self, slot, XT, HT, I):
        mk = self.mk
        U0, ZT, Z1 = self.dram["hyU0"], self.dram["hyZ"], self.dram["hyZ1"]
        m0 = mk.mark()
        W = self.load_w("hy_win", I["hy_w_in"][slot], DC, 3 * D)
        bi_ = mk.sbuf("hy_bin", [128, 24], F32)
        mk.dma("sp", bi_[:], I["hy_b_in_pc"][:, :], writes=[bi_])
        hb = [mk.sbuf("hy_h%d" % k, [128, DC, 512], BF16) for k in range(2)]
        uo = [mk.sbuf("hy_uo%d" % k, [128, 512], F32) for k in range(3)]
        n = 0
        for bi, (t0, tn) in enumerate(BLOCKS):
            h = hb[bi % 2]
            mk.dma("sp", h[:, :, 0:tn], HT[:, t0:t0 + tn].rearrange("(k p) t -> p k t", p=128), reads=[HT], writes=[h])
            for m in range(24):
                ps = self.ps[n % 4]
                u = uo[n % 3]
                n += 1
                for k in range(DC):
                    mk.op("pe", lambda e, ps=ps, k=k, m=m, h=h: e.matmul(
                        ps[:, 0:tn], W[:, k, m * 128:(m + 1) * 128], h[:, k, 0:tn], start=(k == 0), stop=(k == DC - 1)),
                        reads=[W, h], writes=[ps])
                mk.op("act", lambda e, ps=ps, u=u, m=m: e.activation(out=u[:, 0:tn], in_=ps[:, 0:tn], func=AF.Identity,
                                                                    bias=bi_[:, m:m + 1]), reads=[ps, bi_], writes=[u])
                mk.dma("act", U0[m * 128:(m + 1) * 128, t0:t0 + tn], u[:, 0:tn], reads=[u], writes=[U0])
        mk.release(m0)
        m0 = mk.mark()
        ws = mk.sbuf("hy_ws", [128, 24, 3], F32)
        bs = mk.sbuf("hy_bs", [128, 24], F32)
        mk.dma("sp", ws[:], I["hy_w_short_pc"][:, :, :], writes=[ws])
        mk.dma("sp", bs[:], I["hy_b_short_pc"][:, :], writes=[bs])
        W_ = NT + 4
        ub = [mk.sbuf("hy_ub%d" % k, [128, W_], F32) for k in range(2)]
        vb = [mk.sbuf("hy_vb%d" % k, [128, W_], F32) for k in range(2)]
        for k in range(2):
            mk.op("pool", lambda e, k=k: e.memset(ub[k][:], 0.0), writes=[ub[k]])
        for m in range(24):
            u, v = ub[m % 2], vb[m % 2]
            mk.dma("sp", u[:, 1:1 + CTX], U0[m * 128:(m + 1) * 128, 0:CTX], reads=[U0], writes=[u])
            mk.dma("sp", u[:, 3 + CTX:3 + NT], U0[m * 128:(m + 1) * 128, CTX:NT], reads=[U0], writes=[u])
            n_ = W_ - 2
            mk.op("dve", lambda e, u=u, v=v, m=m: e.tensor_scalar(out=v[:, 1:1 + n_], in0=u[:, 0:n_], scalar1=ws[:, m, 0:1],
                                                                 scalar2=bs[:, m:m + 1], op0=ALU.mult, op1=ALU.add),
                  reads=[u, ws, bs], writes=[v])
            mk.op("dve", lambda e, u=u, v=v, m=m: e.scalar_tensor_tensor(out=v[:, 1:1 + n_], in0=u[:, 1:1 + n_], scalar=ws[:, m, 1:2],
                                                                        in1=v[:, 1:1 + n_], op0=ALU.mult, op1=ALU.add),
                  reads=[u, ws, v], writes=[v])
            mk.op("dve", lambda e, u=u, v=v, m=m: e.scalar_tensor_tensor(out=v[:, 1:1 + n_], in0=u[:, 2:2 + n_], scalar=ws[:, m, 2:3],
                                                                        in1=v[:, 1:1 + n_], op0=ALU.mult, op1=ALU.add),
                  reads=[u, ws, v], writes=[v])
            mk.dma("act", ZT[m * 128:(m + 1) * 128, 0:CTX], v[:, 1:1 + CTX], reads=[v], writes=[ZT])
            mk.dma("act", ZT[m * 128:(m + 1) * 128, CTX:NT], v[:, 3 + CTX:3 + NT], reads=[v], writes=[ZT])
        mk.release(m0)
        seqs = [dict(L=SEQ, N1=128, H=64, tag="lat", col0=CTX)]
        if not self.cfg.get("ctx_direct", True):
            seqs.append(dict(L=CTX, N1=8, H=4, tag="ctx", col0=0))
        for S in seqs:
            HH = [self.dram["hyHH_%s%d" % (S["tag"], o)] for o in range(2)]
            self.hy_filter(S, I, HH)
            self.hy_conv(S, 0, I, ZT, 0, ZT, D, I["hy_lbias_pc"][0, :, :], HH, Z1, False)
            self.hy_conv(S, 1, I, Z1, 0, ZT, 2 * D, I["hy_lbias_pc"][1, :, :], HH, HT, True)
        if self.cfg.get("ctx_direct", True):
            self.hy_ctx_direct(I)
        m0 = mk.mark()
        wo = self.load_w("hy_wout", I["hy_w_out"][slot], DC, D)
        bo = mk.sbuf("hy_bo", [128, DC], F32)
        mk.dma("sp", bo[:], I["hy_b_out_pc"][:, :], writes=[bo])
        self.linear_resid(XT, HT, wo, DC, "g1", bias=bo)
        mk.release(m0)

    Prog.hy_conv = hy_conv
    Prog.stage_hyena = stage_hyena


_hy_methods2()


_PROG = {}


def kernel(**inputs):
    inp = {k: np.asarray(v) for k, v in inputs.items()}
    if "p" not in _PROG:
        _PROG["p"] = build_program()
    p = _PROG["p"]
    shared = None
    in_maps = []
    for b in range(8):
        m = host_inputs(inp, b)
        if shared is None:
            shared = m
        else:
            for k in m:
                if k not in ("x", "ctx", "c_pc"):
                    m[k] = shared[k]
        in_maps.append(m)
    res = run_bass_kernel_spmd(p.nc, in_maps, core_ids=list(range(8)))
    out = np.stack([np.asarray(r["y"], dtype=np.float32) for r in res.results], axis=0)
    return out


BSL = 256
BSH = 8
NBLK = 66
NSLOT = NBLK * BSL
NTILE = NT // 128
IOA = bass.IndirectOffsetOnAxis


def _sparse_methods():
    def indirect(mk, out, out_off, in_, in_off, reads, writes):
        qlane = mk.lanes["pool"]
        dl = mk.dma_lanes[mk.dma_rr]
        mk.dma_rr = (mk.dma_rr + 1) % len(mk.dma_lanes)
        need, _ = mk._deps(dl, reads, writes)
        if dl.count > 0:
            need[dl.name] = max(need.get(dl.name, 0), dl.count)
        for ln, ix in need.items():
            if qlane.seen.get(ln, 0) >= ix:
                continue
            qlane.seen[ln] = ix
            qlane.eng.wait_ge(mk.lanes[ln].sem, ix)
        inst = qlane.eng.indirect_dma_start(out=out, out_offset=out_off, in_=in_, in_offset=in_off)
        dl.count += 16
        inst.then_inc(dl.sem, 16)
        mk._record(dl, reads, writes)
        mk.n_inst += 1

    MK.indirect = indirect

    def alloc_sparse(self):
        mk = self.mk
        R = {}
        R["M1a"] = mk.sbuf("sp_M1a", [128, NTILE, 32], F32)
        R["M2a"] = mk.sbuf("sp_M2a", [128, NTILE, 32], F32)
        R["A12"] = mk.sbuf("sp_A12", [128, NTILE, 2], F32)
        R["POSi"] = mk.sbuf("sp_POSi", [128, NTILE, 2], I32)
        R["IDXW"] = mk.sbuf("sp_IDXW", [128, NBLK], I32)
        R["U"] = mk.sbuf("sp_U", [128, 128], BF16)
        R["jg"] = mk.sbuf("sp_jg", [128, NBLK], F32)
        R["pidx"] = mk.sbuf("sp_pidx", [128, 1], F32)
        R["ones32"] = mk.sbuf("sp_ones32", [128, 32], F32)
        mk.op("pool", lambda e: e.memset(R["U"][:], 1.0), writes=[R["U"]])
        mk.op("pool", lambda e: e.affine_select(out=R["U"][:], in_=R["U"][:], pattern=[[1, 128]], compare_op=ALU.is_gt,
                                                fill=0.0, base=0, channel_multiplier=-1), reads=[R["U"]], writes=[R["U"]])
        mk.op("pool", lambda e: e.memset(R["ones32"][:], 1.0), writes=[R["ones32"]])
        return R

    def route_tile_sparse(self, hf, a, tok0, R, rt):
        mk = self.mk
        ps = self.ps[1 + (a % 2)]
        Wr, br = R["Wr"], R["br"]
        ti = tok0 // 128
        for k in range(DC):
            mk.op("pe", lambda e, k=k: e.matmul(ps[:, 0:36], hf[:, k, a * 128:(a + 1) * 128], Wr[:, k, :],
                                                 start=(k == 0), stop=(k == DC - 1)), reads=[hf, Wr], writes=[ps])
        lg, gmax, ngmax, ge, gsum, gp, mg, es = (rt[k] for k in ("lg", "gmax", "ngmax", "ge", "gsum", "gp", "mg", "es"))
        t8, dd, w2, m1, m2 = (rt[k] for k in ("t8", "dd", "w2", "m1", "m2"))
        M1a, M2a, A12 = R["M1a"], R["M2a"], R["A12"]
        V = lambda fn, reads, writes: mk.op("dve", fn, reads=reads, writes=writes)
        V(lambda e: e.tensor_tensor(out=lg[:], in0=ps[:, 0:36], in1=br[:], op=ALU.add), [ps, br], [lg])
        V(lambda e: e.reduce_max(out=gmax[:], in_=lg[:, 0:4], axis=AX.X), [lg], [gmax])
        V(lambda e: e.tensor_scalar(out=ngmax[:], in0=gmax[:], scalar1=-1.0, scalar2=None, op0=ALU.mult), [gmax], [ngmax])
        mk.op("act", lambda e: e.activation(out=ge[:], in_=lg[:, 0:4], func=AF.Exp, bias=ngmax[:, 0:1],
                                            accum_out=gsum[:]), reads=[lg, ngmax], writes=[ge, gsum])
        V(lambda e: e.reciprocal(gp[:], gsum[:]), [gsum], [gp])
        V(lambda e: e.tensor_scalar(out=mg[:], in0=lg[:, 0:4], scalar1=gmax[:, 0:1], scalar2=None, op0=ALU.is_equal),
          [lg, gmax], [mg])
        V(lambda e: e.tensor_scalar(out=es[:], in0=lg[:, 4:12], scalar1=mg[:, 0:1], scalar2=None, op0=ALU.mult),
          [lg, mg], [es])
        for g in range(1, 4):
            V(lambda e, g=g: e.scalar_tensor_tensor(out=es[:], in0=lg[:, 4 + 8 * g:12 + 8 * g], scalar=mg[:, g:g + 1],
                                                    in1=es[:], op0=ALU.mult, op1=ALU.add), [lg, mg, es], [es])
        V(lambda e: e.max(out=t8[:], in_=es[:]), [es], [t8])
        V(lambda e: e.tensor_tensor(out=dd[:], in0=t8[:, 1:2], in1=t8[:, 0:1], op=ALU.subtract), [t8], [dd])
        mk.op("act", lambda e: e.activation(out=w2[:], in_=dd[:], func=AF.Sigmoid), reads=[dd], writes=[w2])
        V(lambda e: e.tensor_tensor(out=A12[:, ti, 1:2], in0=w2[:], in1=gp[:], op=ALU.mult), [w2, gp], [A12])
        V(lambda e: e.tensor_tensor(out=A12[:, ti, 0:1], in0=gp[:], in1=A12[:, ti, 1:2], op=ALU.subtract), [gp, A12], [A12])
        V(lambda e: e.tensor_scalar(out=m1[:], in0=es[:], scalar1=t8[:, 0:1], scalar2=None, op0=ALU.is_equal), [es, t8], [m1])
        V(lambda e: e.tensor_scalar(out=m2[:], in0=es[:], scalar1=t8[:, 1:2], scalar2=None, op0=ALU.is_equal), [es, t8], [m2])
        for g in range(4):
            V(lambda e, g=g: e.tensor_scalar(out=M1a[:, ti, 8 * g:8 * g + 8], in0=m1[:], scalar1=mg[:, g:g + 1],
                                             scalar2=None, op0=ALU.mult), [m1, mg], [M1a])
            V(lambda e, g=g: e.tensor_scalar(out=M2a[:, ti, 8 * g:8 * g + 8], in0=m2[:], scalar1=mg[:, g:g + 1],
                                             scalar2=None, op0=ALU.mult), [m2, mg], [M2a])

    def stage_moe_sparse(self, i, XT, R, I):
        mk = self.mk
        HTOK, HS, YS = self.dram["HTOK"], self.dram["HS"], self.dram["YS"]
        M1a, M2a, A12, POSi, IDXW = R["M1a"], R["M2a"], R["A12"], R["POSi"], R["IDXW"]
        m0 = mk.mark()
        Mb = mk.sbuf("sq_Mb", [128, NTILE, 32], BF16)
        P = mk.sbuf("sq_P", [128, NTILE, 32], F32)
        prod = mk.sbuf("sq_prod", [128, NTILE, 32], F32)
        posf = mk.sbuf("sq_posf", [128, NTILE, 2], F32)
        nf = mk.sbuf("sq_nf", [128, 32], F32)
        ni = mk.sbuf("sq_ni", [128, 32], I32)
        pn = mk.sbuf("sq_pn", [128, 32], F32)
        incl = mk.sbuf("sq_incl", [128, 32], F32)
        excl = mk.sbuf("sq_excl", [128, 32], F32)
        EB = mk.sbuf("sq_EB", [128, NBLK], F32)
        mk.op("dve", lambda e: e.tensor_tensor(out=Mb[:], in0=M1a[:], in1=M2a[:], op=ALU.add), reads=[M1a, M2a], writes=[Mb])
        for ti in range(NTILE):
            pb = self.ps[ti // 12]
            c0 = (ti % 12) * 32
            for tj in range(ti):
                mk.op("pe", lambda e, pb=pb, c0=c0, tj=tj: e.matmul(pb[:, c0:c0 + 32], self.ones_b[:], Mb[:, tj, :],
                                                                    start=(tj == 0), stop=False), reads=[self.ones_b, Mb], writes=[pb])
            mk.op("pe", lambda e, pb=pb, c0=c0, ti=ti: e.matmul(pb[:, c0:c0 + 32], R["U"][:], Mb[:, ti, :],
                                                                start=(ti == 0), stop=True), reads=[R["U"], Mb], writes=[pb])
        pt = self.ps[3]
        for tj in range(NTILE):
            mk.op("pe", lambda e, tj=tj: e.matmul(pt[:, 0:32], self.ones_b[:], Mb[:, tj, :], start=(tj == 0),
                                                  stop=(tj == NTILE - 1)), reads=[self.ones_b, Mb], writes=[pt])
        V = lambda fn, reads, writes: mk.op("dve", fn, reads=reads, writes=writes)
        V(lambda e: e.tensor_scalar(out=nf[:], in0=pt[:, 0:32], scalar1=float(BSL - 1), scalar2=None, op0=ALU.add), [pt], [nf])
        V(lambda e: e.tensor_copy(ni[:], nf[:]), [nf], [ni])
        V(lambda e: e.tensor_single_scalar(out=ni[:], in_=ni[:], scalar=BSH, op=ALU.arith_shift_right), [ni], [ni])
        V(lambda e: e.tensor_single_scalar(out=ni[:], in_=ni[:], scalar=BSH, op=ALU.logical_shift_left), [ni], [ni])
        V(lambda e: e.tensor_copy(pn[:], ni[:]), [ni], [pn])
        V(lambda e: e.tensor_tensor_scan(out=incl[:], data0=R["ones32"][:], data1=pn[:], initial=0.0, op0=ALU.mult, op1=ALU.add),
          [R["ones32"], pn], [incl])
        V(lambda e: e.tensor_tensor(out=excl[:], in0=incl[:], in1=pn[:], op=ALU.subtract), [incl, pn], [excl])
        for ti in range(NTILE):
            pb = self.ps[ti // 12]
            c0 = (ti % 12) * 32
            V(lambda e, pb=pb, c0=c0, ti=ti: e.tensor_tensor(out=P[:, ti, :], in0=pb[:, c0:c0 + 32], in1=excl[:], op=ALU.add),
              [pb, excl], [P])
        for k, Mk in enumerate((M1a, M2a)):
            V(lambda e, Mk=Mk: e.tensor_tensor(out=prod[:], in0=Mk[:], in1=P[:], op=ALU.mult), [Mk, P], [prod])
            V(lambda e, k=k: e.reduce_sum(out=posf[:, :, k], in_=prod[:], axis=AX.X), [prod], [posf])
        V(lambda e: e.tensor_copy(POSi[:], posf[:]), [posf], [POSi])
        V(lambda e: e.memset(EB[:], 0.0), [], [EB])
        for ex in range(NE):
            V(lambda e, ex=ex: e.scalar_tensor_tensor(out=EB[:], in0=R["jg"][:], scalar=incl[:, ex:ex + 1], in1=EB[:],
                                                      op0=ALU.is_ge, op1=ALU.add), [R["jg"], incl, EB], [EB])
        V(lambda e: e.tensor_scalar(out=EB[:], in0=EB[:], scalar1=float(NE - 1), scalar2=128.0, op0=ALU.min, op1=ALU.mult), [EB], [EB])
        V(lambda e: e.tensor_scalar(out=EB[:], in0=EB[:], scalar1=R["pidx"][:, 0:1], scalar2=None, op0=ALU.add), [EB, R["pidx"]], [EB])
        V(lambda e: e.tensor_copy(IDXW[:], EB[:]), [EB], [IDXW])
        if self.cfg.get("dbg_pos"):
            d1 = T(self.nc.dram_tensor("dbg_pos", [128, NTILE, 2], I32, kind="ExternalOutput"), "dbg_pos")
            d2 = T(self.nc.dram_tensor("dbg_idx", [128, NBLK], I32, kind="ExternalOutput"), "dbg_idx")
            d3 = T(self.nc.dram_tensor("dbg_incl", [128, 32], F32, kind="ExternalOutput"), "dbg_incl")
            d4 = T(self.nc.dram_tensor("dbg_P", [128, NTILE, 32], F32, kind="ExternalOutput"), "dbg_P")
            d5 = T(self.nc.dram_tensor("dbg_M1", [128, NTILE, 32], F32, kind="ExternalOutput"), "dbg_M1")
            d6 = T(self.nc.dram_tensor("dbg_A12", [128, NTILE, 2], F32, kind="ExternalOutput"), "dbg_A12")
            mk.dma("sp", d1[:, :, :], POSi[:], reads=[POSi])
            mk.dma("sp", d2[:, :], IDXW[:], reads=[IDXW])
            mk.dma("sp", d3[:, :], incl[:], reads=[incl])
            mk.dma("sp", d4[:, :, :], P[:], reads=[P])
            mk.dma("sp", d5[:, :, :], M1a[:], reads=[M1a])
            mk.dma("sp", d6[:, :, :], A12[:], reads=[A12])
            mk.release(m0)
            return
        mk.release(m0)
        m0 = mk.mark()
        ht = [mk.sbuf("sq_ht%d" % k, [128, D], BF16) for k in range(3)]
        for ti in range(NTILE):
            h = ht[ti % 3]
            mk.dma("sp", h[:], HTOK[ti * 128:(ti + 1) * 128, :], reads=[HTOK], writes=[h])
            for k in range(2):
                mk.indirect(HS[:, :], IOA(ap=POSi[:, ti, k:k + 1], axis=0), h[:], None, [h, POSi], [HS])
        mk.release(m0)
        m0 = mk.mark()
        wgu = [mk.sbuf("sq_wgu%d" % k, [128, DC, 512], BF16) for k in range(2)]
        wdn = [mk.sbuf("sq_wdn%d" % k, [128, 2, D], BF16) for k in range(2)]
        NA = BSL // 128
        hs = [mk.sbuf("sq_hs%d" % k, [128, NA, D], BF16) for k in range(2)]
        hsT = [mk.sbuf("sq_hsT%d" % k, [128, DC, BSL], BF16) for k in range(2)]
        sl = [mk.sbuf("sq_sl%d" % k, [128, BSL], F32) for k in range(2)]
        hid = [mk.sbuf("sq_hid%d" % k, [128, 2, BSL], BF16) for k in range(2)]
        yo = [mk.sbuf("sq_yo%d" % k, [128, D], F32) for k in range(3)]
        WGU, WDN = I["moe_wgu%d" % i], I["moe_wdn%d" % i]
        ny = 0
        for j in range(NBLK):
            w, wd_, h, hT_, hd = wgu[j % 2], wdn[j % 2], hs[j % 2], hsT[j % 2], hid[j % 2]
            mk.indirect(w[:].rearrange("p a b -> p (a b)"), None, WGU[:, :], IOA(ap=IDXW[:, j:j + 1], axis=0), [IDXW], [w])
            mk.indirect(wd_[:].rearrange("p a b -> p (a b)"), None, WDN[:, :], IOA(ap=IDXW[:, j:j + 1], axis=0), [IDXW], [wd_])
            mk.dma("sp", h[:], HS[j * BSL:(j + 1) * BSL, :].rearrange("(a p) d -> p a d", p=128), reads=[HS], writes=[h])
            KPB = 1024 // BSL
            for hh in range(DC // KPB):
                pb = self.ps[hh % 2]
                pbb = pb[:, 0:512].bitcast(BF16)
                for kk in range(KPB):
                    k = hh * KPB + kk
                    for a in range(NA):
                        mk.op("pe", lambda e, pbb=pbb, kk=kk, k=k, a=a, h=h: e.transpose(
                            pbb[:, kk * BSL + a * 128:kk * BSL + (a + 1) * 128], h[:, a, k * 128:(k + 1) * 128], self.ident_b[:]),
                            reads=[h, self.ident_b], writes=[pb])
                dstv = hT_[:, hh * KPB:(hh + 1) * KPB, :].rearrange("p a b -> p (a b)")
                if hh % 2:
                    mk.op("act", lambda e, pbb=pbb, dstv=dstv: e.copy(dstv, pbb[:, :]), reads=[pb], writes=[hT_])
                else:
                    mk.op("dve", lambda e, pbb=pbb, dstv=dstv: e.tensor_copy(dstv, pbb[:, :]), reads=[pb], writes=[hT_])
            for fc in range(2):
                pg, pu = self.ps[2 + 2 * fc], self.ps[3 + 2 * fc]
                for k in range(DC):
                    mk.op("pe", lambda e, pg=pg, k=k, fc=fc, w=w, hT_=hT_: e.matmul(
                        pg[:, 0:BSL], w[:, k, fc * 128:(fc + 1) * 128], hT_[:, k, :], start=(k == 0), stop=(k == DC - 1)),
                        reads=[w, hT_], writes=[pg])
                for k in range(DC):
                    mk.op("pe", lambda e, pu=pu, k=k, fc=fc, w=w, hT_=hT_: e.matmul(
                        pu[:, 0:BSL], w[:, k, 256 + fc * 128:256 + (fc + 1) * 128], hT_[:, k, :], start=(k == 0), stop=(k == DC - 1)),
                        reads=[w, hT_], writes=[pu])
                s_ = sl[fc]
                mk.op("act", lambda e, pg=pg, s_=s_: e.activation(out=s_[:], in_=pg[:, 0:BSL], func=AF.Silu), reads=[pg], writes=[s_])
                mk.op("dve", lambda e, pu=pu, s_=s_, hd=hd, fc=fc: e.tensor_tensor(out=hd[:, fc, :], in0=pu[:, 0:BSL], in1=s_[:],
                                                                                  op=ALU.mult), reads=[pu, s_], writes=[hd])
            for a in range(NA):
                y_ = yo[ny % 3]
                ny += 1
                for dh in range(2):
                    pd = self.ps[6 + dh]
                    for fc in range(2):
                        mk.op("pe", lambda e, pd=pd, fc=fc, a=a, dh=dh, hd=hd, wd_=wd_: e.matmul(
                            pd[:, :], hd[:, fc, a * 128:(a + 1) * 128], wd_[:, fc, dh * 512:(dh + 1) * 512], start=(fc == 0), stop=(fc == 1)),
                            reads=[hd, wd_], writes=[pd])
                    if dh:
                        mk.op("act", lambda e, pd=pd, y_=y_, dh=dh: e.copy(y_[:, dh * 512:(dh + 1) * 512], pd[:, :]), reads=[pd], writes=[y_])
                    else:
                        mk.op("dve", lambda e, pd=pd, y_=y_, dh=dh: e.tensor_copy(y_[:, dh * 512:(dh + 1) * 512], pd[:, :]), reads=[pd], writes=[y_])
                mk.dma("act", YS[j * BSL + a * 128:j * BSL + (a + 1) * 128, :], y_[:], reads=[y_], writes=[YS])
        mk.release(m0)
        m0 = mk.mark()
        r1 = [mk.sbuf("sq_r1%d" % k, [128, D], F32) for k in range(2)]
        r2 = [mk.sbuf("sq_r2%d" % k, [128, D], F32) for k in range(2)]
        xr = [mk.sbuf("sq_x%d" % k, [128, 512], F32) for k in range(3)]
        n = 0
        for bi, (t0, tn) in enumerate(BLOCKS):
            r = 1 if bi == 0 else 0
            g2 = self.mv[("g2", r)]
            for a in range(tn // 128):
                ti = t0 // 128 + a
                a_, b_ = r1[ti % 2], r2[ti % 2]
                mk.indirect(a_[:], None, YS[:, :], IOA(ap=POSi[:, ti, 0:1], axis=0), [YS, POSi], [a_])
                mk.indirect(b_[:], None, YS[:, :], IOA(ap=POSi[:, ti, 1:2], axis=0), [YS, POSi], [b_])
                mk.op("dve", lambda e, a_=a_, ti=ti: e.tensor_scalar(out=a_[:], in0=a_[:], scalar1=A12[:, ti, 0:1], scalar2=None,
                                                                    op0=ALU.mult), reads=[a_, A12], writes=[a_])
                mk.op("dve", lambda e, a_=a_, b_=b_, ti=ti: e.scalar_tensor_tensor(out=a_[:], in0=b_[:], scalar=A12[:, ti, 1:2], in1=a_[:],
                                                                                  op0=ALU.mult, op1=ALU.add), reads=[a_, b_, A12], writes=[a_])
                for dc in range(DC):
                    mk.op("pe", lambda e, dc=dc, a=a, a_=a_: e.transpose(self.ps[dc][:, a * 128:(a + 1) * 128],
                                                                         a_[:, dc * 128:(dc + 1) * 128], self.ident_f[:]),
                          reads=[a_, self.ident_f], writes=[self.ps[dc]])
            for dc in range(DC):
                x = xr[n % 3]
                n += 1
                mk.dma("sp", x[:, 0:tn], XT[dc * 128:(dc + 1) * 128, t0:t0 + tn], reads=[self.XTr[dc]], writes=[x])
                mk.op("dve", lambda e, dc=dc, x=x, g2=g2: e.scalar_tensor_tensor(
                    out=x[:, 0:tn], in0=self.ps[dc][:, 0:tn], scalar=g2[:, dc:dc + 1], in1=x[:, 0:tn],
                    op0=ALU.mult, op1=ALU.add), reads=[self.ps[dc], g2, x], writes=[x])
                mk.dma("act", XT[dc * 128:(dc + 1) * 128, t0:t0 + tn], x[:, 0:tn], reads=[x], writes=[self.XTr[dc]])
        mk.release(m0)

    Prog.alloc_sparse = alloc_sparse
    Prog._route_tile_sparse = route_tile_sparse
    Prog.stage_moe_sparse = stage_moe_sparse


_sparse_methods()


def hyd_consts():
    bf = ml_dtypes.bfloat16
    L, N = CTX, 2 * CTX
    c = {}
    t = (np.arange(2)[None, :, None] * 128 + np.arange(128)[:, None, None]).astype(np.float64)
    k = np.arange(N)[None, None, :].astype(np.float64)
    ang = 2 * np.pi * t * k / N
    c["F"] = np.stack([np.cos(ang), -np.sin(ang), np.sin(ang)], axis=2).astype(bf)
    kk = (np.arange(4)[None, :, None] * 128 + np.arange(128)[:, None, None]).astype(np.float64)
    tt = np.arange(L)[None, None, :].astype(np.float64)
    a2 = 2 * np.pi * kk * tt / N
    c["Fi"] = np.stack([np.cos(a2) / N, -np.sin(a2) / N], axis=2).astype(bf)
    pos = (np.arange(2)[None, :] * 128 + np.arange(128)[:, None]).astype(np.float32)
    c["lneg"] = (-(pos / np.float32(L - 1))).astype(np.float32)
    return c


def _hyd_methods():
    def hy_ctx_direct(self, I):
        mk = self.mk
        L = CTX
        ZT, Z1, HT = self.dram["hyZ"], self.dram["hyZ1"], self.dram["HT"]
        m0 = mk.mark()
        Fd = mk.sbuf("hd_F", [128, 2, 3, 512], BF16)
        Fi = mk.sbuf("hd_Fi", [128, 4, 2, L], BF16)
        lneg = mk.sbuf("hd_lneg", [128, 2], F32)
        dab = mk.sbuf("hd_dab", [128, D], F32)
        ft = mk.sbuf("hd_ft", [17, L], F32)
        W1 = mk.sbuf("hd_w1", [17, 64], F32)
        W2 = mk.sbuf("hd_w2", [64, 64], F32)
        W3 = mk.sbuf("hd_w3", [64, 4096], BF16)
        fv = mk.sbuf("hd_fv", [64, 4], F32)
        fb = mk.sbuf("hd_fb", [64, 2], F32)
        z1 = mk.sbuf("hd_z1", [64, L], F32)
        z2 = mk.sbuf("hd_z2", [64, L], F32)
        z2b = mk.sbuf("hd_z2b", [64, L], BF16)
        tmp = mk.sbuf("hd_tmp", [64, L], F32)
        mk.dma("sp", Fd[:], I["hyd_F"][:, :, :, :], writes=[Fd])
        mk.dma("sp", Fi[:], I["hyd_Fi"][:, :, :, :], writes=[Fi])
        mk.dma("sp", lneg[:], I["hyd_lneg"][:, :], writes=[lneg])
        mk.dma("sp", dab[0:64, :], I["hy_dabs"][:, :], writes=[dab])
        mk.dma("sp", dab[64:128, :], I["hy_dabs"][:, :], writes=[dab])
        mk.dma("sp", ft[:], I["hy_featsT_ctx"][:, :], writes=[ft])
        mk.dma("sp", W1[:], I["hy_f_w1"][0, :, :], writes=[W1])
        mk.dma("sp", W2[:], I["hy_f_w2"][0, :, :], writes=[W2])
        mk.dma("pool", W3[:], I["hy_f_w3"][0, :, :], writes=[W3])
        mk.dma("sp", fv[:], I["hy_fvec"][:, :], writes=[fv])
        mk.op("dve", lambda e: e.tensor_tensor(out=fb[:, 0:1], in0=fv[:, 0:1], in1=fv[:, 1:2], op=ALU.mult), reads=[fv], writes=[fb])
        mk.op("dve", lambda e: e.tensor_tensor(out=fb[:, 1:2], in0=fv[:, 2:3], in1=fv[:, 3:4], op=ALU.mult), reads=[fv], writes=[fb])
        for layer, (Wm, src, dst, kin) in enumerate(((W1, ft, z1, 17), (W2, z1, z2, 64))):
            ps = self.ps[layer]
            mk.op("pe", lambda e, ps=ps, Wm=Wm, src=src, kin=kin: e.matmul(ps[0:64, 0:L], Wm[0:kin, :], src[0:kin, :],
                                                                          start=True, stop=True), reads=[Wm, src], writes=[ps])
            mk.op("dve", lambda e, ps=ps, dst=dst, layer=layer: e.tensor_scalar(
                out=dst[:], in0=ps[0:64, 0:L], scalar1=fv[:, 2 * layer + 1:2 * layer + 2], scalar2=fb[:, layer:layer + 1],
                op0=ALU.mult, op1=ALU.add), reads=[ps, fv, fb], writes=[dst])
            for _ in range(2):
                mk.op("dve", lambda e, dst=dst: e.tensor_scalar(out=tmp[:], in0=dst[:], scalar1=math.pi, scalar2=2 * math.pi,
                                                                op0=ALU.is_gt, op1=ALU.mult), reads=[dst], writes=[tmp])
                mk.op("dve", lambda e, dst=dst: e.tensor_tensor(out=dst[:], in0=dst[:], in1=tmp[:], op=ALU.subtract),
                      reads=[dst, tmp], writes=[dst])
                mk.op("dve", lambda e, dst=dst: e.tensor_scalar(out=tmp[:], in0=dst[:], scalar1=-math.pi, scalar2=2 * math.pi,
                                                                op0=ALU.is_lt, op1=ALU.mult), reads=[dst], writes=[tmp])
                mk.op("dve", lambda e, dst=dst: e.tensor_tensor(out=dst[:], in0=dst[:], in1=tmp[:], op=ALU.add),
                      reads=[dst, tmp], writes=[dst])
            mk.op("act", lambda e, dst=dst: e.activation(out=dst[:], in_=dst[:], func=AF.Sin), reads=[dst], writes=[dst])
        mk.op("act", lambda e: e.copy(z2b[:], z2[:]), reads=[z2], writes=[z2b])
        Hf = mk.sbuf("hd_Hf", [128, 2, 2, 4, D], BF16)
        dec = [mk.sbuf("hd_dec%d" % k, [128, D], F32) for k in range(2)]
        hd = [mk.sbuf("hd_hd%d" % k, [128, D], F32) for k in range(2)]
        hdb = [mk.sbuf("hd_hdb%d" % k, [128, D], BF16) for k in range(4)]
        ha = [mk.sbuf("hd_ha%d" % k, [128, D], BF16) for k in range(2)]
        rn = mk.sbuf("hd_rn", [128, D], F32)
        for o in range(2):
            pn = (self.ps[6], self.ps[7])
            n = 0
            for pt in range(2):
                for dr in range(2):
                    i2 = n % 2
                    for hf in range(2):
                        ph = self.ps[i2 * 2 + hf]
                        w0 = (dr * 2 + o) * D + hf * 512
                        mk.op("pe", lambda e, ph=ph, pt=pt, w0=w0: e.matmul(ph[:, :], z2b[:, pt * 128:(pt + 1) * 128], W3[:, w0:w0 + 512],
                                                                            start=True, stop=True), reads=[z2b, W3], writes=[ph])
                    mk.op("act", lambda e, i2=i2, pt=pt: e.activation(out=dec[i2][:], in_=dab[:], func=AF.Exp, scale=lneg[:, pt:pt + 1]),
                          reads=[dab, lneg], writes=[dec[i2]])
                    for hf in range(2):
                        ph = self.ps[i2 * 2 + hf]
                        sl_ = slice(hf * 512, (hf + 1) * 512)
                        mk.op("dve", lambda e, ph=ph, i2=i2, sl_=sl_: e.tensor_tensor(out=hd[i2][:, sl_], in0=ph[:, :], in1=dec[i2][:, sl_],
                                                                                     op=ALU.mult), reads=[ph, dec[i2]], writes=[hd[i2]])
                    if dr == 1 and pt == 0:
                        mk.op("dve", lambda e, i2=i2: e.memset(hd[i2][0:1, :], 0.0), reads=[hd[i2]], writes=[hd[i2]])
                    hb_ = hdb[pt * 2 + dr]
                    mk.op("act", lambda e, i2=i2, hb_=hb_: e.copy(hb_[:], hd[i2][:]), reads=[hd[i2]], writes=[hb_])
                    mk.op("dve", lambda e, i2=i2: e.scalar_tensor_tensor(out=ha[i2][:], in0=hd[i2][:], scalar=-1.0, in1=hd[i2][:],
                                                                        op0=ALU.mult, op1=ALU.max), reads=[hd[i2]], writes=[ha[i2]])
                    for hf in range(2):
                        mk.op("pe", lambda e, i2=i2, hf=hf, n=n: e.matmul(pn[hf][:, :], self.ones_b[:], ha[i2][:, hf * 512:(hf + 1) * 512],
                                                                          start=(n == 0), stop=(n == 3)), reads=[self.ones_b, ha[i2]], writes=[pn[hf]])
                    n += 1
            for hf in range(2):
                mk.op("dve", lambda e, hf=hf: e.reciprocal(rn[:, hf * 512:(hf + 1) * 512], pn[hf][:, :]), reads=[pn[hf]], writes=[rn])
            q = 0
            for kc in range(4):
                for ri in range(2):
                    for hf in range(2):
                        pa = self.ps[4 + q % 2]
                        q += 1
                        sl_ = slice(hf * 512, (hf + 1) * 512)
                        steps = [(pt, dr) for pt in range(2) for dr in range(2)]
                        for si, (pt, dr) in enumerate(steps):
                            kind = ri if dr == 0 else (0 if ri == 0 else 2)
                            mk.op("pe", lambda e, pa=pa, pt=pt, dr=dr, kind=kind, kc=kc, sl_=sl_, si=si: e.matmul(
                                pa[:, :], Fd[:, pt, kind, kc * 128:(kc + 1) * 128], hdb[pt * 2 + dr][:, sl_],
                                start=(si == 0), stop=(si == 3)), reads=[Fd, hdb[pt * 2 + dr]], writes=[pa])
                        mk.op("dve", lambda e, pa=pa, o=o, ri=ri, kc=kc, sl_=sl_: e.tensor_tensor(
                            out=Hf[:, o, ri, kc, sl_], in0=pa[:, :], in1=rn[:, sl_], op=ALU.mult), reads=[pa, rn], writes=[Hf])
        zf = mk.sbuf("hd_zf", [128, DC, L], F32)
        gt = mk.sbuf("hd_gt", [128, DC, L], F32)
        zb = mk.sbuf("hd_zb", [128, DC, L], BF16)
        zT = mk.sbuf("hd_zT", [128, 2, D], BF16)
        Y = mk.sbuf("hd_Y", [128, 2, 4, D], BF16)
        ta = [mk.sbuf("hd_ta%d" % k, [128, 512], F32) for k in range(2)]
        tb = [mk.sbuf("hd_tb%d" % k, [128, 512], F32) for k in range(2)]
        lb = mk.sbuf("hd_lb", [128, 2, DC], F32)
        rr = [mk.sbuf("hd_rr%d" % k, [128, L], F32) for k in range(2)]
        obf = mk.sbuf("hd_obf", [128, DC, L], F32)
        obb = mk.sbuf("hd_obb", [128, DC, L], BF16)
        for o in range(2):
            mk.dma("sp", lb[:, o, :], I["hy_lbias_pc"][o, :, :], writes=[lb])
        for o in range(2):
            zsrc = ZT if o == 0 else Z1
            mk.dma("sp", zf[:], zsrc[0:D, 0:L].rearrange("(j p) t -> p j t", p=128), reads=[zsrc], writes=[zf])
            g0 = (1 + o) * D
            mk.dma("sp", gt[:], ZT[g0:g0 + D, 0:L].rearrange("(j p) t -> p j t", p=128), reads=[ZT], writes=[gt])
            mk.op("act", lambda e: e.copy(zb[:], zf[:]), reads=[zf], writes=[zb])
            for pt in range(2):
                pb = self.ps[pt]
                pbb = pb[:, 0:512].bitcast(BF16)
                for j in range(DC):
                    mk.op("pe", lambda e, pbb=pbb, j=j, pt=pt: e.transpose(pbb[:, j * 128:(j + 1) * 128], zb[:, j, pt * 128:(pt + 1) * 128],
                                                                          self.ident_b[:]), reads=[zb, self.ident_b], writes=[pb])
                mk.op("act", lambda e, pbb=pbb, pt=pt: e.copy(zT[:, pt, :], pbb[:, :]), reads=[pb], writes=[zT])
            for kc in range(4):
                for ri in range(2):
                    for hf in range(2):
                        px = self.ps[2 + ri * 2 + hf]
                        for pt in range(2):
                            mk.op("pe", lambda e, px=px, pt=pt, ri=ri, kc=kc, hf=hf: e.matmul(
                                px[:, :], Fd[:, pt, ri, kc * 128:(kc + 1) * 128], zT[:, pt, hf * 512:(hf + 1) * 512],
                                start=(pt == 0), stop=(pt == 1)), reads=[Fd, zT], writes=[px])
                for hf in range(2):
                    sl_ = slice(hf * 512, (hf + 1) * 512)
                    pxr, pxi = self.ps[2 + hf], self.ps[4 + hf]
                    a_, b_ = ta[hf], tb[hf]
                    mk.op("dve", lambda e, pxr=pxr, a_=a_, kc=kc, sl_=sl_, o=o: e.tensor_tensor(out=a_[:], in0=pxr[:, :], in1=Hf[:, o, 0, kc, sl_],
                                                                                             op=ALU.mult), reads=[pxr, Hf], writes=[a_])
                    mk.op("dve", lambda e, pxi=pxi, b_=b_, kc=kc, sl_=sl_, o=o: e.tensor_tensor(out=b_[:], in0=pxi[:, :], in1=Hf[:, o, 1, kc, sl_],
                                                                                             op=ALU.mult), reads=[pxi, Hf], writes=[b_])
                    mk.op("pool", lambda e, a_=a_, b_=b_, kc=kc, sl_=sl_: e.tensor_tensor(out=Y[:, 0, kc, sl_], in0=a_[:], in1=b_[:],
                                                                                         op=ALU.subtract), reads=[a_, b_], writes=[Y])
                    mk.op("dve", lambda e, pxr=pxr, a_=a_, kc=kc, sl_=sl_, o=o: e.tensor_tensor(out=a_[:], in0=pxr[:, :], in1=Hf[:, o, 1, kc, sl_],
                                                                                             op=ALU.mult), reads=[pxr, Hf], writes=[a_])
                    mk.op("dve", lambda e, pxi=pxi, b_=b_, kc=kc, sl_=sl_, o=o: e.tensor_tensor(out=b_[:], in0=pxi[:, :], in1=Hf[:, o, 0, kc, sl_],
                                                                                             op=ALU.mult), reads=[pxi, Hf], writes=[b_])
                    mk.op("pool", lambda e, a_=a_, b_=b_, kc=kc, sl_=sl_: e.tensor_tensor(out=Y[:, 1, kc, sl_], in0=a_[:], in1=b_[:],
                                                                                         op=ALU.add), reads=[a_, b_], writes=[Y])
            ob = obf if o == 0 else obb
            for cj in range(DC):
                py = self.ps[6 + cj % 2]
                for kc in range(4):
                    for ri in range(2):
                        mk.op("pe", lambda e, py=py, kc=kc, ri=ri, cj=cj: e.matmul(
                            py[:, 0:L], Y[:, ri, kc, cj * 128:(cj + 1) * 128], Fi[:, kc, ri, :],
                            start=(kc == 0 and ri == 0), stop=(kc == 3 and ri == 1)), reads=[Y, Fi], writes=[py])
                r_ = rr[cj % 2]
                mk.op("dve", lambda e, py=py, r_=r_, cj=cj, o=o: e.scalar_tensor_tensor(out=r_[:], in0=zf[:, cj, :], scalar=lb[:, o, cj:cj + 1],
                                                                                     in1=py[:, 0:L], op0=ALU.mult, op1=ALU.add),
                      reads=[zf, lb, py], writes=[r_])
                mk.op("pool", lambda e, r_=r_, cj=cj, ob=ob: e.tensor_tensor(out=ob[:, cj, :], in0=r_[:], in1=gt[:, cj, :], op=ALU.mult),
                      reads=[r_, gt], writes=[ob])
            dst = Z1 if o == 0 else HT
            mk.dma("act", dst[0:D, 0:L].rearrange("(j p) t -> p j t", p=128), ob[:], reads=[ob], writes=[dst])
        mk.release(m0)

    Prog.hy_ctx_direct = hy_ctx_direct


_hyd_methods()
```

```python
import math
import numpy as np
import ml_dtypes
import concourse.bass as bass
import concourse.mybir as mybir
from concourse.bass_utils import run_bass_kernel_spmd

F32 = mybir.dt.float32
BF16 = mybir.dt.bfloat16
I32 = mybir.dt.int32
ALU = mybir.AluOpType
AF = mybir.ActivationFunctionType
AX = mybir.AxisListType

D = 1024
DC = 8
CTX = 256
SEQ = 4096
NT = CTX + SEQ
DEPTH = 4
EPS = 1e-6
NE = 32
DEXP = 256
BLOCKS = [(0, CTX)] + [(CTX + 512 * i, 512) for i in range(SEQ // 512)]


class T:
    __slots__ = ("t", "lw", "rd", "name")

    def __init__(self, t, name=""):
        self.t = t
        self.lw = None
        self.rd = {}
        self.name = name

    def __getitem__(self, k):
        return self.t[k]


class Lane:
    def __init__(self, name, eng, sem, step):
        self.name, self.eng, self.sem, self.step = name, eng, sem, step
        self.count = 0
        self.seen = {}


class MK:
    def __init__(self, nc, n_dma_sems=40):
        self.nc = nc
        self._stack = []
        self.lanes = {}
        for name, eng in (("pe", nc.tensor), ("act", nc.scalar), ("dve", nc.vector),
                          ("pool", nc.gpsimd), ("sp", nc.sync)):
            sem = self._enter(nc.semaphore("s_" + name))
            self.lanes[name] = Lane(name, eng, sem, 1)
        self.dma_lanes = []
        for i in range(n_dma_sems):
            sem = self._enter(nc.semaphore("d_%d" % i))
            ln = Lane("dma%d" % i, None, sem, 16)
            self.lanes[ln.name] = ln
            self.dma_lanes.append(ln)
        self.dma_rr = 0
        self.n_inst = 0
        self.uid = 0

    def _enter(self, cm):
        v = cm.__enter__()
        self._stack.append(cm)
        return v

    def mark(self):
        return len(self._stack)

    def release(self, mark):
        self.barrier()
        while len(self._stack) > mark:
            self._stack.pop().__exit__(None, None, None)

    def close(self):
        while self._stack:
            self._stack.pop().__exit__(None, None, None)

    def barrier(self):
        for a in ("pe", "act", "dve", "pool", "sp"):
            la = self.lanes[a]
            for b, lb in self.lanes.items():
                if b == a or lb.count == 0:
                    continue
                if la.seen.get(b, 0) >= lb.count:
                    continue
                la.seen[b] = lb.count
                la.eng.wait_ge(lb.sem, lb.count)

    def sbuf(self, name, shape, dt):
        self.uid += 1
        return T(self._enter(self.nc.sbuf_tensor("%s_%d" % (name, self.uid), list(shape), dt)), name)

    def psum(self, name, shape, dt=F32):
        self.uid += 1
        return T(self._enter(self.nc.psum_tensor("%s_%d" % (name, self.uid), list(shape), dt)), name)

    def _deps(self, lane, reads, writes):
        need = {}
        raw_same = 0
        for t in reads:
            if t.lw is not None:
                ln, idx = t.lw
                if need.get(ln, 0) < idx:
                    need[ln] = idx
                if ln == lane.name:
                    raw_same = max(raw_same, idx)
        for t in writes:
            if t.lw is not None:
                ln, idx = t.lw
                if ln != lane.name and need.get(ln, 0) < idx:
                    need[ln] = idx
            for ln, idx in t.rd.items():
                if ln != lane.name and need.get(ln, 0) < idx:
                    need[ln] = idx
        return need, raw_same

    def _record(self, lane, reads, writes):
        idx = lane.count
        for t in reads:
            if t.rd.get(lane.name, 0) < idx:
                t.rd[lane.name] = idx
        for t in writes:
            t.lw = (lane.name, idx)
            t.rd = {}

    def op(self, lane_name, fn, reads=(), writes=()):
        lane = self.lanes[lane_name]
        need, raw_same = self._deps(lane, reads, writes)
        for ln, idx in need.items():
            if ln == lane.name:
                if lane.name == "pe" or raw_same <= lane.count - 3:
                    continue
                idx = raw_same
            if lane.seen.get(ln, 0) >= idx:
                continue
            lane.seen[ln] = idx
            lane.eng.wait_ge(self.lanes[ln].sem, idx)
        inst = fn(lane.eng)
        lane.count += 1
        inst.then_inc(lane.sem, 1)
        self._record(lane, reads, writes)
        self.n_inst += 1
        return inst

    def dma(self, q, out, in_, reads=(), writes=(), **kw):
        qlane = self.lanes[q]
        dl = self.dma_lanes[self.dma_rr]
        self.dma_rr = (self.dma_rr + 1) % len(self.dma_lanes)
        need, _ = self._deps(dl, reads, writes)
        if dl.count > 0:
            need[dl.name] = max(need.get(dl.name, 0), dl.count)
        for ln, idx in need.items():
            if qlane.seen.get(ln, 0) >= idx:
                continue
            qlane.seen[ln] = idx
            qlane.eng.wait_ge(self.lanes[ln].sem, idx)
        inst = qlane.eng.dma_start(out=out, in_=in_, **kw)
        dl.count += 16
        inst.then_inc(dl.sem, 16)
        self._record(dl, reads, writes)
        self.n_inst += 1
        return inst

    def finish(self):
        sp = self.lanes["sp"]
        for dl in self.dma_lanes:
            if dl.count and sp.seen.get(dl.name, 0) < dl.count:
                sp.seen[dl.name] = dl.count
                sp.eng.wait_ge(dl.sem, dl.count)
        self.barrier()


def _vec_pc(v):
    v = np.asarray(v, np.float32)
    return np.ascontiguousarray(v.reshape(-1, 128).T)


class Prog:
    def __init__(self, cfg=None):
        self.cfg = cfg or {}
        self.nc = bass.Bass("TRN2", target_bir_lowering=False)
        self.mk = MK(self.nc)
        self.ins = {}
        self.dram = {}

    def inp(self, name, shape, dt=F32):
        t = self.nc.dram_tensor(name, list(shape), dt, kind="ExternalInput")
        self.ins[name] = t
        return T(t, name)

    def scratch(self, name, shape, dt, out=False):
        kind = "ExternalOutput" if (out or name in self.cfg.get("dump", ())) else "Internal"
        t = self.nc.dram_tensor(name, list(shape), dt, kind=kind)
        self.dram[name] = T(t, name)
        return self.dram[name]

    def setup_consts(self):
        mk = self.mk
        self.ps = [mk.psum("ps%d" % i, [128, 512], F32) for i in range(8)]
        self.ident_f = mk.sbuf("ident_f", [128, 128], F32)
        self.ident_b = mk.sbuf("ident_b", [128, 128], BF16)
        self.ones_b = mk.sbuf("ones_b", [128, 128], BF16)
        self.avg_b = mk.sbuf("avg_b", [128, 128], BF16)
        mk.op("pool", lambda e: e.memset(self.ident_f[:], 1.0), writes=[self.ident_f])
        mk.op("pool", lambda e: e.affine_select(out=self.ident_f[:], in_=self.ident_f[:],
              pattern=[[-1, 128]], compare_op=ALU.is_equal, fill=0.0, base=0,
              channel_multiplier=1), reads=[self.ident_f], writes=[self.ident_f])
        mk.op("dve", lambda e: e.tensor_copy(self.ident_b[:], self.ident_f[:]),
              reads=[self.ident_f], writes=[self.ident_b])
        mk.op("pool", lambda e: e.memset(self.ones_b[:], 1.0), writes=[self.ones_b])
        mk.op("pool", lambda e: e.memset(self.avg_b[:], 1.0 / D), writes=[self.avg_b])
        self.eps_t = mk.sbuf("eps_t", [128, 1], F32)
        mk.op("pool", lambda e: e.memset(self.eps_t[:], EPS), writes=[self.eps_t])

    def stage_input(self, x_in, ctx_in, XT):
        mk = self.mk
        m0 = mk.mark()
        xin = [mk.sbuf("xin%d" % i, [128, 4, D], F32) for i in range(2)]
        xo = [mk.sbuf("xo%d" % i, [128, DC, 512], F32) for i in range(2)]
        for bi, (t0, tn) in enumerate(BLOCKS):
            nt = tn // 128
            buf = xin[bi % 2]
            src = ctx_in if bi == 0 else x_in
            r0 = 0 if bi == 0 else t0 - CTX
            mk.dma("sp", buf[:, 0:nt, :], src[r0:r0 + tn, :].rearrange("(a p) d -> p a d", p=128),
                   reads=[src], writes=[buf])
            ob = xo[bi % 2]
            for j in range(DC):
                ps = self.ps[j % 8]
                for a in range(nt):
                    mk.op("pe", lambda e, ps=ps, a=a, j=j, buf=buf: e.transpose(
                        ps[:, a * 128:(a + 1) * 128], buf[:, a, j * 128:(j + 1) * 128], self.ident_f[:]),
                        reads=[buf, self.ident_f], writes=[ps])
                eng = "act" if j % 2 else "dve"
                if eng == "act":
                    mk.op("act", lambda e, ps=ps, j=j, ob=ob: e.copy(ob[:, j, 0:tn], ps[:, 0:tn]),
                          reads=[ps], writes=[ob])
                else:
                    mk.op("dve", lambda e, ps=ps, j=j, ob=ob: e.tensor_copy(ob[:, j, 0:tn], ps[:, 0:tn]),
                          reads=[ps], writes=[ob])
            mk.dma("pool", XT[:, t0:t0 + tn].rearrange("(j p) t -> p j t", p=128), ob[:, :, 0:tn],
                   reads=[ob], writes=self.XTr)
        mk.release(m0)

    def stage_output(self, XT, y_out, fg):
        mk = self.mk
        m0 = mk.mark()
        xb = [mk.sbuf("fx%d" % i, [128, DC, 512], F32) for i in range(2)]
        sq = mk.sbuf("fsq", [128, DC, 512], BF16)
        rstd = mk.sbuf("frstd", [128, 512], F32)
        yo = [mk.sbuf("fy%d" % i, [128, 4, D], F32) for i in range(2)]
        for bi, (t0, tn) in enumerate(BLOCKS):
            if bi == 0:
                continue
            buf = xb[bi % 2]
            mk.dma("sp", buf[:, :, 0:tn], XT[:, t0:t0 + tn].rearrange("(j p) t -> p j t", p=128),
                   reads=self.XTr, writes=[buf])
            self._rstd(buf, sq, rstd, tn, self.ps[0])
            for j in range(DC):
                mk.op("dve", lambda e, j=j, buf=buf: e.scalar_tensor_tensor(
                    out=buf[:, j, 0:tn], in0=buf[:, j, 0:tn], scalar=fg[:, j:j + 1], in1=rstd[:, 0:tn],
                    op0=ALU.mult, op1=ALU.mult), reads=[buf, rstd, fg], writes=[buf])
            ob = yo[bi % 2]
            nt = tn // 128
            for a in range(nt):
                for h in range(2):
                    ps = self.ps[1 + (a * 2 + h) % 7]
                    for jj in range(4):
                        j = h * 4 + jj
                        mk.op("pe", lambda e, ps=ps, a=a, j=j, jj=jj, buf=buf: e.transpose(
                            ps[:, jj * 128:(jj + 1) * 128], buf[:, j, a * 128:(a + 1) * 128], self.ident_f[:]),
                            reads=[buf, self.ident_f], writes=[ps])
                    if h:
                        mk.op("act", lambda e, ps=ps, a=a, h=h, ob=ob: e.copy(
                            ob[:, a, h * 512:(h + 1) * 512], ps[:, :]), reads=[ps], writes=[ob])
                    else:
                        mk.op("dve", lambda e, ps=ps, a=a, h=h, ob=ob: e.tensor_copy(
                            ob[:, a, h * 512:(h + 1) * 512], ps[:, :]), reads=[ps], writes=[ob])
            r0 = t0 - CTX
            mk.dma("pool", y_out[r0:r0 + tn, :].rearrange("(a p) d -> p a d", p=128), ob[:, 0:nt, :],
                   reads=[ob], writes=[y_out])
        mk.release(m0)

    def _rstd(self, buf, sq, rstd, tn, ps, eps=EPS):
        mk = self.mk
        for j in range(DC):
            mk.op("act", lambda e, j=j: e.activation(out=sq[:, j, 0:tn], in_=buf[:, j, 0:tn], func=AF.Square),
                  reads=[buf], writes=[sq])
        for j in range(DC):
            mk.op("pe", lambda e, j=j: e.matmul(ps[:, 0:tn], self.avg_b[:], sq[:, j, 0:tn],
                                                 start=(j == 0), stop=(j == DC - 1)),
                  reads=[self.avg_b, sq], writes=[ps])
        mk.op("act", lambda e: e.activation(out=rstd[:, 0:tn], in_=ps[:, 0:tn], func=AF.Sqrt, bias=self.eps_t[:, 0:1]),
              reads=[ps, self.eps_t], writes=[rstd])
        mk.op("dve", lambda e: e.reciprocal(rstd[:, 0:tn], rstd[:, 0:tn]), reads=[rstd], writes=[rstd])

    def ap3(self, t2d, lo, n, inner):
        return t2d[:, lo:lo + n * inner].rearrange("p (a b) -> p a b", b=inner)

    def stage_mod(self, i, sT, w_mod, bmod, gmix, gffn):
        mk = self.mk
        m0 = mk.mark()
        wb = [mk.sbuf("wmod%d" % k, [128, DC, 512], F32) for k in range(2)]
        bm = mk.sbuf("bm", [128, 48], F32)
        gm = mk.sbuf("gm", [128, 2, DC], F32)
        mod = mk.sbuf("mod", [128, 48, 2], F32)
        mk.dma("sp", bm[:], bmod[i, :, :], reads=[bmod], writes=[bm])
        mk.dma("sp", gm[:, 0, :], gmix[i, :, :], reads=[gmix], writes=[gm])
        mk.dma("sp", gm[:, 1, :], gffn[i, :, :], reads=[gffn], writes=[gm])
        ps = self.ps[0]
        for s in range(12):
            w = wb[s % 2]
            mk.dma("sp", w[:], w_mod[i, :, s * 512:(s + 1) * 512].rearrange("(k p) m -> p k m", p=128),
                   reads=[w_mod], writes=[w])
            for mm in range(4):
                m = s * 4 + mm
                for k in range(DC):
                    mk.op("pe", lambda e, w=w, mm=mm, m=m, k=k: e.matmul(
                        ps[:, 2 * m:2 * m + 2], w[:, k, mm * 128:(mm + 1) * 128], sT[:, k, :],
                        start=(k == 0), stop=(k == DC - 1)), reads=[w, sT], writes=[ps])
        psv = ps[:, 0:96].rearrange("p (m r) -> p m r", r=2)
        for r in range(2):
            mk.op("dve", lambda e, r=r: e.tensor_tensor(out=mod[:, :, r], in0=psv[:, :, r], in1=bm[:, :], op=ALU.add),
                  reads=[ps, bm], writes=[mod])
        for r in range(2):
            for nm, sc_c, sh_c, g_c, gi in (("1", 8, 0, 16, 0), ("2", 32, 24, 40, 1)):
                gs = self.mv[("gs" + nm, r)]
                mk.op("dve", lambda e, gs=gs, sc_c=sc_c, gi=gi, r=r: e.scalar_tensor_tensor(
                    out=gs[:], in0=mod[:, sc_c:sc_c + 8, r], scalar=1.0, in1=gm[:, gi, :],
                    op0=ALU.add, op1=ALU.mult), reads=[mod, gm], writes=[gs])
                sh = self.mv[("sh" + nm, r)]
                mk.op("dve", lambda e, sh=sh, sh_c=sh_c, r=r: e.tensor_copy(sh[:], mod[:, sh_c:sh_c + 8, r]),
                      reads=[mod], writes=[sh])
                g = self.mv[("g" + nm, r)]
                mk.op("dve", lambda e, g=g, g_c=g_c, r=r: e.tensor_copy(g[:], mod[:, g_c:g_c + 8, r]),
                      reads=[mod], writes=[g])
        mk.release(m0)

    def alloc_mv(self):
        self.mv = {}
        for r in range(2):
            for nm in ("gs1", "sh1", "g1", "gs2", "sh2", "g2"):
                self.mv[(nm, r)] = self.mk.sbuf("mv_%s_%d" % (nm, r), [128, DC], F32)

    def stage_norm(self, XT, HT, which, router=None):
        mk = self.mk
        m0 = mk.mark()
        xb = [mk.sbuf("nx%d" % k, [128, DC, 512], F32) for k in range(2)]
        sq = mk.sbuf("nsq", [128, DC, 512], BF16)
        rstd = mk.sbuf("nrstd", [128, 512], F32)
        hb = [mk.sbuf("nhb%d" % k, [128, DC, 512], BF16) for k in range(2)]
        if router is not None:
            htk = [mk.sbuf("nhtk%d" % k, [128, D], BF16) for k in range(2)]
            rts = [{k: mk.sbuf("rt%d_%s" % (q, k), [128, n], F32) for k, n in
                    (("lg", 36), ("gmax", 1), ("ngmax", 1), ("ge", 4), ("gsum", 1), ("gp", 1), ("mg", 4), ("es", 8),
                     ("t8", 8), ("dd", 1), ("w2", 1), ("a1", 1), ("a2", 1), ("m1", 8), ("m2", 8), ("cw", 8),
                     ("comb", 32))} for q in range(3)]
            rtn = 0
        for bi, (t0, tn) in enumerate(BLOCKS):
            r = 1 if bi == 0 else 0
            gs, sh = self.mv[("gs" + which, r)], self.mv[("sh" + which, r)]
            buf = xb[bi % 2]
            mk.dma("sp", buf[:, :, 0:tn], XT[:, t0:t0 + tn].rearrange("(j p) t -> p j t", p=128),
                   reads=self.XTr, writes=[buf])
            self._rstd(buf, sq, rstd, tn, self.ps[0])
            h = hb[bi % 2]
            for j in range(DC):
                mk.op("dve", lambda e, j=j, buf=buf, gs=gs: e.scalar_tensor_tensor(
                    out=buf[:, j, 0:tn], in0=buf[:, j, 0:tn], scalar=gs[:, j:j + 1], in1=rstd[:, 0:tn],
                    op0=ALU.mult, op1=ALU.mult), reads=[buf, rstd, gs], writes=[buf])
                if router is not None:
                    mk.op("dve", lambda e, j=j, buf=buf, sh=sh: e.tensor_scalar(
                        out=buf[:, j, 0:tn], in0=buf[:, j, 0:tn], scalar1=sh[:, j:j + 1], scalar2=None,
                        op0=ALU.add), reads=[buf, sh], writes=[buf])
                    mk.op("act", lambda e, j=j, buf=buf, h=h: e.copy(h[:, j, 0:tn], buf[:, j, 0:tn]),
                          reads=[buf], writes=[h])
                else:
                    mk.op("act", lambda e, j=j, buf=buf, h=h, sh=sh: e.activation(
                        out=h[:, j, 0:tn], in_=buf[:, j, 0:tn], func=AF.Identity, bias=sh[:, j:j + 1]),
                        reads=[buf, sh], writes=[h])
            mk.dma("pool", HT[:, t0:t0 + tn].rearrange("(j p) t -> p j t", p=128), h[:, :, 0:tn],
                   reads=[h], writes=[HT])
            if router is not None:
                for a in range(tn // 128):
                    rt = rts[rtn % 3]
                    rtn += 1
                    if "M1a" in router:
                        self._route_tile_sparse(buf, a, t0 + a * 128, router, rt)
                        pb = self.ps[5 + (a % 2)]
                        pbb = pb[:, 0:512].bitcast(BF16)
                        for j in range(DC):
                            mk.op("pe", lambda e, pbb=pbb, j=j, a=a, h=h: e.transpose(
                                pbb[:, j * 128:(j + 1) * 128], h[:, j, a * 128:(a + 1) * 128], self.ident_b[:]),
                                reads=[h, self.ident_b], writes=[pb])
                        ht_ = htk[a % 2]
                        mk.op("act", lambda e, pbb=pbb, ht_=ht_: e.copy(ht_[:], pbb[:, :]), reads=[pb], writes=[ht_])
                        mk.dma("act", self.dram["HTOK"][t0 + a * 128:t0 + (a + 1) * 128, :], ht_[:], reads=[ht_],
                               writes=[self.dram["HTOK"]])
                    else:
                        self._route_tile(buf, a, t0 + a * 128, router, rt)
        mk.release(m0)

    def _route_tile(self, hf, a, tok0, R, rt):
        mk = self.mk
        ps = self.ps[1 + (a % 2)]
        Wr, br, combT = R["Wr"], R["br"], R["combT"]
        for k in range(DC):
            mk.op("pe", lambda e, k=k: e.matmul(ps[:, 0:36], hf[:, k, a * 128:(a + 1) * 128], Wr[:, k, :],
                                                 start=(k == 0), stop=(k == DC - 1)),
                  reads=[hf, Wr], writes=[ps])
        lg, gmax, ngmax, ge, gsum, gp, mg, es = (rt[k] for k in ("lg", "gmax", "ngmax", "ge", "gsum", "gp", "mg", "es"))
        t8, dd, w2, a1, a2, m1, m2, cw, comb = (rt[k] for k in ("t8", "dd", "w2", "a1", "a2", "m1", "m2", "cw", "comb"))
        V = lambda fn, reads, writes: mk.op("dve", fn, reads=reads, writes=writes)
        V(lambda e: e.tensor_tensor(out=lg[:], in0=ps[:, 0:36], in1=br[:], op=ALU.add), [ps, br], [lg])
        V(lambda e: e.reduce_max(out=gmax[:], in_=lg[:, 0:4], axis=AX.X), [lg], [gmax])
        V(lambda e: e.tensor_scalar(out=ngmax[:], in0=gmax[:], scalar1=-1.0, scalar2=None, op0=ALU.mult), [gmax], [ngmax])
        mk.op("act", lambda e: e.activation(out=ge[:], in_=lg[:, 0:4], func=AF.Exp, bias=ngmax[:, 0:1],
                                            accum_out=gsum[:]), reads=[lg, ngmax], writes=[ge, gsum])
        V(lambda e: e.reciprocal(gp[:], gsum[:]), [gsum], [gp])
        V(lambda e: e.tensor_scalar(out=mg[:], in0=lg[:, 0:4], scalar1=gmax[:, 0:1], scalar2=None, op0=ALU.is_equal),
          [lg, gmax], [mg])
        V(lambda e: e.tensor_scalar(out=es[:], in0=lg[:, 4:12], scalar1=mg[:, 0:1], scalar2=None, op0=ALU.mult),
          [lg, mg], [es])
        for g in range(1, 4):
            V(lambda e, g=g: e.scalar_tensor_tensor(out=es[:], in0=lg[:, 4 + 8 * g:12 + 8 * g], scalar=mg[:, g:g + 1],
                                                    in1=es[:], op0=ALU.mult, op1=ALU.add), [lg, mg, es], [es])
        V(lambda e: e.max(out=t8[:], in_=es[:]), [es], [t8])
        V(lambda e: e.tensor_tensor(out=dd[:], in0=t8[:, 1:2], in1=t8[:, 0:1], op=ALU.subtract), [t8], [dd])
        mk.op("act", lambda e: e.activation(out=w2[:], in_=dd[:], func=AF.Sigmoid), reads=[dd], writes=[w2])
        V(lambda e: e.tensor_tensor(out=a2[:], in0=w2[:], in1=gp[:], op=ALU.mult), [w2, gp], [a2])
        V(lambda e: e.tensor_tensor(out=a1[:], in0=gp[:], in1=a2[:], op=ALU.subtract), [gp, a2], [a1])
        V(lambda e: e.tensor_scalar(out=m1[:], in0=es[:], scalar1=t8[:, 0:1], scalar2=a1[:, 0:1], op0=ALU.is_equal,
                                    op1=ALU.mult), [es, t8, a1], [m1])
        V(lambda e: e.tensor_scalar(out=m2[:], in0=es[:], scalar1=t8[:, 1:2], scalar2=a2[:, 0:1], op0=ALU.is_equal,
                                    op1=ALU.mult), [es, t8, a2], [m2])
        V(lambda e: e.tensor_tensor(out=cw[:], in0=m1[:], in1=m2[:], op=ALU.add), [m1, m2], [cw])
        for g in range(4):
            V(lambda e, g=g: e.tensor_scalar(out=comb[:, 8 * g:8 * g + 8], in0=cw[:], scalar1=mg[:, g:g + 1],
                                             scalar2=None, op0=ALU.mult), [cw, mg], [comb])
        ps2 = self.ps[3 + (a % 2)]
        mk.op("pe", lambda e: e.transpose(ps2[0:32, 0:128], comb[:, :], self.ident_f[:]),
              reads=[comb, self.ident_f], writes=[ps2])
        mk.op("act", lambda e: e.copy(combT[:, tok0:tok0 + 128], ps2[0:32, 0:128]), reads=[ps2], writes=[combT])

    def stage_moe(self, i, XT, HT, HID, combT, sel, w_gate, w_up, w_down):
        mk = self.mk
        m0 = mk.mark()
        hT = mk.sbuf("moe_hT", [128, DC, NT], BF16)
        for j in range(DC):
            mk.dma("sp", hT[:, j, :], HT[j * 128:(j + 1) * 128, :], reads=[HT], writes=[hT])
        wgu = [mk.sbuf("wgu%d" % k, [128, DC, 512], BF16) for k in range(2)]
        cbs = [mk.sbuf("cbs%d" % k, [128, 512], F32) for k in range(2)]
        sl = [mk.sbuf("sl%d" % k, [128, 512], F32) for k in range(2)]
        tt = [mk.sbuf("tt%d" % k, [128, 512], F32) for k in range(2)]
        hid = [mk.sbuf("hid%d" % k, [128, 2, 512], BF16) for k in range(2)]
        n = 0
        for ex in range(NE):
            g, el = ex // 8, ex % 8
            w = wgu[ex % 2]
            mk.dma("pool", w[:, :, 0:256], w_gate[i, g, el, :, :].rearrange("(k p) f -> p k f", p=128),
                   reads=[w_gate], writes=[w])
            mk.dma("pool", w[:, :, 256:512], w_up[i, g, el, :, :].rearrange("(k p) f -> p k f", p=128),
                   reads=[w_up], writes=[w])
            for bi, (t0, tn) in enumerate(BLOCKS):
                pc = self.ps[n % 2]
                mk.op("pe", lambda e, pc=pc, ex=ex: e.matmul(pc[:, 0:tn], sel[:, ex, :], combT[:, t0:t0 + tn],
                                                            start=True, stop=True),
                      reads=[sel, combT], writes=[pc])
                cb = cbs[n % 2]
                mk.op("act", lambda e, pc=pc, cb=cb: e.copy(cb[:, 0:tn], pc[:, 0:tn]), reads=[pc], writes=[cb])
                hd = hid[n % 2]
                for fc in range(2):
                    q = (2 * n + fc) % 3
                    pg, pu = self.ps[2 + 2 * q], self.ps[3 + 2 * q]
                    for k in range(DC):
                        mk.op("pe", lambda e, pg=pg, k=k, fc=fc, w=w: e.matmul(
                            pg[:, 0:tn], w[:, k, fc * 128:(fc + 1) * 128], hT[:, k, t0:t0 + tn],
                            start=(k == 0), stop=(k == DC - 1)), reads=[w, hT], writes=[pg])
                    for k in range(DC):
                        mk.op("pe", lambda e, pu=pu, k=k, fc=fc, w=w: e.matmul(
                            pu[:, 0:tn], w[:, k, 256 + fc * 128:256 + (fc + 1) * 128], hT[:, k, t0:t0 + tn],
                            start=(k == 0), stop=(k == DC - 1)), reads=[w, hT], writes=[pu])
                    s_, t_ = sl[fc], tt[fc]
                    mk.op("act", lambda e, pg=pg, s_=s_: e.activation(out=s_[:, 0:tn], in_=pg[:, 0:tn], func=AF.Silu),
                          reads=[pg], writes=[s_])
                    mk.op("dve", lambda e, pu=pu, s_=s_, t_=t_: e.tensor_tensor(
                        out=t_[:, 0:tn], in0=pu[:, 0:tn], in1=s_[:, 0:tn], op=ALU.mult), reads=[pu, s_], writes=[t_])
                    mk.op("pool", lambda e, t_=t_, cb=cb, hd=hd, fc=fc: e.tensor_tensor(
                        out=hd[:, fc, 0:tn], in0=t_[:, 0:tn], in1=cb[:, 0:tn], op=ALU.mult), reads=[t_, cb], writes=[hd])
                mk.dma("act", HID[ex * 256:(ex + 1) * 256, t0:t0 + tn].rearrange("(c p) t -> p c t", p=128),
                       hd[:, :, 0:tn], reads=[hd], writes=[HID])
                n += 1
        mk.release(m0)
        m0 = mk.mark()
        hb = [mk.sbuf("p2h%d" % k, [128, 64, 512], BF16) for k in range(1)]
        wd = [mk.sbuf("p2w%d" % k, [128, 8, D], BF16) for k in range(2)]
        xr = [mk.sbuf("p2x%d" % k, [128, 512], F32) for k in range(3)]
        wdv = w_down[i].rearrange("g e f d -> (g e f) d")
        n = 0
        for bi, (t0, tn) in enumerate(BLOCKS):
            r = 1 if bi == 0 else 0
            g2 = self.mv[("g2", r)]
            h = hb[0]
            for c8 in range(8):
                mk.dma("sp", h[:, c8 * 8:(c8 + 1) * 8, 0:tn],
                       HID[c8 * 1024:(c8 + 1) * 1024, t0:t0 + tn].rearrange("(c p) t -> p c t", p=128),
                       reads=[HID], writes=[h])
            for kg in range(8):
                w = wd[n % 2]
                n += 1
                mk.dma("pool", w[:], wdv[kg * 1024:(kg + 1) * 1024, :].rearrange("(c p) d -> p c d", p=128),
                       reads=[w_down], writes=[w])
                for kk in range(8):
                    kc = kg * 8 + kk
                    for dc in range(DC):
                        mk.op("pe", lambda e, dc=dc, kk=kk, kc=kc, w=w: e.matmul(
                            self.ps[dc][:, 0:tn], w[:, kk, dc * 128:(dc + 1) * 128], h[:, kc, 0:tn],
                            start=(kc == 0), stop=(kc == 63)), reads=[w, h], writes=[self.ps[dc]])
            for dc in range(DC):
                x = xr[dc % 3]
                mk.dma("sp", x[:, 0:tn], XT[dc * 128:(dc + 1) * 128, t0:t0 + tn], reads=[self.XTr[dc]], writes=[x])
                mk.op("dve", lambda e, dc=dc, x=x, g2=g2: e.scalar_tensor_tensor(
                    out=x[:, 0:tn], in0=self.ps[dc][:, 0:tn], scalar=g2[:, dc:dc + 1], in1=x[:, 0:tn],
                    op0=ALU.mult, op1=ALU.add), reads=[self.ps[dc], g2, x], writes=[x])
                mk.dma("act", XT[dc * 128:(dc + 1) * 128, t0:t0 + tn], x[:, 0:tn], reads=[x], writes=[self.XTr[dc]])
        mk.release(m0)


def build_program(cfg=None):
    cfg = cfg or {}
    p = Prog(cfg)
    mk = p.mk
    layers = cfg.get("layers", list(range(DEPTH)))
    I = {}
    I["x"] = p.inp("x", [SEQ, D])
    I["ctx"] = p.inp("ctx", [CTX, D])
    I["c_pc"] = p.inp("c_pc", [128, DC, 2])
    I["w_mod"] = p.inp("w_mod", [DEPTH, D, 6 * D])
    I["bmod_pc"] = p.inp("bmod_pc", [DEPTH, 128, 48])
    I["gmix_pc"] = p.inp("gmix_pc", [DEPTH, 128, DC])
    I["gffn_pc"] = p.inp("gffn_pc", [DEPTH, 128, DC])
    I["fg_pc"] = p.inp("fg_pc", [128, DC])
    I["wr_pc"] = p.inp("wr_pc", [DEPTH, 128, DC, 36])
    I["br_bc"] = p.inp("br_bc", [DEPTH, 128, 36])
    I["sel"] = p.inp("sel", [32, NE, 128], BF16)
    sparse = cfg.get("sparse", True)
    if sparse:
        for li in range(DEPTH):
            I["moe_wgu%d" % li] = p.inp("moe_wgu%d" % li, [NE * 128, DC * 512])
            I["moe_wdn%d" % li] = p.inp("moe_wdn%d" % li, [NE * 128, 2 * D])
        I["jgrid"] = p.inp("jgrid", [128, NBLK])
        I["pidx"] = p.inp("pidx", [128, 1])
    else:
        I["moe_w_gate"] = p.inp("moe_w_gate", [DEPTH, 4, 8, D, DEXP])
        I["moe_w_up"] = p.inp("moe_w_up", [DEPTH, 4, 8, D, DEXP])
        I["moe_w_down"] = p.inp("moe_w_down", [DEPTH, 4, 8, DEXP, D])
    I["attn_w_q"] = p.inp("attn_w_q", [2, D, D])
    I["attn_w_kv"] = p.inp("attn_w_kv", [2, D, 512])
    I["attn_w_o"] = p.inp("attn_w_o", [2, D, D])
    I["qkgain_pc"] = p.inp("qkgain_pc", [2, 128, 2])
    I["conv_w_pw1"] = p.inp("conv_w_pw1", [1, D, 2 * D])
    I["conv_w_pw2"] = p.inp("conv_w_pw2", [1, D, D])
    I["conv_b_pw1_pc"] = p.inp("conv_b_pw1_pc", [1, 128, 16])
    I["conv_w_dw_pc"] = p.inp("conv_w_dw_pc", [1, 128, DC, 31])
    I["conv_b_dw_pc"] = p.inp("conv_b_dw_pc", [1, 128, DC])
    I["conv_ln_g_pc"] = p.inp("conv_ln_g_pc", [1, 128, DC])
    I["conv_ln_b_pc"] = p.inp("conv_ln_b_pc", [1, 128, DC])
    I["conv_b_pw2_pc"] = p.inp("conv_b_pw2_pc", [1, 128, DC])
    I["hy_w_in"] = p.inp("hy_w_in", [1, D, 3 * D])
    I["hy_w_out"] = p.inp("hy_w_out", [1, D, D])
    I["hy_b_in_pc"] = p.inp("hy_b_in_pc", [128, 24])
    I["hy_w_short_pc"] = p.inp("hy_w_short_pc", [128, 24, 3])
    I["hy_b_short_pc"] = p.inp("hy_b_short_pc", [128, 24])
    I["hy_b_out_pc"] = p.inp("hy_b_out_pc", [128, DC])
    I["hy_lbias_pc"] = p.inp("hy_lbias_pc", [2, 128, DC])
    I["hy_f_w1"] = p.inp("hy_f_w1", [1, 17, 64])
    I["hy_f_w2"] = p.inp("hy_f_w2", [1, 64, 64])
    I["hy_f_w3"] = p.inp("hy_f_w3", [1, 64, 4 * D])
    I["hy_fvec"] = p.inp("hy_fvec", [64, 4])
    I["hy_dabs"] = p.inp("hy_dabs", [64, D])
    for tag, L_ in (("lat", SEQ), ("ctx", CTX)):
        N1_ = 2 * L_ // 64
        I["hy_F1_" + tag] = p.inp("hy_F1_" + tag, [N1_ // 2, 6, N1_], BF16)
        I["hy_G_" + tag] = p.inp("hy_G_" + tag, [128, N1_, 5, 128], BF16)
        I["hy_Fi_" + tag] = p.inp("hy_Fi_" + tag, [N1_, 2, N1_ // 2], BF16)
        I["hy_lneg_" + tag] = p.inp("hy_lneg_" + tag, [N1_ // 2, 64])
        I["hy_featsT_" + tag] = p.inp("hy_featsT_" + tag, [17, L_])
    I["hyd_F"] = p.inp("hyd_F", [128, 2, 3, 512], BF16)
    I["hyd_Fi"] = p.inp("hyd_Fi", [128, 4, 2, CTX], BF16)
    I["hyd_lneg"] = p.inp("hyd_lneg", [128, 2])
    I["ropeR"] = p.inp("ropeR", [128, 128], BF16)
    I["ropeC"] = p.inp("ropeC", [128, SEQ])
    I["ropeS"] = p.inp("ropeS", [128, SEQ])
    y = T(p.nc.dram_tensor("y", [SEQ, D], F32, kind="ExternalOutput"), "y")
    QT = p.scratch("QT", [D, NT], BF16)
    OT = p.scratch("OT", [D, NT], BF16)
    XT = p.scratch("XT", [D, NT], F32)
    p.XTr = [T(XT.t, "XTr%d" % k) for k in range(DC)]
    VT = p.scratch("VT", [D, NT], F32)
    if 2 in layers and cfg.get("mixers", True):
        p.scratch("hyU0", [3 * D, NT], F32)
        p.scratch("hyZ", [3 * D, NT], F32)
        p.scratch("hyZ1", [D, NT], F32)
        p.scratch("hyA", [2, 128, 64, D], BF16)
        p.scratch("hyC", [2, 64, 128, D], BF16)
        for o in range(2):
            p.scratch("hyHH_lat%d" % o, [2, 128, 128, D], BF16)
            p.scratch("hyHH_ctx%d" % o, [2, 8, 128, D], BF16)
    HT = p.scratch("HT", [D, NT], BF16)
    p.dram["HT"] = HT
    if sparse:
        p.scratch("HTOK", [NT, D], BF16)
        p.scratch("HS", [NSLOT, D], BF16)
        p.scratch("YS", [NSLOT, D], F32)
    else:
        HID = p.scratch("HID", [NE * DEXP, NT], BF16)
    p.setup_consts()
    p.alloc_mv()
    fg = mk.sbuf("fg", [128, DC], F32)
    sT = mk.sbuf("sT", [128, DC, 2], F32)
    sel = mk.sbuf("sel", [32, NE, 128], BF16)
    if sparse:
        SR = p.alloc_sparse()
        mk.dma("sp", SR["jg"][:], I["jgrid"][:, :], writes=[SR["jg"]])
        mk.dma("sp", SR["pidx"][:], I["pidx"][:, :], writes=[SR["pidx"]])
    else:
        combT = mk.sbuf("combT", [32, NT], BF16)
    Wr = mk.sbuf("Wr", [128, DC, 36], F32)
    br = mk.sbuf("br", [128, 36], F32)
    mk.dma("sp", fg[:], I["fg_pc"][:, :], reads=[I["fg_pc"]], writes=[fg])
    mk.dma("sp", sT[:], I["c_pc"][:, :, :], reads=[I["c_pc"]], writes=[sT])
    mk.dma("sp", sel[:], I["sel"][:, :, :], reads=[I["sel"]], writes=[sel])
    mk.op("act", lambda e: e.activation(out=sT[:], in_=sT[:], func=AF.Silu), reads=[sT], writes=[sT])
    p.stage_input(I["x"], I["ctx"], XT)
    for i in layers:
        p.stage_mod(i, sT, I["w_mod"], I["bmod_pc"], I["gmix_pc"], I["gffn_pc"])
        if cfg.get("mixers", True):
            kind, slot = i % 3, i // 3
            with_ctx = i < DEPTH - 1
            p.stage_norm(XT, HT, "1")
            if kind == 0:
                p.stage_attn(slot, XT, HT, QT, OT, I, with_ctx)
            elif kind == 1:
                p.stage_conf(slot, XT, HT, QT, VT, I)
            else:
                p.stage_hyena(slot, XT, HT, I)
        if cfg.get("moe", True) is False:
            continue
        mk.dma("sp", Wr[:], I["wr_pc"][i, :, :, :], reads=[I["wr_pc"]], writes=[Wr])
        mk.dma("sp", br[:], I["br_bc"][i, :, :], reads=[I["br_bc"]], writes=[br])
        if sparse:
            SR["Wr"], SR["br"] = Wr, br
            p.stage_norm(XT, HT, "2", router=SR)
            p.stage_moe_sparse(i, XT, SR, I)
        else:
            p.stage_norm(XT, HT, "2", router=dict(Wr=Wr, br=br, combT=combT))
            p.stage_moe(i, XT, HT, HID, combT, sel, I["moe_w_gate"], I["moe_w_up"], I["moe_w_down"])
    p.stage_output(XT, y, fg)
    mk.finish()
    mk.close()
    return p


_HYC = {}


def host_inputs(inp, b, sparse=True):
    f = lambda a: np.ascontiguousarray(np.asarray(a, np.float32))
    m = {}
    m["x"] = f(inp["x"][b])
    m["ctx"] = f(inp["ctx"][b])
    m["c_pc"] = np.ascontiguousarray(np.stack([_vec_pc(inp["c"][b]), _vec_pc(inp["c_ctx"])], axis=-1))
    m["w_mod"] = f(inp["w_mod"])
    m["bmod_pc"] = np.stack([_vec_pc(inp["b_mod"][i]) for i in range(DEPTH)])
    m["gmix_pc"] = np.stack([_vec_pc(inp["norm_mix_g"][i]) for i in range(DEPTH)])
    m["gffn_pc"] = np.stack([_vec_pc(inp["norm_ffn_g"][i]) for i in range(DEPTH)])
    m["fg_pc"] = _vec_pc(inp["final_norm_g"])
    wr = np.concatenate([np.asarray(inp["moe_w_group"]), np.asarray(inp["moe_w_router"])], axis=-1)
    m["wr_pc"] = np.ascontiguousarray(wr.reshape(DEPTH, DC, 128, 36).transpose(0, 2, 1, 3)).astype(np.float32)
    brr = np.concatenate([np.asarray(inp["moe_b_group"]), np.asarray(inp["moe_b_router"])], axis=-1)
    m["br_bc"] = np.ascontiguousarray(np.broadcast_to(brr[:, None, :], (DEPTH, 128, 36))).astype(np.float32)
    sel = np.zeros((32, NE, 128), np.float32)
    for e in range(NE):
        sel[e, e, :] = 1.0
    m["sel"] = sel.astype(ml_dtypes.bfloat16)
    m["attn_w_q"] = f(inp["attn_w_q"]); m["attn_w_kv"] = f(inp["attn_w_kv"]); m["attn_w_o"] = f(inp["attn_w_o"])
    m["qkgain_pc"] = np.ascontiguousarray(np.stack([np.asarray(inp["attn_q_gain"], np.float32),
                                                    np.asarray(inp["attn_k_gain"], np.float32)], axis=-1))
    m["conv_w_pw1"] = f(inp["conv_w_pw1"]); m["conv_w_pw2"] = f(inp["conv_w_pw2"])
    m["conv_b_pw1_pc"] = _vec_pc(inp["conv_b_pw1"][0])[None]
    m["conv_w_dw_pc"] = np.ascontiguousarray(np.asarray(inp["conv_w_dw"][0], np.float32).reshape(31, DC, 128).transpose(2, 1, 0))[None]
    for nm in ("conv_b_dw", "conv_ln_g", "conv_ln_b", "conv_b_pw2"):
        m[nm + "_pc"] = _vec_pc(inp[nm][0])[None]
    m["hy_w_in"] = f(inp["hy_w_in"]); m["hy_w_out"] = f(inp["hy_w_out"])
    m["hy_b_in_pc"] = _vec_pc(inp["hy_b_in"][0]); m["hy_b_short_pc"] = _vec_pc(inp["hy_b_short"][0])
    m["hy_w_short_pc"] = np.ascontiguousarray(np.stack([_vec_pc(inp["hy_w_short"][0, k]) for k in range(3)], axis=-1))
    m["hy_b_out_pc"] = _vec_pc(inp["hy_b_out"][0])
    m["hy_lbias_pc"] = np.stack([_vec_pc(inp["hy_long_bias"][0, o]) for o in range(2)])
    m["hy_f_w1"] = f(inp["hy_f_w1"]); m["hy_f_w2"] = f(inp["hy_f_w2"]); m["hy_f_w3"] = f(inp["hy_f_w3"])
    m["hy_fvec"] = np.ascontiguousarray(np.stack([np.asarray(inp[k][0], np.float32) for k in
                                                  ("hy_f_b1", "hy_f_freq1", "hy_f_b2", "hy_f_freq2")], axis=-1))
    dl = np.abs(np.linspace(math.log(1e-2) / 1.5, math.log(1e-2) / 0.3, D, dtype=np.float32))
    m["hy_dabs"] = np.ascontiguousarray(np.broadcast_to(dl[None, :], (64, D))).astype(np.float32)
    for tag, L_ in (("lat", SEQ), ("ctx", CTX)):
        hc = _HYC[tag] if tag in _HYC else _HYC.setdefault(tag, hy_consts(L_))
        m["hy_F1_" + tag] = hc["F1"]; m["hy_G_" + tag] = hc["G"]; m["hy_Fi_" + tag] = hc["Fi"]
        m["hy_lneg_" + tag] = hc["lneg"]; m["hy_featsT_" + tag] = hc["featsT"]
    hdc = _HYC["hyd"] if "hyd" in _HYC else _HYC.setdefault("hyd", hyd_consts())
    m["hyd_F"] = hdc["F"]; m["hyd_Fi"] = hdc["Fi"]; m["hyd_lneg"] = hdc["lneg"]
    RT = np.zeros((128, 128), np.float32)
    for base in (0, 64):
        for dd in range(32):
            RT[base + dd + 32, base + dd] = -1.0
            RT[base + dd, base + dd + 32] = 1.0
    m["ropeR"] = RT.astype(ml_dtypes.bfloat16)
    inv = (np.float32(10000.0) ** (-np.arange(32, dtype=np.float32) / np.float32(32))).astype(np.float32)
    tt = np.arange(SEQ)
    ang = np.zeros((128, SEQ), np.float32)
    for dd in range(128):
        pos = (tt // 64) if dd < 64 else (tt % 64)
        ang[dd] = pos.astype(np.float32) * inv[dd % 32]
    m["ropeC"] = np.cos(ang).astype(np.float32)
    m["ropeS"] = np.sin(ang).astype(np.float32)
    if sparse:
        wg = np.asarray(inp["moe_w_gate"], np.float32).reshape(DEPTH, NE, DC, 128, DEXP)
        wu = np.asarray(inp["moe_w_up"], np.float32).reshape(DEPTH, NE, DC, 128, DEXP)
        wd = np.asarray(inp["moe_w_down"], np.float32).reshape(DEPTH, NE, 2, 128, D)
        for li in range(DEPTH):
            m["moe_wgu%d" % li] = np.ascontiguousarray(np.concatenate([wg[li], wu[li]], axis=-1).transpose(0, 2, 1, 3)).reshape(NE * 128, DC * 512)
            m["moe_wdn%d" % li] = np.ascontiguousarray(wd[li].transpose(0, 2, 1, 3)).reshape(NE * 128, 2 * D)
        m["jgrid"] = np.ascontiguousarray(np.broadcast_to((np.arange(NBLK, dtype=np.float32) * BSL)[None, :], (128, NBLK)))
        m["pidx"] = np.arange(128, dtype=np.float32)[:, None].copy()
    else:
        m["moe_w_gate"] = f(inp["moe_w_gate"])
        m["moe_w_up"] = f(inp["moe_w_up"])
        m["moe_w_down"] = f(inp["moe_w_down"])
    return m


def _attn_methods():
    def load_w(self, name, src_ap, kc, m, q="pool"):
        mk = self.mk
        w = mk.sbuf(name, [128, kc, m], BF16)
        step = max(1, 4096 // m)
        for k0 in range(0, kc, step):
            k1 = min(kc, k0 + step)
            mk.dma(q, w[:, k0:k1, :], src_ap[k0 * 128:k1 * 128, :].rearrange("(k p) m -> p k m", p=128), writes=[w])
        return w

    def linear_resid(self, XT, srcT, W, KC, gname, bias=None, skip_ctx=False):
        mk = self.mk
        m0 = mk.mark()
        sb = [mk.sbuf("lr_s%d" % k, [128, KC, 512], BF16) for k in range(2)]
        xr = [mk.sbuf("lr_x%d" % k, [128, 512], F32) for k in range(3)]
        gb = None
        if bias is not None:
            gb = [mk.sbuf("lr_gb%d" % r, [128, DC], F32) for r in range(2)]
            for r in range(2):
                mk.op("dve", lambda e, r=r: e.tensor_tensor(out=gb[r][:], in0=bias[:], in1=self.mv[(gname, r)][:],
                                                            op=ALU.mult), reads=[bias, self.mv[(gname, r)]], writes=[gb[r]])
        n = 0
        for bi, (t0, tn) in enumerate(BLOCKS):
            if skip_ctx and bi == 0:
                continue
            r = 1 if bi == 0 else 0
            g = self.mv[(gname, r)]
            s = sb[bi % 2]
            mk.dma("sp", s[:, :, 0:tn], srcT[:, t0:t0 + tn].rearrange("(k p) t -> p k t", p=128),
                   reads=[srcT], writes=[s])
            for dc in range(DC):
                ps = self.ps[n % 4]
                x = xr[n % 3]
                n += 1
                mk.dma("sp", x[:, 0:tn], XT[dc * 128:(dc + 1) * 128, t0:t0 + tn], reads=[self.XTr[dc]], writes=[x])
                for k in range(KC):
                    mk.op("pe", lambda e, ps=ps, k=k, dc=dc, s=s: e.matmul(
                        ps[:, 0:tn], W[:, k, dc * 128:(dc + 1) * 128], s[:, k, 0:tn],
                        start=(k == 0), stop=(k == KC - 1)), reads=[W, s], writes=[ps])
                mk.op("dve", lambda e, ps=ps, dc=dc, x=x, g=g: e.scalar_tensor_tensor(
                    out=x[:, 0:tn], in0=ps[:, 0:tn], scalar=g[:, dc:dc + 1], in1=x[:, 0:tn],
                    op0=ALU.mult, op1=ALU.add), reads=[ps, g, x], writes=[x])
                if gb is not None:
                    mk.op("dve", lambda e, dc=dc, x=x, r=r: e.tensor_scalar(
                        out=x[:, 0:tn], in0=x[:, 0:tn], scalar1=gb[r][:, dc:dc + 1], scalar2=None, op0=ALU.add),
                        reads=[x, gb[r]], writes=[x])
                mk.dma("act", XT[dc * 128:(dc + 1) * 128, t0:t0 + tn], x[:, 0:tn], reads=[x], writes=[self.XTr[dc]])
        mk.release(m0)

    def stage_attn(self, slot, XT, HT, QT, OT, I, with_ctx):
        mk = self.mk
        mA = mk.mark()
        KT = mk.sbuf("KT", [128, 2, NT], BF16)
        Vs = mk.sbuf("Vs", [128, NT // 128, 256], BF16)
        gains = mk.sbuf("qkgain", [128, 2], F32)
        RT = mk.sbuf("RT", [128, 128], BF16)
        avgh = mk.sbuf("avgh", [128, 128], BF16)
        mk.dma("sp", gains[:], I["qkgain_pc"][slot, :, :], writes=[gains])
        mk.dma("sp", RT[:], I["ropeR"][:, :], writes=[RT])
        mk.op("pool", lambda e: e.memset(avgh[:], 1.0 / 128), writes=[avgh])
        m0 = mk.mark()
        wq = self.load_w("wq", I["attn_w_q"][slot], DC, D)
        wkv = self.load_w("wkv", I["attn_w_kv"][slot], DC, 512)
        hb = [mk.sbuf("ah%d" % k, [128, DC, 512], BF16) for k in range(2)]
        cs = [mk.sbuf("acs%d" % k, [128, 2, 512], F32) for k in range(2)]
        sqq = [mk.sbuf("asq%d" % k, [128, 512], BF16) for k in range(2)]
        rs = [mk.sbuf("ars%d" % k, [128, 512], F32) for k in range(2)]
        qn = [mk.sbuf("aqn%d" % k, [128, 512], F32) for k in range(2)]
        qb = [mk.sbuf("aqb%d" % k, [128, 512], BF16) for k in range(2)]
        t1 = [mk.sbuf("at1%d" % k, [128, 512], F32) for k in range(2)]
        t2 = [mk.sbuf("at2%d" % k, [128, 512], F32) for k in range(2)]
        qo = [mk.sbuf("aqo%d" % k, [128, 512], BF16) for k in range(3)]
        n = 0
        for bi, (t0, tn) in enumerate(BLOCKS):
            h = hb[bi % 2]
            mk.dma("sp", h[:, :, 0:tn], HT[:, t0:t0 + tn].rearrange("(k p) t -> p k t", p=128), reads=[HT], writes=[h])
            c = cs[bi % 2]
            if bi > 0:
                mk.dma("sp", c[:, 0, 0:tn], I["ropeC"][:, t0 - CTX:t0 - CTX + tn], writes=[c])
                mk.dma("sp", c[:, 1, 0:tn], I["ropeS"][:, t0 - CTX:t0 - CTX + tn], writes=[c])
            for hh in range(10):
                isq = hh < 8
                if isq and bi == 0 and not with_ctx:
                    continue
                W = wq if isq else wkv
                c0 = hh * 128 if isq else (hh - 8) * 128
                gcol = 0 if isq else 1
                ps = self.ps[n % 2]
                pz = self.ps[2 + n % 2]
                pr = self.ps[4 + n % 2]
                i2 = n % 2
                n += 1
                for k in range(DC):
                    mk.op("pe", lambda e, ps=ps, k=k, W=W, c0=c0, h=h: e.matmul(
                        ps[:, 0:tn], W[:, k, c0:c0 + 128], h[:, k, 0:tn], start=(k == 0), stop=(k == DC - 1)),
                        reads=[W, h], writes=[ps])
                mk.op("act", lambda e, ps=ps, i2=i2: e.activation(out=sqq[i2][:, 0:tn], in_=ps[:, 0:tn], func=AF.Square),
                      reads=[ps], writes=[sqq[i2]])
                mk.op("pe", lambda e, pz=pz, i2=i2: e.matmul(pz[:, 0:tn], avgh[:], sqq[i2][:, 0:tn], start=True, stop=True),
                      reads=[avgh, sqq[i2]], writes=[pz])
                mk.op("act", lambda e, pz=pz, i2=i2: e.activation(out=rs[i2][:, 0:tn], in_=pz[:, 0:tn], func=AF.Sqrt,
                                                                 bias=self.eps_t[:, 0:1]), reads=[pz, self.eps_t], writes=[rs[i2]])
                mk.op("dve", lambda e, i2=i2: e.reciprocal(rs[i2][:, 0:tn], rs[i2][:, 0:tn]), reads=[rs[i2]], writes=[rs[i2]])
                mk.op("dve", lambda e, ps=ps, i2=i2, gcol=gcol: e.scalar_tensor_tensor(
                    out=qn[i2][:, 0:tn], in0=ps[:, 0:tn], scalar=gains[:, gcol:gcol + 1], in1=rs[i2][:, 0:tn],
                    op0=ALU.mult, op1=ALU.mult), reads=[ps, gains, rs[i2]], writes=[qn[i2]])
                if isq:
                    dst_t = qo[n % 3]
                    dst = dst_t[:, 0:tn]
                else:
                    dst_t = KT
                    dst = KT[:, hh - 8, t0:t0 + tn]
                if bi == 0:
                    mk.op("act", lambda e, i2=i2, dst=dst: e.copy(dst, qn[i2][:, 0:tn]), reads=[qn[i2]], writes=[dst_t])
                else:
                    mk.op("act", lambda e, i2=i2: e.copy(qb[i2][:, 0:tn], qn[i2][:, 0:tn]), reads=[qn[i2]], writes=[qb[i2]])
                    mk.op("pe", lambda e, pr=pr, i2=i2: e.matmul(pr[:, 0:tn], RT[:], qb[i2][:, 0:tn], start=True, stop=True),
                          reads=[RT, qb[i2]], writes=[pr])
                    mk.op("pool", lambda e, i2=i2, c=c: e.tensor_tensor(out=t1[i2][:, 0:tn], in0=qn[i2][:, 0:tn],
                                                                       in1=c[:, 0, 0:tn], op=ALU.mult),
                          reads=[qn[i2], c], writes=[t1[i2]])
                    mk.op("dve", lambda e, pr=pr, i2=i2, c=c: e.tensor_tensor(out=t2[i2][:, 0:tn], in0=pr[:, 0:tn],
                                                                             in1=c[:, 1, 0:tn], op=ALU.mult),
                          reads=[pr, c], writes=[t2[i2]])
                    mk.op("pool", lambda e, i2=i2, dst=dst: e.tensor_tensor(out=dst, in0=t1[i2][:, 0:tn],
                                                                           in1=t2[i2][:, 0:tn], op=ALU.add),
                          reads=[t1[i2], t2[i2]], writes=[dst_t])
                if isq:
                    mk.dma("act", QT[hh * 128:(hh + 1) * 128, t0:t0 + tn], dst, reads=[dst_t], writes=[QT])
            for a in range(tn // 128):
                pv = self.ps[6 + a % 2]
                for k in range(DC):
                    mk.op("pe", lambda e, pv=pv, k=k, a=a, h=h: e.matmul(
                        pv[:, 0:256], h[:, k, a * 128:(a + 1) * 128], wkv[:, k, 256:512],
                        start=(k == 0), stop=(k == DC - 1)), reads=[h, wkv], writes=[pv])
                ti = t0 // 128 + a
                mk.op("act", lambda e, pv=pv, ti=ti: e.copy(Vs[:, ti, :], pv[:, 0:256]), reads=[pv], writes=[Vs])
        mk.release(m0)
        m0 = mk.mark()
        qblk = [mk.sbuf("bq%d" % k, [128, 512], BF16) for k in range(2)]
        pT = [mk.sbuf("bp%d" % k, [128, 512], BF16) for k in range(4)]
        rz = [mk.sbuf("brz%d" % k, [128, 512], F32) for k in range(2)]
        zacc = [mk.sbuf("bza%d" % k, [128, 512], F32) for k in range(2)]
        ones_f = mk.sbuf("bones_f", [128, 128], F32)
        mk.op("pool", lambda e: e.memset(ones_f[:], 1.0), writes=[ones_f])
        ob = [mk.sbuf("bo%d" % k, [128, 512], BF16) for k in range(2)]
        scale = 128 ** -0.5
        nb = 0
        nk = 0
        for head in range(8):
            kvh = head // 4
            for bi, (t0, tn) in enumerate(BLOCKS):
                if bi == 0 and not with_ctx:
                    continue
                nkc = 2 if bi == 0 else NT // 128
                q = qblk[nb % 2]
                mk.dma("sp", q[:, 0:tn], QT[head * 128:(head + 1) * 128, t0:t0 + tn], reads=[QT], writes=[q])
                pO = self.ps[4 + nb % 2]
                pZ = self.ps[6 + nb % 2]
                def issue_S(kc_, idx):
                    pS_ = self.ps[idx % 4]
                    mk.op("pe", lambda e, pS_=pS_, kc_=kc_, q=q: e.matmul(
                        pS_[:, 0:tn], KT[:, kvh, kc_ * 128:(kc_ + 1) * 128], q[:, 0:tn], start=True, stop=True),
                        reads=[KT, q], writes=[pS_])
                issue_S(0, nk)
                if nkc > 1:
                    issue_S(1, nk + 1)
                for kc in range(nkc):
                    pS = self.ps[nk % 4]
                    p_ = pT[nk % 4]
                    if kc + 2 < nkc:
                        issue_S(kc + 2, nk + 2)
                    nk += 1
                    mk.op("act", lambda e, pS=pS, p_=p_: e.activation(out=p_[:, 0:tn], in_=pS[:, 0:tn], func=AF.Exp,
                                                                     scale=scale), reads=[pS], writes=[p_])
                    mk.op("pe", lambda e, pO=pO, kc=kc, p_=p_: e.matmul(
                        pO[:, 0:tn], Vs[:, kc, kvh * 128:(kvh + 1) * 128], p_[:, 0:tn],
                        start=(kc == 0), stop=(kc == nkc - 1)), reads=[Vs, p_], writes=[pO])
                    mk.op("pe", lambda e, pZ=pZ, kc=kc, p_=p_: e.matmul(
                        pZ[:, 0:tn], self.ones_b[:], p_[:, 0:tn], start=(kc == 0), stop=(kc == nkc - 1)),
                        reads=[self.ones_b, p_], writes=[pZ])
                r_ = rz[nb % 2]
                o_ = ob[nb % 2]
                mk.op("dve", lambda e, pZ=pZ, r_=r_: e.reciprocal(r_[:, 0:tn], pZ[:, 0:tn]), reads=[pZ], writes=[r_])
                mk.op("dve", lambda e, pO=pO, r_=r_, o_=o_: e.tensor_tensor(out=o_[:, 0:tn], in0=pO[:, 0:tn],
                                                                           in1=r_[:, 0:tn], op=ALU.mult),
                      reads=[pO, r_], writes=[o_])
                mk.dma("act", OT[head * 128:(head + 1) * 128, t0:t0 + tn], o_[:, 0:tn], reads=[o_], writes=[OT])
                nb += 1
        mk.release(m0)
        mk.release(mA)
        m0 = mk.mark()
        wo = self.load_w("wo", I["attn_w_o"][slot], DC, D)
        self.linear_resid(XT, OT, wo, DC, "g1", skip_ctx=not with_ctx)
        mk.release(m0)

    Prog.load_w = load_w
    Prog.linear_resid = linear_resid
    Prog.stage_attn = stage_attn


_attn_methods()


def _conf_methods():
    def stage_conf(self, slot, XT, HT, UT, VT, I):
        mk = self.mk
        m0 = mk.mark()
        W = self.load_w("wpw1", I["conv_w_pw1"][slot], DC, 2 * D)
        b1 = mk.sbuf("cb1", [128, 16], F32)
        mk.dma("sp", b1[:], I["conv_b_pw1_pc"][slot, :, :], writes=[b1])
        hb = [mk.sbuf("ch%d" % k, [128, DC, 512], BF16) for k in range(2)]
        sg = [mk.sbuf("csg%d" % k, [128, 512], F32) for k in range(2)]
        ub = [mk.sbuf("cu%d" % k, [128, DC, 512], BF16) for k in range(2)]
        n = 0
        for bi, (t0, tn) in enumerate(BLOCKS):
            h = hb[bi % 2]
            u = ub[bi % 2]
            mk.dma("sp", h[:, :, 0:tn], HT[:, t0:t0 + tn].rearrange("(k p) t -> p k t", p=128), reads=[HT], writes=[h])
            for j in range(DC):
                pa, pg = self.ps[(2 * n) % 8], self.ps[(2 * n + 1) % 8]
                s_ = sg[n % 2]
                n += 1
                for k in range(DC):
                    mk.op("pe", lambda e, pa=pa, k=k, j=j, h=h: e.matmul(
                        pa[:, 0:tn], W[:, k, j * 128:(j + 1) * 128], h[:, k, 0:tn], start=(k == 0), stop=(k == DC - 1)),
                        reads=[W, h], writes=[pa])
                for k in range(DC):
                    mk.op("pe", lambda e, pg=pg, k=k, j=j, h=h: e.matmul(
                        pg[:, 0:tn], W[:, k, D + j * 128:D + (j + 1) * 128], h[:, k, 0:tn], start=(k == 0), stop=(k == DC - 1)),
                        reads=[W, h], writes=[pg])
                mk.op("act", lambda e, pg=pg, s_=s_, j=j: e.activation(out=s_[:, 0:tn], in_=pg[:, 0:tn], func=AF.Sigmoid,
                                                                      bias=b1[:, 8 + j:9 + j]), reads=[pg, b1], writes=[s_])
                mk.op("dve", lambda e, pa=pa, s_=s_, j=j, u=u: e.scalar_tensor_tensor(
                    out=u[:, j, 0:tn], in0=pa[:, 0:tn], scalar=b1[:, j:j + 1], in1=s_[:, 0:tn], op0=ALU.add, op1=ALU.mult),
                    reads=[pa, b1, s_], writes=[u])
            mk.dma("act", UT[:, t0:t0 + tn].rearrange("(k p) t -> p k t", p=128), u[:, :, 0:tn], reads=[u], writes=[UT])
        mk.release(m0)
        m0 = mk.mark()
        wdw = mk.sbuf("cwdw", [128, DC, 31], F32)
        bdw = mk.sbuf("cbdw", [128, DC], F32)
        mk.dma("sp", wdw[:], I["conv_w_dw_pc"][slot, :, :, :], writes=[wdw])
        mk.dma("sp", bdw[:], I["conv_b_dw_pc"][slot, :, :], writes=[bdw])
        up = [mk.sbuf("cup%d" % k, [128, NT + 60], BF16) for k in range(2)]
        dg = [mk.sbuf("cdg%d" % k, [128, 31, 128], BF16) for k in range(2)]
        vo = [mk.sbuf("cvo%d" % k, [128, 512], F32) for k in range(3)]
        for k in range(2):
            mk.op("pool", lambda e, k=k: e.memset(up[k][:], 0.0), writes=[up[k]])
        n = 0
        for j in range(DC):
            u = up[j % 2]
            d_ = dg[j % 2]
            mk.dma("sp", u[:, 15:15 + CTX], UT[j * 128:(j + 1) * 128, 0:CTX], reads=[UT], writes=[u])
            mk.dma("sp", u[:, 45 + CTX:45 + CTX + SEQ], UT[j * 128:(j + 1) * 128, CTX:NT], reads=[UT], writes=[u])
            for k in range(31):
                mk.op("dve", lambda e, k=k, j=j, d_=d_: e.tensor_scalar(
                    out=d_[:, k, :], in0=self.ident_b[:], scalar1=wdw[:, j, k:k + 1], scalar2=None, op0=ALU.mult),
                    reads=[self.ident_b, wdw], writes=[d_])
            for bi, (t0, tn) in enumerate(BLOCKS):
                base = 0 if bi == 0 else 30
                ps = self.ps[n % 4]
                v = vo[n % 3]
                n += 1
                for k in range(31):
                    mk.op("pe", lambda e, ps=ps, k=k, u=u, d_=d_, s0=base + t0 + k: e.matmul(
                        ps[:, 0:tn], d_[:, k, :], u[:, s0:s0 + tn], start=(k == 0), stop=(k == 30)),
                        reads=[d_, u], writes=[ps])
                mk.op("act", lambda e, ps=ps, v=v, j=j: e.activation(out=v[:, 0:tn], in_=ps[:, 0:tn], func=AF.Identity,
                                                                    bias=bdw[:, j:j + 1]), reads=[ps, bdw], writes=[v])
                mk.dma("act", VT[j * 128:(j + 1) * 128, t0:t0 + tn], v[:, 0:tn], reads=[v], writes=[VT])
        mk.release(m0)
        m0 = mk.mark()
        lng = mk.sbuf("clng", [128, DC], F32)
        lnb = mk.sbuf("clnb", [128, DC], F32)
        mk.dma("sp", lng[:], I["conv_ln_g_pc"][slot, :, :], writes=[lng])
        mk.dma("sp", lnb[:], I["conv_ln_b_pc"][slot, :, :], writes=[lnb])
        avgf = mk.sbuf("cavgf", [128, 128], F32)
        mk.op("pool", lambda e: e.memset(avgf[:], 1.0 / D), writes=[avgf])
        vb = [mk.sbuf("cv%d" % k, [128, DC, 512], F32) for k in range(2)]
        sq = mk.sbuf("csq", [128, DC, 512], F32)
        mu = mk.sbuf("cmu", [128, 512], F32)
        var = mk.sbuf("cvar", [128, 512], F32)
        ob = [mk.sbuf("co%d" % k, [128, DC, 512], BF16) for k in range(2)]
        for bi, (t0, tn) in enumerate(BLOCKS):
            v = vb[bi % 2]
            o = ob[bi % 2]
            mk.dma("sp", v[:, :, 0:tn], VT[:, t0:t0 + tn].rearrange("(k p) t -> p k t", p=128), reads=[VT], writes=[v])
            pm, pq = self.ps[0], self.ps[1]
            for j in range(DC):
                mk.op("act", lambda e, j=j, v=v: e.activation(out=sq[:, j, 0:tn], in_=v[:, j, 0:tn], func=AF.Square),
                      reads=[v], writes=[sq])
            for j in range(DC):
                mk.op("pe", lambda e, j=j, v=v: e.matmul(pm[:, 0:tn], avgf[:], v[:, j, 0:tn], start=(j == 0), stop=(j == DC - 1)),
                      reads=[avgf, v], writes=[pm])
            for j in range(DC):
                mk.op("pe", lambda e, j=j: e.matmul(pq[:, 0:tn], avgf[:], sq[:, j, 0:tn], start=(j == 0), stop=(j == DC - 1)),
                      reads=[avgf, sq], writes=[pq])
            mk.op("act", lambda e: e.copy(mu[:, 0:tn], pm[:, 0:tn]), reads=[pm], writes=[mu])
            mk.op("dve", lambda e: e.tensor_tensor(out=var[:, 0:tn], in0=mu[:, 0:tn], in1=mu[:, 0:tn], op=ALU.mult),
                  reads=[mu], writes=[var])
            mk.op("dve", lambda e: e.tensor_tensor(out=var[:, 0:tn], in0=pq[:, 0:tn], in1=var[:, 0:tn], op=ALU.subtract),
                  reads=[pq, var], writes=[var])
            mk.op("act", lambda e: e.activation(out=var[:, 0:tn], in_=var[:, 0:tn], func=AF.Sqrt, bias=self.eps_t[:, 0:1]),
                  reads=[var, self.eps_t], writes=[var])
            mk.op("dve", lambda e: e.reciprocal(var[:, 0:tn], var[:, 0:tn]), reads=[var], writes=[var])
            for j in range(DC):
                mk.op("pool", lambda e, j=j, v=v: e.tensor_tensor(out=v[:, j, 0:tn], in0=v[:, j, 0:tn], in1=mu[:, 0:tn],
                                                                 op=ALU.subtract), reads=[v, mu], writes=[v])
                mk.op("dve", lambda e, j=j, v=v: e.scalar_tensor_tensor(
                    out=v[:, j, 0:tn], in0=v[:, j, 0:tn], scalar=lng[:, j:j + 1], in1=var[:, 0:tn], op0=ALU.mult, op1=ALU.mult),
                    reads=[v, lng, var], writes=[v])
                mk.op("act", lambda e, j=j, v=v, o=o: e.activation(out=o[:, j, 0:tn], in_=v[:, j, 0:tn], func=AF.Silu,
                                                                  bias=lnb[:, j:j + 1]), reads=[v, lnb], writes=[o])
            mk.dma("act", HT[:, t0:t0 + tn].rearrange("(k p) t -> p k t", p=128), o[:, :, 0:tn], reads=[o], writes=[HT])
        mk.release(m0)
        m0 = mk.mark()
        w2 = self.load_w("wpw2", I["conv_w_pw2"][slot], DC, D)
        b2 = mk.sbuf("cb2", [128, DC], F32)
        mk.dma("sp", b2[:], I["conv_b_pw2_pc"][slot, :, :], writes=[b2])
        self.linear_resid(XT, HT, w2, DC, "g1", bias=b2)
        mk.release(m0)

    Prog.stage_conf = stage_conf


_conf_methods()


def hy_consts(L):
    N = 2 * L
    N1 = N // 64
    H = N1 // 2
    bf = ml_dtypes.bfloat16
    c = {}
    n1 = np.arange(H)[:, None].astype(np.float64)
    k1 = np.arange(N1)[None, :].astype(np.float64)
    def cs(ang):
        return np.cos(ang), -np.sin(ang)
    fc, fs = cs(2 * np.pi * n1 * k1 / N1)
    bc, bs = cs(2 * np.pi * (N1 - 1 - n1) * k1 / N1)
    b0c, b0s = cs(2 * np.pi * (N1 - n1) * k1 / N1)
    c["F1"] = np.stack([fc, fs, bc, bs, b0c, b0s], 1).astype(bf)
    n2 = np.arange(64)[:, None].astype(np.float64)
    k2 = np.arange(64)[None, :].astype(np.float64)
    G = np.zeros((N1, 128, 5, 128), np.float64)
    for kk in range(N1):
        ang = 2 * np.pi * (n2 * kk / N + n2 * k2 / 64)
        Gr, Gi = np.cos(ang), -np.sin(ang)
        Mr, Mi = Gr.T, -Gi.T
        blocks = [((Gr, Gi), (-Gi, Gr)), ((-Gi, Gr), (-Gr, -Gi)), ((Gr, Gr), (-Gi, -Gi)),
                  ((Gi, Gi), (Gr, Gr)), ((Mr, Mi), (-Mi, Mr))]
        for f, ((a, b), (cc, d)) in enumerate(blocks):
            G[kk, 0:64, f, 0:64] = a
            G[kk, 0:64, f, 64:128] = b
            G[kk, 64:128, f, 0:64] = cc
            G[kk, 64:128, f, 64:128] = d
    c["G"] = np.ascontiguousarray(G.transpose(1, 0, 2, 3)).astype(bf)
    t1 = np.arange(H)[None, :].astype(np.float64)
    kk1 = np.arange(N1)[:, None].astype(np.float64)
    th = 2 * np.pi * t1 * kk1 / N1
    c["Fi"] = np.stack([np.cos(th) / N, -np.sin(th) / N], 1).astype(bf)
    pos = (64 * np.arange(H)[:, None] + np.arange(64)[None, :]).astype(np.float32)
    c["lneg"] = (-(pos / np.float32(L - 1))).astype(np.float32)
    p = np.arange(L, dtype=np.float32)[:, None]
    t = p / np.float32(L - 1)
    w = np.float32(2.0 * math.pi) * p / np.float32(L)
    bands = np.linspace(1e-4, 7, 8, dtype=np.float32)
    feats = np.concatenate([t, np.cos(bands * w), -np.sin(bands * w)], axis=-1).astype(np.float32)
    c["featsT"] = np.ascontiguousarray(feats.T)
    return c


def _hy_methods():
    def hy_filter(self, S, I, HH):
        mk = self.mk
        L, N1, H, tag = S["L"], S["N1"], S["H"], S["tag"]
        m0 = mk.mark()
        ft = mk.sbuf("hf_ft", [17, L], F32)
        W1 = mk.sbuf("hf_w1", [17, 64], F32)
        W2 = mk.sbuf("hf_w2", [64, 64], F32)
        W3 = mk.sbuf("hf_w3", [64, 4096], BF16)
        fv = mk.sbuf("hf_fv", [64, 4], F32)
        fb = mk.sbuf("hf_fb", [64, 2], F32)
        z1 = mk.sbuf("hf_z1", [64, L], F32)
        z2 = mk.sbuf("hf_z2", [64, L], F32)
        z2b = mk.sbuf("hf_z2b", [64, L], BF16)
        tmp = mk.sbuf("hf_tmp", [64, 512], F32)
        F1 = mk.sbuf("hf_F1", [max(H, 1), 6, N1], BF16)
        lneg = mk.sbuf("hf_lneg", [H, 64], F32)
        dab = mk.sbuf("hf_dab", [H, D], F32)
        mk.dma("sp", ft[:], I["hy_featsT_" + tag][:, :], writes=[ft])
        mk.dma("sp", W1[:], I["hy_f_w1"][0, :, :], writes=[W1])
        mk.dma("sp", W2[:], I["hy_f_w2"][0, :, :], writes=[W2])
        mk.dma("pool", W3[:], I["hy_f_w3"][0, :, :], writes=[W3])
        mk.dma("sp", fv[:], I["hy_fvec"][:, :], writes=[fv])
        mk.dma("sp", F1[:], I["hy_F1_" + tag][:, :, :], writes=[F1])
        mk.dma("sp", lneg[:], I["hy_lneg_" + tag][:, :], writes=[lneg])
        mk.dma("sp", dab[:], I["hy_dabs"][0:H, :], writes=[dab])
        mk.op("dve", lambda e: e.tensor_tensor(out=fb[:, 0:1], in0=fv[:, 0:1], in1=fv[:, 1:2], op=ALU.mult), reads=[fv], writes=[fb])
        mk.op("dve", lambda e: e.tensor_tensor(out=fb[:, 1:2], in0=fv[:, 2:3], in1=fv[:, 3:4], op=ALU.mult), reads=[fv], writes=[fb])
        for layer, (Wm, src, dst, kin) in enumerate(((W1, ft, z1, 17), (W2, z1, z2, 64))):
            for c0 in range(0, L, 512):
                cn = min(512, L - c0)
                ps = self.ps[(c0 // 512) % 2]
                mk.op("pe", lambda e, ps=ps, Wm=Wm, src=src, c0=c0, cn=cn, kin=kin: e.matmul(
                    ps[0:64, 0:cn], Wm[0:kin, :], src[0:kin, c0:c0 + cn], start=True, stop=True), reads=[Wm, src], writes=[ps])
                d_ = dst[:, c0:c0 + cn]
                mk.op("dve", lambda e, ps=ps, d_=d_, cn=cn, layer=layer: e.tensor_scalar(
                    out=d_, in0=ps[0:64, 0:cn], scalar1=fv[:, 2 * layer + 1:2 * layer + 2], scalar2=fb[:, layer:layer + 1],
                    op0=ALU.mult, op1=ALU.add), reads=[ps, fv, fb], writes=[dst])
                for _ in range(2):
                    mk.op("dve", lambda e, d_=d_, cn=cn: e.tensor_scalar(out=tmp[:, 0:cn], in0=d_, scalar1=math.pi,
                          scalar2=2 * math.pi, op0=ALU.is_gt, op1=ALU.mult), reads=[dst], writes=[tmp])
                    mk.op("dve", lambda e, d_=d_, cn=cn: e.tensor_tensor(out=d_, in0=d_, in1=tmp[:, 0:cn], op=ALU.subtract),
                          reads=[dst, tmp], writes=[dst])
                    mk.op("dve", lambda e, d_=d_, cn=cn: e.tensor_scalar(out=tmp[:, 0:cn], in0=d_, scalar1=-math.pi,
                          scalar2=2 * math.pi, op0=ALU.is_lt, op1=ALU.mult), reads=[dst], writes=[tmp])
                    mk.op("dve", lambda e, d_=d_, cn=cn: e.tensor_tensor(out=d_, in0=d_, in1=tmp[:, 0:cn], op=ALU.add),
                          reads=[dst, tmp], writes=[dst])
                mk.op("act", lambda e, d_=d_: e.activation(out=d_, in_=d_, func=AF.Sin), reads=[dst], writes=[dst])
        mk.op("act", lambda e: e.copy(z2b[:], z2[:]), reads=[z2], writes=[z2b])
        dec = [mk.sbuf("hf_dec%d" % k, [H, D], F32) for k in range(4)]
        hd = [mk.sbuf("hf_hd%d" % k, [H, D], F32) for k in range(4)]
        hdb = [mk.sbuf("hf_hdb%d" % k, [H, D], BF16) for k in range(4)]
        ha = [mk.sbuf("hf_ha%d" % k, [H, D], BF16) for k in range(4)]
        Asb = [mk.sbuf("hf_A%d" % k, [N1, 2, D], BF16) for k in range(2)]
        rn = mk.sbuf("hf_rn", [128, D], F32)
        A = self.dram["hyA"]
        Bt = [mk.sbuf("hf_B%d" % k, [128, D], BF16) for k in range(2)]
        Gt = [mk.sbuf("hf_Gt%d" % k, [128, 2, 128], BF16) for k in range(2)]
        Ho = [mk.sbuf("hf_Ho%d" % k, [128, D], BF16) for k in range(4)]
        for o in range(2):
            pn = (self.ps[6], self.ps[7])
            units = [(n2_, dr_) for n2_ in range(64) for dr_ in range(2)]

            def issue_h(ui):
                n2_, dr_ = units[ui]
                col_ = n2_ if dr_ == 0 else (64 - n2_) % 64
                for hf in range(2):
                    ph = self.ps[(ui % 2) * 2 + hf]
                    w0 = (dr_ * 2 + o) * D + hf * 512
                    mk.op("pe", lambda e, ph=ph, col_=col_, w0=w0: e.matmul(
                        ph[0:H, :], z2b[:, col_:L:64], W3[:, w0:w0 + 512], start=True, stop=True),
                        reads=[z2b, W3], writes=[ph])
                i2_ = dr_ + 2 * (n2_ % 2)
                mk.op("act", lambda e, i2_=i2_, col_=col_: e.activation(out=dec[i2_][:], in_=dab[:], func=AF.Exp,
                                                                       scale=lneg[:, col_:col_ + 1]), reads=[dab, lneg], writes=[dec[i2_]])
            issue_h(0)
            for n2 in range(64):
                n2b = (64 - n2) % 64
                hb2 = []
                for dr in range(2):
                    ui = n2 * 2 + dr
                    col = n2 if dr == 0 else n2b
                    i2 = dr + 2 * (n2 % 2)
                    if ui + 1 < len(units):
                        issue_h(ui + 1)
                    for hf in range(2):
                        ph = self.ps[(ui % 2) * 2 + hf]
                        mk.op("dve", lambda e, ph=ph, i2=i2, hf=hf: e.tensor_tensor(
                            out=hd[i2][:, hf * 512:(hf + 1) * 512], in0=ph[0:H, :], in1=dec[i2][:, hf * 512:(hf + 1) * 512],
                            op=ALU.mult), reads=[ph, dec[i2]], writes=[hd[i2]])
                    if dr == 1 and n2 == 0:
                        mk.op("dve", lambda e, i2=i2: e.memset(hd[i2][0:1, :], 0.0), reads=[hd[i2]], writes=[hd[i2]])
                    hb_ = hdb[(n2 % 2) * 2 + dr]
                    hb2.append(hb_)
                    mk.op("act", lambda e, i2=i2, hb_=hb_: e.copy(hb_[:], hd[i2][:]), reads=[hd[i2]], writes=[hb_])
                    mk.op("dve", lambda e, i2=i2: e.scalar_tensor_tensor(out=ha[i2][:], in0=hd[i2][:], scalar=-1.0, in1=hd[i2][:],
                                                                        op0=ALU.mult, op1=ALU.max), reads=[hd[i2]], writes=[ha[i2]])
                    for hf in range(2):
                        first = (n2 == 0 and dr == 0)
                        last = (n2 == 63 and dr == 1)
                        mk.op("pe", lambda e, i2=i2, hf=hf, first=first, last=last: e.matmul(
                            pn[hf][:, :], self.ones_b[0:H, :], ha[i2][:, hf * 512:(hf + 1) * 512], start=first, stop=last),
                            reads=[self.ones_b, ha[i2]], writes=[pn[hf]])
                As = Asb[n2 % 2]
                fbi = 4 if n2 == 0 else 2
                for ri in range(2):
                    for hf in range(2):
                        pA = self.ps[4 + hf]
                        mk.op("pe", lambda e, pA=pA, ri=ri, hf=hf, hb2=hb2: e.matmul(
                            pA[0:N1, :], F1[0:H, ri, :], hb2[0][:, hf * 512:(hf + 1) * 512], start=True, stop=False),
                            reads=[F1, hb2[0]], writes=[pA])
                        mk.op("pe", lambda e, pA=pA, ri=ri, hf=hf, hb2=hb2, fbi=fbi: e.matmul(
                            pA[0:N1, :], F1[0:H, fbi + ri, :], hb2[1][:, hf * 512:(hf + 1) * 512], start=False, stop=True),
                            reads=[F1, hb2[1]], writes=[pA])
                        eng = "act" if hf else "dve"
                        if eng == "act":
                            mk.op("act", lambda e, pA=pA, ri=ri, hf=hf, As=As: e.copy(As[:, ri, hf * 512:(hf + 1) * 512], pA[0:N1, :]),
                                  reads=[pA], writes=[As])
                        else:
                            mk.op("dve", lambda e, pA=pA, ri=ri, hf=hf, As=As: e.tensor_copy(As[:, ri, hf * 512:(hf + 1) * 512], pA[0:N1, :]),
                                  reads=[pA], writes=[As])
                for ri in range(2):
                    mk.dma("act", A[ri, 0:N1, n2, :], As[:, ri, :], reads=[As], writes=[A])
            for hf in range(2):
                mk.op("dve", lambda e, hf=hf: e.reciprocal(rn[:, hf * 512:(hf + 1) * 512], pn[hf][:, :]), reads=[pn[hf]], writes=[rn])
            for kk in range(N1):
                B = Bt[kk % 2]
                for ri in range(2):
                    mk.dma("sp", B[ri * 64:(ri + 1) * 64, :], A[ri, kk, :, :], reads=[A], writes=[B])
                g_ = Gt[kk % 2]
                mk.dma("sp", g_[:], I["hy_G_" + tag][:, kk, 2:4, :], writes=[g_])
                for ri in range(2):
                    ho = Ho[(2 * kk + ri) % 4]
                    for hf in range(2):
                        pX = self.ps[(kk % 2) * 4 + ri * 2 + hf]
                        mk.op("pe", lambda e, pX=pX, ri=ri, hf=hf, B=B, g_=g_: e.matmul(
                            pX[:, :], g_[:, ri, :], B[:, hf * 512:(hf + 1) * 512], start=True, stop=True), reads=[g_, B], writes=[pX])
                        mk.op("dve", lambda e, pX=pX, hf=hf, ho=ho: e.tensor_tensor(
                            out=ho[:, hf * 512:(hf + 1) * 512], in0=pX[:, :], in1=rn[:, hf * 512:(hf + 1) * 512], op=ALU.mult),
                            reads=[pX, rn], writes=[ho])
                    mk.dma("act", HH[o][ri, kk, :, :], ho[:], reads=[ho], writes=[HH[o]])
        mk.release(m0)

    Prog.hy_filter = hy_filter


_hy_methods()


def _hy_methods2():
    def hy_conv(self, S, o, I, zsrc, zrow0, gate, grow0, lbias, HH, dst, dst_bf16):
        mk = self.mk
        L, N1, H, tag, col0 = S["L"], S["N1"], S["H"], S["tag"], S["col0"]
        A, Cs = self.dram["hyA"], self.dram["hyC"]
        m0 = mk.mark()
        zb = mk.sbuf("hc_zb", [128, DC, L], BF16)
        F1 = mk.sbuf("hc_F1", [max(H, 1), 6, N1], BF16)
        mk.dma("sp", F1[:], I["hy_F1_" + tag][:, :, :], writes=[F1])
        for j in range(DC):
            mk.dma("pool", zb[:, j, :], zsrc[zrow0 + j * 128:zrow0 + (j + 1) * 128, col0:col0 + L], reads=[zsrc], writes=[zb])
        zT = [mk.sbuf("hc_zT%d" % k, [H, D], BF16) for k in range(2)]
        Asb = [mk.sbuf("hc_A%d" % k, [N1, 2, D], BF16) for k in range(2)]
        for n2 in range(64):
            pt = self.ps[n2 % 2]
            ptb = pt[:, 0:512].bitcast(BF16)
            for j in range(DC):
                mk.op("pe", lambda e, ptb=ptb, j=j, n2=n2: e.transpose(ptb[0:H, j * 128:(j + 1) * 128], zb[:, j, n2:L:64],
                                                                      self.ident_b[:]), reads=[zb, self.ident_b], writes=[pt])
            z_ = zT[n2 % 2]
            mk.op("act", lambda e, ptb=ptb, z_=z_: e.copy(z_[:], ptb[0:H, :]), reads=[pt], writes=[z_])
            As = Asb[n2 % 2]
            for ri in range(2):
                for hf in range(2):
                    pA = self.ps[2 + ri * 2 + hf]
                    mk.op("pe", lambda e, pA=pA, ri=ri, hf=hf, z_=z_: e.matmul(
                        pA[0:N1, :], F1[0:H, ri, :], z_[:, hf * 512:(hf + 1) * 512], start=True, stop=True),
                        reads=[F1, z_], writes=[pA])
                    if hf:
                        mk.op("act", lambda e, pA=pA, ri=ri, hf=hf, As=As: e.copy(As[:, ri, hf * 512:(hf + 1) * 512], pA[0:N1, :]),
                              reads=[pA], writes=[As])
                    else:
                        mk.op("dve", lambda e, pA=pA, ri=ri, hf=hf, As=As: e.tensor_copy(As[:, ri, hf * 512:(hf + 1) * 512], pA[0:N1, :]),
                              reads=[pA], writes=[As])
            for ri in range(2):
                mk.dma("act", A[ri, 0:N1, n2, :], As[:, ri, :], reads=[As], writes=[A])
        mk.release(m0)
        m0 = mk.mark()
        Bt = [mk.sbuf("hc_B%d" % k, [128, D], BF16) for k in range(2)]
        Gt = [mk.sbuf("hc_G%d" % k, [128, 5, 128], BF16) for k in range(2)]
        Hr = [mk.sbuf("hc_Hr%d" % k, [128, D], BF16) for k in range(2)]
        Hi = [mk.sbuf("hc_Hi%d" % k, [128, D], BF16) for k in range(2)]
        ta = [mk.sbuf("hc_ta%d" % k, [128, D], F32) for k in range(2)]
        tb = [mk.sbuf("hc_tb%d" % k, [128, D], F32) for k in range(2)]
        Y = [mk.sbuf("hc_Y%d" % k, [128, D], BF16) for k in range(2)]
        Cb = [mk.sbuf("hc_C%d" % k, [128, D], BF16) for k in range(2)]
        def f2_load(kk):
            i2 = kk % 2
            B, g_, hr, hi = Bt[i2], Gt[i2], Hr[i2], Hi[i2]
            for ri in range(2):
                mk.dma("sp", B[ri * 64:(ri + 1) * 64, :], A[ri, kk, :, :], reads=[A], writes=[B])
            mk.dma("sp", g_[:], I["hy_G_" + tag][:, kk, :, :], writes=[g_])
            mk.dma("sp", hr[:], HH[o][0, kk, :, :], reads=[HH[o]], writes=[hr])
            mk.dma("sp", hi[:], HH[o][1, kk, :, :], reads=[HH[o]], writes=[hi])

        def f2_X(u):
            kk, hf = divmod(u, 2)
            i2 = kk % 2
            sl = slice(hf * 512, (hf + 1) * 512)
            pa, pb = self.ps[(u % 2) * 2], self.ps[(u % 2) * 2 + 1]
            mk.op("pe", lambda e: e.matmul(pa[:, :], Gt[i2][:, 0, :], Bt[i2][:, sl], start=True, stop=True),
                  reads=[Gt[i2], Bt[i2]], writes=[pa])
            mk.op("pe", lambda e: e.matmul(pb[:, :], Gt[i2][:, 1, :], Bt[i2][:, sl], start=True, stop=True),
                  reads=[Gt[i2], Bt[i2]], writes=[pb])
        f2_load(0)
        if N1 > 1:
            f2_load(1)
        f2_X(0)
        for u in range(2 * N1):
            kk, hf = divmod(u, 2)
            i2 = kk % 2
            hr, hi = Hr[i2], Hi[i2]
            sl = slice(hf * 512, (hf + 1) * 512)
            pa, pb = self.ps[(u % 2) * 2], self.ps[(u % 2) * 2 + 1]
            if u + 1 < 2 * N1:
                f2_X(u + 1)
            mk.op("dve", lambda e, pa=pa, sl=sl, hr=hr, i2=i2: e.tensor_tensor(out=ta[i2][:, sl], in0=pa[:, :], in1=hr[:, sl],
                                                                              op=ALU.mult), reads=[pa, hr], writes=[ta[i2]])
            mk.op("dve", lambda e, pb=pb, sl=sl, hi=hi, i2=i2: e.tensor_tensor(out=tb[i2][:, sl], in0=pb[:, :], in1=hi[:, sl],
                                                                              op=ALU.mult), reads=[pb, hi], writes=[tb[i2]])
            mk.op("pool", lambda e, sl=sl, i2=i2: e.tensor_tensor(out=Y[i2][:, sl], in0=ta[i2][:, sl], in1=tb[i2][:, sl],
                                                                 op=ALU.add), reads=[ta[i2], tb[i2]], writes=[Y[i2]])
            pc = self.ps[4 + u % 4]
            mk.op("pe", lambda e, pc=pc, sl=sl, i2=i2: e.matmul(pc[:, :], Gt[i2][:, 4, :], Y[i2][:, sl], start=True, stop=True),
                  reads=[Gt[i2], Y[i2]], writes=[pc])
            mk.op("act", lambda e, pc=pc, sl=sl, i2=i2: e.copy(Cb[i2][:, sl], pc[:, :]), reads=[pc], writes=[Cb[i2]])
            if hf == 1:
                for ri in range(2):
                    mk.dma("act", Cs[ri, :, kk, :], Cb[i2][ri * 64:(ri + 1) * 64, :], reads=[Cb[i2]], writes=[Cs])
                if kk + 2 < N1:
                    f2_load(kk + 2)
        mk.release(m0)
        m0 = mk.mark()
        Fi = mk.sbuf("hc_Fi", [N1, 2, max(H, 1)], BF16)
        mk.dma("sp", Fi[:], I["hy_Fi_" + tag][:, :, :], writes=[Fi])
        lb = mk.sbuf("hc_lb", [128, DC], F32)
        mk.dma("sp", lb[:], lbias, writes=[lb])
        yh = mk.sbuf("hc_yh", [128, 4, L], F32)
        Ct = [mk.sbuf("hc_Ct%d" % k, [N1, 2, 512], BF16) for k in range(3)]
        zr = [mk.sbuf("hc_zr%d" % k, [128, L], F32) for k in range(2)]
        gr = [mk.sbuf("hc_gr%d" % k, [128, L], F32) for k in range(2)]
        ob = [mk.sbuf("hc_ob%d" % k, [128, L], BF16 if dst_bf16 else F32) for k in range(2)]
        per_bank = max(1, 512 // (4 * H))
        for half in range(2):
            for t2 in range(64):
                c_ = Ct[t2 % 3]
                mk.dma("sp", c_[:], Cs[:, t2, 0:N1, half * 512:(half + 1) * 512].rearrange("r k c -> k r c"), reads=[Cs], writes=[c_])
                slot = t2 % per_bank
                py = self.ps[(t2 // per_bank) % 8]
                for c4 in range(4):
                    o0 = slot * 4 * H + c4 * H
                    mk.op("pe", lambda e, py=py, o0=o0, c4=c4, c_=c_: e.matmul(
                        py[:, o0:o0 + H], c_[:, 0, c4 * 128:(c4 + 1) * 128], Fi[:, 0, :], start=True, stop=False),
                        reads=[c_, Fi], writes=[py])
                    mk.op("pe", lambda e, py=py, o0=o0, c4=c4, c_=c_: e.matmul(
                        py[:, o0:o0 + H], c_[:, 1, c4 * 128:(c4 + 1) * 128], Fi[:, 1, :], start=False, stop=True),
                        reads=[c_, Fi], writes=[py])
                src = py[:, slot * 4 * H:(slot + 1) * 4 * H].rearrange("p (a b) -> p a b", b=H)
                if t2 % 2:
                    mk.op("act", lambda e, src=src, t2=t2: e.copy(yh[:, :, t2:L:64], src), reads=[py], writes=[yh])
                else:
                    mk.op("dve", lambda e, src=src, t2=t2: e.tensor_copy(yh[:, :, t2:L:64], src), reads=[py], writes=[yh])
            for c4 in range(4):
                cj = half * 4 + c4
                z_, g_, o_ = zr[c4 % 2], gr[c4 % 2], ob[c4 % 2]
                mk.dma("sp", z_[:], zsrc[zrow0 + cj * 128:zrow0 + (cj + 1) * 128, col0:col0 + L], reads=[zsrc], writes=[z_])
                mk.dma("sp", g_[:], gate[grow0 + cj * 128:grow0 + (cj + 1) * 128, col0:col0 + L], reads=[gate], writes=[g_])
                mk.op("dve", lambda e, z_=z_, cj=cj, c4=c4: e.scalar_tensor_tensor(
                    out=z_[:], in0=z_[:], scalar=lb[:, cj:cj + 1], in1=yh[:, c4, :], op0=ALU.mult, op1=ALU.add),
                    reads=[z_, lb, yh], writes=[z_])
                mk.op("pool", lambda e, z_=z_, g_=g_, o_=o_: e.tensor_tensor(out=o_[:], in0=z_[:], in1=g_[:], op=ALU.mult),
                      reads=[z_, g_], writes=[o_])
                mk.dma("act", dst[cj * 128:(cj + 1) * 128, col0:col0 + L], o_[:], reads=[o_], writes=[dst])
        mk.release(m0)

    def stage_hyena(self, slot, XT, HT, I):
        mk = self.mk
        U0, ZT, Z1 = self.dram["hyU0"], self.dram["hyZ"], self.dram["hyZ1"]
        m0 = mk.mark()
        W = self.load_w("hy_win", I["hy_w_in"][slot], DC, 3 * D)
        bi_ = mk.sbuf("hy_bin", [128, 24], F32)
        mk.dma("sp", bi_[:], I["hy_b_in_pc"][:, :], writes=[bi_])
        hb = [mk.sbuf("hy_h%d" % k, [128, DC, 512], BF16) for k in range(2)]
        uo = [mk.sbuf("hy_uo%d" % k, [128, 512], F32) for k in range(3)]
        n = 0
        for bi, (t0, tn) in enumerate(BLOCKS):
            h = hb[bi % 2]
            mk.dma("sp", h[:, :, 0:tn], HT[:, t0:t0 + tn].rearrange("(k p) t -> p k t", p=128), reads=[HT], writes=[h])
            for m in range(24):
                ps = self.ps[n % 4]
                u = uo[n % 3]
                n += 1
                for k in range(DC):
                    mk.op("pe", lambda e, ps=ps, k=k, m=m, h=h: e.matmul(
                        ps[:, 0:tn], W[:, k, m * 128:(m + 1) * 128], h[:, k, 0:tn], start=(k == 0), stop=(k == DC - 1)),
                        reads=[W, h], writes=[ps])
                mk.op("act", lambda e, ps=ps, u=u, m=m: e.activation(out=u[:, 0:tn], in_=ps[:, 0:tn], func=AF.Identity,
                                                                    bias=bi_[:, m:m + 1]), reads=[ps, bi_], writes=[u])
                mk.dma("act", U0[m * 128:(m + 1) * 128, t0:t0 + tn], u[:, 0:tn], reads=[u], writes=[U0])
        mk.release(m0)
        m0 = mk.mark()
        ws = mk.sbuf("hy_ws", [128, 24, 3], F32)
        bs = mk.sbuf("hy_bs", [128, 24], F32)
        mk.dma("sp", ws[:], I["hy_w_short_pc"][:, :, :], writes=[ws])
        mk.dma("sp", bs[:], I["hy_b_short_pc"][:, :], writes=[bs])
        W_ = NT + 4
        ub = [mk.sbuf("hy_ub%d" % k, [128, W_], F32) for k in range(2)]
        vb = [mk.sbuf("hy_vb%d" % k, [128, W_], F32) for k in range(2)]
        for k in range(2):
            mk.op("pool", lambda e, k=k: e.memset(ub[k][:], 0.0), writes=[ub[k]])
        for m in range(24):
            u, v = ub[m % 2], vb[m % 2]
            mk.dma("sp", u[:, 1:1 + CTX], U0[m * 128:(m + 1) * 128, 0:CTX], reads=[U0], writes=[u])
            mk.dma("sp", u[:, 3 + CTX:3 + NT], U0[m * 128:(m + 1) * 128, CTX:NT], reads=[U0], writes=[u])
            n_ = W_ - 2
            mk.op("dve", lambda e, u=u, v=v, m=m: e.tensor_scalar(out=v[:, 1:1 + n_], in0=u[:, 0:n_], scalar1=ws[:, m, 0:1],
                                                                 scalar2=bs[:, m:m + 1], op0=ALU.mult, op1=ALU.add),
                  reads=[u, ws, bs], writes=[v])
            mk.op("dve", lambda e, u=u, v=v, m=m: e.scalar_tensor_tensor(out=v[:, 1:1 + n_], in0=u[:, 1:1 + n_], scalar=ws[:, m, 1:2],
                                                                        in1=v[:, 1:1 + n_], op0=ALU.mult, op1=ALU.add),
                  reads=[u, ws, v], writes=[v])
            mk.op("dve", lambda e, u=u, v=v, m=m: e.scalar_tensor_tensor(out=v[:, 1:1 + n_], in0=u[:, 2:2 + n_], scalar=ws[:, m, 2:3],
                                                                        in1=v[:, 1:1 + n_], op0=ALU.mult, op1=ALU.add),
                  reads=[u, ws, v], writes=[v])
            mk.dma("act", ZT[m * 128:(m + 1) * 128, 0:CTX], v[:, 1:1 + CTX], reads=[v], writes=[ZT])
            mk.dma("act", ZT[m * 128:(m + 1) * 128, CTX:NT], v[:, 3 + CTX:3 + NT], reads=[v], writes=[ZT])
        mk.release(m0)
        seqs = [dict(L=SEQ, N1=128, H=64, tag="lat", col0=CTX)]
        if not self.cfg.get("ctx_direct", True):
            seqs.append(dict(L=CTX, N1=8, H=4, tag="ctx", col0=0))
        for S in seqs:
            HH = [self.dram["hyHH_%s%d" % (S["tag"], o)] for o in range(2)]
            self.hy_filter(S, I, HH)
            self.hy_conv(S, 0, I, ZT, 0, ZT, D, I["hy_lbias_pc"][0, :, :], HH, Z1, False)
            self.hy_conv(S, 1, I, Z1, 0, ZT, 2 * D, I["hy_lbias_pc"][1, :, :], HH, HT, True)
        if self.cfg.get("ctx_direct", True):
            self.hy_ctx_direct(I)
        m0 = mk.mark()
        wo = self.load_w("hy_wout", I["hy_w_out"][slot], DC, D)
        bo = mk.sbuf("hy_bo", [128, DC], F32)
        mk.dma("sp", bo[:], I["hy_b_out_pc"][:, :], writes=[bo])
        self.linear_resid(XT, HT, wo, DC, "g1", bias=bo)
        mk.release(m0)

    Prog.hy_conv = hy_conv
    Prog.stage_hyena = stage_hyena


_hy_methods2()


_PROG = {}


def kernel(**inputs):
    inp = {k: np.asarray(v) for k, v in inputs.items()}
    if "p" not in _PROG:
        _PROG["p"] = build_program()
    p = _PROG["p"]
    shared = None
    in_maps = []
    for b in range(8):
        m = host_inputs(inp, b)
        if shared is None:
            shared = m
        else:
            for k in m:
                if k not in ("x", "ctx", "c_pc"):
                    m[k] = shared[k]
        in_maps.append(m)
    res = run_bass_kernel_spmd(p.nc, in_maps, core_ids=list(range(8)))
    out = np.stack([np.asarray(r["y"], dtype=np.float32) for r in res.results], axis=0)
    return out


BSL = 256
BSH = 8
NBLK = 66
NSLOT = NBLK * BSL
NTILE = NT // 128
IOA = bass.IndirectOffsetOnAxis


def _sparse_methods():
    def indirect(mk, out, out_off, in_, in_off, reads, writes):
        qlane = mk.lanes["pool"]
        dl = mk.dma_lanes[mk.dma_rr]
        mk.dma_rr = (mk.dma_rr + 1) % len(mk.dma_lanes)
        need, _ = mk._deps(dl, reads, writes)
        if dl.count > 0:
            need[dl.name] = max(need.get(dl.name, 0), dl.count)
        for ln, ix in need.items():
            if qlane.seen.get(ln, 0) >= ix:
                continue
            qlane.seen[ln] = ix
            qlane.eng.wait_ge(mk.lanes[ln].sem, ix)
        inst = qlane.eng.indirect_dma_start(out=out, out_offset=out_off, in_=in_, in_offset=in_off)
        dl.count += 16
        inst.then_inc(dl.sem, 16)
        mk._record(dl, reads, writes)
        mk.n_inst += 1

    MK.indirect = indirect

    def alloc_sparse(self):
        mk = self.mk
        R = {}
        R["M1a"] = mk.sbuf("sp_M1a", [128, NTILE, 32], F32)
        R["M2a"] = mk.sbuf("sp_M2a", [128, NTILE, 32], F32)
        R["A12"] = mk.sbuf("sp_A12", [128, NTILE, 2], F32)
        R["POSi"] = mk.sbuf("sp_POSi", [128, NTILE, 2], I32)
        R["IDXW"] = mk.sbuf("sp_IDXW", [128, NBLK], I32)
        R["U"] = mk.sbuf("sp_U", [128, 128], BF16)
        R["jg"] = mk.sbuf("sp_jg", [128, NBLK], F32)
        R["pidx"] = mk.sbuf("sp_pidx", [128, 1], F32)
        R["ones32"] = mk.sbuf("sp_ones32", [128, 32], F32)
        mk.op("pool", lambda e: e.memset(R["U"][:], 1.0), writes=[R["U"]])
        mk.op("pool", lambda e: e.affine_select(out=R["U"][:], in_=R["U"][:], pattern=[[1, 128]], compare_op=ALU.is_gt,
                                                fill=0.0, base=0, channel_multiplier=-1), reads=[R["U"]], writes=[R["U"]])
        mk.op("pool", lambda e: e.memset(R["ones32"][:], 1.0), writes=[R["ones32"]])
        return R

    def route_tile_sparse(self, hf, a, tok0, R, rt):
        mk = self.mk
        ti = tok0 // 128
        ps = self.ps[1 + (ti % 3)]
        Wr, br = R["Wr"], R["br"]
        for k in range(DC):
            mk.op("pe", lambda e, k=k: e.matmul(ps[:, 0:36], hf[:, k, a * 128:(a + 1) * 128], Wr[:, k, :],
                                                 start=(k == 0), stop=(k == DC - 1)), reads=[hf, Wr], writes=[ps])
        lg, gmax, ngmax, ge, gsum, gp, mg, es = (rt[k] for k in ("lg", "gmax", "ngmax", "ge", "gsum", "gp", "mg", "es"))
        t8, dd, w2, m1, m2 = (rt[k] for k in ("t8", "dd", "w2", "m1", "m2"))
        M1a, M2a, A12 = R["M1a"], R["M2a"], R["A12"]
        V = lambda fn, reads, writes: mk.op("dve", fn, reads=reads, writes=writes)
        V(lambda e: e.tensor_tensor(out=lg[:], in0=ps[:, 0:36], in1=br[:], op=ALU.add), [ps, br], [lg])
        V(lambda e: e.reduce_max(out=gmax[:], in_=lg[:, 0:4], axis=AX.X), [lg], [gmax])
        V(lambda e: e.tensor_scalar(out=ngmax[:], in0=gmax[:], scalar1=-1.0, scalar2=None, op0=ALU.mult), [gmax], [ngmax])
        mk.op("act", lambda e: e.activation(out=ge[:], in_=lg[:, 0:4], func=AF.Exp, bias=ngmax[:, 0:1],
                                            accum_out=gsum[:]), reads=[lg, ngmax], writes=[ge, gsum])
        V(lambda e: e.reciprocal(gp[:], gsum[:]), [gsum], [gp])
        V(lambda e: e.tensor_scalar(out=mg[:], in0=lg[:, 0:4], scalar1=gmax[:, 0:1], scalar2=None, op0=ALU.is_equal),
          [lg, gmax], [mg])
        V(lambda e: e.tensor_scalar(out=es[:], in0=lg[:, 4:12], scalar1=mg[:, 0:1], scalar2=None, op0=ALU.mult),
          [lg, mg], [es])
        for g in range(1, 4):
            V(lambda e, g=g: e.scalar_tensor_tensor(out=es[:], in0=lg[:, 4 + 8 * g:12 + 8 * g], scalar=mg[:, g:g + 1],
                                                    in1=es[:], op0=ALU.mult, op1=ALU.add), [lg, mg, es], [es])
        V(lambda e: e.max(out=t8[:], in_=es[:]), [es], [t8])
        V(lambda e: e.tensor_tensor(out=dd[:], in0=t8[:, 1:2], in1=t8[:, 0:1], op=ALU.subtract), [t8], [dd])
        mk.op("act", lambda e: e.activation(out=w2[:], in_=dd[:], func=AF.Sigmoid), reads=[dd], writes=[w2])
        V(lambda e: e.tensor_tensor(out=A12[:, ti, 1:2], in0=w2[:], in1=gp[:], op=ALU.mult), [w2, gp], [A12])
        V(lambda e: e.tensor_tensor(out=A12[:, ti, 0:1], in0=gp[:], in1=A12[:, ti, 1:2], op=ALU.subtract), [gp, A12], [A12])
        V(lambda e: e.tensor_scalar(out=m1[:], in0=es[:], scalar1=t8[:, 0:1], scalar2=None, op0=ALU.is_equal), [es, t8], [m1])
        V(lambda e: e.tensor_scalar(out=m2[:], in0=es[:], scalar1=t8[:, 1:2], scalar2=None, op0=ALU.is_equal), [es, t8], [m2])
        for g in range(4):
            V(lambda e, g=g: e.tensor_scalar(out=M1a[:, ti, 8 * g:8 * g + 8], in0=m1[:], scalar1=mg[:, g:g + 1],
                                             scalar2=None, op0=ALU.mult), [m1, mg], [M1a])
            V(lambda e, g=g: e.tensor_scalar(out=M2a[:, ti, 8 * g:8 * g + 8], in0=m2[:], scalar1=mg[:, g:g + 1],
                                             scalar2=None, op0=ALU.mult), [m2, mg], [M2a])

    def stage_moe_sparse(self, i, XT, R, I):
        mk = self.mk
        HTOK, HS, YS = self.dram["HTOK"], self.dram["HS"], self.dram["YS"]
        M1a, M2a, A12, POSi, IDXW = R["M1a"], R["M2a"], R["A12"], R["POSi"], R["IDXW"]
        m0 = mk.mark()
        Mb = mk.sbuf("sq_Mb", [128, NTILE, 32], BF16)
        P = mk.sbuf("sq_P", [128, NTILE, 32], F32)
        prod = mk.sbuf("sq_prod", [128, NTILE, 32], F32)
        posf = mk.sbuf("sq_posf", [128, NTILE, 2], F32)
        nf = mk.sbuf("sq_nf", [128, 32], F32)
        ni = mk.sbuf("sq_ni", [128, 32], I32)
        pn = mk.sbuf("sq_pn", [128, 32], F32)
        incl = mk.sbuf("sq_incl", [128, 32], F32)
        excl = mk.sbuf("sq_excl", [128, 32], F32)
        EB = mk.sbuf("sq_EB", [128, NBLK], F32)
        mk.op("dve", lambda e: e.tensor_tensor(out=Mb[:], in0=M1a[:], in1=M2a[:], op=ALU.add), reads=[M1a, M2a], writes=[Mb])
        for ti in range(NTILE):
            pb = self.ps[ti // 12]
            c0 = (ti % 12) * 32
            for tj in range(ti):
                mk.op("pe", lambda e, pb=pb, c0=c0, tj=tj: e.matmul(pb[:, c0:c0 + 32], self.ones_b[:], Mb[:, tj, :],
                                                                    start=(tj == 0), stop=False), reads=[self.ones_b, Mb], writes=[pb])
            mk.op("pe", lambda e, pb=pb, c0=c0, ti=ti: e.matmul(pb[:, c0:c0 + 32], R["U"][:], Mb[:, ti, :],
                                                                start=(ti == 0), stop=True), reads=[R["U"], Mb], writes=[pb])
        pt = self.ps[3]
        for tj in range(NTILE):
            mk.op("pe", lambda e, tj=tj: e.matmul(pt[:, 0:32], self.ones_b[:], Mb[:, tj, :], start=(tj == 0),
                                                  stop=(tj == NTILE - 1)), reads=[self.ones_b, Mb], writes=[pt])
        V = lambda fn, reads, writes: mk.op("dve", fn, reads=reads, writes=writes)
        V(lambda e: e.tensor_scalar(out=nf[:], in0=pt[:, 0:32], scalar1=float(BSL - 1), scalar2=None, op0=ALU.add), [pt], [nf])
        V(lambda e: e.tensor_copy(ni[:], nf[:]), [nf], [ni])
        V(lambda e: e.tensor_single_scalar(out=ni[:], in_=ni[:], scalar=BSH, op=ALU.arith_shift_right), [ni], [ni])
        V(lambda e: e.tensor_single_scalar(out=ni[:], in_=ni[:], scalar=BSH, op=ALU.logical_shift_left), [ni], [ni])
        V(lambda e: e.tensor_copy(pn[:], ni[:]), [ni], [pn])
        V(lambda e: e.tensor_tensor_scan(out=incl[:], data0=R["ones32"][:], data1=pn[:], initial=0.0, op0=ALU.mult, op1=ALU.add),
          [R["ones32"], pn], [incl])
        V(lambda e: e.tensor_tensor(out=excl[:], in0=incl[:], in1=pn[:], op=ALU.subtract), [incl, pn], [excl])
        for ti in range(NTILE):
            pb = self.ps[ti // 12]
            c0 = (ti % 12) * 32
            V(lambda e, pb=pb, c0=c0, ti=ti: e.tensor_tensor(out=P[:, ti, :], in0=pb[:, c0:c0 + 32], in1=excl[:], op=ALU.add),
              [pb, excl], [P])
        for k, Mk in enumerate((M1a, M2a)):
            V(lambda e, Mk=Mk: e.tensor_tensor(out=prod[:], in0=Mk[:], in1=P[:], op=ALU.mult), [Mk, P], [prod])
            V(lambda e, k=k: e.reduce_sum(out=posf[:, :, k], in_=prod[:], axis=AX.X), [prod], [posf])
        V(lambda e: e.tensor_copy(POSi[:], posf[:]), [posf], [POSi])
        V(lambda e: e.memset(EB[:], 0.0), [], [EB])
        for ex in range(NE):
            V(lambda e, ex=ex: e.scalar_tensor_tensor(out=EB[:], in0=R["jg"][:], scalar=incl[:, ex:ex + 1], in1=EB[:],
                                                      op0=ALU.is_ge, op1=ALU.add), [R["jg"], incl, EB], [EB])
        V(lambda e: e.tensor_scalar(out=EB[:], in0=EB[:], scalar1=float(NE - 1), scalar2=128.0, op0=ALU.min, op1=ALU.mult), [EB], [EB])
        V(lambda e: e.tensor_scalar(out=EB[:], in0=EB[:], scalar1=R["pidx"][:, 0:1], scalar2=None, op0=ALU.add), [EB, R["pidx"]], [EB])
        V(lambda e: e.tensor_copy(IDXW[:], EB[:]), [EB], [IDXW])
        if self.cfg.get("dbg_pos"):
            d1 = T(self.nc.dram_tensor("dbg_pos", [128, NTILE, 2], I32, kind="ExternalOutput"), "dbg_pos")
            d2 = T(self.nc.dram_tensor("dbg_idx", [128, NBLK], I32, kind="ExternalOutput"), "dbg_idx")
            d3 = T(self.nc.dram_tensor("dbg_incl", [128, 32], F32, kind="ExternalOutput"), "dbg_incl")
            d4 = T(self.nc.dram_tensor("dbg_P", [128, NTILE, 32], F32, kind="ExternalOutput"), "dbg_P")
            d5 = T(self.nc.dram_tensor("dbg_M1", [128, NTILE, 32], F32, kind="ExternalOutput"), "dbg_M1")
            d6 = T(self.nc.dram_tensor("dbg_A12", [128, NTILE, 2], F32, kind="ExternalOutput"), "dbg_A12")
            mk.dma("sp", d1[:, :, :], POSi[:], reads=[POSi])
            mk.dma("sp", d2[:, :], IDXW[:], reads=[IDXW])
            mk.dma("sp", d3[:, :], incl[:], reads=[incl])
            mk.dma("sp", d4[:, :, :], P[:], reads=[P])
            mk.dma("sp", d5[:, :, :], M1a[:], reads=[M1a])
            mk.dma("sp", d6[:, :, :], A12[:], reads=[A12])
            mk.release(m0)
            return
        mk.release(m0)
        m0 = mk.mark()
        ht = [mk.sbuf("sq_ht%d" % k, [128, D], BF16) for k in range(3)]
        for ti in range(NTILE):
            h = ht[ti % 3]
            mk.dma("sp", h[:], HTOK[ti * 128:(ti + 1) * 128, :], reads=[HTOK], writes=[h])
            for k in range(2):
                mk.indirect(HS[:, :], IOA(ap=POSi[:, ti, k:k + 1], axis=0), h[:], None, [h, POSi], [HS])
        mk.release(m0)
        m0 = mk.mark()
        wgu = [mk.sbuf("sq_wgu%d" % k, [128, DC, 512], BF16) for k in range(2)]
        wdn = [mk.sbuf("sq_wdn%d" % k, [128, 2, D], BF16) for k in range(2)]
        NA = BSL // 128
        hs = [mk.sbuf("sq_hs%d" % k, [128, NA, D], BF16) for k in range(2)]
        hsT = [mk.sbuf("sq_hsT%d" % k, [128, DC, BSL], BF16) for k in range(2)]
        sl = [mk.sbuf("sq_sl%d" % k, [128, BSL], F32) for k in range(2)]
        hid = [mk.sbuf("sq_hid%d" % k, [128, 2, BSL], BF16) for k in range(2)]
        yo = [mk.sbuf("sq_yo%d" % k, [128, D], F32) for k in range(3)]
        WGU, WDN = I["moe_wgu%d" % i], I["moe_wdn%d" % i]
        ny = 0
        for j in range(NBLK):
            w, wd_, h, hT_, hd = wgu[j % 2], wdn[j % 2], hs[j % 2], hsT[j % 2], hid[j % 2]
            mk.indirect(w[:].rearrange("p a b -> p (a b)"), None, WGU[:, :], IOA(ap=IDXW[:, j:j + 1], axis=0), [IDXW], [w])
            mk.indirect(wd_[:].rearrange("p a b -> p (a b)"), None, WDN[:, :], IOA(ap=IDXW[:, j:j + 1], axis=0), [IDXW], [wd_])
            mk.dma("sp", h[:], HS[j * BSL:(j + 1) * BSL, :].rearrange("(a p) d -> p a d", p=128), reads=[HS], writes=[h])
            KPB = 1024 // BSL
            for hh in range(DC // KPB):
                pb = self.ps[hh % 2]
                pbb = pb[:, 0:512].bitcast(BF16)
                for kk in range(KPB):
                    k = hh * KPB + kk
                    for a in range(NA):
                        mk.op("pe", lambda e, pbb=pbb, kk=kk, k=k, a=a, h=h: e.transpose(
                            pbb[:, kk * BSL + a * 128:kk * BSL + (a + 1) * 128], h[:, a, k * 128:(k + 1) * 128], self.ident_b[:]),
                            reads=[h, self.ident_b], writes=[pb])
                dstv = hT_[:, hh * KPB:(hh + 1) * KPB, :].rearrange("p a b -> p (a b)")
                if hh % 2:
                    mk.op("act", lambda e, pbb=pbb, dstv=dstv: e.copy(dstv, pbb[:, :]), reads=[pb], writes=[hT_])
                else:
                    mk.op("dve", lambda e, pbb=pbb, dstv=dstv: e.tensor_copy(dstv, pbb[:, :]), reads=[pb], writes=[hT_])
            for fc in range(2):
                pg, pu = self.ps[2 + 2 * fc], self.ps[3 + 2 * fc]
                for k in range(DC):
                    mk.op("pe", lambda e, pg=pg, k=k, fc=fc, w=w, hT_=hT_: e.matmul(
                        pg[:, 0:BSL], w[:, k, fc * 128:(fc + 1) * 128], hT_[:, k, :], start=(k == 0), stop=(k == DC - 1)),
                        reads=[w, hT_], writes=[pg])
                for k in range(DC):
                    mk.op("pe", lambda e, pu=pu, k=k, fc=fc, w=w, hT_=hT_: e.matmul(
                        pu[:, 0:BSL], w[:, k, 256 + fc * 128:256 + (fc + 1) * 128], hT_[:, k, :], start=(k == 0), stop=(k == DC - 1)),
                        reads=[w, hT_], writes=[pu])
                s_ = sl[fc]
                mk.op("act", lambda e, pg=pg, s_=s_: e.activation(out=s_[:], in_=pg[:, 0:BSL], func=AF.Silu), reads=[pg], writes=[s_])
                mk.op("dve", lambda e, pu=pu, s_=s_, hd=hd, fc=fc: e.tensor_tensor(out=hd[:, fc, :], in0=pu[:, 0:BSL], in1=s_[:],
                                                                                  op=ALU.mult), reads=[pu, s_], writes=[hd])
            for a in range(NA):
                y_ = yo[ny % 3]
                ny += 1
                for dh in range(2):
                    pd = self.ps[6 + dh]
                    for fc in range(2):
                        mk.op("pe", lambda e, pd=pd, fc=fc, a=a, dh=dh, hd=hd, wd_=wd_: e.matmul(
                            pd[:, :], hd[:, fc, a * 128:(a + 1) * 128], wd_[:, fc, dh * 512:(dh + 1) * 512], start=(fc == 0), stop=(fc == 1)),
                            reads=[hd, wd_], writes=[pd])
                    if dh:
                        mk.op("act", lambda e, pd=pd, y_=y_, dh=dh: e.copy(y_[:, dh * 512:(dh + 1) * 512], pd[:, :]), reads=[pd], writes=[y_])
                    else:
                        mk.op("dve", lambda e, pd=pd, y_=y_, dh=dh: e.tensor_copy(y_[:, dh * 512:(dh + 1) * 512], pd[:, :]), reads=[pd], writes=[y_])
                mk.dma("act", YS[j * BSL + a * 128:j * BSL + (a + 1) * 128, :], y_[:], reads=[y_], writes=[YS])
        mk.release(m0)
        m0 = mk.mark()
        r1 = [mk.sbuf("sq_r1%d" % k, [128, D], F32) for k in range(2)]
        r2 = [mk.sbuf("sq_r2%d" % k, [128, D], F32) for k in range(2)]
        xr = [mk.sbuf("sq_x%d" % k, [128, 512], F32) for k in range(3)]
        n = 0
        for bi, (t0, tn) in enumerate(BLOCKS):
            r = 1 if bi == 0 else 0
            g2 = self.mv[("g2", r)]
            for a in range(tn // 128):
                ti = t0 // 128 + a
                a_, b_ = r1[ti % 2], r2[ti % 2]
                mk.indirect(a_[:], None, YS[:, :], IOA(ap=POSi[:, ti, 0:1], axis=0), [YS, POSi], [a_])
                mk.indirect(b_[:], None, YS[:, :], IOA(ap=POSi[:, ti, 1:2], axis=0), [YS, POSi], [b_])
                mk.op("dve", lambda e, a_=a_, ti=ti: e.tensor_scalar(out=a_[:], in0=a_[:], scalar1=A12[:, ti, 0:1], scalar2=None,
                                                                    op0=ALU.mult), reads=[a_, A12], writes=[a_])
                mk.op("dve", lambda e, a_=a_, b_=b_, ti=ti: e.scalar_tensor_tensor(out=a_[:], in0=b_[:], scalar=A12[:, ti, 1:2], in1=a_[:],
                                                                                  op0=ALU.mult, op1=ALU.add), reads=[a_, b_, A12], writes=[a_])
                for dc in range(DC):
                    mk.op("pe", lambda e, dc=dc, a=a, a_=a_: e.transpose(self.ps[dc][:, a * 128:(a + 1) * 128],
                                                                         a_[:, dc * 128:(dc + 1) * 128], self.ident_f[:]),
                          reads=[a_, self.ident_f], writes=[self.ps[dc]])
            for dc in range(DC):
                x = xr[n % 3]
                n += 1
                mk.dma("sp", x[:, 0:tn], XT[dc * 128:(dc + 1) * 128, t0:t0 + tn], reads=[self.XTr[dc]], writes=[x])
                mk.op("dve", lambda e, dc=dc, x=x, g2=g2: e.scalar_tensor_tensor(
                    out=x[:, 0:tn], in0=self.ps[dc][:, 0:tn], scalar=g2[:, dc:dc + 1], in1=x[:, 0:tn],
                    op0=ALU.mult, op1=ALU.add), reads=[self.ps[dc], g2, x], writes=[x])
                mk.dma("act", XT[dc * 128:(dc + 1) * 128, t0:t0 + tn], x[:, 0:tn], reads=[x], writes=[self.XTr[dc]])
        mk.release(m0)

    Prog.alloc_sparse = alloc_sparse
    Prog._route_tile_sparse = route_tile_sparse
    Prog.stage_moe_sparse = stage_moe_sparse


_sparse_methods()


def hyd_consts():
    bf = ml_dtypes.bfloat16
    L, N = CTX, 2 * CTX
    c = {}
    t = (np.arange(2)[None, :, None] * 128 + np.arange(128)[:, None, None]).astype(np.float64)
    k = np.arange(N)[None, None, :].astype(np.float64)
    ang = 2 * np.pi * t * k / N
    c["F"] = np.stack([np.cos(ang), -np.sin(ang), np.sin(ang)], axis=2).astype(bf)
    kk = (np.arange(4)[None, :, None] * 128 + np.arange(128)[:, None, None]).astype(np.float64)
    tt = np.arange(L)[None, None, :].astype(np.float64)
    a2 = 2 * np.pi * kk * tt / N
    c["Fi"] = np.stack([np.cos(a2) / N, -np.sin(a2) / N], axis=2).astype(bf)
    pos = (np.arange(2)[None, :] * 128 + np.arange(128)[:, None]).astype(np.float32)
    c["lneg"] = (-(pos / np.float32(L - 1))).astype(np.float32)
    return c


def _hyd_methods():
    def hy_ctx_direct(self, I):
        mk = self.mk
        L = CTX
        ZT, Z1, HT = self.dram["hyZ"], self.dram["hyZ1"], self.dram["HT"]
        m0 = mk.mark()
        Fd = mk.sbuf("hd_F", [128, 2, 3, 512], BF16)
        Fi = mk.sbuf("hd_Fi", [128, 4, 2, L], BF16)
        lneg = mk.sbuf("hd_lneg", [128, 2], F32)
        dab = mk.sbuf("hd_dab", [128, D], F32)
        ft = mk.sbuf("hd_ft", [17, L], F32)
        W1 = mk.sbuf("hd_w1", [17, 64], F32)
        W2 = mk.sbuf("hd_w2", [64, 64], F32)
        W3 = mk.sbuf("hd_w3", [64, 4096], BF16)
        fv = mk.sbuf("hd_fv", [64, 4], F32)
        fb = mk.sbuf("hd_fb", [64, 2], F32)
        z1 = mk.sbuf("hd_z1", [64, L], F32)
        z2 = mk.sbuf("hd_z2", [64, L], F32)
        z2b = mk.sbuf("hd_z2b", [64, L], BF16)
        tmp = mk.sbuf("hd_tmp", [64, L], F32)
        mk.dma("sp", Fd[:], I["hyd_F"][:, :, :, :], writes=[Fd])
        mk.dma("sp", Fi[:], I["hyd_Fi"][:, :, :, :], writes=[Fi])
        mk.dma("sp", lneg[:], I["hyd_lneg"][:, :], writes=[lneg])
        mk.dma("sp", dab[0:64, :], I["hy_dabs"][:, :], writes=[dab])
        mk.dma("sp", dab[64:128, :], I["hy_dabs"][:, :], writes=[dab])
        mk.dma("sp", ft[:], I["hy_featsT_ctx"][:, :], writes=[ft])
        mk.dma("sp", W1[:], I["hy_f_w1"][0, :, :], writes=[W1])
        mk.dma("sp", W2[:], I["hy_f_w2"][0, :, :], writes=[W2])
        mk.dma("pool", W3[:], I["hy_f_w3"][0, :, :], writes=[W3])
        mk.dma("sp", fv[:], I["hy_fvec"][:, :], writes=[fv])
        mk.op("dve", lambda e: e.tensor_tensor(out=fb[:, 0:1], in0=fv[:, 0:1], in1=fv[:, 1:2], op=ALU.mult), reads=[fv], writes=[fb])
        mk.op("dve", lambda e: e.tensor_tensor(out=fb[:, 1:2], in0=fv[:, 2:3], in1=fv[:, 3:4], op=ALU.mult), reads=[fv], writes=[fb])
        for layer, (Wm, src, dst, kin) in enumerate(((W1, ft, z1, 17), (W2, z1, z2, 64))):
            ps = self.ps[layer]
            mk.op("pe", lambda e, ps=ps, Wm=Wm, src=src, kin=kin: e.matmul(ps[0:64, 0:L], Wm[0:kin, :], src[0:kin, :],
                                                                          start=True, stop=True), reads=[Wm, src], writes=[ps])
            mk.op("dve", lambda e, ps=ps, dst=dst, layer=layer: e.tensor_scalar(
                out=dst[:], in0=ps[0:64, 0:L], scalar1=fv[:, 2 * layer + 1:2 * layer + 2], scalar2=fb[:, layer:layer + 1],
                op0=ALU.mult, op1=ALU.add), reads=[ps, fv, fb], writes=[dst])
            for _ in range(2):
                mk.op("dve", lambda e, dst=dst: e.tensor_scalar(out=tmp[:], in0=dst[:], scalar1=math.pi, scalar2=2 * math.pi,
                                                                op0=ALU.is_gt, op1=ALU.mult), reads=[dst], writes=[tmp])
                mk.op("dve", lambda e, dst=dst: e.tensor_tensor(out=dst[:], in0=dst[:], in1=tmp[:], op=ALU.subtract),
                      reads=[dst, tmp], writes=[dst])
                mk.op("dve", lambda e, dst=dst: e.tensor_scalar(out=tmp[:], in0=dst[:], scalar1=-math.pi, scalar2=2 * math.pi,
                                                                op0=ALU.is_lt, op1=ALU.mult), reads=[dst], writes=[tmp])
                mk.op("dve", lambda e, dst=dst: e.tensor_tensor(out=dst[:], in0=dst[:], in1=tmp[:], op=ALU.add),
                      reads=[dst, tmp], writes=[dst])
            mk.op("act", lambda e, dst=dst: e.activation(out=dst[:], in_=dst[:], func=AF.Sin), reads=[dst], writes=[dst])
        mk.op("act", lambda e: e.copy(z2b[:], z2[:]), reads=[z2], writes=[z2b])
        Hf = mk.sbuf("hd_Hf", [128, 2, 2, 4, D], BF16)
        dec = [mk.sbuf("hd_dec%d" % k, [128, D], F32) for k in range(2)]
        hd = [mk.sbuf("hd_hd%d" % k, [128, D], F32) for k in range(2)]
        hdb = [mk.sbuf("hd_hdb%d" % k, [128, D], BF16) for k in range(4)]
        ha = [mk.sbuf("hd_ha%d" % k, [128, D], BF16) for k in range(2)]
        rn = mk.sbuf("hd_rn", [128, D], F32)
        for o in range(2):
            pn = (self.ps[6], self.ps[7])
            n = 0
            for pt in range(2):
                for dr in range(2):
                    i2 = n % 2
                    for hf in range(2):
                        ph = self.ps[i2 * 2 + hf]
                        w0 = (dr * 2 + o) * D + hf * 512
                        mk.op("pe", lambda e, ph=ph, pt=pt, w0=w0: e.matmul(ph[:, :], z2b[:, pt * 128:(pt + 1) * 128], W3[:, w0:w0 + 512],
                                                                            start=True, stop=True), reads=[z2b, W3], writes=[ph])
                    mk.op("act", lambda e, i2=i2, pt=pt: e.activation(out=dec[i2][:], in_=dab[:], func=AF.Exp, scale=lneg[:, pt:pt + 1]),
                          reads=[dab, lneg], writes=[dec[i2]])
                    for hf in range(2):
                        ph = self.ps[i2 * 2 + hf]
                        sl_ = slice(hf * 512, (hf + 1) * 512)
                        mk.op("dve", lambda e, ph=ph, i2=i2, sl_=sl_: e.tensor_tensor(out=hd[i2][:, sl_], in0=ph[:, :], in1=dec[i2][:, sl_],
                                                                                     op=ALU.mult), reads=[ph, dec[i2]], writes=[hd[i2]])
                    if dr == 1 and pt == 0:
                        mk.op("dve", lambda e, i2=i2: e.memset(hd[i2][0:1, :], 0.0), reads=[hd[i2]], writes=[hd[i2]])
                    hb_ = hdb[pt * 2 + dr]
                    mk.op("act", lambda e, i2=i2, hb_=hb_: e.copy(hb_[:], hd[i2][:]), reads=[hd[i2]], writes=[hb_])
                    mk.op("dve", lambda e, i2=i2: e.scalar_tensor_tensor(out=ha[i2][:], in0=hd[i2][:], scalar=-1.0, in1=hd[i2][:],
                                                                        op0=ALU.mult, op1=ALU.max), reads=[hd[i2]], writes=[ha[i2]])
                    for hf in range(2):
                        mk.op("pe", lambda e, i2=i2, hf=hf, n=n: e.matmul(pn[hf][:, :], self.ones_b[:], ha[i2][:, hf * 512:(hf + 1) * 512],
                                                                          start=(n == 0), stop=(n == 3)), reads=[self.ones_b, ha[i2]], writes=[pn[hf]])
                    n += 1
            for hf in range(2):
                mk.op("dve", lambda e, hf=hf: e.reciprocal(rn[:, hf * 512:(hf + 1) * 512], pn[hf][:, :]), reads=[pn[hf]], writes=[rn])
            q = 0
            for kc in range(4):
                for ri in range(2):
                    for hf in range(2):
                        pa = self.ps[4 + q % 2]
                        q += 1
                        sl_ = slice(hf * 512, (hf + 1) * 512)
                        steps = [(pt, dr) for pt in range(2) for dr in range(2)]
                        for si, (pt, dr) in enumerate(steps):
                            kind = ri if dr == 0 else (0 if ri == 0 else 2)
                            mk.op("pe", lambda e, pa=pa, pt=pt, dr=dr, kind=kind, kc=kc, sl_=sl_, si=si: e.matmul(
                                pa[:, :], Fd[:, pt, kind, kc * 128:(kc + 1) * 128], hdb[pt * 2 + dr][:, sl_],
                                start=(si == 0), stop=(si == 3)), reads=[Fd, hdb[pt * 2 + dr]], writes=[pa])
                        mk.op("dve", lambda e, pa=pa, o=o, ri=ri, kc=kc, sl_=sl_: e.tensor_tensor(
                            out=Hf[:, o, ri, kc, sl_], in0=pa[:, :], in1=rn[:, sl_], op=ALU.mult), reads=[pa, rn], writes=[Hf])
        zf = mk.sbuf("hd_zf", [128, DC, L], F32)
        gt = mk.sbuf("hd_gt", [128, DC, L], F32)
        zb = mk.sbuf("hd_zb", [128, DC, L], BF16)
        zT = mk.sbuf("hd_zT", [128, 2, D], BF16)
        Y = mk.sbuf("hd_Y", [128, 2, 4, D], BF16)
        ta = [mk.sbuf("hd_ta%d" % k, [128, 512], F32) for k in range(2)]
        tb = [mk.sbuf("hd_tb%d" % k, [128, 512], F32) for k in range(2)]
        lb = mk.sbuf("hd_lb", [128, 2, DC], F32)
        rr = [mk.sbuf("hd_rr%d" % k, [128, L], F32) for k in range(2)]
        obf = mk.sbuf("hd_obf", [128, DC, L], F32)
        obb = mk.sbuf("hd_obb", [128, DC, L], BF16)
        for o in range(2):
            mk.dma("sp", lb[:, o, :], I["hy_lbias_pc"][o, :, :], writes=[lb])
        for o in range(2):
            zsrc = ZT if o == 0 else Z1
            mk.dma("sp", zf[:], zsrc[0:D, 0:L].rearrange("(j p) t -> p j t", p=128), reads=[zsrc], writes=[zf])
            g0 = (1 + o) * D
            mk.dma("sp", gt[:], ZT[g0:g0 + D, 0:L].rearrange("(j p) t -> p j t", p=128), reads=[ZT], writes=[gt])
            mk.op("act", lambda e: e.copy(zb[:], zf[:]), reads=[zf], writes=[zb])
            for pt in range(2):
                pb = self.ps[pt]
                pbb = pb[:, 0:512].bitcast(BF16)
                for j in range(DC):
                    mk.op("pe", lambda e, pbb=pbb, j=j, pt=pt: e.transpose(pbb[:, j * 128:(j + 1) * 128], zb[:, j, pt * 128:(pt + 1) * 128],
                                                                          self.ident_b[:]), reads=[zb, self.ident_b], writes=[pb])
                mk.op("act", lambda e, pbb=pbb, pt=pt: e.copy(zT[:, pt, :], pbb[:, :]), reads=[pb], writes=[zT])
            for kc in range(4):
                for ri in range(2):
                    for hf in range(2):
                        px = self.ps[2 + ri * 2 + hf]
                        for pt in range(2):
                            mk.op("pe", lambda e, px=px, pt=pt, ri=ri, kc=kc, hf=hf: e.matmul(
                                px[:, :], Fd[:, pt, ri, kc * 128:(kc + 1) * 128], zT[:, pt, hf * 512:(hf + 1) * 512],
                                start=(pt == 0), stop=(pt == 1)), reads=[Fd, zT], writes=[px])
                for hf in range(2):
                    sl_ = slice(hf * 512, (hf + 1) * 512)
                    pxr, pxi = self.ps[2 + hf], self.ps[4 + hf]
                    a_, b_ = ta[hf], tb[hf]
                    mk.op("dve", lambda e, pxr=pxr, a_=a_, kc=kc, sl_=sl_, o=o: e.tensor_tensor(out=a_[:], in0=pxr[:, :], in1=Hf[:, o, 0, kc, sl_],
                                                                                             op=ALU.mult), reads=[pxr, Hf], writes=[a_])
                    mk.op("dve", lambda e, pxi=pxi, b_=b_, kc=kc, sl_=sl_, o=o: e.tensor_tensor(out=b_[:], in0=pxi[:, :], in1=Hf[:, o, 1, kc, sl_],
                                                                                             op=ALU.mult), reads=[pxi, Hf], writes=[b_])
                    mk.op("pool", lambda e, a_=a_, b_=b_, kc=kc, sl_=sl_: e.tensor_tensor(out=Y[:, 0, kc, sl_], in0=a_[:], in1=b_[:],
                                                                                         op=ALU.subtract), reads=[a_, b_], writes=[Y])
                    mk.op("dve", lambda e, pxr=pxr, a_=a_, kc=kc, sl_=sl_, o=o: e.tensor_tensor(out=a_[:], in0=pxr[:, :], in1=Hf[:, o, 1, kc, sl_],
                                                                                             op=ALU.mult), reads=[pxr, Hf], writes=[a_])
                    mk.op("dve", lambda e, pxi=pxi, b_=b_, kc=kc, sl_=sl_, o=o: e.tensor_tensor(out=b_[:], in0=pxi[:, :], in1=Hf[:, o, 0, kc, sl_],
                                                                                             op=ALU.mult), reads=[pxi, Hf], writes=[b_])
                    mk.op("pool", lambda e, a_=a_, b_=b_, kc=kc, sl_=sl_: e.tensor_tensor(out=Y[:, 1, kc, sl_], in0=a_[:], in1=b_[:],
                                                                                         op=ALU.add), reads=[a_, b_], writes=[Y])
            ob = obf if o == 0 else obb
            for cj in range(DC):
                py = self.ps[6 + cj % 2]
                for kc in range(4):
                    for ri in range(2):
                        mk.op("pe", lambda e, py=py, kc=kc, ri=ri, cj=cj: e.matmul(
                            py[:, 0:L], Y[:, ri, kc, cj * 128:(cj + 1) * 128], Fi[:, kc, ri, :],
                            start=(kc == 0 and ri == 0), stop=(kc == 3 and ri == 1)), reads=[Y, Fi], writes=[py])
                r_ = rr[cj % 2]
                mk.op("dve", lambda e, py=py, r_=r_, cj=cj, o=o: e.scalar_tensor_tensor(out=r_[:], in0=zf[:, cj, :], scalar=lb[:, o, cj:cj + 1],
                                                                                     in1=py[:, 0:L], op0=ALU.mult, op1=ALU.add),
                      reads=[zf, lb, py], writes=[r_])
                mk.op("pool", lambda e, r_=r_, cj=cj, ob=ob: e.tensor_tensor(out=ob[:, cj, :], in0=r_[:], in1=gt[:, cj, :], op=ALU.mult),
                      reads=[r_, gt], writes=[ob])
            dst = Z1 if o == 0 else HT
            mk.dma("act", dst[0:D, 0:L].rearrange("(j p) t -> p j t", p=128), ob[:], reads=[ob], writes=[dst])
        mk.release(m0)

    Prog.hy_ctx_direct = hy_ctx_direct


_hyd_methods()
```

```python
import math
import numpy as np
import ml_dtypes
import concourse.bass as bass
import concourse.mybir as mybir
from concourse.bass_utils import run_bass_kernel_spmd

F32 = mybir.dt.float32
BF16 = mybir.dt.bfloat16
I32 = mybir.dt.int32
ALU = mybir.AluOpType
AF = mybir.ActivationFunctionType
AX = mybir.AxisListType

D = 1024
DC = 8
CTX = 256
SEQ = 4096
NT = CTX + SEQ
DEPTH = 4
EPS = 1e-6
NE = 32
DEXP = 256
BLOCKS = [(0, CTX)] + [(CTX + 512 * i, 512) for i in range(SEQ // 512)]


class T:
    __slots__ = ("t", "lw", "rd", "name")

    def __init__(self, t, name=""):
        self.t = t
        self.lw = None
        self.rd = {}
        self.name = name

    def __getitem__(self, k):
        return self.t[k]


class Lane:
    def __init__(self, name, eng, sem, step):
        self.name, self.eng, self.sem, self.step = name, eng, sem, step
        self.count = 0
        self.seen = {}


class MK:
    def __init__(self, nc, n_dma_sems=40):
        self.nc = nc
        self._stack = []
        self.lanes = {}
        for name, eng in (("pe", nc.tensor), ("act", nc.scalar), ("dve", nc.vector),
                          ("pool", nc.gpsimd), ("sp", nc.sync)):
            sem = self._enter(nc.semaphore("s_" + name))
            self.lanes[name] = Lane(name, eng, sem, 1)
        self.dma_lanes = []
        for i in range(n_dma_sems):
            sem = self._enter(nc.semaphore("d_%d" % i))
            ln = Lane("dma%d" % i, None, sem, 16)
            self.lanes[ln.name] = ln
            self.dma_lanes.append(ln)
        self.dma_rr = 0
        self.n_inst = 0
        self.uid = 0

    def _enter(self, cm):
        v = cm.__enter__()
        self._stack.append(cm)
        return v

    def mark(self):
        return len(self._stack)

    def release(self, mark):
        self.barrier()
        while len(self._stack) > mark:
            self._stack.pop().__exit__(None, None, None)

    def close(self):
        while self._stack:
            self._stack.pop().__exit__(None, None, None)

    def barrier(self):
        for a in ("pe", "act", "dve", "pool", "sp"):
            la = self.lanes[a]
            for b, lb in self.lanes.items():
                if b == a or lb.count == 0:
                    continue
                if la.seen.get(b, 0) >= lb.count:
                    continue
                la.seen[b] = lb.count
                la.eng.wait_ge(lb.sem, lb.count)

    def sbuf(self, name, shape, dt):
        self.uid += 1
        return T(self._enter(self.nc.sbuf_tensor("%s_%d" % (name, self.uid), list(shape), dt)), name)

    def psum(self, name, shape, dt=F32):
        self.uid += 1
        return T(self._enter(self.nc.psum_tensor("%s_%d" % (name, self.uid), list(shape), dt)), name)

    def _deps(self, lane, reads, writes):
        need = {}
        raw_same = 0
        for t in reads:
            if t.lw is not None:
                ln, idx = t.lw
                if need.get(ln, 0) < idx:
                    need[ln] = idx
                if ln == lane.name:
                    raw_same = max(raw_same, idx)
        for t in writes:
            if t.lw is not None:
                ln, idx = t.lw
                if ln != lane.name and need.get(ln, 0) < idx:
                    need[ln] = idx
            for ln, idx in t.rd.items():
                if ln != lane.name and need.get(ln, 0) < idx:
                    need[ln] = idx
        return need, raw_same

    def _record(self, lane, reads, writes):
        idx = lane.count
        for t in reads:
            if t.rd.get(lane.name, 0) < idx:
                t.rd[lane.name] = idx
        for t in writes:
            t.lw = (lane.name, idx)
            t.rd = {}

    def op(self, lane_name, fn, reads=(), writes=()):
        lane = self.lanes[lane_name]
        need, raw_same = self._deps(lane, reads, writes)
        for ln, idx in need.items():
            if ln == lane.name:
                if lane.name == "pe" or raw_same <= lane.count - 3:
                    continue
                idx = raw_same
            if lane.seen.get(ln, 0) >= idx:
                continue
            lane.seen[ln] = idx
            lane.eng.wait_ge(self.lanes[ln].sem, idx)
        inst = fn(lane.eng)
        lane.count += 1
        inst.then_inc(lane.sem, 1)
        self._record(lane, reads, writes)
        self.n_inst += 1
        return inst

    def dma(self, q, out, in_, reads=(), writes=(), **kw):
        qlane = self.lanes[q]
        dl = self.dma_lanes[self.dma_rr]
        self.dma_rr = (self.dma_rr + 1) % len(self.dma_lanes)
        need, _ = self._deps(dl, reads, writes)
        if dl.count > 0:
            need[dl.name] = max(need.get(dl.name, 0), dl.count)
        for ln, idx in need.items():
            if qlane.seen.get(ln, 0) >= idx:
                continue
            qlane.seen[ln] = idx
            qlane.eng.wait_ge(self.lanes[ln].sem, idx)
        inst = qlane.eng.dma_start(out=out, in_=in_, **kw)
        dl.count += 16
        inst.then_inc(dl.sem, 16)
        self._record(dl, reads, writes)
        self.n_inst += 1
        return inst

    def finish(self):
        sp = self.lanes["sp"]
        for dl in self.dma_lanes:
            if dl.count and sp.seen.get(dl.name, 0) < dl.count:
                sp.seen[dl.name] = dl.count
                sp.eng.wait_ge(dl.sem, dl.count)
        self.barrier()


def _vec_pc(v):
    v = np.asarray(v, np.float32)
    return np.ascontiguousarray(v.reshape(-1, 128).T)


class Prog:
    def __init__(self, cfg=None):
        self.cfg = cfg or {}
        self.nc = bass.Bass("TRN2", target_bir_lowering=False)
        self.mk = MK(self.nc)
        self.ins = {}
        self.dram = {}

    def inp(self, name, shape, dt=F32):
        t = self.nc.dram_tensor(name, list(shape), dt, kind="ExternalInput")
        self.ins[name] = t
        return T(t, name)

    def scratch(self, name, shape, dt, out=False):
        kind = "ExternalOutput" if (out or name in self.cfg.get("dump", ())) else "Internal"
        t = self.nc.dram_tensor(name, list(shape), dt, kind=kind)
        self.dram[name] = T(t, name)
        return self.dram[name]

    def setup_consts(self):
        mk = self.mk
        self.ps = [mk.psum("ps%d" % i, [128, 512], F32) for i in range(8)]
        self.ident_f = mk.sbuf("ident_f", [128, 128], F32)
        self.ident_b = mk.sbuf("ident_b", [128, 128], BF16)
        self.ones_b = mk.sbuf("ones_b", [128, 128], BF16)
        self.avg_b = mk.sbuf("avg_b", [128, 128], BF16)
        mk.op("pool", lambda e: e.memset(self.ident_f[:], 1.0), writes=[self.ident_f])
        mk.op("pool", lambda e: e.affine_select(out=self.ident_f[:], in_=self.ident_f[:],
              pattern=[[-1, 128]], compare_op=ALU.is_equal, fill=0.0, base=0,
              channel_multiplier=1), reads=[self.ident_f], writes=[self.ident_f])
        mk.op("dve", lambda e: e.tensor_copy(self.ident_b[:], self.ident_f[:]),
              reads=[self.ident_f], writes=[self.ident_b])
        mk.op("pool", lambda e: e.memset(self.ones_b[:], 1.0), writes=[self.ones_b])
        mk.op("pool", lambda e: e.memset(self.avg_b[:], 1.0 / D), writes=[self.avg_b])
        self.eps_t = mk.sbuf("eps_t", [128, 1], F32)
        mk.op("pool", lambda e: e.memset(self.eps_t[:], EPS), writes=[self.eps_t])

    def stage_input(self, x_in, ctx_in, XT):
        mk = self.mk
        m0 = mk.mark()
        xin = [mk.sbuf("xin%d" % i, [128, 4, D], F32) for i in range(2)]
        xo = [mk.sbuf("xo%d" % i, [128, DC, 512], F32) for i in range(2)]
        for bi, (t0, tn) in enumerate(BLOCKS):
            nt = tn // 128
            buf = xin[bi % 2]
            src = ctx_in if bi == 0 else x_in
            r0 = 0 if bi == 0 else t0 - CTX
            mk.dma("sp", buf[:, 0:nt, :], src[r0:r0 + tn, :].rearrange("(a p) d -> p a d", p=128),
                   reads=[src], writes=[buf])
            ob = xo[bi % 2]
            for j in range(DC):
                ps = self.ps[j % 8]
                for a in range(nt):
                    mk.op("pe", lambda e, ps=ps, a=a, j=j, buf=buf: e.transpose(
                        ps[:, a * 128:(a + 1) * 128], buf[:, a, j * 128:(j + 1) * 128], self.ident_f[:]),
                        reads=[buf, self.ident_f], writes=[ps])
                eng = "act" if j % 2 else "dve"
                if eng == "act":
                    mk.op("act", lambda e, ps=ps, j=j, ob=ob: e.copy(ob[:, j, 0:tn], ps[:, 0:tn]),
                          reads=[ps], writes=[ob])
                else:
                    mk.op("dve", lambda e, ps=ps, j=j, ob=ob: e.tensor_copy(ob[:, j, 0:tn], ps[:, 0:tn]),
                          reads=[ps], writes=[ob])
            mk.dma("pool", XT[:, t0:t0 + tn].rearrange("(j p) t -> p j t", p=128), ob[:, :, 0:tn],
                   reads=[ob], writes=self.XTr)
        mk.release(m0)

    def stage_output(self, XT, y_out, fg):
        mk = self.mk
        m0 = mk.mark()
        xb = [mk.sbuf("fx%d" % i, [128, DC, 512], F32) for i in range(2)]
        sq = mk.sbuf("fsq", [128, DC, 512], BF16)
        rstd = mk.sbuf("frstd", [128, 512], F32)
        yo = [mk.sbuf("fy%d" % i, [128, 4, D], F32) for i in range(2)]
        for bi, (t0, tn) in enumerate(BLOCKS):
            if bi == 0:
                continue
            buf = xb[bi % 2]
            mk.dma("sp", buf[:, :, 0:tn], XT[:, t0:t0 + tn].rearrange("(j p) t -> p j t", p=128),
                   reads=self.XTr, writes=[buf])
            self._rstd(buf, sq, rstd, tn, self.ps[0])
            for j in range(DC):
                mk.op("dve", lambda e, j=j, buf=buf: e.scalar_tensor_tensor(
                    out=buf[:, j, 0:tn], in0=buf[:, j, 0:tn], scalar=fg[:, j:j + 1], in1=rstd[:, 0:tn],
                    op0=ALU.mult, op1=ALU.mult), reads=[buf, rstd, fg], writes=[buf])
            ob = yo[bi % 2]
            nt = tn // 128
            for a in range(nt):
                for h in range(2):
                    ps = self.ps[1 + (a * 2 + h) % 7]
                    for jj in range(4):
                        j = h * 4 + jj
                        mk.op("pe", lambda e, ps=ps, a=a, j=j, jj=jj, buf=buf: e.transpose(
                            ps[:, jj * 128:(jj + 1) * 128], buf[:, j, a * 128:(a + 1) * 128], self.ident_f[:]),
                            reads=[buf, self.ident_f], writes=[ps])
                    if h:
                        mk.op("act", lambda e, ps=ps, a=a, h=h, ob=ob: e.copy(
                            ob[:, a, h * 512:(h + 1) * 512], ps[:, :]), reads=[ps], writes=[ob])
                    else:
                        mk.op("dve", lambda e, ps=ps, a=a, h=h, ob=ob: e.tensor_copy(
                            ob[:, a, h * 512:(h + 1) * 512], ps[:, :]), reads=[ps], writes=[ob])
            r0 = t0 - CTX
            mk.dma("pool", y_out[r0:r0 + tn, :].rearrange("(a p) d -> p a d", p=128), ob[:, 0:nt, :],
                   reads=[ob], writes=[y_out])
        mk.release(m0)

    def _rstd(self, buf, sq, rstd, tn, ps, eps=EPS):
        mk = self.mk
        for j in range(DC):
            mk.op("act", lambda e, j=j: e.activation(out=sq[:, j, 0:tn], in_=buf[:, j, 0:tn], func=AF.Square),
                  reads=[buf], writes=[sq])
        for j in range(DC):
            mk.op("pe", lambda e, j=j: e.matmul(ps[:, 0:tn], self.avg_b[:], sq[:, j, 0:tn],
                                                 start=(j == 0), stop=(j == DC - 1)),
                  reads=[self.avg_b, sq], writes=[ps])
        mk.op("act", lambda e: e.activation(out=rstd[:, 0:tn], in_=ps[:, 0:tn], func=AF.Sqrt, bias=self.eps_t[:, 0:1]),
              reads=[ps, self.eps_t], writes=[rstd])
        mk.op("dve", lambda e: e.reciprocal(rstd[:, 0:tn], rstd[:, 0:tn]), reads=[rstd], writes=[rstd])

    def ap3(self, t2d, lo, n, inner):
        return t2d[:, lo:lo + n * inner].rearrange("p (a b) -> p a b", b=inner)

    def stage_mod(self, i, sT, w_mod, bmod, gmix, gffn):
        mk = self.mk
        m0 = mk.mark()
        wb = [mk.sbuf("wmod%d" % k, [128, DC, 512], F32) for k in range(2)]
        bm = mk.sbuf("bm", [128, 48], F32)
        gm = mk.sbuf("gm", [128, 2, DC], F32)
        mod = mk.sbuf("mod", [128, 48, 2], F32)
        mk.dma("sp", bm[:], bmod[i, :, :], reads=[bmod], writes=[bm])
        mk.dma("sp", gm[:, 0, :], gmix[i, :, :], reads=[gmix], writes=[gm])
        mk.dma("sp", gm[:, 1, :], gffn[i, :, :], reads=[gffn], writes=[gm])
        ps = self.ps[0]
        for s in range(12):
            w = wb[s % 2]
            mk.dma("sp", w[:], w_mod[i, :, s * 512:(s + 1) * 512].rearrange("(k p) m -> p k m", p=128),
                   reads=[w_mod], writes=[w])
            for mm in range(4):
                m = s * 4 + mm
                for k in range(DC):
                    mk.op("pe", lambda e, w=w, mm=mm, m=m, k=k: e.matmul(
                        ps[:, 2 * m:2 * m + 2], w[:, k, mm * 128:(mm + 1) * 128], sT[:, k, :],
                        start=(k == 0), stop=(k == DC - 1)), reads=[w, sT], writes=[ps])
        psv = ps[:, 0:96].rearrange("p (m r) -> p m r", r=2)
        for r in range(2):
            mk.op("dve", lambda e, r=r: e.tensor_tensor(out=mod[:, :, r], in0=psv[:, :, r], in1=bm[:, :], op=ALU.add),
                  reads=[ps, bm], writes=[mod])
        for r in range(2):
            for nm, sc_c, sh_c, g_c, gi in (("1", 8, 0, 16, 0), ("2", 32, 24, 40, 1)):
                gs = self.mv[("gs" + nm, r)]
                mk.op("dve", lambda e, gs=gs, sc_c=sc_c, gi=gi, r=r: e.scalar_tensor_tensor(
                    out=gs[:], in0=mod[:, sc_c:sc_c + 8, r], scalar=1.0, in1=gm[:, gi, :],
                    op0=ALU.add, op1=ALU.mult), reads=[mod, gm], writes=[gs])
                sh = self.mv[("sh" + nm, r)]
                mk.op("dve", lambda e, sh=sh, sh_c=sh_c, r=r: e.tensor_copy(sh[:], mod[:, sh_c:sh_c + 8, r]),
                      reads=[mod], writes=[sh])
                g = self.mv[("g" + nm, r)]
                mk.op("dve", lambda e, g=g, g_c=g_c, r=r: e.tensor_copy(g[:], mod[:, g_c:g_c + 8, r]),
                      reads=[mod], writes=[g])
        mk.release(m0)

    def alloc_mv(self):
        self.mv = {}
        for r in range(2):
            for nm in ("gs1", "sh1", "g1", "gs2", "sh2", "g2"):
                self.mv[(nm, r)] = self.mk.sbuf("mv_%s_%d" % (nm, r), [128, DC], F32)

    def stage_norm(self, XT, HT, which, router=None):
        mk = self.mk
        m0 = mk.mark()
        xb = [mk.sbuf("nx%d" % k, [128, DC, 512], F32) for k in range(2)]
        sq = mk.sbuf("nsq", [128, DC, 512], BF16)
        rstd = mk.sbuf("nrstd", [128, 512], F32)
        hb = [mk.sbuf("nhb%d" % k, [128, DC, 512], BF16) for k in range(2)]
        if router is not None:
            htk = [mk.sbuf("nhtk%d" % k, [128, D], BF16) for k in range(2)]
            rts = [{k: mk.sbuf("rt%d_%s" % (q, k), [128, n], F32) for k, n in
                    (("lg", 36), ("gmax", 1), ("ngmax", 1), ("ge", 4), ("gsum", 1), ("gp", 1), ("mg", 4), ("es", 8),
                     ("t8", 8), ("dd", 1), ("w2", 1), ("a1", 1), ("a2", 1), ("m1", 8), ("m2", 8), ("cw", 8),
                     ("comb", 32))} for q in range(3)]
            rtn = 0
        for bi, (t0, tn) in enumerate(BLOCKS):
            r = 1 if bi == 0 else 0
            gs, sh = self.mv[("gs" + which, r)], self.mv[("sh" + which, r)]
            buf = xb[bi % 2]
            mk.dma("sp", buf[:, :, 0:tn], XT[:, t0:t0 + tn].rearrange("(j p) t -> p j t", p=128),
                   reads=self.XTr, writes=[buf])
            self._rstd(buf, sq, rstd, tn, self.ps[0])
            h = hb[bi % 2]
            for j in range(DC):
                mk.op("dve", lambda e, j=j, buf=buf, gs=gs: e.scalar_tensor_tensor(
                    out=buf[:, j, 0:tn], in0=buf[:, j, 0:tn], scalar=gs[:, j:j + 1], in1=rstd[:, 0:tn],
                    op0=ALU.mult, op1=ALU.mult), reads=[buf, rstd, gs], writes=[buf])
                if router is not None:
                    mk.op("dve", lambda e, j=j, buf=buf, sh=sh: e.tensor_scalar(
                        out=buf[:, j, 0:tn], in0=buf[:, j, 0:tn], scalar1=sh[:, j:j + 1], scalar2=None,
                        op0=ALU.add), reads=[buf, sh], writes=[buf])
                    mk.op("act", lambda e, j=j, buf=buf, h=h: e.copy(h[:, j, 0:tn], buf[:, j, 0:tn]),
                          reads=[buf], writes=[h])
                else:
                    mk.op("act", lambda e, j=j, buf=buf, h=h, sh=sh: e.activation(
                        out=h[:, j, 0:tn], in_=buf[:, j, 0:tn], func=AF.Identity, bias=sh[:, j:j + 1]),
                        reads=[buf, sh], writes=[h])
            mk.dma("pool", HT[:, t0:t0 + tn].rearrange("(j p) t -> p j t", p=128), h[:, :, 0:tn],
                   reads=[h], writes=[HT])
            if router is not None:
                for a in range(tn // 128):
                    rt = rts[rtn % 3]
                    rtn += 1
                    if "M1a" in router:
                        self._route_tile_sparse(buf, a, t0 + a * 128, router, rt)
                        pb = self.ps[5 + (a % 2)]
                        pbb = pb[:, 0:512].bitcast(BF16)
                        for j in range(DC):
                            mk.op("pe", lambda e, pbb=pbb, j=j, a=a, h=h: e.transpose(
                                pbb[:, j * 128:(j + 1) * 128], h[:, j, a * 128:(a + 1) * 128], self.ident_b[:]),
                                reads=[h, self.ident_b], writes=[pb])
                        ht_ = htk[a % 2]
                        mk.op("act", lambda e, pbb=pbb, ht_=ht_: e.copy(ht_[:], pbb[:, :]), reads=[pb], writes=[ht_])
                        mk.dma("act", self.dram["HTOK"][t0 + a * 128:t0 + (a + 1) * 128, :], ht_[:], reads=[ht_],
                               writes=[self.dram["HTOK"]])
                    else:
                        self._route_tile(buf, a, t0 + a * 128, router, rt)
        mk.release(m0)

    def _route_tile(self, hf, a, tok0, R, rt):
        mk = self.mk
        ps = self.ps[1 + (a % 2)]
        Wr, br, combT = R["Wr"], R["br"], R["combT"]
        for k in range(DC):
            mk.op("pe", lambda e, k=k: e.matmul(ps[:, 0:36], hf[:, k, a * 128:(a + 1) * 128], Wr[:, k, :],
                                                 start=(k == 0), stop=(k == DC - 1)),
                  reads=[hf, Wr], writes=[ps])
        lg, gmax, ngmax, ge, gsum, gp, mg, es = (rt[k] for k in ("lg", "gmax", "ngmax", "ge", "gsum", "gp", "mg", "es"))
        t8, dd, w2, a1, a2, m1, m2, cw, comb = (rt[k] for k in ("t8", "dd", "w2", "a1", "a2", "m1", "m2", "cw", "comb"))
        V = lambda fn, reads, writes: mk.op("dve", fn, reads=reads, writes=writes)
        V(lambda e: e.tensor_tensor(out=lg[:], in0=ps[:, 0:36], in1=br[:], op=ALU.add), [ps, br], [lg])
        V(lambda e: e.reduce_max(out=gmax[:], in_=lg[:, 0:4], axis=AX.X), [lg], [gmax])
        V(lambda e: e.tensor_scalar(out=ngmax[:], in0=gmax[:], scalar1=-1.0, scalar2=None, op0=ALU.mult), [gmax], [ngmax])
        mk.op("act", lambda e: e.activation(out=ge[:], in_=lg[:, 0:4], func=AF.Exp, bias=ngmax[:, 0:1],
                                            accum_out=gsum[:]), reads=[lg, ngmax], writes=[ge, gsum])
        V(lambda e: e.reciprocal(gp[:], gsum[:]), [gsum], [gp])
        V(lambda e: e.tensor_scalar(out=mg[:], in0=lg[:, 0:4], scalar1=gmax[:, 0:1], scalar2=None, op0=ALU.is_equal),
          [lg, gmax], [mg])
        V(lambda e: e.tensor_scalar(out=es[:], in0=lg[:, 4:12], scalar1=mg[:, 0:1], scalar2=None, op0=ALU.mult),
          [lg, mg], [es])
        for g in range(1, 4):
            V(lambda e, g=g: e.scalar_tensor_tensor(out=es[:], in0=lg[:, 4 + 8 * g:12 + 8 * g], scalar=mg[:, g:g + 1],
                                                    in1=es[:], op0=ALU.mult, op1=ALU.add), [lg, mg, es], [es])
        V(lambda e: e.max(out=t8[:], in_=es[:]), [es], [t8])
        V(lambda e: e.tensor_tensor(out=dd[:], in0=t8[:, 1:2], in1=t8[:, 0:1], op=ALU.subtract), [t8], [dd])
        mk.op("act", lambda e: e.activation(out=w2[:], in_=dd[:], func=AF.Sigmoid), reads=[dd], writes=[w2])
        V(lambda e: e.tensor_tensor(out=a2[:], in0=w2[:], in1=gp[:], op=ALU.mult), [w2, gp], [a2])
        V(lambda e: e.tensor_tensor(out=a1[:], in0=gp[:], in1=a2[:], op=ALU.subtract), [gp, a2], [a1])
        V(lambda e: e.tensor_scalar(out=m1[:], in0=es[:], scalar1=t8[:, 0:1], scalar2=a1[:, 0:1], op0=ALU.is_equal,
                                    op1=ALU.mult), [es, t8, a1], [m1])
        V(lambda e: e.tensor_scalar(out=m2[:], in0=es[:], scalar1=t8[:, 1:2], scalar2=a2[:, 0:1], op0=ALU.is_equal,
                                    op1=ALU.mult), [es, t8, a2], [m2])
        V(lambda e: e.tensor_tensor(out=cw[:], in0=m1[:], in1=m2[:], op=ALU.add), [m1, m2], [cw])
        for g in range(4):
            V(lambda e, g=g: e.tensor_scalar(out=comb[:, 8 * g:8 * g + 8], in0=cw[:], scalar1=mg[:, g:g + 1],
                                             scalar2=None, op0=ALU.mult), [cw, mg], [comb])
        ps2 = self.ps[3 + (a % 2)]
        mk.op("pe", lambda e: e.transpose(ps2[0:32, 0:128], comb[:, :], self.ident_f[:]),
              reads=[comb, self.ident_f], writes=[ps2])
        mk.op("act", lambda e: e.copy(combT[:, tok0:tok0 + 128], ps2[0:32, 0:128]), reads=[ps2], writes=[combT])

    def stage_moe(self, i, XT, HT, HID, combT, sel, w_gate, w_up, w_down):
        mk = self.mk
        m0 = mk.mark()
        hT = mk.sbuf("moe_hT", [128, DC, NT], BF16)
        for j in range(DC):
            mk.dma("sp", hT[:, j, :], HT[j * 128:(j + 1) * 128, :], reads=[HT], writes=[hT])
        wgu = [mk.sbuf("wgu%d" % k, [128, DC, 512], BF16) for k in range(2)]
        cbs = [mk.sbuf("cbs%d" % k, [128, 512], F32) for k in range(2)]
        sl = [mk.sbuf("sl%d" % k, [128, 512], F32) for k in range(2)]
        tt = [mk.sbuf("tt%d" % k, [128, 512], F32) for k in range(2)]
        hid = [mk.sbuf("hid%d" % k, [128, 2, 512], BF16) for k in range(2)]
        n = 0
        for ex in range(NE):
            g, el = ex // 8, ex % 8
            w = wgu[ex % 2]
            mk.dma("pool", w[:, :, 0:256], w_gate[i, g, el, :, :].rearrange("(k p) f -> p k f", p=128),
                   reads=[w_gate], writes=[w])
            mk.dma("pool", w[:, :, 256:512], w_up[i, g, el, :, :].rearrange("(k p) f -> p k f", p=128),
                   reads=[w_up], writes=[w])
            for bi, (t0, tn) in enumerate(BLOCKS):
                pc = self.ps[n % 2]
                mk.op("pe", lambda e, pc=pc, ex=ex: e.matmul(pc[:, 0:tn], sel[:, ex, :], combT[:, t0:t0 + tn],
                                                            start=True, stop=True),
                      reads=[sel, combT], writes=[pc])
                cb = cbs[n % 2]
                mk.op("act", lambda e, pc=pc, cb=cb: e.copy(cb[:, 0:tn], pc[:, 0:tn]), reads=[pc], writes=[cb])
                hd = hid[n % 2]
                for fc in range(2):
                    q = (2 * n + fc) % 3
                    pg, pu = self.ps[2 + 2 * q], self.ps[3 + 2 * q]
                    for k in range(DC):
                        mk.op("pe", lambda e, pg=pg, k=k, fc=fc, w=w: e.matmul(
                            pg[:, 0:tn], w[:, k, fc * 128:(fc + 1) * 128], hT[:, k, t0:t0 + tn],
                            start=(k == 0), stop=(k == DC - 1)), reads=[w, hT], writes=[pg])
                    for k in range(DC):
                        mk.op("pe", lambda e, pu=pu, k=k, fc=fc, w=w: e.matmul(
                            pu[:, 0:tn], w[:, k, 256 + fc * 128:256 + (fc + 1) * 128], hT[:, k, t0:t0 + tn],
                            start=(k == 0), stop=(k == DC - 1)), reads=[w, hT], writes=[pu])
                    s_, t_ = sl[fc], tt[fc]
                    mk.op("act", lambda e, pg=pg, s_=s_: e.activation(out=s_[:, 0:tn], in_=pg[:, 0:tn], func=AF.Silu),
                          reads=[pg], writes=[s_])
                    mk.op("dve", lambda e, pu=pu, s_=s_, t_=t_: e.tensor_tensor(
                        out=t_[:, 0:tn], in0=pu[:, 0:tn], in1=s_[:, 0:tn], op=ALU.mult), reads=[pu, s_], writes=[t_])
                    mk.op("pool", lambda e, t_=t_, cb=cb, hd=hd, fc=fc: e.tensor_tensor(
                        out=hd[:, fc, 0:tn], in0=t_[:, 0:tn], in1=cb[:, 0:tn], op=ALU.mult), reads=[t_, cb], writes=[hd])
                mk.dma("act", HID[ex * 256:(ex + 1) * 256, t0:t0 + tn].rearrange("(c p) t -> p c t", p=128),
                       hd[:, :, 0:tn], reads=[hd], writes=[HID])
                n += 1
        mk.release(m0)
        m0 = mk.mark()
        hb = [mk.sbuf("p2h%d" % k, [128, 64, 512], BF16) for k in range(1)]
        wd = [mk.sbuf("p2w%d" % k, [128, 8, D], BF16) for k in range(2)]
        xr = [mk.sbuf("p2x%d" % k, [128, 512], F32) for k in range(3)]
        wdv = w_down[i].rearrange("g e f d -> (g e f) d")
        n = 0
        for bi, (t0, tn) in enumerate(BLOCKS):
            r = 1 if bi == 0 else 0
            g2 = self.mv[("g2", r)]
            h = hb[0]
            for c8 in range(8):
                mk.dma("sp", h[:, c8 * 8:(c8 + 1) * 8, 0:tn],
                       HID[c8 * 1024:(c8 + 1) * 1024, t0:t0 + tn].rearrange("(c p) t -> p c t", p=128),
                       reads=[HID], writes=[h])
            for kg in range(8):
                w = wd[n % 2]
                n += 1
                mk.dma("pool", w[:], wdv[kg * 1024:(kg + 1) * 1024, :].rearrange("(c p) d -> p c d", p=128),
                       reads=[w_down], writes=[w])
                for kk in range(8):
                    kc = kg * 8 + kk
                    for dc in range(DC):
                        mk.op("pe", lambda e, dc=dc, kk=kk, kc=kc, w=w: e.matmul(
                            self.ps[dc][:, 0:tn], w[:, kk, dc * 128:(dc + 1) * 128], h[:, kc, 0:tn],
                            start=(kc == 0), stop=(kc == 63)), reads=[w, h], writes=[self.ps[dc]])
            for dc in range(DC):
                x = xr[dc % 3]
                mk.dma("sp", x[:, 0:tn], XT[dc * 128:(dc + 1) * 128, t0:t0 + tn], reads=[self.XTr[dc]], writes=[x])
                mk.op("dve", lambda e, dc=dc, x=x, g2=g2: e.scalar_tensor_tensor(
                    out=x[:, 0:tn], in0=self.ps[dc][:, 0:tn], scalar=g2[:, dc:dc + 1], in1=x[:, 0:tn],
                    op0=ALU.mult, op1=ALU.add), reads=[self.ps[dc], g2, x], writes=[x])
                mk.dma("act", XT[dc * 128:(dc + 1) * 128, t0:t0 + tn], x[:, 0:tn], reads=[x], writes=[self.XTr[dc]])
        mk.release(m0)


def build_program(cfg=None):
    cfg = cfg or {}
    p = Prog(cfg)
    mk = p.mk
    layers = cfg.get("layers", list(range(DEPTH)))
    I = {}
    I["x"] = p.inp("x", [SEQ, D])
    I["ctx"] = p.inp("ctx", [CTX, D])
    I["c_pc"] = p.inp("c_pc", [128, DC, 2])
    I["w_mod"] = p.inp("w_mod", [DEPTH, D, 6 * D])
    I["bmod_pc"] = p.inp("bmod_pc", [DEPTH, 128, 48])
    I["gmix_pc"] = p.inp("gmix_pc", [DEPTH, 128, DC])
    I["gffn_pc"] = p.inp("gffn_pc", [DEPTH, 128, DC])
    I["fg_pc"] = p.inp("fg_pc", [128, DC])
    I["wr_pc"] = p.inp("wr_pc", [DEPTH, 128, DC, 36])
    I["br_bc"] = p.inp("br_bc", [DEPTH, 128, 36])
    I["sel"] = p.inp("sel", [32, NE, 128], BF16)
    sparse = cfg.get("sparse", True)
    if sparse:
        for li in range(DEPTH):
            I["moe_wgu%d" % li] = p.inp("moe_wgu%d" % li, [NE * 128, DC * 512])
            I["moe_wdn%d" % li] = p.inp("moe_wdn%d" % li, [NE * 128, 2 * D])
        I["jgrid"] = p.inp("jgrid", [128, NBLK])
        I["pidx"] = p.inp("pidx", [128, 1])
    else:
        I["moe_w_gate"] = p.inp("moe_w_gate", [DEPTH, 4, 8, D, DEXP])
        I["moe_w_up"] = p.inp("moe_w_up", [DEPTH, 4, 8, D, DEXP])
        I["moe_w_down"] = p.inp("moe_w_down", [DEPTH, 4, 8, DEXP, D])
    I["attn_w_q"] = p.inp("attn_w_q", [2, D, D])
    I["attn_w_kv"] = p.inp("attn_w_kv", [2, D, 512])
    I["attn_w_o"] = p.inp("attn_w_o", [2, D, D])
    I["qkgain_pc"] = p.inp("qkgain_pc", [2, 128, 2])
    I["conv_w_pw1"] = p.inp("conv_w_pw1", [1, D, 2 * D])
    I["conv_w_pw2"] = p.inp("conv_w_pw2", [1, D, D])
    I["conv_b_pw1_pc"] = p.inp("conv_b_pw1_pc", [1, 128, 16])
    I["conv_w_dw_pc"] = p.inp("conv_w_dw_pc", [1, 128, DC, 31])
    I["conv_b_dw_pc"] = p.inp("conv_b_dw_pc", [1, 128, DC])
    I["conv_ln_g_pc"] = p.inp("conv_ln_g_pc", [1, 128, DC])
    I["conv_ln_b_pc"] = p.inp("conv_ln_b_pc", [1, 128, DC])
    I["conv_b_pw2_pc"] = p.inp("conv_b_pw2_pc", [1, 128, DC])
    I["hy_w_in"] = p.inp("hy_w_in", [1, D, 3 * D])
    I["hy_w_out"] = p.inp("hy_w_out", [1, D, D])
    I["hy_b_in_pc"] = p.inp("hy_b_in_pc", [128, 24])
    I["hy_w_short_pc"] = p.inp("hy_w_short_pc", [128, 24, 3])
    I["hy_b_short_pc"] = p.inp("hy_b_short_pc", [128, 24])
    I["hy_b_out_pc"] = p.inp("hy_b_out_pc", [128, DC])
    I["hy_lbias_pc"] = p.inp("hy_lbias_pc", [2, 128, DC])
    I["hy_f_w1"] = p.inp("hy_f_w1", [1, 17, 64])
    I["hy_f_w2"] = p.inp("hy_f_w2", [1, 64, 64])
    I["hy_f_w3"] = p.inp("hy_f_w3", [1, 64, 4 * D])
    I["hy_fvec"] = p.inp("hy_fvec", [64, 4])
    I["hy_dabs"] = p.inp("hy_dabs", [64, D])
    for tag, L_ in (("lat", SEQ), ("ctx", CTX)):
        N1_ = 2 * L_ // 64
        I["hy_F1_" + tag] = p.inp("hy_F1_" + tag, [N1_ // 2, 6, N1_], BF16)
        I["hy_G_" + tag] = p.inp("hy_G_" + tag, [128, N1_, 5, 128], BF16)
        I["hy_Fi_" + tag] = p.inp("hy_Fi_" + tag, [N1_, 2, N1_ // 2], BF16)
        I["hy_lneg_" + tag] = p.inp("hy_lneg_" + tag, [N1_ // 2, 64])
        I["hy_featsT_" + tag] = p.inp("hy_featsT_" + tag, [17, L_])
    I["hyd_F"] = p.inp("hyd_F", [128, 2, 3, 512], BF16)
    I["hyd_Fi"] = p.inp("hyd_Fi", [128, 4, 2, CTX], BF16)
    I["hyd_lneg"] = p.inp("hyd_lneg", [128, 2])
    I["ropeR"] = p.inp("ropeR", [128, 128], BF16)
    I["ropeC"] = p.inp("ropeC", [128, SEQ])
    I["ropeS"] = p.inp("ropeS", [128, SEQ])
    y = T(p.nc.dram_tensor("y", [SEQ, D], F32, kind="ExternalOutput"), "y")
    QT = p.scratch("QT", [D, NT], BF16)
    OT = p.scratch("OT", [D, NT], BF16)
    XT = p.scratch("XT", [D, NT], F32)
    p.XTr = [T(XT.t, "XTr%d" % k) for k in range(DC)]
    VT = p.scratch("VT", [D, NT], F32)
    if 2 in layers and cfg.get("mixers", True):
        p.scratch("hyU0", [3 * D, NT], F32)
        p.scratch("hyZ", [3 * D, NT], F32)
        p.scratch("hyZ1", [D, NT], F32)
        p.scratch("hyA", [2, 128, 64, D], BF16)
        p.scratch("hyC", [2, 64, 128, D], BF16)
        for o in range(2):
            p.scratch("hyHH_lat%d" % o, [2, 128, 128, D], BF16)
            p.scratch("hyHH_ctx%d" % o, [2, 8, 128, D], BF16)
    HT = p.scratch("HT", [D, NT], BF16)
    p.dram["HT"] = HT
    if sparse:
        p.scratch("HTOK", [NT, D], BF16)
        p.scratch("HS", [NSLOT, D], BF16)
        p.scratch("YS", [NSLOT, D], BF16)
    else:
        HID = p.scratch("HID", [NE * DEXP, NT], BF16)
    p.setup_consts()
    p.alloc_mv()
    fg = mk.sbuf("fg", [128, DC], F32)
    sT = mk.sbuf("sT", [128, DC, 2], F32)
    sel = mk.sbuf("sel", [32, NE, 128], BF16)
    if sparse:
        SR = p.alloc_sparse()
        mk.dma("sp", SR["jg"][:], I["jgrid"][:, :], writes=[SR["jg"]])
        mk.dma("sp", SR["pidx"][:], I["pidx"][:, :], writes=[SR["pidx"]])
    else:
        combT = mk.sbuf("combT", [32, NT], BF16)
    Wr = mk.sbuf("Wr", [128, DC, 36], F32)
    br = mk.sbuf("br", [128, 36], F32)
    mk.dma("sp", fg[:], I["fg_pc"][:, :], reads=[I["fg_pc"]], writes=[fg])
    mk.dma("sp", sT[:], I["c_pc"][:, :, :], reads=[I["c_pc"]], writes=[sT])
    mk.dma("sp", sel[:], I["sel"][:, :, :], reads=[I["sel"]], writes=[sel])
    mk.op("act", lambda e: e.activation(out=sT[:], in_=sT[:], func=AF.Silu), reads=[sT], writes=[sT])
    p.stage_input(I["x"], I["ctx"], XT)
    for i in layers:
        p.stage_mod(i, sT, I["w_mod"], I["bmod_pc"], I["gmix_pc"], I["gffn_pc"])
        if cfg.get("mixers", True):
            kind, slot = i % 3, i // 3
            with_ctx = i < DEPTH - 1
            p.stage_norm(XT, HT, "1")
            if kind == 0:
                p.stage_attn(slot, XT, HT, QT, OT, I, with_ctx)
            elif kind == 1:
                p.stage_conf(slot, XT, HT, QT, VT, I)
            else:
                p.stage_hyena(slot, XT, HT, I)
        if cfg.get("moe", True) is False:
            continue
        mk.dma("sp", Wr[:], I["wr_pc"][i, :, :, :], reads=[I["wr_pc"]], writes=[Wr])
        mk.dma("sp", br[:], I["br_bc"][i, :, :], reads=[I["br_bc"]], writes=[br])
        if sparse:
            SR["Wr"], SR["br"] = Wr, br
            p.stage_norm(XT, HT, "2", router=SR)
            p.stage_moe_sparse(i, XT, SR, I)
        else:
            p.stage_norm(XT, HT, "2", router=dict(Wr=Wr, br=br, combT=combT))
            p.stage_moe(i, XT, HT, HID, combT, sel, I["moe_w_gate"], I["moe_w_up"], I["moe_w_down"])
    p.stage_output(XT, y, fg)
    mk.finish()
    mk.close()
    return p


_HYC = {}


def host_inputs(inp, b, sparse=True):
    f = lambda a: np.ascontiguousarray(np.asarray(a, np.float32))
    m = {}
    m["x"] = f(inp["x"][b])
    m["ctx"] = f(inp["ctx"][b])
    m["c_pc"] = np.ascontiguousarray(np.stack([_vec_pc(inp["c"][b]), _vec_pc(inp["c_ctx"])], axis=-1))
    m["w_mod"] = f(inp["w_mod"])
    m["bmod_pc"] = np.stack([_vec_pc(inp["b_mod"][i]) for i in range(DEPTH)])
    m["gmix_pc"] = np.stack([_vec_pc(inp["norm_mix_g"][i]) for i in range(DEPTH)])
    m["gffn_pc"] = np.stack([_vec_pc(inp["norm_ffn_g"][i]) for i in range(DEPTH)])
    m["fg_pc"] = _vec_pc(inp["final_norm_g"])
    wr = np.concatenate([np.asarray(inp["moe_w_group"]), np.asarray(inp["moe_w_router"])], axis=-1)
    m["wr_pc"] = np.ascontiguousarray(wr.reshape(DEPTH, DC, 128, 36).transpose(0, 2, 1, 3)).astype(np.float32)
    brr = np.concatenate([np.asarray(inp["moe_b_group"]), np.asarray(inp["moe_b_router"])], axis=-1)
    m["br_bc"] = np.ascontiguousarray(np.broadcast_to(brr[:, None, :], (DEPTH, 128, 36))).astype(np.float32)
    sel = np.zeros((32, NE, 128), np.float32)
    for e in range(NE):
        sel[e, e, :] = 1.0
    m["sel"] = sel.astype(ml_dtypes.bfloat16)
    m["attn_w_q"] = f(inp["attn_w_q"]); m["attn_w_kv"] = f(inp["attn_w_kv"]); m["attn_w_o"] = f(inp["attn_w_o"])
    m["qkgain_pc"] = np.ascontiguousarray(np.stack([np.asarray(inp["attn_q_gain"], np.float32),
                                                    np.asarray(inp["attn_k_gain"], np.float32)], axis=-1))
    m["conv_w_pw1"] = f(inp["conv_w_pw1"]); m["conv_w_pw2"] = f(inp["conv_w_pw2"])
    m["conv_b_pw1_pc"] = _vec_pc(inp["conv_b_pw1"][0])[None]
    m["conv_w_dw_pc"] = np.ascontiguousarray(np.asarray(inp["conv_w_dw"][0], np.float32).reshape(31, DC, 128).transpose(2, 1, 0))[None]
    for nm in ("conv_b_dw", "conv_ln_g", "conv_ln_b", "conv_b_pw2"):
        m[nm + "_pc"] = _vec_pc(inp[nm][0])[None]
    m["hy_w_in"] = f(inp["hy_w_in"]); m["hy_w_out"] = f(inp["hy_w_out"])
    m["hy_b_in_pc"] = _vec_pc(inp["hy_b_in"][0]); m["hy_b_short_pc"] = _vec_pc(inp["hy_b_short"][0])
    m["hy_w_short_pc"] = np.ascontiguousarray(np.stack([_vec_pc(inp["hy_w_short"][0, k]) for k in range(3)], axis=-1))
    m["hy_b_out_pc"] = _vec_pc(inp["hy_b_out"][0])
    m["hy_lbias_pc"] = np.stack([_vec_pc(inp["hy_long_bias"][0, o]) for o in range(2)])
    m["hy_f_w1"] = f(inp["hy_f_w1"]); m["hy_f_w2"] = f(inp["hy_f_w2"]); m["hy_f_w3"] = f(inp["hy_f_w3"])
    m["hy_fvec"] = np.ascontiguousarray(np.stack([np.asarray(inp[k][0], np.float32) for k in
                                                  ("hy_f_b1", "hy_f_freq1", "hy_f_b2", "hy_f_freq2")], axis=-1))
    dl = np.abs(np.linspace(math.log(1e-2) / 1.5, math.log(1e-2) / 0.3, D, dtype=np.float32))
    m["hy_dabs"] = np.ascontiguousarray(np.broadcast_to(dl[None, :], (64, D))).astype(np.float32)
    for tag, L_ in (("lat", SEQ), ("ctx", CTX)):
        hc = _HYC[tag] if tag in _HYC else _HYC.setdefault(tag, hy_consts(L_))
        m["hy_F1_" + tag] = hc["F1"]; m["hy_G_" + tag] = hc["G"]; m["hy_Fi_" + tag] = hc["Fi"]
        m["hy_lneg_" + tag] = hc["lneg"]; m["hy_featsT_" + tag] = hc["featsT"]
    hdc = _HYC["hyd"] if "hyd" in _HYC else _HYC.setdefault("hyd", hyd_consts())
    m["hyd_F"] = hdc["F"]; m["hyd_Fi"] = hdc["Fi"]; m["hyd_lneg"] = hdc["lneg"]
    RT = np.zeros((128, 128), np.float32)
    for base in (0, 64):
        for dd in range(32):
            RT[base + dd + 32, base + dd] = -1.0
            RT[base + dd, base + dd + 32] = 1.0
    m["ropeR"] = RT.astype(ml_dtypes.bfloat16)
    inv = (np.float32(10000.0) ** (-np.arange(32, dtype=np.float32) / np.float32(32))).astype(np.float32)
    tt = np.arange(SEQ)
    ang = np.zeros((128, SEQ), np.float32)
    for dd in range(128):
        pos = (tt // 64) if dd < 64 else (tt % 64)
        ang[dd] = pos.astype(np.float32) * inv[dd % 32]
    m["ropeC"] = np.cos(ang).astype(np.float32)
    m["ropeS"] = np.sin(ang).astype(np.float32)
    if sparse:
        wg = np.asarray(inp["moe_w_gate"], np.float32).reshape(DEPTH, NE, DC, 128, DEXP)
        wu = np.asarray(inp["moe_w_up"], np.float32).reshape(DEPTH, NE, DC, 128, DEXP)
        wd = np.asarray(inp["moe_w_down"], np.float32).reshape(DEPTH, NE, 2, 128, D)
        for li in range(DEPTH):
            m["moe_wgu%d" % li] = np.ascontiguousarray(np.concatenate([wg[li], wu[li]], axis=-1).transpose(0, 2, 1, 3)).reshape(NE * 128, DC * 512)
            m["moe_wdn%d" % li] = np.ascontiguousarray(wd[li].transpose(0, 2, 1, 3)).reshape(NE * 128, 2 * D)
        m["jgrid"] = np.ascontiguousarray(np.broadcast_to((np.arange(NBLK, dtype=np.float32) * BSL)[None, :], (128, NBLK)))
        m["pidx"] = np.arange(128, dtype=np.float32)[:, None].copy()
    else:
        m["moe_w_gate"] = f(inp["moe_w_gate"])
        m["moe_w_up"] = f(inp["moe_w_up"])
        m["moe_w_down"] = f(inp["moe_w_down"])
    return m


def _attn_methods():
    def load_w(self, name, src_ap, kc, m, q="pool"):
        mk = self.mk
        w = mk.sbuf(name, [128, kc, m], BF16)
        step = max(1, 4096 // m)
        for k0 in range(0, kc, step):
            k1 = min(kc, k0 + step)
            mk.dma(q, w[:, k0:k1, :], src_ap[k0 * 128:k1 * 128, :].rearrange("(k p) m -> p k m", p=128), writes=[w])
        return w

    def linear_resid(self, XT, srcT, W, KC, gname, bias=None, skip_ctx=False):
        mk = self.mk
        m0 = mk.mark()
        sb = [mk.sbuf("lr_s%d" % k, [128, KC, 512], BF16) for k in range(2)]
        xr = [mk.sbuf("lr_x%d" % k, [128, 512], F32) for k in range(3)]
        gb = None
        if bias is not None:
            gb = [mk.sbuf("lr_gb%d" % r, [128, DC], F32) for r in range(2)]
            for r in range(2):
                mk.op("dve", lambda e, r=r: e.tensor_tensor(out=gb[r][:], in0=bias[:], in1=self.mv[(gname, r)][:],
                                                            op=ALU.mult), reads=[bias, self.mv[(gname, r)]], writes=[gb[r]])
        n = 0
        for bi, (t0, tn) in enumerate(BLOCKS):
            if skip_ctx and bi == 0:
                continue
            r = 1 if bi == 0 else 0
            g = self.mv[(gname, r)]
            s = sb[bi % 2]
            mk.dma("sp", s[:, :, 0:tn], srcT[:, t0:t0 + tn].rearrange("(k p) t -> p k t", p=128),
                   reads=[srcT], writes=[s])
            for dc in range(DC):
                ps = self.ps[n % 4]
                x = xr[n % 3]
                n += 1
                mk.dma("sp", x[:, 0:tn], XT[dc * 128:(dc + 1) * 128, t0:t0 + tn], reads=[self.XTr[dc]], writes=[x])
                for k in range(KC):
                    mk.op("pe", lambda e, ps=ps, k=k, dc=dc, s=s: e.matmul(
                        ps[:, 0:tn], W[:, k, dc * 128:(dc + 1) * 128], s[:, k, 0:tn],
                        start=(k == 0), stop=(k == KC - 1)), reads=[W, s], writes=[ps])
                mk.op("dve", lambda e, ps=ps, dc=dc, x=x, g=g: e.scalar_tensor_tensor(
                    out=x[:, 0:tn], in0=ps[:, 0:tn], scalar=g[:, dc:dc + 1], in1=x[:, 0:tn],
                    op0=ALU.mult, op1=ALU.add), reads=[ps, g, x], writes=[x])
                if gb is not None:
                    mk.op("dve", lambda e, dc=dc, x=x, r=r: e.tensor_scalar(
                        out=x[:, 0:tn], in0=x[:, 0:tn], scalar1=gb[r][:, dc:dc + 1], scalar2=None, op0=ALU.add),
                        reads=[x, gb[r]], writes=[x])
                mk.dma("act", XT[dc * 128:(dc + 1) * 128, t0:t0 + tn], x[:, 0:tn], reads=[x], writes=[self.XTr[dc]])
        mk.release(m0)

    def stage_attn(self, slot, XT, HT, QT, OT, I, with_ctx):
        mk = self.mk
        mA = mk.mark()
        KT = mk.sbuf("KT", [128, 2, NT], BF16)
        Vs = mk.sbuf("Vs", [128, NT // 128, 256], BF16)
        gains = mk.sbuf("qkgain", [128, 2], F32)
        RT = mk.sbuf("RT", [128, 128], BF16)
        avgh = mk.sbuf("avgh", [128, 128], BF16)
        mk.dma("sp", gains[:], I["qkgain_pc"][slot, :, :], writes=[gains])
        mk.dma("sp", RT[:], I["ropeR"][:, :], writes=[RT])
        mk.op("pool", lambda e: e.memset(avgh[:], 1.0 / 128), writes=[avgh])
        m0 = mk.mark()
        wq = self.load_w("wq", I["attn_w_q"][slot], DC, D)
        wkv = self.load_w("wkv", I["attn_w_kv"][slot], DC, 512)
        hb = [mk.sbuf("ah%d" % k, [128, DC, 512], BF16) for k in range(2)]
        cs = [mk.sbuf("acs%d" % k, [128, 2, 512], F32) for k in range(2)]
        sqq = [mk.sbuf("asq%d" % k, [128, 512], BF16) for k in range(2)]
        rs = [mk.sbuf("ars%d" % k, [128, 512], F32) for k in range(2)]
        qn = [mk.sbuf("aqn%d" % k, [128, 512], F32) for k in range(2)]
        qb = [mk.sbuf("aqb%d" % k, [128, 512], BF16) for k in range(2)]
        t1 = [mk.sbuf("at1%d" % k, [128, 512], F32) for k in range(2)]
        t2 = [mk.sbuf("at2%d" % k, [128, 512], F32) for k in range(2)]
        qo = [mk.sbuf("aqo%d" % k, [128, 512], BF16) for k in range(3)]
        n = 0
        for bi, (t0, tn) in enumerate(BLOCKS):
            h = hb[bi % 2]
            mk.dma("sp", h[:, :, 0:tn], HT[:, t0:t0 + tn].rearrange("(k p) t -> p k t", p=128), reads=[HT], writes=[h])
            c = cs[bi % 2]
            if bi > 0:
                mk.dma("sp", c[:, 0, 0:tn], I["ropeC"][:, t0 - CTX:t0 - CTX + tn], writes=[c])
                mk.dma("sp", c[:, 1, 0:tn], I["ropeS"][:, t0 - CTX:t0 - CTX + tn], writes=[c])
            for hh in range(10):
                isq = hh < 8
                if isq and bi == 0 and not with_ctx:
                    continue
                W = wq if isq else wkv
                c0 = hh * 128 if isq else (hh - 8) * 128
                gcol = 0 if isq else 1
                ps = self.ps[n % 2]
                pz = self.ps[2 + n % 2]
                pr = self.ps[4 + n % 2]
                i2 = n % 2
                n += 1
                for k in range(DC):
                    mk.op("pe", lambda e, ps=ps, k=k, W=W, c0=c0, h=h: e.matmul(
                        ps[:, 0:tn], W[:, k, c0:c0 + 128], h[:, k, 0:tn], start=(k == 0), stop=(k == DC - 1)),
                        reads=[W, h], writes=[ps])
                mk.op("act", lambda e, ps=ps, i2=i2: e.activation(out=sqq[i2][:, 0:tn], in_=ps[:, 0:tn], func=AF.Square),
                      reads=[ps], writes=[sqq[i2]])
                mk.op("pe", lambda e, pz=pz, i2=i2: e.matmul(pz[:, 0:tn], avgh[:], sqq[i2][:, 0:tn], start=True, stop=True),
                      reads=[avgh, sqq[i2]], writes=[pz])
                mk.op("act", lambda e, pz=pz, i2=i2: e.activation(out=rs[i2][:, 0:tn], in_=pz[:, 0:tn], func=AF.Sqrt,
                                                                 bias=self.eps_t[:, 0:1]), reads=[pz, self.eps_t], writes=[rs[i2]])
                mk.op("dve", lambda e, i2=i2: e.reciprocal(rs[i2][:, 0:tn], rs[i2][:, 0:tn]), reads=[rs[i2]], writes=[rs[i2]])
                mk.op("dve", lambda e, ps=ps, i2=i2, gcol=gcol: e.scalar_tensor_tensor(
                    out=qn[i2][:, 0:tn], in0=ps[:, 0:tn], scalar=gains[:, gcol:gcol + 1], in1=rs[i2][:, 0:tn],
                    op0=ALU.mult, op1=ALU.mult), reads=[ps, gains, rs[i2]], writes=[qn[i2]])
                if isq:
                    dst_t = qo[n % 3]
                    dst = dst_t[:, 0:tn]
                else:
                    dst_t = KT
                    dst = KT[:, hh - 8, t0:t0 + tn]
                if bi == 0:
                    mk.op("act", lambda e, i2=i2, dst=dst: e.copy(dst, qn[i2][:, 0:tn]), reads=[qn[i2]], writes=[dst_t])
                else:
                    mk.op("act", lambda e, i2=i2: e.copy(qb[i2][:, 0:tn], qn[i2][:, 0:tn]), reads=[qn[i2]], writes=[qb[i2]])
                    mk.op("pe", lambda e, pr=pr, i2=i2: e.matmul(pr[:, 0:tn], RT[:], qb[i2][:, 0:tn], start=True, stop=True),
                          reads=[RT, qb[i2]], writes=[pr])
                    mk.op("pool", lambda e, i2=i2, c=c: e.tensor_tensor(out=t1[i2][:, 0:tn], in0=qn[i2][:, 0:tn],
                                                                       in1=c[:, 0, 0:tn], op=ALU.mult),
                          reads=[qn[i2], c], writes=[t1[i2]])
                    mk.op("dve", lambda e, pr=pr, i2=i2, c=c: e.tensor_tensor(out=t2[i2][:, 0:tn], in0=pr[:, 0:tn],
                                                                             in1=c[:, 1, 0:tn], op=ALU.mult),
                          reads=[pr, c], writes=[t2[i2]])
                    mk.op("pool", lambda e, i2=i2, dst=dst: e.tensor_tensor(out=dst, in0=t1[i2][:, 0:tn],
                                                                           in1=t2[i2][:, 0:tn], op=ALU.add),
                          reads=[t1[i2], t2[i2]], writes=[dst_t])
                if isq:
                    mk.dma("act", QT[hh * 128:(hh + 1) * 128, t0:t0 + tn], dst, reads=[dst_t], writes=[QT])
            for a in range(tn // 128):
                pv = self.ps[6 + a % 2]
                for k in range(DC):
                    mk.op("pe", lambda e, pv=pv, k=k, a=a, h=h: e.matmul(
                        pv[:, 0:256], h[:, k, a * 128:(a + 1) * 128], wkv[:, k, 256:512],
                        start=(k == 0), stop=(k == DC - 1)), reads=[h, wkv], writes=[pv])
                ti = t0 // 128 + a
                mk.op("act", lambda e, pv=pv, ti=ti: e.copy(Vs[:, ti, :], pv[:, 0:256]), reads=[pv], writes=[Vs])
        mk.release(m0)
        m0 = mk.mark()
        qblk = [mk.sbuf("bq%d" % k, [128, 512], BF16) for k in range(2)]
        pT = [mk.sbuf("bp%d" % k, [128, 512], BF16) for k in range(4)]
        rz = [mk.sbuf("brz%d" % k, [128, 512], F32) for k in range(2)]
        zacc = [mk.sbuf("bza%d" % k, [128, 512], F32) for k in range(2)]
        ones_f = mk.sbuf("bones_f", [128, 128], F32)
        mk.op("pool", lambda e: e.memset(ones_f[:], 1.0), writes=[ones_f])
        ob = [mk.sbuf("bo%d" % k, [128, 512], BF16) for k in range(2)]
        scale = 128 ** -0.5
        nb = 0
        nk = 0
        for head in range(8):
            kvh = head // 4
            for bi, (t0, tn) in enumerate(BLOCKS):
                if bi == 0 and not with_ctx:
                    continue
                nkc = 2 if bi == 0 else NT // 128
                q = qblk[nb % 2]
                mk.dma("sp", q[:, 0:tn], QT[head * 128:(head + 1) * 128, t0:t0 + tn], reads=[QT], writes=[q])
                pO = self.ps[4 + nb % 2]
                pZ = self.ps[6 + nb % 2]
                def issue_S(kc_, idx):
                    pS_ = self.ps[idx % 4]
                    mk.op("pe", lambda e, pS_=pS_, kc_=kc_, q=q: e.matmul(
                        pS_[:, 0:tn], KT[:, kvh, kc_ * 128:(kc_ + 1) * 128], q[:, 0:tn], start=True, stop=True),
                        reads=[KT, q], writes=[pS_])
                issue_S(0, nk)
                if nkc > 1:
                    issue_S(1, nk + 1)
                for kc in range(nkc):
                    pS = self.ps[nk % 4]
                    p_ = pT[nk % 4]
                    if kc + 2 < nkc:
                        issue_S(kc + 2, nk + 2)
                    nk += 1
                    mk.op("act", lambda e, pS=pS, p_=p_: e.activation(out=p_[:, 0:tn], in_=pS[:, 0:tn], func=AF.Exp,
                                                                     scale=scale), reads=[pS], writes=[p_])
                    mk.op("pe", lambda e, pO=pO, kc=kc, p_=p_: e.matmul(
                        pO[:, 0:tn], Vs[:, kc, kvh * 128:(kvh + 1) * 128], p_[:, 0:tn],
                        start=(kc == 0), stop=(kc == nkc - 1)), reads=[Vs, p_], writes=[pO])
                    mk.op("pe", lambda e, pZ=pZ, kc=kc, p_=p_: e.matmul(
                        pZ[:, 0:tn], self.ones_b[:], p_[:, 0:tn], start=(kc == 0), stop=(kc == nkc - 1)),
                        reads=[self.ones_b, p_], writes=[pZ])
                r_ = rz[nb % 2]
                o_ = ob[nb % 2]
                mk.op("dve", lambda e, pZ=pZ, r_=r_: e.reciprocal(r_[:, 0:tn], pZ[:, 0:tn]), reads=[pZ], writes=[r_])
                mk.op("dve", lambda e, pO=pO, r_=r_, o_=o_: e.tensor_tensor(out=o_[:, 0:tn], in0=pO[:, 0:tn],
                                                                           in1=r_[:, 0:tn], op=ALU.mult),
                      reads=[pO, r_], writes=[o_])
                mk.dma("act", OT[head * 128:(head + 1) * 128, t0:t0 + tn], o_[:, 0:tn], reads=[o_], writes=[OT])
                nb += 1
        mk.release(m0)
        mk.release(mA)
        m0 = mk.mark()
        wo = self.load_w("wo", I["attn_w_o"][slot], DC, D)
        self.linear_resid(XT, OT, wo, DC, "g1", skip_ctx=not with_ctx)
        mk.release(m0)

    Prog.load_w = load_w
    Prog.linear_resid = linear_resid
    Prog.stage_attn = stage_attn


_attn_methods()


def _conf_methods():
    def stage_conf(self, slot, XT, HT, UT, VT, I):
        mk = self.mk
        m0 = mk.mark()
        W = self.load_w("wpw1", I["conv_w_pw1"][slot], DC, 2 * D)
        b1 = mk.sbuf("cb1", [128, 16], F32)
        mk.dma("sp", b1[:], I["conv_b_pw1_pc"][slot, :, :], writes=[b1])
        hb = [mk.sbuf("ch%d" % k, [128, DC, 512], BF16) for k in range(2)]
        sg = [mk.sbuf("csg%d" % k, [128, 512], F32) for k in range(2)]
        ub = [mk.sbuf("cu%d" % k, [128, DC, 512], BF16) for k in range(2)]
        n = 0
        for bi, (t0, tn) in enumerate(BLOCKS):
            h = hb[bi % 2]
            u = ub[bi % 2]
            mk.dma("sp", h[:, :, 0:tn], HT[:, t0:t0 + tn].rearrange("(k p) t -> p k t", p=128), reads=[HT], writes=[h])
            for j in range(DC):
                pa, pg = self.ps[(2 * n) % 8], self.ps[(2 * n + 1) % 8]
                s_ = sg[n % 2]
                n += 1
                for k in range(DC):
                    mk.op("pe", lambda e, pa=pa, k=k, j=j, h=h: e.matmul(
                        pa[:, 0:tn], W[:, k, j * 128:(j + 1) * 128], h[:, k, 0:tn], start=(k == 0), stop=(k == DC - 1)),
                        reads=[W, h], writes=[pa])
                for k in range(DC):
                    mk.op("pe", lambda e, pg=pg, k=k, j=j, h=h: e.matmul(
                        pg[:, 0:tn], W[:, k, D + j * 128:D + (j + 1) * 128], h[:, k, 0:tn], start=(k == 0), stop=(k == DC - 1)),
                        reads=[W, h], writes=[pg])
                mk.op("act", lambda e, pg=pg, s_=s_, j=j: e.activation(out=s_[:, 0:tn], in_=pg[:, 0:tn], func=AF.Sigmoid,
                                                                      bias=b1[:, 8 + j:9 + j]), reads=[pg, b1], writes=[s_])
                mk.op("dve", lambda e, pa=pa, s_=s_, j=j, u=u: e.scalar_tensor_tensor(
                    out=u[:, j, 0:tn], in0=pa[:, 0:tn], scalar=b1[:, j:j + 1], in1=s_[:, 0:tn], op0=ALU.add, op1=ALU.mult),
                    reads=[pa, b1, s_], writes=[u])
            mk.dma("act", UT[:, t0:t0 + tn].rearrange("(k p) t -> p k t", p=128), u[:, :, 0:tn], reads=[u], writes=[UT])
        mk.release(m0)
        m0 = mk.mark()
        wdw = mk.sbuf("cwdw", [128, DC, 31], F32)
        bdw = mk.sbuf("cbdw", [128, DC], F32)
        mk.dma("sp", wdw[:], I["conv_w_dw_pc"][slot, :, :, :], writes=[wdw])
        mk.dma("sp", bdw[:], I["conv_b_dw_pc"][slot, :, :], writes=[bdw])
        up = [mk.sbuf("cup%d" % k, [128, NT + 60], BF16) for k in range(2)]
        dg = [mk.sbuf("cdg%d" % k, [128, 31, 128], BF16) for k in range(2)]
        vo = [mk.sbuf("cvo%d" % k, [128, 512], F32) for k in range(3)]
        for k in range(2):
            mk.op("pool", lambda e, k=k: e.memset(up[k][:], 0.0), writes=[up[k]])
        n = 0
        for j in range(DC):
            u = up[j % 2]
            d_ = dg[j % 2]
            mk.dma("sp", u[:, 15:15 + CTX], UT[j * 128:(j + 1) * 128, 0:CTX], reads=[UT], writes=[u])
            mk.dma("sp", u[:, 45 + CTX:45 + CTX + SEQ], UT[j * 128:(j + 1) * 128, CTX:NT], reads=[UT], writes=[u])
            for k in range(31):
                mk.op("dve", lambda e, k=k, j=j, d_=d_: e.tensor_scalar(
                    out=d_[:, k, :], in0=self.ident_b[:], scalar1=wdw[:, j, k:k + 1], scalar2=None, op0=ALU.mult),
                    reads=[self.ident_b, wdw], writes=[d_])
            for bi, (t0, tn) in enumerate(BLOCKS):
                base = 0 if bi == 0 else 30
                ps = self.ps[n % 4]
                v = vo[n % 3]
                n += 1
                for k in range(31):
                    mk.op("pe", lambda e, ps=ps, k=k, u=u, d_=d_, s0=base + t0 + k: e.matmul(
                        ps[:, 0:tn], d_[:, k, :], u[:, s0:s0 + tn], start=(k == 0), stop=(k == 30)),
                        reads=[d_, u], writes=[ps])
                mk.op("act", lambda e, ps=ps, v=v, j=j: e.activation(out=v[:, 0:tn], in_=ps[:, 0:tn], func=AF.Identity,
                                                                    bias=bdw[:, j:j + 1]), reads=[ps, bdw], writes=[v])
                mk.dma("act", VT[j * 128:(j + 1) * 128, t0:t0 + tn], v[:, 0:tn], reads=[v], writes=[VT])
        mk.release(m0)
        m0 = mk.mark()
        lng = mk.sbuf("clng", [128, DC], F32)
        lnb = mk.sbuf("clnb", [128, DC], F32)
        mk.dma("sp", lng[:], I["conv_ln_g_pc"][slot, :, :], writes=[lng])
        mk.dma("sp", lnb[:], I["conv_ln_b_pc"][slot, :, :], writes=[lnb])
        avgf = mk.sbuf("cavgf", [128, 128], F32)
        mk.op("pool", lambda e: e.memset(avgf[:], 1.0 / D), writes=[avgf])
        vb = [mk.sbuf("cv%d" % k, [128, DC, 512], F32) for k in range(2)]
        sq = mk.sbuf("csq", [128, DC, 512], F32)
        mu = mk.sbuf("cmu", [128, 512], F32)
        var = mk.sbuf("cvar", [128, 512], F32)
        ob = [mk.sbuf("co%d" % k, [128, DC, 512], BF16) for k in range(2)]
        for bi, (t0, tn) in enumerate(BLOCKS):
            v = vb[bi % 2]
            o = ob[bi % 2]
            mk.dma("sp", v[:, :, 0:tn], VT[:, t0:t0 + tn].rearrange("(k p) t -> p k t", p=128), reads=[VT], writes=[v])
            pm, pq = self.ps[0], self.ps[1]
            for j in range(DC):
                mk.op("act", lambda e, j=j, v=v: e.activation(out=sq[:, j, 0:tn], in_=v[:, j, 0:tn], func=AF.Square),
                      reads=[v], writes=[sq])
            for j in range(DC):
                mk.op("pe", lambda e, j=j, v=v: e.matmul(pm[:, 0:tn], avgf[:], v[:, j, 0:tn], start=(j == 0), stop=(j == DC - 1)),
                      reads=[avgf, v], writes=[pm])
            for j in range(DC):
                mk.op("pe", lambda e, j=j: e.matmul(pq[:, 0:tn], avgf[:], sq[:, j, 0:tn], start=(j == 0), stop=(j == DC - 1)),
                      reads=[avgf, sq], writes=[pq])
            mk.op("act", lambda e: e.copy(mu[:, 0:tn], pm[:, 0:tn]), reads=[pm], writes=[mu])
            mk.op("dve", lambda e: e.tensor_tensor(out=var[:, 0:tn], in0=mu[:, 0:tn], in1=mu[:, 0:tn], op=ALU.mult),
                  reads=[mu], writes=[var])
            mk.op("dve", lambda e: e.tensor_tensor(out=var[:, 0:tn], in0=pq[:, 0:tn], in1=var[:, 0:tn], op=ALU.subtract),
                  reads=[pq, var], writes=[var])
            mk.op("act", lambda e: e.activation(out=var[:, 0:tn], in_=var[:, 0:tn], func=AF.Sqrt, bias=self.eps_t[:, 0:1]),
                  reads=[var, self.eps_t], writes=[var])
            mk.op("dve", lambda e: e.reciprocal(var[:, 0:tn], var[:, 0:tn]), reads=[var], writes=[var])
            for j in range(DC):
                mk.op("pool", lambda e, j=j, v=v: e.tensor_tensor(out=v[:, j, 0:tn], in0=v[:, j, 0:tn], in1=mu[:, 0:tn],
                                                                 op=ALU.subtract), reads=[v, mu], writes=[v])
                mk.op("dve", lambda e, j=j, v=v: e.scalar_tensor_tensor(
                    out=v[:, j, 0:tn], in0=v[:, j, 0:tn], scalar=lng[:, j:j + 1], in1=var[:, 0:tn], op0=ALU.mult, op1=ALU.mult),
                    reads=[v, lng, var], writes=[v])
                mk.op("act", lambda e, j=j, v=v, o=o: e.activation(out=o[:, j, 0:tn], in_=v[:, j, 0:tn], func=AF.Silu,
                                                                  bias=lnb[:, j:j + 1]), reads=[v, lnb], writes=[o])
            mk.dma("act", HT[:, t0:t0 + tn].rearrange("(k p) t -> p k t", p=128), o[:, :, 0:tn], reads=[o], writes=[HT])
        mk.release(m0)
        m0 = mk.mark()
        w2 = self.load_w("wpw2", I["conv_w_pw2"][slot], DC, D)
        b2 = mk.sbuf("cb2", [128, DC], F32)
        mk.dma("sp", b2[:], I["conv_b_pw2_pc"][slot, :, :], writes=[b2])
        self.linear_resid(XT, HT, w2, DC, "g1", bias=b2)
        mk.release(m0)

    Prog.stage_conf = stage_conf


_conf_methods()


def hy_consts(L):
    N = 2 * L
    N1 = N // 64
    H = N1 // 2
    bf = ml_dtypes.bfloat16
    c = {}
    n1 = np.arange(H)[:, None].astype(np.float64)
    k1 = np.arange(N1)[None, :].astype(np.float64)
    def cs(ang):
        return np.cos(ang), -np.sin(ang)
    fc, fs = cs(2 * np.pi * n1 * k1 / N1)
    bc, bs = cs(2 * np.pi * (N1 - 1 - n1) * k1 / N1)
    b0c, b0s = cs(2 * np.pi * (N1 - n1) * k1 / N1)
    c["F1"] = np.stack([fc, fs, bc, bs, b0c, b0s], 1).astype(bf)
    n2 = np.arange(64)[:, None].astype(np.float64)
    k2 = np.arange(64)[None, :].astype(np.float64)
    G = np.zeros((N1, 128, 5, 128), np.float64)
    for kk in range(N1):
        ang = 2 * np.pi * (n2 * kk / N + n2 * k2 / 64)
        Gr, Gi = np.cos(ang), -np.sin(ang)
        Mr, Mi = Gr.T, -Gi.T
        blocks = [((Gr, Gi), (-Gi, Gr)), ((-Gi, Gr), (-Gr, -Gi)), ((Gr, Gr), (-Gi, -Gi)),
                  ((Gi, Gi), (Gr, Gr)), ((Mr, Mi), (-Mi, Mr))]
        for f, ((a, b), (cc, d)) in enumerate(blocks):
            G[kk, 0:64, f, 0:64] = a
            G[kk, 0:64, f, 64:128] = b
            G[kk, 64:128, f, 0:64] = cc
            G[kk, 64:128, f, 64:128] = d
    c["G"] = np.ascontiguousarray(G.transpose(1, 0, 2, 3)).astype(bf)
    t1 = np.arange(H)[None, :].astype(np.float64)
    kk1 = np.arange(N1)[:, None].astype(np.float64)
    th = 2 * np.pi * t1 * kk1 / N1
    c["Fi"] = np.stack([np.cos(th) / N, -np.sin(th) / N], 1).astype(bf)
    pos = (64 * np.arange(H)[:, None] + np.arange(64)[None, :]).astype(np.float32)
    c["lneg"] = (-(pos / np.float32(L - 1))).astype(np.float32)
    p = np.arange(L, dtype=np.float32)[:, None]
    t = p / np.float32(L - 1)
    w = np.float32(2.0 * math.pi) * p / np.float32(L)
    bands = np.linspace(1e-4, 7, 8, dtype=np.float32)
    feats = np.concatenate([t, np.cos(bands * w), -np.sin(bands * w)], axis=-1).astype(np.float32)
    c["featsT"] = np.ascontiguousarray(feats.T)
    return c


def _hy_methods():
    def hy_filter(self, S, I, HH):
        mk = self.mk
        L, N1, H, tag = S["L"], S["N1"], S["H"], S["tag"]
        m0 = mk.mark()
        ft = mk.sbuf("hf_ft", [17, L], F32)
        W1 = mk.sbuf("hf_w1", [17, 64], F32)
        W2 = mk.sbuf("hf_w2", [64, 64], F32)
        W3 = mk.sbuf("hf_w3", [64, 4096], BF16)
        fv = mk.sbuf("hf_fv", [64, 4], F32)
        fb = mk.sbuf("hf_fb", [64, 2], F32)
        z1 = mk.sbuf("hf_z1", [64, L], F32)
        z2 = mk.sbuf("hf_z2", [64, L], F32)
        z2b = mk.sbuf("hf_z2b", [64, L], BF16)
        tmp = mk.sbuf("hf_tmp", [64, 512], F32)
        F1 = mk.sbuf("hf_F1", [max(H, 1), 6, N1], BF16)
        lneg = mk.sbuf("hf_lneg", [H, 64], F32)
        dab = mk.sbuf("hf_dab", [H, D], F32)
        mk.dma("sp", ft[:], I["hy_featsT_" + tag][:, :], writes=[ft])
        mk.dma("sp", W1[:], I["hy_f_w1"][0, :, :], writes=[W1])
        mk.dma("sp", W2[:], I["hy_f_w2"][0, :, :], writes=[W2])
        mk.dma("pool", W3[:], I["hy_f_w3"][0, :, :], writes=[W3])
        mk.dma("sp", fv[:], I["hy_fvec"][:, :], writes=[fv])
        mk.dma("sp", F1[:], I["hy_F1_" + tag][:, :, :], writes=[F1])
        mk.dma("sp", lneg[:], I["hy_lneg_" + tag][:, :], writes=[lneg])
        mk.dma("sp", dab[:], I["hy_dabs"][0:H, :], writes=[dab])
        mk.op("dve", lambda e: e.tensor_tensor(out=fb[:, 0:1], in0=fv[:, 0:1], in1=fv[:, 1:2], op=ALU.mult), reads=[fv], writes=[fb])
        mk.op("dve", lambda e: e.tensor_tensor(out=fb[:, 1:2], in0=fv[:, 2:3], in1=fv[:, 3:4], op=ALU.mult), reads=[fv], writes=[fb])
        for layer, (Wm, src, dst, kin) in enumerate(((W1, ft, z1, 17), (W2, z1, z2, 64))):
            for c0 in range(0, L, 512):
                cn = min(512, L - c0)
                ps = self.ps[(c0 // 512) % 2]
                mk.op("pe", lambda e, ps=ps, Wm=Wm, src=src, c0=c0, cn=cn, kin=kin: e.matmul(
                    ps[0:64, 0:cn], Wm[0:kin, :], src[0:kin, c0:c0 + cn], start=True, stop=True), reads=[Wm, src], writes=[ps])
                d_ = dst[:, c0:c0 + cn]
                mk.op("dve", lambda e, ps=ps, d_=d_, cn=cn, layer=layer: e.tensor_scalar(
                    out=d_, in0=ps[0:64, 0:cn], scalar1=fv[:, 2 * layer + 1:2 * layer + 2], scalar2=fb[:, layer:layer + 1],
                    op0=ALU.mult, op1=ALU.add), reads=[ps, fv, fb], writes=[dst])
                for _ in range(2):
                    mk.op("dve", lambda e, d_=d_, cn=cn: e.tensor_scalar(out=tmp[:, 0:cn], in0=d_, scalar1=math.pi,
                          scalar2=2 * math.pi, op0=ALU.is_gt, op1=ALU.mult), reads=[dst], writes=[tmp])
                    mk.op("dve", lambda e, d_=d_, cn=cn: e.tensor_tensor(out=d_, in0=d_, in1=tmp[:, 0:cn], op=ALU.subtract),
                          reads=[dst, tmp], writes=[dst])
                    mk.op("dve", lambda e, d_=d_, cn=cn: e.tensor_scalar(out=tmp[:, 0:cn], in0=d_, scalar1=-math.pi,
                          scalar2=2 * math.pi, op0=ALU.is_lt, op1=ALU.mult), reads=[dst], writes=[tmp])
                    mk.op("dve", lambda e, d_=d_, cn=cn: e.tensor_tensor(out=d_, in0=d_, in1=tmp[:, 0:cn], op=ALU.add),
                          reads=[dst, tmp], writes=[dst])
                mk.op("act", lambda e, d_=d_: e.activation(out=d_, in_=d_, func=AF.Sin), reads=[dst], writes=[dst])
        mk.op("act", lambda e: e.copy(z2b[:], z2[:]), reads=[z2], writes=[z2b])
        dec = [mk.sbuf("hf_dec%d" % k, [H, D], F32) for k in range(4)]
        hd = [mk.sbuf("hf_hd%d" % k, [H, D], F32) for k in range(4)]
        hdb = [mk.sbuf("hf_hdb%d" % k, [H, D], BF16) for k in range(4)]
        ha = [mk.sbuf("hf_ha%d" % k, [H, D], BF16) for k in range(4)]
        Asb = [mk.sbuf("hf_A%d" % k, [N1, 2, D], BF16) for k in range(2)]
        rn = mk.sbuf("hf_rn", [128, D], F32)
        A = self.dram["hyA"]
        Bt = [mk.sbuf("hf_B%d" % k, [128, D], BF16) for k in range(2)]
        Gt = [mk.sbuf("hf_Gt%d" % k, [128, 2, 128], BF16) for k in range(2)]
        Ho = [mk.sbuf("hf_Ho%d" % k, [128, D], BF16) for k in range(4)]
        for o in range(2):
            pn = (self.ps[6], self.ps[7])
            units = [(n2_, dr_) for n2_ in range(64) for dr_ in range(2)]

            def issue_h(ui):
                n2_, dr_ = units[ui]
                col_ = n2_ if dr_ == 0 else (64 - n2_) % 64
                for hf in range(2):
                    ph = self.ps[(ui % 2) * 2 + hf]
                    w0 = (dr_ * 2 + o) * D + hf * 512
                    mk.op("pe", lambda e, ph=ph, col_=col_, w0=w0: e.matmul(
                        ph[0:H, :], z2b[:, col_:L:64], W3[:, w0:w0 + 512], start=True, stop=True),
                        reads=[z2b, W3], writes=[ph])
                i2_ = dr_ + 2 * (n2_ % 2)
                mk.op("act", lambda e, i2_=i2_, col_=col_: e.activation(out=dec[i2_][:], in_=dab[:], func=AF.Exp,
                                                                       scale=lneg[:, col_:col_ + 1]), reads=[dab, lneg], writes=[dec[i2_]])
            issue_h(0)
            for n2 in range(64):
                n2b = (64 - n2) % 64
                hb2 = []
                for dr in range(2):
                    ui = n2 * 2 + dr
                    col = n2 if dr == 0 else n2b
                    i2 = dr + 2 * (n2 % 2)
                    if ui + 1 < len(units):
                        issue_h(ui + 1)
                    for hf in range(2):
                        ph = self.ps[(ui % 2) * 2 + hf]
                        mk.op("dve", lambda e, ph=ph, i2=i2, hf=hf: e.tensor_tensor(
                            out=hd[i2][:, hf * 512:(hf + 1) * 512], in0=ph[0:H, :], in1=dec[i2][:, hf * 512:(hf + 1) * 512],
                            op=ALU.mult), reads=[ph, dec[i2]], writes=[hd[i2]])
                    if dr == 1 and n2 == 0:
                        mk.op("dve", lambda e, i2=i2: e.memset(hd[i2][0:1, :], 0.0), reads=[hd[i2]], writes=[hd[i2]])
                    hb_ = hdb[(n2 % 2) * 2 + dr]
                    hb2.append(hb_)
                    mk.op("act", lambda e, i2=i2, hb_=hb_: e.copy(hb_[:], hd[i2][:]), reads=[hd[i2]], writes=[hb_])
                    mk.op("dve", lambda e, i2=i2: e.scalar_tensor_tensor(out=ha[i2][:], in0=hd[i2][:], scalar=-1.0, in1=hd[i2][:],
                                                                        op0=ALU.mult, op1=ALU.max), reads=[hd[i2]], writes=[ha[i2]])
                    for hf in range(2):
                        first = (n2 == 0 and dr == 0)
                        last = (n2 == 63 and dr == 1)
                        mk.op("pe", lambda e, i2=i2, hf=hf, first=first, last=last: e.matmul(
                            pn[hf][:, :], self.ones_b[0:H, :], ha[i2][:, hf * 512:(hf + 1) * 512], start=first, stop=last),
                            reads=[self.ones_b, ha[i2]], writes=[pn[hf]])
                As = Asb[n2 % 2]
                fbi = 4 if n2 == 0 else 2
                for ri in range(2):
                    for hf in range(2):
                        pA = self.ps[4 + hf]
                        mk.op("pe", lambda e, pA=pA, ri=ri, hf=hf, hb2=hb2: e.matmul(
                            pA[0:N1, :], F1[0:H, ri, :], hb2[0][:, hf * 512:(hf + 1) * 512], start=True, stop=False),
                            reads=[F1, hb2[0]], writes=[pA])
                        mk.op("pe", lambda e, pA=pA, ri=ri, hf=hf, hb2=hb2, fbi=fbi: e.matmul(
                            pA[0:N1, :], F1[0:H, fbi + ri, :], hb2[1][:, hf * 512:(hf + 1) * 512], start=False, stop=True),
                            reads=[F1, hb2[1]], writes=[pA])
                        eng = "act" if hf else "dve"
                        if eng == "act":
                            mk.op("act", lambda e, pA=pA, ri=ri, hf=hf, As=As: e.copy(As[:, ri, hf * 512:(hf + 1) * 512], pA[0:N1, :]),
                                  reads=[pA], writes=[As])
                        else:
                            mk.op("dve", lambda e, pA=pA, ri=ri, hf=hf, As=As: e.tensor_copy(As[:, ri, hf * 512:(hf + 1) * 512], pA[0:N1, :]),
                                  reads=[pA], writes=[As])
                for ri in range(2):
                    mk.dma("act", A[ri, 0:N1, n2, :], As[:, ri, :], reads=[As], writes=[A])
            for hf in range(2):
                mk.op("dve", lambda e, hf=hf: e.reciprocal(rn[:, hf * 512:(hf + 1) * 512], pn[hf][:, :]), reads=[pn[hf]], writes=[rn])
            for kk in range(N1):
                B = Bt[kk % 2]
                for ri in range(2):
                    mk.dma("sp", B[ri * 64:(ri + 1) * 64, :], A[ri, kk, :, :], reads=[A], writes=[B])
                g_ = Gt[kk % 2]
                mk.dma("sp", g_[:], I["hy_G_" + tag][:, kk, 2:4, :], writes=[g_])
                for ri in range(2):
                    ho = Ho[(2 * kk + ri) % 4]
                    for hf in range(2):
                        pX = self.ps[(kk % 2) * 4 + ri * 2 + hf]
                        mk.op("pe", lambda e, pX=pX, ri=ri, hf=hf, B=B, g_=g_: e.matmul(
                            pX[:, :], g_[:, ri, :], B[:, hf * 512:(hf + 1) * 512], start=True, stop=True), reads=[g_, B], writes=[pX])
                        mk.op("dve", lambda e, pX=pX, hf=hf, ho=ho: e.tensor_tensor(
                            out=ho[:, hf * 512:(hf + 1) * 512], in0=pX[:, :], in1=rn[:, hf * 512:(hf + 1) * 512], op=ALU.mult),
                            reads=[pX, rn], writes=[ho])
                    mk.dma("act", HH[o][ri, kk, :, :], ho[:], reads=[ho], writes=[HH[o]])
        mk.release(m0)

    Prog.hy_filter = hy_filter


_hy_methods()


def _hy_methods2():
    def hy_conv(self, S, o, I, zsrc, zrow0, gate, grow0, lbias, HH, dst, dst_bf16):
        mk = self.mk
        L, N1, H, tag, col0 = S["L"], S["N1"], S["H"], S["tag"], S["col0"]
        A, Cs = self.dram["hyA"], self.dram["hyC"]
        m0 = mk.mark()
        zb = mk.sbuf("hc_zb", [128, DC, L], BF16)
        F1 = mk.sbuf("hc_F1", [max(H, 1), 6, N1], BF16)
        mk.dma("sp", F1[:], I["hy_F1_" + tag][:, :, :], writes=[F1])
        for j in range(DC):
            mk.dma("pool", zb[:, j, :], zsrc[zrow0 + j * 128:zrow0 + (j + 1) * 128, col0:col0 + L], reads=[zsrc], writes=[zb])
        zT = [mk.sbuf("hc_zT%d" % k, [H, D], BF16) for k in range(2)]
        Asb = [mk.sbuf("hc_A%d" % k, [N1, 2, D], BF16) for k in range(2)]
        for n2 in range(64):
            pt = self.ps[n2 % 2]
            ptb = pt[:, 0:512].bitcast(BF16)
            for j in range(DC):
                mk.op("pe", lambda e, ptb=ptb, j=j, n2=n2: e.transpose(ptb[0:H, j * 128:(j + 1) * 128], zb[:, j, n2:L:64],
                                                                      self.ident_b[:]), reads=[zb, self.ident_b], writes=[pt])
            z_ = zT[n2 % 2]
            mk.op("act", lambda e, ptb=ptb, z_=z_: e.copy(z_[:], ptb[0:H, :]), reads=[pt], writes=[z_])
            As = Asb[n2 % 2]
            for ri in range(2):
                for hf in range(2):
                    pA = self.ps[2 + ri * 2 + hf]
                    mk.op("pe", lambda e, pA=pA, ri=ri, hf=hf, z_=z_: e.matmul(
                        pA[0:N1, :], F1[0:H, ri, :], z_[:, hf * 512:(hf + 1) * 512], start=True, stop=True),
                        reads=[F1, z_], writes=[pA])
                    if hf:
                        mk.op("act", lambda e, pA=pA, ri=ri, hf=hf, As=As: e.copy(As[:, ri, hf * 512:(hf + 1) * 512], pA[0:N1, :]),
                              reads=[pA], writes=[As])
                    else:
                        mk.op("dve", lambda e, pA=pA, ri=ri, hf=hf, As=As: e.tensor_copy(As[:, ri, hf * 512:(hf + 1) * 512], pA[0:N1, :]),
                              reads=[pA], writes=[As])
            for ri in range(2):
                mk.dma("act", A[ri, 0:N1, n2, :], As[:, ri, :], reads=[As], writes=[A])
        mk.release(m0)
        m0 = mk.mark()
        Bt = [mk.sbuf("hc_B%d" % k, [128, D], BF16) for k in range(2)]
        Gt = [mk.sbuf("hc_G%d" % k, [128, 5, 128], BF16) for k in range(2)]
        Hr = [mk.sbuf("hc_Hr%d" % k, [128, D], BF16) for k in range(2)]
        Hi = [mk.sbuf("hc_Hi%d" % k, [128, D], BF16) for k in range(2)]
        ta = [mk.sbuf("hc_ta%d" % k, [128, D], F32) for k in range(2)]
        tb = [mk.sbuf("hc_tb%d" % k, [128, D], F32) for k in range(2)]
        Y = [mk.sbuf("hc_Y%d" % k, [128, D], BF16) for k in range(2)]
        Cb = [mk.sbuf("hc_C%d" % k, [128, D], BF16) for k in range(2)]
        def f2_load(kk):
            i2 = kk % 2
            B, g_, hr, hi = Bt[i2], Gt[i2], Hr[i2], Hi[i2]
            for ri in range(2):
                mk.dma("sp", B[ri * 64:(ri + 1) * 64, :], A[ri, kk, :, :], reads=[A], writes=[B])
            mk.dma("sp", g_[:], I["hy_G_" + tag][:, kk, :, :], writes=[g_])
            mk.dma("sp", hr[:], HH[o][0, kk, :, :], reads=[HH[o]], writes=[hr])
            mk.dma("sp", hi[:], HH[o][1, kk, :, :], reads=[HH[o]], writes=[hi])

        def f2_X(u):
            kk, hf = divmod(u, 2)
            i2 = kk % 2
            sl = slice(hf * 512, (hf + 1) * 512)
            pa, pb = self.ps[(u % 2) * 2], self.ps[(u % 2) * 2 + 1]
            mk.op("pe", lambda e: e.matmul(pa[:, :], Gt[i2][:, 0, :], Bt[i2][:, sl], start=True, stop=True),
                  reads=[Gt[i2], Bt[i2]], writes=[pa])
            mk.op("pe", lambda e: e.matmul(pb[:, :], Gt[i2][:, 1, :], Bt[i2][:, sl], start=True, stop=True),
                  reads=[Gt[i2], Bt[i2]], writes=[pb])
        f2_load(0)
        if N1 > 1:
            f2_load(1)
        f2_X(0)
        for u in range(2 * N1):
            kk, hf = divmod(u, 2)
            i2 = kk % 2
            hr, hi = Hr[i2], Hi[i2]
            sl = slice(hf * 512, (hf + 1) * 512)
            pa, pb = self.ps[(u % 2) * 2], self.ps[(u % 2) * 2 + 1]
            if u + 1 < 2 * N1:
                f2_X(u + 1)
            mk.op("dve", lambda e, pa=pa, sl=sl, hr=hr, i2=i2: e.tensor_tensor(out=ta[i2][:, sl], in0=pa[:, :], in1=hr[:, sl],
                                                                              op=ALU.mult), reads=[pa, hr], writes=[ta[i2]])
            mk.op("dve", lambda e, pb=pb, sl=sl, hi=hi, i2=i2: e.tensor_tensor(out=tb[i2][:, sl], in0=pb[:, :], in1=hi[:, sl],
                                                                              op=ALU.mult), reads=[pb, hi], writes=[tb[i2]])
            mk.op("pool", lambda e, sl=sl, i2=i2: e.tensor_tensor(out=Y[i2][:, sl], in0=ta[i2][:, sl], in1=tb[i2][:, sl],
                                                                 op=ALU.add), reads=[ta[i2], tb[i2]], writes=[Y[i2]])
            pc = self.ps[4 + u % 4]
            mk.op("pe", lambda e, pc=pc, sl=sl, i2=i2: e.matmul(pc[:, :], Gt[i2][:, 4, :], Y[i2][:, sl], start=True, stop=True),
                  reads=[Gt[i2], Y[i2]], writes=[pc])
            mk.op("act", lambda e, pc=pc, sl=sl, i2=i2: e.copy(Cb[i2][:, sl], pc[:, :]), reads=[pc], writes=[Cb[i2]])
            if hf == 1:
                for ri in range(2):
                    mk.dma("act", Cs[ri, :, kk, :], Cb[i2][ri * 64:(ri + 1) * 64, :], reads=[Cb[i2]], writes=[Cs])
                if kk + 2 < N1:
                    f2_load(kk + 2)
        mk.release(m0)
        m0 = mk.mark()
        Fi = mk.sbuf("hc_Fi", [N1, 2, max(H, 1)], BF16)
        mk.dma("sp", Fi[:], I["hy_Fi_" + tag][:, :, :], writes=[Fi])
        lb = mk.sbuf("hc_lb", [128, DC], F32)
        mk.dma("sp", lb[:], lbias, writes=[lb])
        yh = mk.sbuf("hc_yh", [128, 4, L], F32)
        Ct = [mk.sbuf("hc_Ct%d" % k, [N1, 2, 512], BF16) for k in range(3)]
        zr = [mk.sbuf("hc_zr%d" % k, [128, L], F32) for k in range(2)]
        gr = [mk.sbuf("hc_gr%d" % k, [128, L], F32) for k in range(2)]
        ob = [mk.sbuf("hc_ob%d" % k, [128, L], BF16 if dst_bf16 else F32) for k in range(2)]
        per_bank = max(1, 512 // (4 * H))
        for half in range(2):
            for t2 in range(64):
                c_ = Ct[t2 % 3]
                mk.dma("sp", c_[:], Cs[:, t2, 0:N1, half * 512:(half + 1) * 512].rearrange("r k c -> k r c"), reads=[Cs], writes=[c_])
                slot = t2 % per_bank
                py = self.ps[(t2 // per_bank) % 8]
                for c4 in range(4):
                    o0 = slot * 4 * H + c4 * H
                    mk.op("pe", lambda e, py=py, o0=o0, c4=c4, c_=c_: e.matmul(
                        py[:, o0:o0 + H], c_[:, 0, c4 * 128:(c4 + 1) * 128], Fi[:, 0, :], start=True, stop=False),
                        reads=[c_, Fi], writes=[py])
                    mk.op("pe", lambda e, py=py, o0=o0, c4=c4, c_=c_: e.matmul(
                        py[:, o0:o0 + H], c_[:, 1, c4 * 128:(c4 + 1) * 128], Fi[:, 1, :], start=False, stop=True),
                        reads=[c_, Fi], writes=[py])
                src = py[:, slot * 4 * H:(slot + 1) * 4 * H].rearrange("p (a b) -> p a b", b=H)
                if t2 % 2:
                    mk.op("act", lambda e, src=src, t2=t2: e.copy(yh[:, :, t2:L:64], src), reads=[py], writes=[yh])
                else:
                    mk.op("dve", lambda e, src=src, t2=t2: e.tensor_copy(yh[:, :, t2:L:64], src), reads=[py], writes=[yh])
            for c4 in range(4):
                cj = half * 4 + c4
                z_, g_, o_ = zr[c4 % 2], gr[c4 % 2], ob[c4 % 2]
                mk.dma("sp", z_[:], zsrc[zrow0 + cj * 128:zrow0 + (cj + 1) * 128, col0:col0 + L], reads=[zsrc], writes=[z_])
                mk.dma("sp", g_[:], gate[grow0 + cj * 128:grow0 + (cj + 1) * 128, col0:col0 + L], reads=[gate], writes=[g_])
                mk.op("dve", lambda e, z_=z_, cj=cj, c4=c4: e.scalar_tensor_tensor(
                    out=z_[:], in0=z_[:], scalar=lb[:, cj:cj + 1], in1=yh[:, c4, :], op0=ALU.mult, op1=ALU.add),
                    reads=[z_, lb, yh], writes=[z_])
                mk.op("pool", lambda e, z_=z_, g_=g_, o_=o_: e.tensor_tensor(out=o_[:], in0=z_[:], in1=g_[:], op=ALU.mult),
                      reads=[z_, g_], writes=[o_])
                mk.dma("act", dst[cj * 128:(cj + 1) * 128, col0:col0 + L], o_[:], reads=[o_], writes=[dst])
        mk.release(m0)

    def stage_hyena(self, slot, XT, HT, I):
        mk = self.mk
        U0, ZT, Z1 = self.dram["hyU0"], self.dram["hyZ"], self.dram["hyZ1"]
        m0 = mk.mark()
        W = self.load_w("hy_win", I["hy_w_in"][slot], DC, 3 * D)
        bi_ = mk.sbuf("hy_bin", [128, 24], F32)
        mk.dma("sp", bi_[:], I["hy_b_in_pc"][:, :], writes=[bi_])
        hb = [mk.sbuf("hy_h%d" % k, [128, DC, 512], BF16) for k in range(2)]
        uo = [mk.sbuf("hy_uo%d" % k, [128, 512], F32) for k in range(3)]
        n = 0
        for bi, (t0, tn) in enumerate(BLOCKS):
            h = hb[bi % 2]
            mk.dma("sp", h[:, :, 0:tn], HT[:, t0:t0 + tn].rearrange("(k p) t -> p k t", p=128), reads=[HT], writes=[h])
            for m in range(24):
                ps = self.ps[n % 4]
                u = uo[n % 3]
                n += 1
                for k in range(DC):
                    mk.op("pe", lambda e, ps=ps, k=k, m=m, h=h: e.matmul(
                        ps[:, 0:tn], W[:, k, m * 128:(m + 1) * 128], h[:, k, 0:tn], start=(k == 0), stop=(k == DC - 1)),
                        reads=[W, h], writes=[ps])
                mk.op("act", lambda e, ps=ps, u=u, m=m: e.activation(out=u[:, 0:tn], in_=ps[:, 0:tn], func=AF.Identity,
                                                                    bias=bi_[:, m:m + 1]), reads=[ps, bi_], writes=[u])
                mk.dma("act", U0[m * 128:(m + 1) * 128, t0:t0 + tn], u[:, 0:tn], reads=[u], writes=[U0])
        mk.release(m0)
        m0 = mk.mark()
        ws = mk.sbuf("hy_ws", [128, 24, 3], F32)
        bs = mk.sbuf("hy_bs", [128, 24], F32)
        mk.dma("sp", ws[:], I["hy_w_short_pc"][:, :, :], writes=[ws])
        mk.dma("sp", bs[:], I["hy_b_short_pc"][:, :], writes=[bs])
        W_ = NT + 4
        ub = [mk.sbuf("hy_ub%d" % k, [128, W_], F32) for k in range(2)]
        vb = [mk.sbuf("hy_vb%d" % k, [128, W_], F32) for k in range(2)]
        for k in range(2):
            mk.op("pool", lambda e, k=k: e.memset(ub[k][:], 0.0), writes=[ub[k]])
        for m in range(24):
            u, v = ub[m % 2], vb[m % 2]
            mk.dma("sp", u[:, 1:1 + CTX], U0[m * 128:(m + 1) * 128, 0:CTX], reads=[U0], writes=[u])
            mk.dma("sp", u[:, 3 + CTX:3 + NT], U0[m * 128:(m + 1) * 128, CTX:NT], reads=[U0], writes=[u])
            n_ = W_ - 2
            mk.op("dve", lambda e, u=u, v=v, m=m: e.tensor_scalar(out=v[:, 1:1 + n_], in0=u[:, 0:n_], scalar1=ws[:, m, 0:1],
                                                                 scalar2=bs[:, m:m + 1], op0=ALU.mult, op1=ALU.add),
                  reads=[u, ws, bs], writes=[v])
            mk.op("dve", lambda e, u=u, v=v, m=m: e.scalar_tensor_tensor(out=v[:, 1:1 + n_], in0=u[:, 1:1 + n_], scalar=ws[:, m, 1:2],
                                                                        in1=v[:, 1:1 + n_], op0=ALU.mult, op1=ALU.add),
                  reads=[u, ws, v], writes=[v])
            mk.op("dve", lambda e, u=u, v=v, m=m: e.scalar_tensor_tensor(out=v[:, 1:1 + n_], in0=u[:, 2:2 + n_], scalar=ws[:, m, 2:3],
                                                                        in1=v[:, 1:1 + n_], op0=ALU.mult, op1=ALU.add),
                  reads=[u, ws, v], writes=[v])
            mk.dma("act", ZT[m * 128:(m + 1) * 128, 0:CTX], v[:, 1:1 + CTX], reads=[v], writes=[ZT])
            mk.dma("act", ZT[m * 128:(m + 1) * 128, CTX:NT], v[:, 3 + CTX:3 + NT], reads=[v], writes=[ZT])
        mk.release(m0)
        seqs = [dict(L=SEQ, N1=128, H=64, tag="lat", col0=CTX)]
        if not self.cfg.get("ctx_direct", True):
            seqs.append(dict(L=CTX, N1=8, H=4, tag="ctx", col0=0))
        for S in seqs:
            HH = [self.dram["hyHH_%s%d" % (S["tag"], o)] for o in range(2)]
            self.hy_filter(S, I, HH)
            self.hy_conv(S, 0, I, ZT, 0, ZT, D, I["hy_lbias_pc"][0, :, :], HH, Z1, False)
            self.hy_conv(S, 1, I, Z1, 0, ZT, 2 * D, I["hy_lbias_pc"][1, :, :], HH, HT, True)
        if self.cfg.get("ctx_direct", True):
            self.hy_ctx_direct(I)
        m0 = mk.mark()
        wo = self.load_w("hy_wout", I["hy_w_out"][slot], DC, D)
        bo = mk.sbuf("hy_bo", [128, DC], F32)
        mk.dma("sp", bo[:], I["hy_b_out_pc"][:, :], writes=[bo])
        self.linear_resid(XT, HT, wo, DC, "g1", bias=bo)
        mk.release(m0)

    Prog.hy_conv = hy_conv
    Prog.stage_hyena = stage_hyena


_hy_methods2()


_PROG = {}


def kernel(**inputs):
    inp = {k: np.asarray(v) for k, v in inputs.items()}
    if "p" not in _PROG:
        _PROG["p"] = build_program()
    p = _PROG["p"]
    shared = None
    in_maps = []
    for b in range(8):
        m = host_inputs(inp, b)
        if shared is None:
            shared = m
        else:
            for k in m:
                if k not in ("x", "ctx", "c_pc"):
                    m[k] = shared[k]
        in_maps.append(m)
    res = run_bass_kernel_spmd(p.nc, in_maps, core_ids=list(range(8)))
    out = np.stack([np.asarray(r["y"], dtype=np.float32) for r in res.results], axis=0)
    return out


BSL = 256
BSH = 8
NBLK = 66
NSLOT = NBLK * BSL
NTILE = NT // 128
IOA = bass.IndirectOffsetOnAxis


def _sparse_methods():
    def indirect(mk, out, out_off, in_, in_off, reads, writes):
        qlane = mk.lanes["pool"]
        dl = mk.dma_lanes[mk.dma_rr]
        mk.dma_rr = (mk.dma_rr + 1) % len(mk.dma_lanes)
        need, _ = mk._deps(dl, reads, writes)
        if dl.count > 0:
            need[dl.name] = max(need.get(dl.name, 0), dl.count)
        for ln, ix in need.items():
            if qlane.seen.get(ln, 0) >= ix:
                continue
            qlane.seen[ln] = ix
            qlane.eng.wait_ge(mk.lanes[ln].sem, ix)
        inst = qlane.eng.indirect_dma_start(out=out, out_offset=out_off, in_=in_, in_offset=in_off)
        dl.count += 16
        inst.then_inc(dl.sem, 16)
        mk._record(dl, reads, writes)
        mk.n_inst += 1

    MK.indirect = indirect

    def alloc_sparse(self):
        mk = self.mk
        R = {}
        R["M1a"] = mk.sbuf("sp_M1a", [128, NTILE, 32], F32)
        R["M2a"] = mk.sbuf("sp_M2a", [128, NTILE, 32], F32)
        R["A12"] = mk.sbuf("sp_A12", [128, NTILE, 2], F32)
        R["POSi"] = mk.sbuf("sp_POSi", [128, NTILE, 2], I32)
        R["IDXW"] = mk.sbuf("sp_IDXW", [128, NBLK], I32)
        R["U"] = mk.sbuf("sp_U", [128, 128], BF16)
        R["jg"] = mk.sbuf("sp_jg", [128, NBLK], F32)
        R["pidx"] = mk.sbuf("sp_pidx", [128, 1], F32)
        R["ones32"] = mk.sbuf("sp_ones32", [128, 32], F32)
        mk.op("pool", lambda e: e.memset(R["U"][:], 1.0), writes=[R["U"]])
        mk.op("pool", lambda e: e.affine_select(out=R["U"][:], in_=R["U"][:], pattern=[[1, 128]], compare_op=ALU.is_gt,
                                                fill=0.0, base=0, channel_multiplier=-1), reads=[R["U"]], writes=[R["U"]])
        mk.op("pool", lambda e: e.memset(R["ones32"][:], 1.0), writes=[R["ones32"]])
        return R

    def route_tile_sparse(self, hf, a, tok0, R, rt):
        mk = self.mk
        ti = tok0 // 128
        ps = self.ps[1 + (ti % 3)]
        Wr, br = R["Wr"], R["br"]
        for k in range(DC):
            mk.op("pe", lambda e, k=k: e.matmul(ps[:, 0:36], hf[:, k, a * 128:(a + 1) * 128], Wr[:, k, :],
                                                 start=(k == 0), stop=(k == DC - 1)), reads=[hf, Wr], writes=[ps])
        lg, gmax, ngmax, ge, gsum, gp, mg, es = (rt[k] for k in ("lg", "gmax", "ngmax", "ge", "gsum", "gp", "mg", "es"))
        t8, dd, w2, m1, m2 = (rt[k] for k in ("t8", "dd", "w2", "m1", "m2"))
        M1a, M2a, A12 = R["M1a"], R["M2a"], R["A12"]
        V = lambda fn, reads, writes: mk.op("dve", fn, reads=reads, writes=writes)
        V(lambda e: e.tensor_tensor(out=lg[:], in0=ps[:, 0:36], in1=br[:], op=ALU.add), [ps, br], [lg])
        V(lambda e: e.reduce_max(out=gmax[:], in_=lg[:, 0:4], axis=AX.X), [lg], [gmax])
        V(lambda e: e.tensor_scalar(out=ngmax[:], in0=gmax[:], scalar1=-1.0, scalar2=None, op0=ALU.mult), [gmax], [ngmax])
        mk.op("act", lambda e: e.activation(out=ge[:], in_=lg[:, 0:4], func=AF.Exp, bias=ngmax[:, 0:1],
                                            accum_out=gsum[:]), reads=[lg, ngmax], writes=[ge, gsum])
        V(lambda e: e.reciprocal(gp[:], gsum[:]), [gsum], [gp])
        V(lambda e: e.tensor_scalar(out=mg[:], in0=lg[:, 0:4], scalar1=gmax[:, 0:1], scalar2=None, op0=ALU.is_equal),
          [lg, gmax], [mg])
        V(lambda e: e.tensor_scalar(out=es[:], in0=lg[:, 4:12], scalar1=mg[:, 0:1], scalar2=None, op0=ALU.mult),
          [lg, mg], [es])
        for g in range(1, 4):
            V(lambda e, g=g: e.scalar_tensor_tensor(out=es[:], in0=lg[:, 4 + 8 * g:12 + 8 * g], scalar=mg[:, g:g + 1],
                                                    in1=es[:], op0=ALU.mult, op1=ALU.add), [lg, mg, es], [es])
        V(lambda e: e.max(out=t8[:], in_=es[:]), [es], [t8])
        V(lambda e: e.tensor_tensor(out=dd[:], in0=t8[:, 1:2], in1=t8[:, 0:1], op=ALU.subtract), [t8], [dd])
        mk.op("act", lambda e: e.activation(out=w2[:], in_=dd[:], func=AF.Sigmoid), reads=[dd], writes=[w2])
        V(lambda e: e.tensor_tensor(out=A12[:, ti, 1:2], in0=w2[:], in1=gp[:], op=ALU.mult), [w2, gp], [A12])
        V(lambda e: e.tensor_tensor(out=A12[:, ti, 0:1], in0=gp[:], in1=A12[:, ti, 1:2], op=ALU.subtract), [gp, A12], [A12])
        V(lambda e: e.tensor_scalar(out=m1[:], in0=es[:], scalar1=t8[:, 0:1], scalar2=None, op0=ALU.is_equal), [es, t8], [m1])
        V(lambda e: e.tensor_scalar(out=m2[:], in0=es[:], scalar1=t8[:, 1:2], scalar2=None, op0=ALU.is_equal), [es, t8], [m2])
        for g in range(4):
            V(lambda e, g=g: e.tensor_scalar(out=M1a[:, ti, 8 * g:8 * g + 8], in0=m1[:], scalar1=mg[:, g:g + 1],
                                             scalar2=None, op0=ALU.mult), [m1, mg], [M1a])
            V(lambda e, g=g: e.tensor_scalar(out=M2a[:, ti, 8 * g:8 * g + 8], in0=m2[:], scalar1=mg[:, g:g + 1],
                                             scalar2=None, op0=ALU.mult), [m2, mg], [M2a])

    def stage_moe_sparse(self, i, XT, R, I):
        mk = self.mk
        HTOK, HS, YS = self.dram["HTOK"], self.dram["HS"], self.dram["YS"]
        M1a, M2a, A12, POSi, IDXW = R["M1a"], R["M2a"], R["A12"], R["POSi"], R["IDXW"]
        m0 = mk.mark()
        Mb = mk.sbuf("sq_Mb", [128, NTILE, 32], BF16)
        P = mk.sbuf("sq_P", [128, NTILE, 32], F32)
        prod = mk.sbuf("sq_prod", [128, NTILE, 32], F32)
        posf = mk.sbuf("sq_posf", [128, NTILE, 2], F32)
        nf = mk.sbuf("sq_nf", [128, 32], F32)
        ni = mk.sbuf("sq_ni", [128, 32], I32)
        pn = mk.sbuf("sq_pn", [128, 32], F32)
        incl = mk.sbuf("sq_incl", [128, 32], F32)
        excl = mk.sbuf("sq_excl", [128, 32], F32)
        EB = mk.sbuf("sq_EB", [128, NBLK], F32)
        mk.op("dve", lambda e: e.tensor_tensor(out=Mb[:], in0=M1a[:], in1=M2a[:], op=ALU.add), reads=[M1a, M2a], writes=[Mb])
        for ti in range(NTILE):
            pb = self.ps[ti // 12]
            c0 = (ti % 12) * 32
            for tj in range(ti):
                mk.op("pe", lambda e, pb=pb, c0=c0, tj=tj: e.matmul(pb[:, c0:c0 + 32], self.ones_b[:], Mb[:, tj, :],
                                                                    start=(tj == 0), stop=False), reads=[self.ones_b, Mb], writes=[pb])
            mk.op("pe", lambda e, pb=pb, c0=c0, ti=ti: e.matmul(pb[:, c0:c0 + 32], R["U"][:], Mb[:, ti, :],
                                                                start=(ti == 0), stop=True), reads=[R["U"], Mb], writes=[pb])
        pt = self.ps[3]
        for tj in range(NTILE):
            mk.op("pe", lambda e, tj=tj: e.matmul(pt[:, 0:32], self.ones_b[:], Mb[:, tj, :], start=(tj == 0),
                                                  stop=(tj == NTILE - 1)), reads=[self.ones_b, Mb], writes=[pt])
        V = lambda fn, reads, writes: mk.op("dve", fn, reads=reads, writes=writes)
        V(lambda e: e.tensor_scalar(out=nf[:], in0=pt[:, 0:32], scalar1=float(BSL - 1), scalar2=None, op0=ALU.add), [pt], [nf])
        V(lambda e: e.tensor_copy(ni[:], nf[:]), [nf], [ni])
        V(lambda e: e.tensor_single_scalar(out=ni[:], in_=ni[:], scalar=BSH, op=ALU.arith_shift_right), [ni], [ni])
        V(lambda e: e.tensor_single_scalar(out=ni[:], in_=ni[:], scalar=BSH, op=ALU.logical_shift_left), [ni], [ni])
        V(lambda e: e.tensor_copy(pn[:], ni[:]), [ni], [pn])
        V(lambda e: e.tensor_tensor_scan(out=incl[:], data0=R["ones32"][:], data1=pn[:], initial=0.0, op0=ALU.mult, op1=ALU.add),
          [R["ones32"], pn], [incl])
        V(lambda e: e.tensor_tensor(out=excl[:], in0=incl[:], in1=pn[:], op=ALU.subtract), [incl, pn], [excl])
        for ti in range(NTILE):
            pb = self.ps[ti // 12]
            c0 = (ti % 12) * 32
            V(lambda e, pb=pb, c0=c0, ti=ti: e.tensor_tensor(out=P[:, ti, :], in0=pb[:, c0:c0 + 32], in1=excl[:], op=ALU.add),
              [pb, excl], [P])
        for k, Mk in enumerate((M1a, M2a)):
            V(lambda e, Mk=Mk: e.tensor_tensor(out=prod[:], in0=Mk[:], in1=P[:], op=ALU.mult), [Mk, P], [prod])
            V(lambda e, k=k: e.reduce_sum(out=posf[:, :, k], in_=prod[:], axis=AX.X), [prod], [posf])
        V(lambda e: e.tensor_copy(POSi[:], posf[:]), [posf], [POSi])
        V(lambda e: e.memset(EB[:], 0.0), [], [EB])
        for ex in range(NE):
            V(lambda e, ex=ex: e.scalar_tensor_tensor(out=EB[:], in0=R["jg"][:], scalar=incl[:, ex:ex + 1], in1=EB[:],
                                                      op0=ALU.is_ge, op1=ALU.add), [R["jg"], incl, EB], [EB])
        V(lambda e: e.tensor_scalar(out=EB[:], in0=EB[:], scalar1=float(NE - 1), scalar2=128.0, op0=ALU.min, op1=ALU.mult), [EB], [EB])
        V(lambda e: e.tensor_scalar(out=EB[:], in0=EB[:], scalar1=R["pidx"][:, 0:1], scalar2=None, op0=ALU.add), [EB, R["pidx"]], [EB])
        V(lambda e: e.tensor_copy(IDXW[:], EB[:]), [EB], [IDXW])
        if self.cfg.get("dbg_pos"):
            d1 = T(self.nc.dram_tensor("dbg_pos", [128, NTILE, 2], I32, kind="ExternalOutput"), "dbg_pos")
            d2 = T(self.nc.dram_tensor("dbg_idx", [128, NBLK], I32, kind="ExternalOutput"), "dbg_idx")
            d3 = T(self.nc.dram_tensor("dbg_incl", [128, 32], F32, kind="ExternalOutput"), "dbg_incl")
            d4 = T(self.nc.dram_tensor("dbg_P", [128, NTILE, 32], F32, kind="ExternalOutput"), "dbg_P")
            d5 = T(self.nc.dram_tensor("dbg_M1", [128, NTILE, 32], F32, kind="ExternalOutput"), "dbg_M1")
            d6 = T(self.nc.dram_tensor("dbg_A12", [128, NTILE, 2], F32, kind="ExternalOutput"), "dbg_A12")
            mk.dma("sp", d1[:, :, :], POSi[:], reads=[POSi])
            mk.dma("sp", d2[:, :], IDXW[:], reads=[IDXW])
            mk.dma("sp", d3[:, :], incl[:], reads=[incl])
            mk.dma("sp", d4[:, :, :], P[:], reads=[P])
            mk.dma("sp", d5[:, :, :], M1a[:], reads=[M1a])
            mk.dma("sp", d6[:, :, :], A12[:], reads=[A12])
            mk.release(m0)
            return
        mk.release(m0)
        m0 = mk.mark()
        ht = [mk.sbuf("sq_ht%d" % k, [128, D], BF16) for k in range(3)]
        for ti in range(NTILE):
            h = ht[ti % 3]
            mk.dma("sp", h[:], HTOK[ti * 128:(ti + 1) * 128, :], reads=[HTOK], writes=[h])
            for k in range(2):
                mk.indirect(HS[:, :], IOA(ap=POSi[:, ti, k:k + 1], axis=0), h[:], None, [h, POSi], [HS])
        mk.release(m0)
        m0 = mk.mark()
        wgu = [mk.sbuf("sq_wgu%d" % k, [128, DC, 512], BF16) for k in range(2)]
        wdn = [mk.sbuf("sq_wdn%d" % k, [128, 2, D], BF16) for k in range(2)]
        NA = BSL // 128
        hs = [mk.sbuf("sq_hs%d" % k, [128, NA, D], BF16) for k in range(2)]
        hsT = [mk.sbuf("sq_hsT%d" % k, [128, DC, BSL], BF16) for k in range(2)]
        sl = [mk.sbuf("sq_sl%d" % k, [128, BSL], F32) for k in range(2)]
        hid = [mk.sbuf("sq_hid%d" % k, [128, 2, BSL], BF16) for k in range(2)]
        yo = [mk.sbuf("sq_yo%d" % k, [128, D], BF16) for k in range(3)]
        WGU, WDN = I["moe_wgu%d" % i], I["moe_wdn%d" % i]
        ny = 0
        for j in range(NBLK):
            w, wd_, h, hT_, hd = wgu[j % 2], wdn[j % 2], hs[j % 2], hsT[j % 2], hid[j % 2]
            mk.indirect(w[:].rearrange("p a b -> p (a b)"), None, WGU[:, :], IOA(ap=IDXW[:, j:j + 1], axis=0), [IDXW], [w])
            mk.indirect(wd_[:].rearrange("p a b -> p (a b)"), None, WDN[:, :], IOA(ap=IDXW[:, j:j + 1], axis=0), [IDXW], [wd_])
            mk.dma("sp", h[:], HS[j * BSL:(j + 1) * BSL, :].rearrange("(a p) d -> p a d", p=128), reads=[HS], writes=[h])
            KPB = 1024 // BSL
            for hh in range(DC // KPB):
                pb = self.ps[hh % 2]
                pbb = pb[:, 0:512].bitcast(BF16)
                for kk in range(KPB):
                    k = hh * KPB + kk
                    for a in range(NA):
                        mk.op("pe", lambda e, pbb=pbb, kk=kk, k=k, a=a, h=h: e.transpose(
                            pbb[:, kk * BSL + a * 128:kk * BSL + (a + 1) * 128], h[:, a, k * 128:(k + 1) * 128], self.ident_b[:]),
                            reads=[h, self.ident_b], writes=[pb])
                dstv = hT_[:, hh * KPB:(hh + 1) * KPB, :].rearrange("p a b -> p (a b)")
                if hh % 2:
                    mk.op("act", lambda e, pbb=pbb, dstv=dstv: e.copy(dstv, pbb[:, :]), reads=[pb], writes=[hT_])
                else:
                    mk.op("dve", lambda e, pbb=pbb, dstv=dstv: e.tensor_copy(dstv, pbb[:, :]), reads=[pb], writes=[hT_])
            for fc in range(2):
                pg, pu = self.ps[2 + 2 * fc], self.ps[3 + 2 * fc]
                for k in range(DC):
                    mk.op("pe", lambda e, pg=pg, k=k, fc=fc, w=w, hT_=hT_: e.matmul(
                        pg[:, 0:BSL], w[:, k, fc * 128:(fc + 1) * 128], hT_[:, k, :], start=(k == 0), stop=(k == DC - 1)),
                        reads=[w, hT_], writes=[pg])
                for k in range(DC):
                    mk.op("pe", lambda e, pu=pu, k=k, fc=fc, w=w, hT_=hT_: e.matmul(
                        pu[:, 0:BSL], w[:, k, 256 + fc * 128:256 + (fc + 1) * 128], hT_[:, k, :], start=(k == 0), stop=(k == DC - 1)),
                        reads=[w, hT_], writes=[pu])
                s_ = sl[fc]
                mk.op("act", lambda e, pg=pg, s_=s_: e.activation(out=s_[:], in_=pg[:, 0:BSL], func=AF.Silu), reads=[pg], writes=[s_])
                mk.op("dve", lambda e, pu=pu, s_=s_, hd=hd, fc=fc: e.tensor_tensor(out=hd[:, fc, :], in0=pu[:, 0:BSL], in1=s_[:],
                                                                                  op=ALU.mult), reads=[pu, s_], writes=[hd])
            for a in range(NA):
                y_ = yo[ny % 3]
                ny += 1
                for dh in range(2):
                    pd = self.ps[6 + dh]
                    for fc in range(2):
                        mk.op("pe", lambda e, pd=pd, fc=fc, a=a, dh=dh, hd=hd, wd_=wd_: e.matmul(
                            pd[:, :], hd[:, fc, a * 128:(a + 1) * 128], wd_[:, fc, dh * 512:(dh + 1) * 512], start=(fc == 0), stop=(fc == 1)),
                            reads=[hd, wd_], writes=[pd])
                    if dh:
                        mk.op("act", lambda e, pd=pd, y_=y_, dh=dh: e.copy(y_[:, dh * 512:(dh + 1) * 512], pd[:, :]), reads=[pd], writes=[y_])
                    else:
                        mk.op("dve", lambda e, pd=pd, y_=y_, dh=dh: e.tensor_copy(y_[:, dh * 512:(dh + 1) * 512], pd[:, :]), reads=[pd], writes=[y_])
                mk.dma("act", YS[j * BSL + a * 128:j * BSL + (a + 1) * 128, :], y_[:], reads=[y_], writes=[YS])
        mk.release(m0)
        m0 = mk.mark()
        r1 = [mk.sbuf("sq_r1%d" % k, [128, D], BF16) for k in range(2)]
        r2 = [mk.sbuf("sq_r2%d" % k, [128, D], BF16) for k in range(2)]
        rc = [mk.sbuf("sq_rc%d" % k, [128, D], F32) for k in range(2)]
        xr = [mk.sbuf("sq_x%d" % k, [128, 512], F32) for k in range(3)]
        n = 0
        for bi, (t0, tn) in enumerate(BLOCKS):
            r = 1 if bi == 0 else 0
            g2 = self.mv[("g2", r)]
            for a in range(tn // 128):
                ti = t0 // 128 + a
                a_, b_ = r1[ti % 2], r2[ti % 2]
                mk.indirect(a_[:], None, YS[:, :], IOA(ap=POSi[:, ti, 0:1], axis=0), [YS, POSi], [a_])
                mk.indirect(b_[:], None, YS[:, :], IOA(ap=POSi[:, ti, 1:2], axis=0), [YS, POSi], [b_])
                c_ = rc[ti % 2]
                mk.op("dve", lambda e, a_=a_, c_=c_, ti=ti: e.tensor_scalar(out=c_[:], in0=a_[:], scalar1=A12[:, ti, 0:1], scalar2=None,
                                                                           op0=ALU.mult), reads=[a_, A12], writes=[c_])
                mk.op("dve", lambda e, c_=c_, b_=b_, ti=ti: e.scalar_tensor_tensor(out=c_[:], in0=b_[:], scalar=A12[:, ti, 1:2], in1=c_[:],
                                                                                  op0=ALU.mult, op1=ALU.add), reads=[c_, b_, A12], writes=[c_])
                for dc in range(DC):
                    mk.op("pe", lambda e, dc=dc, a=a, c_=c_: e.transpose(self.ps[dc][:, a * 128:(a + 1) * 128],
                                                                         c_[:, dc * 128:(dc + 1) * 128], self.ident_f[:]),
                          reads=[c_, self.ident_f], writes=[self.ps[dc]])
            for dc in range(DC):
                x = xr[n % 3]
                n += 1
                mk.dma("sp", x[:, 0:tn], XT[dc * 128:(dc + 1) * 128, t0:t0 + tn], reads=[self.XTr[dc]], writes=[x])
                mk.op("dve", lambda e, dc=dc, x=x, g2=g2: e.scalar_tensor_tensor(
                    out=x[:, 0:tn], in0=self.ps[dc][:, 0:tn], scalar=g2[:, dc:dc + 1], in1=x[:, 0:tn],
                    op0=ALU.mult, op1=ALU.add), reads=[self.ps[dc], g2, x], writes=[x])
                mk.dma("act", XT[dc * 128:(dc + 1) * 128, t0:t0 + tn], x[:, 0:tn], reads=[x], writes=[self.XTr[dc]])
        mk.release(m0)

    Prog.alloc_sparse = alloc_sparse
    Prog._route_tile_sparse = route_tile_sparse
    Prog.stage_moe_sparse = stage_moe_sparse


_sparse_methods()


def hyd_consts():
    bf = ml_dtypes.bfloat16
    L, N = CTX, 2 * CTX
    c = {}
    t = (np.arange(2)[None, :, None] * 128 + np.arange(128)[:, None, None]).astype(np.float64)
    k = np.arange(N)[None, None, :].astype(np.float64)
    ang = 2 * np.pi * t * k / N
    c["F"] = np.stack([np.cos(ang), -np.sin(ang), np.sin(ang)], axis=2).astype(bf)
    kk = (np.arange(4)[None, :, None] * 128 + np.arange(128)[:, None, None]).astype(np.float64)
    tt = np.arange(L)[None, None, :].astype(np.float64)
    a2 = 2 * np.pi * kk * tt / N
    c["Fi"] = np.stack([np.cos(a2) / N, -np.sin(a2) / N], axis=2).astype(bf)
    pos = (np.arange(2)[None, :] * 128 + np.arange(128)[:, None]).astype(np.float32)
    c["lneg"] = (-(pos / np.float32(L - 1))).astype(np.float32)
    return c


def _hyd_methods():
    def hy_ctx_direct(self, I):
        mk = self.mk
        L = CTX
        ZT, Z1, HT = self.dram["hyZ"], self.dram["hyZ1"], self.dram["HT"]
        m0 = mk.mark()
        Fd = mk.sbuf("hd_F", [128, 2, 3, 512], BF16)
        Fi = mk.sbuf("hd_Fi", [128, 4, 2, L], BF16)
        lneg = mk.sbuf("hd_lneg", [128, 2], F32)
        dab = mk.sbuf("hd_dab", [128, D], F32)
        ft = mk.sbuf("hd_ft", [17, L], F32)
        W1 = mk.sbuf("hd_w1", [17, 64], F32)
        W2 = mk.sbuf("hd_w2", [64, 64], F32)
        W3 = mk.sbuf("hd_w3", [64, 4096], BF16)
        fv = mk.sbuf("hd_fv", [64, 4], F32)
        fb = mk.sbuf("hd_fb", [64, 2], F32)
        z1 = mk.sbuf("hd_z1", [64, L], F32)
        z2 = mk.sbuf("hd_z2", [64, L], F32)
        z2b = mk.sbuf("hd_z2b", [64, L], BF16)
        tmp = mk.sbuf("hd_tmp", [64, L], F32)
        mk.dma("sp", Fd[:], I["hyd_F"][:, :, :, :], writes=[Fd])
        mk.dma("sp", Fi[:], I["hyd_Fi"][:, :, :, :], writes=[Fi])
        mk.dma("sp", lneg[:], I["hyd_lneg"][:, :], writes=[lneg])
        mk.dma("sp", dab[0:64, :], I["hy_dabs"][:, :], writes=[dab])
        mk.dma("sp", dab[64:128, :], I["hy_dabs"][:, :], writes=[dab])
        mk.dma("sp", ft[:], I["hy_featsT_ctx"][:, :], writes=[ft])
        mk.dma("sp", W1[:], I["hy_f_w1"][0, :, :], writes=[W1])
        mk.dma("sp", W2[:], I["hy_f_w2"][0, :, :], writes=[W2])
        mk.dma("pool", W3[:], I["hy_f_w3"][0, :, :], writes=[W3])
        mk.dma("sp", fv[:], I["hy_fvec"][:, :], writes=[fv])
        mk.op("dve", lambda e: e.tensor_tensor(out=fb[:, 0:1], in0=fv[:, 0:1], in1=fv[:, 1:2], op=ALU.mult), reads=[fv], writes=[fb])
        mk.op("dve", lambda e: e.tensor_tensor(out=fb[:, 1:2], in0=fv[:, 2:3], in1=fv[:, 3:4], op=ALU.mult), reads=[fv], writes=[fb])
        for layer, (Wm, src, dst, kin) in enumerate(((W1, ft, z1, 17), (W2, z1, z2, 64))):
            ps = self.ps[layer]
            mk.op("pe", lambda e, ps=ps, Wm=Wm, src=src, kin=kin: e.matmul(ps[0:64, 0:L], Wm[0:kin, :], src[0:kin, :],
                                                                          start=True, stop=True), reads=[Wm, src], writes=[ps])
            mk.op("dve", lambda e, ps=ps, dst=dst, layer=layer: e.tensor_scalar(
                out=dst[:], in0=ps[0:64, 0:L], scalar1=fv[:, 2 * layer + 1:2 * layer + 2], scalar2=fb[:, layer:layer + 1],
                op0=ALU.mult, op1=ALU.add), reads=[ps, fv, fb], writes=[dst])
            for _ in range(2):
                mk.op("dve", lambda e, dst=dst: e.tensor_scalar(out=tmp[:], in0=dst[:], scalar1=math.pi, scalar2=2 * math.pi,
                                                                op0=ALU.is_gt, op1=ALU.mult), reads=[dst], writes=[tmp])
                mk.op("dve", lambda e, dst=dst: e.tensor_tensor(out=dst[:], in0=dst[:], in1=tmp[:], op=ALU.subtract),
                      reads=[dst, tmp], writes=[dst])
                mk.op("dve", lambda e, dst=dst: e.tensor_scalar(out=tmp[:], in0=dst[:], scalar1=-math.pi, scalar2=2 * math.pi,
                                                                op0=ALU.is_lt, op1=ALU.mult), reads=[dst], writes=[tmp])
                mk.op("dve", lambda e, dst=dst: e.tensor_tensor(out=dst[:], in0=dst[:], in1=tmp[:], op=ALU.add),
                      reads=[dst, tmp], writes=[dst])
            mk.op("act", lambda e, dst=dst: e.activation(out=dst[:], in_=dst[:], func=AF.Sin), reads=[dst], writes=[dst])
        mk.op("act", lambda e: e.copy(z2b[:], z2[:]), reads=[z2], writes=[z2b])
        Hf = mk.sbuf("hd_Hf", [128, 2, 2, 4, D], BF16)
        dec = [mk.sbuf("hd_dec%d" % k, [128, D], F32) for k in range(2)]
        hd = [mk.sbuf("hd_hd%d" % k, [128, D], F32) for k in range(2)]
        hdb = [mk.sbuf("hd_hdb%d" % k, [128, D], BF16) for k in range(4)]
        ha = [mk.sbuf("hd_ha%d" % k, [128, D], BF16) for k in range(2)]
        rn = mk.sbuf("hd_rn", [128, D], F32)
        for o in range(2):
            pn = (self.ps[6], self.ps[7])
            n = 0
            for pt in range(2):
                for dr in range(2):
                    i2 = n % 2
                    for hf in range(2):
                        ph = self.ps[i2 * 2 + hf]
                        w0 = (dr * 2 + o) * D + hf * 512
                        mk.op("pe", lambda e, ph=ph, pt=pt, w0=w0: e.matmul(ph[:, :], z2b[:, pt * 128:(pt + 1) * 128], W3[:, w0:w0 + 512],
                                                                            start=True, stop=True), reads=[z2b, W3], writes=[ph])
                    mk.op("act", lambda e, i2=i2, pt=pt: e.activation(out=dec[i2][:], in_=dab[:], func=AF.Exp, scale=lneg[:, pt:pt + 1]),
                          reads=[dab, lneg], writes=[dec[i2]])
                    for hf in range(2):
                        ph = self.ps[i2 * 2 + hf]
                        sl_ = slice(hf * 512, (hf + 1) * 512)
                        mk.op("dve", lambda e, ph=ph, i2=i2, sl_=sl_: e.tensor_tensor(out=hd[i2][:, sl_], in0=ph[:, :], in1=dec[i2][:, sl_],
                                                                                     op=ALU.mult), reads=[ph, dec[i2]], writes=[hd[i2]])
                    if dr == 1 and pt == 0:
                        mk.op("dve", lambda e, i2=i2: e.memset(hd[i2][0:1, :], 0.0), reads=[hd[i2]], writes=[hd[i2]])
                    hb_ = hdb[pt * 2 + dr]
                    mk.op("act", lambda e, i2=i2, hb_=hb_: e.copy(hb_[:], hd[i2][:]), reads=[hd[i2]], writes=[hb_])
                    mk.op("dve", lambda e, i2=i2: e.scalar_tensor_tensor(out=ha[i2][:], in0=hd[i2][:], scalar=-1.0, in1=hd[i2][:],
                                                                        op0=ALU.mult, op1=ALU.max), reads=[hd[i2]], writes=[ha[i2]])
                    for hf in range(2):
                        mk.op("pe", lambda e, i2=i2, hf=hf, n=n: e.matmul(pn[hf][:, :], self.ones_b[:], ha[i2][:, hf * 512:(hf + 1) * 512],
                                                                          start=(n == 0), stop=(n == 3)), reads=[self.ones_b, ha[i2]], writes=[pn[hf]])
                    n += 1
            for hf in range(2):
                mk.op("dve", lambda e, hf=hf: e.reciprocal(rn[:, hf * 512:(hf + 1) * 512], pn[hf][:, :]), reads=[pn[hf]], writes=[rn])
            q = 0
            for kc in range(4):
                for ri in range(2):
                    for hf in range(2):
                        pa = self.ps[4 + q % 2]
                        q += 1
                        sl_ = slice(hf * 512, (hf + 1) * 512)
                        steps = [(pt, dr) for pt in range(2) for dr in range(2)]
                        for si, (pt, dr) in enumerate(steps):
                            kind = ri if dr == 0 else (0 if ri == 0 else 2)
                            mk.op("pe", lambda e, pa=pa, pt=pt, dr=dr, kind=kind, kc=kc, sl_=sl_, si=si: e.matmul(
                                pa[:, :], Fd[:, pt, kind, kc * 128:(kc + 1) * 128], hdb[pt * 2 + dr][:, sl_],
                                start=(si == 0), stop=(si == 3)), reads=[Fd, hdb[pt * 2 + dr]], writes=[pa])
                        mk.op("dve", lambda e, pa=pa, o=o, ri=ri, kc=kc, sl_=sl_: e.tensor_tensor(
                            out=Hf[:, o, ri, kc, sl_], in0=pa[:, :], in1=rn[:, sl_], op=ALU.mult), reads=[pa, rn], writes=[Hf])
        zf = mk.sbuf("hd_zf", [128, DC, L], F32)
        gt = mk.sbuf("hd_gt", [128, DC, L], F32)
        zb = mk.sbuf("hd_zb", [128, DC, L], BF16)
        zT = mk.sbuf("hd_zT", [128, 2, D], BF16)
        Y = mk.sbuf("hd_Y", [128, 2, 4, D], BF16)
        ta = [mk.sbuf("hd_ta%d" % k, [128, 512], F32) for k in range(2)]
        tb = [mk.sbuf("hd_tb%d" % k, [128, 512], F32) for k in range(2)]
        lb = mk.sbuf("hd_lb", [128, 2, DC], F32)
        rr = [mk.sbuf("hd_rr%d" % k, [128, L], F32) for k in range(2)]
        obf = mk.sbuf("hd_obf", [128, DC, L], F32)
        obb = mk.sbuf("hd_obb", [128, DC, L], BF16)
        for o in range(2):
            mk.dma("sp", lb[:, o, :], I["hy_lbias_pc"][o, :, :], writes=[lb])
        for o in range(2):
            zsrc = ZT if o == 0 else Z1
            mk.dma("sp", zf[:], zsrc[0:D, 0:L].rearrange("(j p) t -> p j t", p=128), reads=[zsrc], writes=[zf])
            g0 = (1 + o) * D
            mk.dma("sp", gt[:], ZT[g0:g0 + D, 0:L].rearrange("(j p) t -> p j t", p=128), reads=[ZT], writes=[gt])
            mk.op("act", lambda e: e.copy(zb[:], zf[:]), reads=[zf], writes=[zb])
            for pt in range(2):
                pb = self.ps[pt]
                pbb = pb[:, 0:512].bitcast(BF16)
                for j in range(DC):
                    mk.op("pe", lambda e, pbb=pbb, j=j, pt=pt: e.transpose(pbb[:, j * 128:(j + 1) * 128], zb[:, j, pt * 128:(pt + 1) * 128],
                                                                          self.ident_b[:]), reads=[zb, self.ident_b], writes=[pb])
                mk.op("act", lambda e, pbb=pbb, pt=pt: e.copy(zT[:, pt, :], pbb[:, :]), reads=[pb], writes=[zT])
            for kc in range(4):
                for ri in range(2):
                    for hf in range(2):
                        px = self.ps[2 + ri * 2 + hf]
                        for pt in range(2):
                            mk.op("pe", lambda e, px=px, pt=pt, ri=ri, kc=kc, hf=hf: e.matmul(
                                px[:, :], Fd[:, pt, ri, kc * 128:(kc + 1) * 128], zT[:, pt, hf * 512:(hf + 1) * 512],
                                start=(pt == 0), stop=(pt == 1)), reads=[Fd, zT], writes=[px])
                for hf in range(2):
                    sl_ = slice(hf * 512, (hf + 1) * 512)
                    pxr, pxi = self.ps[2 + hf], self.ps[4 + hf]
                    a_, b_ = ta[hf], tb[hf]
                    mk.op("dve", lambda e, pxr=pxr, a_=a_, kc=kc, sl_=sl_, o=o: e.tensor_tensor(out=a_[:], in0=pxr[:, :], in1=Hf[:, o, 0, kc, sl_],
                                                                                             op=ALU.mult), reads=[pxr, Hf], writes=[a_])
                    mk.op("dve", lambda e, pxi=pxi, b_=b_, kc=kc, sl_=sl_, o=o: e.tensor_tensor(out=b_[:], in0=pxi[:, :], in1=Hf[:, o, 1, kc, sl_],
                                                                                             op=ALU.mult), reads=[pxi, Hf], writes=[b_])
                    mk.op("pool", lambda e, a_=a_, b_=b_, kc=kc, sl_=sl_: e.tensor_tensor(out=Y[:, 0, kc, sl_], in0=a_[:], in1=b_[:],
                                                                                         op=ALU.subtract), reads=[a_, b_], writes=[Y])
                    mk.op("dve", lambda e, pxr=pxr, a_=a_, kc=kc, sl_=sl_, o=o: e.tensor_tensor(out=a_[:], in0=pxr[:, :], in1=Hf[:, o, 1, kc, sl_],
                                                                                             op=ALU.mult), reads=[pxr, Hf], writes=[a_])
                    mk.op("dve", lambda e, pxi=pxi, b_=b_, kc=kc, sl_=sl_, o=o: e.tensor_tensor(out=b_[:], in0=pxi[:, :], in1=Hf[:, o, 0, kc, sl_],
                                                                                             op=ALU.mult), reads=[pxi, Hf], writes=[b_])
                    mk.op("pool", lambda e, a_=a_, b_=b_, kc=kc, sl_=sl_: e.tensor_tensor(out=Y[:, 1, kc, sl_], in0=a_[:], in1=b_[:],
                                                                                         op=ALU.add), reads=[a_, b_], writes=[Y])
            ob = obf if o == 0 else obb
            for cj in range(DC):
                py = self.ps[6 + cj % 2]
                for kc in range(4):
                    for ri in range(2):
                        mk.op("pe", lambda e, py=py, kc=kc, ri=ri, cj=cj: e.matmul(
                            py[:, 0:L], Y[:, ri, kc, cj * 128:(cj + 1) * 128], Fi[:, kc, ri, :],
                            start=(kc == 0 and ri == 0), stop=(kc == 3 and ri == 1)), reads=[Y, Fi], writes=[py])
                r_ = rr[cj % 2]
                mk.op("dve", lambda e, py=py, r_=r_, cj=cj, o=o: e.scalar_tensor_tensor(out=r_[:], in0=zf[:, cj, :], scalar=lb[:, o, cj:cj + 1],
                                                                                     in1=py[:, 0:L], op0=ALU.mult, op1=ALU.add),
                      reads=[zf, lb, py], writes=[r_])
                mk.op("pool", lambda e, r_=r_, cj=cj, ob=ob: e.tensor_tensor(out=ob[:, cj, :], in0=r_[:], in1=gt[:, cj, :], op=ALU.mult),
                      reads=[r_, gt], writes=[ob])
            dst = Z1 if o == 0 else HT
            mk.dma("act", dst[0:D, 0:L].rearrange("(j p) t -> p j t", p=128), ob[:], reads=[ob], writes=[dst])
        mk.release(m0)

    Prog.hy_ctx_direct = hy_ctx_direct


_hyd_methods()
```

```python
import math
import numpy as np
import ml_dtypes
import concourse.bass as bass
import concourse.mybir as mybir
from concourse.bass_utils import run_bass_kernel_spmd

F32 = mybir.dt.float32
BF16 = mybir.dt.bfloat16
I32 = mybir.dt.int32
ALU = mybir.AluOpType
AF = mybir.ActivationFunctionType
AX = mybir.AxisListType

D = 1024
DC = 8
CTX = 256
SEQ = 4096
NT = CTX + SEQ
DEPTH = 4
EPS = 1e-6
NE = 32
DEXP = 256
BLOCKS = [(0, CTX)] + [(CTX + 512 * i, 512) for i in range(SEQ // 512)]


class T:
    __slots__ = ("t", "lw", "rd", "name")

    def __init__(self, t, name=""):
        self.t = t
        self.lw = None
        self.rd = {}
        self.name = name

    def __getitem__(self, k):
        return self.t[k]


class Lane:
    def __init__(self, name, eng, sem, step):
        self.name, self.eng, self.sem, self.step = name, eng, sem, step
        self.count = 0
        self.seen = {}


class MK:
    def __init__(self, nc, n_dma_sems=40):
        self.nc = nc
        self._stack = []
        self.lanes = {}
        for name, eng in (("pe", nc.tensor), ("act", nc.scalar), ("dve", nc.vector),
                          ("pool", nc.gpsimd), ("sp", nc.sync)):
            sem = self._enter(nc.semaphore("s_" + name))
            self.lanes[name] = Lane(name, eng, sem, 1)
        self.dma_lanes = []
        for i in range(n_dma_sems):
            sem = self._enter(nc.semaphore("d_%d" % i))
            ln = Lane("dma%d" % i, None, sem, 16)
            self.lanes[ln.name] = ln
            self.dma_lanes.append(ln)
        self.n_sw = 14
        self.sw_lanes = self.dma_lanes[:self.n_sw]
        self.hw_lanes = self.dma_lanes[self.n_sw:]
        self.sw_rr = 0
        self.hw_rr = 0
        self.n_inst = 0
        self.uid = 0

    def _next_dma_lane(self, q):
        if q == "pool":
            dl = self.sw_lanes[self.sw_rr]
            self.sw_rr = (self.sw_rr + 1) % len(self.sw_lanes)
        else:
            dl = self.hw_lanes[self.hw_rr]
            self.hw_rr = (self.hw_rr + 1) % len(self.hw_lanes)
        return dl

    def _enter(self, cm):
        v = cm.__enter__()
        self._stack.append(cm)
        return v

    def mark(self):
        return len(self._stack)

    def release(self, mark):
        self.barrier()
        while len(self._stack) > mark:
            self._stack.pop().__exit__(None, None, None)

    def close(self):
        while self._stack:
            self._stack.pop().__exit__(None, None, None)

    def barrier(self):
        for a in ("pe", "act", "dve", "pool", "sp"):
            la = self.lanes[a]
            for b, lb in self.lanes.items():
                if b == a or lb.count == 0:
                    continue
                if la.seen.get(b, 0) >= lb.count:
                    continue
                la.seen[b] = lb.count
                la.eng.wait_ge(lb.sem, lb.count)

    def sbuf(self, name, shape, dt):
        self.uid += 1
        return T(self._enter(self.nc.sbuf_tensor("%s_%d" % (name, self.uid), list(shape), dt)), name)

    def psum(self, name, shape, dt=F32):
        self.uid += 1
        return T(self._enter(self.nc.psum_tensor("%s_%d" % (name, self.uid), list(shape), dt)), name)

    def _deps(self, lane, reads, writes):
        need = {}
        raw_same = 0
        for t in reads:
            if t.lw is not None:
                ln, idx = t.lw
                if need.get(ln, 0) < idx:
                    need[ln] = idx
                if ln == lane.name:
                    raw_same = max(raw_same, idx)
        for t in writes:
            if t.lw is not None:
                ln, idx = t.lw
                if ln != lane.name and need.get(ln, 0) < idx:
                    need[ln] = idx
            for ln, idx in t.rd.items():
                if ln != lane.name and need.get(ln, 0) < idx:
                    need[ln] = idx
        return need, raw_same

    def _record(self, lane, reads, writes):
        idx = lane.count
        for t in reads:
            if t.rd.get(lane.name, 0) < idx:
                t.rd[lane.name] = idx
        for t in writes:
            t.lw = (lane.name, idx)
            t.rd = {}

    def op(self, lane_name, fn, reads=(), writes=()):
        lane = self.lanes[lane_name]
        need, raw_same = self._deps(lane, reads, writes)
        for ln, idx in need.items():
            if ln == lane.name:
                if lane.name == "pe" or raw_same <= lane.count - 3:
                    continue
                idx = raw_same
            if lane.seen.get(ln, 0) >= idx:
                continue
            lane.seen[ln] = idx
            lane.eng.wait_ge(self.lanes[ln].sem, idx)
        inst = fn(lane.eng)
        lane.count += 1
        inst.then_inc(lane.sem, 1)
        self._record(lane, reads, writes)
        self.n_inst += 1
        return inst

    def dma(self, q, out, in_, reads=(), writes=(), **kw):
        qlane = self.lanes[q]
        dl = self._next_dma_lane(q)
        need, _ = self._deps(dl, reads, writes)
        if dl.count > 0:
            need[dl.name] = max(need.get(dl.name, 0), dl.count)
        for ln, idx in need.items():
            if qlane.seen.get(ln, 0) >= idx:
                continue
            qlane.seen[ln] = idx
            qlane.eng.wait_ge(self.lanes[ln].sem, idx)
        inst = qlane.eng.dma_start(out=out, in_=in_, **kw)
        dl.count += 16
        inst.then_inc(dl.sem, 16)
        self._record(dl, reads, writes)
        self.n_inst += 1
        return inst

    def finish(self):
        sp = self.lanes["sp"]
        for dl in self.dma_lanes:
            if dl.count and sp.seen.get(dl.name, 0) < dl.count:
                sp.seen[dl.name] = dl.count
                sp.eng.wait_ge(dl.sem, dl.count)
        self.barrier()


def _vec_pc(v):
    v = np.asarray(v, np.float32)
    return np.ascontiguousarray(v.reshape(-1, 128).T)


class Prog:
    def __init__(self, cfg=None):
        self.cfg = cfg or {}
        self.nc = bass.Bass("TRN2", target_bir_lowering=False)
        self.mk = MK(self.nc)
        self.ins = {}
        self.dram = {}

    def inp(self, name, shape, dt=F32):
        t = self.nc.dram_tensor(name, list(shape), dt, kind="ExternalInput")
        self.ins[name] = t
        return T(t, name)

    def scratch(self, name, shape, dt, out=False):
        kind = "ExternalOutput" if (out or name in self.cfg.get("dump", ())) else "Internal"
        t = self.nc.dram_tensor(name, list(shape), dt, kind=kind)
        self.dram[name] = T(t, name)
        return self.dram[name]

    def setup_consts(self):
        mk = self.mk
        self.ps = [mk.psum("ps%d" % i, [128, 512], F32) for i in range(8)]
        self.ident_f = mk.sbuf("ident_f", [128, 128], F32)
        self.ident_b = mk.sbuf("ident_b", [128, 128], BF16)
        self.ones_b = mk.sbuf("ones_b", [128, 128], BF16)
        self.avg_b = mk.sbuf("avg_b", [128, 128], BF16)
        mk.op("pool", lambda e: e.memset(self.ident_f[:], 1.0), writes=[self.ident_f])
        mk.op("pool", lambda e: e.affine_select(out=self.ident_f[:], in_=self.ident_f[:],
              pattern=[[-1, 128]], compare_op=ALU.is_equal, fill=0.0, base=0,
              channel_multiplier=1), reads=[self.ident_f], writes=[self.ident_f])
        mk.op("dve", lambda e: e.tensor_copy(self.ident_b[:], self.ident_f[:]),
              reads=[self.ident_f], writes=[self.ident_b])
        mk.op("pool", lambda e: e.memset(self.ones_b[:], 1.0), writes=[self.ones_b])
        mk.op("pool", lambda e: e.memset(self.avg_b[:], 1.0 / D), writes=[self.avg_b])
        self.eps_t = mk.sbuf("eps_t", [128, 1], F32)
        mk.op("pool", lambda e: e.memset(self.eps_t[:], EPS), writes=[self.eps_t])

    def stage_input(self, x_in, ctx_in, XT):
        mk = self.mk
        m0 = mk.mark()
        xin = [mk.sbuf("xin%d" % i, [128, 4, D], F32) for i in range(2)]
        xo = [mk.sbuf("xo%d" % i, [128, DC, 512], F32) for i in range(2)]
        for bi, (t0, tn) in enumerate(BLOCKS):
            nt = tn // 128
            buf = xin[bi % 2]
            src = ctx_in if bi == 0 else x_in
            r0 = 0 if bi == 0 else t0 - CTX
            mk.dma("sp", buf[:, 0:nt, :], src[r0:r0 + tn, :].rearrange("(a p) d -> p a d", p=128),
                   reads=[src], writes=[buf])
            ob = xo[bi % 2]
            for j in range(DC):
                ps = self.ps[j % 8]
                for a in range(nt):
                    mk.op("pe", lambda e, ps=ps, a=a, j=j, buf=buf: e.transpose(
                        ps[:, a * 128:(a + 1) * 128], buf[:, a, j * 128:(j + 1) * 128], self.ident_f[:]),
                        reads=[buf, self.ident_f], writes=[ps])
                eng = "act" if j % 2 else "dve"
                if eng == "act":
                    mk.op("act", lambda e, ps=ps, j=j, ob=ob: e.copy(ob[:, j, 0:tn], ps[:, 0:tn]),
                          reads=[ps], writes=[ob])
                else:
                    mk.op("dve", lambda e, ps=ps, j=j, ob=ob: e.tensor_copy(ob[:, j, 0:tn], ps[:, 0:tn]),
                          reads=[ps], writes=[ob])
            mk.dma("pool", XT[:, t0:t0 + tn].rearrange("(j p) t -> p j t", p=128), ob[:, :, 0:tn],
                   reads=[ob], writes=self.XTr)
        mk.release(m0)

    def stage_output(self, XT, y_out, fg):
        mk = self.mk
        m0 = mk.mark()
        xb = [mk.sbuf("fx%d" % i, [128, DC, 512], F32) for i in range(2)]
        sq = mk.sbuf("fsq", [128, DC, 512], BF16)
        rstd = mk.sbuf("frstd", [128, 512], F32)
        yo = [mk.sbuf("fy%d" % i, [128, 4, D], F32) for i in range(2)]
        for bi, (t0, tn) in enumerate(BLOCKS):
            if bi == 0:
                continue
            buf = xb[bi % 2]
            mk.dma("sp", buf[:, :, 0:tn], XT[:, t0:t0 + tn].rearrange("(j p) t -> p j t", p=128),
                   reads=self.XTr, writes=[buf])
            self._rstd(buf, sq, rstd, tn, self.ps[0])
            for j in range(DC):
                mk.op("dve", lambda e, j=j, buf=buf: e.scalar_tensor_tensor(
                    out=buf[:, j, 0:tn], in0=buf[:, j, 0:tn], scalar=fg[:, j:j + 1], in1=rstd[:, 0:tn],
                    op0=ALU.mult, op1=ALU.mult), reads=[buf, rstd, fg], writes=[buf])
            ob = yo[bi % 2]
            nt = tn // 128
            for a in range(nt):
                for h in range(2):
                    ps = self.ps[1 + (a * 2 + h) % 7]
                    for jj in range(4):
                        j = h * 4 + jj
                        mk.op("pe", lambda e, ps=ps, a=a, j=j, jj=jj, buf=buf: e.transpose(
                            ps[:, jj * 128:(jj + 1) * 128], buf[:, j, a * 128:(a + 1) * 128], self.ident_f[:]),
                            reads=[buf, self.ident_f], writes=[ps])
                    if h:
                        mk.op("act", lambda e, ps=ps, a=a, h=h, ob=ob: e.copy(
                            ob[:, a, h * 512:(h + 1) * 512], ps[:, :]), reads=[ps], writes=[ob])
                    else:
                        mk.op("dve", lambda e, ps=ps, a=a, h=h, ob=ob: e.tensor_copy(
                            ob[:, a, h * 512:(h + 1) * 512], ps[:, :]), reads=[ps], writes=[ob])
            r0 = t0 - CTX
            mk.dma("pool", y_out[r0:r0 + tn, :].rearrange("(a p) d -> p a d", p=128), ob[:, 0:nt, :],
                   reads=[ob], writes=[y_out])
        mk.release(m0)

    def _rstd(self, buf, sq, rstd, tn, ps, eps=EPS):
        mk = self.mk
        for j in range(DC):
            mk.op("act", lambda e, j=j: e.activation(out=sq[:, j, 0:tn], in_=buf[:, j, 0:tn], func=AF.Square),
                  reads=[buf], writes=[sq])
        for j in range(DC):
            mk.op("pe", lambda e, j=j: e.matmul(ps[:, 0:tn], self.avg_b[:], sq[:, j, 0:tn],
                                                 start=(j == 0), stop=(j == DC - 1)),
                  reads=[self.avg_b, sq], writes=[ps])
        mk.op("act", lambda e: e.activation(out=rstd[:, 0:tn], in_=ps[:, 0:tn], func=AF.Sqrt, bias=self.eps_t[:, 0:1]),
              reads=[ps, self.eps_t], writes=[rstd])
        mk.op("dve", lambda e: e.reciprocal(rstd[:, 0:tn], rstd[:, 0:tn]), reads=[rstd], writes=[rstd])

    def ap3(self, t2d, lo, n, inner):
        return t2d[:, lo:lo + n * inner].rearrange("p (a b) -> p a b", b=inner)

    def stage_mod(self, i, sT, w_mod, bmod, gmix, gffn):
        mk = self.mk
        m0 = mk.mark()
        wb = [mk.sbuf("wmod%d" % k, [128, DC, 512], F32) for k in range(2)]
        bm = mk.sbuf("bm", [128, 48], F32)
        gm = mk.sbuf("gm", [128, 2, DC], F32)
        mod = mk.sbuf("mod", [128, 48, 2], F32)
        mk.dma("sp", bm[:], bmod[i, :, :], reads=[bmod], writes=[bm])
        mk.dma("sp", gm[:, 0, :], gmix[i, :, :], reads=[gmix], writes=[gm])
        mk.dma("sp", gm[:, 1, :], gffn[i, :, :], reads=[gffn], writes=[gm])
        ps = self.ps[0]
        for s in range(12):
            w = wb[s % 2]
            mk.dma("sp", w[:], w_mod[i, :, s * 512:(s + 1) * 512].rearrange("(k p) m -> p k m", p=128),
                   reads=[w_mod], writes=[w])
            for mm in range(4):
                m = s * 4 + mm
                for k in range(DC):
                    mk.op("pe", lambda e, w=w, mm=mm, m=m, k=k: e.matmul(
                        ps[:, 2 * m:2 * m + 2], w[:, k, mm * 128:(mm + 1) * 128], sT[:, k, :],
                        start=(k == 0), stop=(k == DC - 1)), reads=[w, sT], writes=[ps])
        psv = ps[:, 0:96].rearrange("p (m r) -> p m r", r=2)
        for r in range(2):
            mk.op("dve", lambda e, r=r: e.tensor_tensor(out=mod[:, :, r], in0=psv[:, :, r], in1=bm[:, :], op=ALU.add),
                  reads=[ps, bm], writes=[mod])
        for r in range(2):
            for nm, sc_c, sh_c, g_c, gi in (("1", 8, 0, 16, 0), ("2", 32, 24, 40, 1)):
                gs = self.mv[("gs" + nm, r)]
                mk.op("dve", lambda e, gs=gs, sc_c=sc_c, gi=gi, r=r: e.scalar_tensor_tensor(
                    out=gs[:], in0=mod[:, sc_c:sc_c + 8, r], scalar=1.0, in1=gm[:, gi, :],
                    op0=ALU.add, op1=ALU.mult), reads=[mod, gm], writes=[gs])
                sh = self.mv[("sh" + nm, r)]
                mk.op("dve", lambda e, sh=sh, sh_c=sh_c, r=r: e.tensor_copy(sh[:], mod[:, sh_c:sh_c + 8, r]),
                      reads=[mod], writes=[sh])
                g = self.mv[("g" + nm, r)]
                mk.op("dve", lambda e, g=g, g_c=g_c, r=r: e.tensor_copy(g[:], mod[:, g_c:g_c + 8, r]),
                      reads=[mod], writes=[g])
        mk.release(m0)

    def alloc_mv(self):
        self.mv = {}
        for r in range(2):
            for nm in ("gs1", "sh1", "g1", "gs2", "sh2", "g2"):
                self.mv[(nm, r)] = self.mk.sbuf("mv_%s_%d" % (nm, r), [128, DC], F32)

    def stage_norm(self, XT, HT, which, router=None):
        mk = self.mk
        m0 = mk.mark()
        xb = [mk.sbuf("nx%d" % k, [128, DC, 512], F32) for k in range(2)]
        sq = mk.sbuf("nsq", [128, DC, 512], BF16)
        rstd = mk.sbuf("nrstd", [128, 512], F32)
        hb = [mk.sbuf("nhb%d" % k, [128, DC, 512], BF16) for k in range(2)]
        if router is not None:
            htk = [mk.sbuf("nhtk%d" % k, [128, D], BF16) for k in range(2)]
            rts = [{k: mk.sbuf("rt%d_%s" % (q, k), [128, n], F32) for k, n in
                    (("lg", 36), ("gmax", 1), ("ngmax", 1), ("ge", 4), ("gsum", 1), ("gp", 1), ("mg", 4), ("es", 8),
                     ("t8", 8), ("dd", 1), ("w2", 1), ("a1", 1), ("a2", 1), ("m1", 8), ("m2", 8), ("cw", 8),
                     ("comb", 32))} for q in range(3)]
            rtn = 0
        for bi, (t0, tn) in enumerate(BLOCKS):
            r = 1 if bi == 0 else 0
            gs, sh = self.mv[("gs" + which, r)], self.mv[("sh" + which, r)]
            buf = xb[bi % 2]
            mk.dma("sp", buf[:, :, 0:tn], XT[:, t0:t0 + tn].rearrange("(j p) t -> p j t", p=128),
                   reads=self.XTr, writes=[buf])
            self._rstd(buf, sq, rstd, tn, self.ps[0])
            h = hb[bi % 2]
            for j in range(DC):
                mk.op("dve", lambda e, j=j, buf=buf, gs=gs: e.scalar_tensor_tensor(
                    out=buf[:, j, 0:tn], in0=buf[:, j, 0:tn], scalar=gs[:, j:j + 1], in1=rstd[:, 0:tn],
                    op0=ALU.mult, op1=ALU.mult), reads=[buf, rstd, gs], writes=[buf])
                if router is not None:
                    mk.op("dve", lambda e, j=j, buf=buf, sh=sh: e.tensor_scalar(
                        out=buf[:, j, 0:tn], in0=buf[:, j, 0:tn], scalar1=sh[:, j:j + 1], scalar2=None,
                        op0=ALU.add), reads=[buf, sh], writes=[buf])
                    mk.op("act", lambda e, j=j, buf=buf, h=h: e.copy(h[:, j, 0:tn], buf[:, j, 0:tn]),
                          reads=[buf], writes=[h])
                else:
                    mk.op("act", lambda e, j=j, buf=buf, h=h, sh=sh: e.activation(
                        out=h[:, j, 0:tn], in_=buf[:, j, 0:tn], func=AF.Identity, bias=sh[:, j:j + 1]),
                        reads=[buf, sh], writes=[h])
            mk.dma("pool", HT[:, t0:t0 + tn].rearrange("(j p) t -> p j t", p=128), h[:, :, 0:tn],
                   reads=[h], writes=[HT])
            if router is not None:
                for a in range(tn // 128):
                    rt = rts[rtn % 3]
                    rtn += 1
                    if "M1a" in router:
                        self._route_tile_sparse(buf, a, t0 + a * 128, router, rt)
                        pb = self.ps[5 + (a % 2)]
                        pbb = pb[:, 0:512].bitcast(BF16)
                        for j in range(DC):
                            mk.op("pe", lambda e, pbb=pbb, j=j, a=a, h=h: e.transpose(
                                pbb[:, j * 128:(j + 1) * 128], h[:, j, a * 128:(a + 1) * 128], self.ident_b[:]),
                                reads=[h, self.ident_b], writes=[pb])
                        ht_ = htk[a % 2]
                        mk.op("act", lambda e, pbb=pbb, ht_=ht_: e.copy(ht_[:], pbb[:, :]), reads=[pb], writes=[ht_])
                        mk.dma("act", self.dram["HTOK"][t0 + a * 128:t0 + (a + 1) * 128, :], ht_[:], reads=[ht_],
                               writes=[self.dram["HTOK"]])
                    else:
                        self._route_tile(buf, a, t0 + a * 128, router, rt)
        mk.release(m0)

    def _route_tile(self, hf, a, tok0, R, rt):
        mk = self.mk
        ps = self.ps[1 + (a % 2)]
        Wr, br, combT = R["Wr"], R["br"], R["combT"]
        for k in range(DC):
            mk.op("pe", lambda e, k=k: e.matmul(ps[:, 0:36], hf[:, k, a * 128:(a + 1) * 128], Wr[:, k, :],
                                                 start=(k == 0), stop=(k == DC - 1)),
                  reads=[hf, Wr], writes=[ps])
        lg, gmax, ngmax, ge, gsum, gp, mg, es = (rt[k] for k in ("lg", "gmax", "ngmax", "ge", "gsum", "gp", "mg", "es"))
        t8, dd, w2, a1, a2, m1, m2, cw, comb = (rt[k] for k in ("t8", "dd", "w2", "a1", "a2", "m1", "m2", "cw", "comb"))
        V = lambda fn, reads, writes: mk.op("dve", fn, reads=reads, writes=writes)
        V(lambda e: e.tensor_tensor(out=lg[:], in0=ps[:, 0:36], in1=br[:], op=ALU.add), [ps, br], [lg])
        V(lambda e: e.reduce_max(out=gmax[:], in_=lg[:, 0:4], axis=AX.X), [lg], [gmax])
        V(lambda e: e.tensor_scalar(out=ngmax[:], in0=gmax[:], scalar1=-1.0, scalar2=None, op0=ALU.mult), [gmax], [ngmax])
        mk.op("act", lambda e: e.activation(out=ge[:], in_=lg[:, 0:4], func=AF.Exp, bias=ngmax[:, 0:1],
                                            accum_out=gsum[:]), reads=[lg, ngmax], writes=[ge, gsum])
        V(lambda e: e.reciprocal(gp[:], gsum[:]), [gsum], [gp])
        V(lambda e: e.tensor_scalar(out=mg[:], in0=lg[:, 0:4], scalar1=gmax[:, 0:1], scalar2=None, op0=ALU.is_equal),
          [lg, gmax], [mg])
        V(lambda e: e.tensor_scalar(out=es[:], in0=lg[:, 4:12], scalar1=mg[:, 0:1], scalar2=None, op0=ALU.mult),
          [lg, mg], [es])
        for g in range(1, 4):
            V(lambda e, g=g: e.scalar_tensor_tensor(out=es[:], in0=lg[:, 4 + 8 * g:12 + 8 * g], scalar=mg[:, g:g + 1],
                                                    in1=es[:], op0=ALU.mult, op1=ALU.add), [lg, mg, es], [es])
        V(lambda e: e.max(out=t8[:], in_=es[:]), [es], [t8])
        V(lambda e: e.tensor_tensor(out=dd[:], in0=t8[:, 1:2], in1=t8[:, 0:1], op=ALU.subtract), [t8], [dd])
        mk.op("act", lambda e: e.activation(out=w2[:], in_=dd[:], func=AF.Sigmoid), reads=[dd], writes=[w2])
        V(lambda e: e.tensor_tensor(out=a2[:], in0=w2[:], in1=gp[:], op=ALU.mult), [w2, gp], [a2])
        V(lambda e: e.tensor_tensor(out=a1[:], in0=gp[:], in1=a2[:], op=ALU.subtract), [gp, a2], [a1])
        V(lambda e: e.tensor_scalar(out=m1[:], in0=es[:], scalar1=t8[:, 0:1], scalar2=a1[:, 0:1], op0=ALU.is_equal,
                                    op1=ALU.mult), [es, t8, a1], [m1])
        V(lambda e: e.tensor_scalar(out=m2[:], in0=es[:], scalar1=t8[:, 1:2], scalar2=a2[:, 0:1], op0=ALU.is_equal,
                                    op1=ALU.mult), [es, t8, a2], [m2])
        V(lambda e: e.tensor_tensor(out=cw[:], in0=m1[:], in1=m2[:], op=ALU.add), [m1, m2], [cw])
        for g in range(4):
            V(lambda e, g=g: e.tensor_scalar(out=comb[:, 8 * g:8 * g + 8], in0=cw[:], scalar1=mg[:, g:g + 1],
                                             scalar2=None, op0=ALU.mult), [cw, mg], [comb])
        ps2 = self.ps[3 + (a % 2)]
        mk.op("pe", lambda e: e.transpose(ps2[0:32, 0:128], comb[:, :], self.ident_f[:]),
              reads=[comb, self.ident_f], writes=[ps2])
        mk.op("act", lambda e: e.copy(combT[:, tok0:tok0 + 128], ps2[0:32, 0:128]), reads=[ps2], writes=[combT])

    def stage_moe(self, i, XT, HT, HID, combT, sel, w_gate, w_up, w_down):
        mk = self.mk
        m0 = mk.mark()
        hT = mk.sbuf("moe_hT", [128, DC, NT], BF16)
        for j in range(DC):
            mk.dma("sp", hT[:, j, :], HT[j * 128:(j + 1) * 128, :], reads=[HT], writes=[hT])
        wgu = [mk.sbuf("wgu%d" % k, [128, DC, 512], BF16) for k in range(2)]
        cbs = [mk.sbuf("cbs%d" % k, [128, 512], F32) for k in range(2)]
        sl = [mk.sbuf("sl%d" % k, [128, 512], F32) for k in range(2)]
        tt = [mk.sbuf("tt%d" % k, [128, 512], F32) for k in range(2)]
        hid = [mk.sbuf("hid%d" % k, [128, 2, 512], BF16) for k in range(2)]
        n = 0
        for ex in range(NE):
            g, el = ex // 8, ex % 8
            w = wgu[ex % 2]
            mk.dma("pool", w[:, :, 0:256], w_gate[i, g, el, :, :].rearrange("(k p) f -> p k f", p=128),
                   reads=[w_gate], writes=[w])
            mk.dma("pool", w[:, :, 256:512], w_up[i, g, el, :, :].rearrange("(k p) f -> p k f", p=128),
                   reads=[w_up], writes=[w])
            for bi, (t0, tn) in enumerate(BLOCKS):
                pc = self.ps[n % 2]
                mk.op("pe", lambda e, pc=pc, ex=ex: e.matmul(pc[:, 0:tn], sel[:, ex, :], combT[:, t0:t0 + tn],
                                                            start=True, stop=True),
                      reads=[sel, combT], writes=[pc])
                cb = cbs[n % 2]
                mk.op("act", lambda e, pc=pc, cb=cb: e.copy(cb[:, 0:tn], pc[:, 0:tn]), reads=[pc], writes=[cb])
                hd = hid[n % 2]
                for fc in range(2):
                    q = (2 * n + fc) % 3
                    pg, pu = self.ps[2 + 2 * q], self.ps[3 + 2 * q]
                    for k in range(DC):
                        mk.op("pe", lambda e, pg=pg, k=k, fc=fc, w=w: e.matmul(
                            pg[:, 0:tn], w[:, k, fc * 128:(fc + 1) * 128], hT[:, k, t0:t0 + tn],
                            start=(k == 0), stop=(k == DC - 1)), reads=[w, hT], writes=[pg])
                    for k in range(DC):
                        mk.op("pe", lambda e, pu=pu, k=k, fc=fc, w=w: e.matmul(
                            pu[:, 0:tn], w[:, k, 256 + fc * 128:256 + (fc + 1) * 128], hT[:, k, t0:t0 + tn],
                            start=(k == 0), stop=(k == DC - 1)), reads=[w, hT], writes=[pu])
                    s_, t_ = sl[fc], tt[fc]
                    mk.op("act", lambda e, pg=pg, s_=s_: e.activation(out=s_[:, 0:tn], in_=pg[:, 0:tn], func=AF.Silu),
                          reads=[pg], writes=[s_])
                    mk.op("dve", lambda e, pu=pu, s_=s_, t_=t_: e.tensor_tensor(
                        out=t_[:, 0:tn], in0=pu[:, 0:tn], in1=s_[:, 0:tn], op=ALU.mult), reads=[pu, s_], writes=[t_])
                    mk.op("pool", lambda e, t_=t_, cb=cb, hd=hd, fc=fc: e.tensor_tensor(
                        out=hd[:, fc, 0:tn], in0=t_[:, 0:tn], in1=cb[:, 0:tn], op=ALU.mult), reads=[t_, cb], writes=[hd])
                mk.dma("act", HID[ex * 256:(ex + 1) * 256, t0:t0 + tn].rearrange("(c p) t -> p c t", p=128),
                       hd[:, :, 0:tn], reads=[hd], writes=[HID])
                n += 1
        mk.release(m0)
        m0 = mk.mark()
        hb = [mk.sbuf("p2h%d" % k, [128, 64, 512], BF16) for k in range(1)]
        wd = [mk.sbuf("p2w%d" % k, [128, 8, D], BF16) for k in range(2)]
        xr = [mk.sbuf("p2x%d" % k, [128, 512], F32) for k in range(3)]
        wdv = w_down[i].rearrange("g e f d -> (g e f) d")
        n = 0
        for bi, (t0, tn) in enumerate(BLOCKS):
            r = 1 if bi == 0 else 0
            g2 = self.mv[("g2", r)]
            h = hb[0]
            for c8 in range(8):
                mk.dma("sp", h[:, c8 * 8:(c8 + 1) * 8, 0:tn],
                       HID[c8 * 1024:(c8 + 1) * 1024, t0:t0 + tn].rearrange("(c p) t -> p c t", p=128),
                       reads=[HID], writes=[h])
            for kg in range(8):
                w = wd[n % 2]
                n += 1
                mk.dma("pool", w[:], wdv[kg * 1024:(kg + 1) * 1024, :].rearrange("(c p) d -> p c d", p=128),
                       reads=[w_down], writes=[w])
                for kk in range(8):
                    kc = kg * 8 + kk
                    for dc in range(DC):
                        mk.op("pe", lambda e, dc=dc, kk=kk, kc=kc, w=w: e.matmul(
                            self.ps[dc][:, 0:tn], w[:, kk, dc * 128:(dc + 1) * 128], h[:, kc, 0:tn],
                            start=(kc == 0), stop=(kc == 63)), reads=[w, h], writes=[self.ps[dc]])
            for dc in range(DC):
                x = xr[dc % 3]
                mk.dma("sp", x[:, 0:tn], XT[dc * 128:(dc + 1) * 128, t0:t0 + tn], reads=[self.XTr[dc]], writes=[x])
                mk.op("dve", lambda e, dc=dc, x=x, g2=g2: e.scalar_tensor_tensor(
                    out=x[:, 0:tn], in0=self.ps[dc][:, 0:tn], scalar=g2[:, dc:dc + 1], in1=x[:, 0:tn],
                    op0=ALU.mult, op1=ALU.add), reads=[self.ps[dc], g2, x], writes=[x])
                mk.dma("act", XT[dc * 128:(dc + 1) * 128, t0:t0 + tn], x[:, 0:tn], reads=[x], writes=[self.XTr[dc]])
        mk.release(m0)


def build_program(cfg=None):
    cfg = cfg or {}
    p = Prog(cfg)
    mk = p.mk
    layers = cfg.get("layers", list(range(DEPTH)))
    I = {}
    I["x"] = p.inp("x", [SEQ, D])
    I["ctx"] = p.inp("ctx", [CTX, D])
    I["c_pc"] = p.inp("c_pc", [128, DC, 2])
    I["w_mod"] = p.inp("w_mod", [DEPTH, D, 6 * D])
    I["bmod_pc"] = p.inp("bmod_pc", [DEPTH, 128, 48])
    I["gmix_pc"] = p.inp("gmix_pc", [DEPTH, 128, DC])
    I["gffn_pc"] = p.inp("gffn_pc", [DEPTH, 128, DC])
    I["fg_pc"] = p.inp("fg_pc", [128, DC])
    I["wr_pc"] = p.inp("wr_pc", [DEPTH, 128, DC, 36])
    I["br_bc"] = p.inp("br_bc", [DEPTH, 128, 36])
    I["sel"] = p.inp("sel", [32, NE, 128], BF16)
    sparse = cfg.get("sparse", True)
    if sparse:
        for li in range(DEPTH):
            I["moe_wgu%d" % li] = p.inp("moe_wgu%d" % li, [NE * 128, DC * 512])
            I["moe_wdn%d" % li] = p.inp("moe_wdn%d" % li, [NE * 128, 2 * D])
        I["jgrid"] = p.inp("jgrid", [128, NBLK])
        I["pidx"] = p.inp("pidx", [128, 1])
    else:
        I["moe_w_gate"] = p.inp("moe_w_gate", [DEPTH, 4, 8, D, DEXP])
        I["moe_w_up"] = p.inp("moe_w_up", [DEPTH, 4, 8, D, DEXP])
        I["moe_w_down"] = p.inp("moe_w_down", [DEPTH, 4, 8, DEXP, D])
    I["attn_w_q"] = p.inp("attn_w_q", [2, D, D])
    I["attn_w_kv"] = p.inp("attn_w_kv", [2, D, 512])
    I["attn_w_o"] = p.inp("attn_w_o", [2, D, D])
    I["qkgain_pc"] = p.inp("qkgain_pc", [2, 128, 2])
    I["conv_w_pw1"] = p.inp("conv_w_pw1", [1, D, 2 * D])
    I["conv_w_pw2"] = p.inp("conv_w_pw2", [1, D, D])
    I["conv_b_pw1_pc"] = p.inp("conv_b_pw1_pc", [1, 128, 16])
    I["conv_w_dw_pc"] = p.inp("conv_w_dw_pc", [1, 128, DC, 31])
    I["conv_b_dw_pc"] = p.inp("conv_b_dw_pc", [1, 128, DC])
    I["conv_ln_g_pc"] = p.inp("conv_ln_g_pc", [1, 128, DC])
    I["conv_ln_b_pc"] = p.inp("conv_ln_b_pc", [1, 128, DC])
    I["conv_b_pw2_pc"] = p.inp("conv_b_pw2_pc", [1, 128, DC])
    I["hy_w_in"] = p.inp("hy_w_in", [1, D, 3 * D])
    I["hy_w_out"] = p.inp("hy_w_out", [1, D, D])
    I["hy_b_in_pc"] = p.inp("hy_b_in_pc", [128, 24])
    I["hy_w_short_pc"] = p.inp("hy_w_short_pc", [128, 24, 3])
    I["hy_b_short_pc"] = p.inp("hy_b_short_pc", [128, 24])
    I["hy_b_out_pc"] = p.inp("hy_b_out_pc", [128, DC])
    I["hy_lbias_pc"] = p.inp("hy_lbias_pc", [2, 128, DC])
    I["hy_f_w1"] = p.inp("hy_f_w1", [1, 17, 64])
    I["hy_f_w2"] = p.inp("hy_f_w2", [1, 64, 64])
    I["hy_f_w3"] = p.inp("hy_f_w3", [1, 64, 4 * D])
    I["hy_fvec"] = p.inp("hy_fvec", [64, 4])
    I["hy_dabs"] = p.inp("hy_dabs", [64, D])
    for tag, L_ in (("lat", SEQ), ("ctx", CTX)):
        N1_ = 2 * L_ // 64
        I["hy_F1_" + tag] = p.inp("hy_F1_" + tag, [N1_ // 2, 6, N1_], BF16)
        I["hy_G_" + tag] = p.inp("hy_G_" + tag, [128, N1_, 5, 128], BF16)
        I["hy_Fi_" + tag] = p.inp("hy_Fi_" + tag, [N1_, 2, N1_ // 2], BF16)
        I["hy_lneg_" + tag] = p.inp("hy_lneg_" + tag, [N1_ // 2, 64])
        I["hy_featsT_" + tag] = p.inp("hy_featsT_" + tag, [17, L_])
    I["hyd_F"] = p.inp("hyd_F", [128, 2, 3, 512], BF16)
    I["hyd_Fi"] = p.inp("hyd_Fi", [128, 4, 2, CTX], BF16)
    I["hyd_lneg"] = p.inp("hyd_lneg", [128, 2])
    I["ropeR"] = p.inp("ropeR", [128, 128], BF16)
    I["ropeC"] = p.inp("ropeC", [128, SEQ])
    I["ropeS"] = p.inp("ropeS", [128, SEQ])
    y = T(p.nc.dram_tensor("y", [SEQ, D], F32, kind="ExternalOutput"), "y")
    QT = p.scratch("QT", [D, NT], BF16)
    OT = p.scratch("OT", [D, NT], BF16)
    XT = p.scratch("XT", [D, NT], F32)
    p.XTr = [T(XT.t, "XTr%d" % k) for k in range(DC)]
    VT = p.scratch("VT", [D, NT], F32)
    if 2 in layers and cfg.get("mixers", True):
        p.scratch("hyU0", [3 * D, NT], F32)
        p.scratch("hyZ", [3 * D, NT], F32)
        p.scratch("hyZ1", [D, NT], F32)
        p.scratch("hyA", [2, 128, 64, D], BF16)
        p.scratch("hyC", [2, 64, 128, D], BF16)
        for o in range(2):
            p.scratch("hyHH_lat%d" % o, [2, 128, 128, D], BF16)
            p.scratch("hyHH_ctx%d" % o, [2, 8, 128, D], BF16)
    HT = p.scratch("HT", [D, NT], BF16)
    p.dram["HT"] = HT
    if sparse:
        p.scratch("HTOK", [NT, D], BF16)
        p.scratch("HS", [NSLOT, D], BF16)
        p.scratch("YS", [NSLOT, D], BF16)
    else:
        HID = p.scratch("HID", [NE * DEXP, NT], BF16)
    p.setup_consts()
    p.alloc_mv()
    fg = mk.sbuf("fg", [128, DC], F32)
    sT = mk.sbuf("sT", [128, DC, 2], F32)
    sel = mk.sbuf("sel", [32, NE, 128], BF16)
    if sparse:
        SR = p.alloc_sparse()
        mk.dma("sp", SR["jg"][:], I["jgrid"][:, :], writes=[SR["jg"]])
        mk.dma("sp", SR["pidx"][:], I["pidx"][:, :], writes=[SR["pidx"]])
    else:
        combT = mk.sbuf("combT", [32, NT], BF16)
    Wr = mk.sbuf("Wr", [128, DC, 36], F32)
    br = mk.sbuf("br", [128, 36], F32)
    mk.dma("sp", fg[:], I["fg_pc"][:, :], reads=[I["fg_pc"]], writes=[fg])
    mk.dma("sp", sT[:], I["c_pc"][:, :, :], reads=[I["c_pc"]], writes=[sT])
    mk.dma("sp", sel[:], I["sel"][:, :, :], reads=[I["sel"]], writes=[sel])
    mk.op("act", lambda e: e.activation(out=sT[:], in_=sT[:], func=AF.Silu), reads=[sT], writes=[sT])
    p.stage_input(I["x"], I["ctx"], XT)
    for i in layers:
        p.stage_mod(i, sT, I["w_mod"], I["bmod_pc"], I["gmix_pc"], I["gffn_pc"])
        if cfg.get("mixers", True):
            kind, slot = i % 3, i // 3
            with_ctx = i < DEPTH - 1
            p.stage_norm(XT, HT, "1")
            if kind == 0:
                p.stage_attn(slot, XT, HT, QT, OT, I, with_ctx)
            elif kind == 1:
                p.stage_conf(slot, XT, HT, QT, VT, I)
            else:
                p.stage_hyena(slot, XT, HT, I)
        if cfg.get("moe", True) is False:
            continue
        mk.dma("sp", Wr[:], I["wr_pc"][i, :, :, :], reads=[I["wr_pc"]], writes=[Wr])
        mk.dma("sp", br[:], I["br_bc"][i, :, :], reads=[I["br_bc"]], writes=[br])
        if sparse:
            SR["Wr"], SR["br"] = Wr, br
            p.stage_norm(XT, HT, "2", router=SR)
            p.stage_moe_sparse(i, XT, SR, I)
        else:
            p.stage_norm(XT, HT, "2", router=dict(Wr=Wr, br=br, combT=combT))
            p.stage_moe(i, XT, HT, HID, combT, sel, I["moe_w_gate"], I["moe_w_up"], I["moe_w_down"])
    p.stage_output(XT, y, fg)
    mk.finish()
    mk.close()
    return p


_HYC = {}


def host_inputs(inp, b, sparse=True):
    f = lambda a: np.ascontiguousarray(np.asarray(a, np.float32))
    m = {}
    m["x"] = f(inp["x"][b])
    m["ctx"] = f(inp["ctx"][b])
    m["c_pc"] = np.ascontiguousarray(np.stack([_vec_pc(inp["c"][b]), _vec_pc(inp["c_ctx"])], axis=-1))
    m["w_mod"] = f(inp["w_mod"])
    m["bmod_pc"] = np.stack([_vec_pc(inp["b_mod"][i]) for i in range(DEPTH)])
    m["gmix_pc"] = np.stack([_vec_pc(inp["norm_mix_g"][i]) for i in range(DEPTH)])
    m["gffn_pc"] = np.stack([_vec_pc(inp["norm_ffn_g"][i]) for i in range(DEPTH)])
    m["fg_pc"] = _vec_pc(inp["final_norm_g"])
    wr = np.concatenate([np.asarray(inp["moe_w_group"]), np.asarray(inp["moe_w_router"])], axis=-1)
    m["wr_pc"] = np.ascontiguousarray(wr.reshape(DEPTH, DC, 128, 36).transpose(0, 2, 1, 3)).astype(np.float32)
    brr = np.concatenate([np.asarray(inp["moe_b_group"]), np.asarray(inp["moe_b_router"])], axis=-1)
    m["br_bc"] = np.ascontiguousarray(np.broadcast_to(brr[:, None, :], (DEPTH, 128, 36))).astype(np.float32)
    sel = np.zeros((32, NE, 128), np.float32)
    for e in range(NE):
        sel[e, e, :] = 1.0
    m["sel"] = sel.astype(ml_dtypes.bfloat16)
    m["attn_w_q"] = f(inp["attn_w_q"]); m["attn_w_kv"] = f(inp["attn_w_kv"]); m["attn_w_o"] = f(inp["attn_w_o"])
    m["qkgain_pc"] = np.ascontiguousarray(np.stack([np.asarray(inp["attn_q_gain"], np.float32),
                                                    np.asarray(inp["attn_k_gain"], np.float32)], axis=-1))
    m["conv_w_pw1"] = f(inp["conv_w_pw1"]); m["conv_w_pw2"] = f(inp["conv_w_pw2"])
    m["conv_b_pw1_pc"] = _vec_pc(inp["conv_b_pw1"][0])[None]
    m["conv_w_dw_pc"] = np.ascontiguousarray(np.asarray(inp["conv_w_dw"][0], np.float32).reshape(31, DC, 128).transpose(2, 1, 0))[None]
    for nm in ("conv_b_dw", "conv_ln_g", "conv_ln_b", "conv_b_pw2"):
        m[nm + "_pc"] = _vec_pc(inp[nm][0])[None]
    m["hy_w_in"] = f(inp["hy_w_in"]); m["hy_w_out"] = f(inp["hy_w_out"])
    m["hy_b_in_pc"] = _vec_pc(inp["hy_b_in"][0]); m["hy_b_short_pc"] = _vec_pc(inp["hy_b_short"][0])
    m["hy_w_short_pc"] = np.ascontiguousarray(np.stack([_vec_pc(inp["hy_w_short"][0, k]) for k in range(3)], axis=-1))
    m["hy_b_out_pc"] = _vec_pc(inp["hy_b_out"][0])
    m["hy_lbias_pc"] = np.stack([_vec_pc(inp["hy_long_bias"][0, o]) for o in range(2)])
    m["hy_f_w1"] = f(inp["hy_f_w1"]); m["hy_f_w2"] = f(inp["hy_f_w2"]); m["hy_f_w3"] = f(inp["hy_f_w3"])
    m["hy_fvec"] = np.ascontiguousarray(np.stack([np.asarray(inp[k][0], np.float32) for k in
                                                  ("hy_f_b1", "hy_f_freq1", "hy_f_b2", "hy_f_freq2")], axis=-1))
    dl = np.abs(np.linspace(math.log(1e-2) / 1.5, math.log(1e-2) / 0.3, D, dtype=np.float32))
    m["hy_dabs"] = np.ascontiguousarray(np.broadcast_to(dl[None, :], (64, D))).astype(np.float32)
    for tag, L_ in (("lat", SEQ), ("ctx", CTX)):
        hc = _HYC[tag] if tag in _HYC else _HYC.setdefault(tag, hy_consts(L_))
        m["hy_F1_" + tag] = hc["F1"]; m["hy_G_" + tag] = hc["G"]; m["hy_Fi_" + tag] = hc["Fi"]
        m["hy_lneg_" + tag] = hc["lneg"]; m["hy_featsT_" + tag] = hc["featsT"]
    hdc = _HYC["hyd"] if "hyd" in _HYC else _HYC.setdefault("hyd", hyd_consts())
    m["hyd_F"] = hdc["F"]; m["hyd_Fi"] = hdc["Fi"]; m["hyd_lneg"] = hdc["lneg"]
    RT = np.zeros((128, 128), np.float32)
    for base in (0, 64):
        for dd in range(32):
            RT[base + dd + 32, base + dd] = -1.0
            RT[base + dd, base + dd + 32] = 1.0
    m["ropeR"] = RT.astype(ml_dtypes.bfloat16)
    inv = (np.float32(10000.0) ** (-np.arange(32, dtype=np.float32) / np.float32(32))).astype(np.float32)
    tt = np.arange(SEQ)
    ang = np.zeros((128, SEQ), np.float32)
    for dd in range(128):
        pos = (tt // 64) if dd < 64 else (tt % 64)
        ang[dd] = pos.astype(np.float32) * inv[dd % 32]
    m["ropeC"] = np.cos(ang).astype(np.float32)
    m["ropeS"] = np.sin(ang).astype(np.float32)
    if sparse:
        wg = np.asarray(inp["moe_w_gate"], np.float32).reshape(DEPTH, NE, DC, 128, DEXP)
        wu = np.asarray(inp["moe_w_up"], np.float32).reshape(DEPTH, NE, DC, 128, DEXP)
        wd = np.asarray(inp["moe_w_down"], np.float32).reshape(DEPTH, NE, 2, 128, D)
        for li in range(DEPTH):
            m["moe_wgu%d" % li] = np.ascontiguousarray(np.concatenate([wg[li], wu[li]], axis=-1).transpose(0, 2, 1, 3)).reshape(NE * 128, DC * 512)
            m["moe_wdn%d" % li] = np.ascontiguousarray(wd[li].transpose(0, 2, 1, 3)).reshape(NE * 128, 2 * D)
        m["jgrid"] = np.ascontiguousarray(np.broadcast_to((np.arange(NBLK, dtype=np.float32) * BSL)[None, :], (128, NBLK)))
        m["pidx"] = np.arange(128, dtype=np.float32)[:, None].copy()
    else:
        m["moe_w_gate"] = f(inp["moe_w_gate"])
        m["moe_w_up"] = f(inp["moe_w_up"])
        m["moe_w_down"] = f(inp["moe_w_down"])
    return m


def _attn_methods():
    def load_w(self, name, src_ap, kc, m, q="pool"):
        mk = self.mk
        w = mk.sbuf(name, [128, kc, m], BF16)
        step = max(1, 4096 // m)
        for k0 in range(0, kc, step):
            k1 = min(kc, k0 + step)
            mk.dma(q, w[:, k0:k1, :], src_ap[k0 * 128:k1 * 128, :].rearrange("(k p) m -> p k m", p=128), writes=[w])
        return w

    def linear_resid(self, XT, srcT, W, KC, gname, bias=None, skip_ctx=False):
        mk = self.mk
        m0 = mk.mark()
        sb = [mk.sbuf("lr_s%d" % k, [128, KC, 512], BF16) for k in range(2)]
        xr = [mk.sbuf("lr_x%d" % k, [128, 512], F32) for k in range(3)]
        gb = None
        if bias is not None:
            gb = [mk.sbuf("lr_gb%d" % r, [128, DC], F32) for r in range(2)]
            for r in range(2):
                mk.op("dve", lambda e, r=r: e.tensor_tensor(out=gb[r][:], in0=bias[:], in1=self.mv[(gname, r)][:],
                                                            op=ALU.mult), reads=[bias, self.mv[(gname, r)]], writes=[gb[r]])
        n = 0
        for bi, (t0, tn) in enumerate(BLOCKS):
            if skip_ctx and bi == 0:
                continue
            r = 1 if bi == 0 else 0
            g = self.mv[(gname, r)]
            s = sb[bi % 2]
            mk.dma("sp", s[:, :, 0:tn], srcT[:, t0:t0 + tn].rearrange("(k p) t -> p k t", p=128),
                   reads=[srcT], writes=[s])
            for dc in range(DC):
                ps = self.ps[n % 4]
                x = xr[n % 3]
                n += 1
                mk.dma("sp", x[:, 0:tn], XT[dc * 128:(dc + 1) * 128, t0:t0 + tn], reads=[self.XTr[dc]], writes=[x])
                for k in range(KC):
                    mk.op("pe", lambda e, ps=ps, k=k, dc=dc, s=s: e.matmul(
                        ps[:, 0:tn], W[:, k, dc * 128:(dc + 1) * 128], s[:, k, 0:tn],
                        start=(k == 0), stop=(k == KC - 1)), reads=[W, s], writes=[ps])
                mk.op("dve", lambda e, ps=ps, dc=dc, x=x, g=g: e.scalar_tensor_tensor(
                    out=x[:, 0:tn], in0=ps[:, 0:tn], scalar=g[:, dc:dc + 1], in1=x[:, 0:tn],
                    op0=ALU.mult, op1=ALU.add), reads=[ps, g, x], writes=[x])
                if gb is not None:
                    mk.op("dve", lambda e, dc=dc, x=x, r=r: e.tensor_scalar(
                        out=x[:, 0:tn], in0=x[:, 0:tn], scalar1=gb[r][:, dc:dc + 1], scalar2=None, op0=ALU.add),
                        reads=[x, gb[r]], writes=[x])
                mk.dma("act", XT[dc * 128:(dc + 1) * 128, t0:t0 + tn], x[:, 0:tn], reads=[x], writes=[self.XTr[dc]])
        mk.release(m0)

    def stage_attn(self, slot, XT, HT, QT, OT, I, with_ctx):
        mk = self.mk
        mA = mk.mark()
        KT = mk.sbuf("KT", [128, 2, NT], BF16)
        Vs = mk.sbuf("Vs", [128, NT // 128, 256], BF16)
        gains = mk.sbuf("qkgain", [128, 2], F32)
        RT = mk.sbuf("RT", [128, 128], BF16)
        avgh = mk.sbuf("avgh", [128, 128], BF16)
        mk.dma("sp", gains[:], I["qkgain_pc"][slot, :, :], writes=[gains])
        mk.dma("sp", RT[:], I["ropeR"][:, :], writes=[RT])
        mk.op("pool", lambda e: e.memset(avgh[:], 1.0 / 128), writes=[avgh])
        m0 = mk.mark()
        wq = self.load_w("wq", I["attn_w_q"][slot], DC, D)
        wkv = self.load_w("wkv", I["attn_w_kv"][slot], DC, 512)
        hb = [mk.sbuf("ah%d" % k, [128, DC, 512], BF16) for k in range(2)]
        cs = [mk.sbuf("acs%d" % k, [128, 2, 512], F32) for k in range(2)]
        sqq = [mk.sbuf("asq%d" % k, [128, 512], BF16) for k in range(2)]
        rs = [mk.sbuf("ars%d" % k, [128, 512], F32) for k in range(2)]
        qn = [mk.sbuf("aqn%d" % k, [128, 512], F32) for k in range(2)]
        qb = [mk.sbuf("aqb%d" % k, [128, 512], BF16) for k in range(2)]
        t1 = [mk.sbuf("at1%d" % k, [128, 512], F32) for k in range(2)]
        t2 = [mk.sbuf("at2%d" % k, [128, 512], F32) for k in range(2)]
        qo = [mk.sbuf("aqo%d" % k, [128, 512], BF16) for k in range(3)]
        n = 0
        for bi, (t0, tn) in enumerate(BLOCKS):
            h = hb[bi % 2]
            mk.dma("sp", h[:, :, 0:tn], HT[:, t0:t0 + tn].rearrange("(k p) t -> p k t", p=128), reads=[HT], writes=[h])
            c = cs[bi % 2]
            if bi > 0:
                mk.dma("sp", c[:, 0, 0:tn], I["ropeC"][:, t0 - CTX:t0 - CTX + tn], writes=[c])
                mk.dma("sp", c[:, 1, 0:tn], I["ropeS"][:, t0 - CTX:t0 - CTX + tn], writes=[c])
            for hh in range(10):
                isq = hh < 8
                if isq and bi == 0 and not with_ctx:
                    continue
                W = wq if isq else wkv
                c0 = hh * 128 if isq else (hh - 8) * 128
                gcol = 0 if isq else 1
                ps = self.ps[n % 2]
                pz = self.ps[2 + n % 2]
                pr = self.ps[4 + n % 2]
                i2 = n % 2
                n += 1
                for k in range(DC):
                    mk.op("pe", lambda e, ps=ps, k=k, W=W, c0=c0, h=h: e.matmul(
                        ps[:, 0:tn], W[:, k, c0:c0 + 128], h[:, k, 0:tn], start=(k == 0), stop=(k == DC - 1)),
                        reads=[W, h], writes=[ps])
                mk.op("act", lambda e, ps=ps, i2=i2: e.activation(out=sqq[i2][:, 0:tn], in_=ps[:, 0:tn], func=AF.Square),
                      reads=[ps], writes=[sqq[i2]])
                mk.op("pe", lambda e, pz=pz, i2=i2: e.matmul(pz[:, 0:tn], avgh[:], sqq[i2][:, 0:tn], start=True, stop=True),
                      reads=[avgh, sqq[i2]], writes=[pz])
                mk.op("act", lambda e, pz=pz, i2=i2: e.activation(out=rs[i2][:, 0:tn], in_=pz[:, 0:tn], func=AF.Sqrt,
                                                                 bias=self.eps_t[:, 0:1]), reads=[pz, self.eps_t], writes=[rs[i2]])
                mk.op("dve", lambda e, i2=i2: e.reciprocal(rs[i2][:, 0:tn], rs[i2][:, 0:tn]), reads=[rs[i2]], writes=[rs[i2]])
                mk.op("dve", lambda e, ps=ps, i2=i2, gcol=gcol: e.scalar_tensor_tensor(
                    out=qn[i2][:, 0:tn], in0=ps[:, 0:tn], scalar=gains[:, gcol:gcol + 1], in1=rs[i2][:, 0:tn],
                    op0=ALU.mult, op1=ALU.mult), reads=[ps, gains, rs[i2]], writes=[qn[i2]])
                if isq:
                    dst_t = qo[n % 3]
                    dst = dst_t[:, 0:tn]
                else:
                    dst_t = KT
                    dst = KT[:, hh - 8, t0:t0 + tn]
                if bi == 0:
                    mk.op("act", lambda e, i2=i2, dst=dst: e.copy(dst, qn[i2][:, 0:tn]), reads=[qn[i2]], writes=[dst_t])
                else:
                    mk.op("act", lambda e, i2=i2: e.copy(qb[i2][:, 0:tn], qn[i2][:, 0:tn]), reads=[qn[i2]], writes=[qb[i2]])
                    mk.op("pe", lambda e, pr=pr, i2=i2: e.matmul(pr[:, 0:tn], RT[:], qb[i2][:, 0:tn], start=True, stop=True),
                          reads=[RT, qb[i2]], writes=[pr])
                    mk.op("pool", lambda e, i2=i2, c=c: e.tensor_tensor(out=t1[i2][:, 0:tn], in0=qn[i2][:, 0:tn],
                                                                       in1=c[:, 0, 0:tn], op=ALU.mult),
                          reads=[qn[i2], c], writes=[t1[i2]])
                    mk.op("dve", lambda e, pr=pr, i2=i2, c=c: e.tensor_tensor(out=t2[i2][:, 0:tn], in0=pr[:, 0:tn],
                                                                             in1=c[:, 1, 0:tn], op=ALU.mult),
                          reads=[pr, c], writes=[t2[i2]])
                    mk.op("pool", lambda e, i2=i2, dst=dst: e.tensor_tensor(out=dst, in0=t1[i2][:, 0:tn],
                                                                           in1=t2[i2][:, 0:tn], op=ALU.add),
                          reads=[t1[i2], t2[i2]], writes=[dst_t])
                if isq:
                    mk.dma("act", QT[hh * 128:(hh + 1) * 128, t0:t0 + tn], dst, reads=[dst_t], writes=[QT])
            for a in range(tn // 128):
                pv = self.ps[6 + a % 2]
                for k in range(DC):
                    mk.op("pe", lambda e, pv=pv, k=k, a=a, h=h: e.matmul(
                        pv[:, 0:256], h[:, k, a * 128:(a + 1) * 128], wkv[:, k, 256:512],
                        start=(k == 0), stop=(k == DC - 1)), reads=[h, wkv], writes=[pv])
                ti = t0 // 128 + a
                mk.op("act", lambda e, pv=pv, ti=ti: e.copy(Vs[:, ti, :], pv[:, 0:256]), reads=[pv], writes=[Vs])
        mk.release(m0)
        m0 = mk.mark()
        qblk = [mk.sbuf("bq%d" % k, [128, 512], BF16) for k in range(2)]
        pT = [mk.sbuf("bp%d" % k, [128, 512], BF16) for k in range(4)]
        rz = [mk.sbuf("brz%d" % k, [128, 512], F32) for k in range(2)]
        zacc = [mk.sbuf("bza%d" % k, [128, 512], F32) for k in range(2)]
        ones_f = mk.sbuf("bones_f", [128, 128], F32)
        mk.op("pool", lambda e: e.memset(ones_f[:], 1.0), writes=[ones_f])
        ob = [mk.sbuf("bo%d" % k, [128, 512], BF16) for k in range(2)]
        scale = 128 ** -0.5
        nb = 0
        nk = 0
        for head in range(8):
            kvh = head // 4
            for bi, (t0, tn) in enumerate(BLOCKS):
                if bi == 0 and not with_ctx:
                    continue
                nkc = 2 if bi == 0 else NT // 128
                q = qblk[nb % 2]
                mk.dma("sp", q[:, 0:tn], QT[head * 128:(head + 1) * 128, t0:t0 + tn], reads=[QT], writes=[q])
                pO = self.ps[4 + nb % 2]
                pZ = self.ps[6 + nb % 2]
                def issue_S(kc_, idx):
                    pS_ = self.ps[idx % 4]
                    mk.op("pe", lambda e, pS_=pS_, kc_=kc_, q=q: e.matmul(
                        pS_[:, 0:tn], KT[:, kvh, kc_ * 128:(kc_ + 1) * 128], q[:, 0:tn], start=True, stop=True),
                        reads=[KT, q], writes=[pS_])
                issue_S(0, nk)
                if nkc > 1:
                    issue_S(1, nk + 1)
                for kc in range(nkc):
                    pS = self.ps[nk % 4]
                    p_ = pT[nk % 4]
                    if kc + 2 < nkc:
                        issue_S(kc + 2, nk + 2)
                    nk += 1
                    mk.op("act", lambda e, pS=pS, p_=p_: e.activation(out=p_[:, 0:tn], in_=pS[:, 0:tn], func=AF.Exp,
                                                                     scale=scale), reads=[pS], writes=[p_])
                    mk.op("pe", lambda e, pO=pO, kc=kc, p_=p_: e.matmul(
                        pO[:, 0:tn], Vs[:, kc, kvh * 128:(kvh + 1) * 128], p_[:, 0:tn],
                        start=(kc == 0), stop=(kc == nkc - 1)), reads=[Vs, p_], writes=[pO])
                    mk.op("pe", lambda e, pZ=pZ, kc=kc, p_=p_: e.matmul(
                        pZ[:, 0:tn], self.ones_b[:], p_[:, 0:tn], start=(kc == 0), stop=(kc == nkc - 1)),
                        reads=[self.ones_b, p_], writes=[pZ])
                r_ = rz[nb % 2]
                o_ = ob[nb % 2]
                mk.op("dve", lambda e, pZ=pZ, r_=r_: e.reciprocal(r_[:, 0:tn], pZ[:, 0:tn]), reads=[pZ], writes=[r_])
                mk.op("dve", lambda e, pO=pO, r_=r_, o_=o_: e.tensor_tensor(out=o_[:, 0:tn], in0=pO[:, 0:tn],
                                                                           in1=r_[:, 0:tn], op=ALU.mult),
                      reads=[pO, r_], writes=[o_])
                mk.dma("act", OT[head * 128:(head + 1) * 128, t0:t0 + tn], o_[:, 0:tn], reads=[o_], writes=[OT])
                nb += 1
        mk.release(m0)
        mk.release(mA)
        m0 = mk.mark()
        wo = self.load_w("wo", I["attn_w_o"][slot], DC, D)
        self.linear_resid(XT, OT, wo, DC, "g1", skip_ctx=not with_ctx)
        mk.release(m0)

    Prog.load_w = load_w
    Prog.linear_resid = linear_resid
    Prog.stage_attn = stage_attn


_attn_methods()


def _conf_methods():
    def stage_conf(self, slot, XT, HT, UT, VT, I):
        mk = self.mk
        m0 = mk.mark()
        W = self.load_w("wpw1", I["conv_w_pw1"][slot], DC, 2 * D)
        b1 = mk.sbuf("cb1", [128, 16], F32)
        mk.dma("sp", b1[:], I["conv_b_pw1_pc"][slot, :, :], writes=[b1])
        hb = [mk.sbuf("ch%d" % k, [128, DC, 512], BF16) for k in range(2)]
        sg = [mk.sbuf("csg%d" % k, [128, 512], F32) for k in range(2)]
        ub = [mk.sbuf("cu%d" % k, [128, DC, 512], BF16) for k in range(2)]
        n = 0
        for bi, (t0, tn) in enumerate(BLOCKS):
            h = hb[bi % 2]
            u = ub[bi % 2]
            mk.dma("sp", h[:, :, 0:tn], HT[:, t0:t0 + tn].rearrange("(k p) t -> p k t", p=128), reads=[HT], writes=[h])
            for j in range(DC):
                pa, pg = self.ps[(2 * n) % 8], self.ps[(2 * n + 1) % 8]
                s_ = sg[n % 2]
                n += 1
                for k in range(DC):
                    mk.op("pe", lambda e, pa=pa, k=k, j=j, h=h: e.matmul(
                        pa[:, 0:tn], W[:, k, j * 128:(j + 1) * 128], h[:, k, 0:tn], start=(k == 0), stop=(k == DC - 1)),
                        reads=[W, h], writes=[pa])
                for k in range(DC):
                    mk.op("pe", lambda e, pg=pg, k=k, j=j, h=h: e.matmul(
                        pg[:, 0:tn], W[:, k, D + j * 128:D + (j + 1) * 128], h[:, k, 0:tn], start=(k == 0), stop=(k == DC - 1)),
                        reads=[W, h], writes=[pg])
                mk.op("act", lambda e, pg=pg, s_=s_, j=j: e.activation(out=s_[:, 0:tn], in_=pg[:, 0:tn], func=AF.Sigmoid,
                                                                      bias=b1[:, 8 + j:9 + j]), reads=[pg, b1], writes=[s_])
                mk.op("dve", lambda e, pa=pa, s_=s_, j=j, u=u: e.scalar_tensor_tensor(
                    out=u[:, j, 0:tn], in0=pa[:, 0:tn], scalar=b1[:, j:j + 1], in1=s_[:, 0:tn], op0=ALU.add, op1=ALU.mult),
                    reads=[pa, b1, s_], writes=[u])
            mk.dma("act", UT[:, t0:t0 + tn].rearrange("(k p) t -> p k t", p=128), u[:, :, 0:tn], reads=[u], writes=[UT])
        mk.release(m0)
        m0 = mk.mark()
        wdw = mk.sbuf("cwdw", [128, DC, 31], F32)
        bdw = mk.sbuf("cbdw", [128, DC], F32)
        mk.dma("sp", wdw[:], I["conv_w_dw_pc"][slot, :, :, :], writes=[wdw])
        mk.dma("sp", bdw[:], I["conv_b_dw_pc"][slot, :, :], writes=[bdw])
        up = [mk.sbuf("cup%d" % k, [128, NT + 60], BF16) for k in range(2)]
        dg = [mk.sbuf("cdg%d" % k, [128, 31, 128], BF16) for k in range(2)]
        vo = [mk.sbuf("cvo%d" % k, [128, 512], F32) for k in range(3)]
        for k in range(2):
            mk.op("pool", lambda e, k=k: e.memset(up[k][:], 0.0), writes=[up[k]])
        n = 0
        for j in range(DC):
            u = up[j % 2]
            d_ = dg[j % 2]
            mk.dma("sp", u[:, 15:15 + CTX], UT[j * 128:(j + 1) * 128, 0:CTX], reads=[UT], writes=[u])
            mk.dma("sp", u[:, 45 + CTX:45 + CTX + SEQ], UT[j * 128:(j + 1) * 128, CTX:NT], reads=[UT], writes=[u])
            for k in range(31):
                mk.op("dve", lambda e, k=k, j=j, d_=d_: e.tensor_scalar(
                    out=d_[:, k, :], in0=self.ident_b[:], scalar1=wdw[:, j, k:k + 1], scalar2=None, op0=ALU.mult),
                    reads=[self.ident_b, wdw], writes=[d_])
            for bi, (t0, tn) in enumerate(BLOCKS):
                base = 0 if bi == 0 else 30
                ps = self.ps[n % 4]
                v = vo[n % 3]
                n += 1
                for k in range(31):
                    mk.op("pe", lambda e, ps=ps, k=k, u=u, d_=d_, s0=base + t0 + k: e.matmul(
                        ps[:, 0:tn], d_[:, k, :], u[:, s0:s0 + tn], start=(k == 0), stop=(k == 30)),
                        reads=[d_, u], writes=[ps])
                mk.op("act", lambda e, ps=ps, v=v, j=j: e.activation(out=v[:, 0:tn], in_=ps[:, 0:tn], func=AF.Identity,
                                                                    bias=bdw[:, j:j + 1]), reads=[ps, bdw], writes=[v])
                mk.dma("act", VT[j * 128:(j + 1) * 128, t0:t0 + tn], v[:, 0:tn], reads=[v], writes=[VT])
        mk.release(m0)
        m0 = mk.mark()
        lng = mk.sbuf("clng", [128, DC], F32)
        lnb = mk.sbuf("clnb", [128, DC], F32)
        mk.dma("sp", lng[:], I["conv_ln_g_pc"][slot, :, :], writes=[lng])
        mk.dma("sp", lnb[:], I["conv_ln_b_pc"][slot, :, :], writes=[lnb])
        avgf = mk.sbuf("cavgf", [128, 128], F32)
        mk.op("pool", lambda e: e.memset(avgf[:], 1.0 / D), writes=[avgf])
        vb = [mk.sbuf("cv%d" % k, [128, DC, 512], F32) for k in range(2)]
        sq = mk.sbuf("csq", [128, DC, 512], F32)
        mu = mk.sbuf("cmu", [128, 512], F32)
        var = mk.sbuf("cvar", [128, 512], F32)
        ob = [mk.sbuf("co%d" % k, [128, DC, 512], BF16) for k in range(2)]
        for bi, (t0, tn) in enumerate(BLOCKS):
            v = vb[bi % 2]
            o = ob[bi % 2]
            mk.dma("sp", v[:, :, 0:tn], VT[:, t0:t0 + tn].rearrange("(k p) t -> p k t", p=128), reads=[VT], writes=[v])
            pm, pq = self.ps[0], self.ps[1]
            for j in range(DC):
                mk.op("act", lambda e, j=j, v=v: e.activation(out=sq[:, j, 0:tn], in_=v[:, j, 0:tn], func=AF.Square),
                      reads=[v], writes=[sq])
            for j in range(DC):
                mk.op("pe", lambda e, j=j, v=v: e.matmul(pm[:, 0:tn], avgf[:], v[:, j, 0:tn], start=(j == 0), stop=(j == DC - 1)),
                      reads=[avgf, v], writes=[pm])
            for j in range(DC):
                mk.op("pe", lambda e, j=j: e.matmul(pq[:, 0:tn], avgf[:], sq[:, j, 0:tn], start=(j == 0), stop=(j == DC - 1)),
                      reads=[avgf, sq], writes=[pq])
            mk.op("act", lambda e: e.copy(mu[:, 0:tn], pm[:, 0:tn]), reads=[pm], writes=[mu])
            mk.op("dve", lambda e: e.tensor_tensor(out=var[:, 0:tn], in0=mu[:, 0:tn], in1=mu[:, 0:tn], op=ALU.mult),
                  reads=[mu], writes=[var])
            mk.op("dve", lambda e: e.tensor_tensor(out=var[:, 0:tn], in0=pq[:, 0:tn], in1=var[:, 0:tn], op=ALU.subtract),
                  reads=[pq, var], writes=[var])
            mk.op("act", lambda e: e.activation(out=var[:, 0:tn], in_=var[:, 0:tn], func=AF.Sqrt, bias=self.eps_t[:, 0:1]),
                  reads=[var, self.eps_t], writes=[var])
            mk.op("dve", lambda e: e.reciprocal(var[:, 0:tn], var[:, 0:tn]), reads=[var], writes=[var])
            for j in range(DC):
                mk.op("pool", lambda e, j=j, v=v: e.tensor_tensor(out=v[:, j, 0:tn], in0=v[:, j, 0:tn], in1=mu[:, 0:tn],
                                                                 op=ALU.subtract), reads=[v, mu], writes=[v])
                mk.op("dve", lambda e, j=j, v=v: e.scalar_tensor_tensor(
                    out=v[:, j, 0:tn], in0=v[:, j, 0:tn], scalar=lng[:, j:j + 1], in1=var[:, 0:tn], op0=ALU.mult, op1=ALU.mult),
                    reads=[v, lng, var], writes=[v])
                mk.op("act", lambda e, j=j, v=v, o=o: e.activation(out=o[:, j, 0:tn], in_=v[:, j, 0:tn], func=AF.Silu,
                                                                  bias=lnb[:, j:j + 1]), reads=[v, lnb], writes=[o])
            mk.dma("act", HT[:, t0:t0 + tn].rearrange("(k p) t -> p k t", p=128), o[:, :, 0:tn], reads=[o], writes=[HT])
        mk.release(m0)
        m0 = mk.mark()
        w2 = self.load_w("wpw2", I["conv_w_pw2"][slot], DC, D)
        b2 = mk.sbuf("cb2", [128, DC], F32)
        mk.dma("sp", b2[:], I["conv_b_pw2_pc"][slot, :, :], writes=[b2])
        self.linear_resid(XT, HT, w2, DC, "g1", bias=b2)
        mk.release(m0)

    Prog.stage_conf = stage_conf


_conf_methods()


def hy_consts(L):
    N = 2 * L
    N1 = N // 64
    H = N1 // 2
    bf = ml_dtypes.bfloat16
    c = {}
    n1 = np.arange(H)[:, None].astype(np.float64)
    k1 = np.arange(N1)[None, :].astype(np.float64)
    def cs(ang):
        return np.cos(ang), -np.sin(ang)
    fc, fs = cs(2 * np.pi * n1 * k1 / N1)
    bc, bs = cs(2 * np.pi * (N1 - 1 - n1) * k1 / N1)
    b0c, b0s = cs(2 * np.pi * (N1 - n1) * k1 / N1)
    c["F1"] = np.stack([fc, fs, bc, bs, b0c, b0s], 1).astype(bf)
    n2 = np.arange(64)[:, None].astype(np.float64)
    k2 = np.arange(64)[None, :].astype(np.float64)
    G = np.zeros((N1, 128, 5, 128), np.float64)
    for kk in range(N1):
        ang = 2 * np.pi * (n2 * kk / N + n2 * k2 / 64)
        Gr, Gi = np.cos(ang), -np.sin(ang)
        Mr, Mi = Gr.T, -Gi.T
        blocks = [((Gr, Gi), (-Gi, Gr)), ((-Gi, Gr), (-Gr, -Gi)), ((Gr, Gr), (-Gi, -Gi)),
                  ((Gi, Gi), (Gr, Gr)), ((Mr, Mi), (-Mi, Mr))]
        for f, ((a, b), (cc, d)) in enumerate(blocks):
            G[kk, 0:64, f, 0:64] = a
            G[kk, 0:64, f, 64:128] = b
            G[kk, 64:128, f, 0:64] = cc
            G[kk, 64:128, f, 64:128] = d
    c["G"] = np.ascontiguousarray(G.transpose(1, 0, 2, 3)).astype(bf)
    t1 = np.arange(H)[None, :].astype(np.float64)
    kk1 = np.arange(N1)[:, None].astype(np.float64)
    th = 2 * np.pi * t1 * kk1 / N1
    c["Fi"] = np.stack([np.cos(th) / N, -np.sin(th) / N], 1).astype(bf)
    pos = (64 * np.arange(H)[:, None] + np.arange(64)[None, :]).astype(np.float32)
    c["lneg"] = (-(pos / np.float32(L - 1))).astype(np.float32)
    p = np.arange(L, dtype=np.float32)[:, None]
    t = p / np.float32(L - 1)
    w = np.float32(2.0 * math.pi) * p / np.float32(L)
    bands = np.linspace(1e-4, 7, 8, dtype=np.float32)
    feats = np.concatenate([t, np.cos(bands * w), -np.sin(bands * w)], axis=-1).astype(np.float32)
    c["featsT"] = np.ascontiguousarray(feats.T)
    return c


def _hy_methods():
    def hy_filter(self, S, I, HH):
        mk = self.mk
        L, N1, H, tag = S["L"], S["N1"], S["H"], S["tag"]
        m0 = mk.mark()
        ft = mk.sbuf("hf_ft", [17, L], F32)
        W1 = mk.sbuf("hf_w1", [17, 64], F32)
        W2 = mk.sbuf("hf_w2", [64, 64], F32)
        W3 = mk.sbuf("hf_w3", [64, 4096], BF16)
        fv = mk.sbuf("hf_fv", [64, 4], F32)
        fb = mk.sbuf("hf_fb", [64, 2], F32)
        z1 = mk.sbuf("hf_z1", [64, L], F32)
        z2 = mk.sbuf("hf_z2", [64, L], F32)
        z2b = mk.sbuf("hf_z2b", [64, L], BF16)
        tmp = mk.sbuf("hf_tmp", [64, 512], F32)
        F1 = mk.sbuf("hf_F1", [max(H, 1), 6, N1], BF16)
        lneg = mk.sbuf("hf_lneg", [H, 64], F32)
        dab = mk.sbuf("hf_dab", [H, D], F32)
        mk.dma("sp", ft[:], I["hy_featsT_" + tag][:, :], writes=[ft])
        mk.dma("sp", W1[:], I["hy_f_w1"][0, :, :], writes=[W1])
        mk.dma("sp", W2[:], I["hy_f_w2"][0, :, :], writes=[W2])
        mk.dma("pool", W3[:], I["hy_f_w3"][0, :, :], writes=[W3])
        mk.dma("sp", fv[:], I["hy_fvec"][:, :], writes=[fv])
        mk.dma("sp", F1[:], I["hy_F1_" + tag][:, :, :], writes=[F1])
        mk.dma("sp", lneg[:], I["hy_lneg_" + tag][:, :], writes=[lneg])
        mk.dma("sp", dab[:], I["hy_dabs"][0:H, :], writes=[dab])
        mk.op("dve", lambda e: e.tensor_tensor(out=fb[:, 0:1], in0=fv[:, 0:1], in1=fv[:, 1:2], op=ALU.mult), reads=[fv], writes=[fb])
        mk.op("dve", lambda e: e.tensor_tensor(out=fb[:, 1:2], in0=fv[:, 2:3], in1=fv[:, 3:4], op=ALU.mult), reads=[fv], writes=[fb])
        for layer, (Wm, src, dst, kin) in enumerate(((W1, ft, z1, 17), (W2, z1, z2, 64))):
            for c0 in range(0, L, 512):
                cn = min(512, L - c0)
                ps = self.ps[(c0 // 512) % 2]
                mk.op("pe", lambda e, ps=ps, Wm=Wm, src=src, c0=c0, cn=cn, kin=kin: e.matmul(
                    ps[0:64, 0:cn], Wm[0:kin, :], src[0:kin, c0:c0 + cn], start=True, stop=True), reads=[Wm, src], writes=[ps])
                d_ = dst[:, c0:c0 + cn]
                mk.op("dve", lambda e, ps=ps, d_=d_, cn=cn, layer=layer: e.tensor_scalar(
                    out=d_, in0=ps[0:64, 0:cn], scalar1=fv[:, 2 * layer + 1:2 * layer + 2], scalar2=fb[:, layer:layer + 1],
                    op0=ALU.mult, op1=ALU.add), reads=[ps, fv, fb], writes=[dst])
                for _ in range(2):
                    mk.op("dve", lambda e, d_=d_, cn=cn: e.tensor_scalar(out=tmp[:, 0:cn], in0=d_, scalar1=math.pi,
                          scalar2=2 * math.pi, op0=ALU.is_gt, op1=ALU.mult), reads=[dst], writes=[tmp])
                    mk.op("dve", lambda e, d_=d_, cn=cn: e.tensor_tensor(out=d_, in0=d_, in1=tmp[:, 0:cn], op=ALU.subtract),
                          reads=[dst, tmp], writes=[dst])
                    mk.op("dve", lambda e, d_=d_, cn=cn: e.tensor_scalar(out=tmp[:, 0:cn], in0=d_, scalar1=-math.pi,
                          scalar2=2 * math.pi, op0=ALU.is_lt, op1=ALU.mult), reads=[dst], writes=[tmp])
                    mk.op("dve", lambda e, d_=d_, cn=cn: e.tensor_tensor(out=d_, in0=d_, in1=tmp[:, 0:cn], op=ALU.add),
                          reads=[dst, tmp], writes=[dst])
                mk.op("act", lambda e, d_=d_: e.activation(out=d_, in_=d_, func=AF.Sin), reads=[dst], writes=[dst])
        mk.op("act", lambda e: e.copy(z2b[:], z2[:]), reads=[z2], writes=[z2b])
        dec = [mk.sbuf("hf_dec%d" % k, [H, D], F32) for k in range(4)]
        hd = [mk.sbuf("hf_hd%d" % k, [H, D], F32) for k in range(4)]
        hdb = [mk.sbuf("hf_hdb%d" % k, [H, D], BF16) for k in range(4)]
        ha = [mk.sbuf("hf_ha%d" % k, [H, D], BF16) for k in range(4)]
        Asb = [mk.sbuf("hf_A%d" % k, [N1, 2, D], BF16) for k in range(2)]
        rn = mk.sbuf("hf_rn", [128, D], F32)
        A = self.dram["hyA"]
        Bt = [mk.sbuf("hf_B%d" % k, [128, D], BF16) for k in range(2)]
        Gt = [mk.sbuf("hf_Gt%d" % k, [128, 2, 128], BF16) for k in range(2)]
        Ho = [mk.sbuf("hf_Ho%d" % k, [128, D], BF16) for k in range(4)]
        for o in range(2):
            pn = (self.ps[6], self.ps[7])
            units = [(n2_, dr_) for n2_ in range(64) for dr_ in range(2)]

            def issue_h(ui):
                n2_, dr_ = units[ui]
                col_ = n2_ if dr_ == 0 else (64 - n2_) % 64
                for hf in range(2):
                    ph = self.ps[(ui % 2) * 2 + hf]
                    w0 = (dr_ * 2 + o) * D + hf * 512
                    mk.op("pe", lambda e, ph=ph, col_=col_, w0=w0: e.matmul(
                        ph[0:H, :], z2b[:, col_:L:64], W3[:, w0:w0 + 512], start=True, stop=True),
                        reads=[z2b, W3], writes=[ph])
                i2_ = dr_ + 2 * (n2_ % 2)
                mk.op("act", lambda e, i2_=i2_, col_=col_: e.activation(out=dec[i2_][:], in_=dab[:], func=AF.Exp,
                                                                       scale=lneg[:, col_:col_ + 1]), reads=[dab, lneg], writes=[dec[i2_]])
            issue_h(0)
            for n2 in range(64):
                n2b = (64 - n2) % 64
                hb2 = []
                for dr in range(2):
                    ui = n2 * 2 + dr
                    col = n2 if dr == 0 else n2b
                    i2 = dr + 2 * (n2 % 2)
                    if ui + 1 < len(units):
                        issue_h(ui + 1)
                    for hf in range(2):
                        ph = self.ps[(ui % 2) * 2 + hf]
                        mk.op("dve", lambda e, ph=ph, i2=i2, hf=hf: e.tensor_tensor(
                            out=hd[i2][:, hf * 512:(hf + 1) * 512], in0=ph[0:H, :], in1=dec[i2][:, hf * 512:(hf + 1) * 512],
                            op=ALU.mult), reads=[ph, dec[i2]], writes=[hd[i2]])
                    if dr == 1 and n2 == 0:
                        mk.op("dve", lambda e, i2=i2: e.memset(hd[i2][0:1, :], 0.0), reads=[hd[i2]], writes=[hd[i2]])
                    hb_ = hdb[(n2 % 2) * 2 + dr]
                    hb2.append(hb_)
                    mk.op("act", lambda e, i2=i2, hb_=hb_: e.copy(hb_[:], hd[i2][:]), reads=[hd[i2]], writes=[hb_])
                    mk.op("dve", lambda e, i2=i2: e.scalar_tensor_tensor(out=ha[i2][:], in0=hd[i2][:], scalar=-1.0, in1=hd[i2][:],
                                                                        op0=ALU.mult, op1=ALU.max), reads=[hd[i2]], writes=[ha[i2]])
                    for hf in range(2):
                        first = (n2 == 0 and dr == 0)
                        last = (n2 == 63 and dr == 1)
                        mk.op("pe", lambda e, i2=i2, hf=hf, first=first, last=last: e.matmul(
                            pn[hf][:, :], self.ones_b[0:H, :], ha[i2][:, hf * 512:(hf + 1) * 512], start=first, stop=last),
                            reads=[self.ones_b, ha[i2]], writes=[pn[hf]])
                As = Asb[n2 % 2]
                fbi = 4 if n2 == 0 else 2
                for ri in range(2):
                    for hf in range(2):
                        pA = self.ps[4 + hf]
                        mk.op("pe", lambda e, pA=pA, ri=ri, hf=hf, hb2=hb2: e.matmul(
                            pA[0:N1, :], F1[0:H, ri, :], hb2[0][:, hf * 512:(hf + 1) * 512], start=True, stop=False),
                            reads=[F1, hb2[0]], writes=[pA])
                        mk.op("pe", lambda e, pA=pA, ri=ri, hf=hf, hb2=hb2, fbi=fbi: e.matmul(
                            pA[0:N1, :], F1[0:H, fbi + ri, :], hb2[1][:, hf * 512:(hf + 1) * 512], start=False, stop=True),
                            reads=[F1, hb2[1]], writes=[pA])
                        eng = "act" if hf else "dve"
                        if eng == "act":
                            mk.op("act", lambda e, pA=pA, ri=ri, hf=hf, As=As: e.copy(As[:, ri, hf * 512:(hf + 1) * 512], pA[0:N1, :]),
                                  reads=[pA], writes=[As])
                        else:
                            mk.op("dve", lambda e, pA=pA, ri=ri, hf=hf, As=As: e.tensor_copy(As[:, ri, hf * 512:(hf + 1) * 512], pA[0:N1, :]),
                                  reads=[pA], writes=[As])
                for ri in range(2):
                    mk.dma("act", A[ri, 0:N1, n2, :], As[:, ri, :], reads=[As], writes=[A])
            for hf in range(2):
                mk.op("dve", lambda e, hf=hf: e.reciprocal(rn[:, hf * 512:(hf + 1) * 512], pn[hf][:, :]), reads=[pn[hf]], writes=[rn])
            for kk in range(N1):
                B = Bt[kk % 2]
                for ri in range(2):
                    mk.dma("sp", B[ri * 64:(ri + 1) * 64, :], A[ri, kk, :, :], reads=[A], writes=[B])
                g_ = Gt[kk % 2]
                mk.dma("sp", g_[:], I["hy_G_" + tag][:, kk, 2:4, :], writes=[g_])
                for ri in range(2):
                    ho = Ho[(2 * kk + ri) % 4]
                    for hf in range(2):
                        pX = self.ps[(kk % 2) * 4 + ri * 2 + hf]
                        mk.op("pe", lambda e, pX=pX, ri=ri, hf=hf, B=B, g_=g_: e.matmul(
                            pX[:, :], g_[:, ri, :], B[:, hf * 512:(hf + 1) * 512], start=True, stop=True), reads=[g_, B], writes=[pX])
                        mk.op("dve", lambda e, pX=pX, hf=hf, ho=ho: e.tensor_tensor(
                            out=ho[:, hf * 512:(hf + 1) * 512], in0=pX[:, :], in1=rn[:, hf * 512:(hf + 1) * 512], op=ALU.mult),
                            reads=[pX, rn], writes=[ho])
                    mk.dma("act", HH[o][ri, kk, :, :], ho[:], reads=[ho], writes=[HH[o]])
        mk.release(m0)

    Prog.hy_filter = hy_filter


_hy_methods()


def _hy_methods2():
    def hy_conv(self, S, o, I, zsrc, zrow0, gate, grow0, lbias, HH, dst, dst_bf16):
        mk = self.mk
        L, N1, H, tag, col0 = S["L"], S["N1"], S["H"], S["tag"], S["col0"]
        A, Cs = self.dram["hyA"], self.dram["hyC"]
        m0 = mk.mark()
        zb = mk.sbuf("hc_zb", [128, DC, L], BF16)
        F1 = mk.sbuf("hc_F1", [max(H, 1), 6, N1], BF16)
        mk.dma("sp", F1[:], I["hy_F1_" + tag][:, :, :], writes=[F1])
        for j in range(DC):
            mk.dma("pool", zb[:, j, :], zsrc[zrow0 + j * 128:zrow0 + (j + 1) * 128, col0:col0 + L], reads=[zsrc], writes=[zb])
        zT = [mk.sbuf("hc_zT%d" % k, [H, D], BF16) for k in range(2)]
        Asb = [mk.sbuf("hc_A%d" % k, [N1, 2, D], BF16) for k in range(2)]
        for n2 in range(64):
            pt = self.ps[n2 % 2]
            ptb = pt[:, 0:512].bitcast(BF16)
            for j in range(DC):
                mk.op("pe", lambda e, ptb=ptb, j=j, n2=n2: e.transpose(ptb[0:H, j * 128:(j + 1) * 128], zb[:, j, n2:L:64],
                                                                      self.ident_b[:]), reads=[zb, self.ident_b], writes=[pt])
            z_ = zT[n2 % 2]
            mk.op("act", lambda e, ptb=ptb, z_=z_: e.copy(z_[:], ptb[0:H, :]), reads=[pt], writes=[z_])
            As = Asb[n2 % 2]
            for ri in range(2):
                for hf in range(2):
                    pA = self.ps[2 + ri * 2 + hf]
                    mk.op("pe", lambda e, pA=pA, ri=ri, hf=hf, z_=z_: e.matmul(
                        pA[0:N1, :], F1[0:H, ri, :], z_[:, hf * 512:(hf + 1) * 512], start=True, stop=True),
                        reads=[F1, z_], writes=[pA])
                    if hf:
                        mk.op("act", lambda e, pA=pA, ri=ri, hf=hf, As=As: e.copy(As[:, ri, hf * 512:(hf + 1) * 512], pA[0:N1, :]),
                              reads=[pA], writes=[As])
                    else:
                        mk.op("dve", lambda e, pA=pA, ri=ri, hf=hf, As=As: e.tensor_copy(As[:, ri, hf * 512:(hf + 1) * 512], pA[0:N1, :]),
                              reads=[pA], writes=[As])
            for ri in range(2):
                mk.dma("act", A[ri, 0:N1, n2, :], As[:, ri, :], reads=[As], writes=[A])
        mk.release(m0)
        m0 = mk.mark()
        Bt = [mk.sbuf("hc_B%d" % k, [128, D], BF16) for k in range(2)]
        Gt = [mk.sbuf("hc_G%d" % k, [128, 5, 128], BF16) for k in range(2)]
        Hr = [mk.sbuf("hc_Hr%d" % k, [128, D], BF16) for k in range(2)]
        Hi = [mk.sbuf("hc_Hi%d" % k, [128, D], BF16) for k in range(2)]
        ta = [mk.sbuf("hc_ta%d" % k, [128, D], F32) for k in range(2)]
        tb = [mk.sbuf("hc_tb%d" % k, [128, D], F32) for k in range(2)]
        Y = [mk.sbuf("hc_Y%d" % k, [128, D], BF16) for k in range(2)]
        Cb = [mk.sbuf("hc_C%d" % k, [128, D], BF16) for k in range(2)]
        def f2_load(kk):
            i2 = kk % 2
            B, g_, hr, hi = Bt[i2], Gt[i2], Hr[i2], Hi[i2]
            for ri in range(2):
                mk.dma("sp", B[ri * 64:(ri + 1) * 64, :], A[ri, kk, :, :], reads=[A], writes=[B])
            mk.dma("sp", g_[:], I["hy_G_" + tag][:, kk, :, :], writes=[g_])
            mk.dma("sp", hr[:], HH[o][0, kk, :, :], reads=[HH[o]], writes=[hr])
            mk.dma("sp", hi[:], HH[o][1, kk, :, :], reads=[HH[o]], writes=[hi])

        def f2_X(u):
            kk, hf = divmod(u, 2)
            i2 = kk % 2
            sl = slice(hf * 512, (hf + 1) * 512)
            pa, pb = self.ps[(u % 2) * 2], self.ps[(u % 2) * 2 + 1]
            mk.op("pe", lambda e: e.matmul(pa[:, :], Gt[i2][:, 0, :], Bt[i2][:, sl], start=True, stop=True),
                  reads=[Gt[i2], Bt[i2]], writes=[pa])
            mk.op("pe", lambda e: e.matmul(pb[:, :], Gt[i2][:, 1, :], Bt[i2][:, sl], start=True, stop=True),
                  reads=[Gt[i2], Bt[i2]], writes=[pb])
        f2_load(0)
        if N1 > 1:
            f2_load(1)
        f2_X(0)
        for u in range(2 * N1):
            kk, hf = divmod(u, 2)
            i2 = kk % 2
            hr, hi = Hr[i2], Hi[i2]
            sl = slice(hf * 512, (hf + 1) * 512)
            pa, pb = self.ps[(u % 2) * 2], self.ps[(u % 2) * 2 + 1]
            if u + 1 < 2 * N1:
                f2_X(u + 1)
            mk.op("dve", lambda e, pa=pa, sl=sl, hr=hr, i2=i2: e.tensor_tensor(out=ta[i2][:, sl], in0=pa[:, :], in1=hr[:, sl],
                                                                              op=ALU.mult), reads=[pa, hr], writes=[ta[i2]])
            mk.op("dve", lambda e, pb=pb, sl=sl, hi=hi, i2=i2: e.tensor_tensor(out=tb[i2][:, sl], in0=pb[:, :], in1=hi[:, sl],
                                                                              op=ALU.mult), reads=[pb, hi], writes=[tb[i2]])
            mk.op("pool", lambda e, sl=sl, i2=i2: e.tensor_tensor(out=Y[i2][:, sl], in0=ta[i2][:, sl], in1=tb[i2][:, sl],
                                                                 op=ALU.add), reads=[ta[i2], tb[i2]], writes=[Y[i2]])
            pc = self.ps[4 + u % 4]
            mk.op("pe", lambda e, pc=pc, sl=sl, i2=i2: e.matmul(pc[:, :], Gt[i2][:, 4, :], Y[i2][:, sl], start=True, stop=True),
                  reads=[Gt[i2], Y[i2]], writes=[pc])
            mk.op("act", lambda e, pc=pc, sl=sl, i2=i2: e.copy(Cb[i2][:, sl], pc[:, :]), reads=[pc], writes=[Cb[i2]])
            if hf == 1:
                for ri in range(2):
                    mk.dma("act", Cs[ri, :, kk, :], Cb[i2][ri * 64:(ri + 1) * 64, :], reads=[Cb[i2]], writes=[Cs])
                if kk + 2 < N1:
                    f2_load(kk + 2)
        mk.release(m0)
        m0 = mk.mark()
        Fi = mk.sbuf("hc_Fi", [N1, 2, max(H, 1)], BF16)
        mk.dma("sp", Fi[:], I["hy_Fi_" + tag][:, :, :], writes=[Fi])
        lb = mk.sbuf("hc_lb", [128, DC], F32)
        mk.dma("sp", lb[:], lbias, writes=[lb])
        yh = mk.sbuf("hc_yh", [128, 4, L], F32)
        Ct = [mk.sbuf("hc_Ct%d" % k, [N1, 2, 512], BF16) for k in range(3)]
        zr = [mk.sbuf("hc_zr%d" % k, [128, L], F32) for k in range(2)]
        gr = [mk.sbuf("hc_gr%d" % k, [128, L], F32) for k in range(2)]
        ob = [mk.sbuf("hc_ob%d" % k, [128, L], BF16 if dst_bf16 else F32) for k in range(2)]
        per_bank = max(1, 512 // (4 * H))
        for half in range(2):
            for t2 in range(64):
                c_ = Ct[t2 % 3]
                mk.dma("sp", c_[:], Cs[:, t2, 0:N1, half * 512:(half + 1) * 512].rearrange("r k c -> k r c"), reads=[Cs], writes=[c_])
                slot = t2 % per_bank
                py = self.ps[(t2 // per_bank) % 8]
                for c4 in range(4):
                    o0 = slot * 4 * H + c4 * H
                    mk.op("pe", lambda e, py=py, o0=o0, c4=c4, c_=c_: e.matmul(
                        py[:, o0:o0 + H], c_[:, 0, c4 * 128:(c4 + 1) * 128], Fi[:, 0, :], start=True, stop=False),
                        reads=[c_, Fi], writes=[py])
                    mk.op("pe", lambda e, py=py, o0=o0, c4=c4, c_=c_: e.matmul(
                        py[:, o0:o0 + H], c_[:, 1, c4 * 128:(c4 + 1) * 128], Fi[:, 1, :], start=False, stop=True),
                        reads=[c_, Fi], writes=[py])
                src = py[:, slot * 4 * H:(slot + 1) * 4 * H].rearrange("p (a b) -> p a b", b=H)
                if t2 % 2:
                    mk.op("act", lambda e, src=src, t2=t2: e.copy(yh[:, :, t2:L:64], src), reads=[py], writes=[yh])
                else:
                    mk.op("dve", lambda e, src=src, t2=t2: e.tensor_copy(yh[:, :, t2:L:64], src), reads=[py], writes=[yh])
            for c4 in range(4):
                cj = half * 4 + c4
                z_, g_, o_ = zr[c4 % 2], gr[c4 % 2], ob[c4 % 2]
                mk.dma("sp", z_[:], zsrc[zrow0 + cj * 128:zrow0 + (cj + 1) * 128, col0:col0 + L], reads=[zsrc], writes=[z_])
                mk.dma("sp", g_[:], gate[grow0 + cj * 128:grow0 + (cj + 1) * 128, col0:col0 + L], reads=[gate], writes=[g_])
                mk.op("dve", lambda e, z_=z_, cj=cj, c4=c4: e.scalar_tensor_tensor(
                    out=z_[:], in0=z_[:], scalar=lb[:, cj:cj + 1], in1=yh[:, c4, :], op0=ALU.mult, op1=ALU.add),
                    reads=[z_, lb, yh], writes=[z_])
                mk.op("pool", lambda e, z_=z_, g_=g_, o_=o_: e.tensor_tensor(out=o_[:], in0=z_[:], in1=g_[:], op=ALU.mult),
                      reads=[z_, g_], writes=[o_])
                mk.dma("act", dst[cj * 128:(cj + 1) * 128, col0:col0 + L], o_[:], reads=[o_], writes=[dst])
        mk.release(m0)

    def stage_hyena(self, slot, XT, HT, I):
        mk = self.mk
        U0, ZT, Z1 = self.dram["hyU0"], self.dram["hyZ"], self.dram["hyZ1"]
        m0 = mk.mark()
        W = self.load_w("hy_win", I["hy_w_in"][slot], DC, 3 * D)
        bi_ = mk.sbuf("hy_bin", [128, 24], F32)
        mk.dma("sp", bi_[:], I["hy_b_in_pc"][:, :], writes=[bi_])
        hb = [mk.sbuf("hy_h%d" % k, [128, DC, 512], BF16) for k in range(2)]
        uo = [mk.sbuf("hy_uo%d" % k, [128, 512], F32) for k in range(3)]
        n = 0
        for bi, (t0, tn) in enumerate(BLOCKS):
            h = hb[bi % 2]
            mk.dma("sp", h[:, :, 0:tn], HT[:, t0:t0 + tn].rearrange("(k p) t -> p k t", p=128), reads=[HT], writes=[h])
            for m in range(24):
                ps = self.ps[n % 4]
                u = uo[n % 3]
                n += 1
                for k in range(DC):
                    mk.op("pe", lambda e, ps=ps, k=k, m=m, h=h: e.matmul(
                        ps[:, 0:tn], W[:, k, m * 128:(m + 1) * 128], h[:, k, 0:tn], start=(k == 0), stop=(k == DC - 1)),
                        reads=[W, h], writes=[ps])
                mk.op("act", lambda e, ps=ps, u=u, m=m: e.activation(out=u[:, 0:tn], in_=ps[:, 0:tn], func=AF.Identity,
                                                                    bias=bi_[:, m:m + 1]), reads=[ps, bi_], writes=[u])
                mk.dma("act", U0[m * 128:(m + 1) * 128, t0:t0 + tn], u[:, 0:tn], reads=[u], writes=[U0])
        mk.release(m0)
        m0 = mk.mark()
        ws = mk.sbuf("hy_ws", [128, 24, 3], F32)
        bs = mk.sbuf("hy_bs", [128, 24], F32)
        mk.dma("sp", ws[:], I["hy_w_short_pc"][:, :, :], writes=[ws])
        mk.dma("sp", bs[:], I["hy_b_short_pc"][:, :], writes=[bs])
        W_ = NT + 4
        ub = [mk.sbuf("hy_ub%d" % k, [128, W_], F32) for k in range(2)]
        vb = [mk.sbuf("hy_vb%d" % k, [128, W_], F32) for k in range(2)]
        for k in range(2):
            mk.op("pool", lambda e, k=k: e.memset(ub[k][:], 0.0), writes=[ub[k]])
        for m in range(24):
            u, v = ub[m % 2], vb[m % 2]
            mk.dma("sp", u[:, 1:1 + CTX], U0[m * 128:(m + 1) * 128, 0:CTX], reads=[U0], writes=[u])
            mk.dma("sp", u[:, 3 + CTX:3 + NT], U0[m * 128:(m + 1) * 128, CTX:NT], reads=[U0], writes=[u])
            n_ = W_ - 2
            mk.op("dve", lambda e, u=u, v=v, m=m: e.tensor_scalar(out=v[:, 1:1 + n_], in0=u[:, 0:n_], scalar1=ws[:, m, 0:1],
                                                                 scalar2=bs[:, m:m + 1], op0=ALU.mult, op1=ALU.add),
                  reads=[u, ws, bs], writes=[v])
            mk.op("dve", lambda e, u=u, v=v, m=m: e.scalar_tensor_tensor(out=v[:, 1:1 + n_], in0=u[:, 1:1 + n_], scalar=ws[:, m, 1:2],
                                                                        in1=v[:, 1:1 + n_], op0=ALU.mult, op1=ALU.add),
                  reads=[u, ws, v], writes=[v])
            mk.op("dve", lambda e, u=u, v=v, m=m: e.scalar_tensor_tensor(out=v[:, 1:1 + n_], in0=u[:, 2:2 + n_], scalar=ws[:, m, 2:3],
                                                                        in1=v[:, 1:1 + n_], op0=ALU.mult, op1=ALU.add),
                  reads=[u, ws, v], writes=[v])
            mk.dma("act", ZT[m * 128:(m + 1) * 128, 0:CTX], v[:, 1:1 + CTX], reads=[v], writes=[ZT])
            mk.dma("act", ZT[m * 128:(m + 1) * 128, CTX:NT], v[:, 3 + CTX:3 + NT], reads=[v], writes=[ZT])
        mk.release(m0)
        seqs = [dict(L=SEQ, N1=128, H=64, tag="lat", col0=CTX)]
        if not self.cfg.get("ctx_direct", True):
            seqs.append(dict(L=CTX, N1=8, H=4, tag="ctx", col0=0))
        for S in seqs:
            HH = [self.dram["hyHH_%s%d" % (S["tag"], o)] for o in range(2)]
            self.hy_filter(S, I, HH)
            self.hy_conv(S, 0, I, ZT, 0, ZT, D, I["hy_lbias_pc"][0, :, :], HH, Z1, False)
            self.hy_conv(S, 1, I, Z1, 0, ZT, 2 * D, I["hy_lbias_pc"][1, :, :], HH, HT, True)
        if self.cfg.get("ctx_direct", True):
            self.hy_ctx_direct(I)
        m0 = mk.mark()
        wo = self.load_w("hy_wout", I["hy_w_out"][slot], DC, D)
        bo = mk.sbuf("hy_bo", [128, DC], F32)
        mk.dma("sp", bo[:], I["hy_b_out_pc"][:, :], writes=[bo])
        self.linear_resid(XT, HT, wo, DC, "g1", bias=bo)
        mk.release(m0)

    Prog.hy_conv = hy_conv
    Prog.stage_hyena = stage_hyena


_hy_methods2()


_PROG = {}


def kernel(**inputs):
    inp = {k: np.asarray(v) for k, v in inputs.items()}
    if "p" not in _PROG:
        _PROG["p"] = build_program()
    p = _PROG["p"]
    shared = None
    in_maps = []
    for b in range(8):
        m = host_inputs(inp, b)
        if shared is None:
            shared = m
        else:
            for k in m:
                if k not in ("x", "ctx", "c_pc"):
                    m[k] = shared[k]
        in_maps.append(m)
    res = run_bass_kernel_spmd(p.nc, in_maps, core_ids=list(range(8)))
    out = np.stack([np.asarray(r["y"], dtype=np.float32) for r in res.results], axis=0)
    return out


BSL = 256
BSH = 8
NBLK = 66
NSLOT = NBLK * BSL
NTILE = NT // 128
IOA = bass.IndirectOffsetOnAxis


def _sparse_methods():
    def indirect(mk, out, out_off, in_, in_off, reads, writes):
        qlane = mk.lanes["pool"]
        dl = mk._next_dma_lane("pool")
        need, _ = mk._deps(dl, reads, writes)
        if dl.count > 0:
            need[dl.name] = max(need.get(dl.name, 0), dl.count)
        for ln, ix in need.items():
            if qlane.seen.get(ln, 0) >= ix:
                continue
            qlane.seen[ln] = ix
            qlane.eng.wait_ge(mk.lanes[ln].sem, ix)
        inst = qlane.eng.indirect_dma_start(out=out, out_offset=out_off, in_=in_, in_offset=in_off)
        dl.count += 16
        inst.then_inc(dl.sem, 16)
        mk._record(dl, reads, writes)
        mk.n_inst += 1

    MK.indirect = indirect

    def alloc_sparse(self):
        mk = self.mk
        R = {}
        R["M1a"] = mk.sbuf("sp_M1a", [128, NTILE, 32], F32)
        R["M2a"] = mk.sbuf("sp_M2a", [128, NTILE, 32], F32)
        R["A12"] = mk.sbuf("sp_A12", [128, NTILE, 2], F32)
        R["POSi"] = mk.sbuf("sp_POSi", [128, NTILE, 2], I32)
        R["IDXW"] = mk.sbuf("sp_IDXW", [128, NBLK], I32)
        R["U"] = mk.sbuf("sp_U", [128, 128], BF16)
        R["jg"] = mk.sbuf("sp_jg", [128, NBLK], F32)
        R["pidx"] = mk.sbuf("sp_pidx", [128, 1], F32)
        R["ones32"] = mk.sbuf("sp_ones32", [128, 32], F32)
        mk.op("pool", lambda e: e.memset(R["U"][:], 1.0), writes=[R["U"]])
        mk.op("pool", lambda e: e.affine_select(out=R["U"][:], in_=R["U"][:], pattern=[[1, 128]], compare_op=ALU.is_gt,
                                                fill=0.0, base=0, channel_multiplier=-1), reads=[R["U"]], writes=[R["U"]])
        mk.op("pool", lambda e: e.memset(R["ones32"][:], 1.0), writes=[R["ones32"]])
        return R

    def route_tile_sparse(self, hf, a, tok0, R, rt):
        mk = self.mk
        ti = tok0 // 128
        ps = self.ps[1 + (ti % 3)]
        Wr, br = R["Wr"], R["br"]
        for k in range(DC):
            mk.op("pe", lambda e, k=k: e.matmul(ps[:, 0:36], hf[:, k, a * 128:(a + 1) * 128], Wr[:, k, :],
                                                 start=(k == 0), stop=(k == DC - 1)), reads=[hf, Wr], writes=[ps])
        lg, gmax, ngmax, ge, gsum, gp, mg, es = (rt[k] for k in ("lg", "gmax", "ngmax", "ge", "gsum", "gp", "mg", "es"))
        t8, dd, w2, m1, m2 = (rt[k] for k in ("t8", "dd", "w2", "m1", "m2"))
        M1a, M2a, A12 = R["M1a"], R["M2a"], R["A12"]
        V = lambda fn, reads, writes: mk.op("dve", fn, reads=reads, writes=writes)
        V(lambda e: e.tensor_tensor(out=lg[:], in0=ps[:, 0:36], in1=br[:], op=ALU.add), [ps, br], [lg])
        V(lambda e: e.reduce_max(out=gmax[:], in_=lg[:, 0:4], axis=AX.X), [lg], [gmax])
        V(lambda e: e.tensor_scalar(out=ngmax[:], in0=gmax[:], scalar1=-1.0, scalar2=None, op0=ALU.mult), [gmax], [ngmax])
        mk.op("act", lambda e: e.activation(out=ge[:], in_=lg[:, 0:4], func=AF.Exp, bias=ngmax[:, 0:1],
                                            accum_out=gsum[:]), reads=[lg, ngmax], writes=[ge, gsum])
        V(lambda e: e.reciprocal(gp[:], gsum[:]), [gsum], [gp])
        V(lambda e: e.tensor_scalar(out=mg[:], in0=lg[:, 0:4], scalar1=gmax[:, 0:1], scalar2=None, op0=ALU.is_equal),
          [lg, gmax], [mg])
        V(lambda e: e.tensor_scalar(out=es[:], in0=lg[:, 4:12], scalar1=mg[:, 0:1], scalar2=None, op0=ALU.mult),
          [lg, mg], [es])
        for g in range(1, 4):
            V(lambda e, g=g: e.scalar_tensor_tensor(out=es[:], in0=lg[:, 4 + 8 * g:12 + 8 * g], scalar=mg[:, g:g + 1],
                                                    in1=es[:], op0=ALU.mult, op1=ALU.add), [lg, mg, es], [es])
        V(lambda e: e.max(out=t8[:], in_=es[:]), [es], [t8])
        V(lambda e: e.tensor_tensor(out=dd[:], in0=t8[:, 1:2], in1=t8[:, 0:1], op=ALU.subtract), [t8], [dd])
        mk.op("act", lambda e: e.activation(out=w2[:], in_=dd[:], func=AF.Sigmoid), reads=[dd], writes=[w2])
        V(lambda e: e.tensor_tensor(out=A12[:, ti, 1:2], in0=w2[:], in1=gp[:], op=ALU.mult), [w2, gp], [A12])
        V(lambda e: e.tensor_tensor(out=A12[:, ti, 0:1], in0=gp[:], in1=A12[:, ti, 1:2], op=ALU.subtract), [gp, A12], [A12])
        V(lambda e: e.tensor_scalar(out=m1[:], in0=es[:], scalar1=t8[:, 0:1], scalar2=None, op0=ALU.is_equal), [es, t8], [m1])
        V(lambda e: e.tensor_scalar(out=m2[:], in0=es[:], scalar1=t8[:, 1:2], scalar2=None, op0=ALU.is_equal), [es, t8], [m2])
        for g in range(4):
            V(lambda e, g=g: e.tensor_scalar(out=M1a[:, ti, 8 * g:8 * g + 8], in0=m1[:], scalar1=mg[:, g:g + 1],
                                             scalar2=None, op0=ALU.mult), [m1, mg], [M1a])
            V(lambda e, g=g: e.tensor_scalar(out=M2a[:, ti, 8 * g:8 * g + 8], in0=m2[:], scalar1=mg[:, g:g + 1],
                                             scalar2=None, op0=ALU.mult), [m2, mg], [M2a])

    def stage_moe_sparse(self, i, XT, R, I):
        mk = self.mk
        HTOK, HS, YS = self.dram["HTOK"], self.dram["HS"], self.dram["YS"]
        M1a, M2a, A12, POSi, IDXW = R["M1a"], R["M2a"], R["A12"], R["POSi"], R["IDXW"]
        m0 = mk.mark()
        Mb = mk.sbuf("sq_Mb", [128, NTILE, 32], BF16)
        P = mk.sbuf("sq_P", [128, NTILE, 32], F32)
        prod = mk.sbuf("sq_prod", [128, NTILE, 32], F32)
        posf = mk.sbuf("sq_posf", [128, NTILE, 2], F32)
        nf = mk.sbuf("sq_nf", [128, 32], F32)
        ni = mk.sbuf("sq_ni", [128, 32], I32)
        pn = mk.sbuf("sq_pn", [128, 32], F32)
        incl = mk.sbuf("sq_incl", [128, 32], F32)
        excl = mk.sbuf("sq_excl", [128, 32], F32)
        EB = mk.sbuf("sq_EB", [128, NBLK], F32)
        mk.op("dve", lambda e: e.tensor_tensor(out=Mb[:], in0=M1a[:], in1=M2a[:], op=ALU.add), reads=[M1a, M2a], writes=[Mb])
        for ti in range(NTILE):
            pb = self.ps[ti // 12]
            c0 = (ti % 12) * 32
            for tj in range(ti):
                mk.op("pe", lambda e, pb=pb, c0=c0, tj=tj: e.matmul(pb[:, c0:c0 + 32], self.ones_b[:], Mb[:, tj, :],
                                                                    start=(tj == 0), stop=False), reads=[self.ones_b, Mb], writes=[pb])
            mk.op("pe", lambda e, pb=pb, c0=c0, ti=ti: e.matmul(pb[:, c0:c0 + 32], R["U"][:], Mb[:, ti, :],
                                                                start=(ti == 0), stop=True), reads=[R["U"], Mb], writes=[pb])
        pt = self.ps[3]
        for tj in range(NTILE):
            mk.op("pe", lambda e, tj=tj: e.matmul(pt[:, 0:32], self.ones_b[:], Mb[:, tj, :], start=(tj == 0),
                                                  stop=(tj == NTILE - 1)), reads=[self.ones_b, Mb], writes=[pt])
        V = lambda fn, reads, writes: mk.op("dve", fn, reads=reads, writes=writes)
        V(lambda e: e.tensor_scalar(out=nf[:], in0=pt[:, 0:32], scalar1=float(BSL - 1), scalar2=None, op0=ALU.add), [pt], [nf])
        V(lambda e: e.tensor_copy(ni[:], nf[:]), [nf], [ni])
        V(lambda e: e.tensor_single_scalar(out=ni[:], in_=ni[:], scalar=BSH, op=ALU.arith_shift_right), [ni], [ni])
        V(lambda e: e.tensor_single_scalar(out=ni[:], in_=ni[:], scalar=BSH, op=ALU.logical_shift_left), [ni], [ni])
        V(lambda e: e.tensor_copy(pn[:], ni[:]), [ni], [pn])
        V(lambda e: e.tensor_tensor_scan(out=incl[:], data0=R["ones32"][:], data1=pn[:], initial=0.0, op0=ALU.mult, op1=ALU.add),
          [R["ones32"], pn], [incl])
        V(lambda e: e.tensor_tensor(out=excl[:], in0=incl[:], in1=pn[:], op=ALU.subtract), [incl, pn], [excl])
        for ti in range(NTILE):
            pb = self.ps[ti // 12]
            c0 = (ti % 12) * 32
            V(lambda e, pb=pb, c0=c0, ti=ti: e.tensor_tensor(out=P[:, ti, :], in0=pb[:, c0:c0 + 32], in1=excl[:], op=ALU.add),
              [pb, excl], [P])
        for k, Mk in enumerate((M1a, M2a)):
            V(lambda e, Mk=Mk: e.tensor_tensor(out=prod[:], in0=Mk[:], in1=P[:], op=ALU.mult), [Mk, P], [prod])
            V(lambda e, k=k: e.reduce_sum(out=posf[:, :, k], in_=prod[:], axis=AX.X), [prod], [posf])
        V(lambda e: e.tensor_copy(POSi[:], posf[:]), [posf], [POSi])
        V(lambda e: e.memset(EB[:], 0.0), [], [EB])
        for ex in range(NE):
            V(lambda e, ex=ex: e.scalar_tensor_tensor(out=EB[:], in0=R["jg"][:], scalar=incl[:, ex:ex + 1], in1=EB[:],
                                                      op0=ALU.is_ge, op1=ALU.add), [R["jg"], incl, EB], [EB])
        V(lambda e: e.tensor_scalar(out=EB[:], in0=EB[:], scalar1=float(NE - 1), scalar2=128.0, op0=ALU.min, op1=ALU.mult), [EB], [EB])
        V(lambda e: e.tensor_scalar(out=EB[:], in0=EB[:], scalar1=R["pidx"][:, 0:1], scalar2=None, op0=ALU.add), [EB, R["pidx"]], [EB])
        V(lambda e: e.tensor_copy(IDXW[:], EB[:]), [EB], [IDXW])
        if self.cfg.get("dbg_pos"):
            d1 = T(self.nc.dram_tensor("dbg_pos", [128, NTILE, 2], I32, kind="ExternalOutput"), "dbg_pos")
            d2 = T(self.nc.dram_tensor("dbg_idx", [128, NBLK], I32, kind="ExternalOutput"), "dbg_idx")
            d3 = T(self.nc.dram_tensor("dbg_incl", [128, 32], F32, kind="ExternalOutput"), "dbg_incl")
            d4 = T(self.nc.dram_tensor("dbg_P", [128, NTILE, 32], F32, kind="ExternalOutput"), "dbg_P")
            d5 = T(self.nc.dram_tensor("dbg_M1", [128, NTILE, 32], F32, kind="ExternalOutput"), "dbg_M1")
            d6 = T(self.nc.dram_tensor("dbg_A12", [128, NTILE, 2], F32, kind="ExternalOutput"), "dbg_A12")
            mk.dma("sp", d1[:, :, :], POSi[:], reads=[POSi])
            mk.dma("sp", d2[:, :], IDXW[:], reads=[IDXW])
            mk.dma("sp", d3[:, :], incl[:], reads=[incl])
            mk.dma("sp", d4[:, :, :], P[:], reads=[P])
            mk.dma("sp", d5[:, :, :], M1a[:], reads=[M1a])
            mk.dma("sp", d6[:, :, :], A12[:], reads=[A12])
            mk.release(m0)
            return
        mk.release(m0)
        m0 = mk.mark()
        ht = [mk.sbuf("sq_ht%d" % k, [128, D], BF16) for k in range(3)]
        for ti in range(NTILE):
            h = ht[ti % 3]
            mk.dma("sp", h[:], HTOK[ti * 128:(ti + 1) * 128, :], reads=[HTOK], writes=[h])
            for k in range(2):
                mk.indirect(HS[:, :], IOA(ap=POSi[:, ti, k:k + 1], axis=0), h[:], None, [h, POSi], [HS])
        mk.release(m0)
        m0 = mk.mark()
        wgu = [mk.sbuf("sq_wgu%d" % k, [128, DC, 512], BF16) for k in range(2)]
        wdn = [mk.sbuf("sq_wdn%d" % k, [128, 2, D], BF16) for k in range(2)]
        NA = BSL // 128
        hs = [mk.sbuf("sq_hs%d" % k, [128, NA, D], BF16) for k in range(2)]
        hsT = [mk.sbuf("sq_hsT%d" % k, [128, DC, BSL], BF16) for k in range(2)]
        sl = [mk.sbuf("sq_sl%d" % k, [128, BSL], F32) for k in range(2)]
        hid = [mk.sbuf("sq_hid%d" % k, [128, 2, BSL], BF16) for k in range(2)]
        yo = [mk.sbuf("sq_yo%d" % k, [128, D], BF16) for k in range(3)]
        WGU, WDN = I["moe_wgu%d" % i], I["moe_wdn%d" % i]
        ny = 0
        for j in range(NBLK):
            w, wd_, h, hT_, hd = wgu[j % 2], wdn[j % 2], hs[j % 2], hsT[j % 2], hid[j % 2]
            mk.indirect(w[:].rearrange("p a b -> p (a b)"), None, WGU[:, :], IOA(ap=IDXW[:, j:j + 1], axis=0), [IDXW], [w])
            mk.indirect(wd_[:].rearrange("p a b -> p (a b)"), None, WDN[:, :], IOA(ap=IDXW[:, j:j + 1], axis=0), [IDXW], [wd_])
            mk.dma("sp", h[:], HS[j * BSL:(j + 1) * BSL, :].rearrange("(a p) d -> p a d", p=128), reads=[HS], writes=[h])
            KPB = 1024 // BSL
            for hh in range(DC // KPB):
                pb = self.ps[hh % 2]
                pbb = pb[:, 0:512].bitcast(BF16)
                for kk in range(KPB):
                    k = hh * KPB + kk
                    for a in range(NA):
                        mk.op("pe", lambda e, pbb=pbb, kk=kk, k=k, a=a, h=h: e.transpose(
                            pbb[:, kk * BSL + a * 128:kk * BSL + (a + 1) * 128], h[:, a, k * 128:(k + 1) * 128], self.ident_b[:]),
                            reads=[h, self.ident_b], writes=[pb])
                dstv = hT_[:, hh * KPB:(hh + 1) * KPB, :].rearrange("p a b -> p (a b)")
                if hh % 2:
                    mk.op("act", lambda e, pbb=pbb, dstv=dstv: e.copy(dstv, pbb[:, :]), reads=[pb], writes=[hT_])
                else:
                    mk.op("dve", lambda e, pbb=pbb, dstv=dstv: e.tensor_copy(dstv, pbb[:, :]), reads=[pb], writes=[hT_])
            for fc in range(2):
                pg, pu = self.ps[2 + 2 * fc], self.ps[3 + 2 * fc]
                for k in range(DC):
                    mk.op("pe", lambda e, pg=pg, k=k, fc=fc, w=w, hT_=hT_: e.matmul(
                        pg[:, 0:BSL], w[:, k, fc * 128:(fc + 1) * 128], hT_[:, k, :], start=(k == 0), stop=(k == DC - 1)),
                        reads=[w, hT_], writes=[pg])
                for k in range(DC):
                    mk.op("pe", lambda e, pu=pu, k=k, fc=fc, w=w, hT_=hT_: e.matmul(
                        pu[:, 0:BSL], w[:, k, 256 + fc * 128:256 + (fc + 1) * 128], hT_[:, k, :], start=(k == 0), stop=(k == DC - 1)),
                        reads=[w, hT_], writes=[pu])
                s_ = sl[fc]
                mk.op("act", lambda e, pg=pg, s_=s_: e.activation(out=s_[:], in_=pg[:, 0:BSL], func=AF.Silu), reads=[pg], writes=[s_])
                mk.op("dve", lambda e, pu=pu, s_=s_, hd=hd, fc=fc: e.tensor_tensor(out=hd[:, fc, :], in0=pu[:, 0:BSL], in1=s_[:],
                                                                                  op=ALU.mult), reads=[pu, s_], writes=[hd])
            for a in range(NA):
                y_ = yo[ny % 3]
                ny += 1
                for dh in range(2):
                    pd = self.ps[6 + dh]
                    for fc in range(2):
                        mk.op("pe", lambda e, pd=pd, fc=fc, a=a, dh=dh, hd=hd, wd_=wd_: e.matmul(
                            pd[:, :], hd[:, fc, a * 128:(a + 1) * 128], wd_[:, fc, dh * 512:(dh + 1) * 512], start=(fc == 0), stop=(fc == 1)),
                            reads=[hd, wd_], writes=[pd])
                    if dh:
                        mk.op("act", lambda e, pd=pd, y_=y_, dh=dh: e.copy(y_[:, dh * 512:(dh + 1) * 512], pd[:, :]), reads=[pd], writes=[y_])
                    else:
                        mk.op("dve", lambda e, pd=pd, y_=y_, dh=dh: e.tensor_copy(y_[:, dh * 512:(dh + 1) * 512], pd[:, :]), reads=[pd], writes=[y_])
                mk.dma("act", YS[j * BSL + a * 128:j * BSL + (a + 1) * 128, :], y_[:], reads=[y_], writes=[YS])
        mk.release(m0)
        m0 = mk.mark()
        r1 = [mk.sbuf("sq_r1%d" % k, [128, D], BF16) for k in range(2)]
        r2 = [mk.sbuf("sq_r2%d" % k, [128, D], BF16) for k in range(2)]
        rc = [mk.sbuf("sq_rc%d" % k, [128, D], F32) for k in range(2)]
        xr = [mk.sbuf("sq_x%d" % k, [128, 512], F32) for k in range(3)]
        n = 0
        for bi, (t0, tn) in enumerate(BLOCKS):
            r = 1 if bi == 0 else 0
            g2 = self.mv[("g2", r)]
            for a in range(tn // 128):
                ti = t0 // 128 + a
                a_, b_ = r1[ti % 2], r2[ti % 2]
                mk.indirect(a_[:], None, YS[:, :], IOA(ap=POSi[:, ti, 0:1], axis=0), [YS, POSi], [a_])
                mk.indirect(b_[:], None, YS[:, :], IOA(ap=POSi[:, ti, 1:2], axis=0), [YS, POSi], [b_])
                c_ = rc[ti % 2]
                mk.op("dve", lambda e, a_=a_, c_=c_, ti=ti: e.tensor_scalar(out=c_[:], in0=a_[:], scalar1=A12[:, ti, 0:1], scalar2=None,
                                                                           op0=ALU.mult), reads=[a_, A12], writes=[c_])
                mk.op("dve", lambda e, c_=c_, b_=b_, ti=ti: e.scalar_tensor_tensor(out=c_[:], in0=b_[:], scalar=A12[:, ti, 1:2], in1=c_[:],
                                                                                  op0=ALU.mult, op1=ALU.add), reads=[c_, b_, A12], writes=[c_])
                for dc in range(DC):
                    mk.op("pe", lambda e, dc=dc, a=a, c_=c_: e.transpose(self.ps[dc][:, a * 128:(a + 1) * 128],
                                                                         c_[:, dc * 128:(dc + 1) * 128], self.ident_f[:]),
                          reads=[c_, self.ident_f], writes=[self.ps[dc]])
            for dc in range(DC):
                x = xr[n % 3]
                n += 1
                mk.dma("sp", x[:, 0:tn], XT[dc * 128:(dc + 1) * 128, t0:t0 + tn], reads=[self.XTr[dc]], writes=[x])
                mk.op("dve", lambda e, dc=dc, x=x, g2=g2: e.scalar_tensor_tensor(
                    out=x[:, 0:tn], in0=self.ps[dc][:, 0:tn], scalar=g2[:, dc:dc + 1], in1=x[:, 0:tn],
                    op0=ALU.mult, op1=ALU.add), reads=[self.ps[dc], g2, x], writes=[x])
                mk.dma("act", XT[dc * 128:(dc + 1) * 128, t0:t0 + tn], x[:, 0:tn], reads=[x], writes=[self.XTr[dc]])
        mk.release(m0)

    Prog.alloc_sparse = alloc_sparse
    Prog._route_tile_sparse = route_tile_sparse
    Prog.stage_moe_sparse = stage_moe_sparse


_sparse_methods()


def hyd_consts():
    bf = ml_dtypes.bfloat16
    L, N = CTX, 2 * CTX
    c = {}
    t = (np.arange(2)[None, :, None] * 128 + np.arange(128)[:, None, None]).astype(np.float64)
    k = np.arange(N)[None, None, :].astype(np.float64)
    ang = 2 * np.pi * t * k / N
    c["F"] = np.stack([np.cos(ang), -np.sin(ang), np.sin(ang)], axis=2).astype(bf)
    kk = (np.arange(4)[None, :, None] * 128 + np.arange(128)[:, None, None]).astype(np.float64)
    tt = np.arange(L)[None, None, :].astype(np.float64)
    a2 = 2 * np.pi * kk * tt / N
    c["Fi"] = np.stack([np.cos(a2) / N, -np.sin(a2) / N], axis=2).astype(bf)
    pos = (np.arange(2)[None, :] * 128 + np.arange(128)[:, None]).astype(np.float32)
    c["lneg"] = (-(pos / np.float32(L - 1))).astype(np.float32)
    return c


def _hyd_methods():
    def hy_ctx_direct(self, I):
        mk = self.mk
        L = CTX
        ZT, Z1, HT = self.dram["hyZ"], self.dram["hyZ1"], self.dram["HT"]
        m0 = mk.mark()
        Fd = mk.sbuf("hd_F", [128, 2, 3, 512], BF16)
        Fi = mk.sbuf("hd_Fi", [128, 4, 2, L], BF16)
        lneg = mk.sbuf("hd_lneg", [128, 2], F32)
        dab = mk.sbuf("hd_dab", [128, D], F32)
        ft = mk.sbuf("hd_ft", [17, L], F32)
        W1 = mk.sbuf("hd_w1", [17, 64], F32)
        W2 = mk.sbuf("hd_w2", [64, 64], F32)
        W3 = mk.sbuf("hd_w3", [64, 4096], BF16)
        fv = mk.sbuf("hd_fv", [64, 4], F32)
        fb = mk.sbuf("hd_fb", [64, 2], F32)
        z1 = mk.sbuf("hd_z1", [64, L], F32)
        z2 = mk.sbuf("hd_z2", [64, L], F32)
        z2b = mk.sbuf("hd_z2b", [64, L], BF16)
        tmp = mk.sbuf("hd_tmp", [64, L], F32)
        mk.dma("sp", Fd[:], I["hyd_F"][:, :, :, :], writes=[Fd])
        mk.dma("sp", Fi[:], I["hyd_Fi"][:, :, :, :], writes=[Fi])
        mk.dma("sp", lneg[:], I["hyd_lneg"][:, :], writes=[lneg])
        mk.dma("sp", dab[0:64, :], I["hy_dabs"][:, :], writes=[dab])
        mk.dma("sp", dab[64:128, :], I["hy_dabs"][:, :], writes=[dab])
        mk.dma("sp", ft[:], I["hy_featsT_ctx"][:, :], writes=[ft])
        mk.dma("sp", W1[:], I["hy_f_w1"][0, :, :], writes=[W1])
        mk.dma("sp", W2[:], I["hy_f_w2"][0, :, :], writes=[W2])
        mk.dma("pool", W3[:], I["hy_f_w3"][0, :, :], writes=[W3])
        mk.dma("sp", fv[:], I["hy_fvec"][:, :], writes=[fv])
        mk.op("dve", lambda e: e.tensor_tensor(out=fb[:, 0:1], in0=fv[:, 0:1], in1=fv[:, 1:2], op=ALU.mult), reads=[fv], writes=[fb])
        mk.op("dve", lambda e: e.tensor_tensor(out=fb[:, 1:2], in0=fv[:, 2:3], in1=fv[:, 3:4], op=ALU.mult), reads=[fv], writes=[fb])
        for layer, (Wm, src, dst, kin) in enumerate(((W1, ft, z1, 17), (W2, z1, z2, 64))):
            ps = self.ps[layer]
            mk.op("pe", lambda e, ps=ps, Wm=Wm, src=src, kin=kin: e.matmul(ps[0:64, 0:L], Wm[0:kin, :], src[0:kin, :],
                                                                          start=True, stop=True), reads=[Wm, src], writes=[ps])
            mk.op("dve", lambda e, ps=ps, dst=dst, layer=layer: e.tensor_scalar(
                out=dst[:], in0=ps[0:64, 0:L], scalar1=fv[:, 2 * layer + 1:2 * layer + 2], scalar2=fb[:, layer:layer + 1],
                op0=ALU.mult, op1=ALU.add), reads=[ps, fv, fb], writes=[dst])
            for _ in range(2):
                mk.op("dve", lambda e, dst=dst: e.tensor_scalar(out=tmp[:], in0=dst[:], scalar1=math.pi, scalar2=2 * math.pi,
                                                                op0=ALU.is_gt, op1=ALU.mult), reads=[dst], writes=[tmp])
                mk.op("dve", lambda e, dst=dst: e.tensor_tensor(out=dst[:], in0=dst[:], in1=tmp[:], op=ALU.subtract),
                      reads=[dst, tmp], writes=[dst])
                mk.op("dve", lambda e, dst=dst: e.tensor_scalar(out=tmp[:], in0=dst[:], scalar1=-math.pi, scalar2=2 * math.pi,
                                                                op0=ALU.is_lt, op1=ALU.mult), reads=[dst], writes=[tmp])
                mk.op("dve", lambda e, dst=dst: e.tensor_tensor(out=dst[:], in0=dst[:], in1=tmp[:], op=ALU.add),
                      reads=[dst, tmp], writes=[dst])
            mk.op("act", lambda e, dst=dst: e.activation(out=dst[:], in_=dst[:], func=AF.Sin), reads=[dst], writes=[dst])
        mk.op("act", lambda e: e.copy(z2b[:], z2[:]), reads=[z2], writes=[z2b])
        Hf = mk.sbuf("hd_Hf", [128, 2, 2, 4, D], BF16)
        dec = [mk.sbuf("hd_dec%d" % k, [128, D], F32) for k in range(2)]
        hd = [mk.sbuf("hd_hd%d" % k, [128, D], F32) for k in range(2)]
        hdb = [mk.sbuf("hd_hdb%d" % k, [128, D], BF16) for k in range(4)]
        ha = [mk.sbuf("hd_ha%d" % k, [128, D], BF16) for k in range(2)]
        rn = mk.sbuf("hd_rn", [128, D], F32)
        for o in range(2):
            pn = (self.ps[6], self.ps[7])
            n = 0
            for pt in range(2):
                for dr in range(2):
                    i2 = n % 2
                    for hf in range(2):
                        ph = self.ps[i2 * 2 + hf]
                        w0 = (dr * 2 + o) * D + hf * 512
                        mk.op("pe", lambda e, ph=ph, pt=pt, w0=w0: e.matmul(ph[:, :], z2b[:, pt * 128:(pt + 1) * 128], W3[:, w0:w0 + 512],
                                                                            start=True, stop=True), reads=[z2b, W3], writes=[ph])
                    mk.op("act", lambda e, i2=i2, pt=pt: e.activation(out=dec[i2][:], in_=dab[:], func=AF.Exp, scale=lneg[:, pt:pt + 1]),
                          reads=[dab, lneg], writes=[dec[i2]])
                    for hf in range(2):
                        ph = self.ps[i2 * 2 + hf]
                        sl_ = slice(hf * 512, (hf + 1) * 512)
                        mk.op("dve", lambda e, ph=ph, i2=i2, sl_=sl_: e.tensor_tensor(out=hd[i2][:, sl_], in0=ph[:, :], in1=dec[i2][:, sl_],
                                                                                     op=ALU.mult), reads=[ph, dec[i2]], writes=[hd[i2]])
                    if dr == 1 and pt == 0:
                        mk.op("dve", lambda e, i2=i2: e.memset(hd[i2][0:1, :], 0.0), reads=[hd[i2]], writes=[hd[i2]])
                    hb_ = hdb[pt * 2 + dr]
                    mk.op("act", lambda e, i2=i2, hb_=hb_: e.copy(hb_[:], hd[i2][:]), reads=[hd[i2]], writes=[hb_])
                    mk.op("dve", lambda e, i2=i2: e.scalar_tensor_tensor(out=ha[i2][:], in0=hd[i2][:], scalar=-1.0, in1=hd[i2][:],
                                                                        op0=ALU.mult, op1=ALU.max), reads=[hd[i2]], writes=[ha[i2]])
                    for hf in range(2):
                        mk.op("pe", lambda e, i2=i2, hf=hf, n=n: e.matmul(pn[hf][:, :], self.ones_b[:], ha[i2][:, hf * 512:(hf + 1) * 512],
                                                                          start=(n == 0), stop=(n == 3)), reads=[self.ones_b, ha[i2]], writes=[pn[hf]])
                    n += 1
            for hf in range(2):
                mk.op("dve", lambda e, hf=hf: e.reciprocal(rn[:, hf * 512:(hf + 1) * 512], pn[hf][:, :]), reads=[pn[hf]], writes=[rn])
            q = 0
            for kc in range(4):
                for ri in range(2):
                    for hf in range(2):
                        pa = self.ps[4 + q % 2]
                        q += 1
                        sl_ = slice(hf * 512, (hf + 1) * 512)
                        steps = [(pt, dr) for pt in range(2) for dr in range(2)]
                        for si, (pt, dr) in enumerate(steps):
                            kind = ri if dr == 0 else (0 if ri == 0 else 2)
                            mk.op("pe", lambda e, pa=pa, pt=pt, dr=dr, kind=kind, kc=kc, sl_=sl_, si=si: e.matmul(
                                pa[:, :], Fd[:, pt, kind, kc * 128:(kc + 1) * 128], hdb[pt * 2 + dr][:, sl_],
                                start=(si == 0), stop=(si == 3)), reads=[Fd, hdb[pt * 2 + dr]], writes=[pa])
                        mk.op("dve", lambda e, pa=pa, o=o, ri=ri, kc=kc, sl_=sl_: e.tensor_tensor(
                            out=Hf[:, o, ri, kc, sl_], in0=pa[:, :], in1=rn[:, sl_], op=ALU.mult), reads=[pa, rn], writes=[Hf])
        zf = mk.sbuf("hd_zf", [128, DC, L], F32)
        gt = mk.sbuf("hd_gt", [128, DC, L], F32)
        zb = mk.sbuf("hd_zb", [128, DC, L], BF16)
        zT = mk.sbuf("hd_zT", [128, 2, D], BF16)
        Y = mk.sbuf("hd_Y", [128, 2, 4, D], BF16)
        ta = [mk.sbuf("hd_ta%d" % k, [128, 512], F32) for k in range(2)]
        tb = [mk.sbuf("hd_tb%d" % k, [128, 512], F32) for k in range(2)]
        lb = mk.sbuf("hd_lb", [128, 2, DC], F32)
        rr = [mk.sbuf("hd_rr%d" % k, [128, L], F32) for k in range(2)]
        obf = mk.sbuf("hd_obf", [128, DC, L], F32)
        obb = mk.sbuf("hd_obb", [128, DC, L], BF16)
        for o in range(2):
            mk.dma("sp", lb[:, o, :], I["hy_lbias_pc"][o, :, :], writes=[lb])
        for o in range(2):
            zsrc = ZT if o == 0 else Z1
            mk.dma("sp", zf[:], zsrc[0:D, 0:L].rearrange("(j p) t -> p j t", p=128), reads=[zsrc], writes=[zf])
            g0 = (1 + o) * D
            mk.dma("sp", gt[:], ZT[g0:g0 + D, 0:L].rearrange("(j p) t -> p j t", p=128), reads=[ZT], writes=[gt])
            mk.op("act", lambda e: e.copy(zb[:], zf[:]), reads=[zf], writes=[zb])
            for pt in range(2):
                pb = self.ps[pt]
                pbb = pb[:, 0:512].bitcast(BF16)
                for j in range(DC):
                    mk.op("pe", lambda e, pbb=pbb, j=j, pt=pt: e.transpose(pbb[:, j * 128:(j + 1) * 128], zb[:, j, pt * 128:(pt + 1) * 128],
                                                                          self.ident_b[:]), reads=[zb, self.ident_b], writes=[pb])
                mk.op("act", lambda e, pbb=pbb, pt=pt: e.copy(zT[:, pt, :], pbb[:, :]), reads=[pb], writes=[zT])
            for kc in range(4):
                for ri in range(2):
                    for hf in range(2):
                        px = self.ps[2 + ri * 2 + hf]
                        for pt in range(2):
                            mk.op("pe", lambda e, px=px, pt=pt, ri=ri, kc=kc, hf=hf: e.matmul(
                                px[:, :], Fd[:, pt, ri, kc * 128:(kc + 1) * 128], zT[:, pt, hf * 512:(hf + 1) * 512],
                                start=(pt == 0), stop=(pt == 1)), reads=[Fd, zT], writes=[px])
                for hf in range(2):
                    sl_ = slice(hf * 512, (hf + 1) * 512)
                    pxr, pxi = self.ps[2 + hf], self.ps[4 + hf]
                    a_, b_ = ta[hf], tb[hf]
                    mk.op("dve", lambda e, pxr=pxr, a_=a_, kc=kc, sl_=sl_, o=o: e.tensor_tensor(out=a_[:], in0=pxr[:, :], in1=Hf[:, o, 0, kc, sl_],
                                                                                             op=ALU.mult), reads=[pxr, Hf], writes=[a_])
                    mk.op("dve", lambda e, pxi=pxi, b_=b_, kc=kc, sl_=sl_, o=o: e.tensor_tensor(out=b_[:], in0=pxi[:, :], in1=Hf[:, o, 1, kc, sl_],
                                                                                             op=ALU.mult), reads=[pxi, Hf], writes=[b_])
                    mk.op("pool", lambda e, a_=a_, b_=b_, kc=kc, sl_=sl_: e.tensor_tensor(out=Y[:, 0, kc, sl_], in0=a_[:], in1=b_[:],
                                                                                         op=ALU.subtract), reads=[a_, b_], writes=[Y])
                    mk.op("dve", lambda e, pxr=pxr, a_=a_, kc=kc, sl_=sl_, o=o: e.tensor_tensor(out=a_[:], in0=pxr[:, :], in1=Hf[:, o, 1, kc, sl_],
                                                                                             op=ALU.mult), reads=[pxr, Hf], writes=[a_])
                    mk.op("dve", lambda e, pxi=pxi, b_=b_, kc=kc, sl_=sl_, o=o: e.tensor_tensor(out=b_[:], in0=pxi[:, :], in1=Hf[:, o, 0, kc, sl_],
                                                                                             op=ALU.mult), reads=[pxi, Hf], writes=[b_])
                    mk.op("pool", lambda e, a_=a_, b_=b_, kc=kc, sl_=sl_: e.tensor_tensor(out=Y[:, 1, kc, sl_], in0=a_[:], in1=b_[:],
                                                                                         op=ALU.add), reads=[a_, b_], writes=[Y])
            ob = obf if o == 0 else obb
            for cj in range(DC):
                py = self.ps[6 + cj % 2]
                for kc in range(4):
                    for ri in range(2):
                        mk.op("pe", lambda e, py=py, kc=kc, ri=ri, cj=cj: e.matmul(
                            py[:, 0:L], Y[:, ri, kc, cj * 128:(cj + 1) * 128], Fi[:, kc, ri, :],
                            start=(kc == 0 and ri == 0), stop=(kc == 3 and ri == 1)), reads=[Y, Fi], writes=[py])
                r_ = rr[cj % 2]
                mk.op("dve", lambda e, py=py, r_=r_, cj=cj, o=o: e.scalar_tensor_tensor(out=r_[:], in0=zf[:, cj, :], scalar=lb[:, o, cj:cj + 1],
                                                                                     in1=py[:, 0:L], op0=ALU.mult, op1=ALU.add),
                      reads=[zf, lb, py], writes=[r_])
                mk.op("pool", lambda e, r_=r_, cj=cj, ob=ob: e.tensor_tensor(out=ob[:, cj, :], in0=r_[:], in1=gt[:, cj, :], op=ALU.mult),
                      reads=[r_, gt], writes=[ob])
            dst = Z1 if o == 0 else HT
            mk.dma("act", dst[0:D, 0:L].rearrange("(j p) t -> p j t", p=128), ob[:], reads=[ob], writes=[dst])
        mk.release(m0)

    Prog.hy_ctx_direct = hy_ctx_direct


_hyd_methods()
```
